# Optimizing a Trainium2 kernel written in Bass

```python
import math
import jax, jax.numpy as jnp
from jax import lax
import numpy as np

D_MODEL = 1024
BATCH = 8
SEQ = 2048
DEPTH = 2

CTX_LEN = 256
GRID_W = 64
F32 = jnp.float32
NORM_EPS = 1e-6
N_MOD = 6
N_EVEN = (DEPTH + 1) // 2
N_ODD = DEPTH // 2

A_HEAD_DIM = 64
A_HEADS = D_MODEL // (2 * A_HEAD_DIM)
A_WIDTH = A_HEADS * A_HEAD_DIM
A_DECAY_LORA = 64
A_ICLR_LORA = 64
A_GATE_LORA = 128
A_LN_EPS = 64e-5
A_SIZES = (A_WIDTH, A_WIDTH, A_WIDTH, 2 * A_DECAY_LORA, 2 * A_ICLR_LORA, A_GATE_LORA)
A_COLS = sum(A_SIZES)

B_HEAD_DIM = 128
B_HEADS = D_MODEL // (2 * B_HEAD_DIM)
B_WIDTH = B_HEADS * B_HEAD_DIM
B_CONV = 5
B_CHUNK = 64
B_SIZES = (B_WIDTH, B_WIDTH, B_WIDTH, 2 * B_HEADS, 2 * B_HEADS, B_WIDTH)
B_COLS = sum(B_SIZES)
EVEN_IN = A_COLS + B_COLS

C_HEAD_DIM = 64
C_Q_HEADS = D_MODEL // C_HEAD_DIM
C_KV_HEADS = C_Q_HEADS // 4
C_GROUP = C_Q_HEADS // C_KV_HEADS
C_WINDOW = 128
C_BLOCK = 128
ROPE_BASE = 10000.0
ODD_IN = (C_Q_HEADS + 2 * C_KV_HEADS) * C_HEAD_DIM

N_EXPERTS = 16
MOE_HIDDEN = 1024
EC_CAPACITY = 2

kernel_name = "hybrid_rwkv7_gdn_swa_ec_moe_dit"


def _split(t, sizes):
    return jnp.split(t, np.cumsum(sizes)[:-1].tolist(), axis=-1)


def _heads(t, n_heads):
    return t.reshape(t.shape[:-1] + (n_heads, -1))


def _rms_norm(t, gain, eps=NORM_EPS):
    tf = t.astype(F32)
    tf = tf * lax.rsqrt(jnp.mean(tf * tf, axis=-1, keepdims=True) + eps)
    return (tf * gain.astype(F32)).astype(t.dtype)


def _l2norm(t, eps=1e-6):
    tf = t.astype(F32)
    return (tf * lax.rsqrt(jnp.sum(tf * tf, axis=-1, keepdims=True) + eps)).astype(t.dtype)


def _modulate(t, shift, scale):
    return t * (1 + scale) + shift


def _centred_shift_delta(p):
    pad = jnp.pad(p, ((0, 0), (1, 1), (0, 0)))
    return 0.5 * (pad[:, :-2] + pad[:, 2:]) - p


def _centred_dwconv(p, w):
    n_tap = w.shape[0]
    half = n_tap // 2
    n_tok = p.shape[1]
    pad = jnp.pad(p, ((0, 0), (half, half), (0, 0)))
    out = pad[:, 0:n_tok] * w[0]
    for j in range(1, n_tap):
        out = out + pad[:, j:j + n_tok] * w[j]
    return out


def _two_stream_scan(scan_fn, state0, ctx_in, lat_in, reverse):
    if reverse:
        ctx_in = [jnp.flip(t, axis=1) for t in ctx_in]
        lat_in = [jnp.flip(t, axis=1) for t in lat_in]
    state_ctx, y_ctx = scan_fn(state0, *ctx_in)
    _, y_lat = scan_fn(state_ctx, *lat_in)
    if reverse:
        y_ctx, y_lat = jnp.flip(y_ctx, axis=1), jnp.flip(y_lat, axis=1)
    return y_ctx, y_lat


def _rwkv_inputs(pa, mu, w0, w2, a0, a2, g2, k_k, k_a):
    bsz, n_tok, _ = pa.shape
    pa = pa + mu * _centred_shift_delta(pa)
    r, k, v, wd, ad, gd = _split(pa, A_SIZES)
    wd = wd.reshape(bsz, n_tok, 2, A_DECAY_LORA)
    ad = ad.reshape(bsz, n_tok, 2, A_ICLR_LORA)
    w_log = -jax.nn.softplus(-(w0 + jnp.einsum('btdr,drc->btdc', jnp.tanh(wd), w2))) - 0.5
    decay = jnp.exp(-jnp.exp(w_log.astype(F32)))
    a = jax.nn.sigmoid(a0 + jnp.einsum('btdr,drc->btdc', ad, a2))
    g = jax.nn.sigmoid(gd) @ g2
    kk = _l2norm(_heads(k * k_k, A_HEADS)).reshape(k.shape)
    k_dir = k[:, :, None] * (1 + (a - 1) * k_a)
    b_dir = kk[:, :, None] * a
    return r, v, g, kk, decay, k_dir, b_dir


def _rwkv_dir(f, d):
    r, v, g, kk, decay, k_dir, b_dir = f
    return [_heads(t, A_HEADS) for t in (r, decay[:, :, d], k_dir[:, :, d], v, kk, b_dir[:, :, d])]


def _rwkv_scan(state0, r, decay, k, v, kk, b):
    xs = [jnp.moveaxis(t.astype(F32), 1, 0) for t in (r, decay, k, v, kk, b)]

    def step(state, inp):
        r_t, w_t, k_t, v_t, kk_t, b_t = inp
        sa = jnp.einsum('bhij,bhj->bhi', state, kk_t)
        state = (state * w_t[:, :, None, :] - sa[..., None] * b_t[:, :, None, :]
                 + v_t[..., None] * k_t[:, :, None, :])
        return state, jnp.einsum('bhij,bhj->bhi', state, r_t)

    state, y = lax.scan(step, state0, xs)
    return state, jnp.moveaxis(y, 0, 1)


def _rwkv_output(f, y, ln_w, ln_b, r_k):
    r, v, g, kk, decay, k_dir, b_dir = f
    rh, vh = _heads(r, A_HEADS), _heads(v, A_HEADS)
    kh = _heads(k_dir, A_HEADS)
    mean = jnp.mean(y, axis=-1, keepdims=True)
    var = jnp.mean(jnp.square(y - mean), axis=-1, keepdims=True)
    yn = ((y - mean) * lax.rsqrt(var + A_LN_EPS)).reshape(y.shape[:2] + (A_WIDTH,))
    yn = (yn * ln_w.astype(F32) + ln_b.astype(F32)).astype(r.dtype)
    bonus = jnp.sum(rh[:, :, None] * kh * r_k, axis=(2, 4))[..., None] * vh
    return (yn + bonus.reshape(yn.shape)) * g


def _gdn_inputs(pb, conv_w, a_log, dt_bias):
    bsz, n_tok, _ = pb.shape
    q, k, v, al, be, z = _split(pb, B_SIZES)
    q, k, v = jnp.split(jax.nn.silu(_centred_dwconv(jnp.concatenate([q, k, v], axis=-1), conv_w)), 3, axis=-1)
    q = _l2norm(_heads(q, B_HEADS)) * B_HEAD_DIM ** -0.5
    k = _l2norm(_heads(k, B_HEADS))
    v = _heads(v, B_HEADS)
    al = al.reshape(bsz, n_tok, 2, B_HEADS)
    be = be.reshape(bsz, n_tok, 2, B_HEADS)
    g = -jnp.exp(a_log.astype(F32)) * jax.nn.softplus((al + dt_bias).astype(F32))
    beta = jax.nn.sigmoid(be.astype(F32))
    return q, k, v, g, beta, z


def _gdn_dir(f, d):
    q, k, v, g, beta, z = f
    return [q, k, v, g[:, :, d], beta[:, :, d]]


def _gdn_chunked(state0, q, k, v, g, beta):
    bsz, n_tok, n_heads, _ = q.shape
    dv = v.shape[-1]
    n_chunk = n_tok // B_CHUNK

    def chunked(t):
        t = t.astype(F32).reshape((bsz, n_chunk, B_CHUNK) + t.shape[2:])
        return jnp.moveaxis(jnp.moveaxis(t, 1, 0), 2, 3)

    qc, kc, vc, gc, bc = [chunked(t) for t in (q, k, v, g, beta)]
    gcum = jnp.cumsum(gc, axis=-1)
    pos = jnp.arange(B_CHUNK)
    incl = pos[:, None] >= pos[None, :]
    strict = pos[:, None] > pos[None, :]
    decay = jnp.exp(jnp.where(incl, gcum[..., :, None] - gcum[..., None, :], -jnp.inf))
    k_beta = kc * bc[..., None]
    l_mat = jnp.where(strict, jnp.einsum('nbhid,nbhjd->nbhij', k_beta, kc) * decay, 0.0)
    rhs = jnp.concatenate([vc * bc[..., None], k_beta * jnp.exp(gcum)[..., None]], axis=-1)
    sol = lax.linalg.triangular_solve(l_mat, rhs, left_side=True, lower=True, unit_diagonal=True)
    u, w = sol[..., :dv], sol[..., dv:]
    a_intra = jnp.where(incl, jnp.einsum('nbhid,nbhjd->nbhij', qc, kc) * decay, 0.0)

    def step(state, inp):
        q_i, k_i, u_i, w_i, g_i, a_i = inp
        v_new = u_i - jnp.einsum('bhlk,bhkv->bhlv', w_i, state)
        o_i = (jnp.einsum('bhlk,bhkv->bhlv', q_i * jnp.exp(g_i)[..., None], state)
               + jnp.einsum('bhlm,bhmv->bhlv', a_i, v_new))
        g_last = g_i[..., -1:]
        state = (state * jnp.exp(g_last)[..., None]
                 + jnp.einsum('bhlk,bhlv->bhkv', k_i * jnp.exp(g_last - g_i)[..., None], v_new))
        return state, o_i

    state, o = lax.scan(step, state0, (qc, kc, u, w, gcum, a_intra))
    o = jnp.moveaxis(jnp.moveaxis(o, 3, 2), 0, 1).reshape(bsz, n_tok, n_heads, dv)
    return state, o


def _gdn_output(f, o, norm_w):
    z = f[-1]
    on = o * lax.rsqrt(jnp.mean(o * o, axis=-1, keepdims=True) + NORM_EPS) * norm_w.astype(F32)
    return (on.astype(z.dtype) * jax.nn.silu(_heads(z, B_HEADS))).reshape(z.shape)


def _even_mixer(h_ctx, h_lat, w_in, w_out, a_mu, a_w0, a_w2, a_a0, a_a2, a_g2, a_k_k, a_k_a, a_r_k,
                a_ln_w, a_ln_b, b_conv, b_a_log, b_dt_bias, b_norm):
    bsz = h_lat.shape[0]
    pa_c, pb_c = _split(h_ctx @ w_in, (A_COLS, B_COLS))
    pa_l, pb_l = _split(h_lat @ w_in, (A_COLS, B_COLS))
    ra_c = _rwkv_inputs(pa_c, a_mu, a_w0, a_w2, a_a0, a_a2, a_g2, a_k_k, a_k_a)
    ra_l = _rwkv_inputs(pa_l, a_mu, a_w0, a_w2, a_a0, a_a2, a_g2, a_k_k, a_k_a)
    gb_c = _gdn_inputs(pb_c, b_conv, b_a_log, b_dt_bias)
    gb_l = _gdn_inputs(pb_l, b_conv, b_a_log, b_dt_bias)
    s0_a = jnp.zeros((bsz, A_HEADS, A_HEAD_DIM, A_HEAD_DIM), F32)
    s0_b = jnp.zeros((bsz, B_HEADS, B_HEAD_DIM, B_HEAD_DIM), F32)
    ya_cf, ya_lf = _two_stream_scan(_rwkv_scan, s0_a, _rwkv_dir(ra_c, 0), _rwkv_dir(ra_l, 0), False)
    ya_cb, ya_lb = _two_stream_scan(_rwkv_scan, s0_a, _rwkv_dir(ra_c, 1), _rwkv_dir(ra_l, 1), True)
    yb_cf, yb_lf = _two_stream_scan(_gdn_chunked, s0_b, _gdn_dir(gb_c, 0), _gdn_dir(gb_l, 0), False)
    yb_cb, yb_lb = _two_stream_scan(_gdn_chunked, s0_b, _gdn_dir(gb_c, 1), _gdn_dir(gb_l, 1), True)
    out_ctx = jnp.concatenate([_rwkv_output(ra_c, ya_cf + ya_cb, a_ln_w, a_ln_b, a_r_k),
                               _gdn_output(gb_c, yb_cf + yb_cb, b_norm)], axis=-1) @ w_out
    out_lat = jnp.concatenate([_rwkv_output(ra_l, ya_lf + ya_lb, a_ln_w, a_ln_b, a_r_k),
                               _gdn_output(gb_l, yb_lf + yb_lb, b_norm)], axis=-1) @ w_out
    return out_ctx, out_lat


def _axial_rope(t, row, col):
    quarter = C_HEAD_DIM // 4
    half = C_HEAD_DIM // 2
    inv = ROPE_BASE ** (-jnp.arange(quarter, dtype=F32) / quarter)

    def rot(part, pos):
        ang = pos[:, None] * inv[None, :]
        cos = jnp.cos(ang)[None, :, None, :].astype(t.dtype)
        sin = jnp.sin(ang)[None, :, None, :].astype(t.dtype)
        p1, p2 = part[..., :quarter], part[..., quarter:]
        return jnp.concatenate([p1 * cos - p2 * sin, p2 * cos + p1 * sin], axis=-1)

    return jnp.concatenate([rot(t[..., :half], row), rot(t[..., half:], col)], axis=-1)


def _odd_mixer(h_ctx, h_lat, w_in, w_out, sink):
    bsz, n_lat, _ = h_lat.shape
    q_cols = C_Q_HEADS * C_HEAD_DIM
    kv_cols = C_KV_HEADS * C_HEAD_DIM
    k_ctx, v_ctx = [_heads(t, C_KV_HEADS) for t in jnp.split(h_ctx @ w_in[:, q_cols:], 2, axis=-1)]
    q, k, v = _split(h_lat @ w_in, (q_cols, kv_cols, kv_cols))
    rows = n_lat // GRID_W
    row = jnp.repeat(jnp.arange(rows, dtype=F32), GRID_W)
    col = jnp.tile(jnp.arange(GRID_W, dtype=F32), rows)
    q = _axial_rope(_heads(q, C_Q_HEADS), row, col) * C_HEAD_DIM ** -0.5
    q = q.reshape(bsz, n_lat, C_KV_HEADS, C_GROUP, C_HEAD_DIM)
    k = _axial_rope(_heads(k, C_KV_HEADS), row, col)
    v = _heads(v, C_KV_HEADS)
    pad = ((0, 0), (C_BLOCK, C_BLOCK), (0, 0), (0, 0))
    k_pad, v_pad = jnp.pad(k, pad), jnp.pad(v, pad)
    span = 3 * C_BLOCK
    sink_logit = jnp.broadcast_to(sink.astype(F32).reshape(1, C_KV_HEADS, C_GROUP, 1, 1),
                                  (bsz, C_KV_HEADS, C_GROUP, C_BLOCK, 1))

    def block(n):
        start = n * C_BLOCK
        qb = lax.dynamic_slice_in_dim(q, start, C_BLOCK, axis=1)
        kb = lax.dynamic_slice_in_dim(k_pad, start, span, axis=1)
        vb = lax.dynamic_slice_in_dim(v_pad, start, span, axis=1)
        q_pos = start + jnp.arange(C_BLOCK)
        k_pos = start - C_BLOCK + jnp.arange(span)
        valid = ((jnp.abs(q_pos[:, None] - k_pos[None, :]) <= C_WINDOW)
                 & (k_pos >= 0)[None, :] & (k_pos < n_lat)[None, :])
        s_loc = jnp.where(valid, jnp.einsum('bqhgd,bshd->bhgqs', qb, kb).astype(F32), -jnp.inf)
        s_ctx = jnp.einsum('bqhgd,bshd->bhgqs', qb, k_ctx).astype(F32)
        p = jax.nn.softmax(jnp.concatenate([s_loc, s_ctx, sink_logit], axis=-1), axis=-1).astype(vb.dtype)
        return (jnp.einsum('bhgqs,bshd->bqhgd', p[..., :span], vb)
                + jnp.einsum('bhgqs,bshd->bqhgd', p[..., span:-1], v_ctx))

    out = lax.map(block, jnp.arange(n_lat // C_BLOCK))
    out = jnp.moveaxis(out, 0, 1).reshape(bsz, n_lat, q_cols)
    return out @ w_out


def _ec_moe(h, router, w1, w3, w2):
    bsz, n_tok, d = h.shape
    cap = EC_CAPACITY * n_tok // N_EXPERTS
    aff = jax.nn.softmax(jnp.einsum('btd,de->bte', h, router).astype(F32), axis=-1)
    gate, idx = lax.top_k(jnp.swapaxes(aff, 1, 2), cap)
    xe = jax.vmap(lambda hb, ib: hb[ib])(h, idx)
    hid = jax.nn.silu(jnp.einsum('becd,edf->becf', xe, w1)) * jnp.einsum('becd,edf->becf', xe, w3)
    ye = jnp.einsum('becf,efd->becd', hid, w2) * gate[..., None].astype(h.dtype)
    return jax.vmap(lambda ib, yb: jnp.zeros((n_tok, d), yb.dtype).at[ib.reshape(-1)].add(yb.reshape(-1, d)))(idx, ye)


def setup_inputs(seed: int = 0) -> dict:
    key = jax.random.key(seed)
    ks = iter(jax.random.split(key, 48))

    def nrm(shape, scale):
        return scale * jax.random.normal(next(ks), shape, F32)

    def unif(shape, lo, hi):
        return jax.random.uniform(next(ks), shape, F32, lo, hi)

    dt = jnp.exp(unif((N_EVEN, 2, B_HEADS), math.log(1e-3), math.log(1e-1)))
    return {
        "x": nrm((BATCH, SEQ, D_MODEL), 1.0),
        "c": nrm((BATCH, D_MODEL), 1.0),
        "ctx": nrm((BATCH, CTX_LEN, D_MODEL), 1.0),
        "c_ctx": nrm((D_MODEL,), 1.0),
        "ada_w": nrm((DEPTH, D_MODEL, N_MOD * D_MODEL), 0.5 * D_MODEL ** -0.5),
        "ada_b": nrm((DEPTH, N_MOD * D_MODEL), 0.01),
        "norm_mix": 1.0 + nrm((DEPTH, D_MODEL), 0.1),
        "norm_ffn": 1.0 + nrm((DEPTH, D_MODEL), 0.1),
        "e_w_in": nrm((N_EVEN, D_MODEL, EVEN_IN), D_MODEL ** -0.5),
        "e_w_out": nrm((N_EVEN, A_WIDTH + B_WIDTH, D_MODEL), (A_WIDTH + B_WIDTH) ** -0.5),
        "a_mu": unif((N_EVEN, A_COLS), 0.0, 1.0),
        "a_w0": unif((N_EVEN, 2, A_WIDTH), -6.0, -1.0),
        "a_w2": nrm((N_EVEN, 2, A_DECAY_LORA, A_WIDTH), 0.1 * A_DECAY_LORA ** -0.5),
        "a_a0": nrm((N_EVEN, 2, A_WIDTH), 0.1),
        "a_a2": nrm((N_EVEN, 2, A_ICLR_LORA, A_WIDTH), 0.1 * A_ICLR_LORA ** -0.5),
        "a_g2": nrm((N_EVEN, A_GATE_LORA, A_WIDTH), A_GATE_LORA ** -0.5),
        "a_k_k": 0.85 + nrm((N_EVEN, A_WIDTH), 0.05),
        "a_k_a": 1.0 + nrm((N_EVEN, A_WIDTH), 0.05),
        "a_r_k": nrm((N_EVEN, A_HEADS, A_HEAD_DIM), 0.1),
        "a_ln_w": 1.0 + nrm((N_EVEN, A_WIDTH), 0.1),
        "a_ln_b": nrm((N_EVEN, A_WIDTH), 0.01),
        "b_conv": nrm((N_EVEN, B_CONV, 3 * B_WIDTH), B_CONV ** -0.5),
        "b_a_log": jnp.log(unif((N_EVEN, 2, B_HEADS), 1.0, 16.0)),
        "b_dt_bias": dt + jnp.log(-jnp.expm1(-dt)),
        "b_norm": 1.0 + nrm((N_EVEN, B_HEAD_DIM), 0.1),
        "o_w_in": nrm((N_ODD, D_MODEL, ODD_IN), D_MODEL ** -0.5),
        "o_w_out": nrm((N_ODD, C_Q_HEADS * C_HEAD_DIM, D_MODEL), (C_Q_HEADS * C_HEAD_DIM) ** -0.5),
        "o_sink": nrm((N_ODD, C_Q_HEADS), 0.5),
        "moe_router": nrm((DEPTH, D_MODEL, N_EXPERTS), D_MODEL ** -0.5),
        "moe_w1": nrm((DEPTH, N_EXPERTS, D_MODEL, MOE_HIDDEN), D_MODEL ** -0.5),
        "moe_w3": nrm((DEPTH, N_EXPERTS, D_MODEL, MOE_HIDDEN), D_MODEL ** -0.5),
        "moe_w2": nrm((DEPTH, N_EXPERTS, MOE_HIDDEN, D_MODEL), MOE_HIDDEN ** -0.5),
        "final_norm": 1.0 + nrm((D_MODEL,), 0.1),
    }


def reference(x, c, ctx, c_ctx, ada_w, ada_b, norm_mix, norm_ffn, e_w_in, e_w_out, a_mu, a_w0, a_w2, a_a0,
              a_a2, a_g2, a_k_k, a_k_a, a_r_k, a_ln_w, a_ln_b, b_conv, b_a_log, b_dt_bias, b_norm, o_w_in,
              o_w_out, o_sink, moe_router, moe_w1, moe_w3, moe_w2, final_norm):
    x_lat, x_ctx = x, ctx
    silu_c, silu_cc = jax.nn.silu(c), jax.nn.silu(c_ctx)
    for i in range(DEPTH):
        j = i // 2
        mod_l = jnp.split((silu_c @ ada_w[i] + ada_b[i])[:, None, :], N_MOD, axis=-1)
        mod_c = jnp.split(silu_cc @ ada_w[i] + ada_b[i], N_MOD, axis=-1)
        h_lat = _modulate(_rms_norm(x_lat, norm_mix[i]), mod_l[0], mod_l[1])
        h_ctx = _modulate(_rms_norm(x_ctx, norm_mix[i]), mod_c[0], mod_c[1])
        if i % 2 == 0:
            o_ctx, o_lat = _even_mixer(h_ctx, h_lat, e_w_in[j], e_w_out[j], a_mu[j], a_w0[j], a_w2[j], a_a0[j],
                                       a_a2[j], a_g2[j], a_k_k[j], a_k_a[j], a_r_k[j], a_ln_w[j], a_ln_b[j],
                                       b_conv[j], b_a_log[j], b_dt_bias[j], b_norm[j])
            x_ctx = x_ctx + mod_c[2] * o_ctx
        else:
            o_lat = _odd_mixer(h_ctx, h_lat, o_w_in[j], o_w_out[j], o_sink[j])
        x_lat = x_lat + mod_l[2] * o_lat
        x_lat = x_lat + mod_l[5] * _ec_moe(_modulate(_rms_norm(x_lat, norm_ffn[i]), mod_l[3], mod_l[4]),
                                           moe_router[i], moe_w1[i], moe_w3[i], moe_w2[i])
        if i < DEPTH - 1:
            x_ctx = x_ctx + mod_c[5] * _ec_moe(_modulate(_rms_norm(x_ctx, norm_ffn[i]), mod_c[3], mod_c[4]),
                                               moe_router[i], moe_w1[i], moe_w3[i], moe_w2[i])
    return _rms_norm(x_lat, final_norm)
```

```python
import numpy as np
import concourse.bass as bass
import concourse.mybir as mybir
from concourse.bass_utils import run_bass_kernel_spmd

F32 = mybir.dt.float32
BF16 = mybir.dt.bfloat16
I32 = mybir.dt.int32
U32 = mybir.dt.uint32
ALU = mybir.AluOpType
AF = mybir.ActivationFunctionType
AX = mybir.AxisListType

SEM_ROTATE = 20000
N_DMA_SEMS = 24


class KB:
    def __init__(self, nc, same_engine_sync=True):
        self.nc = nc
        self.engs = {"pe": nc.tensor, "act": nc.scalar, "dve": nc.vector, "pool": nc.gpsimd, "sp": nc.sync}
        self.same_engine_sync = same_engine_sync
        self.esem = {}
        self.ecnt = {}
        self.sem_id = 0
        for e in ("pe", "act", "dve", "pool"):
            self._new_esem(e)
        self.dsems = [self._alloc_sem(f"dma{i}") for i in range(N_DMA_SEMS)]
        self.dcnt = [0] * N_DMA_SEMS
        self.dnext = 0
        self.known = {e: {} for e in self.engs}
        self.state = {}
        self.n_ins = 0
        self._uid = 0
        self.out_tokens = []

    def _alloc_sem(self, name):
        self.sem_id += 1
        return self.nc.alloc_semaphore(f"{name}_{self.sem_id}")

    def _new_esem(self, e):
        self.esem[e] = self._alloc_sem(f"s_{e}")
        self.ecnt[e] = 0

    def uid(self, p="t"):
        self._uid += 1
        return f"{p}{self._uid}"

    def _deps(self, reads, writes):
        deps = []
        for r in reads:
            st = self.state.get(r)
            if st and st[0] is not None:
                deps.append(st[0])
        for w in writes:
            st = self.state.get(w)
            if st:
                if st[0] is not None:
                    deps.append(st[0])
                deps.extend(st[1].values())
        return deps

    def _wait(self, e, deps):
        eng = self.engs[e]
        kn = self.known[e]
        best = {}
        for (sem, val, src) in deps:
            if src == e and not (self.same_engine_sync and e != "pe"):
                continue
            key = id(sem)
            if kn.get(key, 0) >= val:
                continue
            if key not in best or best[key][1] < val:
                best[key] = (sem, val)
        for key, (sem, val) in best.items():
            eng.wait_ge(sem, val)
            kn[key] = val
            self.n_ins += 1

    def _commit(self, token, reads, writes):
        for w in writes:
            self.state[w] = [token, {}]
        for r in reads:
            st = self.state.get(r)
            if st is None:
                st = [None, {}]
                self.state[r] = st
            st[1][id(token[0])] = token

    def op(self, e, fn, reads=(), writes=()):
        reads = list(reads)
        writes = list(writes)
        writes += [r for r in reads if isinstance(r, str) and r.startswith("ps")]
        self._wait(e, self._deps(reads, writes))
        if self.ecnt[e] >= SEM_ROTATE:
            self._new_esem(e)
        ins = fn(self.engs[e])
        self.ecnt[e] += 1
        ins.then_inc(self.esem[e], 1)
        token = (self.esem[e], self.ecnt[e], e)
        self._commit(token, reads, writes)
        self.n_ins += 1
        return token

    def dma(self, q, out, in_, reads=(), writes=(), **kw):
        reads = list(reads)
        writes = list(writes)
        i = self.dnext
        self.dnext = (self.dnext + 1) % N_DMA_SEMS
        deps = self._deps(reads, writes)
        if self.dcnt[i] > 0:
            deps.append((self.dsems[i], self.dcnt[i], "dma"))
        self._wait(q, deps)
        ins = self.engs[q].dma_start(out=out, in_=in_, **kw)
        self.dcnt[i] += 16
        ins.then_inc(self.dsems[i], 16)
        token = (self.dsems[i], self.dcnt[i], "dma")
        self._commit(token, reads, writes)
        self.n_ins += 1
        return token

    def finish(self, out_keys):
        deps = []
        for k in out_keys:
            st = self.state.get(k)
            if st and st[0] is not None:
                deps.append(st[0])
        self._wait("sp", deps)
        deps = [(self.dsems[i], self.dcnt[i], "dma") for i in range(N_DMA_SEMS) if self.dcnt[i] > 0]
        self._wait("sp", deps)

    def barrier(self):
        deps = [(self.esem[e], self.ecnt[e], "x") for e in self.esem if self.ecnt[e] > 0]
        deps += [(self.dsems[i], self.dcnt[i], "dma") for i in range(N_DMA_SEMS) if self.dcnt[i] > 0]
        for e in self.engs:
            self._wait(e, deps)
        self.state = {}


D = 1024
KC = 8
EPS = 1e-6
TP = 2310
CTX0, LAT0 = 2, 260
CHUNK = 128
CHUNK_COLS = [CTX0 + CHUNK * j for j in range(256 // CHUNK)] + [LAT0 + CHUNK * j for j in range(2048 // CHUNK)]
BLOCKS = [(0, 256, CTX0)] + [(256 + 512 * j, 512, LAT0 + 512 * j) for j in range(4)]
DECAY_K = float(np.exp(-0.5))


class Ctx:
    pass


def load_consts(kb, nc, es, g):
    c = Ctx()
    c.ident = es.enter_context(nc.sbuf_tensor("c_ident", [128, 128], F32))
    c.identb = es.enter_context(nc.sbuf_tensor("c_identb", [128, 128], BF16))
    c.ones = es.enter_context(nc.sbuf_tensor("c_ones", [128, 128], F32))
    c.iota = es.enter_context(nc.sbuf_tensor("c_iota", [128, 256], F32))
    kb.dma("sp", c.ident[:], g["k_ident"][:, :], writes=["c_ident"])
    kb.dma("sp", c.iota[:], g["k_iota"][:, :], writes=["c_iota"])
    kb.op("dve", lambda e: e.memset(c.ones[:], 1.0), writes=["c_ones"])
    c.eps6 = es.enter_context(nc.sbuf_tensor("c_eps6", [128, 1], F32))
    c.one1 = es.enter_context(nc.sbuf_tensor("c_one1", [128, 1], F32))
    kb.op("dve", lambda e: e.memset(c.eps6[:], 1e-6), writes=["c_eps"])
    kb.op("dve", lambda e: e.memset(c.one1[:], 1.0), writes=["c_eps"])
    kb.op("dve", lambda e: e.tensor_copy(out=c.identb[:], in_=c.ident[:]), reads=["c_ident"], writes=["c_identb"])
    return c


def prologue(kb, nc, es, g, c):
    mods = []
    for l in range(2):
        mods.append(es.enter_context(nc.sbuf_tensor(f"mod{l}", [128, 48, 2], F32)))
    with nc.sbuf_tensor("pl_sc", [128, 2, 8], F32) as sc, \
            nc.sbuf_tensor("pl_w0", [128, 8, 512], F32) as w0, \
            nc.sbuf_tensor("pl_w1", [128, 8, 512], F32) as w1, \
            nc.sbuf_tensor("pl_b", [128, 2, 48], F32) as adab, \
            nc.sbuf_tensor("pl_n", [128, 2, 2, 8], F32) as nrm, \
            nc.psum_tensor("pl_ps", [128, 512], F32) as ps:
        wb = [w0, w1]
        kb.dma("sp", sc[:, 0, :], g["cT"][:, :], writes=["sc"])
        kb.dma("sp", sc[:, 1, :], g["ccT"][:, :], writes=["sc"])
        kb.dma("sp", adab[:], g["ada_bT"][:, :, :], writes=["adab"])
        kb.dma("sp", nrm[:], g["normT"][:, :, :, :], writes=["nrm"])
        kb.op("act", lambda e: e.activation(out=sc[:], in_=sc[:], func=AF.Silu), reads=["sc"], writes=["sc"])
        blk = 0
        for l in range(2):
            wv = g["ada_w"][l].rearrange("(kc p) n -> p kc n", p=128)
            for nb in range(12):
                wt = wb[blk % 2]
                wk = f"plw{blk % 2}"
                kb.dma("sp" if blk % 2 == 0 else "act", wt[:], wv[:, :, nb * 512:(nb + 1) * 512], writes=[wk])
                for j in range(4):
                    for kc in range(8):
                        kb.op("pe", lambda e, kc=kc, j=j, wt=wt: e.matmul(
                            ps[:, (j * 2):(j * 2 + 2)], lhsT=wt[:, kc, j * 128:(j + 1) * 128], rhs=sc[:, :, kc],
                            start=(kc == 0), stop=(kc == 7)), reads=[wk, "sc"], writes=["psPL"])
                kb.op("dve", lambda e, l=l, nb=nb: e.tensor_tensor(
                    out=mods[l][:, nb * 4:(nb + 1) * 4, :],
                    in0=ps[:, 0:8].rearrange("p (j s) -> p j s", s=2),
                    in1=adab[:, l, nb * 4:(nb + 1) * 4].unsqueeze(2).broadcast_to([128, 4, 2]),
                    op=ALU.add), reads=["psPL", "adab"], writes=[f"mod{l}"])
                blk += 1
            for (m, which) in ((1, 0), (4, 1)):
                for s in range(2):
                    kb.op("dve", lambda e, l=l, m=m, which=which, s=s: e.scalar_tensor_tensor(
                        out=mods[l][:, m * 8:(m + 1) * 8, s], in0=mods[l][:, m * 8:(m + 1) * 8, s], scalar=1.0,
                        in1=nrm[:, l, which, :], op0=ALU.add, op1=ALU.mult),
                        reads=[f"mod{l}", "nrm"], writes=[f"mod{l}"])
    kb.barrier()
    return mods


def make_bc(kb, nc, c, col_ap_fn, out_tile, ps, key, src_keys):
    with nc.sbuf_tensor(kb.uid("bcd"), [128, 128], F32) as dg:
        dk = kb.uid("dg")
        for half in range(2):
            for q in range(4):
                kc = half * 4 + q
                kb.op("dve", lambda e, kc=kc: e.tensor_scalar(
                    out=dg[:], in0=c.ident[:], scalar1=col_ap_fn(kc), scalar2=None, op0=ALU.mult),
                    reads=["c_ident"] + src_keys, writes=[dk])
                kb.op("pe", lambda e, q=q: e.matmul(ps[:, q * 128:(q + 1) * 128], lhsT=c.ones[:], rhs=dg[:],
                                                   start=True, stop=True),
                      reads=[dk, "c_ones"], writes=["psBC" + key])
            kb.op("act", lambda e, half=half: e.copy(out=out_tile[:, half * 512:(half + 1) * 512], in_=ps[:]),
                  reads=["psBC" + key], writes=[key])
        kb.barrier()


def norm_tile(kb, nc, xt, xk, st, G, S, hout, hk, eps=EPS):
    sk = kb.uid("st")
    kb.op("act", lambda e: e.activation(out=hout, in_=xt, func=AF.Square, accum_out=st[:, 0:1]),
          reads=[xk], writes=[hk, sk])
    kb.op("dve", lambda e: e.tensor_scalar(out=st[:, 1:2], in0=st[:, 0:1], scalar1=1.0 / D, scalar2=eps,
                                           op0=ALU.mult, op1=ALU.add), reads=[sk], writes=[sk])
    kb.op("act", lambda e: e.activation(out=st[:, 2:3], in_=st[:, 1:2], func=AF.Sqrt), reads=[sk], writes=[sk])
    kb.op("dve", lambda e: e.reciprocal(out=st[:, 3:4], in_=st[:, 2:3]), reads=[sk], writes=[sk])
    if G is None:
        kb.op("dve", lambda e: e.tensor_scalar(out=hout, in0=xt, scalar1=st[:, 3:4], scalar2=None, op0=ALU.mult),
              reads=[xk, sk], writes=[hk])
        return
    kb.op("dve", lambda e: e.scalar_tensor_tensor(out=hout, in0=xt, scalar=st[:, 3:4], in1=G[:],
                                                  op0=ALU.mult, op1=ALU.mult),
          reads=[xk, sk, "Gbc"], writes=[hk])
    if S is not None:
        kb.op("pool", lambda e: e.tensor_tensor(out=hout, in0=hout, in1=S[:], op=ALU.add),
              reads=[hk, "Sbc"], writes=[hk])


def moe_stage(kb, nc, g, c, mods, layer, stream, x_in, x_out, T, final_norm=False):
    NT = T // 128
    cap = 2 * T // 16
    CW = cap
    CT = (cap + 127) // 128
    cs = min(cap, 128)
    mod = mods[layer]
    xin_v = x_in.rearrange("(n p) d -> n p d", p=128)
    xout_v = x_out.rearrange("(n p) d -> n p d", p=128)
    sx = f"L{layer}s{stream}"
    from contextlib import ExitStack
    with ExitStack() as es:
        def sb(name, shape, dt):
            return es.enter_context(nc.sbuf_tensor(f"moe_{name}_{sx}", shape, dt))
        Gbc = sb("G", [128, D], F32)
        Sbc = sb("S", [128, D], F32)
        gate2 = Sbc
        hbf = sb("hbf", [128, NT, D], BF16)
        xb = [sb("x0", [128, D], F32), sb("x1", [128, D], F32)]
        h32 = [sb("h0", [128, D], F32), sb("h1", [128, D], F32)]
        hT = [sb("hT0", [128, 8, 128], F32), sb("hT1", [128, 8, 128], F32)]
        stt = [sb("st0", [128, 4], F32), sb("st1", [128, 4], F32)]
        rt = sb("rt", [128, 8, 16], F32)
        aff = sb("aff", [128, NT, 16], F32)
        sm = sb("sm", [128, 4], F32)
        ex = sb("ex", [128, 16], F32)
        affT = sb("affT", [16, T], F32)
        work = sb("work", [16, T], F32)
        mx8 = sb("mx8", [16, 8], F32)
        maskT = sb("maskT", [16, T], F32)
        onesT = work
        slotT = sb("slotT", [16, T], F32)
        gateT = affT
        slot = sb("slot", [128, NT, 16], F32)
        gate = sb("gate", [128, NT, 16], F32)
        selT = [sb("selT0", [128, CW], BF16), sb("selT1", [128, CW], BF16)]
        xeT = sb("xeT", [128, 8, CW], BF16)
        big = sb("big", [128, 4 * 8 * D], BF16)
        wviews = [big[:, j * 8 * D:(j + 1) * 8 * D].rearrange("p (k n) -> p k n", n=D) for j in range(4)]
        w1b = [wviews[0], wviews[1]]
        w3b = [wviews[2]]
        w2b = [wviews[3]]
        yest = [sb("yest0", [128, CT, D], BF16), sb("yest1", [128, CT, D], BF16)]
        sil = [sb("sil0", [128, CW], F32), sb("sil1", [128, CW], F32)]
        hidT = sb("hidT", [128, 8, CW], BF16)
        yeall = big[:, 0:16 * CT * D].rearrange("p (e c n) -> p e c n", e=16, c=CT)
        selGa = [sb("selGa0", [128, 4, CW], BF16), sb("selGa1", [128, 4, CW], BF16)]
        selGca = [sb("selGca0", [128, 4 * CT, 128], BF16), sb("selGca1", [128, 4 * CT, 128], BF16)]
        tmpo = [sb("tmpo0", [128, 512], F32), sb("tmpo1", [128, 512], F32)]
        ps = [es.enter_context(nc.psum_tensor(f"moe_ps{i}_{sx}", [128, 512], F32)) for i in range(8)]

        print("moe sbuf remaining", nc.sbuf_bytes_remaining)
        make_bc(kb, nc, c, lambda kc: mod[:, 4 * 8 + kc, stream:stream + 1], Gbc, ps[0], "Gbc", [f"mod{layer}"])
        make_bc(kb, nc, c, lambda kc: mod[:, 3 * 8 + kc, stream:stream + 1], Sbc, ps[0], "Sbc", [f"mod{layer}"])
        kb.dma("sp", rt[:], g["moe_router"][layer].rearrange("(kc p) e -> p kc e", p=128), writes=["rt"])

        for i in range(NT):
            b = i % 2
            kb.dma("sp", xb[b][:], xin_v[i], writes=[f"xb{b}"])
            norm_tile(kb, nc, xb[b][:], f"xb{b}", stt[b], Gbc, Sbc, h32[b][:], f"h32{b}")
            kb.op("act", lambda e, i=i, b=b: e.copy(out=hbf[:, i, :], in_=h32[b][:]), reads=[f"h32{b}"],
                  writes=[f"hbf{i}"])
            for half in range(2):
                for q in range(4):
                    kc = half * 4 + q
                    kb.op("pe", lambda e, kc=kc, q=q, b=b, half=half: e.transpose(
                        out=ps[half][:, q * 128:(q + 1) * 128], in_=h32[b][:, kc * 128:(kc + 1) * 128],
                        identity=c.ident[:]), reads=[f"h32{b}", "c_ident"], writes=[f"ps{half}"])
                kb.op("dve" if half == 0 else "act", (lambda e, half=half, b=b: e.tensor_copy(
                    out=hT[b][:, half * 4:(half + 1) * 4, :], in_=ps[half][:].rearrange("p (q t) -> p q t", q=4)))
                    if half == 0 else (lambda e, half=half, b=b: e.copy(
                        out=hT[b][:, half * 4:(half + 1) * 4, :], in_=ps[half][:].rearrange("p (q t) -> p q t", q=4))),
                    reads=[f"ps{half}"], writes=[f"hT{b}"])
            for kc in range(8):
                kb.op("pe", lambda e, kc=kc, b=b: e.matmul(ps[2][:, 0:16], lhsT=hT[b][:, kc, :], rhs=rt[:, kc, :],
                                                         start=(kc == 0), stop=(kc == 7)),
                      reads=[f"hT{b}", "rt"], writes=["ps2"])
            kb.op("dve", lambda e: e.reduce_max(out=sm[:, 0:1], in_=ps[2][:, 0:16], axis=AX.X),
                  reads=["ps2"], writes=["sm"])
            kb.op("dve", lambda e: e.tensor_scalar(out=sm[:, 1:2], in0=sm[:, 0:1], scalar1=-1.0, scalar2=None,
                                                   op0=ALU.mult), reads=["sm"], writes=["sm"])
            kb.op("act", lambda e: e.activation(out=ex[:], in_=ps[2][:, 0:16], func=AF.Exp, bias=sm[:, 1:2],
                                                accum_out=sm[:, 2:3]), reads=["ps2", "sm"], writes=["ex", "sm"])
            kb.op("dve", lambda e: e.reciprocal(out=sm[:, 3:4], in_=sm[:, 2:3]), reads=["sm"], writes=["sm"])
            kb.op("dve", lambda e, i=i: e.tensor_scalar(out=aff[:, i, :], in0=ex[:], scalar1=sm[:, 3:4], scalar2=None,
                                                        op0=ALU.mult), reads=["ex", "sm"], writes=["aff"])
            kb.op("pe", lambda e, i=i: e.transpose(out=ps[3][0:16, 0:128], in_=aff[:, i, :], identity=c.ident[:]),
                  reads=["aff", "c_ident"], writes=["ps3"])
            kb.op("act", lambda e, i=i: e.copy(out=affT[:, i * 128:(i + 1) * 128], in_=ps[3][0:16, 0:128]),
                  reads=["ps3"], writes=["affT"])

        import os
        PH = int(os.environ.get("MOE_PH", "9"))
        if PH < 2:
            kb.dma("sp", x_out[0:128, 0:NT * 16], aff[:].rearrange("p n e -> p (n e)"), reads=["aff"], writes=["o"])
            kb.barrier()
            return
        kb.op("dve", lambda e: e.tensor_copy(out=work[:], in_=affT[:]), reads=["affT"], writes=["work"])
        nr = cap // 8
        for r in range(nr):
            kb.op("dve", lambda e: e.max(out=mx8[:], in_=work[:]), reads=["work"], writes=["mx8"])
            if r < nr - 1:
                kb.op("dve", lambda e: e.match_replace(out=work[:], in_to_replace=mx8[:], in_values=work[:],
                                                       imm_value=-1.0), reads=["work", "mx8"], writes=["work"])
        kb.op("dve", lambda e: e.tensor_scalar(out=maskT[:], in0=affT[:], scalar1=mx8[:, 7:8], scalar2=None,
                                               op0=ALU.is_ge), reads=["affT", "mx8"], writes=["maskT"])
        kb.op("pool", lambda e: e.memset(onesT[:], 1.0), reads=[], writes=["work"])
        kb.op("dve", lambda e: e.tensor_tensor_scan(out=slotT[:], data0=onesT[:], data1=maskT[:], initial=0.0,
                                                    op0=ALU.mult, op1=ALU.add),
              reads=["work", "maskT"], writes=["slotT"])
        kb.op("dve", lambda e: e.tensor_tensor(out=slotT[:], in0=slotT[:], in1=maskT[:], op=ALU.mult),
              reads=["slotT", "maskT"], writes=["slotT"])
        kb.op("dve", lambda e: e.tensor_scalar(out=slotT[:], in0=slotT[:], scalar1=-1.0, scalar2=None, op0=ALU.add),
              reads=["slotT"], writes=["slotT"])
        kb.op("pool", lambda e: e.tensor_tensor(out=gateT[:], in0=affT[:], in1=maskT[:], op=ALU.mult),
              reads=["affT", "maskT"], writes=["affT"])
        for i in range(NT):
            kb.op("pe", lambda e, i=i: e.transpose(out=ps[0][:, i * 16:(i + 1) * 16],
                                                   in_=slotT[:, i * 128:(i + 1) * 128], identity=c.ident[0:16, 0:16]),
                  reads=["slotT", "c_ident"], writes=["ps0"])
            kb.op("pe", lambda e, i=i: e.transpose(out=ps[1][:, i * 16:(i + 1) * 16],
                                                   in_=gateT[:, i * 128:(i + 1) * 128], identity=c.ident[0:16, 0:16]),
                  reads=["affT", "c_ident"], writes=["ps1"])
        kb.op("dve", lambda e: e.tensor_copy(out=slot[:], in_=ps[0][:, 0:NT * 16].rearrange("p (n e) -> p n e", e=16)),
              reads=["ps0"], writes=["slot"])
        kb.op("act", lambda e: e.copy(out=gate[:], in_=ps[1][:, 0:NT * 16].rearrange("p (n e) -> p n e", e=16)),
              reads=["ps1"], writes=["gate"])

        if PH < 3:
            kb.dma("sp", x_out[0:128, 0:NT * 16], slot[:].rearrange("p n e -> p (n e)"), reads=["slot"], writes=["o"])
            kb.dma("sp", x_out[128:256, 0:NT * 16], gate[:].rearrange("p n e -> p (n e)"), reads=["gate"], writes=["o2"])
            kb.barrier()
            return
        stg = [xb[0], xb[1], h32[0], h32[1]]
        stgk = ["xb0", "xb1", "h320", "h321"]
        wcnt = [0]

        def load_w(wv, wt, wk):
            for hh in range(2):
                kb.dma("pool", wt[:, hh * 4:(hh + 1) * 4, :], wv[:, hh * 4:(hh + 1) * 4, :], writes=[wk])

        def wviews_of(e_):
            return (g["moe_w1"][layer, e_].rearrange("(kc p) n -> p kc n", p=128),
                    g["moe_w3"][layer, e_].rearrange("(kc p) n -> p kc n", p=128),
                    g["moe_w2"][layer, e_].rearrange("(kc p) n -> p kc n", p=128))
        nsel = 0
        SUB = int(os.environ.get("MOE_SUB", "9"))
        NEXP = int(os.environ.get("MOE_NEXP", "16"))
        wv1, wv3, wv2 = wviews_of(0)
        load_w(wv1, w1b[0], "w1_0")
        load_w(wv3, w3b[0], "w3")
        load_w(wv2, w2b[0], "w2")
        for ex_i in range(NEXP):
            w1t = w1b[ex_i % 2]
            w1k = f"w1_{ex_i % 2}"
            if ex_i + 1 < NEXP:
                nwv1, nwv3, nwv2 = wviews_of(ex_i + 1)
                load_w(nwv1, w1b[(ex_i + 1) % 2], f"w1_{(ex_i + 1) % 2}")
            for i in range(NT):
                b = nsel % 2
                nsel += 1
                kb.op("dve", lambda e, i=i, b=b, ex_i=ex_i: e.tensor_scalar(
                    out=selT[b][:], in0=c.iota[:, 0:CW], scalar1=slot[:, i, ex_i:ex_i + 1], scalar2=None,
                    op0=ALU.is_equal), reads=["c_iota", "slot"], writes=[f"selT{b}"])
                for kc in range(8):
                    bank = (kc * CW) // 512
                    off = (kc * CW) % 512
                    kb.op("pe", lambda e, i=i, b=b, kc=kc, bank=bank, off=off: e.matmul(
                        ps[bank][:, off:off + CW], lhsT=hbf[:, i, kc * 128:(kc + 1) * 128], rhs=selT[b][:],
                        start=(i == 0 and off == 0), stop=(i == NT - 1), skip_group_check=True), reads=[f"hbf{i}", f"selT{b}"], writes=[f"ps{bank}"])
            nb = (8 * CW + 511) // 512
            per = 512 // CW if CW < 512 else 1
            for bank in range(nb):
                k0 = bank * per
                k1 = min(8, k0 + per)
                kb.op("act" if bank % 2 else "dve", (lambda e, bank=bank, k0=k0, k1=k1: e.copy(
                    out=xeT[:, k0:k1, :], in_=ps[bank][:, 0:(k1 - k0) * CW].rearrange("p (k c) -> p k c", c=CW)))
                    if bank % 2 else (lambda e, bank=bank, k0=k0, k1=k1: e.tensor_copy(
                        out=xeT[:, k0:k1, :], in_=ps[bank][:, 0:(k1 - k0) * CW].rearrange("p (k c) -> p k c", c=CW))),
                    reads=[f"ps{bank}"], writes=["xeT"])
            if SUB < 2:
                continue
            for fc in range(8):
                pb = ps[4 + fc % 2]
                pk = f"ps{4 + fc % 2}"
                for kc in range(8):
                    kb.op("pe", lambda e, fc=fc, kc=kc, pb=pb, w1t=w1t: e.matmul(
                        pb[:, 0:CW], lhsT=w1t[:, kc, fc * 128:(fc + 1) * 128], rhs=xeT[:, kc, :],
                        start=(kc == 0), stop=(kc == 7)), reads=[w1k, "xeT"], writes=[pk])
                for kc in range(8):
                    kb.op("pe", lambda e, fc=fc, kc=kc, pb=pb: e.matmul(
                        pb[:, 256:256 + CW], lhsT=w3b[0][:, kc, fc * 128:(fc + 1) * 128], rhs=xeT[:, kc, :],
                        start=(kc == 0), stop=(kc == 7)), reads=["w3", "xeT"], writes=[pk])
                sb_ = sil[fc % 2]
                DBG = int(os.environ.get("MOE_DBG", "9"))
                if DBG < 1:
                    continue
                kb.op("act", lambda e, pb=pb, sb_=sb_: e.activation(out=sb_[:], in_=pb[:, 0:CW], func=AF.Silu),
                      reads=[pk], writes=[f"sil{fc % 2}"])
                if DBG < 2:
                    continue
                kb.op("dve", lambda e, pb=pb, sb_=sb_, fc=fc: e.tensor_tensor(
                    out=hidT[:, fc, :], in0=sb_[:], in1=pb[:, 256:256 + CW], op=ALU.mult),
                    reads=[f"sil{fc % 2}", pk], writes=["hidT"])
            if ex_i + 1 < NEXP:
                load_w(nwv3, w3b[0], "w3")
            if SUB < 3:
                continue
            for ct in range(CT):
                for half in range(2):
                    pb = ps[6 + half]
                    pk = f"ps{6 + half}"
                    for fc in range(8):
                        kb.op("pe", lambda e, ct=ct, half=half, fc=fc, pb=pb: e.matmul(
                            pb[0:cs, :], lhsT=hidT[:, fc, ct * 128:ct * 128 + cs],
                            rhs=w2b[0][:, fc, half * 512:(half + 1) * 512], start=(fc == 0), stop=(fc == 7)),
                            reads=["hidT", "w2"], writes=[pk])
                    ys = yest[ex_i % 2]
                    kb.op("act" if half else "dve", (lambda e, ct=ct, half=half, pb=pb, ys=ys: e.copy(
                        out=ys[0:cs, ct, half * 512:(half + 1) * 512], in_=pb[0:cs, :]))
                        if half else (lambda e, ct=ct, half=half, pb=pb, ys=ys: e.tensor_copy(
                            out=ys[0:cs, ct, half * 512:(half + 1) * 512], in_=pb[0:cs, :])),
                        reads=[pk], writes=[f"yest{ex_i % 2}"])
            if ex_i + 1 < NEXP:
                load_w(nwv2, w2b[0], "w2")
            kb.dma("sp", g["ye_scr"][ex_i, 0:cs, 0:CT, :], yest[ex_i % 2][0:cs, :, :], reads=[f"yest{ex_i % 2}"],
                   writes=[("yescr", ex_i)])

        if PH < 4:
            kb.barrier()
            return
        kb.barrier()
        make_bc(kb, nc, c, lambda kc: mod[:, 5 * 8 + kc, stream:stream + 1], gate2, ps[0], "gate2", [f"mod{layer}"])
        if final_norm:
            make_bc(kb, nc, c, lambda kc: c.fnT[:, kc:kc + 1], Gbc, ps[0], "Gbc", ["c_fnT"])
        for ex_i in range(16):
            kb.dma(["sp", "act"][ex_i % 2], yeall[0:cs, ex_i, :, :], g["ye_scr"][ex_i, 0:cs, 0:CT, :],
                   reads=[("yescr", ex_i)], writes=[f"ye{ex_i}"])
        nsg = 0
        EG = 4
        for i in range(NT):
            b = i % 2
            kb.dma("sp", xb[b][:], xin_v[i], writes=[f"xb{b}"])
            for g0 in range(0, 16, EG):
                sg = nsg % 2
                nsg += 1
                sga = selGa[sg]
                kb.op("dve", lambda e, i=i, g0=g0, sga=sga: e.tensor_tensor(
                    out=sga[:, :, :], in0=c.iota[:, 0:CW].unsqueeze(1).broadcast_to([128, EG, CW]),
                    in1=slot[:, i, g0:g0 + EG].unsqueeze(2).broadcast_to([128, EG, CW]), op=ALU.is_equal),
                    reads=["c_iota", "slot"], writes=[f"selGa{sg}"])
                kb.op("pool", lambda e, i=i, g0=g0, sga=sga: e.tensor_tensor(
                    out=sga[:, :, :], in0=sga[:, :, :],
                    in1=gate[:, i, g0:g0 + EG].unsqueeze(2).broadcast_to([128, EG, CW]), op=ALU.mult),
                    reads=[f"selGa{sg}", "gate"], writes=[f"selGa{sg}"])
                pst = ps[2 + sg][:].bitcast(BF16)
                for ee in range(EG):
                    for ct in range(CT):
                        kb.op("pe", lambda e, ct=ct, ee=ee, sga=sga, pst=pst: e.transpose(
                            out=pst[0:cs, (ee * CT + ct) * 128:(ee * CT + ct + 1) * 128],
                            in_=sga[:, ee, ct * 128:ct * 128 + cs], identity=c.identb[:]),
                            reads=[f"selGa{sg}", "c_identb"], writes=[f"ps{2 + sg}"])
                kb.op("act", lambda e, sg=sg, pst=pst: e.copy(
                    out=selGca[sg][0:cs, :, :], in_=pst[0:cs, 0:EG * CT * 128].rearrange("p (c t) -> p c t", t=128)),
                    reads=[f"ps{2 + sg}"], writes=[f"selGca{sg}"])
                for ee in range(EG):
                    ex_i = g0 + ee
                    for half in range(2):
                        for ct in range(CT):
                            kb.op("pe", lambda e, half=half, ct=ct, sg=sg, ex_i=ex_i, ee=ee: e.matmul(
                                ps[half][:, :], lhsT=selGca[sg][0:cs, ee * CT + ct, :],
                                rhs=yeall[0:cs, ex_i, ct, half * 512:(half + 1) * 512],
                                start=(ex_i == 0 and ct == 0), stop=(ex_i == 15 and ct == CT - 1)),
                                reads=[f"selGca{sg}", f"ye{ex_i}"], writes=[f"ps{half}"])
            for half in range(2):
                sl = slice(half * 512, (half + 1) * 512)
                kb.op("dve", lambda e, half=half, sl=sl: e.tensor_tensor(
                    out=tmpo[half][:], in0=ps[half][:], in1=gate2[:, sl], op=ALU.mult),
                    reads=[f"ps{half}", "gate2"], writes=[f"tmpo{half}"])
                kb.op("pool", lambda e, half=half, sl=sl, b=b: e.tensor_tensor(
                    out=xb[b][:, sl], in0=xb[b][:, sl], in1=tmpo[half][:], op=ALU.add),
                    reads=[f"tmpo{half}", f"xb{b}"], writes=[f"xb{b}"])
            if final_norm:
                norm_tile(kb, nc, xb[b][:], f"xb{b}", stt[b], Gbc, None, h32[b][:], f"h32{b}")
                kb.dma("sp", xout_v[i], h32[b][:], reads=[f"h32{b}"], writes=[("dram", x_out.tensor.name, i)])
            else:
                kb.dma("sp", xout_v[i], xb[b][:], reads=[f"xb{b}"], writes=[("dram", x_out.tensor.name, i)])
        kb.barrier()


def host_consts():
    k = {}
    k["k_ident"] = np.eye(128, dtype=np.float32)
    k["k_iota"] = np.tile(np.arange(256, dtype=np.float32)[None, :], (128, 1))
    C, S, perm = rope_tables()
    k["k_ropeC"], k["k_ropeS"], k["k_perm"] = C, S, perm
    kk = np.arange(128)[:, None]
    qq = np.arange(128)[None, :]
    k["k_mL"] = np.tile((kk >= qq).astype(np.float32), (1, 4))
    k["k_mU"] = np.tile((kk <= qq).astype(np.float32), (1, 4))
    ss = np.arange(CHUNK)[:, None]
    tt = np.arange(CHUNK)[None, :]
    mus = (ss < tt).astype(np.float32)
    mui = (ss <= tt).astype(np.float32)
    k["k_maskA"] = np.ascontiguousarray(np.concatenate([-mus, mus, mui, mui], axis=1))
    k["k_maskT"] = np.ascontiguousarray(-(tt < ss).astype(np.float32))
    rm = np.ones((128, TP), np.float32)
    rm[:, CHUNK_COLS] = 0.0
    k["k_rmask"] = rm
    k["k_J"] = np.ascontiguousarray(np.eye(128, dtype=np.float32)[::-1])
    sel = np.zeros((16, 16, 128), np.float32)
    for r in range(16):
        sel[r, r, :] = 1.0
    k["k_sel"] = sel
    bdm = np.zeros((128, 128), np.float32)
    bdm[0:64, 0:64] = 1.0
    bdm[64:128, 64:128] = 1.0
    k["k_bd"] = bdm
    return k


def fm(v):
    v = np.asarray(v, np.float32)
    return np.ascontiguousarray(v.reshape(-1, 128).T)


def host_inputs(inp, b):
    m = dict(host_consts())
    m["x"] = np.ascontiguousarray(inp["x"][b])
    m["ctx"] = np.ascontiguousarray(inp["ctx"][b])
    m["cT"] = fm(inp["c"][b])
    m["ccT"] = fm(inp["c_ctx"])
    m["ada_w"] = inp["ada_w"]
    m["ada_bT"] = np.ascontiguousarray(np.stack([fm(inp["ada_b"][l]) for l in range(2)], axis=1))
    m["normT"] = np.ascontiguousarray(np.stack(
        [np.stack([fm(inp["norm_mix"][l]), fm(inp["norm_ffn"][l])], axis=1) for l in range(2)], axis=1))
    m["fnT"] = fm(inp["final_norm"])
    for k in ("moe_router", "moe_w1", "moe_w3", "moe_w2", "o_w_out"):
        m[k] = inp[k]
    w = inp["o_w_in"][0]
    kd = w[:, 1024:1280].reshape(1024, 4, 1, 64)
    kd = np.concatenate([kd, kd], axis=2).reshape(1024, 512)
    m["o_w_in2"] = np.ascontiguousarray(np.concatenate([w[:, 0:1024], kd, w[:, 1280:1536]], axis=1))
    m["o_sink"] = np.ascontiguousarray(inp["o_sink"].reshape(1, 16))
    idx = []
    for p in range(4):
        for off in (0, 512, 1024):
            idx += list(range(off + p * 128, off + p * 128 + 128))
    for d in range(2):
        idx += list(range(1536 + d * 64, 1536 + d * 64 + 64)) + list(range(1664 + d * 64, 1664 + d * 64 + 64))
    idx += list(range(1792, 1920))
    idxa = list(idx)
    for h in range(4):
        for part in range(3):
            idx += list(range(1920 + part * 512 + h * 128, 1920 + part * 512 + h * 128 + 128))
        idx += list(range(3472 + h * 128, 3472 + h * 128 + 128))
    idx += list(range(3456, 3472))
    m["e_w_in2"] = np.ascontiguousarray(inp["e_w_in"][0][:, idx])
    mu2 = inp["a_mu"][0][idxa]
    p64 = np.zeros((64, 64), np.float32)
    p64[:, 0:30] = mu2.reshape(30, 64).T
    for d in range(2):
        for h in range(8):
            p64[:, 30 + d * 8 + h] = inp["a_w0"][0, d, h * 64:(h + 1) * 64]
            p64[:, 46 + d * 8 + h] = inp["a_a0"][0, d, h * 64:(h + 1) * 64]
    m["mx_par64"] = p64
    p128 = np.zeros((128, 112), np.float32)
    p128[:, 0] = mu2[1536:1664]; p128[:, 1] = mu2[1664:1792]; p128[:, 2] = mu2[1792:1920]
    for h in range(8):
        p128[0:64, 8 + h] = inp["a_k_k"][0, h * 64:(h + 1) * 64]
        p128[0:64, 16 + h] = inp["a_k_a"][0, h * 64:(h + 1) * 64]
        p128[0:64, 32 + h] = inp["a_r_k"][0, h]
    p128[0:8, 40] = inp["b_dt_bias"][0].reshape(8)
    p128[0:8, 41] = inp["b_a_log"][0].reshape(8)
    for h in range(4):
        for part in range(3):
            for j in range(5):
                p128[:, 48 + (h * 3 + part) * 5 + j] = inp["b_conv"][0, j, part * 512 + h * 128:part * 512 + (h + 1) * 128]
    m["mx_par128"] = p128
    pP = np.zeros((128, 64), np.float32)
    pP[:, 0:12] = mu2[0:1536].reshape(12, 128).T
    for p in range(4):
        sl = slice(p * 128, (p + 1) * 128)
        for d in range(2):
            pP[:, 12 + d * 4 + p] = inp["a_w0"][0, d, sl]
            pP[:, 20 + d * 4 + p] = inp["a_a0"][0, d, sl]
        pP[:, 28 + p] = inp["a_k_k"][0, sl]
        pP[:, 32 + p] = inp["a_k_a"][0, sl]
        pP[:, 40 + p] = inp["a_r_k"][0].reshape(512)[sl]
    m["mx_parP"] = pP
    m["mx_w2"] = np.ascontiguousarray(np.concatenate([inp["a_w2"][0].transpose(1, 0, 2), inp["a_a2"][0].transpose(1, 0, 2)], axis=0))
    m["mx_bc"] = np.ascontiguousarray(np.concatenate([inp["a_ln_w"][0], inp["a_ln_b"][0], np.tile(inp["b_norm"][0], 4)])[None, :])
    m["a_g2"] = inp["a_g2"]
    m["e_w_out"] = inp["e_w_out"]
    return m


IN_SHAPES = {
    "k_ident": [128, 128], "k_iota": [128, 256],
    "x": [2048, D], "ctx": [256, D], "cT": [128, 8], "ccT": [128, 8],
    "ada_w": [2, D, 6 * D], "ada_bT": [128, 2, 48], "normT": [128, 2, 2, 8], "fnT": [128, 8],
    "k_ropeC": [128, 2048], "k_ropeS": [128, 2048], "k_perm": [128, 128], "k_mL": [128, 512], "k_mU": [128, 512],
    "o_w_in2": [D, 1792], "o_w_out": [1, D, D], "o_sink": [1, 16],
    "k_maskA": [CHUNK, 4 * CHUNK], "k_maskT": [CHUNK, CHUNK], "k_rmask": [128, TP], "k_J": [128, 128], "k_sel": [16, 16, 128],
    "e_w_in2": [D, 3984], "mx_par64": [64, 64], "mx_parP": [128, 64], "k_bd": [128, 128], "mx_par128": [128, 112], "mx_w2": [128, 2, 512], "mx_bc": [1, 1536],
    "a_g2": [1, 128, 512], "e_w_out": [1, D, D],
    "moe_router": [2, D, 16], "moe_w1": [2, 16, D, D], "moe_w3": [2, 16, D, D], "moe_w2": [2, 16, D, D],
}


def build(stages=("all",), extra_in=(), outs=(("out", [2048, D]),)):
    from contextlib import ExitStack
    nc = bass.Bass("TRN2", target_bir_lowering=False)
    g = {}
    for name, shape in IN_SHAPES.items():
        g[name] = nc.dram_tensor(name, shape, F32, kind="ExternalInput").ap()
    for name, shape in extra_in:
        g[name] = nc.dram_tensor(name, shape, F32, kind="ExternalInput").ap()
    for name, shape in outs:
        g[name] = nc.dram_tensor(name, shape, F32, kind="ExternalOutput").ap()
    g["ye_scr"] = nc.dram_tensor("ye_scr", [16, 128, 2, D], BF16).ap()
    g["ys_scr"] = nc.dram_tensor("ys_scr", [2, 2304, 1536], F32).ap()
    g["z_scr"] = nc.dram_tensor("z_scr", [2304, 512], F32).ap()
    for nm, rows in (("xm_lat", 2048), ("xm_ctx", 256), ("x1_lat", 2048), ("x1_ctx", 256), ("x2_lat", 2048)):
        g[nm] = nc.dram_tensor(nm, [rows, D], F32).ap()
    kb = KB(nc)
    with ExitStack() as es:
        c = load_consts(kb, nc, es, g)
        c.fnT = es.enter_context(nc.sbuf_tensor("c_fnT", [128, 8], F32))
        kb.dma("sp", c.fnT[:], g["fnT"][:, :], writes=["c_fnT"])
        mods = prologue(kb, nc, es, g, c)
        for st in stages:
            if st == "moe0l_test":
                moe_stage(kb, nc, g, c, mods, 0, 0, g["t_in"], g["out"], 2048)
            elif st == "moe0c_test":
                moe_stage(kb, nc, g, c, mods, 0, 1, g["t_in"], g["out"], 256)
            elif st == "moe1l_test":
                moe_stage(kb, nc, g, c, mods, 1, 0, g["t_in"], g["out"], 2048, final_norm=True)
            elif st == "all":
                mixer_stage(kb, nc, g, c, mods, g["x"], g["ctx"], g["xm_lat"], g["xm_ctx"])
                moe_stage(kb, nc, g, c, mods, 0, 0, g["xm_lat"], g["x1_lat"], 2048)
                moe_stage(kb, nc, g, c, mods, 0, 1, g["xm_ctx"], g["x1_ctx"], 256)
                attn_stage(kb, nc, g, c, mods, g["x1_lat"], g["x1_ctx"], g["x2_lat"])
                moe_stage(kb, nc, g, c, mods, 1, 0, g["x2_lat"], g["out"], 2048, final_norm=True)
            elif st == "mixer_test":
                mixer_stage(kb, nc, g, c, mods, g["x"], g["ctx"], g["out"], g["out2"])
            elif st == "attn_test":
                attn_stage(kb, nc, g, c, mods, g["t_in"], g["t_in2"], g["out"])
            elif st == "mods_test":
                for l in range(2):
                    kb.dma("sp", g["out"][l * 128:(l + 1) * 128, 0:96], mods[l][:].rearrange("p a b -> p (a b)"),
                           reads=[f"mod{l}"], writes=[("o", l)])
        kb.finish([])
    return nc, kb


def rope_tables():
    quarter = 16
    inv = (10000.0 ** (-np.arange(quarter, dtype=np.float32) / quarter)).astype(np.float32)
    t = np.arange(2048)
    row = (t // 64).astype(np.float32)
    col = (t % 64).astype(np.float32)
    C = np.zeros((128, 2048), np.float32)
    S = np.zeros((128, 2048), np.float32)
    perm = np.zeros((128, 128), np.float32)
    for p in range(128):
        d = p % 64
        pos = row if d < 32 else col
        i = d % 16
        ang = (pos * inv[i]).astype(np.float32)
        C[p] = np.cos(ang)
        second = (d % 32) >= 16
        S[p] = np.sin(ang) if second else -np.sin(ang)
        partner = p - 16 if second else p + 16
        perm[partner, p] = 1.0
    return C, S, perm


def attn_stage(kb, nc, g, c, mods, x_lat_in, x_ctx_in, x_out):
    layer = 1
    mod = mods[layer]
    NT = 18
    from contextlib import ExitStack
    with ExitStack() as es:
        def sb(name, shape, dt):
            return es.enter_context(nc.sbuf_tensor(f"at_{name}", shape, dt))
        Gbc = sb("G", [128, D], F32)
        Sbc = sb("S", [128, D], F32)
        xb = [sb("x0", [128, D], F32), sb("x1", [128, D], F32)]
        hb = [sb("h0", [128, D], BF16), sb("h1", [128, D], BF16)]
        stt = [sb("st0", [128, 4], F32), sb("st1", [128, 4], F32)]
        hT = sb("hT", [128, 8, 2304], BF16)
        win = sb("win", [128, 8, 1792], BF16)
        wo = sb("wo", [128, 8, D], BF16)
        stg = [sb("stg0", [128, D], F32), sb("stg1", [128, D], F32)]
        Ct = sb("Ct", [128, 2048], F32)
        St = sb("St", [128, 2048], F32)
        perm = sb("perm", [128, 128], F32)
        qraw = [sb("qraw0", [128, 512], F32), sb("qraw1", [128, 512], F32)]
        rt1 = [sb("rt10", [128, 512], F32), sb("rt11", [128, 512], F32)]
        qT = sb("qT", [128, 8, 2048], BF16)
        kT = sb("kT", [128, 4, 2304], BF16)
        V = sb("V", [128, NT, 4, 65], BF16)
        mL = sb("mL", [128, 512], BF16)
        mU = sb("mU", [128, 512], BF16)
        esink = sb("esink", [128, 16], F32)
        PT = [sb(f"PT{i}", [128, 512], BF16) for i in range(2)]
        osb = stg[0]
        den = sb("den", [128, 4], F32)
        oT = sb("oT", [128, 8, 128], BF16)
        tmpo = qraw
        ps = [es.enter_context(nc.psum_tensor(f"at_ps{i}", [128, 512], F32)) for i in range(8)]

        kb.dma("sp", Ct[:], g["k_ropeC"][:, :], writes=["Ct"])
        kb.dma("sp", St[:], g["k_ropeS"][:, :], writes=["St"])
        kb.dma("sp", perm[:], g["k_perm"][:, :], writes=["perm"])
        kb.dma("sp", esink[:], g["o_sink"].partition_broadcast(128), writes=["esink"])
        kb.op("act", lambda e: e.activation(out=esink[:], in_=esink[:], func=AF.Exp), reads=["esink"], writes=["esink"])
        kb.dma("sp", stg[0][:, 0:512], g["k_mL"][:, :], writes=["stg0"])
        kb.op("dve", lambda e: e.tensor_copy(out=mL[:], in_=stg[0][:, 0:512]), reads=["stg0"], writes=["mL"])
        kb.dma("sp", stg[1][:, 0:512], g["k_mU"][:, :], writes=["stg1"])
        kb.op("dve", lambda e: e.tensor_copy(out=mU[:], in_=stg[1][:, 0:512]), reads=["stg1"], writes=["mU"])
        make_bc(kb, nc, c, lambda kc: mod[:, 1 * 8 + kc, 1:2], Gbc, ps[0], "Gbc", [f"mod{layer}"])
        make_bc(kb, nc, c, lambda kc: mod[:, 0 * 8 + kc, 1:2], Sbc, ps[0], "Sbc", [f"mod{layer}"])

        wcnt = [0]

        def load_rows(src_ap, dst_fn, ncols, key):
            for kc in range(8):
                j = wcnt[0] % 2
                wcnt[0] += 1
                kb.dma(["sp", "act"][kc % 2], stg[j][:, 0:ncols], src_ap[:, kc, :], writes=[f"stg{j}"])
                ce = ["dve", "pool"][wcnt[0] % 2]
                kb.op(ce, lambda e, j=j, kc=kc: e.tensor_copy(out=dst_fn(kc), in_=stg[j][:, 0:ncols]),
                      reads=[f"stg{j}"], writes=[key])
        wv = g["o_w_in2"].rearrange("(kc p) n -> p kc n", p=128)
        load_rows(wv[:, :, 0:1024], lambda kc: win[:, kc, 0:1024], 1024, "win")
        load_rows(wv[:, :, 1024:1792], lambda kc: win[:, kc, 1024:1792], 768, "win")
        load_rows(g["o_w_out"][0].rearrange("(kc p) n -> p kc n", p=128), lambda kc: wo[:, kc, :], 1024, "wo")

        for i in range(NT):
            b = i % 2
            if i == 2:
                kb.barrier()
                make_bc(kb, nc, c, lambda kc: mod[:, 1 * 8 + kc, 0:1], Gbc, ps[0], "Gbc", [f"mod{layer}"])
                make_bc(kb, nc, c, lambda kc: mod[:, 0 * 8 + kc, 0:1], Sbc, ps[0], "Sbc", [f"mod{layer}"])
            src = x_ctx_in[i * 128:(i + 1) * 128, :] if i < 2 else x_lat_in[(i - 2) * 128:(i - 1) * 128, :]
            kb.dma("sp", xb[b][:], src, writes=[f"xb{b}"])
            norm_tile(kb, nc, xb[b][:], f"xb{b}", stt[b], Gbc, Sbc, stg[b][:], f"stg{b}")
            kb.op("act", lambda e, b=b: e.copy(out=hb[b][:], in_=stg[b][:]), reads=[f"stg{b}"], writes=[f"hb{b}"])
            for half in range(2):
                pst = ps[half][:].bitcast(BF16)
                for q in range(4):
                    kc = half * 4 + q
                    kb.op("pe", lambda e, kc=kc, q=q, b=b, pst=pst: e.transpose(
                        out=pst[:, q * 128:(q + 1) * 128], in_=hb[b][:, kc * 128:(kc + 1) * 128],
                        identity=c.identb[:]), reads=[f"hb{b}", "c_identb"], writes=[f"ps{half}"])
                kb.op("dve" if half == 0 else "pool" if False else "act",
                      (lambda e, half=half, pst=pst, i=i: e.tensor_copy(
                          out=hT[:, half * 4:(half + 1) * 4, i * 128:(i + 1) * 128],
                          in_=pst[:, 0:512].rearrange("p (q t) -> p q t", q=4))) if half == 0 else
                      (lambda e, half=half, pst=pst, i=i: e.copy(
                          out=hT[:, half * 4:(half + 1) * 4, i * 128:(i + 1) * 128],
                          in_=pst[:, 0:512].rearrange("p (q t) -> p q t", q=4))),
                      reads=[f"ps{half}"], writes=["hT"])

        import os
        APH = int(os.environ.get("ATT_PH", "9"))
        if APH < 1:
            kb.barrier(); return
        nb = 0
        for nq in range(8):
            for tb in range(4):
                b = nb % 2
                nb += 1
                pq = ps[2 + b]
                t0 = 256 + tb * 512
                for kc in range(8):
                    kb.op("pe", lambda e, kc=kc, nq=nq, t0=t0, pq=pq: e.matmul(
                        pq[:], lhsT=win[:, kc, nq * 128:(nq + 1) * 128], rhs=hT[:, kc, t0:t0 + 512],
                        start=(kc == 0), stop=(kc == 7)), reads=["win", "hT"], writes=[f"ps{2 + b}"])
                kb.op("act", lambda e, b=b, pq=pq: e.copy(out=qraw[b][:], in_=pq[:]), reads=[f"ps{2 + b}"],
                      writes=[f"qraw{b}"])
                pw = ps[4 + b]
                kb.op("pe", lambda e, b=b, pw=pw: e.matmul(pw[:], lhsT=perm[:], rhs=qraw[b][:], start=True, stop=True),
                      reads=["perm", f"qraw{b}"], writes=[f"ps{4 + b}"])
                cs_ = slice(tb * 512, (tb + 1) * 512)
                kb.op("dve", lambda e, b=b, pw=pw, cs_=cs_: e.scalar_tensor_tensor(
                    out=rt1[b][:], in0=pw[:], scalar=0.125, in1=St[:, cs_], op0=ALU.mult, op1=ALU.mult),
                      reads=[f"ps{4 + b}", "St"], writes=[f"rt1{b}"])
                kb.op("pool", lambda e, b=b, cs_=cs_: e.tensor_tensor(out=qraw[b][:], in0=qraw[b][:], in1=Ct[:, cs_],
                                                                    op=ALU.mult),
                      reads=[f"qraw{b}", "Ct"], writes=[f"qraw{b}"])
                kb.op("dve", lambda e, b=b, nq=nq, cs_=cs_: e.scalar_tensor_tensor(
                    out=qT[:, nq, cs_], in0=qraw[b][:], scalar=0.125, in1=rt1[b][:], op0=ALU.mult, op1=ALU.add),
                    reads=[f"qraw{b}", f"rt1{b}"], writes=["qT"])
        if APH < 2:
            kb.barrier(); return
        for hk in range(4):
            for tb in range(5):
                b = nb % 2
                nb += 1
                pq = ps[2 + b]
                t0 = 0 if tb == 0 else 256 + (tb - 1) * 512
                tw = 256 if tb == 0 else 512
                for kc in range(8):
                    kb.op("pe", lambda e, kc=kc, hk=hk, t0=t0, tw=tw, pq=pq: e.matmul(
                        pq[:, 0:tw], lhsT=win[:, kc, 1024 + hk * 128:1024 + (hk + 1) * 128],
                        rhs=hT[:, kc, t0:t0 + tw], start=(kc == 0), stop=(kc == 7)),
                        reads=["win", "hT"], writes=[f"ps{2 + b}"])
                if tb == 0:
                    kb.op("act", lambda e, hk=hk, pq=pq: e.copy(out=kT[:, hk, 0:256], in_=pq[:, 0:256]),
                          reads=[f"ps{2 + b}"], writes=["kT"])
                    continue
                kb.op("act", lambda e, b=b, pq=pq: e.copy(out=qraw[b][:], in_=pq[:]), reads=[f"ps{2 + b}"],
                      writes=[f"qraw{b}"])
                pw = ps[4 + b]
                kb.op("pe", lambda e, b=b, pw=pw: e.matmul(pw[:], lhsT=perm[:], rhs=qraw[b][:], start=True, stop=True),
                      reads=["perm", f"qraw{b}"], writes=[f"ps{4 + b}"])
                cs_ = slice((tb - 1) * 512, tb * 512)
                kb.op("dve", lambda e, b=b, pw=pw, cs_=cs_: e.tensor_tensor(out=rt1[b][:], in0=pw[:], in1=St[:, cs_],
                                                                         op=ALU.mult),
                      reads=[f"ps{4 + b}", "St"], writes=[f"rt1{b}"])
                kb.op("pool", lambda e, b=b, cs_=cs_: e.tensor_tensor(out=qraw[b][:], in0=qraw[b][:], in1=Ct[:, cs_],
                                                                    op=ALU.mult),
                      reads=[f"qraw{b}", "Ct"], writes=[f"qraw{b}"])
                kb.op("dve", lambda e, b=b, hk=hk, t0=t0: e.tensor_tensor(
                    out=kT[:, hk, t0:t0 + 512], in0=qraw[b][:], in1=rt1[b][:], op=ALU.add),
                    reads=[f"qraw{b}", f"rt1{b}"], writes=["kT"])
        if APH < 3:
            kb.barrier(); return
        kb.op("pool", lambda e: e.memset(V[:], 1.0), writes=["V"])
        for i in range(NT):
            b = nb % 2
            nb += 1
            pq = ps[2 + b]
            for kc in range(8):
                kb.op("pe", lambda e, kc=kc, i=i, pq=pq: e.matmul(
                    pq[:, 0:256], lhsT=hT[:, kc, i * 128:(i + 1) * 128], rhs=win[:, kc, 1536:1792],
                    start=(kc == 0), stop=(kc == 7)), reads=["win", "hT"], writes=[f"ps{2 + b}"])
            kb.op("act", lambda e, i=i, pq=pq: e.copy(out=V[:, i, :, 0:64],
                                                     in_=pq[:, 0:256].rearrange("p (h d) -> p h d", h=4)),
                  reads=[f"ps{2 + b}"], writes=["V"])

        if APH < 4:
            kb.barrier(); return
        make_bc(kb, nc, c, lambda kc: mod[:, 2 * 8 + kc, 0:1], Gbc, ps[0], "Gbc", [f"mod{layer}"])
        nsb = 0
        for n in range(16):
            kb.dma("sp", xb[n % 2][:], x_lat_in[n * 128:(n + 1) * 128, :], writes=[f"xb{n % 2}"])
            for hk in range(4):
                tiles = []
                if n > 0:
                    tiles.append((2 + n - 1, mL, "mL"))
                tiles.append((2 + n, None, None))
                if n < 15:
                    tiles.append((2 + n + 1, mU, "mU"))
                tiles.append((0, None, None))
                tiles.append((1, None, None))
                po = ps[6 + (n * 4 + hk) % 2]
                pok = f"ps{6 + (n * 4 + hk) % 2}"
                for ti, (kt, msk, mk) in enumerate(tiles):
                    sbk = nsb % 2
                    nsb += 1
                    pSa, pSb = ps[1 + 2 * sbk], ps[2 + 2 * sbk]
                    ka, kbk = f"ps{1 + 2 * sbk}", f"ps{2 + 2 * sbk}"
                    for gq in range(4):
                        hq = hk * 4 + gq
                        bp = (hq % 2) * 64
                        pS = pSa if bp == 0 else pSb
                        kb.op("pe", lambda e, gq=gq, hq=hq, bp=bp, kt=kt, pS=pS, hk=hk, n=n: e.matmul(
                            pS[:, (gq // 2) * 128:(gq // 2 + 1) * 128], lhsT=kT[bp:bp + 64, hk, kt * 128:(kt + 1) * 128],
                            rhs=qT[bp:bp + 64, hq // 2, n * 128:(n + 1) * 128], start=True, stop=True),
                            reads=["kT", "qT"], writes=[ka if bp == 0 else kbk])
                    ptv = PT[sbk][:].rearrange("p (a b q) -> p a b q", a=2, b=2)
                    kb.op("act", lambda e, pSa=pSa, ptv=ptv: e.activation(
                        out=ptv[:, :, 0, :], in_=pSa[:, 0:256].rearrange("p (a q) -> p a q", a=2), func=AF.Exp),
                        reads=[ka], writes=[f"PT{sbk}"])
                    kb.op("act", lambda e, pSb=pSb, ptv=ptv: e.activation(
                        out=ptv[:, :, 1, :], in_=pSb[:, 0:256].rearrange("p (a q) -> p a q", a=2), func=AF.Exp),
                        reads=[kbk], writes=[f"PT{sbk}"])
                    ADBG = int(os.environ.get("ATT_DBG", "9"))
                    if ADBG < 2:
                        continue
                    if msk is not None:
                        kb.op("dve", lambda e, sbk=sbk, msk=msk: e.tensor_tensor(out=PT[sbk][:], in0=PT[sbk][:],
                                                                               in1=msk[:], op=ALU.mult),
                              reads=[f"PT{sbk}", mk], writes=[f"PT{sbk}"])
                    for gq in range(4):
                        kb.op("pe", lambda e, gq=gq, sbk=sbk, kt=kt, hk=hk, po=po, ti=ti: e.matmul(
                            po[:, gq * 65:(gq + 1) * 65], lhsT=PT[sbk][:, gq * 128:(gq + 1) * 128],
                            rhs=V[:, kt, hk, :], start=(ti == 0 and gq == 0), stop=(ti == len(tiles) - 1),
                            skip_group_check=True), reads=[f"PT{sbk}", "V"], writes=[pok])
                if ADBG < 3:
                    continue
                pov = po[:, 0:260].rearrange("p (g d) -> p g d", g=4)
                kb.op("dve", lambda e, pov=pov, hk=hk: e.tensor_tensor(
                    out=den[:], in0=pov[:, :, 64], in1=esink[:, hk * 4:(hk + 1) * 4], op=ALU.add),
                    reads=[pok, "esink"], writes=["den"])
                kb.op("dve", lambda e: e.reciprocal(out=den[:], in_=den[:]), reads=["den"], writes=["den"])
                kb.op("dve", lambda e, pov=pov, hk=hk: e.tensor_tensor(
                    out=osb[:, hk * 256:(hk + 1) * 256].rearrange("p (g d) -> p g d", g=4), in0=pov[:, :, 0:64],
                    in1=den[:].unsqueeze(2).broadcast_to([128, 4, 64]), op=ALU.mult),
                    reads=[pok, "den"], writes=["stg0"])
            if ADBG < 4:
                continue
            kb.op("act", lambda e: e.copy(out=hb[0][:], in_=osb[:]), reads=["stg0"], writes=["hb0"])
            pst = ps[0][:].bitcast(BF16)
            for kc in range(8):
                kb.op("pe", lambda e, kc=kc, pst=pst: e.transpose(
                    out=pst[:, kc * 128:(kc + 1) * 128], in_=hb[0][:, kc * 128:(kc + 1) * 128], identity=c.identb[:]),
                    reads=["hb0", "c_identb"], writes=["ps0"])
            kb.op("act", lambda e, pst=pst: e.copy(out=oT[:], in_=pst[:].rearrange("p (k t) -> p k t", k=8)),
                  reads=["ps0"], writes=["oT"])
            for half in range(2):
                pp = ps[5] if half == 0 else ps[0]
                for kc in range(8):
                    kb.op("pe", lambda e, kc=kc, half=half, pp=pp: e.matmul(
                        pp[:], lhsT=oT[:, kc, :], rhs=wo[:, kc, half * 512:(half + 1) * 512],
                        start=(kc == 0), stop=(kc == 7)), reads=["oT", "wo"], writes=["ps5" if half == 0 else "ps0"])
                sl = slice(half * 512, (half + 1) * 512)
                kb.op("dve", lambda e, half=half, pp=pp, sl=sl: e.tensor_tensor(
                    out=tmpo[half][:], in0=pp[:], in1=Gbc[:, sl], op=ALU.mult),
                    reads=["ps5" if half == 0 else "ps0", "Gbc"], writes=[f"qraw{half}"])
                kb.op("pool", lambda e, half=half, sl=sl, n=n: e.tensor_tensor(
                    out=xb[n % 2][:, sl], in0=xb[n % 2][:, sl], in1=tmpo[half][:], op=ALU.add),
                    reads=[f"qraw{half}", f"xb{n % 2}"], writes=[f"xb{n % 2}"])
            kb.dma("sp", x_out[n * 128:(n + 1) * 128, :], xb[n % 2][:], reads=[f"xb{n % 2}"],
                   writes=[("dram", x_out.tensor.name, n)])
        kb.barrier()


def dplr_scan(kb, nc, c, T, dk, rT, kkT, kT, bT, vT, Pinc, prodT, store_cb, hk_, Gb=None, rA=None, kkA=None, bp=0):
    ps = T["ps"]
    rA = rT if rA is None else rA
    kkA = kkT if kkA is None else kkA
    ident, maskA, maskT, ident64 = c.ident, T["maskA"], T["maskT"], c.ident
    ST = T["ST"]
    kb.op("dve", lambda e: e.memset(ST[bp:bp + dk, 0:dk], 0.0), writes=["ST"])
    CH = CHUNK
    NLV = {64: 5, 128: 6}[CH]
    NCH = len(CHUNK_COLS)
    GB = 2

    def inv_gen(g0):
        grp = list(range(g0, min(NCH, g0 + GB)))
        par = (g0 // GB) % 2
        for ci in grp:
            s = ci % GB + GB * par
            cs = slice(CHUNK_COLS[ci], CHUNK_COLS[ci] + CH)
            pa = ps[s % 2]
            pk = f"ps{s % 2}"
            pn = ps[2 + s % 2]
            pnk = f"ps{2 + s % 2}"
            for j, (l, r) in enumerate(((bT, kkA), (kT, kkA), (bT, rA), (kT, rA), (kkA, bT))):
                if j < 4:
                    kb.op("pe", lambda e, j=j, l=l, r=r, cs=cs, pa=pa: e.matmul(
                        pa[0:CH, j * CH:(j + 1) * CH], lhsT=l[bp:bp + dk, cs], rhs=r[bp:bp + dk, cs], start=True, stop=True),
                        reads=[hk_], writes=[pk])
                else:
                    kb.op("pe", lambda e, l=l, r=r, cs=cs, pn=pn: e.matmul(
                        pn[0:CH, 256:256 + CH], lhsT=l[bp:bp + dk, cs], rhs=r[bp:bp + dk, cs], start=True, stop=True),
                        reads=[hk_], writes=[pnk])
            mA, mT_, mAk, mTk = maskA[0:CH, :], maskT[0:CH, :], "maskA", "maskT"
            if Gb is not None:
                c0_ = CHUNK_COLS[ci]
                kb.op("pe", lambda e, cs=cs, pn=pn: e.transpose(out=pn[0:CH, 384:385], in_=Gb[0:1, cs],
                                                              identity=ident[0:1, 0:1]), reads=[hk_, "c_ident"], writes=[pnk])
                kb.op("act", lambda e, s=s, pn=pn: e.copy(out=T["Gc"][0:CH, s:s + 1], in_=pn[0:CH, 384:385]),
                      reads=[pnk], writes=[f"Gc{s}"])
                kb.op("dve", lambda e, s=s, cs=cs: e.tensor_scalar(out=T["Dt"][0:CH, s % GB, :], in0=Gb[0:CH, cs],
                                                                 scalar1=T["Gc"][0:CH, s:s + 1], scalar2=0.0,
                                                                 op0=ALU.subtract, op1=ALU.min),
                      reads=[hk_, f"Gc{s}"], writes=[f"Dt{s % GB}"])
                kb.op("act", lambda e, s=s: e.activation(out=T["Dt"][0:CH, s % GB, :], in_=T["Dt"][0:CH, s % GB, :], func=AF.Exp),
                      reads=[f"Dt{s % GB}"], writes=[f"Dt{s % GB}"])
                kb.op("dve", lambda e, s=s, cs=cs: e.tensor_scalar(out=T["Dts"][0:CH, s % GB, :], in0=Gb[0:CH, cs],
                                                                 scalar1=T["Gc"][0:CH, s:s + 1], scalar2=0.0,
                                                                 op0=ALU.subtract, op1=ALU.max),
                      reads=[hk_, f"Gc{s}"], writes=[f"Dts{s % GB}"])
                kb.op("act", lambda e, s=s: e.activation(out=T["Dts"][0:CH, s % GB, :], in_=T["Dts"][0:CH, s % GB, :], func=AF.Exp,
                                                         scale=-1.0), reads=[f"Dts{s % GB}"], writes=[f"Dts{s % GB}"])
                kb.op("dve", lambda e, s=s: e.tensor_tensor(
                    out=T["mD"][0:CH, s % GB, :].rearrange("p (a t) -> p a t", a=4),
                    in0=maskA[0:CH, :].rearrange("p (a t) -> p a t", a=4),
                    in1=T["Dt"][0:CH, s % GB, :].unsqueeze(1).broadcast_to([CH, 4, CH]), op=ALU.mult),
                    reads=["maskA", f"Dt{s % GB}"], writes=[f"mD{s % GB}"])
                kb.op("pool", lambda e, s=s: e.tensor_tensor(out=T["Dts"][0:CH, s % GB, :], in0=T["Dts"][0:CH, s % GB, :],
                                                            in1=maskT[0:CH, :], op=ALU.mult),
                      reads=["maskT", f"Dts{s % GB}"], writes=[f"Dts{s % GB}"])
                kb.op("act", lambda e, s=s, c0_=c0_: e.activation(out=T["dL"][0:CH, s:s + 1], in_=T["Gc"][0:CH, s:s + 1],
                                                                func=AF.Exp, scale=-1.0, bias=Gb[0:CH, c0_ + CH - 1:c0_ + CH]),
                      reads=[f"Gc{s}", hk_], writes=[f"dL{s}"])
                mA, mT_, mAk, mTk = T["mD"][0:CH, s % GB, :], T["Dts"][0:CH, s % GB, :], f"mD{s % GB}", f"Dts{s % GB}"
            kb.op("dve", lambda e, s=s, pa=pa, mA=mA: e.tensor_tensor(out=T["AM"][0:CH, s, :], in0=pa[0:CH, 0:4 * CH],
                                                                     in1=mA, op=ALU.mult),
                  reads=[pk, mAk], writes=[f"AM{s}"])
            kb.op("dve", lambda e, s=s, pn=pn, mT_=mT_: e.tensor_tensor(out=T["MM"][0][0:CH, s, CH:2 * CH],
                                                                       in0=pn[0:CH, 256:256 + CH], in1=mT_, op=ALU.mult),
                  reads=[pnk, mTk], writes=[f"MM0_{s}"])
            kb.op("pool", lambda e, s=s: e.tensor_copy(out=T["MM"][0][0:CH, s, 0:CH], in_=T["AM"][0:CH, s, 0:CH]),
                  reads=[f"AM{s}"], writes=[f"MM0_{s}"])
            kb.op("pool", lambda e, s=s: e.tensor_tensor(out=T["Q"][0][0:CH, s, :], in0=T["AM"][0:CH, s, 0:CH],
                                                        in1=ident64[0:CH, 0:CH], op=ALU.add),
                  reads=[f"AM{s}", "c_ident"], writes=[f"Q0_{s}"])
            yield
        for lv in range(NLV):
            a, b = lv % 2, (lv + 1) % 2
            last = lv == NLV - 1
            for ci in grp:
                s = ci % GB + GB * par
                pm = ps[2 + s % 2]
                pmk = f"ps{2 + s % 2}"
                MMa = T["MM"][a]
                kb.op("pe", lambda e, s=s, pm=pm, MMa=MMa: e.matmul(
                    pm[0:CH, 0:CH], lhsT=MMa[0:CH, s, CH:2 * CH], rhs=MMa[0:CH, s, 0:CH], start=True, stop=True),
                    reads=[f"MM{a}_{s}"], writes=[pmk])
                kb.op("pe", lambda e, s=s, pm=pm, MMa=MMa: e.matmul(
                    pm[0:CH, CH:2 * CH], lhsT=MMa[0:CH, s, 0:CH], rhs=MMa[0:CH, s, CH:2 * CH], start=True, stop=True),
                    reads=[f"MM{a}_{s}"], writes=[pmk])
                kb.op("act", lambda e, s=s, pm=pm, b=b: e.copy(out=T["MM"][b][0:CH, s, :], in_=pm[0:CH, 0:2 * CH]),
                      reads=[pmk], writes=[f"MM{b}_{s}"])
                yield
            for ci in grp:
                s = ci % GB + GB * par
                pq = ps[4 + s % 2]
                pqk = f"ps{4 + s % 2}"
                kb.op("pe", lambda e, s=s, pq=pq, b=b, a=a: e.matmul(
                    pq[0:CH, 0:CH], lhsT=T["MM"][b][0:CH, s, CH:2 * CH], rhs=T["Q"][a][0:CH, s, :], start=True, stop=True),
                    reads=[f"MM{b}_{s}", f"Q{a}_{s}"], writes=[pqk])
                kb.op("dve", lambda e, s=s, pq=pq, a=a, b=b: e.tensor_tensor(
                    out=T["Q"][b][0:CH, s, :], in0=pq[0:CH, 0:CH], in1=T["Q"][a][0:CH, s, :], op=ALU.add),
                    reads=[pqk, f"Q{a}_{s}"], writes=[f"Q{b}_{s}"])
                yield

    def chain_gen(g0):
        grp = list(range(g0, min(NCH, g0 + GB)))
        par = (g0 // GB) % 2
        QF = T["Q"][NLV % 2]
        for ci in grp:
            s = ci % GB + GB * par
            c0 = CHUNK_COLS[ci]
            cs = slice(c0, c0 + CH)
            AM = T["AM"]
            pt = ps[6]
            for j, src in enumerate((vT, kT, bT)):
                kb.op("pe", lambda e, j=j, src=src, cs=cs, pt=pt: e.transpose(
                    out=pt[0:CH, j * dk:(j + 1) * dk], in_=src[bp:bp + dk, cs], identity=ident[bp:bp + dk, bp:bp + dk]),
                    reads=[hk_, "c_ident"], writes=["ps6"])
            TM = T["TM"][ci % 2]
            tmk = f"TM{ci % 2}"
            kb.op("act", lambda e, TM=TM, pt=pt: e.copy(out=TM[0:CH, 0:3 * dk], in_=pt[0:CH, 0:3 * dk]),
                  reads=["ps6"], writes=[tmk])
            yield
            Vtm, Ktm, Btm = TM[0:CH, 0:dk], TM[0:CH, dk:2 * dk], TM[0:CH, 2 * dk:3 * dk]
            if Gb is not None:
                kb.op("dve", lambda e, TM=TM, s=s: e.tensor_scalar(out=TM[0:CH, dk:3 * dk], in0=TM[0:CH, dk:3 * dk],
                                                                 scalar1=T["dL"][0:CH, s:s + 1], scalar2=None, op0=ALU.mult),
                      reads=[tmk, f"dL{s}"], writes=[tmk])
            pr = ps[7]
            kb.op("pe", lambda e, cs=cs, pr=pr: e.matmul(pr[0:CH, 0:dk], lhsT=kkT[bp:bp + dk, cs], rhs=ST[bp:bp + dk, 0:dk],
                                                       start=True, stop=False), reads=[hk_, "ST"], writes=["ps7"])
            kb.op("pe", lambda e, s=s, pr=pr, Vtm=Vtm: e.matmul(pr[0:CH, 0:dk], lhsT=AM[0:CH, s, CH:2 * CH], rhs=Vtm,
                                                              start=False, stop=True),
                  reads=[f"AM{s}", tmk], writes=["ps7"])
            yield
            kb.op("dve", lambda e, pr=pr: e.tensor_scalar(out=T["nR"][0:CH, 0:dk], in0=pr[0:CH, 0:dk], scalar1=-1.0,
                                                         scalar2=None, op0=ALU.mult), reads=["ps7"], writes=["nR"])
            yield
            kb.op("pe", lambda e, s=s, pr=pr: e.matmul(pr[0:CH, 128:128 + dk], lhsT=QF[0:CH, s, :], rhs=T["nR"][0:CH, 0:dk],
                                                     start=True, stop=True), reads=[f"Q{NLV % 2}_{s}", "nR"], writes=["ps7"])
            yield
            kb.op("act", lambda e, pr=pr: e.copy(out=T["U"][0:CH, 0:dk], in_=pr[0:CH, 128:128 + dk]),
                  reads=["ps7"], writes=["U"])
            yield
            U = T["U"]
            kb.op("pe", lambda e, cs=cs, pr=pr: e.matmul(pr[0:CH, 256:256 + dk], lhsT=rT[bp:bp + dk, cs], rhs=ST[bp:bp + dk, 0:dk],
                                                       start=True, stop=False), reads=[hk_, "ST"], writes=["ps7"])
            kb.op("pe", lambda e, s=s, pr=pr: e.matmul(pr[0:CH, 256:256 + dk], lhsT=AM[0:CH, s, 2 * CH:3 * CH],
                                                     rhs=U[0:CH, 0:dk], start=False, stop=False),
                  reads=[f"AM{s}", "U"], writes=["ps7"])
            kb.op("pe", lambda e, s=s, pr=pr, Vtm=Vtm: e.matmul(pr[0:CH, 256:256 + dk], lhsT=AM[0:CH, s, 3 * CH:4 * CH],
                                                              rhs=Vtm, start=False, stop=True),
                  reads=[f"AM{s}", tmk], writes=["ps7"])
            kb.op("pe", lambda e, pr=pr, Btm=Btm: e.matmul(pr[bp:bp + dk, 384:384 + dk], lhsT=Btm, rhs=U[0:CH, 0:dk],
                                                         start=True, stop=False), reads=[tmk, "U"], writes=["ps7"])
            kb.op("pe", lambda e, pr=pr, Ktm=Ktm, Vtm=Vtm: e.matmul(pr[bp:bp + dk, 384:384 + dk], lhsT=Ktm, rhs=Vtm,
                                                                  start=False, stop=True),
                  reads=[tmk], writes=["ps7"])
            yield
            Ysb = T["Y"][ci % 2]
            yk = f"Y{ci % 2}"
            kb.op("act", lambda e, pr=pr, Ysb=Ysb: e.copy(out=Ysb[0:CH, 0:dk], in_=pr[0:CH, 256:256 + dk]),
                  reads=["ps7"], writes=[yk])
            if Gb is not None:
                kb.op("dve", lambda e, pr=pr, c0=c0: e.scalar_tensor_tensor(
                    out=ST[bp:bp + dk, 0:dk], in0=ST[bp:bp + dk, 0:dk], scalar=Pinc[bp:bp + dk, c0 + CH - 1:c0 + CH],
                    in1=pr[bp:bp + dk, 384:384 + dk], op0=ALU.mult, op1=ALU.add), reads=["ps7", "ST", hk_], writes=["ST"])
            else:
                kb.op("dve", lambda e, pr=pr: e.tensor_tensor(out=ST[bp:bp + dk, 0:dk], in0=pr[bp:bp + dk, 384:384 + dk],
                                                             in1=ST[bp:bp + dk, 0:dk], op=ALU.add),
                      reads=["ps7", "ST"], writes=["ST"])
                kb.op("dve", lambda e, c0=c0: e.tensor_scalar(out=ST[bp:bp + dk, 0:dk], in0=ST[bp:bp + dk, 0:dk],
                                                             scalar1=Pinc[bp:bp + dk, c0 + CH - 1:c0 + CH], scalar2=None,
                                                             op0=ALU.mult), reads=["ST", hk_], writes=["ST"])
            if prodT is not None:
                pb = ps[6]
                kb.op("pe", lambda e, cs=cs, pb=pb: e.matmul(pb[0:CH, 448:449], lhsT=prodT[bp:bp + dk, cs],
                                                           rhs=c.ones[bp:bp + dk, 0:1], start=True, stop=True),
                      reads=[hk_, "c_ones"], writes=["ps6"])
                kb.op("dve", lambda e, pb=pb, Ysb=Ysb, Vtm=Vtm: e.tensor_scalar(
                    out=Ysb[0:CH, dk:2 * dk], in0=Vtm, scalar1=pb[0:CH, 448:449], scalar2=None, op0=ALU.mult),
                    reads=["ps6", tmk], writes=[yk])
            store_cb(ci, Ysb, yk)
            yield

    from itertools import zip_longest
    for _ in inv_gen(0):
        pass
    for g0 in range(0, NCH, GB):
        gens = [chain_gen(g0)]
        if g0 + GB < NCH:
            gens.append(inv_gen(g0 + GB))
        for _ in zip_longest(*gens):
            pass


def mixer_stage(kb, nc, g, c, mods, x_lat_in, x_ctx_in, x_lat_out, x_ctx_out):
    from contextlib import ExitStack
    mod = mods[0]
    Ys = g["ys_scr"]
    Zs = g["z_scr"]
    with ExitStack() as es:
        def sb(name, shape, dt):
            return es.enter_context(nc.sbuf_tensor(f"mxs_{name}", shape, dt))
        hT = None
        es2 = ExitStack()

        def sb2(name, shape, dt):
            return es2.enter_context(nc.sbuf_tensor(f"mxs_{name}", shape, dt))
        hb = sb("hb", [128, D], BF16)
        stt = [sb("st0", [128, 4], F32), sb("st1", [128, 4], F32)]
        F11 = sb("F11", [128, TP], F32)
        Jb = sb("Jb", [128, 128], BF16)
        J32 = sb("J32", [128, 128], F32)
        par = sb("par", [64, 64], F32)
        parP = sb("parP", [128, 64], F32)
        bd = sb("bd", [128, 128], F32)
        par128 = sb("par128", [128, 112], F32)
        w2sb = sb("w2sb", [128, 2, 512], F32)
        sel = sb("sel", [16, 16, 128], F32)
        ST = sb("ST", [128, 128], F32)
        hT = es2.enter_context(nc.sbuf_tensor("mxs_hT", [128, 8, 2304], BF16))
        F = [sb2(f"F{i}", [128, TP], F32) for i in range(11)] + [F11]
        FK = [f"F{i}" for i in range(12)]
        Gbc = F[1][:, 0:D]
        Sbc = F[2][:, 0:D]
        h32 = F[3][:, 0:D]
        xb = [F[4][:, 0:D], F[5][:, 0:D]]
        rmask = sb2("rmask", [128, TP], BF16)
        rm32 = F[0]
        wsl = sb2("wsl", [128, 8, 128], BF16)
        T = {"ST": ST,
             "AM": sb2("AM", [CHUNK, 4, 4 * CHUNK], F32),
             "MM": [sb2("MMa", [CHUNK, 4, 2 * CHUNK], F32), sb2("MMb", [CHUNK, 4, 2 * CHUNK], F32)],
             "Q": [sb2("Qa", [CHUNK, 4, CHUNK], F32), sb2("Qb", [CHUNK, 4, CHUNK], F32)],
             "TM": [sb2("TMa", [CHUNK, 384], F32), sb2("TMb", [CHUNK, 384], F32)],
             "nR": sb2("nR", [CHUNK, 128], F32), "U": sb2("U", [CHUNK, 128], F32),
             "Y": [sb2("Ya", [CHUNK, 256], F32), sb2("Yb", [CHUNK, 256], F32)],
             "maskA": sb2("maskA", [CHUNK, 4 * CHUNK], F32), "maskT": sb2("maskT", [CHUNK, CHUNK], F32),
             "Gc": sb2("Gc", [CHUNK, 4], F32), "dL": sb2("dL", [CHUNK, 4], F32), "Dt": sb2("Dt", [CHUNK, 2, CHUNK], F32),
             "Dts": sb2("Dts", [CHUNK, 2, CHUNK], F32), "mD": sb2("mD", [CHUNK, 2, 4 * CHUNK], F32)}
        ps = [es.enter_context(nc.psum_tensor(f"mx_ps{i}", [128, 512], F32)) for i in range(8)]
        T["ps"] = ps
        kb.dma("sp", rm32[:], g["k_rmask"][:, :], writes=["F0"])
        kb.op("dve", lambda e: e.tensor_copy(out=rmask[:], in_=rm32[:]), reads=["F0"], writes=["rmask"])
        kb.dma("sp", J32[:], g["k_J"][:, :], writes=["J32"])
        kb.op("dve", lambda e: e.tensor_copy(out=Jb[:], in_=J32[:]), reads=["J32"], writes=["Jb"])
        kb.dma("sp", par[:], g["mx_par64"][:, :], writes=["par"])
        kb.dma("sp", par128[:], g["mx_par128"][:, :], writes=["par128"])
        kb.dma("sp", w2sb[:], g["mx_w2"][:, :, :], writes=["w2sb"])
        kb.dma("sp", parP[:], g["mx_parP"][:, :], writes=["parP"])
        kb.dma("sp", bd[:], g["k_bd"][:, :], writes=["bd"])
        kb.op("dve", lambda e: e.tensor_scalar(out=parP[:, 36:40], in0=parP[:, 32:36], scalar1=-1.0, scalar2=1.0,
                                               op0=ALU.mult, op1=ALU.add), reads=["parP"], writes=["parP"])
        kb.op("dve", lambda e: e.tensor_scalar(out=par128[:, 24:32], in0=par128[:, 16:24], scalar1=-1.0, scalar2=1.0,
                                               op0=ALU.mult, op1=ALU.add), reads=["par128"], writes=["par128"])
        T["zt"] = [sb2("zta", [128, 128], F32), sb2("ztb", [128, 128], F32)]
        kb.dma("sp", sel[:], g["k_sel"][:, :, :], writes=["sel"])
        kb.dma("sp", T["maskA"][:], g["k_maskA"][:, :], writes=["maskA"])
        kb.dma("sp", T["maskT"][:], g["k_maskT"][:, :], writes=["maskT"])
        for f in range(12):
            kb.op("pool", lambda e, f=f: e.memset(F[f][:], 0.0), reads=["rmask"] if f == 0 else [], writes=[FK[f]])
        wv = g["e_w_in2"].rearrange("(kc p) n -> p kc n", p=128)
        P64 = lambda j: par[:, j:j + 1]

        def project(c0, M, dst, dk_, evac_eng="act"):
            kb.dma("pool", wsl[:, :, 0:M], wv[:, :, c0:c0 + M], writes=["wsl"])
            for bi, (t0, tw, col0) in enumerate(BLOCKS):
                pb = ps[bi % 2]
                for kc in range(8):
                    kb.op("pe", lambda e, kc=kc, t0=t0, tw=tw, pb=pb: e.matmul(
                        pb[0:M, 0:tw], lhsT=wsl[:, kc, 0:M], rhs=hT[:, kc, t0:t0 + tw], start=(kc == 0), stop=(kc == 7)),
                        reads=["wsl", "hT"], writes=[f"ps{bi % 2}"])
                kb.op("act", lambda e, tw=tw, col0=col0, pb=pb: e.copy(out=dst[0:M, col0:col0 + tw], in_=pb[0:M, 0:tw]),
                      reads=[f"ps{bi % 2}"], writes=[dk_])

        def tshift(src, sk, dst, dk_, tmp, tk, P, mucol):
            n = TP - 2
            kb.op("dve", lambda e: e.tensor_tensor(out=tmp[0:P, 1:1 + n], in0=src[0:P, 0:n], in1=src[0:P, 2:2 + n],
                                                   op=ALU.add), reads=[sk], writes=[tk])
            kb.op("dve", lambda e: e.scalar_tensor_tensor(out=tmp[0:P, 1:1 + n], in0=tmp[0:P, 1:1 + n], scalar=0.5,
                                                          in1=src[0:P, 1:1 + n], op0=ALU.mult, op1=ALU.subtract),
                  reads=[sk, tk], writes=[tk])
            kb.op("dve", lambda e: e.scalar_tensor_tensor(out=dst[0:P, 1:1 + n], in0=tmp[0:P, 1:1 + n], scalar=mucol,
                                                         in1=src[0:P, 1:1 + n], op0=ALU.mult, op1=ALU.add),
                  reads=[sk, tk, "par", "par128"], writes=[dk_])

        DC = [(CTX0, 256), (LAT0, 2048)]

        def ew(eng, fn, reads, writes):
            kb.op(eng, fn, reads=reads, writes=writes)

        for d in range(2):
            for stream in (1, 0):
                kb.barrier()
                make_bc(kb, nc, c, lambda kc: mod[:, 1 * 8 + kc, stream:stream + 1], Gbc, ps[0], "Gbc", ["mod0"])
                make_bc(kb, nc, c, lambda kc: mod[:, 0 * 8 + kc, stream:stream + 1], Sbc, ps[0], "Sbc", ["mod0"])
                nt = 2 if stream == 1 else 16
                src = x_ctx_in if stream == 1 else x_lat_in
                base = 0 if stream == 1 else 256
                for i in range(nt):
                    b = i % 2
                    kb.dma("sp", xb[b][:], src[i * 128:(i + 1) * 128, :], writes=[f"xb{b}"])
                    norm_tile(kb, nc, xb[b][:], f"xb{b}", stt[b], Gbc, Sbc, h32, "h32")
                    kb.op("act", lambda e: e.copy(out=hb[:], in_=h32), reads=["h32"], writes=["hb"])
                    pos = base + (i if d == 0 else nt - 1 - i) * 128
                    for half in range(2):
                        for q in range(4):
                            kc = half * 4 + q
                            kb.op("pe", lambda e, kc=kc, q=q, half=half: e.matmul(
                                ps[2 + half][:, q * 128:(q + 1) * 128], lhsT=hb[:, kc * 128:(kc + 1) * 128],
                                rhs=(c.identb[:] if d == 0 else Jb[:]), start=True, stop=True),
                                reads=["hb", "c_identb", "Jb"], writes=[f"ps{2 + half}"])
                        kb.op("dve" if half == 0 else "act", (lambda e, half=half, pos=pos: e.tensor_copy(
                            out=hT[:, half * 4:(half + 1) * 4, pos:pos + 128],
                            in_=ps[2 + half][:].rearrange("p (q t) -> p q t", q=4))) if half == 0 else
                            (lambda e, half=half, pos=pos: e.copy(
                                out=hT[:, half * 4:(half + 1) * 4, pos:pos + 128],
                                in_=ps[2 + half][:].rearrange("p (q t) -> p q t", q=4))),
                            reads=[f"ps{2 + half}"], writes=["hT"])
                    if d == 0:
                        pass
            kb.barrier()

            def store_cb_factory(col0, dk_):
                def cb(ci, Ysb, yk):
                    r0 = ci * CHUNK
                    kb.dma("sp", Ys[d, r0:r0 + CHUNK, col0:col0 + dk_], Ysb[0:CHUNK, 0:dk_], reads=[yk],
                           writes=[("ys", d, ci, col0)])
                    if col0 < 512:
                        kb.dma("sp", Ys[d, r0:r0 + CHUNK, 1024 + col0:1024 + col0 + dk_], Ysb[0:CHUNK, dk_:2 * dk_],
                               reads=[yk], writes=[("ysb", d, ci, col0)])
                return cb

            project(1536 + d * 128, 128, F[0], FK[0])
            tshift(F[0], FK[0], F[10], FK[10], F[1], FK[1], 128, par128[:, d:d + 1])
            ew("act", lambda e: e.activation(out=F[10][0:64, :], in_=F[10][0:64, :], func=AF.Tanh), [FK[10]], [FK[10]])
            if d == 0:
                project(1792, 128, F[0], FK[0])
                tshift(F[0], FK[0], F[11], FK[11], F[1], FK[1], 128, par128[:, 2:3])
                ew("act", lambda e: e.activation(out=F[11][:], in_=F[11][:], func=AF.Sigmoid), [FK[11]], [FK[11]])
            for p in range(4):
                hk_ = f"pair{d}_{p}"
                PP = lambda j: parP[:, j:j + 1]
                for part, dst in ((0, 1), (1, 2), (2, 3)):
                    project(p * 384 + part * 128, 128, F[0], FK[0])
                    tshift(F[0], FK[0], F[dst], FK[dst], F[7], FK[7], 128, PP(p * 3 + part))
                for bi, (t0, tw, col0) in enumerate(BLOCKS):
                    cs = slice(col0, col0 + tw)
                    kb.op("pe", lambda e, cs=cs, tw=tw: e.matmul(ps[2][:, 0:tw], lhsT=w2sb[0:64, d, p * 128:(p + 1) * 128],
                                                               rhs=F[10][0:64, cs], start=True, stop=True),
                          reads=["w2sb", FK[10]], writes=["ps2"])
                    kb.op("pe", lambda e, cs=cs, tw=tw: e.matmul(ps[3][:, 0:tw], lhsT=w2sb[64:128, d, p * 128:(p + 1) * 128],
                                                               rhs=F[10][64:128, cs], start=True, stop=True),
                          reads=["w2sb", FK[10]], writes=["ps3"])
                    kb.op("act", lambda e, cs=cs, tw=tw: e.activation(out=F[4][:, cs], in_=ps[2][:, 0:tw],
                                                                    func=AF.Sigmoid, bias=PP(12 + d * 4 + p)),
                          reads=["ps2", "parP"], writes=[FK[4]])
                    kb.op("act", lambda e, cs=cs, tw=tw: e.activation(out=F[5][:, cs], in_=ps[3][:, 0:tw],
                                                                    func=AF.Sigmoid, bias=PP(20 + d * 4 + p)),
                          reads=["ps3", "parP"], writes=[FK[5]])
                kkc, kac, kac1, rkc = PP(28 + p), PP(32 + p), PP(36 + p), PP(40 + p)
                ew("dve", lambda e: e.tensor_scalar(out=F[7][:, :], in0=F[2][:, :], scalar1=kkc, scalar2=None,
                                                    op0=ALU.mult), [FK[2], "parP"], [FK[7]])
                for (a0_, n_) in DC:
                    ew("pool", lambda e, a0_=a0_, n_=n_: e.tensor_tensor(
                        out=F[0][:, a0_:a0_ + n_], in0=F[7][:, a0_:a0_ + n_], in1=F[7][:, a0_:a0_ + n_],
                        op=ALU.mult), [FK[7]], [FK[0]])
                for bi, (t0, tw, col0) in enumerate(BLOCKS):
                    cs = slice(col0, col0 + tw)
                    kb.op("pe", lambda e, cs=cs, tw=tw: e.matmul(ps[2][:, 0:tw], lhsT=bd[:, :],
                                                               rhs=F[0][:, cs], start=True, stop=True),
                          reads=["bd", FK[0]], writes=["ps2"])
                    kb.op("act", lambda e, cs=cs, tw=tw: e.activation(out=F[9][:, cs], in_=ps[2][:, 0:tw],
                                                                    func=AF.Sqrt, bias=c.eps6[:, 0:1]),
                          reads=["ps2", "c_eps"], writes=[FK[9]])
                for (a0_, n_) in DC:
                    cs = slice(a0_, a0_ + n_)
                    ew("dve", lambda e, cs=cs: e.reciprocal(out=F[9][:, cs], in_=F[9][:, cs]), [FK[9]], [FK[9]])
                    ew("dve", lambda e, cs=cs: e.tensor_tensor(out=F[6][:, cs], in0=F[7][:, cs], in1=F[9][:, cs],
                                                               op=ALU.mult), [FK[7], FK[9]], [FK[6]])
                    ew("pool", lambda e, cs=cs: e.tensor_scalar(out=F[7][:, cs], in0=F[5][:, cs], scalar1=kac,
                                                                scalar2=kac1, op0=ALU.mult, op1=ALU.add),
                       [FK[5], "parP"], [FK[7]])
                    ew("pool", lambda e, cs=cs: e.tensor_tensor(out=F[7][:, cs], in0=F[7][:, cs], in1=F[2][:, cs],
                                                                op=ALU.mult), [FK[7], FK[2]], [FK[7]])
                    ew("dve", lambda e, cs=cs: e.tensor_tensor(out=F[9][:, cs], in0=F[6][:, cs], in1=F[5][:, cs],
                                                               op=ALU.mult), [FK[6], FK[5]], [FK[9]])
                    ew("dve", lambda e, cs=cs: e.scalar_tensor_tensor(out=F[0][:, cs], in0=F[1][:, cs], scalar=rkc,
                                                                      in1=F[7][:, cs], op0=ALU.mult, op1=ALU.mult),
                       [FK[1], FK[7], "parP"], [FK[0]])
                ew("dve", lambda e: e.tensor_tensor_scan(out=F[8][:, :], data0=rmask[:, :], data1=F[4][:, :],
                                                         initial=0.0, op0=ALU.mult, op1=ALU.add),
                   ["rmask", FK[4]], [FK[8]])
                for (a0_, n_) in DC:
                    cs = slice(a0_, a0_ + n_)
                    ew("pool", lambda e, cs=cs: e.tensor_tensor(out=F[2][:, cs], in0=F[8][:, cs], in1=F[4][:, cs],
                                                                op=ALU.subtract), [FK[8], FK[4]], [FK[2]])
                    ew("act", lambda e, cs=cs: e.activation(out=F[2][:, cs], in_=F[2][:, cs], func=AF.Exp,
                                                            scale=-DECAY_K), [FK[2]], [FK[2]])
                    ew("dve", lambda e, cs=cs: e.tensor_tensor(out=F[6][:, cs], in0=F[6][:, cs], in1=F[2][:, cs],
                                                               op=ALU.mult), [FK[6], FK[2]], [FK[6]])
                    ew("act", lambda e, cs=cs: e.activation(out=F[2][:, cs], in_=F[8][:, cs], func=AF.Exp,
                                                            scale=DECAY_K), [FK[8], FK[2]], [FK[2]])
                    ew("dve", lambda e, cs=cs: e.tensor_tensor(out=F[7][:, cs], in0=F[7][:, cs], in1=F[2][:, cs],
                                                               op=ALU.mult), [FK[7], FK[2]], [FK[7]])
                    ew("pool", lambda e, cs=cs: e.tensor_tensor(out=F[9][:, cs], in0=F[9][:, cs], in1=F[2][:, cs],
                                                                op=ALU.mult), [FK[9], FK[2]], [FK[9]])
                    ew("act", lambda e, cs=cs: e.activation(out=F[8][:, cs], in_=F[8][:, cs], func=AF.Exp,
                                                            scale=-DECAY_K), [FK[8]], [FK[8]])
                    ew("dve", lambda e, cs=cs: e.tensor_tensor(out=F[1][:, cs], in0=F[1][:, cs], in1=F[8][:, cs],
                                                               op=ALU.mult), [FK[1], FK[8]], [FK[1]])
                kb.op("pool", lambda e: e.memset(T["nR"][:], 0.0),
                      reads=[FK[0], FK[1], FK[3], FK[6], FK[7], FK[8], FK[9]], writes=[hk_, "nR"])
                for hh in range(2):
                    dplr_scan(kb, nc, c, T, 64, F[1], F[6], F[7], F[9], F[3], F[8], F[0],
                              store_cb_factory((2 * p + hh) * 64, 64), hk_, bp=hh * 64)
                kb.op("pool", lambda e: e.memset(T["nR"][:], 0.0), reads=[hk_],
                      writes=[FK[0], FK[1], FK[3], FK[6], FK[7], FK[8], FK[9], "nR"])
            mixer_gdn_pass(kb, nc, g, c, T, d, F, FK, rmask, par128, sel, project, store_cb_factory, ps, DC, wv, wsl,
                           hT, Zs)
        kb.barrier()
        es2.close()
        import os
        if os.environ.get("MIX_DUMP"):
            kb.dma("sp", x_lat_out[0:2048, :], Ys[0, 0:2048, 0:1024], writes=["o1"])
            kb.dma("sp", x_ctx_out[0:256, :], Ys[0, 0:256, 512:1536], writes=["o2"])
            kb.barrier()
            return
        mixer_output(kb, nc, g, c, mods, T, F, FK, x_lat_in, x_ctx_in, x_lat_out, x_ctx_out, Ys, Zs, J32, None, None, hb,
                     ps, par128)
        kb.barrier()


def mixer_gdn_pass(kb, nc, g, c, T, d, F, FK, rmask, par128, sel, project, store_cb_factory, ps, DC, wv, wsl, hT, Zs):
    ew = lambda eng, fn, r, w: kb.op(eng, fn, reads=r, writes=w)
    project(3968, 16, F[10], FK[10])
    R16 = lambda i: F[i][0:16, :]
    ew("act", lambda e: e.activation(out=T["U"][0:16, 0:1], in_=par128[0:16, 41:42], func=AF.Exp), ["par128"], ["U"])
    ew("dve", lambda e: e.tensor_scalar(out=T["U"][0:16, 0:1], in0=T["U"][0:16, 0:1], scalar1=-1.0, scalar2=None,
                                        op0=ALU.mult), ["U"], ["U"])
    ew("dve", lambda e: e.tensor_scalar(out=R16(1), in0=R16(10), scalar1=par128[0:16, 40:41], scalar2=None, op0=ALU.add),
       [FK[10], "par128"], [FK[1]])
    ew("act", lambda e: e.activation(out=R16(4), in_=R16(10), func=AF.Sigmoid), [FK[10]], [FK[4]])
    ew("act", lambda e: e.activation(out=R16(2), in_=R16(1), func=AF.Abs), [FK[1]], [FK[2]])
    ew("act", lambda e: e.activation(out=R16(2), in_=R16(2), func=AF.Exp, scale=-1.0), [FK[2]], [FK[2]])
    ew("dve", lambda e: e.tensor_scalar(out=R16(3), in0=R16(2), scalar1=2.0, scalar2=None, op0=ALU.add), [FK[2]], [FK[3]])
    ew("dve", lambda e: e.reciprocal(out=R16(3), in_=R16(3)), [FK[3]], [FK[3]])
    ew("dve", lambda e: e.tensor_tensor(out=R16(2), in0=R16(2), in1=R16(3), op=ALU.mult), [FK[2], FK[3]], [FK[2]])
    ew("dve", lambda e: e.tensor_tensor(out=R16(3), in0=R16(2), in1=R16(2), op=ALU.mult), [FK[2]], [FK[3]])
    ew("dve", lambda e: e.tensor_scalar(out=R16(6), in0=R16(3), scalar1=1.0 / 13, scalar2=1.0 / 11, op0=ALU.mult,
                                        op1=ALU.add), [FK[3]], [FK[6]])
    for cf in (1.0 / 9, 1.0 / 7, 1.0 / 5, 1.0 / 3, 1.0):
        ew("dve", lambda e: e.tensor_tensor(out=R16(6), in0=R16(6), in1=R16(3), op=ALU.mult), [FK[6], FK[3]], [FK[6]])
        ew("dve", lambda e, cf=cf: e.tensor_scalar(out=R16(6), in0=R16(6), scalar1=cf, scalar2=None, op0=ALU.add),
           [FK[6]], [FK[6]])
    ew("dve", lambda e: e.scalar_tensor_tensor(out=R16(6), in0=R16(6), scalar=2.0, in1=R16(2), op0=ALU.mult, op1=ALU.mult),
       [FK[6], FK[2]], [FK[6]])
    ew("dve", lambda e: e.tensor_scalar(out=R16(1), in0=R16(1), scalar1=0.0, scalar2=None, op0=ALU.max), [FK[1]], [FK[1]])
    ew("dve", lambda e: e.tensor_tensor(out=R16(6), in0=R16(6), in1=R16(1), op=ALU.add), [FK[6], FK[1]], [FK[6]])
    ew("dve", lambda e: e.tensor_scalar(out=R16(6), in0=R16(6), scalar1=T["U"][0:16, 0:1], scalar2=None, op0=ALU.mult),
       [FK[6], "U"], [FK[6]])
    ew("dve", lambda e: e.tensor_tensor_scan(out=R16(10), data0=rmask[0:16, :], data1=R16(6), initial=0.0,
                                             op0=ALU.mult, op1=ALU.add), ["rmask", FK[6]], [FK[10]])
    for h in range(4):
        hk_ = f"ghead{d}_{h}"
        c0 = 1920 + h * 512
        for part, dst in ((0, 1), (1, 2), (2, 3)):
            project(c0 + part * 128, 128, F[0], FK[0])
            n = TP - 4
            for j in range(5):
                jj = j if d == 0 else 4 - j
                wc = par128[:, 48 + (h * 3 + part) * 5 + jj:49 + (h * 3 + part) * 5 + jj]
                if j == 0:
                    ew("dve", lambda e, wc=wc, dst=dst: e.tensor_scalar(out=F[dst][:, 2:2 + n], in0=F[0][:, 0:n],
                                                                      scalar1=wc, scalar2=None, op0=ALU.mult),
                       [FK[0], "par128"], [FK[dst]])
                else:
                    ew("dve", lambda e, wc=wc, dst=dst, j=j: e.scalar_tensor_tensor(
                        out=F[dst][:, 2:2 + n], in0=F[0][:, j:j + n], scalar=wc, in1=F[dst][:, 2:2 + n],
                        op0=ALU.mult, op1=ALU.add), [FK[0], FK[dst], "par128"], [FK[dst]])
            ew("act", lambda e, dst=dst: e.activation(out=F[dst][:, :], in_=F[dst][:, :], func=AF.Silu), [FK[dst]], [FK[dst]])
        for src, scl in ((1, float(128 ** -0.5)), (2, 1.0)):
            for (a0_, n_) in DC:
                ew("pool", lambda e, src=src, a0_=a0_, n_=n_: e.tensor_tensor(
                    out=F[0][:, a0_:a0_ + n_], in0=F[src][:, a0_:a0_ + n_], in1=F[src][:, a0_:a0_ + n_], op=ALU.mult),
                    [FK[src]], [FK[0]])
            for bi, (t0, tw, col0) in enumerate(BLOCKS):
                cs = slice(col0, col0 + tw)
                kb.op("pe", lambda e, cs=cs, tw=tw: e.matmul(ps[2][:, 0:tw], lhsT=c.ones[:, :], rhs=F[0][:, cs],
                                                           start=True, stop=True), reads=["c_ones", FK[0]], writes=["ps2"])
                kb.op("act", lambda e, cs=cs, tw=tw: e.activation(out=F[9][:, cs], in_=ps[2][:, 0:tw], func=AF.Sqrt,
                                                                bias=c.eps6[:, 0:1]), reads=["ps2", "c_eps"], writes=[FK[9]])
            for (a0_, n_) in DC:
                cs = slice(a0_, a0_ + n_)
                ew("dve", lambda e, cs=cs: e.reciprocal(out=F[9][:, cs], in_=F[9][:, cs]), [FK[9]], [FK[9]])
                ew("dve", lambda e, cs=cs, src=src, scl=scl: e.scalar_tensor_tensor(
                    out=F[src][:, cs], in0=F[src][:, cs], scalar=scl, in1=F[9][:, cs], op0=ALU.mult, op1=ALU.mult),
                    [FK[src], FK[9]], [FK[src]])
        ra, rb = d * 4 + h, 8 + d * 4 + h
        for bi, (t0, tw, col0) in enumerate(BLOCKS):
            cs = slice(col0, col0 + tw)
            kb.op("pe", lambda e, cs=cs, tw=tw: e.matmul(ps[2][:, 0:tw], lhsT=sel[0:16, ra, :], rhs=F[10][0:16, cs],
                                                       start=True, stop=True), reads=["sel", FK[10]], writes=["ps2"])
            kb.op("pe", lambda e, cs=cs, tw=tw: e.matmul(ps[3][:, 0:tw], lhsT=sel[0:16, rb, :], rhs=F[4][0:16, cs],
                                                       start=True, stop=True), reads=["sel", FK[4]], writes=["ps3"])
            kb.op("act", lambda e, cs=cs, tw=tw: e.activation(out=F[6][:, cs], in_=ps[2][:, 0:tw], func=AF.Exp),
                  reads=["ps2"], writes=[FK[6]])
            kb.op("dve", lambda e, cs=cs, tw=tw: e.tensor_copy(out=F[0][:, cs], in_=ps[2][:, 0:tw]),
                  reads=["ps2"], writes=[FK[0]])
            kb.op("act", lambda e, cs=cs, tw=tw: e.copy(out=F[9][:, cs], in_=ps[3][:, 0:tw]),
                  reads=["ps3"], writes=[FK[9]])
        for (a0_, n_) in DC:
            cs = slice(a0_, a0_ + n_)
            ew("dve", lambda e, cs=cs: e.tensor_tensor(out=F[9][:, cs], in0=F[9][:, cs], in1=F[2][:, cs], op=ALU.mult),
               [FK[9], FK[2]], [FK[9]])
            ew("pool", lambda e, cs=cs: e.tensor_tensor(out=F[7][:, cs], in0=F[2][:, cs], in1=F[6][:, cs], op=ALU.mult),
               [FK[2], FK[6]], [FK[7]])
            ew("dve", lambda e, cs=cs: e.tensor_tensor(out=F[8][:, cs], in0=F[1][:, cs], in1=F[6][:, cs], op=ALU.mult),
               [FK[1], FK[6]], [FK[8]])
        kb.op("pool", lambda e: e.memset(T["nR"][:], 0.0), reads=[FK[0], FK[1], FK[2], FK[3], FK[6], FK[7], FK[8], FK[9]],
              writes=[hk_, "nR"])
        dplr_scan(kb, nc, c, T, 128, F[8], F[7], F[9], F[9], F[3], F[6], None, store_cb_factory(512 + h * 128, 128), hk_,
                  Gb=F[0], rA=F[1], kkA=F[2])
        kb.op("pool", lambda e: e.memset(T["nR"][:], 0.0), reads=[hk_],
              writes=[FK[0], FK[1], FK[2], FK[3], FK[6], FK[7], FK[8], FK[9], "nR"])
    if d == 0:
        n = 0
        for h in range(4):
            kb.dma("pool", wsl[:, :, 0:128], wv[:, :, 1920 + h * 512 + 384:1920 + h * 512 + 512], writes=["wsl"])
            for i in range(18):
                pb = ps[n % 2]
                for kc in range(8):
                    kb.op("pe", lambda e, kc=kc, i=i, pb=pb: e.matmul(pb[:, 0:128], lhsT=hT[:, kc, i * 128:(i + 1) * 128],
                                                                   rhs=wsl[:, kc, 0:128], start=(kc == 0), stop=(kc == 7)),
                          reads=["wsl", "hT"], writes=[f"ps{n % 2}"])
                zt = T["zt"][n % 2]
                kb.op("act", lambda e, pb=pb, zt=zt: e.activation(out=zt[:], in_=pb[:, 0:128], func=AF.Silu),
                      reads=[f"ps{n % 2}"], writes=[f"zt{n % 2}"])
                kb.dma("sp", Zs[i * 128:(i + 1) * 128, h * 128:(h + 1) * 128], zt[:], reads=[f"zt{n % 2}"],
                       writes=[("zs", i, h)])
                n += 1


def mixer_output(kb, nc, g, c, mods, T, F, FK, x_lat_in, x_ctx_in, x_lat_out, x_ctx_out, Ys, Zs, J32, xb, Gbc, hb, ps,
                 par128):
    ew = lambda eng, fn, r, w: kb.op(eng, fn, reads=r, writes=w)
    kb.barrier()
    from contextlib import ExitStack
    with ExitStack() as es:
        def sb(name, shape, dt):
            return es.enter_context(nc.sbuf_tensor(f"mo_{name}", shape, dt))
        wo = sb("wo", [128, 8, D], BF16)
        g2 = sb("g2", [128, 512], F32)
        bcp = sb("bcp", [128, 3, 512], F32)
        yf = sb("yf", [128, 1536], F32)
        yb = sb("yb", [128, 1536], F32)
        zt = sb("zt", [128, 512], F32)
        o = sb("o", [128, D], F32)
        t1 = sb("t1", [128, 512], F32)
        s8 = sb("s8", [128, 4, 8], F32)
        oT = sb("oT", [128, 8, 128], BF16)
        Gbc = sb("Gbc", [128, D], F32)
        xb = [sb("x0", [128, D], F32), sb("x1", [128, D], F32)]
        for kc in range(8):
            kb.dma("pool", wo[:, kc, :], g["e_w_out"][0].rearrange("(kc p) n -> p kc n", p=128)[:, kc, :], writes=["wo"])
        kb.dma("sp", g2[:], g["a_g2"][0], writes=["g2"])
        kb.dma("sp", bcp[:].rearrange("p a n -> p (a n)"), g["mx_bc"].partition_broadcast(128), writes=["bcp"])
        for stream in (1, 0):
            kb.barrier()
            make_bc(kb, nc, c, lambda kc: mods[0][:, 2 * 8 + kc, stream:stream + 1], Gbc, ps[0], "Gbc", ["mod0"])
            nt = 2 if stream == 1 else 16
            src = x_ctx_in if stream == 1 else x_lat_in
            dst = x_ctx_out if stream == 1 else x_lat_out
            base = 0 if stream == 1 else 256
            colbase = CTX0 if stream == 1 else LAT0
            for i in range(nt):
                b = i % 2
                r0 = base + i * 128
                rb0 = base + (nt - 1 - i) * 128
                kb.dma("sp", xb[b][:], src[i * 128:(i + 1) * 128, :], writes=[f"xb{b}"])
                kb.dma("sp", yf[:], Ys[0, r0:r0 + 128, :], reads=[("ysall",)], writes=["yf"])
                kb.dma("act", yb[:], Ys[1, rb0:rb0 + 128, :], reads=[("ysall",)], writes=["yb"])
                kb.dma("sp", zt[:], Zs[r0:r0 + 128, :], reads=[("ysall",)], writes=["zt"])
                for q in range(3):
                    kb.op("pe", lambda e, q=q: e.matmul(ps[1 + q][:, :], lhsT=J32[:, :], rhs=yb[:, q * 512:(q + 1) * 512],
                                                      start=True, stop=True), reads=["J32", "yb"], writes=[f"ps{1 + q}"])
                    ew("dve", lambda e, q=q: e.tensor_tensor(out=yf[:, q * 512:(q + 1) * 512], in0=yf[:, q * 512:(q + 1) * 512],
                                                             in1=ps[1 + q][:, :], op=ALU.add), [f"ps{1 + q}", "yf"], ["yf"])
                y3 = yf[:, 0:512].rearrange("p (h j) -> p h j", h=8)
                ew("dve", lambda e: e.reduce_sum(out=s8[:, 0, :], in_=y3, axis=AX.X), ["yf"], ["s8"])
                ew("dve", lambda e: e.tensor_scalar(out=s8[:, 0, :], in0=s8[:, 0, :], scalar1=-1.0 / 64, scalar2=None,
                                                    op0=ALU.mult), ["s8"], ["s8"])
                ew("dve", lambda e: e.tensor_tensor(out=y3, in0=y3, in1=s8[:, 0, :].unsqueeze(2).broadcast_to([128, 8, 64]),
                                                    op=ALU.add), ["yf", "s8"], ["yf"])
                ew("pool", lambda e: e.tensor_tensor(out=t1[:], in0=yf[:, 0:512], in1=yf[:, 0:512], op=ALU.mult), ["yf"], ["t1"])
                ew("dve", lambda e: e.reduce_sum(out=s8[:, 1, :], in_=t1[:].rearrange("p (h j) -> p h j", h=8), axis=AX.X),
                   ["t1"], ["s8"])
                ew("dve", lambda e: e.tensor_scalar(out=s8[:, 1, :], in0=s8[:, 1, :], scalar1=1.0 / 64, scalar2=64e-5,
                                                    op0=ALU.mult, op1=ALU.add), ["s8"], ["s8"])
                ew("act", lambda e: e.activation(out=s8[:, 1, :], in_=s8[:, 1, :], func=AF.Sqrt), ["s8"], ["s8"])
                ew("dve", lambda e: e.reciprocal(out=s8[:, 1, :], in_=s8[:, 1, :]), ["s8"], ["s8"])
                ew("dve", lambda e: e.tensor_tensor(out=y3, in0=y3, in1=s8[:, 1, :].unsqueeze(2).broadcast_to([128, 8, 64]),
                                                    op=ALU.mult), ["yf", "s8"], ["yf"])
                ew("dve", lambda e: e.tensor_tensor(out=yf[:, 0:512], in0=yf[:, 0:512], in1=bcp[:, 0, :], op=ALU.mult),
                   ["yf", "bcp"], ["yf"])
                ew("pool", lambda e: e.tensor_tensor(out=yf[:, 0:512], in0=yf[:, 0:512], in1=bcp[:, 1, :], op=ALU.add),
                   ["yf", "bcp"], ["yf"])
                ew("pool", lambda e: e.tensor_tensor(out=yf[:, 0:512], in0=yf[:, 0:512], in1=yf[:, 1024:1536], op=ALU.add),
                   ["yf"], ["yf"])
                cg = colbase + i * 128
                kb.op("pe", lambda e, cg=cg: e.matmul(ps[4][:, :], lhsT=F[11][:, cg:cg + 128], rhs=g2[:, :], start=True, stop=True),
                      reads=[FK[11], "g2"], writes=["ps4"])
                ew("dve", lambda e: e.tensor_tensor(out=o[:, 0:512], in0=yf[:, 0:512], in1=ps[4][:, :], op=ALU.mult),
                   ["yf", "ps4"], ["o"])
                ew("pool", lambda e: e.tensor_tensor(out=t1[:], in0=yf[:, 512:1024], in1=yf[:, 512:1024], op=ALU.mult),
                   ["yf"], ["t1"])
                ew("dve", lambda e: e.reduce_sum(out=s8[:, 2, 0:4], in_=t1[:].rearrange("p (h j) -> p h j", h=4), axis=AX.X),
                   ["t1"], ["s8"])
                ew("dve", lambda e: e.tensor_scalar(out=s8[:, 2, 0:4], in0=s8[:, 2, 0:4], scalar1=1.0 / 128, scalar2=EPS,
                                                    op0=ALU.mult, op1=ALU.add), ["s8"], ["s8"])
                ew("act", lambda e: e.activation(out=s8[:, 2, 0:4], in_=s8[:, 2, 0:4], func=AF.Sqrt), ["s8"], ["s8"])
                ew("dve", lambda e: e.reciprocal(out=s8[:, 2, 0:4], in_=s8[:, 2, 0:4]), ["s8"], ["s8"])
                ew("dve", lambda e: e.tensor_tensor(
                    out=t1[:].rearrange("p (h j) -> p h j", h=4), in0=yf[:, 512:1024].rearrange("p (h j) -> p h j", h=4),
                    in1=s8[:, 2, 0:4].unsqueeze(2).broadcast_to([128, 4, 128]), op=ALU.mult), ["yf", "s8"], ["t1"])
                ew("pool", lambda e: e.tensor_tensor(out=t1[:], in0=t1[:], in1=bcp[:, 2, :], op=ALU.mult), ["t1", "bcp"], ["t1"])
                ew("dve", lambda e: e.tensor_tensor(out=o[:, 512:1024], in0=t1[:], in1=zt[:], op=ALU.mult), ["t1", "zt"], ["o"])
                ew("act", lambda e: e.copy(out=hb[:], in_=o[:]), ["o"], ["hb"])
                pst = ps[0][:].bitcast(BF16)
                for kc in range(8):
                    kb.op("pe", lambda e, kc=kc, pst=pst: e.transpose(out=pst[:, kc * 128:(kc + 1) * 128],
                                                                    in_=hb[:, kc * 128:(kc + 1) * 128], identity=c.identb[:]),
                          reads=["hb", "c_identb"], writes=["ps0"])
                ew("act", lambda e, pst=pst: e.copy(out=oT[:], in_=pst[:].rearrange("p (k t) -> p k t", k=8)), ["ps0"], ["oT"])
                for half in range(2):
                    pp = ps[5 + half]
                    for kc in range(8):
                        kb.op("pe", lambda e, kc=kc, half=half, pp=pp: e.matmul(
                            pp[:], lhsT=oT[:, kc, :], rhs=wo[:, kc, half * 512:(half + 1) * 512], start=(kc == 0), stop=(kc == 7)),
                            reads=["oT", "wo"], writes=[f"ps{5 + half}"])
                    sl = slice(half * 512, (half + 1) * 512)
                    ew("dve", lambda e, pp=pp, sl=sl: e.tensor_tensor(out=t1[:], in0=pp[:], in1=Gbc[:, sl], op=ALU.mult),
                       [f"ps{5 + half}", "Gbc"], ["t1"])
                    ew("pool", lambda e, sl=sl, b=b: e.tensor_tensor(out=xb[b][:, sl], in0=xb[b][:, sl], in1=t1[:], op=ALU.add),
                       ["t1", f"xb{b}"], [f"xb{b}"])
                kb.dma("sp", dst[i * 128:(i + 1) * 128, :], xb[b][:], reads=[f"xb{b}"], writes=[("dram", dst.tensor.name, i)])
        kb.barrier()


_CACHE = {}


def kernel(**inputs):
    inp = {k: np.asarray(v) for k, v in inputs.items()}
    if "nc" not in _CACHE:
        _CACHE["nc"] = build(stages=("all",))[0]
    nc = _CACHE["nc"]
    in_maps = [host_inputs(inp, b) for b in range(8)]
    res = run_bass_kernel_spmd(nc, in_maps, core_ids=list(range(8)))
    return np.stack([np.asarray(r["out"], dtype=np.float32) for r in res.results], axis=0)
```

```python
import numpy as np
import concourse.bass as bass
import concourse.mybir as mybir
from concourse.bass_utils import run_bass_kernel_spmd

F32 = mybir.dt.float32
BF16 = mybir.dt.bfloat16
I32 = mybir.dt.int32
U32 = mybir.dt.uint32
ALU = mybir.AluOpType
AF = mybir.ActivationFunctionType
AX = mybir.AxisListType

SEM_ROTATE = 20000
N_DMA_SEMS = 24


class KB:
    def __init__(self, nc, same_engine_sync=True):
        self.nc = nc
        self.engs = {"pe": nc.tensor, "act": nc.scalar, "dve": nc.vector, "pool": nc.gpsimd, "sp": nc.sync}
        self.same_engine_sync = same_engine_sync
        self.esem = {}
        self.ecnt = {}
        self.sem_id = 0
        for e in ("pe", "act", "dve", "pool"):
            self._new_esem(e)
        self.dsems = [self._alloc_sem(f"dma{i}") for i in range(N_DMA_SEMS)]
        self.dcnt = [0] * N_DMA_SEMS
        self.dnext = 0
        self.known = {e: {} for e in self.engs}
        self.state = {}
        self.n_ins = 0
        self._uid = 0
        self.out_tokens = []

    def _alloc_sem(self, name):
        self.sem_id += 1
        return self.nc.alloc_semaphore(f"{name}_{self.sem_id}")

    def _new_esem(self, e):
        self.esem[e] = self._alloc_sem(f"s_{e}")
        self.ecnt[e] = 0

    def uid(self, p="t"):
        self._uid += 1
        return f"{p}{self._uid}"

    def _deps(self, reads, writes):
        deps = []
        for r in reads:
            st = self.state.get(r)
            if st and st[0] is not None:
                deps.append(st[0])
        for w in writes:
            st = self.state.get(w)
            if st:
                if st[0] is not None:
                    deps.append(st[0])
                deps.extend(st[1].values())
        return deps

    def _wait(self, e, deps):
        eng = self.engs[e]
        kn = self.known[e]
        best = {}
        for (sem, val, src) in deps:
            if src == e and not (self.same_engine_sync and e != "pe"):
                continue
            key = id(sem)
            if kn.get(key, 0) >= val:
                continue
            if key not in best or best[key][1] < val:
                best[key] = (sem, val)
        for key, (sem, val) in best.items():
            eng.wait_ge(sem, val)
            kn[key] = val
            self.n_ins += 1

    def _commit(self, token, reads, writes):
        for w in writes:
            self.state[w] = [token, {}]
        for r in reads:
            st = self.state.get(r)
            if st is None:
                st = [None, {}]
                self.state[r] = st
            st[1][id(token[0])] = token

    def op(self, e, fn, reads=(), writes=()):
        reads = list(reads)
        writes = list(writes)
        writes += [r for r in reads if isinstance(r, str) and r.startswith("ps")]
        self._wait(e, self._deps(reads, writes))
        if self.ecnt[e] >= SEM_ROTATE:
            self._new_esem(e)
        ins = fn(self.engs[e])
        self.ecnt[e] += 1
        ins.then_inc(self.esem[e], 1)
        token = (self.esem[e], self.ecnt[e], e)
        self._commit(token, reads, writes)
        self.n_ins += 1
        return token

    def dma(self, q, out, in_, reads=(), writes=(), **kw):
        reads = list(reads)
        writes = list(writes)
        i = self.dnext
        self.dnext = (self.dnext + 1) % N_DMA_SEMS
        deps = self._deps(reads, writes)
        if self.dcnt[i] > 0:
            deps.append((self.dsems[i], self.dcnt[i], "dma"))
        self._wait(q, deps)
        ins = self.engs[q].dma_start(out=out, in_=in_, **kw)
        self.dcnt[i] += 16
        ins.then_inc(self.dsems[i], 16)
        token = (self.dsems[i], self.dcnt[i], "dma")
        self._commit(token, reads, writes)
        self.n_ins += 1
        return token

    def finish(self, out_keys):
        deps = []
        for k in out_keys:
            st = self.state.get(k)
            if st and st[0] is not None:
                deps.append(st[0])
        self._wait("sp", deps)
        deps = [(self.dsems[i], self.dcnt[i], "dma") for i in range(N_DMA_SEMS) if self.dcnt[i] > 0]
        self._wait("sp", deps)

    def barrier(self):
        deps = [(self.esem[e], self.ecnt[e], "x") for e in self.esem if self.ecnt[e] > 0]
        deps += [(self.dsems[i], self.dcnt[i], "dma") for i in range(N_DMA_SEMS) if self.dcnt[i] > 0]
        for e in self.engs:
            self._wait(e, deps)
        self.state = {}


D = 1024
KC = 8
EPS = 1e-6
TP = 2310
CTX0, LAT0 = 2, 260
CHUNK = 128
CHUNK_COLS = [CTX0 + CHUNK * j for j in range(256 // CHUNK)] + [LAT0 + CHUNK * j for j in range(2048 // CHUNK)]
BLOCKS = [(0, 256, CTX0)] + [(256 + 512 * j, 512, LAT0 + 512 * j) for j in range(4)]
DECAY_K = float(np.exp(-0.5))


class Ctx:
    pass


def load_consts(kb, nc, es, g):
    c = Ctx()
    c.ident = es.enter_context(nc.sbuf_tensor("c_ident", [128, 128], F32))
    c.identb = es.enter_context(nc.sbuf_tensor("c_identb", [128, 128], BF16))
    c.ones = es.enter_context(nc.sbuf_tensor("c_ones", [128, 128], F32))
    c.iota = es.enter_context(nc.sbuf_tensor("c_iota", [128, 256], F32))
    kb.dma("sp", c.ident[:], g["k_ident"][:, :], writes=["c_ident"])
    kb.dma("sp", c.iota[:], g["k_iota"][:, :], writes=["c_iota"])
    kb.op("dve", lambda e: e.memset(c.ones[:], 1.0), writes=["c_ones"])
    c.eps6 = es.enter_context(nc.sbuf_tensor("c_eps6", [128, 1], F32))
    c.one1 = es.enter_context(nc.sbuf_tensor("c_one1", [128, 1], F32))
    kb.op("dve", lambda e: e.memset(c.eps6[:], 1e-6), writes=["c_eps"])
    kb.op("dve", lambda e: e.memset(c.one1[:], 1.0), writes=["c_eps"])
    c.onesb = es.enter_context(nc.sbuf_tensor("c_onesb", [128, 8], BF16))
    kb.op("dve", lambda e: e.memset(c.onesb[:], 1.0), writes=["c_ones"])
    kb.op("dve", lambda e: e.tensor_copy(out=c.identb[:], in_=c.ident[:]), reads=["c_ident"], writes=["c_identb"])
    return c


def prologue(kb, nc, es, g, c):
    mods = []
    for l in range(2):
        mods.append(es.enter_context(nc.sbuf_tensor(f"mod{l}", [128, 48, 2], F32)))
    with nc.sbuf_tensor("pl_sc", [128, 2, 8], F32) as sc, \
            nc.sbuf_tensor("pl_w0", [128, 8, 512], F32) as w0, \
            nc.sbuf_tensor("pl_w1", [128, 8, 512], F32) as w1, \
            nc.sbuf_tensor("pl_b", [128, 2, 48], F32) as adab, \
            nc.sbuf_tensor("pl_n", [128, 2, 2, 8], F32) as nrm, \
            nc.psum_tensor("pl_ps", [128, 512], F32) as ps:
        wb = [w0, w1]
        kb.dma("sp", sc[:, 0, :], g["cT"][:, :], writes=["sc"])
        kb.dma("sp", sc[:, 1, :], g["ccT"][:, :], writes=["sc"])
        kb.dma("sp", adab[:], g["ada_bT"][:, :, :], writes=["adab"])
        kb.dma("sp", nrm[:], g["normT"][:, :, :, :], writes=["nrm"])
        kb.op("act", lambda e: e.activation(out=sc[:], in_=sc[:], func=AF.Silu), reads=["sc"], writes=["sc"])
        blk = 0
        for l in range(2):
            wv = g["ada_w"][l].rearrange("(kc p) n -> p kc n", p=128)
            for nb in range(12):
                wt = wb[blk % 2]
                wk = f"plw{blk % 2}"
                kb.dma("sp" if blk % 2 == 0 else "act", wt[:], wv[:, :, nb * 512:(nb + 1) * 512], writes=[wk])
                for j in range(4):
                    for kc in range(8):
                        kb.op("pe", lambda e, kc=kc, j=j, wt=wt: e.matmul(
                            ps[:, (j * 2):(j * 2 + 2)], lhsT=wt[:, kc, j * 128:(j + 1) * 128], rhs=sc[:, :, kc],
                            start=(kc == 0), stop=(kc == 7)), reads=[wk, "sc"], writes=["psPL"])
                kb.op("dve", lambda e, l=l, nb=nb: e.tensor_tensor(
                    out=mods[l][:, nb * 4:(nb + 1) * 4, :],
                    in0=ps[:, 0:8].rearrange("p (j s) -> p j s", s=2),
                    in1=adab[:, l, nb * 4:(nb + 1) * 4].unsqueeze(2).broadcast_to([128, 4, 2]),
                    op=ALU.add), reads=["psPL", "adab"], writes=[f"mod{l}"])
                blk += 1
            for (m, which) in ((1, 0), (4, 1)):
                for s in range(2):
                    kb.op("dve", lambda e, l=l, m=m, which=which, s=s: e.scalar_tensor_tensor(
                        out=mods[l][:, m * 8:(m + 1) * 8, s], in0=mods[l][:, m * 8:(m + 1) * 8, s], scalar=1.0,
                        in1=nrm[:, l, which, :], op0=ALU.add, op1=ALU.mult),
                        reads=[f"mod{l}", "nrm"], writes=[f"mod{l}"])
    kb.barrier()
    return mods


def make_bc(kb, nc, c, col_ap_fn, out_tile, ps, key, src_keys):
    with nc.sbuf_tensor(kb.uid("bcd"), [128, 128], F32) as dg:
        dk = kb.uid("dg")
        for half in range(2):
            for q in range(4):
                kc = half * 4 + q
                kb.op("dve", lambda e, kc=kc: e.tensor_scalar(
                    out=dg[:], in0=c.ident[:], scalar1=col_ap_fn(kc), scalar2=None, op0=ALU.mult),
                    reads=["c_ident"] + src_keys, writes=[dk])
                kb.op("pe", lambda e, q=q: e.matmul(ps[:, q * 128:(q + 1) * 128], lhsT=c.ones[:], rhs=dg[:],
                                                   start=True, stop=True),
                      reads=[dk, "c_ones"], writes=["psBC" + key])
            kb.op("act", lambda e, half=half: e.copy(out=out_tile[:, half * 512:(half + 1) * 512], in_=ps[:]),
                  reads=["psBC" + key], writes=[key])
        kb.barrier()


def norm_tile_g(kb, nc, xt, xk, st, G, S, hout, hk, eps=EPS):
    sk = kb.uid("st")
    kb.op("act", lambda e: e.activation(out=hout, in_=xt, func=AF.Square, accum_out=st[:, 0:1]),
          reads=[xk], writes=[hk, sk])
    yield
    kb.op("dve", lambda e: e.tensor_scalar(out=st[:, 1:2], in0=st[:, 0:1], scalar1=1.0 / D, scalar2=eps,
                                           op0=ALU.mult, op1=ALU.add), reads=[sk], writes=[sk])
    yield
    kb.op("act", lambda e: e.activation(out=st[:, 2:3], in_=st[:, 1:2], func=AF.Sqrt), reads=[sk], writes=[sk])
    yield
    kb.op("dve", lambda e: e.reciprocal(out=st[:, 3:4], in_=st[:, 2:3]), reads=[sk], writes=[sk])
    yield
    if G is None:
        kb.op("dve", lambda e: e.tensor_scalar(out=hout, in0=xt, scalar1=st[:, 3:4], scalar2=None, op0=ALU.mult),
              reads=[xk, sk], writes=[hk])
        return
    kb.op("dve", lambda e: e.scalar_tensor_tensor(out=hout, in0=xt, scalar=st[:, 3:4], in1=G[:],
                                                  op0=ALU.mult, op1=ALU.mult),
          reads=[xk, sk, "Gbc"], writes=[hk])
    yield
    if S is not None:
        kb.op("pool", lambda e: e.tensor_tensor(out=hout, in0=hout, in1=S[:], op=ALU.add),
              reads=[hk, "Sbc"], writes=[hk])


def norm_tile(*a, **k):
    for _ in norm_tile_g(*a, **k):
        pass


def moe_stage(kb, nc, g, c, mods, layer, stream, x_in, x_out, T, final_norm=False):
    NT = T // 128
    cap = 2 * T // 16
    CW = cap
    CT = (cap + 127) // 128
    cs = min(cap, 128)
    mod = mods[layer]
    xin_v = x_in.rearrange("(n p) d -> n p d", p=128)
    xout_v = x_out.rearrange("(n p) d -> n p d", p=128)
    sx = f"L{layer}s{stream}"
    from contextlib import ExitStack
    with ExitStack() as es:
        def sb(name, shape, dt):
            return es.enter_context(nc.sbuf_tensor(f"moe_{name}_{sx}", shape, dt))
        Gbc = sb("G", [128, D], F32)
        Sbc = sb("S", [128, D], F32)
        gate2 = Sbc
        hbf = sb("hbf", [128, NT, D], BF16)
        xb = [sb("x0", [128, D], F32), sb("x1", [128, D], F32)]
        h32 = [sb("h0", [128, D], F32), sb("h1", [128, D], F32)]
        hT = [sb("hT0", [128, 8, 128], F32), sb("hT1", [128, 8, 128], F32)]
        stt = [sb("st0", [128, 4], F32), sb("st1", [128, 4], F32)]
        rt = sb("rt", [128, 8, 16], F32)
        aff = sb("aff", [128, NT, 16], F32)
        sm = sb("sm", [128, 4], F32)
        ex = sb("ex", [128, 16], F32)
        affT = sb("affT", [16, T], F32)
        work = sb("work", [16, T], F32)
        mx8 = sb("mx8", [16, 8], F32)
        maskT = sb("maskT", [16, T], F32)
        onesT = work
        slotT = sb("slotT", [16, T], F32)
        gateT = affT
        slot = sb("slot", [128, NT, 16], F32)
        gate = sb("gate", [128, NT, 16], F32)
        selT = [sb("selT0", [128, CW], BF16), sb("selT1", [128, CW], BF16)]
        xeT = sb("xeT", [128, 8, CW], BF16)
        big = sb("big", [128, 4 * 8 * D], BF16)
        wviews = [big[:, j * 8 * D:(j + 1) * 8 * D].rearrange("p (k n) -> p k n", n=D) for j in range(4)]
        w1b = [wviews[0], wviews[1]]
        w3b = [wviews[2]]
        w2b = [wviews[3]]
        yest = [sb("yest0", [128, CT, D], BF16), sb("yest1", [128, CT, D], BF16)]
        sil = [sb("sil0", [128, CW], F32), sb("sil1", [128, CW], F32)]
        hidT = sb("hidT", [128, 8, CW], BF16)
        yeall = big[:, 0:16 * CT * D].rearrange("p (e c n) -> p e c n", e=16, c=CT)
        selGa = [sb("selGa0", [128, 4, CW], BF16), sb("selGa1", [128, 4, CW], BF16)]
        selGca = [sb("selGca0", [128, 4 * CT, 128], BF16), sb("selGca1", [128, 4 * CT, 128], BF16)]
        tmpo = [sb("tmpo0", [128, 512], F32), sb("tmpo1", [128, 512], F32)]
        ps = [es.enter_context(nc.psum_tensor(f"moe_ps{i}_{sx}", [128, 512], F32)) for i in range(8)]

        print("moe sbuf remaining", nc.sbuf_bytes_remaining)
        make_bc(kb, nc, c, lambda kc: mod[:, 4 * 8 + kc, stream:stream + 1], Gbc, ps[0], "Gbc", [f"mod{layer}"])
        make_bc(kb, nc, c, lambda kc: mod[:, 3 * 8 + kc, stream:stream + 1], Sbc, ps[0], "Sbc", [f"mod{layer}"])
        kb.dma("sp", rt[:], g["moe_router"][layer].rearrange("(kc p) e -> p kc e", p=128), writes=["rt"])

        def stageA(i):
            b = i % 2
            kb.dma("sp", xb[b][:], xin_v[i], writes=[f"xb{b}"])
            yield
            yield from norm_tile_g(kb, nc, xb[b][:], f"xb{b}", stt[b], Gbc, Sbc, h32[b][:], f"h32{b}")
            yield
            kb.op("act", lambda e, i=i, b=b: e.copy(out=hbf[:, i, :], in_=h32[b][:]), reads=[f"h32{b}"],
                  writes=[f"hbf{i}"])
            for half in range(2):
                for q in range(4):
                    kc = half * 4 + q
                    kb.op("pe", lambda e, kc=kc, q=q, b=b, half=half: e.transpose(
                        out=ps[half][:, q * 128:(q + 1) * 128], in_=h32[b][:, kc * 128:(kc + 1) * 128],
                        identity=c.ident[:]), reads=[f"h32{b}", "c_ident"], writes=[f"ps{half}"])
                yield
                kb.op("dve" if half == 0 else "act", (lambda e, half=half, b=b: e.tensor_copy(
                    out=hT[b][:, half * 4:(half + 1) * 4, :], in_=ps[half][:].rearrange("p (q t) -> p q t", q=4)))
                    if half == 0 else (lambda e, half=half, b=b: e.copy(
                        out=hT[b][:, half * 4:(half + 1) * 4, :], in_=ps[half][:].rearrange("p (q t) -> p q t", q=4))),
                    reads=[f"ps{half}"], writes=[f"hT{b}"])
                yield

        def stageB(i):
            b = i % 2
            for kc in range(8):
                kb.op("pe", lambda e, kc=kc, b=b: e.matmul(ps[2][:, 0:16], lhsT=hT[b][:, kc, :], rhs=rt[:, kc, :],
                                                         start=(kc == 0), stop=(kc == 7)),
                      reads=[f"hT{b}", "rt"], writes=["ps2"])
            yield
            kb.op("dve", lambda e: e.reduce_max(out=sm[:, 0:1], in_=ps[2][:, 0:16], axis=AX.X),
                  reads=["ps2"], writes=["sm"])
            kb.op("dve", lambda e: e.tensor_scalar(out=sm[:, 1:2], in0=sm[:, 0:1], scalar1=-1.0, scalar2=None,
                                                   op0=ALU.mult), reads=["sm"], writes=["sm"])
            yield
            kb.op("act", lambda e: e.activation(out=ex[:], in_=ps[2][:, 0:16], func=AF.Exp, bias=sm[:, 1:2],
                                                accum_out=sm[:, 2:3]), reads=["ps2", "sm"], writes=["ex", "sm"])
            yield
            kb.op("dve", lambda e: e.reciprocal(out=sm[:, 3:4], in_=sm[:, 2:3]), reads=["sm"], writes=["sm"])
            kb.op("dve", lambda e, i=i: e.tensor_scalar(out=aff[:, i, :], in0=ex[:], scalar1=sm[:, 3:4], scalar2=None,
                                                        op0=ALU.mult), reads=["ex", "sm"], writes=["aff"])
            yield
            kb.op("pe", lambda e, i=i: e.transpose(out=ps[3][0:16, 0:128], in_=aff[:, i, :], identity=c.ident[:]),
                  reads=["aff", "c_ident"], writes=["ps3"])
            yield
            kb.op("act", lambda e, i=i: e.copy(out=affT[:, i * 128:(i + 1) * 128], in_=ps[3][0:16, 0:128]),
                  reads=["ps3"], writes=["affT"])
            yield

        from itertools import zip_longest
        prevB = iter(())
        for i in range(NT):
            for _ in zip_longest(stageA(i), prevB):
                pass
            prevB = stageB(i)
        for _ in prevB:
            pass

        import os
        PH = int(os.environ.get("MOE_PH", "9"))
        if PH < 2:
            kb.dma("sp", x_out[0:128, 0:NT * 16], aff[:].rearrange("p n e -> p (n e)"), reads=["aff"], writes=["o"])
            kb.barrier()
            return
        kb.op("dve", lambda e: e.tensor_copy(out=work[:], in_=affT[:]), reads=["affT"], writes=["work"])
        nr = cap // 8
        for r in range(nr):
            kb.op("dve", lambda e: e.max(out=mx8[:], in_=work[:]), reads=["work"], writes=["mx8"])
            if r < nr - 1:
                kb.op("dve", lambda e: e.match_replace(out=work[:], in_to_replace=mx8[:], in_values=work[:],
                                                       imm_value=-1.0), reads=["work", "mx8"], writes=["work"])
        kb.op("dve", lambda e: e.tensor_scalar(out=maskT[:], in0=affT[:], scalar1=mx8[:, 7:8], scalar2=None,
                                               op0=ALU.is_ge), reads=["affT", "mx8"], writes=["maskT"])
        kb.op("pool", lambda e: e.memset(onesT[:], 1.0), reads=[], writes=["work"])
        kb.op("dve", lambda e: e.tensor_tensor_scan(out=slotT[:], data0=onesT[:], data1=maskT[:], initial=0.0,
                                                    op0=ALU.mult, op1=ALU.add),
              reads=["work", "maskT"], writes=["slotT"])
        kb.op("dve", lambda e: e.tensor_tensor(out=slotT[:], in0=slotT[:], in1=maskT[:], op=ALU.mult),
              reads=["slotT", "maskT"], writes=["slotT"])
        kb.op("dve", lambda e: e.tensor_scalar(out=slotT[:], in0=slotT[:], scalar1=-1.0, scalar2=None, op0=ALU.add),
              reads=["slotT"], writes=["slotT"])
        kb.op("pool", lambda e: e.tensor_tensor(out=gateT[:], in0=affT[:], in1=maskT[:], op=ALU.mult),
              reads=["affT", "maskT"], writes=["affT"])
        for i in range(NT):
            kb.op("pe", lambda e, i=i: e.transpose(out=ps[0][:, i * 16:(i + 1) * 16],
                                                   in_=slotT[:, i * 128:(i + 1) * 128], identity=c.ident[0:16, 0:16]),
                  reads=["slotT", "c_ident"], writes=["ps0"])
            kb.op("pe", lambda e, i=i: e.transpose(out=ps[1][:, i * 16:(i + 1) * 16],
                                                   in_=gateT[:, i * 128:(i + 1) * 128], identity=c.ident[0:16, 0:16]),
                  reads=["affT", "c_ident"], writes=["ps1"])
        kb.op("dve", lambda e: e.tensor_copy(out=slot[:], in_=ps[0][:, 0:NT * 16].rearrange("p (n e) -> p n e", e=16)),
              reads=["ps0"], writes=["slot"])
        kb.op("act", lambda e: e.copy(out=gate[:], in_=ps[1][:, 0:NT * 16].rearrange("p (n e) -> p n e", e=16)),
              reads=["ps1"], writes=["gate"])

        if PH < 3:
            kb.dma("sp", x_out[0:128, 0:NT * 16], slot[:].rearrange("p n e -> p (n e)"), reads=["slot"], writes=["o"])
            kb.dma("sp", x_out[128:256, 0:NT * 16], gate[:].rearrange("p n e -> p (n e)"), reads=["gate"], writes=["o2"])
            kb.barrier()
            return
        stg = [xb[0], xb[1], h32[0], h32[1]]
        stgk = ["xb0", "xb1", "h320", "h321"]
        wcnt = [0]

        def load_w(wv, wt, wk):
            for hh in range(2):
                kb.dma("pool", wt[:, hh * 4:(hh + 1) * 4, :], wv[:, hh * 4:(hh + 1) * 4, :], writes=[wk])

        def wviews_of(e_):
            return (g["moe_w1"][layer, e_].rearrange("(kc p) n -> p kc n", p=128),
                    g["moe_w3"][layer, e_].rearrange("(kc p) n -> p kc n", p=128),
                    g["moe_w2"][layer, e_].rearrange("(kc p) n -> p kc n", p=128))
        nsel = 0
        SUB = int(os.environ.get("MOE_SUB", "9"))
        NEXP = int(os.environ.get("MOE_NEXP", "16"))
        wv1, wv3, wv2 = wviews_of(0)
        load_w(wv1, w1b[0], "w1_0")
        load_w(wv3, w3b[0], "w3")
        load_w(wv2, w2b[0], "w2")
        for ex_i in range(NEXP):
            w1t = w1b[ex_i % 2]
            w1k = f"w1_{ex_i % 2}"
            if ex_i + 1 < NEXP:
                nwv1, nwv3, nwv2 = wviews_of(ex_i + 1)
                load_w(nwv1, w1b[(ex_i + 1) % 2], f"w1_{(ex_i + 1) % 2}")
            for i in range(NT):
                b = nsel % 2
                nsel += 1
                kb.op("dve", lambda e, i=i, b=b, ex_i=ex_i: e.tensor_scalar(
                    out=selT[b][:], in0=c.iota[:, 0:CW], scalar1=slot[:, i, ex_i:ex_i + 1], scalar2=None,
                    op0=ALU.is_equal), reads=["c_iota", "slot"], writes=[f"selT{b}"])
                for kc in range(8):
                    bank = (kc * CW) // 512
                    off = (kc * CW) % 512
                    kb.op("pe", lambda e, i=i, b=b, kc=kc, bank=bank, off=off: e.matmul(
                        ps[bank][:, off:off + CW], lhsT=hbf[:, i, kc * 128:(kc + 1) * 128], rhs=selT[b][:],
                        start=(i == 0 and off == 0), stop=(i == NT - 1), skip_group_check=True), reads=[f"hbf{i}", f"selT{b}"], writes=[f"ps{bank}"])
            nb = (8 * CW + 511) // 512
            per = 512 // CW if CW < 512 else 1
            for bank in range(nb):
                k0 = bank * per
                k1 = min(8, k0 + per)
                kb.op("act" if bank % 2 else "dve", (lambda e, bank=bank, k0=k0, k1=k1: e.copy(
                    out=xeT[:, k0:k1, :], in_=ps[bank][:, 0:(k1 - k0) * CW].rearrange("p (k c) -> p k c", c=CW)))
                    if bank % 2 else (lambda e, bank=bank, k0=k0, k1=k1: e.tensor_copy(
                        out=xeT[:, k0:k1, :], in_=ps[bank][:, 0:(k1 - k0) * CW].rearrange("p (k c) -> p k c", c=CW))),
                    reads=[f"ps{bank}"], writes=["xeT"])
            if SUB < 2:
                continue
            for fc in range(8):
                pb = ps[4 + fc % 2]
                pk = f"ps{4 + fc % 2}"
                for kc in range(8):
                    kb.op("pe", lambda e, fc=fc, kc=kc, pb=pb, w1t=w1t: e.matmul(
                        pb[:, 0:CW], lhsT=w1t[:, kc, fc * 128:(fc + 1) * 128], rhs=xeT[:, kc, :],
                        start=(kc == 0), stop=(kc == 7)), reads=[w1k, "xeT"], writes=[pk])
                for kc in range(8):
                    kb.op("pe", lambda e, fc=fc, kc=kc, pb=pb: e.matmul(
                        pb[:, 256:256 + CW], lhsT=w3b[0][:, kc, fc * 128:(fc + 1) * 128], rhs=xeT[:, kc, :],
                        start=(kc == 0), stop=(kc == 7)), reads=["w3", "xeT"], writes=[pk])
                sb_ = sil[fc % 2]
                DBG = int(os.environ.get("MOE_DBG", "9"))
                if DBG < 1:
                    continue
                kb.op("act", lambda e, pb=pb, sb_=sb_: e.activation(out=sb_[:], in_=pb[:, 0:CW], func=AF.Silu),
                      reads=[pk], writes=[f"sil{fc % 2}"])
                if DBG < 2:
                    continue
                kb.op("dve", lambda e, pb=pb, sb_=sb_, fc=fc: e.tensor_tensor(
                    out=hidT[:, fc, :], in0=sb_[:], in1=pb[:, 256:256 + CW], op=ALU.mult),
                    reads=[f"sil{fc % 2}", pk], writes=["hidT"])
            if ex_i + 1 < NEXP:
                load_w(nwv3, w3b[0], "w3")
            if SUB < 3:
                continue
            for ct in range(CT):
                for half in range(2):
                    pb = ps[6 + half]
                    pk = f"ps{6 + half}"
                    for fc in range(8):
                        kb.op("pe", lambda e, ct=ct, half=half, fc=fc, pb=pb: e.matmul(
                            pb[0:cs, :], lhsT=hidT[:, fc, ct * 128:ct * 128 + cs],
                            rhs=w2b[0][:, fc, half * 512:(half + 1) * 512], start=(fc == 0), stop=(fc == 7)),
                            reads=["hidT", "w2"], writes=[pk])
                    ys = yest[ex_i % 2]
                    kb.op("act" if half else "dve", (lambda e, ct=ct, half=half, pb=pb, ys=ys: e.copy(
                        out=ys[0:cs, ct, half * 512:(half + 1) * 512], in_=pb[0:cs, :]))
                        if half else (lambda e, ct=ct, half=half, pb=pb, ys=ys: e.tensor_copy(
                            out=ys[0:cs, ct, half * 512:(half + 1) * 512], in_=pb[0:cs, :])),
                        reads=[pk], writes=[f"yest{ex_i % 2}"])
            if ex_i + 1 < NEXP:
                load_w(nwv2, w2b[0], "w2")
            kb.dma("sp", g["ye_scr"][ex_i, 0:cs, 0:CT, :], yest[ex_i % 2][0:cs, :, :], reads=[f"yest{ex_i % 2}"],
                   writes=[("yescr", ex_i)])

        if PH < 4:
            kb.barrier()
            return
        kb.barrier()
        make_bc(kb, nc, c, lambda kc: mod[:, 5 * 8 + kc, stream:stream + 1], gate2, ps[0], "gate2", [f"mod{layer}"])
        if final_norm:
            make_bc(kb, nc, c, lambda kc: c.fnT[:, kc:kc + 1], Gbc, ps[0], "Gbc", ["c_fnT"])
        for ex_i in range(16):
            kb.dma(["sp", "act"][ex_i % 2], yeall[0:cs, ex_i, :, :], g["ye_scr"][ex_i, 0:cs, 0:CT, :],
                   reads=[("yescr", ex_i)], writes=[f"ye{ex_i}"])
        nsg = 0
        EG = 4
        for i in range(NT):
            b = i % 2
            kb.dma("sp", xb[b][:], xin_v[i], writes=[f"xb{b}"])
            for g0 in range(0, 16, EG):
                sg = nsg % 2
                nsg += 1
                sga = selGa[sg]
                kb.op("dve", lambda e, i=i, g0=g0, sga=sga: e.tensor_tensor(
                    out=sga[:, :, :], in0=c.iota[:, 0:CW].unsqueeze(1).broadcast_to([128, EG, CW]),
                    in1=slot[:, i, g0:g0 + EG].unsqueeze(2).broadcast_to([128, EG, CW]), op=ALU.is_equal),
                    reads=["c_iota", "slot"], writes=[f"selGa{sg}"])
                kb.op("pool", lambda e, i=i, g0=g0, sga=sga: e.tensor_tensor(
                    out=sga[:, :, :], in0=sga[:, :, :],
                    in1=gate[:, i, g0:g0 + EG].unsqueeze(2).broadcast_to([128, EG, CW]), op=ALU.mult),
                    reads=[f"selGa{sg}", "gate"], writes=[f"selGa{sg}"])
                pst = ps[2 + sg][:].bitcast(BF16)
                for ee in range(EG):
                    for ct in range(CT):
                        kb.op("pe", lambda e, ct=ct, ee=ee, sga=sga, pst=pst: e.transpose(
                            out=pst[0:cs, (ee * CT + ct) * 128:(ee * CT + ct + 1) * 128],
                            in_=sga[:, ee, ct * 128:ct * 128 + cs], identity=c.identb[:]),
                            reads=[f"selGa{sg}", "c_identb"], writes=[f"ps{2 + sg}"])
                kb.op("act", lambda e, sg=sg, pst=pst: e.copy(
                    out=selGca[sg][0:cs, :, :], in_=pst[0:cs, 0:EG * CT * 128].rearrange("p (c t) -> p c t", t=128)),
                    reads=[f"ps{2 + sg}"], writes=[f"selGca{sg}"])
                for ee in range(EG):
                    ex_i = g0 + ee
                    for half in range(2):
                        for ct in range(CT):
                            kb.op("pe", lambda e, half=half, ct=ct, sg=sg, ex_i=ex_i, ee=ee: e.matmul(
                                ps[half][:, :], lhsT=selGca[sg][0:cs, ee * CT + ct, :],
                                rhs=yeall[0:cs, ex_i, ct, half * 512:(half + 1) * 512],
                                start=(ex_i == 0 and ct == 0), stop=(ex_i == 15 and ct == CT - 1)),
                                reads=[f"selGca{sg}", f"ye{ex_i}"], writes=[f"ps{half}"])
            for half in range(2):
                sl = slice(half * 512, (half + 1) * 512)
                kb.op("dve", lambda e, half=half, sl=sl: e.tensor_tensor(
                    out=tmpo[half][:], in0=ps[half][:], in1=gate2[:, sl], op=ALU.mult),
                    reads=[f"ps{half}", "gate2"], writes=[f"tmpo{half}"])
                kb.op("pool", lambda e, half=half, sl=sl, b=b: e.tensor_tensor(
                    out=xb[b][:, sl], in0=xb[b][:, sl], in1=tmpo[half][:], op=ALU.add),
                    reads=[f"tmpo{half}", f"xb{b}"], writes=[f"xb{b}"])
            if final_norm:
                norm_tile(kb, nc, xb[b][:], f"xb{b}", stt[b], Gbc, None, h32[b][:], f"h32{b}")
                kb.dma("sp", xout_v[i], h32[b][:], reads=[f"h32{b}"], writes=[("dram", x_out.tensor.name, i)])
            else:
                kb.dma("sp", xout_v[i], xb[b][:], reads=[f"xb{b}"], writes=[("dram", x_out.tensor.name, i)])
        kb.barrier()


def host_consts():
    k = {}
    k["k_ident"] = np.eye(128, dtype=np.float32)
    k["k_iota"] = np.tile(np.arange(256, dtype=np.float32)[None, :], (128, 1))
    C, S, perm = rope_tables()
    k["k_ropeC"], k["k_ropeS"], k["k_perm"] = C, S, perm
    kk = np.arange(128)[:, None]
    qq = np.arange(128)[None, :]
    k["k_mL"] = np.tile((kk >= qq).astype(np.float32), (1, 4))
    k["k_mU"] = np.tile((kk <= qq).astype(np.float32), (1, 4))
    ss = np.arange(CHUNK)[:, None]
    tt = np.arange(CHUNK)[None, :]
    mus = (ss < tt).astype(np.float32)
    mui = (ss <= tt).astype(np.float32)
    k["k_maskA"] = np.ascontiguousarray(np.concatenate([-mus, mus, mui, mui], axis=1))
    k["k_maskT"] = np.ascontiguousarray(-(tt < ss).astype(np.float32))
    rm = np.ones((128, TP), np.float32)
    rm[:, CHUNK_COLS] = 0.0
    k["k_rmask"] = rm
    k["k_J"] = np.ascontiguousarray(np.eye(128, dtype=np.float32)[::-1])
    sel = np.zeros((16, 16, 128), np.float32)
    for r in range(16):
        sel[r, r, :] = 1.0
    k["k_sel"] = sel
    bdm = np.zeros((128, 128), np.float32)
    bdm[0:64, 0:64] = 1.0
    bdm[64:128, 64:128] = 1.0
    k["k_bd"] = bdm
    return k


def fm(v):
    v = np.asarray(v, np.float32)
    return np.ascontiguousarray(v.reshape(-1, 128).T)


def host_inputs(inp, b):
    m = dict(host_consts())
    m["x"] = np.ascontiguousarray(inp["x"][b])
    m["ctx"] = np.ascontiguousarray(inp["ctx"][b])
    m["cT"] = fm(inp["c"][b])
    m["ccT"] = fm(inp["c_ctx"])
    m["ada_w"] = inp["ada_w"]
    m["ada_bT"] = np.ascontiguousarray(np.stack([fm(inp["ada_b"][l]) for l in range(2)], axis=1))
    m["normT"] = np.ascontiguousarray(np.stack(
        [np.stack([fm(inp["norm_mix"][l]), fm(inp["norm_ffn"][l])], axis=1) for l in range(2)], axis=1))
    m["fnT"] = fm(inp["final_norm"])
    for k in ("moe_router", "moe_w1", "moe_w3", "moe_w2", "o_w_out"):
        m[k] = inp[k]
    w = inp["o_w_in"][0]
    kd = w[:, 1024:1280].reshape(1024, 4, 1, 64)
    kd = np.concatenate([kd, kd], axis=2).reshape(1024, 512)
    m["o_w_in2"] = np.ascontiguousarray(np.concatenate([w[:, 0:1024], kd, w[:, 1280:1536]], axis=1))
    m["o_sink"] = np.ascontiguousarray(inp["o_sink"].reshape(1, 16))
    idx = []
    for p in range(4):
        for off in (0, 512, 1024):
            idx += list(range(off + p * 128, off + p * 128 + 128))
    for d in range(2):
        idx += list(range(1536 + d * 64, 1536 + d * 64 + 64)) + list(range(1664 + d * 64, 1664 + d * 64 + 64))
    idx += list(range(1792, 1920))
    idxa = list(idx)
    for h in range(4):
        for part in range(3):
            idx += list(range(1920 + part * 512 + h * 128, 1920 + part * 512 + h * 128 + 128))
        idx += list(range(3472 + h * 128, 3472 + h * 128 + 128))
    idx += list(range(3456, 3472))
    m["e_w_in2"] = np.ascontiguousarray(inp["e_w_in"][0][:, idx])
    mu2 = inp["a_mu"][0][idxa]
    p64 = np.zeros((64, 64), np.float32)
    p64[:, 0:30] = mu2.reshape(30, 64).T
    for d in range(2):
        for h in range(8):
            p64[:, 30 + d * 8 + h] = inp["a_w0"][0, d, h * 64:(h + 1) * 64]
            p64[:, 46 + d * 8 + h] = inp["a_a0"][0, d, h * 64:(h + 1) * 64]
    m["mx_par64"] = p64
    p128 = np.zeros((128, 112), np.float32)
    p128[:, 0] = mu2[1536:1664]; p128[:, 1] = mu2[1664:1792]; p128[:, 2] = mu2[1792:1920]
    for h in range(8):
        p128[0:64, 8 + h] = inp["a_k_k"][0, h * 64:(h + 1) * 64]
        p128[0:64, 16 + h] = inp["a_k_a"][0, h * 64:(h + 1) * 64]
        p128[0:64, 32 + h] = inp["a_r_k"][0, h]
    p128[0:8, 40] = inp["b_dt_bias"][0].reshape(8)
    p128[0:8, 41] = inp["b_a_log"][0].reshape(8)
    for h in range(4):
        for part in range(3):
            for j in range(5):
                p128[:, 48 + (h * 3 + part) * 5 + j] = inp["b_conv"][0, j, part * 512 + h * 128:part * 512 + (h + 1) * 128]
    m["mx_par128"] = p128
    pP = np.zeros((128, 64), np.float32)
    pP[:, 0:12] = mu2[0:1536].reshape(12, 128).T
    for p in range(4):
        sl = slice(p * 128, (p + 1) * 128)
        for d in range(2):
            pP[:, 12 + d * 4 + p] = inp["a_w0"][0, d, sl]
            pP[:, 20 + d * 4 + p] = inp["a_a0"][0, d, sl]
        pP[:, 28 + p] = inp["a_k_k"][0, sl]
        pP[:, 32 + p] = inp["a_k_a"][0, sl]
        pP[:, 40 + p] = inp["a_r_k"][0].reshape(512)[sl]
    m["mx_parP"] = pP
    m["mx_w2"] = np.ascontiguousarray(np.concatenate([inp["a_w2"][0].transpose(1, 0, 2), inp["a_a2"][0].transpose(1, 0, 2)], axis=0))
    m["mx_bc"] = np.ascontiguousarray(np.concatenate([inp["a_ln_w"][0], inp["a_ln_b"][0], np.tile(inp["b_norm"][0], 4)])[None, :])
    m["a_g2"] = inp["a_g2"]
    m["e_w_out"] = inp["e_w_out"]
    return m


IN_SHAPES = {
    "k_ident": [128, 128], "k_iota": [128, 256],
    "x": [2048, D], "ctx": [256, D], "cT": [128, 8], "ccT": [128, 8],
    "ada_w": [2, D, 6 * D], "ada_bT": [128, 2, 48], "normT": [128, 2, 2, 8], "fnT": [128, 8],
    "k_ropeC": [128, 2048], "k_ropeS": [128, 2048], "k_perm": [128, 128], "k_mL": [128, 512], "k_mU": [128, 512],
    "o_w_in2": [D, 1792], "o_w_out": [1, D, D], "o_sink": [1, 16],
    "k_maskA": [CHUNK, 4 * CHUNK], "k_maskT": [CHUNK, CHUNK], "k_rmask": [128, TP], "k_J": [128, 128], "k_sel": [16, 16, 128],
    "e_w_in2": [D, 3984], "mx_par64": [64, 64], "mx_parP": [128, 64], "k_bd": [128, 128], "mx_par128": [128, 112], "mx_w2": [128, 2, 512], "mx_bc": [1, 1536],
    "a_g2": [1, 128, 512], "e_w_out": [1, D, D],
    "moe_router": [2, D, 16], "moe_w1": [2, 16, D, D], "moe_w3": [2, 16, D, D], "moe_w2": [2, 16, D, D],
}


def build(stages=("all",), extra_in=(), outs=(("out", [2048, D]),)):
    from contextlib import ExitStack
    nc = bass.Bass("TRN2", target_bir_lowering=False)
    g = {}
    for name, shape in IN_SHAPES.items():
        g[name] = nc.dram_tensor(name, shape, F32, kind="ExternalInput").ap()
    for name, shape in extra_in:
        g[name] = nc.dram_tensor(name, shape, F32, kind="ExternalInput").ap()
    for name, shape in outs:
        g[name] = nc.dram_tensor(name, shape, F32, kind="ExternalOutput").ap()
    g["ye_scr"] = nc.dram_tensor("ye_scr", [16, 128, 2, D], BF16).ap()
    g["ys_scr"] = nc.dram_tensor("ys_scr", [2, 2304, 1536], F32).ap()
    g["z_scr"] = nc.dram_tensor("z_scr", [2304, 512], F32).ap()
    for nm, rows in (("xm_lat", 2048), ("xm_ctx", 256), ("x1_lat", 2048), ("x1_ctx", 256), ("x2_lat", 2048)):
        g[nm] = nc.dram_tensor(nm, [rows, D], F32).ap()
    kb = KB(nc)
    with ExitStack() as es:
        c = load_consts(kb, nc, es, g)
        c.fnT = es.enter_context(nc.sbuf_tensor("c_fnT", [128, 8], F32))
        kb.dma("sp", c.fnT[:], g["fnT"][:, :], writes=["c_fnT"])
        mods = prologue(kb, nc, es, g, c)
        for st in stages:
            if st == "moe0l_test":
                moe_stage(kb, nc, g, c, mods, 0, 0, g["t_in"], g["out"], 2048)
            elif st == "moe0c_test":
                moe_stage(kb, nc, g, c, mods, 0, 1, g["t_in"], g["out"], 256)
            elif st == "moe1l_test":
                moe_stage(kb, nc, g, c, mods, 1, 0, g["t_in"], g["out"], 2048, final_norm=True)
            elif st == "all":
                mixer_stage(kb, nc, g, c, mods, g["x"], g["ctx"], g["xm_lat"], g["xm_ctx"])
                moe_stage(kb, nc, g, c, mods, 0, 0, g["xm_lat"], g["x1_lat"], 2048)
                moe_stage(kb, nc, g, c, mods, 0, 1, g["xm_ctx"], g["x1_ctx"], 256)
                attn_stage(kb, nc, g, c, mods, g["x1_lat"], g["x1_ctx"], g["x2_lat"])
                moe_stage(kb, nc, g, c, mods, 1, 0, g["x2_lat"], g["out"], 2048, final_norm=True)
            elif st == "mixer_test":
                mixer_stage(kb, nc, g, c, mods, g["x"], g["ctx"], g["out"], g["out2"])
            elif st == "attn_test":
                attn_stage(kb, nc, g, c, mods, g["t_in"], g["t_in2"], g["out"])
            elif st == "mods_test":
                for l in range(2):
                    kb.dma("sp", g["out"][l * 128:(l + 1) * 128, 0:96], mods[l][:].rearrange("p a b -> p (a b)"),
                           reads=[f"mod{l}"], writes=[("o", l)])
        kb.finish([])
    return nc, kb


def rope_tables():
    quarter = 16
    inv = (10000.0 ** (-np.arange(quarter, dtype=np.float32) / quarter)).astype(np.float32)
    t = np.arange(2048)
    row = (t // 64).astype(np.float32)
    col = (t % 64).astype(np.float32)
    C = np.zeros((128, 2048), np.float32)
    S = np.zeros((128, 2048), np.float32)
    perm = np.zeros((128, 128), np.float32)
    for p in range(128):
        d = p % 64
        pos = row if d < 32 else col
        i = d % 16
        ang = (pos * inv[i]).astype(np.float32)
        C[p] = np.cos(ang)
        second = (d % 32) >= 16
        S[p] = np.sin(ang) if second else -np.sin(ang)
        partner = p - 16 if second else p + 16
        perm[partner, p] = 1.0
    return C, S, perm


def attn_stage(kb, nc, g, c, mods, x_lat_in, x_ctx_in, x_out):
    layer = 1
    mod = mods[layer]
    NT = 18
    from contextlib import ExitStack
    with ExitStack() as es:
        def sb(name, shape, dt):
            return es.enter_context(nc.sbuf_tensor(f"at_{name}", shape, dt))
        Gbc = sb("G", [128, D], F32)
        Sbc = sb("S", [128, D], F32)
        xb = [sb("x0", [128, D], F32), sb("x1", [128, D], F32)]
        hb = [sb("h0", [128, D], BF16), sb("h1", [128, D], BF16)]
        stt = [sb("st0", [128, 4], F32), sb("st1", [128, 4], F32)]
        hT = sb("hT", [128, 8, 2304], BF16)
        win = sb("win", [128, 8, 1792], BF16)
        wo = sb("wo", [128, 8, D], BF16)
        stg = [sb("stg0", [128, D], F32), sb("stg1", [128, D], F32)]
        Ct = sb("Ct", [128, 2048], F32)
        St = sb("St", [128, 2048], F32)
        perm = sb("perm", [128, 128], F32)
        qraw = [sb("qraw0", [128, 512], F32), sb("qraw1", [128, 512], F32)]
        rt1 = [sb("rt10", [128, 512], F32), sb("rt11", [128, 512], F32)]
        qT = sb("qT", [128, 8, 2048], BF16)
        kT = sb("kT", [128, 4, 2304], BF16)
        V = sb("V", [128, NT, 4, 65], BF16)
        mL = sb("mL", [128, 512], BF16)
        mU = sb("mU", [128, 512], BF16)
        esink = sb("esink", [128, 16], F32)
        PT = [sb(f"PT{i}", [128, 512], BF16) for i in range(2)]
        osb = stg[0]
        den = sb("den", [128, 4], F32)
        oT = sb("oT", [128, 8, 128], BF16)
        tmpo = qraw
        ps = [es.enter_context(nc.psum_tensor(f"at_ps{i}", [128, 512], F32)) for i in range(8)]

        kb.dma("sp", Ct[:], g["k_ropeC"][:, :], writes=["Ct"])
        kb.dma("sp", St[:], g["k_ropeS"][:, :], writes=["St"])
        kb.dma("sp", perm[:], g["k_perm"][:, :], writes=["perm"])
        kb.dma("sp", esink[:], g["o_sink"].partition_broadcast(128), writes=["esink"])
        kb.op("act", lambda e: e.activation(out=esink[:], in_=esink[:], func=AF.Exp), reads=["esink"], writes=["esink"])
        kb.dma("sp", stg[0][:, 0:512], g["k_mL"][:, :], writes=["stg0"])
        kb.op("dve", lambda e: e.tensor_copy(out=mL[:], in_=stg[0][:, 0:512]), reads=["stg0"], writes=["mL"])
        kb.dma("sp", stg[1][:, 0:512], g["k_mU"][:, :], writes=["stg1"])
        kb.op("dve", lambda e: e.tensor_copy(out=mU[:], in_=stg[1][:, 0:512]), reads=["stg1"], writes=["mU"])
        make_bc(kb, nc, c, lambda kc: mod[:, 1 * 8 + kc, 1:2], Gbc, ps[0], "Gbc", [f"mod{layer}"])
        make_bc(kb, nc, c, lambda kc: mod[:, 0 * 8 + kc, 1:2], Sbc, ps[0], "Sbc", [f"mod{layer}"])

        wcnt = [0]

        def load_rows(src_ap, dst_fn, ncols, key):
            for kc in range(8):
                j = wcnt[0] % 2
                wcnt[0] += 1
                kb.dma(["sp", "act"][kc % 2], stg[j][:, 0:ncols], src_ap[:, kc, :], writes=[f"stg{j}"])
                ce = ["dve", "pool"][wcnt[0] % 2]
                kb.op(ce, lambda e, j=j, kc=kc: e.tensor_copy(out=dst_fn(kc), in_=stg[j][:, 0:ncols]),
                      reads=[f"stg{j}"], writes=[key])
        wv = g["o_w_in2"].rearrange("(kc p) n -> p kc n", p=128)
        load_rows(wv[:, :, 0:1024], lambda kc: win[:, kc, 0:1024], 1024, "win")
        load_rows(wv[:, :, 1024:1792], lambda kc: win[:, kc, 1024:1792], 768, "win")
        load_rows(g["o_w_out"][0].rearrange("(kc p) n -> p kc n", p=128), lambda kc: wo[:, kc, :], 1024, "wo")

        for i in range(NT):
            b = i % 2
            if i == 2:
                kb.barrier()
                make_bc(kb, nc, c, lambda kc: mod[:, 1 * 8 + kc, 0:1], Gbc, ps[0], "Gbc", [f"mod{layer}"])
                make_bc(kb, nc, c, lambda kc: mod[:, 0 * 8 + kc, 0:1], Sbc, ps[0], "Sbc", [f"mod{layer}"])
            src = x_ctx_in[i * 128:(i + 1) * 128, :] if i < 2 else x_lat_in[(i - 2) * 128:(i - 1) * 128, :]
            kb.dma("sp", xb[b][:], src, writes=[f"xb{b}"])
            norm_tile(kb, nc, xb[b][:], f"xb{b}", stt[b], Gbc, Sbc, stg[b][:], f"stg{b}")
            kb.op("act", lambda e, b=b: e.copy(out=hb[b][:], in_=stg[b][:]), reads=[f"stg{b}"], writes=[f"hb{b}"])
            for half in range(2):
                pst = ps[half][:].bitcast(BF16)
                for q in range(4):
                    kc = half * 4 + q
                    kb.op("pe", lambda e, kc=kc, q=q, b=b, pst=pst: e.transpose(
                        out=pst[:, q * 128:(q + 1) * 128], in_=hb[b][:, kc * 128:(kc + 1) * 128],
                        identity=c.identb[:]), reads=[f"hb{b}", "c_identb"], writes=[f"ps{half}"])
                kb.op("dve" if half == 0 else "pool" if False else "act",
                      (lambda e, half=half, pst=pst, i=i: e.tensor_copy(
                          out=hT[:, half * 4:(half + 1) * 4, i * 128:(i + 1) * 128],
                          in_=pst[:, 0:512].rearrange("p (q t) -> p q t", q=4))) if half == 0 else
                      (lambda e, half=half, pst=pst, i=i: e.copy(
                          out=hT[:, half * 4:(half + 1) * 4, i * 128:(i + 1) * 128],
                          in_=pst[:, 0:512].rearrange("p (q t) -> p q t", q=4))),
                      reads=[f"ps{half}"], writes=["hT"])

        import os
        APH = int(os.environ.get("ATT_PH", "9"))
        if APH < 1:
            kb.barrier(); return
        nb = 0
        for nq in range(8):
            for tb in range(4):
                b = nb % 2
                nb += 1
                pq = ps[2 + b]
                t0 = 256 + tb * 512
                for kc in range(8):
                    kb.op("pe", lambda e, kc=kc, nq=nq, t0=t0, pq=pq: e.matmul(
                        pq[:], lhsT=win[:, kc, nq * 128:(nq + 1) * 128], rhs=hT[:, kc, t0:t0 + 512],
                        start=(kc == 0), stop=(kc == 7)), reads=["win", "hT"], writes=[f"ps{2 + b}"])
                kb.op("act", lambda e, b=b, pq=pq: e.copy(out=qraw[b][:], in_=pq[:]), reads=[f"ps{2 + b}"],
                      writes=[f"qraw{b}"])
                pw = ps[4 + b]
                kb.op("pe", lambda e, b=b, pw=pw: e.matmul(pw[:], lhsT=perm[:], rhs=qraw[b][:], start=True, stop=True),
                      reads=["perm", f"qraw{b}"], writes=[f"ps{4 + b}"])
                cs_ = slice(tb * 512, (tb + 1) * 512)
                kb.op("dve", lambda e, b=b, pw=pw, cs_=cs_: e.scalar_tensor_tensor(
                    out=rt1[b][:], in0=pw[:], scalar=0.125, in1=St[:, cs_], op0=ALU.mult, op1=ALU.mult),
                      reads=[f"ps{4 + b}", "St"], writes=[f"rt1{b}"])
                kb.op("pool", lambda e, b=b, cs_=cs_: e.tensor_tensor(out=qraw[b][:], in0=qraw[b][:], in1=Ct[:, cs_],
                                                                    op=ALU.mult),
                      reads=[f"qraw{b}", "Ct"], writes=[f"qraw{b}"])
                kb.op("dve", lambda e, b=b, nq=nq, cs_=cs_: e.scalar_tensor_tensor(
                    out=qT[:, nq, cs_], in0=qraw[b][:], scalar=0.125, in1=rt1[b][:], op0=ALU.mult, op1=ALU.add),
                    reads=[f"qraw{b}", f"rt1{b}"], writes=["qT"])
        if APH < 2:
            kb.barrier(); return
        for hk in range(4):
            for tb in range(5):
                b = nb % 2
                nb += 1
                pq = ps[2 + b]
                t0 = 0 if tb == 0 else 256 + (tb - 1) * 512
                tw = 256 if tb == 0 else 512
                for kc in range(8):
                    kb.op("pe", lambda e, kc=kc, hk=hk, t0=t0, tw=tw, pq=pq: e.matmul(
                        pq[:, 0:tw], lhsT=win[:, kc, 1024 + hk * 128:1024 + (hk + 1) * 128],
                        rhs=hT[:, kc, t0:t0 + tw], start=(kc == 0), stop=(kc == 7)),
                        reads=["win", "hT"], writes=[f"ps{2 + b}"])
                if tb == 0:
                    kb.op("act", lambda e, hk=hk, pq=pq: e.copy(out=kT[:, hk, 0:256], in_=pq[:, 0:256]),
                          reads=[f"ps{2 + b}"], writes=["kT"])
                    continue
                kb.op("act", lambda e, b=b, pq=pq: e.copy(out=qraw[b][:], in_=pq[:]), reads=[f"ps{2 + b}"],
                      writes=[f"qraw{b}"])
                pw = ps[4 + b]
                kb.op("pe", lambda e, b=b, pw=pw: e.matmul(pw[:], lhsT=perm[:], rhs=qraw[b][:], start=True, stop=True),
                      reads=["perm", f"qraw{b}"], writes=[f"ps{4 + b}"])
                cs_ = slice((tb - 1) * 512, tb * 512)
                kb.op("dve", lambda e, b=b, pw=pw, cs_=cs_: e.tensor_tensor(out=rt1[b][:], in0=pw[:], in1=St[:, cs_],
                                                                         op=ALU.mult),
                      reads=[f"ps{4 + b}", "St"], writes=[f"rt1{b}"])
                kb.op("pool", lambda e, b=b, cs_=cs_: e.tensor_tensor(out=qraw[b][:], in0=qraw[b][:], in1=Ct[:, cs_],
                                                                    op=ALU.mult),
                      reads=[f"qraw{b}", "Ct"], writes=[f"qraw{b}"])
                kb.op("dve", lambda e, b=b, hk=hk, t0=t0: e.tensor_tensor(
                    out=kT[:, hk, t0:t0 + 512], in0=qraw[b][:], in1=rt1[b][:], op=ALU.add),
                    reads=[f"qraw{b}", f"rt1{b}"], writes=["kT"])
        if APH < 3:
            kb.barrier(); return
        kb.op("pool", lambda e: e.memset(V[:], 1.0), writes=["V"])
        for i in range(NT):
            b = nb % 2
            nb += 1
            pq = ps[2 + b]
            for kc in range(8):
                kb.op("pe", lambda e, kc=kc, i=i, pq=pq: e.matmul(
                    pq[:, 0:256], lhsT=hT[:, kc, i * 128:(i + 1) * 128], rhs=win[:, kc, 1536:1792],
                    start=(kc == 0), stop=(kc == 7)), reads=["win", "hT"], writes=[f"ps{2 + b}"])
            kb.op("act", lambda e, i=i, pq=pq: e.copy(out=V[:, i, :, 0:64],
                                                     in_=pq[:, 0:256].rearrange("p (h d) -> p h d", h=4)),
                  reads=[f"ps{2 + b}"], writes=["V"])

        if APH < 4:
            kb.barrier(); return
        make_bc(kb, nc, c, lambda kc: mod[:, 2 * 8 + kc, 0:1], Gbc, ps[0], "Gbc", [f"mod{layer}"])
        nsb = 0
        for n in range(16):
            kb.dma("sp", xb[n % 2][:], x_lat_in[n * 128:(n + 1) * 128, :], writes=[f"xb{n % 2}"])
            for hk in range(4):
                tiles = []
                if n > 0:
                    tiles.append((2 + n - 1, mL, "mL"))
                tiles.append((2 + n, None, None))
                if n < 15:
                    tiles.append((2 + n + 1, mU, "mU"))
                tiles.append((0, None, None))
                tiles.append((1, None, None))
                po = ps[6 + (n * 4 + hk) % 2]
                pok = f"ps{6 + (n * 4 + hk) % 2}"
                for ti, (kt, msk, mk) in enumerate(tiles):
                    sbk = nsb % 2
                    nsb += 1
                    pSa, pSb = ps[1 + 2 * sbk], ps[2 + 2 * sbk]
                    ka, kbk = f"ps{1 + 2 * sbk}", f"ps{2 + 2 * sbk}"
                    for gq in range(4):
                        hq = hk * 4 + gq
                        bp = (hq % 2) * 64
                        pS = pSa if bp == 0 else pSb
                        kb.op("pe", lambda e, gq=gq, hq=hq, bp=bp, kt=kt, pS=pS, hk=hk, n=n: e.matmul(
                            pS[:, (gq // 2) * 128:(gq // 2 + 1) * 128], lhsT=kT[bp:bp + 64, hk, kt * 128:(kt + 1) * 128],
                            rhs=qT[bp:bp + 64, hq // 2, n * 128:(n + 1) * 128], start=True, stop=True),
                            reads=["kT", "qT"], writes=[ka if bp == 0 else kbk])
                    ptv = PT[sbk][:].rearrange("p (a b q) -> p a b q", a=2, b=2)
                    kb.op("act", lambda e, pSa=pSa, ptv=ptv: e.activation(
                        out=ptv[:, :, 0, :], in_=pSa[:, 0:256].rearrange("p (a q) -> p a q", a=2), func=AF.Exp),
                        reads=[ka], writes=[f"PT{sbk}"])
                    kb.op("act", lambda e, pSb=pSb, ptv=ptv: e.activation(
                        out=ptv[:, :, 1, :], in_=pSb[:, 0:256].rearrange("p (a q) -> p a q", a=2), func=AF.Exp),
                        reads=[kbk], writes=[f"PT{sbk}"])
                    ADBG = int(os.environ.get("ATT_DBG", "9"))
                    if ADBG < 2:
                        continue
                    if msk is not None:
                        kb.op("dve", lambda e, sbk=sbk, msk=msk: e.tensor_tensor(out=PT[sbk][:], in0=PT[sbk][:],
                                                                               in1=msk[:], op=ALU.mult),
                              reads=[f"PT{sbk}", mk], writes=[f"PT{sbk}"])
                    for gq in range(4):
                        kb.op("pe", lambda e, gq=gq, sbk=sbk, kt=kt, hk=hk, po=po, ti=ti: e.matmul(
                            po[:, gq * 65:(gq + 1) * 65], lhsT=PT[sbk][:, gq * 128:(gq + 1) * 128],
                            rhs=V[:, kt, hk, :], start=(ti == 0 and gq == 0), stop=(ti == len(tiles) - 1),
                            skip_group_check=True), reads=[f"PT{sbk}", "V"], writes=[pok])
                if ADBG < 3:
                    continue
                pov = po[:, 0:260].rearrange("p (g d) -> p g d", g=4)
                kb.op("dve", lambda e, pov=pov, hk=hk: e.tensor_tensor(
                    out=den[:], in0=pov[:, :, 64], in1=esink[:, hk * 4:(hk + 1) * 4], op=ALU.add),
                    reads=[pok, "esink"], writes=["den"])
                kb.op("dve", lambda e: e.reciprocal(out=den[:], in_=den[:]), reads=["den"], writes=["den"])
                kb.op("dve", lambda e, pov=pov, hk=hk: e.tensor_tensor(
                    out=osb[:, hk * 256:(hk + 1) * 256].rearrange("p (g d) -> p g d", g=4), in0=pov[:, :, 0:64],
                    in1=den[:].unsqueeze(2).broadcast_to([128, 4, 64]), op=ALU.mult),
                    reads=[pok, "den"], writes=["stg0"])
            if ADBG < 4:
                continue
            kb.op("act", lambda e: e.copy(out=hb[0][:], in_=osb[:]), reads=["stg0"], writes=["hb0"])
            pst = ps[0][:].bitcast(BF16)
            for kc in range(8):
                kb.op("pe", lambda e, kc=kc, pst=pst: e.transpose(
                    out=pst[:, kc * 128:(kc + 1) * 128], in_=hb[0][:, kc * 128:(kc + 1) * 128], identity=c.identb[:]),
                    reads=["hb0", "c_identb"], writes=["ps0"])
            kb.op("act", lambda e, pst=pst: e.copy(out=oT[:], in_=pst[:].rearrange("p (k t) -> p k t", k=8)),
                  reads=["ps0"], writes=["oT"])
            for half in range(2):
                pp = ps[5] if half == 0 else ps[0]
                for kc in range(8):
                    kb.op("pe", lambda e, kc=kc, half=half, pp=pp: e.matmul(
                        pp[:], lhsT=oT[:, kc, :], rhs=wo[:, kc, half * 512:(half + 1) * 512],
                        start=(kc == 0), stop=(kc == 7)), reads=["oT", "wo"], writes=["ps5" if half == 0 else "ps0"])
                sl = slice(half * 512, (half + 1) * 512)
                kb.op("dve", lambda e, half=half, pp=pp, sl=sl: e.tensor_tensor(
                    out=tmpo[half][:], in0=pp[:], in1=Gbc[:, sl], op=ALU.mult),
                    reads=["ps5" if half == 0 else "ps0", "Gbc"], writes=[f"qraw{half}"])
                kb.op("pool", lambda e, half=half, sl=sl, n=n: e.tensor_tensor(
                    out=xb[n % 2][:, sl], in0=xb[n % 2][:, sl], in1=tmpo[half][:], op=ALU.add),
                    reads=[f"qraw{half}", f"xb{n % 2}"], writes=[f"xb{n % 2}"])
            kb.dma("sp", x_out[n * 128:(n + 1) * 128, :], xb[n % 2][:], reads=[f"xb{n % 2}"],
                   writes=[("dram", x_out.tensor.name, n)])
        kb.barrier()


def dplr_scan(kb, nc, c, T, dk, rT, kkT, kT, bT, vT, Pinc, prodT, store_cb, hk_, Gb=None, rA=None, kkA=None, bp=0, rC=None, kkC=None):
    ps = T["ps"]
    rA = rT if rA is None else rA
    kkA = kkT if kkA is None else kkA
    rC = rT if rC is None else rC
    kkC = kkT if kkC is None else kkC
    tb = vT.dtype == BF16
    ident, maskA, maskT, ident64 = c.ident, T["maskA"], T["maskT"], c.ident
    ST = T["ST"]
    kb.op("dve", lambda e: e.memset(ST[bp:bp + dk, 0:dk], 0.0), writes=["ST"])
    kb.op("dve", lambda e: e.memset(T["STb"][bp:bp + dk, 0:dk], 0.0), writes=["STb"])
    CH = CHUNK
    NLV = {64: 5, 128: 6}[CH]
    NCH = len(CHUNK_COLS)
    GB = 2

    def inv_gen(g0):
        grp = list(range(g0, min(NCH, g0 + GB)))
        par = (g0 // GB) % 2
        for ci in grp:
            s = ci % GB + GB * par
            cs = slice(CHUNK_COLS[ci], CHUNK_COLS[ci] + CH)
            pa = ps[s % 2]
            pk = f"ps{s % 2}"
            pn = ps[2 + s % 2]
            pnk = f"ps{2 + s % 2}"
            for j, (l, r) in enumerate(((bT, kkA), (kT, kkA), (bT, rA), (kT, rA), (kkA, bT))):
                if j < 4:
                    kb.op("pe", lambda e, j=j, l=l, r=r, cs=cs, pa=pa: e.matmul(
                        pa[0:CH, j * CH:(j + 1) * CH], lhsT=l[bp:bp + dk, cs], rhs=r[bp:bp + dk, cs], start=True, stop=True),
                        reads=[hk_], writes=[pk])
                else:
                    kb.op("pe", lambda e, l=l, r=r, cs=cs, pn=pn: e.matmul(
                        pn[0:CH, 256:256 + CH], lhsT=l[bp:bp + dk, cs], rhs=r[bp:bp + dk, cs], start=True, stop=True),
                        reads=[hk_], writes=[pnk])
            mA, mT_, mAk, mTk = maskA[0:CH, :], maskT[0:CH, :], "maskA", "maskT"
            if Gb is not None:
                c0_ = CHUNK_COLS[ci]
                kb.op("pe", lambda e, cs=cs, pn=pn: e.transpose(out=pn[0:CH, 384:385], in_=Gb[0:1, cs],
                                                              identity=ident[0:1, 0:1]), reads=[hk_, "c_ident"], writes=[pnk])
                kb.op("act", lambda e, s=s, pn=pn: e.copy(out=T["Gc"][0:CH, s:s + 1], in_=pn[0:CH, 384:385]),
                      reads=[pnk], writes=[f"Gc{s}"])
                kb.op("dve", lambda e, s=s, cs=cs: e.tensor_scalar(out=T["Dt"][0:CH, s % GB, :], in0=Gb[0:CH, cs],
                                                                 scalar1=T["Gc"][0:CH, s:s + 1], scalar2=0.0,
                                                                 op0=ALU.subtract, op1=ALU.min),
                      reads=[hk_, f"Gc{s}"], writes=[f"Dt{s % GB}"])
                kb.op("act", lambda e, s=s: e.activation(out=T["Dt"][0:CH, s % GB, :], in_=T["Dt"][0:CH, s % GB, :], func=AF.Exp),
                      reads=[f"Dt{s % GB}"], writes=[f"Dt{s % GB}"])
                kb.op("dve", lambda e, s=s, cs=cs: e.tensor_scalar(out=T["Dts"][0:CH, s % GB, :], in0=Gb[0:CH, cs],
                                                                 scalar1=T["Gc"][0:CH, s:s + 1], scalar2=0.0,
                                                                 op0=ALU.subtract, op1=ALU.max),
                      reads=[hk_, f"Gc{s}"], writes=[f"Dts{s % GB}"])
                kb.op("act", lambda e, s=s: e.activation(out=T["Dts"][0:CH, s % GB, :], in_=T["Dts"][0:CH, s % GB, :], func=AF.Exp,
                                                         scale=-1.0), reads=[f"Dts{s % GB}"], writes=[f"Dts{s % GB}"])
                kb.op("dve", lambda e, s=s: e.tensor_tensor(
                    out=T["mD"][0:CH, s % GB, :].rearrange("p (a t) -> p a t", a=4),
                    in0=maskA[0:CH, :].rearrange("p (a t) -> p a t", a=4),
                    in1=T["Dt"][0:CH, s % GB, :].unsqueeze(1).broadcast_to([CH, 4, CH]), op=ALU.mult),
                    reads=["maskA", f"Dt{s % GB}"], writes=[f"mD{s % GB}"])
                kb.op("pool", lambda e, s=s: e.tensor_tensor(out=T["Dts"][0:CH, s % GB, :], in0=T["Dts"][0:CH, s % GB, :],
                                                            in1=maskT[0:CH, :], op=ALU.mult),
                      reads=["maskT", f"Dts{s % GB}"], writes=[f"Dts{s % GB}"])
                kb.op("act", lambda e, s=s, c0_=c0_: e.activation(out=T["dL"][0:CH, s:s + 1], in_=T["Gc"][0:CH, s:s + 1],
                                                                func=AF.Exp, scale=-1.0, bias=Gb[0:CH, c0_ + CH - 1:c0_ + CH]),
                      reads=[f"Gc{s}", hk_], writes=[f"dL{s}"])
                mA, mT_, mAk, mTk = T["mD"][0:CH, s % GB, :], T["Dts"][0:CH, s % GB, :], f"mD{s % GB}", f"Dts{s % GB}"
            kb.op("dve", lambda e, s=s, pa=pa, mA=mA: e.tensor_tensor(out=T["AMf"][0:CH, s, :], in0=pa[0:CH, 0:CH],
                                                                     in1=mA[:, 0:CH], op=ALU.mult),
                  reads=[pk, mAk], writes=[f"AM{s}"])
            kb.op("dve", lambda e, s=s, pa=pa, mA=mA: e.tensor_tensor(out=T["AMb"][0:CH, s, :], in0=pa[0:CH, CH:4 * CH],
                                                                     in1=mA[:, CH:4 * CH], op=ALU.mult),
                  reads=[pk, mAk], writes=[f"AMb{s}"])
            kb.op("dve", lambda e, s=s, pn=pn, mT_=mT_: e.tensor_tensor(out=T["MM"][0][0:CH, s, CH:2 * CH],
                                                                       in0=pn[0:CH, 256:256 + CH], in1=mT_, op=ALU.mult),
                  reads=[pnk, mTk], writes=[f"MM0_{s}"])
            kb.op("pool", lambda e, s=s: e.tensor_copy(out=T["MM"][0][0:CH, s, 0:CH], in_=T["AMf"][0:CH, s, :]),
                  reads=[f"AM{s}"], writes=[f"MM0_{s}"])
            kb.op("pool", lambda e, s=s: e.tensor_tensor(out=T["Q"][0][0:CH, s, :], in0=T["AMf"][0:CH, s, :],
                                                        in1=ident64[0:CH, 0:CH], op=ALU.add),
                  reads=[f"AM{s}", "c_ident"], writes=[f"Q0_{s}"])
            yield
        for lv in range(NLV):
            a, b = lv % 2, (lv + 1) % 2
            last = lv == NLV - 1
            for ci in grp:
                s = ci % GB + GB * par
                pm = ps[2 + s % 2]
                pmk = f"ps{2 + s % 2}"
                MMa = T["MM"][a]
                kb.op("pe", lambda e, s=s, pm=pm, MMa=MMa: e.matmul(
                    pm[0:CH, 0:CH], lhsT=MMa[0:CH, s, CH:2 * CH], rhs=MMa[0:CH, s, 0:CH], start=True, stop=True),
                    reads=[f"MM{a}_{s}"], writes=[pmk])
                kb.op("pe", lambda e, s=s, pm=pm, MMa=MMa: e.matmul(
                    pm[0:CH, CH:2 * CH], lhsT=MMa[0:CH, s, 0:CH], rhs=MMa[0:CH, s, CH:2 * CH], start=True, stop=True),
                    reads=[f"MM{a}_{s}"], writes=[pmk])
                kb.op("act", lambda e, s=s, pm=pm, b=b: e.copy(out=T["MM"][b][0:CH, s, :], in_=pm[0:CH, 0:2 * CH]),
                      reads=[pmk], writes=[f"MM{b}_{s}"])
                yield
            for ci in grp:
                s = ci % GB + GB * par
                pq = ps[4 + s % 2]
                pqk = f"ps{4 + s % 2}"
                kb.op("pe", lambda e, s=s, pq=pq, b=b, a=a: e.matmul(
                    pq[0:CH, 0:CH], lhsT=T["MM"][b][0:CH, s, CH:2 * CH], rhs=T["Q"][a][0:CH, s, :], start=True, stop=True),
                    reads=[f"MM{b}_{s}", f"Q{a}_{s}"], writes=[pqk])
                kb.op("dve", lambda e, s=s, pq=pq, a=a, b=b: e.tensor_tensor(
                    out=T["Q"][b][0:CH, s, :], in0=pq[0:CH, 0:CH], in1=T["Q"][a][0:CH, s, :], op=ALU.add),
                    reads=[pqk, f"Q{a}_{s}"], writes=[f"Q{b}_{s}"])
                yield

    def chain_gen(g0):
        grp = list(range(g0, min(NCH, g0 + GB)))
        par = (g0 // GB) % 2
        QF = T["Q"][NLV % 2]
        for ci in grp:
            s = ci % GB + GB * par
            c0 = CHUNK_COLS[ci]
            cs = slice(c0, c0 + CH)
            AMb = T["AMb"]
            STb = T["STb"]
            pt = ps[6][:].bitcast(BF16) if tb else ps[6]
            idt = c.identb if tb else ident
            for j, src in enumerate((vT, kT, bT)):
                kb.op("pe", lambda e, j=j, src=src, cs=cs, pt=pt: e.transpose(
                    out=pt[0:CH, j * dk:(j + 1) * dk], in_=src[bp:bp + dk, cs], identity=idt[bp:bp + dk, bp:bp + dk]),
                    reads=[hk_, "c_ident", "c_identb"], writes=["ps6"])
            TM = T["TM"][ci % 2]
            tmk = f"TM{ci % 2}"
            kb.op("act", lambda e, TM=TM, pt=pt: e.copy(out=TM[0:CH, 0:3 * dk], in_=pt[0:CH, 0:3 * dk]),
                  reads=["ps6"], writes=[tmk])
            yield
            Vtm, Ktm, Btm = TM[0:CH, 0:dk], TM[0:CH, dk:2 * dk], TM[0:CH, 2 * dk:3 * dk]
            if Gb is not None:
                kb.op("dve", lambda e, TM=TM, s=s: e.tensor_scalar(out=TM[0:CH, dk:3 * dk], in0=TM[0:CH, dk:3 * dk],
                                                                 scalar1=T["dL"][0:CH, s:s + 1], scalar2=None, op0=ALU.mult),
                      reads=[tmk, f"dL{s}"], writes=[tmk])
            pr = ps[7]
            kb.op("pe", lambda e, cs=cs, pr=pr: e.matmul(pr[0:CH, 0:dk], lhsT=kkC[bp:bp + dk, cs], rhs=STb[bp:bp + dk, 0:dk],
                                                       start=True, stop=False), reads=[hk_, "STb"], writes=["ps7"])
            kb.op("pe", lambda e, s=s, pr=pr, Vtm=Vtm: e.matmul(pr[0:CH, 0:dk], lhsT=AMb[0:CH, s, 0:CH], rhs=Vtm,
                                                              start=False, stop=True),
                  reads=[f"AMb{s}", tmk], writes=["ps7"])
            yield
            kb.op("dve", lambda e, pr=pr: e.tensor_scalar(out=T["nR"][0:CH, 0:dk], in0=pr[0:CH, 0:dk], scalar1=-1.0,
                                                         scalar2=None, op0=ALU.mult), reads=["ps7"], writes=["nR"])
            yield
            kb.op("pe", lambda e, s=s, pr=pr: e.matmul(pr[0:CH, 128:128 + dk], lhsT=QF[0:CH, s, :], rhs=T["nR"][0:CH, 0:dk],
                                                     start=True, stop=True), reads=[f"Q{NLV % 2}_{s}", "nR"], writes=["ps7"])
            yield
            kb.op("act", lambda e, pr=pr: e.copy(out=T["U"][0:CH, 0:dk], in_=pr[0:CH, 128:128 + dk]),
                  reads=["ps7"], writes=["U"])
            yield
            U = T["U"]
            kb.op("pe", lambda e, cs=cs, pr=pr: e.matmul(pr[0:CH, 256:256 + dk], lhsT=rC[bp:bp + dk, cs], rhs=STb[bp:bp + dk, 0:dk],
                                                       start=True, stop=False), reads=[hk_, "STb"], writes=["ps7"])
            kb.op("pe", lambda e, s=s, pr=pr: e.matmul(pr[0:CH, 256:256 + dk], lhsT=AMb[0:CH, s, CH:2 * CH],
                                                     rhs=U[0:CH, 0:dk], start=False, stop=False),
                  reads=[f"AMb{s}", "U"], writes=["ps7"])
            kb.op("pe", lambda e, s=s, pr=pr, Vtm=Vtm: e.matmul(pr[0:CH, 256:256 + dk], lhsT=AMb[0:CH, s, 2 * CH:3 * CH],
                                                              rhs=Vtm, start=False, stop=True),
                  reads=[f"AMb{s}", tmk], writes=["ps7"])
            kb.op("pe", lambda e, pr=pr, Btm=Btm: e.matmul(pr[bp:bp + dk, 384:384 + dk], lhsT=Btm, rhs=U[0:CH, 0:dk],
                                                         start=True, stop=False), reads=[tmk, "U"], writes=["ps7"])
            kb.op("pe", lambda e, pr=pr, Ktm=Ktm, Vtm=Vtm: e.matmul(pr[bp:bp + dk, 384:384 + dk], lhsT=Ktm, rhs=Vtm,
                                                                  start=False, stop=True),
                  reads=[tmk], writes=["ps7"])
            yield
            Ysb = T["Y"][ci % 2]
            yk = f"Y{ci % 2}"
            kb.op("act", lambda e, pr=pr, Ysb=Ysb: e.copy(out=Ysb[0:CH, 0:dk], in_=pr[0:CH, 256:256 + dk]),
                  reads=["ps7"], writes=[yk])
            if Gb is not None:
                kb.op("dve", lambda e, pr=pr, c0=c0: e.scalar_tensor_tensor(
                    out=ST[bp:bp + dk, 0:dk], in0=ST[bp:bp + dk, 0:dk], scalar=Pinc[bp:bp + dk, c0 + CH - 1:c0 + CH],
                    in1=pr[bp:bp + dk, 384:384 + dk], op0=ALU.mult, op1=ALU.add), reads=["ps7", "ST", hk_], writes=["ST"])
            else:
                kb.op("dve", lambda e, pr=pr: e.tensor_tensor(out=ST[bp:bp + dk, 0:dk], in0=pr[bp:bp + dk, 384:384 + dk],
                                                             in1=ST[bp:bp + dk, 0:dk], op=ALU.add),
                      reads=["ps7", "ST"], writes=["ST"])
                kb.op("dve", lambda e, c0=c0: e.tensor_scalar(out=ST[bp:bp + dk, 0:dk], in0=ST[bp:bp + dk, 0:dk],
                                                             scalar1=Pinc[bp:bp + dk, c0 + CH - 1:c0 + CH], scalar2=None,
                                                             op0=ALU.mult), reads=["ST", hk_], writes=["ST"])
            kb.op("act", lambda e: e.copy(out=T["STb"][bp:bp + dk, 0:dk], in_=ST[bp:bp + dk, 0:dk]),
                  reads=["ST"], writes=["STb"])
            if prodT is not None:
                pb = ps[6]
                kb.op("pe", lambda e, cs=cs, pb=pb: e.matmul(pb[0:CH, 448:449], lhsT=prodT[bp:bp + dk, cs],
                                                           rhs=c.onesb[bp:bp + dk, 0:1], start=True, stop=True),
                      reads=[hk_, "c_ones"], writes=["ps6"])
                kb.op("dve", lambda e, pb=pb, Ysb=Ysb, Vtm=Vtm: e.tensor_scalar(
                    out=Ysb[0:CH, dk:2 * dk], in0=Vtm, scalar1=pb[0:CH, 448:449], scalar2=None, op0=ALU.mult),
                    reads=["ps6", tmk], writes=[yk])
            store_cb(ci, Ysb, yk)
            yield

    from itertools import zip_longest
    for _ in inv_gen(0):
        pass
    for g0 in range(0, NCH, GB):
        gens = [chain_gen(g0)]
        if g0 + GB < NCH:
            gens.append(inv_gen(g0 + GB))
        for _ in zip_longest(*gens):
            pass


def mixer_stage(kb, nc, g, c, mods, x_lat_in, x_ctx_in, x_lat_out, x_ctx_out):
    from contextlib import ExitStack
    mod = mods[0]
    Ys = g["ys_scr"]
    Zs = g["z_scr"]
    with ExitStack() as es:
        def sb(name, shape, dt):
            return es.enter_context(nc.sbuf_tensor(f"mxs_{name}", shape, dt))
        hT = None
        es2 = ExitStack()

        def sb2(name, shape, dt):
            return es2.enter_context(nc.sbuf_tensor(f"mxs_{name}", shape, dt))
        hb = sb("hb", [128, D], BF16)
        stt = [sb("st0", [128, 4], F32), sb("st1", [128, 4], F32)]
        F11 = sb("F11", [128, TP], F32)
        Jb = sb("Jb", [128, 128], BF16)
        J32 = sb("J32", [128, 128], F32)
        par = sb("par", [64, 64], F32)
        parP = sb("parP", [128, 64], F32)
        bd = sb("bd", [128, 128], F32)
        par128 = sb("par128", [128, 112], F32)
        w2sb = sb("w2sb", [128, 2, 512], F32)
        sel = sb("sel", [16, 16, 128], F32)
        ST = sb("ST", [128, 128], F32)
        hT = es2.enter_context(nc.sbuf_tensor("mxs_hT", [128, 8, 2304], BF16))
        F = [sb2(f"F{i}", [128, TP], F32) for i in range(11)] + [F11]
        FK = [f"F{i}" for i in range(12)]
        Gbc = F[1][:, 0:D]
        Sbc = F[2][:, 0:D]
        h32 = F[3][:, 0:D]
        xb = [F[4][:, 0:D], F[5][:, 0:D]]
        rmask = sb2("rmask", [128, TP], BF16)
        rm32 = F[0]
        wsl = sb2("wsl", [128, 8, 128], BF16)
        T = {"ST": ST,
             "AMf": sb2("AMf", [CHUNK, 4, CHUNK], F32), "AMb": sb2("AMb", [CHUNK, 4, 3 * CHUNK], BF16),
             "STb": sb2("STb", [128, 128], BF16),
             "MM": [sb2("MMa", [CHUNK, 4, 2 * CHUNK], F32), sb2("MMb", [CHUNK, 4, 2 * CHUNK], F32)],
             "Q": [sb2("Qa", [CHUNK, 4, CHUNK], F32), sb2("Qb", [CHUNK, 4, CHUNK], F32)],
             "TM": [sb2("TMa", [CHUNK, 384], BF16), sb2("TMb", [CHUNK, 384], BF16)],
             "nR": sb2("nR", [CHUNK, 128], F32), "U": sb2("U", [CHUNK, 128], BF16), "Uf": sb2("Uf", [16, 4], F32),
             "Y": [sb2("Ya", [CHUNK, 256], F32), sb2("Yb", [CHUNK, 256], F32)],
             "maskA": sb2("maskA", [CHUNK, 4 * CHUNK], F32), "maskT": sb2("maskT", [CHUNK, CHUNK], F32),
             "Gc": sb2("Gc", [CHUNK, 4], F32), "dL": sb2("dL", [CHUNK, 4], F32), "Dt": sb2("Dt", [CHUNK, 2, CHUNK], F32),
             "Dts": sb2("Dts", [CHUNK, 2, CHUNK], F32), "mD": sb2("mD", [CHUNK, 2, 4 * CHUNK], F32)}
        ps = [es.enter_context(nc.psum_tensor(f"mx_ps{i}", [128, 512], F32)) for i in range(8)]
        T["ps"] = ps
        kb.dma("sp", rm32[:], g["k_rmask"][:, :], writes=["F0"])
        kb.op("dve", lambda e: e.tensor_copy(out=rmask[:], in_=rm32[:]), reads=["F0"], writes=["rmask"])
        kb.dma("sp", J32[:], g["k_J"][:, :], writes=["J32"])
        kb.op("dve", lambda e: e.tensor_copy(out=Jb[:], in_=J32[:]), reads=["J32"], writes=["Jb"])
        kb.dma("sp", par[:], g["mx_par64"][:, :], writes=["par"])
        kb.dma("sp", par128[:], g["mx_par128"][:, :], writes=["par128"])
        kb.dma("sp", w2sb[:], g["mx_w2"][:, :, :], writes=["w2sb"])
        kb.dma("sp", parP[:], g["mx_parP"][:, :], writes=["parP"])
        kb.dma("sp", bd[:], g["k_bd"][:, :], writes=["bd"])
        kb.op("dve", lambda e: e.tensor_scalar(out=parP[:, 36:40], in0=parP[:, 32:36], scalar1=-1.0, scalar2=1.0,
                                               op0=ALU.mult, op1=ALU.add), reads=["parP"], writes=["parP"])
        kb.op("dve", lambda e: e.tensor_scalar(out=par128[:, 24:32], in0=par128[:, 16:24], scalar1=-1.0, scalar2=1.0,
                                               op0=ALU.mult, op1=ALU.add), reads=["par128"], writes=["par128"])
        T["zt"] = [sb2("zta", [128, 128], F32), sb2("ztb", [128, 128], F32)]
        kb.dma("sp", sel[:], g["k_sel"][:, :, :], writes=["sel"])
        kb.dma("sp", T["maskA"][:], g["k_maskA"][:, :], writes=["maskA"])
        kb.dma("sp", T["maskT"][:], g["k_maskT"][:, :], writes=["maskT"])
        for f in range(12):
            kb.op("pool", lambda e, f=f: e.memset(F[f][:], 0.0), reads=["rmask"] if f == 0 else [], writes=[FK[f]])
        wv = g["e_w_in2"].rearrange("(kc p) n -> p kc n", p=128)
        P64 = lambda j: par[:, j:j + 1]

        def project(c0, M, dst, dk_, evac_eng="act"):
            kb.dma("pool", wsl[:, :, 0:M], wv[:, :, c0:c0 + M], writes=["wsl"])
            for bi, (t0, tw, col0) in enumerate(BLOCKS):
                pb = ps[bi % 2]
                for kc in range(8):
                    kb.op("pe", lambda e, kc=kc, t0=t0, tw=tw, pb=pb: e.matmul(
                        pb[0:M, 0:tw], lhsT=wsl[:, kc, 0:M], rhs=hT[:, kc, t0:t0 + tw], start=(kc == 0), stop=(kc == 7)),
                        reads=["wsl", "hT"], writes=[f"ps{bi % 2}"])
                kb.op("act", lambda e, tw=tw, col0=col0, pb=pb: e.copy(out=dst[0:M, col0:col0 + tw], in_=pb[0:M, 0:tw]),
                      reads=[f"ps{bi % 2}"], writes=[dk_])

        def tshift(src, sk, dst, dk_, tmp, tk, P, mucol):
            n = TP - 2
            kb.op("dve", lambda e: e.tensor_tensor(out=tmp[0:P, 1:1 + n], in0=src[0:P, 0:n], in1=src[0:P, 2:2 + n],
                                                   op=ALU.add), reads=[sk], writes=[tk])
            kb.op("dve", lambda e: e.scalar_tensor_tensor(out=tmp[0:P, 1:1 + n], in0=tmp[0:P, 1:1 + n], scalar=0.5,
                                                          in1=src[0:P, 1:1 + n], op0=ALU.mult, op1=ALU.subtract),
                  reads=[sk, tk], writes=[tk])
            kb.op("dve", lambda e: e.scalar_tensor_tensor(out=dst[0:P, 1:1 + n], in0=tmp[0:P, 1:1 + n], scalar=mucol,
                                                         in1=src[0:P, 1:1 + n], op0=ALU.mult, op1=ALU.add),
                  reads=[sk, tk, "par", "par128"], writes=[dk_])

        DC = [(CTX0, 256), (LAT0, 2048)]

        def ew(eng, fn, reads, writes):
            kb.op(eng, fn, reads=reads, writes=writes)

        for d in range(2):
            for stream in (1, 0):
                kb.barrier()
                make_bc(kb, nc, c, lambda kc: mod[:, 1 * 8 + kc, stream:stream + 1], Gbc, ps[0], "Gbc", ["mod0"])
                make_bc(kb, nc, c, lambda kc: mod[:, 0 * 8 + kc, stream:stream + 1], Sbc, ps[0], "Sbc", ["mod0"])
                nt = 2 if stream == 1 else 16
                src = x_ctx_in if stream == 1 else x_lat_in
                base = 0 if stream == 1 else 256
                for i in range(nt):
                    b = i % 2
                    kb.dma("sp", xb[b][:], src[i * 128:(i + 1) * 128, :], writes=[f"xb{b}"])
                    norm_tile(kb, nc, xb[b][:], f"xb{b}", stt[b], Gbc, Sbc, h32, "h32")
                    kb.op("act", lambda e: e.copy(out=hb[:], in_=h32), reads=["h32"], writes=["hb"])
                    pos = base + (i if d == 0 else nt - 1 - i) * 128
                    for half in range(2):
                        for q in range(4):
                            kc = half * 4 + q
                            kb.op("pe", lambda e, kc=kc, q=q, half=half: e.matmul(
                                ps[2 + half][:, q * 128:(q + 1) * 128], lhsT=hb[:, kc * 128:(kc + 1) * 128],
                                rhs=(c.identb[:] if d == 0 else Jb[:]), start=True, stop=True),
                                reads=["hb", "c_identb", "Jb"], writes=[f"ps{2 + half}"])
                        kb.op("dve" if half == 0 else "act", (lambda e, half=half, pos=pos: e.tensor_copy(
                            out=hT[:, half * 4:(half + 1) * 4, pos:pos + 128],
                            in_=ps[2 + half][:].rearrange("p (q t) -> p q t", q=4))) if half == 0 else
                            (lambda e, half=half, pos=pos: e.copy(
                                out=hT[:, half * 4:(half + 1) * 4, pos:pos + 128],
                                in_=ps[2 + half][:].rearrange("p (q t) -> p q t", q=4))),
                            reads=[f"ps{2 + half}"], writes=["hT"])
                    if d == 0:
                        pass
            kb.barrier()

            def store_cb_factory(col0, dk_):
                def cb(ci, Ysb, yk):
                    r0 = ci * CHUNK
                    kb.dma("sp", Ys[d, r0:r0 + CHUNK, col0:col0 + dk_], Ysb[0:CHUNK, 0:dk_], reads=[yk],
                           writes=[("ys", d, ci, col0)])
                    if col0 < 512:
                        kb.dma("sp", Ys[d, r0:r0 + CHUNK, 1024 + col0:1024 + col0 + dk_], Ysb[0:CHUNK, dk_:2 * dk_],
                               reads=[yk], writes=[("ysb", d, ci, col0)])
                return cb

            project(1536 + d * 128, 128, F[0], FK[0])
            tshift(F[0], FK[0], F[10], FK[10], F[1], FK[1], 128, par128[:, d:d + 1])
            ew("act", lambda e: e.activation(out=F[10][0:64, :], in_=F[10][0:64, :], func=AF.Tanh), [FK[10]], [FK[10]])
            if d == 0:
                project(1792, 128, F[0], FK[0])
                tshift(F[0], FK[0], F[11], FK[11], F[1], FK[1], 128, par128[:, 2:3])
                ew("act", lambda e: e.activation(out=F[11][:], in_=F[11][:], func=AF.Sigmoid), [FK[11]], [FK[11]])
            for p in range(4):
                hk_ = f"pair{d}_{p}"
                PP = lambda j: parP[:, j:j + 1]
                for part, dst in ((0, 1), (1, 2), (2, 3)):
                    project(p * 384 + part * 128, 128, F[0], FK[0])
                    tshift(F[0], FK[0], F[dst], FK[dst], F[7], FK[7], 128, PP(p * 3 + part))
                for bi, (t0, tw, col0) in enumerate(BLOCKS):
                    cs = slice(col0, col0 + tw)
                    kb.op("pe", lambda e, cs=cs, tw=tw: e.matmul(ps[2][:, 0:tw], lhsT=w2sb[0:64, d, p * 128:(p + 1) * 128],
                                                               rhs=F[10][0:64, cs], start=True, stop=True),
                          reads=["w2sb", FK[10]], writes=["ps2"])
                    kb.op("pe", lambda e, cs=cs, tw=tw: e.matmul(ps[3][:, 0:tw], lhsT=w2sb[64:128, d, p * 128:(p + 1) * 128],
                                                               rhs=F[10][64:128, cs], start=True, stop=True),
                          reads=["w2sb", FK[10]], writes=["ps3"])
                    kb.op("act", lambda e, cs=cs, tw=tw: e.activation(out=F[4][:, cs], in_=ps[2][:, 0:tw],
                                                                    func=AF.Sigmoid, bias=PP(12 + d * 4 + p)),
                          reads=["ps2", "parP"], writes=[FK[4]])
                    kb.op("act", lambda e, cs=cs, tw=tw: e.activation(out=F[5][:, cs], in_=ps[3][:, 0:tw],
                                                                    func=AF.Sigmoid, bias=PP(20 + d * 4 + p)),
                          reads=["ps3", "parP"], writes=[FK[5]])
                kkc, kac, kac1, rkc = PP(28 + p), PP(32 + p), PP(36 + p), PP(40 + p)
                ew("dve", lambda e: e.tensor_scalar(out=F[7][:, :], in0=F[2][:, :], scalar1=kkc, scalar2=None,
                                                    op0=ALU.mult), [FK[2], "parP"], [FK[7]])
                for (a0_, n_) in DC:
                    ew("pool", lambda e, a0_=a0_, n_=n_: e.tensor_tensor(
                        out=F[0][:, a0_:a0_ + n_], in0=F[7][:, a0_:a0_ + n_], in1=F[7][:, a0_:a0_ + n_],
                        op=ALU.mult), [FK[7]], [FK[0]])
                for bi, (t0, tw, col0) in enumerate(BLOCKS):
                    cs = slice(col0, col0 + tw)
                    kb.op("pe", lambda e, cs=cs, tw=tw: e.matmul(ps[2][:, 0:tw], lhsT=bd[:, :],
                                                               rhs=F[0][:, cs], start=True, stop=True),
                          reads=["bd", FK[0]], writes=["ps2"])
                    kb.op("act", lambda e, cs=cs, tw=tw: e.activation(out=F[9][:, cs], in_=ps[2][:, 0:tw],
                                                                    func=AF.Sqrt, bias=c.eps6[:, 0:1]),
                          reads=["ps2", "c_eps"], writes=[FK[9]])
                for (a0_, n_) in DC:
                    cs = slice(a0_, a0_ + n_)
                    ew("dve", lambda e, cs=cs: e.reciprocal(out=F[9][:, cs], in_=F[9][:, cs]), [FK[9]], [FK[9]])
                    ew("dve", lambda e, cs=cs: e.tensor_tensor(out=F[6][:, cs], in0=F[7][:, cs], in1=F[9][:, cs],
                                                               op=ALU.mult), [FK[7], FK[9]], [FK[6]])
                    ew("pool", lambda e, cs=cs: e.tensor_scalar(out=F[7][:, cs], in0=F[5][:, cs], scalar1=kac,
                                                                scalar2=kac1, op0=ALU.mult, op1=ALU.add),
                       [FK[5], "parP"], [FK[7]])
                    ew("pool", lambda e, cs=cs: e.tensor_tensor(out=F[7][:, cs], in0=F[7][:, cs], in1=F[2][:, cs],
                                                                op=ALU.mult), [FK[7], FK[2]], [FK[7]])
                    ew("dve", lambda e, cs=cs: e.tensor_tensor(out=F[9][:, cs], in0=F[6][:, cs], in1=F[5][:, cs],
                                                               op=ALU.mult), [FK[6], FK[5]], [FK[9]])
                    ew("dve", lambda e, cs=cs: e.scalar_tensor_tensor(out=F[0][:, cs], in0=F[1][:, cs], scalar=rkc,
                                                                      in1=F[7][:, cs], op0=ALU.mult, op1=ALU.mult),
                       [FK[1], FK[7], "parP"], [FK[0]])
                ew("dve", lambda e: e.tensor_tensor_scan(out=F[8][:, :], data0=rmask[:, :], data1=F[4][:, :],
                                                         initial=0.0, op0=ALU.mult, op1=ALU.add),
                   ["rmask", FK[4]], [FK[8]])
                B4 = F[4][:, :].bitcast(BF16)
                B5 = F[5][:, :].bitcast(BF16)
                B2 = F[2][:, :].bitcast(BF16)
                DCS = [slice(a0_, a0_ + n_) for (a0_, n_) in DC]
                DCH = [slice(TP + a0_, TP + a0_ + n_) for (a0_, n_) in DC]
                for cs in DCS:
                    ew("pool", lambda e, cs=cs: e.tensor_tensor(out=F[2][:, cs], in0=F[8][:, cs], in1=F[4][:, cs],
                                                                op=ALU.subtract), [FK[8], FK[4]], [FK[2]])
                    ew("act", lambda e, cs=cs: e.activation(out=F[2][:, cs], in_=F[2][:, cs], func=AF.Exp,
                                                            scale=-DECAY_K), [FK[2]], [FK[2]])
                for cs in DCS:
                    ew("dve", lambda e, cs=cs: e.tensor_tensor(out=B4[:, cs], in0=F[6][:, cs], in1=F[2][:, cs],
                                                               op=ALU.mult), [FK[6], FK[2]], [FK[4]])
                for cs in DCS:
                    ew("act", lambda e, cs=cs: e.activation(out=F[2][:, cs], in_=F[8][:, cs], func=AF.Exp,
                                                            scale=DECAY_K), [FK[8], FK[2], FK[4]], [FK[2]])
                for cs, ch in zip(DCS, DCH):
                    ew("dve", lambda e, cs=cs, ch=ch: e.tensor_tensor(out=B4[:, ch], in0=F[7][:, cs], in1=F[2][:, cs],
                                                                      op=ALU.mult), [FK[7], FK[2]], [FK[4]])
                    ew("pool", lambda e, cs=cs: e.tensor_tensor(out=B5[:, cs], in0=F[9][:, cs], in1=F[2][:, cs],
                                                                op=ALU.mult), [FK[9], FK[2]], [FK[5]])
                for cs in DCS:
                    ew("act", lambda e, cs=cs: e.activation(out=F[8][:, cs], in_=F[8][:, cs], func=AF.Exp,
                                                            scale=-DECAY_K), [FK[8]], [FK[8]])
                for cs, ch in zip(DCS, DCH):
                    ew("dve", lambda e, cs=cs: e.tensor_tensor(out=B2[:, cs], in0=F[1][:, cs], in1=F[8][:, cs],
                                                               op=ALU.mult), [FK[1], FK[8], FK[4], FK[5]], [FK[2]])
                    ew("pool", lambda e, cs=cs, ch=ch: e.tensor_copy(out=B5[:, ch], in_=F[3][:, cs]), [FK[3]], [FK[5]])
                    ew("act", lambda e, cs=cs, ch=ch: e.copy(out=B2[:, ch], in_=F[0][:, cs]), [FK[0]], [FK[2]])
                HI = lambda B: B[:, TP:2 * TP]
                kb.op("pool", lambda e: e.memset(T["nR"][:], 0.0),
                      reads=[FK[2], FK[4], FK[5], FK[8]], writes=[hk_, "nR"])
                for hh in range(2):
                    dplr_scan(kb, nc, c, T, 64, B2[:, 0:TP], B4[:, 0:TP], HI(B4), B5[:, 0:TP], HI(B5), F[8], HI(B2),
                              store_cb_factory((2 * p + hh) * 64, 64), hk_, bp=hh * 64)
                kb.op("pool", lambda e: e.memset(T["nR"][:], 0.0), reads=[hk_],
                      writes=[FK[2], FK[4], FK[5], FK[8], "nR"])
            mixer_gdn_pass(kb, nc, g, c, T, d, F, FK, rmask, par128, sel, project, store_cb_factory, ps, DC, wv, wsl,
                           hT, Zs)
        kb.barrier()
        es2.close()
        import os
        if os.environ.get("MIX_DUMP"):
            kb.dma("sp", x_lat_out[0:2048, :], Ys[0, 0:2048, 0:1024], writes=["o1"])
            kb.dma("sp", x_ctx_out[0:256, :], Ys[0, 0:256, 512:1536], writes=["o2"])
            kb.barrier()
            return
        mixer_output(kb, nc, g, c, mods, T, F, FK, x_lat_in, x_ctx_in, x_lat_out, x_ctx_out, Ys, Zs, J32, None, None, hb,
                     ps, par128)
        kb.barrier()


def mixer_gdn_pass(kb, nc, g, c, T, d, F, FK, rmask, par128, sel, project, store_cb_factory, ps, DC, wv, wsl, hT, Zs):
    ew = lambda eng, fn, r, w: kb.op(eng, fn, reads=r, writes=w)
    project(3968, 16, F[10], FK[10])
    R16 = lambda i: F[i][0:16, :]
    ew("act", lambda e: e.activation(out=T["Uf"][0:16, 0:1], in_=par128[0:16, 41:42], func=AF.Exp), ["par128"], ["U"])
    ew("dve", lambda e: e.tensor_scalar(out=T["Uf"][0:16, 0:1], in0=T["Uf"][0:16, 0:1], scalar1=-1.0, scalar2=None,
                                        op0=ALU.mult), ["U"], ["U"])
    ew("dve", lambda e: e.tensor_scalar(out=R16(1), in0=R16(10), scalar1=par128[0:16, 40:41], scalar2=None, op0=ALU.add),
       [FK[10], "par128"], [FK[1]])
    ew("act", lambda e: e.activation(out=R16(4), in_=R16(10), func=AF.Sigmoid), [FK[10]], [FK[4]])
    ew("act", lambda e: e.activation(out=R16(2), in_=R16(1), func=AF.Abs), [FK[1]], [FK[2]])
    ew("act", lambda e: e.activation(out=R16(2), in_=R16(2), func=AF.Exp, scale=-1.0), [FK[2]], [FK[2]])
    ew("dve", lambda e: e.tensor_scalar(out=R16(3), in0=R16(2), scalar1=2.0, scalar2=None, op0=ALU.add), [FK[2]], [FK[3]])
    ew("dve", lambda e: e.reciprocal(out=R16(3), in_=R16(3)), [FK[3]], [FK[3]])
    ew("dve", lambda e: e.tensor_tensor(out=R16(2), in0=R16(2), in1=R16(3), op=ALU.mult), [FK[2], FK[3]], [FK[2]])
    ew("dve", lambda e: e.tensor_tensor(out=R16(3), in0=R16(2), in1=R16(2), op=ALU.mult), [FK[2]], [FK[3]])
    ew("dve", lambda e: e.tensor_scalar(out=R16(6), in0=R16(3), scalar1=1.0 / 13, scalar2=1.0 / 11, op0=ALU.mult,
                                        op1=ALU.add), [FK[3]], [FK[6]])
    for cf in (1.0 / 9, 1.0 / 7, 1.0 / 5, 1.0 / 3, 1.0):
        ew("dve", lambda e: e.tensor_tensor(out=R16(6), in0=R16(6), in1=R16(3), op=ALU.mult), [FK[6], FK[3]], [FK[6]])
        ew("dve", lambda e, cf=cf: e.tensor_scalar(out=R16(6), in0=R16(6), scalar1=cf, scalar2=None, op0=ALU.add),
           [FK[6]], [FK[6]])
    ew("dve", lambda e: e.scalar_tensor_tensor(out=R16(6), in0=R16(6), scalar=2.0, in1=R16(2), op0=ALU.mult, op1=ALU.mult),
       [FK[6], FK[2]], [FK[6]])
    ew("dve", lambda e: e.tensor_scalar(out=R16(1), in0=R16(1), scalar1=0.0, scalar2=None, op0=ALU.max), [FK[1]], [FK[1]])
    ew("dve", lambda e: e.tensor_tensor(out=R16(6), in0=R16(6), in1=R16(1), op=ALU.add), [FK[6], FK[1]], [FK[6]])
    ew("dve", lambda e: e.tensor_scalar(out=R16(6), in0=R16(6), scalar1=T["Uf"][0:16, 0:1], scalar2=None, op0=ALU.mult),
       [FK[6], "U"], [FK[6]])
    ew("dve", lambda e: e.tensor_tensor_scan(out=R16(10), data0=rmask[0:16, :], data1=R16(6), initial=0.0,
                                             op0=ALU.mult, op1=ALU.add), ["rmask", FK[6]], [FK[10]])
    for h in range(4):
        hk_ = f"ghead{d}_{h}"
        c0 = 1920 + h * 512
        for part, dst in ((0, 1), (1, 2), (2, 3)):
            project(c0 + part * 128, 128, F[0], FK[0])
            n = TP - 4
            for j in range(5):
                jj = j if d == 0 else 4 - j
                wc = par128[:, 48 + (h * 3 + part) * 5 + jj:49 + (h * 3 + part) * 5 + jj]
                if j == 0:
                    ew("dve", lambda e, wc=wc, dst=dst: e.tensor_scalar(out=F[dst][:, 2:2 + n], in0=F[0][:, 0:n],
                                                                      scalar1=wc, scalar2=None, op0=ALU.mult),
                       [FK[0], "par128"], [FK[dst]])
                else:
                    ew("dve", lambda e, wc=wc, dst=dst, j=j: e.scalar_tensor_tensor(
                        out=F[dst][:, 2:2 + n], in0=F[0][:, j:j + n], scalar=wc, in1=F[dst][:, 2:2 + n],
                        op0=ALU.mult, op1=ALU.add), [FK[0], FK[dst], "par128"], [FK[dst]])
            ew("act", lambda e, dst=dst: e.activation(out=F[dst][:, :], in_=F[dst][:, :], func=AF.Silu), [FK[dst]], [FK[dst]])
        for src, scl in ((1, float(128 ** -0.5)), (2, 1.0)):
            for (a0_, n_) in DC:
                ew("pool", lambda e, src=src, a0_=a0_, n_=n_: e.tensor_tensor(
                    out=F[0][:, a0_:a0_ + n_], in0=F[src][:, a0_:a0_ + n_], in1=F[src][:, a0_:a0_ + n_], op=ALU.mult),
                    [FK[src]], [FK[0]])
            for bi, (t0, tw, col0) in enumerate(BLOCKS):
                cs = slice(col0, col0 + tw)
                kb.op("pe", lambda e, cs=cs, tw=tw: e.matmul(ps[2][:, 0:tw], lhsT=c.ones[:, :], rhs=F[0][:, cs],
                                                           start=True, stop=True), reads=["c_ones", FK[0]], writes=["ps2"])
                kb.op("act", lambda e, cs=cs, tw=tw: e.activation(out=F[9][:, cs], in_=ps[2][:, 0:tw], func=AF.Sqrt,
                                                                bias=c.eps6[:, 0:1]), reads=["ps2", "c_eps"], writes=[FK[9]])
            for (a0_, n_) in DC:
                cs = slice(a0_, a0_ + n_)
                ew("dve", lambda e, cs=cs: e.reciprocal(out=F[9][:, cs], in_=F[9][:, cs]), [FK[9]], [FK[9]])
                ew("dve", lambda e, cs=cs, src=src, scl=scl: e.scalar_tensor_tensor(
                    out=F[src][:, cs], in0=F[src][:, cs], scalar=scl, in1=F[9][:, cs], op0=ALU.mult, op1=ALU.mult),
                    [FK[src], FK[9]], [FK[src]])
        ra, rb = d * 4 + h, 8 + d * 4 + h
        for bi, (t0, tw, col0) in enumerate(BLOCKS):
            cs = slice(col0, col0 + tw)
            kb.op("pe", lambda e, cs=cs, tw=tw: e.matmul(ps[2][:, 0:tw], lhsT=sel[0:16, ra, :], rhs=F[10][0:16, cs],
                                                       start=True, stop=True), reads=["sel", FK[10]], writes=["ps2"])
            kb.op("pe", lambda e, cs=cs, tw=tw: e.matmul(ps[3][:, 0:tw], lhsT=sel[0:16, rb, :], rhs=F[4][0:16, cs],
                                                       start=True, stop=True), reads=["sel", FK[4]], writes=["ps3"])
            kb.op("act", lambda e, cs=cs, tw=tw: e.activation(out=F[6][:, cs], in_=ps[2][:, 0:tw], func=AF.Exp),
                  reads=["ps2"], writes=[FK[6]])
            kb.op("dve", lambda e, cs=cs, tw=tw: e.tensor_copy(out=F[0][:, cs], in_=ps[2][:, 0:tw]),
                  reads=["ps2"], writes=[FK[0]])
            kb.op("act", lambda e, cs=cs, tw=tw: e.copy(out=F[9][:, cs], in_=ps[3][:, 0:tw]),
                  reads=["ps3"], writes=[FK[9]])
        GB5 = F[5][:, :].bitcast(BF16)
        for (a0_, n_) in DC:
            cs = slice(a0_, a0_ + n_)
            ew("dve", lambda e, cs=cs: e.tensor_tensor(out=F[9][:, cs], in0=F[9][:, cs], in1=F[2][:, cs], op=ALU.mult),
               [FK[9], FK[2]], [FK[9]])
            ew("pool", lambda e, cs=cs: e.tensor_tensor(out=GB5[:, cs], in0=F[2][:, cs], in1=F[6][:, cs], op=ALU.mult),
               [FK[2], FK[6]], [FK[5]])
            ew("dve", lambda e, cs=cs: e.tensor_tensor(out=GB5[:, TP + cs.start:TP + cs.stop], in0=F[1][:, cs],
                                                       in1=F[6][:, cs], op=ALU.mult),
               [FK[1], FK[6]], [FK[5]])
        kb.op("pool", lambda e: e.memset(T["nR"][:], 0.0), reads=[FK[0], FK[1], FK[2], FK[3], FK[5], FK[6], FK[9]],
              writes=[hk_, "nR"])
        dplr_scan(kb, nc, c, T, 128, F[1], F[2], F[9], F[9], F[3], F[6], None, store_cb_factory(512 + h * 128, 128), hk_,
                  Gb=F[0], rA=F[1], kkA=F[2], rC=GB5[:, TP:2 * TP], kkC=GB5[:, 0:TP])
        kb.op("pool", lambda e: e.memset(T["nR"][:], 0.0), reads=[hk_],
              writes=[FK[0], FK[1], FK[2], FK[3], FK[5], FK[6], FK[9], "nR"])
    if d == 0:
        n = 0
        for h in range(4):
            kb.dma("pool", wsl[:, :, 0:128], wv[:, :, 1920 + h * 512 + 384:1920 + h * 512 + 512], writes=["wsl"])
            for i in range(18):
                pb = ps[n % 2]
                for kc in range(8):
                    kb.op("pe", lambda e, kc=kc, i=i, pb=pb: e.matmul(pb[:, 0:128], lhsT=hT[:, kc, i * 128:(i + 1) * 128],
                                                                   rhs=wsl[:, kc, 0:128], start=(kc == 0), stop=(kc == 7)),
                          reads=["wsl", "hT"], writes=[f"ps{n % 2}"])
                zt = T["zt"][n % 2]
                kb.op("act", lambda e, pb=pb, zt=zt: e.activation(out=zt[:], in_=pb[:, 0:128], func=AF.Silu),
                      reads=[f"ps{n % 2}"], writes=[f"zt{n % 2}"])
                kb.dma("sp", Zs[i * 128:(i + 1) * 128, h * 128:(h + 1) * 128], zt[:], reads=[f"zt{n % 2}"],
                       writes=[("zs", i, h)])
                n += 1


def mixer_output(kb, nc, g, c, mods, T, F, FK, x_lat_in, x_ctx_in, x_lat_out, x_ctx_out, Ys, Zs, J32, xb, Gbc, hb, ps,
                 par128):
    ew = lambda eng, fn, r, w: kb.op(eng, fn, reads=r, writes=w)
    kb.barrier()
    from contextlib import ExitStack
    with ExitStack() as es:
        def sb(name, shape, dt):
            return es.enter_context(nc.sbuf_tensor(f"mo_{name}", shape, dt))
        wo = sb("wo", [128, 8, D], BF16)
        g2 = sb("g2", [128, 512], F32)
        bcp = sb("bcp", [128, 3, 512], F32)
        yf = sb("yf", [128, 1536], F32)
        yb = sb("yb", [128, 1536], F32)
        zt = sb("zt", [128, 512], F32)
        o = sb("o", [128, D], F32)
        t1 = sb("t1", [128, 512], F32)
        s8 = sb("s8", [128, 4, 8], F32)
        oT = sb("oT", [128, 8, 128], BF16)
        Gbc = sb("Gbc", [128, D], F32)
        xb = [sb("x0", [128, D], F32), sb("x1", [128, D], F32)]
        for kc in range(8):
            kb.dma("pool", wo[:, kc, :], g["e_w_out"][0].rearrange("(kc p) n -> p kc n", p=128)[:, kc, :], writes=["wo"])
        kb.dma("sp", g2[:], g["a_g2"][0], writes=["g2"])
        kb.dma("sp", bcp[:].rearrange("p a n -> p (a n)"), g["mx_bc"].partition_broadcast(128), writes=["bcp"])
        for stream in (1, 0):
            kb.barrier()
            make_bc(kb, nc, c, lambda kc: mods[0][:, 2 * 8 + kc, stream:stream + 1], Gbc, ps[0], "Gbc", ["mod0"])
            nt = 2 if stream == 1 else 16
            src = x_ctx_in if stream == 1 else x_lat_in
            dst = x_ctx_out if stream == 1 else x_lat_out
            base = 0 if stream == 1 else 256
            colbase = CTX0 if stream == 1 else LAT0
            for i in range(nt):
                b = i % 2
                r0 = base + i * 128
                rb0 = base + (nt - 1 - i) * 128
                kb.dma("sp", xb[b][:], src[i * 128:(i + 1) * 128, :], writes=[f"xb{b}"])
                kb.dma("sp", yf[:], Ys[0, r0:r0 + 128, :], reads=[("ysall",)], writes=["yf"])
                kb.dma("act", yb[:], Ys[1, rb0:rb0 + 128, :], reads=[("ysall",)], writes=["yb"])
                kb.dma("sp", zt[:], Zs[r0:r0 + 128, :], reads=[("ysall",)], writes=["zt"])
                for q in range(3):
                    kb.op("pe", lambda e, q=q: e.matmul(ps[1 + q][:, :], lhsT=J32[:, :], rhs=yb[:, q * 512:(q + 1) * 512],
                                                      start=True, stop=True), reads=["J32", "yb"], writes=[f"ps{1 + q}"])
                    ew("dve", lambda e, q=q: e.tensor_tensor(out=yf[:, q * 512:(q + 1) * 512], in0=yf[:, q * 512:(q + 1) * 512],
                                                             in1=ps[1 + q][:, :], op=ALU.add), [f"ps{1 + q}", "yf"], ["yf"])
                y3 = yf[:, 0:512].rearrange("p (h j) -> p h j", h=8)
                ew("dve", lambda e: e.reduce_sum(out=s8[:, 0, :], in_=y3, axis=AX.X), ["yf"], ["s8"])
                ew("dve", lambda e: e.tensor_scalar(out=s8[:, 0, :], in0=s8[:, 0, :], scalar1=-1.0 / 64, scalar2=None,
                                                    op0=ALU.mult), ["s8"], ["s8"])
                ew("dve", lambda e: e.tensor_tensor(out=y3, in0=y3, in1=s8[:, 0, :].unsqueeze(2).broadcast_to([128, 8, 64]),
                                                    op=ALU.add), ["yf", "s8"], ["yf"])
                ew("pool", lambda e: e.tensor_tensor(out=t1[:], in0=yf[:, 0:512], in1=yf[:, 0:512], op=ALU.mult), ["yf"], ["t1"])
                ew("dve", lambda e: e.reduce_sum(out=s8[:, 1, :], in_=t1[:].rearrange("p (h j) -> p h j", h=8), axis=AX.X),
                   ["t1"], ["s8"])
                ew("dve", lambda e: e.tensor_scalar(out=s8[:, 1, :], in0=s8[:, 1, :], scalar1=1.0 / 64, scalar2=64e-5,
                                                    op0=ALU.mult, op1=ALU.add), ["s8"], ["s8"])
                ew("act", lambda e: e.activation(out=s8[:, 1, :], in_=s8[:, 1, :], func=AF.Sqrt), ["s8"], ["s8"])
                ew("dve", lambda e: e.reciprocal(out=s8[:, 1, :], in_=s8[:, 1, :]), ["s8"], ["s8"])
                ew("dve", lambda e: e.tensor_tensor(out=y3, in0=y3, in1=s8[:, 1, :].unsqueeze(2).broadcast_to([128, 8, 64]),
                                                    op=ALU.mult), ["yf", "s8"], ["yf"])
                ew("dve", lambda e: e.tensor_tensor(out=yf[:, 0:512], in0=yf[:, 0:512], in1=bcp[:, 0, :], op=ALU.mult),
                   ["yf", "bcp"], ["yf"])
                ew("pool", lambda e: e.tensor_tensor(out=yf[:, 0:512], in0=yf[:, 0:512], in1=bcp[:, 1, :], op=ALU.add),
                   ["yf", "bcp"], ["yf"])
                ew("pool", lambda e: e.tensor_tensor(out=yf[:, 0:512], in0=yf[:, 0:512], in1=yf[:, 1024:1536], op=ALU.add),
                   ["yf"], ["yf"])
                cg = colbase + i * 128
                kb.op("pe", lambda e, cg=cg: e.matmul(ps[4][:, :], lhsT=F[11][:, cg:cg + 128], rhs=g2[:, :], start=True, stop=True),
                      reads=[FK[11], "g2"], writes=["ps4"])
                ew("dve", lambda e: e.tensor_tensor(out=o[:, 0:512], in0=yf[:, 0:512], in1=ps[4][:, :], op=ALU.mult),
                   ["yf", "ps4"], ["o"])
                ew("pool", lambda e: e.tensor_tensor(out=t1[:], in0=yf[:, 512:1024], in1=yf[:, 512:1024], op=ALU.mult),
                   ["yf"], ["t1"])
                ew("dve", lambda e: e.reduce_sum(out=s8[:, 2, 0:4], in_=t1[:].rearrange("p (h j) -> p h j", h=4), axis=AX.X),
                   ["t1"], ["s8"])
                ew("dve", lambda e: e.tensor_scalar(out=s8[:, 2, 0:4], in0=s8[:, 2, 0:4], scalar1=1.0 / 128, scalar2=EPS,
                                                    op0=ALU.mult, op1=ALU.add), ["s8"], ["s8"])
                ew("act", lambda e: e.activation(out=s8[:, 2, 0:4], in_=s8[:, 2, 0:4], func=AF.Sqrt), ["s8"], ["s8"])
                ew("dve", lambda e: e.reciprocal(out=s8[:, 2, 0:4], in_=s8[:, 2, 0:4]), ["s8"], ["s8"])
                ew("dve", lambda e: e.tensor_tensor(
                    out=t1[:].rearrange("p (h j) -> p h j", h=4), in0=yf[:, 512:1024].rearrange("p (h j) -> p h j", h=4),
                    in1=s8[:, 2, 0:4].unsqueeze(2).broadcast_to([128, 4, 128]), op=ALU.mult), ["yf", "s8"], ["t1"])
                ew("pool", lambda e: e.tensor_tensor(out=t1[:], in0=t1[:], in1=bcp[:, 2, :], op=ALU.mult), ["t1", "bcp"], ["t1"])
                ew("dve", lambda e: e.tensor_tensor(out=o[:, 512:1024], in0=t1[:], in1=zt[:], op=ALU.mult), ["t1", "zt"], ["o"])
                ew("act", lambda e: e.copy(out=hb[:], in_=o[:]), ["o"], ["hb"])
                pst = ps[0][:].bitcast(BF16)
                for kc in range(8):
                    kb.op("pe", lambda e, kc=kc, pst=pst: e.transpose(out=pst[:, kc * 128:(kc + 1) * 128],
                                                                    in_=hb[:, kc * 128:(kc + 1) * 128], identity=c.identb[:]),
                          reads=["hb", "c_identb"], writes=["ps0"])
                ew("act", lambda e, pst=pst: e.copy(out=oT[:], in_=pst[:].rearrange("p (k t) -> p k t", k=8)), ["ps0"], ["oT"])
                for half in range(2):
                    pp = ps[5 + half]
                    for kc in range(8):
                        kb.op("pe", lambda e, kc=kc, half=half, pp=pp: e.matmul(
                            pp[:], lhsT=oT[:, kc, :], rhs=wo[:, kc, half * 512:(half + 1) * 512], start=(kc == 0), stop=(kc == 7)),
                            reads=["oT", "wo"], writes=[f"ps{5 + half}"])
                    sl = slice(half * 512, (half + 1) * 512)
                    ew("dve", lambda e, pp=pp, sl=sl: e.tensor_tensor(out=t1[:], in0=pp[:], in1=Gbc[:, sl], op=ALU.mult),
                       [f"ps{5 + half}", "Gbc"], ["t1"])
                    ew("pool", lambda e, sl=sl, b=b: e.tensor_tensor(out=xb[b][:, sl], in0=xb[b][:, sl], in1=t1[:], op=ALU.add),
                       ["t1", f"xb{b}"], [f"xb{b}"])
                kb.dma("sp", dst[i * 128:(i + 1) * 128, :], xb[b][:], reads=[f"xb{b}"], writes=[("dram", dst.tensor.name, i)])
        kb.barrier()


_CACHE = {}


def kernel(**inputs):
    inp = {k: np.asarray(v) for k, v in inputs.items()}
    if "nc" not in _CACHE:
        _CACHE["nc"] = build(stages=("all",))[0]
    nc = _CACHE["nc"]
    in_maps = [host_inputs(inp, b) for b in range(8)]
    res = run_bass_kernel_spmd(nc, in_maps, core_ids=list(range(8)))
    return np.stack([np.asarray(r["out"], dtype=np.float32) for r in res.results], axis=0)
```

```python
import numpy as np
import concourse.bass as bass
import concourse.mybir as mybir
from concourse.bass_utils import run_bass_kernel_spmd

F32 = mybir.dt.float32
BF16 = mybir.dt.bfloat16
I32 = mybir.dt.int32
U32 = mybir.dt.uint32
ALU = mybir.AluOpType
AF = mybir.ActivationFunctionType
AX = mybir.AxisListType

SEM_ROTATE = 20000
N_DMA_SEMS = 28
N_HW_SEMS = 18


class KB:
    def __init__(self, nc, same_engine_sync=True):
        self.nc = nc
        self.engs = {"pe": nc.tensor, "act": nc.scalar, "dve": nc.vector, "pool": nc.gpsimd, "sp": nc.sync}
        self.same_engine_sync = same_engine_sync
        self.esem = {}
        self.ecnt = {}
        self.sem_id = 0
        for e in ("pe", "act", "dve", "pool"):
            self._new_esem(e)
        self.dsems = [self._alloc_sem(f"dma{i}") for i in range(N_DMA_SEMS)]
        self.dcnt = [0] * N_DMA_SEMS
        self.dnext = 0
        self.dnext_sw = 0
        self.known = {e: {} for e in self.engs}
        self.state = {}
        self.n_ins = 0
        self._uid = 0
        self.out_tokens = []

    def _alloc_sem(self, name):
        self.sem_id += 1
        return self.nc.alloc_semaphore(f"{name}_{self.sem_id}")

    def _new_esem(self, e):
        self.esem[e] = self._alloc_sem(f"s_{e}")
        self.ecnt[e] = 0

    def uid(self, p="t"):
        self._uid += 1
        return f"{p}{self._uid}"

    def _deps(self, reads, writes):
        deps = []
        for r in reads:
            st = self.state.get(r)
            if st and st[0] is not None:
                deps.append(st[0])
        for w in writes:
            st = self.state.get(w)
            if st:
                if st[0] is not None:
                    deps.append(st[0])
                deps.extend(st[1].values())
        return deps

    def _wait(self, e, deps):
        eng = self.engs[e]
        kn = self.known[e]
        best = {}
        for (sem, val, src) in deps:
            if src == e and not (self.same_engine_sync and e != "pe"):
                continue
            key = id(sem)
            if kn.get(key, 0) >= val:
                continue
            if key not in best or best[key][1] < val:
                best[key] = (sem, val)
        for key, (sem, val) in best.items():
            eng.wait_ge(sem, val)
            kn[key] = val
            self.n_ins += 1

    def _commit(self, token, reads, writes):
        for w in writes:
            self.state[w] = [token, {}]
        for r in reads:
            st = self.state.get(r)
            if st is None:
                st = [None, {}]
                self.state[r] = st
            st[1][id(token[0])] = token

    def op(self, e, fn, reads=(), writes=()):
        reads = list(reads)
        writes = list(writes)
        writes += [r for r in reads if isinstance(r, str) and r.startswith("ps")]
        self._wait(e, self._deps(reads, writes))
        if self.ecnt[e] >= SEM_ROTATE:
            self._new_esem(e)
        ins = fn(self.engs[e])
        self.ecnt[e] += 1
        ins.then_inc(self.esem[e], 1)
        token = (self.esem[e], self.ecnt[e], e)
        self._commit(token, reads, writes)
        self.n_ins += 1
        return token

    def dma(self, q, out, in_, reads=(), writes=(), **kw):
        reads = list(reads)
        writes = list(writes)
        if q == "pool":
            i = N_HW_SEMS + self.dnext_sw
            self.dnext_sw = (self.dnext_sw + 1) % (N_DMA_SEMS - N_HW_SEMS)
        else:
            i = self.dnext
            self.dnext = (self.dnext + 1) % N_HW_SEMS
        deps = self._deps(reads, writes)
        if self.dcnt[i] > 0:
            deps.append((self.dsems[i], self.dcnt[i], "dma"))
        self._wait(q, deps)
        ins = self.engs[q].dma_start(out=out, in_=in_, **kw)
        self.dcnt[i] += 16
        ins.then_inc(self.dsems[i], 16)
        token = (self.dsems[i], self.dcnt[i], "dma")
        self._commit(token, reads, writes)
        self.n_ins += 1
        return token

    def finish(self, out_keys):
        deps = []
        for k in out_keys:
            st = self.state.get(k)
            if st and st[0] is not None:
                deps.append(st[0])
        self._wait("sp", deps)
        deps = [(self.dsems[i], self.dcnt[i], "dma") for i in range(N_DMA_SEMS) if self.dcnt[i] > 0]
        self._wait("sp", deps)

    def barrier(self):
        deps = [(self.esem[e], self.ecnt[e], "x") for e in self.esem if self.ecnt[e] > 0]
        deps += [(self.dsems[i], self.dcnt[i], "dma") for i in range(N_DMA_SEMS) if self.dcnt[i] > 0]
        for e in self.engs:
            self._wait(e, deps)
        self.state = {}


D = 1024
KC = 8
EPS = 1e-6
TP = 2310
CTX0, LAT0 = 2, 260
CHUNK = 128
CHUNK_COLS = [CTX0 + CHUNK * j for j in range(256 // CHUNK)] + [LAT0 + CHUNK * j for j in range(2048 // CHUNK)]
BLOCKS = [(0, 256, CTX0)] + [(256 + 512 * j, 512, LAT0 + 512 * j) for j in range(4)]
DECAY_K = float(np.exp(-0.5))


class Ctx:
    pass


def load_consts(kb, nc, es, g):
    c = Ctx()
    c.ident = es.enter_context(nc.sbuf_tensor("c_ident", [128, 128], F32))
    c.identb = es.enter_context(nc.sbuf_tensor("c_identb", [128, 128], BF16))
    c.ones = es.enter_context(nc.sbuf_tensor("c_ones", [128, 128], F32))
    c.iota = es.enter_context(nc.sbuf_tensor("c_iota", [128, 256], F32))
    kb.dma("sp", c.ident[:], g["k_ident"][:, :], writes=["c_ident"])
    kb.dma("sp", c.iota[:], g["k_iota"][:, :], writes=["c_iota"])
    kb.op("dve", lambda e: e.memset(c.ones[:], 1.0), writes=["c_ones"])
    c.eps6 = es.enter_context(nc.sbuf_tensor("c_eps6", [128, 1], F32))
    c.one1 = es.enter_context(nc.sbuf_tensor("c_one1", [128, 1], F32))
    kb.op("dve", lambda e: e.memset(c.eps6[:], 1e-6), writes=["c_eps"])
    kb.op("dve", lambda e: e.memset(c.one1[:], 1.0), writes=["c_eps"])
    c.onesb = es.enter_context(nc.sbuf_tensor("c_onesb", [128, 8], BF16))
    kb.op("dve", lambda e: e.memset(c.onesb[:], 1.0), writes=["c_ones"])
    kb.op("dve", lambda e: e.tensor_copy(out=c.identb[:], in_=c.ident[:]), reads=["c_ident"], writes=["c_identb"])
    return c


def prologue(kb, nc, es, g, c):
    mods = []
    for l in range(2):
        mods.append(es.enter_context(nc.sbuf_tensor(f"mod{l}", [128, 48, 2], F32)))
    with nc.sbuf_tensor("pl_sc", [128, 2, 8], F32) as sc, \
            nc.sbuf_tensor("pl_w0", [128, 8, 512], F32) as w0, \
            nc.sbuf_tensor("pl_w1", [128, 8, 512], F32) as w1, \
            nc.sbuf_tensor("pl_b", [128, 2, 48], F32) as adab, \
            nc.sbuf_tensor("pl_n", [128, 2, 2, 8], F32) as nrm, \
            nc.psum_tensor("pl_ps", [128, 512], F32) as ps:
        wb = [w0, w1]
        kb.dma("sp", sc[:, 0, :], g["cT"][:, :], writes=["sc"])
        kb.dma("sp", sc[:, 1, :], g["ccT"][:, :], writes=["sc"])
        kb.dma("sp", adab[:], g["ada_bT"][:, :, :], writes=["adab"])
        kb.dma("sp", nrm[:], g["normT"][:, :, :, :], writes=["nrm"])
        kb.op("act", lambda e: e.activation(out=sc[:], in_=sc[:], func=AF.Silu), reads=["sc"], writes=["sc"])
        blk = 0
        for l in range(2):
            wv = g["ada_w"][l].rearrange("(kc p) n -> p kc n", p=128)
            for nb in range(12):
                wt = wb[blk % 2]
                wk = f"plw{blk % 2}"
                kb.dma("sp" if blk % 2 == 0 else "act", wt[:], wv[:, :, nb * 512:(nb + 1) * 512], writes=[wk])
                for j in range(4):
                    for kc in range(8):
                        kb.op("pe", lambda e, kc=kc, j=j, wt=wt: e.matmul(
                            ps[:, (j * 2):(j * 2 + 2)], lhsT=wt[:, kc, j * 128:(j + 1) * 128], rhs=sc[:, :, kc],
                            start=(kc == 0), stop=(kc == 7)), reads=[wk, "sc"], writes=["psPL"])
                kb.op("dve", lambda e, l=l, nb=nb: e.tensor_tensor(
                    out=mods[l][:, nb * 4:(nb + 1) * 4, :],
                    in0=ps[:, 0:8].rearrange("p (j s) -> p j s", s=2),
                    in1=adab[:, l, nb * 4:(nb + 1) * 4].unsqueeze(2).broadcast_to([128, 4, 2]),
                    op=ALU.add), reads=["psPL", "adab"], writes=[f"mod{l}"])
                blk += 1
            for (m, which) in ((1, 0), (4, 1)):
                for s in range(2):
                    kb.op("dve", lambda e, l=l, m=m, which=which, s=s: e.scalar_tensor_tensor(
                        out=mods[l][:, m * 8:(m + 1) * 8, s], in0=mods[l][:, m * 8:(m + 1) * 8, s], scalar=1.0,
                        in1=nrm[:, l, which, :], op0=ALU.add, op1=ALU.mult),
                        reads=[f"mod{l}", "nrm"], writes=[f"mod{l}"])
    kb.barrier()
    return mods


def make_bc(kb, nc, c, col_ap_fn, out_tile, ps, key, src_keys):
    with nc.sbuf_tensor(kb.uid("bcd"), [128, 128], F32) as dg:
        dk = kb.uid("dg")
        for half in range(2):
            for q in range(4):
                kc = half * 4 + q
                kb.op("dve", lambda e, kc=kc: e.tensor_scalar(
                    out=dg[:], in0=c.ident[:], scalar1=col_ap_fn(kc), scalar2=None, op0=ALU.mult),
                    reads=["c_ident"] + src_keys, writes=[dk])
                kb.op("pe", lambda e, q=q: e.matmul(ps[:, q * 128:(q + 1) * 128], lhsT=c.ones[:], rhs=dg[:],
                                                   start=True, stop=True),
                      reads=[dk, "c_ones"], writes=["psBC" + key])
            kb.op("act", lambda e, half=half: e.copy(out=out_tile[:, half * 512:(half + 1) * 512], in_=ps[:]),
                  reads=["psBC" + key], writes=[key])
        kb.barrier()


def norm_tile_g(kb, nc, xt, xk, st, G, S, hout, hk, eps=EPS):
    sk = kb.uid("st")
    kb.op("act", lambda e: e.activation(out=hout, in_=xt, func=AF.Square, accum_out=st[:, 0:1]),
          reads=[xk], writes=[hk, sk])
    yield
    kb.op("dve", lambda e: e.tensor_scalar(out=st[:, 1:2], in0=st[:, 0:1], scalar1=1.0 / D, scalar2=eps,
                                           op0=ALU.mult, op1=ALU.add), reads=[sk], writes=[sk])
    yield
    kb.op("act", lambda e: e.activation(out=st[:, 2:3], in_=st[:, 1:2], func=AF.Sqrt), reads=[sk], writes=[sk])
    yield
    kb.op("dve", lambda e: e.reciprocal(out=st[:, 3:4], in_=st[:, 2:3]), reads=[sk], writes=[sk])
    yield
    if G is None:
        kb.op("dve", lambda e: e.tensor_scalar(out=hout, in0=xt, scalar1=st[:, 3:4], scalar2=None, op0=ALU.mult),
              reads=[xk, sk], writes=[hk])
        return
    kb.op("dve", lambda e: e.scalar_tensor_tensor(out=hout, in0=xt, scalar=st[:, 3:4], in1=G[:],
                                                  op0=ALU.mult, op1=ALU.mult),
          reads=[xk, sk, "Gbc"], writes=[hk])
    yield
    if S is not None:
        kb.op("pool", lambda e: e.tensor_tensor(out=hout, in0=hout, in1=S[:], op=ALU.add),
              reads=[hk, "Sbc"], writes=[hk])


def norm_tile(*a, **k):
    for _ in norm_tile_g(*a, **k):
        pass


def moe_stage(kb, nc, g, c, mods, layer, stream, x_in, x_out, T, final_norm=False):
    NT = T // 128
    cap = 2 * T // 16
    CW = cap
    CT = (cap + 127) // 128
    cs = min(cap, 128)
    mod = mods[layer]
    xin_v = x_in.rearrange("(n p) d -> n p d", p=128)
    xout_v = x_out.rearrange("(n p) d -> n p d", p=128)
    sx = f"L{layer}s{stream}"
    from contextlib import ExitStack
    with ExitStack() as es:
        def sb(name, shape, dt):
            return es.enter_context(nc.sbuf_tensor(f"moe_{name}_{sx}", shape, dt))
        Gbc = sb("G", [128, D], F32)
        Sbc = sb("S", [128, D], F32)
        gate2 = Sbc
        hbf = sb("hbf", [128, NT, D], BF16)
        xb = [sb("x0", [128, D], F32), sb("x1", [128, D], F32)]
        h32 = [sb("h0", [128, D], F32), sb("h1", [128, D], F32)]
        hT = [sb("hT0", [128, 8, 128], F32), sb("hT1", [128, 8, 128], F32)]
        stt = [sb("st0", [128, 4], F32), sb("st1", [128, 4], F32)]
        rt = sb("rt", [128, 8, 16], F32)
        aff = sb("aff", [128, NT, 16], F32)
        sm = sb("sm", [128, 4], F32)
        ex = sb("ex", [128, 16], F32)
        affT = sb("affT", [16, T], F32)
        work = sb("work", [16, T], F32)
        mx8 = sb("mx8", [16, 8], F32)
        maskT = sb("maskT", [16, T], F32)
        onesT = work
        slotT = sb("slotT", [16, T], F32)
        gateT = affT
        slot = sb("slot", [128, NT, 16], F32)
        gate = sb("gate", [128, NT, 16], F32)
        selT = [sb("selT0", [128, CW], BF16), sb("selT1", [128, CW], BF16)]
        xeT = sb("xeT", [128, 8, CW], BF16)
        big = sb("big", [128, 4 * 8 * D], BF16)
        wviews = [big[:, j * 8 * D:(j + 1) * 8 * D].rearrange("p (k n) -> p k n", n=D) for j in range(4)]
        w1b = [wviews[0], wviews[1]]
        w3b = [wviews[2]]
        w2b = [wviews[3]]
        yest = [sb("yest0", [128, CT, D], BF16), sb("yest1", [128, CT, D], BF16)]
        sil = [sb("sil0", [128, CW], F32), sb("sil1", [128, CW], F32)]
        hidT = sb("hidT", [128, 8, CW], BF16)
        yeall = big[:, 0:16 * CT * D].rearrange("p (e c n) -> p e c n", e=16, c=CT)
        selGa = [sb("selGa0", [128, 4, CW], BF16), sb("selGa1", [128, 4, CW], BF16)]
        selGca = [sb("selGca0", [128, 4 * CT, 128], BF16), sb("selGca1", [128, 4 * CT, 128], BF16)]
        tmpo = [sb("tmpo0", [128, 512], F32), sb("tmpo1", [128, 512], F32)]
        ps = [es.enter_context(nc.psum_tensor(f"moe_ps{i}_{sx}", [128, 512], F32)) for i in range(8)]

        print("moe sbuf remaining", nc.sbuf_bytes_remaining)
        make_bc(kb, nc, c, lambda kc: mod[:, 4 * 8 + kc, stream:stream + 1], Gbc, ps[0], "Gbc", [f"mod{layer}"])
        make_bc(kb, nc, c, lambda kc: mod[:, 3 * 8 + kc, stream:stream + 1], Sbc, ps[0], "Sbc", [f"mod{layer}"])
        kb.dma("sp", rt[:], g["moe_router"][layer].rearrange("(kc p) e -> p kc e", p=128), writes=["rt"])

        def stageA(i):
            b = i % 2
            kb.dma("sp", xb[b][:], xin_v[i], writes=[f"xb{b}"])
            yield
            yield from norm_tile_g(kb, nc, xb[b][:], f"xb{b}", stt[b], Gbc, Sbc, h32[b][:], f"h32{b}")
            yield
            kb.op("act", lambda e, i=i, b=b: e.copy(out=hbf[:, i, :], in_=h32[b][:]), reads=[f"h32{b}"],
                  writes=[f"hbf{i}"])
            for half in range(2):
                for q in range(4):
                    kc = half * 4 + q
                    kb.op("pe", lambda e, kc=kc, q=q, b=b, half=half: e.transpose(
                        out=ps[half][:, q * 128:(q + 1) * 128], in_=h32[b][:, kc * 128:(kc + 1) * 128],
                        identity=c.ident[:]), reads=[f"h32{b}", "c_ident"], writes=[f"ps{half}"])
                yield
                kb.op("dve" if half == 0 else "act", (lambda e, half=half, b=b: e.tensor_copy(
                    out=hT[b][:, half * 4:(half + 1) * 4, :], in_=ps[half][:].rearrange("p (q t) -> p q t", q=4)))
                    if half == 0 else (lambda e, half=half, b=b: e.copy(
                        out=hT[b][:, half * 4:(half + 1) * 4, :], in_=ps[half][:].rearrange("p (q t) -> p q t", q=4))),
                    reads=[f"ps{half}"], writes=[f"hT{b}"])
                yield

        def stageB(i):
            b = i % 2
            for kc in range(8):
                kb.op("pe", lambda e, kc=kc, b=b: e.matmul(ps[2][:, 0:16], lhsT=hT[b][:, kc, :], rhs=rt[:, kc, :],
                                                         start=(kc == 0), stop=(kc == 7)),
                      reads=[f"hT{b}", "rt"], writes=["ps2"])
            yield
            kb.op("dve", lambda e: e.reduce_max(out=sm[:, 0:1], in_=ps[2][:, 0:16], axis=AX.X),
                  reads=["ps2"], writes=["sm"])
            kb.op("dve", lambda e: e.tensor_scalar(out=sm[:, 1:2], in0=sm[:, 0:1], scalar1=-1.0, scalar2=None,
                                                   op0=ALU.mult), reads=["sm"], writes=["sm"])
            yield
            kb.op("act", lambda e: e.activation(out=ex[:], in_=ps[2][:, 0:16], func=AF.Exp, bias=sm[:, 1:2],
                                                accum_out=sm[:, 2:3]), reads=["ps2", "sm"], writes=["ex", "sm"])
            yield
            kb.op("dve", lambda e: e.reciprocal(out=sm[:, 3:4], in_=sm[:, 2:3]), reads=["sm"], writes=["sm"])
            kb.op("dve", lambda e, i=i: e.tensor_scalar(out=aff[:, i, :], in0=ex[:], scalar1=sm[:, 3:4], scalar2=None,
                                                        op0=ALU.mult), reads=["ex", "sm"], writes=["aff"])
            yield
            kb.op("pe", lambda e, i=i: e.transpose(out=ps[3][0:16, 0:128], in_=aff[:, i, :], identity=c.ident[:]),
                  reads=["aff", "c_ident"], writes=["ps3"])
            yield
            kb.op("act", lambda e, i=i: e.copy(out=affT[:, i * 128:(i + 1) * 128], in_=ps[3][0:16, 0:128]),
                  reads=["ps3"], writes=["affT"])
            yield

        from itertools import zip_longest
        prevB = iter(())
        for i in range(NT):
            for _ in zip_longest(stageA(i), prevB):
                pass
            prevB = stageB(i)
        for _ in prevB:
            pass

        import os
        PH = int(os.environ.get("MOE_PH", "9"))
        if PH < 2:
            kb.dma("sp", x_out[0:128, 0:NT * 16], aff[:].rearrange("p n e -> p (n e)"), reads=["aff"], writes=["o"])
            kb.barrier()
            return
        kb.op("dve", lambda e: e.tensor_copy(out=work[:], in_=affT[:]), reads=["affT"], writes=["work"])
        nr = cap // 8
        for r in range(nr):
            kb.op("dve", lambda e: e.max(out=mx8[:], in_=work[:]), reads=["work"], writes=["mx8"])
            if r < nr - 1:
                kb.op("dve", lambda e: e.match_replace(out=work[:], in_to_replace=mx8[:], in_values=work[:],
                                                       imm_value=-1.0), reads=["work", "mx8"], writes=["work"])
        kb.op("dve", lambda e: e.tensor_scalar(out=maskT[:], in0=affT[:], scalar1=mx8[:, 7:8], scalar2=None,
                                               op0=ALU.is_ge), reads=["affT", "mx8"], writes=["maskT"])
        kb.op("pool", lambda e: e.memset(onesT[:], 1.0), reads=[], writes=["work"])
        kb.op("dve", lambda e: e.tensor_tensor_scan(out=slotT[:], data0=onesT[:], data1=maskT[:], initial=0.0,
                                                    op0=ALU.mult, op1=ALU.add),
              reads=["work", "maskT"], writes=["slotT"])
        kb.op("dve", lambda e: e.tensor_tensor(out=slotT[:], in0=slotT[:], in1=maskT[:], op=ALU.mult),
              reads=["slotT", "maskT"], writes=["slotT"])
        kb.op("dve", lambda e: e.tensor_scalar(out=slotT[:], in0=slotT[:], scalar1=-1.0, scalar2=None, op0=ALU.add),
              reads=["slotT"], writes=["slotT"])
        kb.op("pool", lambda e: e.tensor_tensor(out=gateT[:], in0=affT[:], in1=maskT[:], op=ALU.mult),
              reads=["affT", "maskT"], writes=["affT"])
        for i in range(NT):
            kb.op("pe", lambda e, i=i: e.transpose(out=ps[0][:, i * 16:(i + 1) * 16],
                                                   in_=slotT[:, i * 128:(i + 1) * 128], identity=c.ident[0:16, 0:16]),
                  reads=["slotT", "c_ident"], writes=["ps0"])
            kb.op("pe", lambda e, i=i: e.transpose(out=ps[1][:, i * 16:(i + 1) * 16],
                                                   in_=gateT[:, i * 128:(i + 1) * 128], identity=c.ident[0:16, 0:16]),
                  reads=["affT", "c_ident"], writes=["ps1"])
        kb.op("dve", lambda e: e.tensor_copy(out=slot[:], in_=ps[0][:, 0:NT * 16].rearrange("p (n e) -> p n e", e=16)),
              reads=["ps0"], writes=["slot"])
        kb.op("act", lambda e: e.copy(out=gate[:], in_=ps[1][:, 0:NT * 16].rearrange("p (n e) -> p n e", e=16)),
              reads=["ps1"], writes=["gate"])

        if PH < 3:
            kb.dma("sp", x_out[0:128, 0:NT * 16], slot[:].rearrange("p n e -> p (n e)"), reads=["slot"], writes=["o"])
            kb.dma("sp", x_out[128:256, 0:NT * 16], gate[:].rearrange("p n e -> p (n e)"), reads=["gate"], writes=["o2"])
            kb.barrier()
            return
        stg = [xb[0], xb[1], h32[0], h32[1]]
        stgk = ["xb0", "xb1", "h320", "h321"]
        wcnt = [0]

        def load_w(wv, wt, wk):
            for hh in range(2):
                kb.dma("pool", wt[:, hh * 4:(hh + 1) * 4, :], wv[:, hh * 4:(hh + 1) * 4, :], writes=[wk])

        def wviews_of(e_):
            return (g["moe_w1"][layer, e_].rearrange("(kc p) n -> p kc n", p=128),
                    g["moe_w3"][layer, e_].rearrange("(kc p) n -> p kc n", p=128),
                    g["moe_w2"][layer, e_].rearrange("(kc p) n -> p kc n", p=128))
        nsel = 0
        SUB = int(os.environ.get("MOE_SUB", "9"))
        NEXP = int(os.environ.get("MOE_NEXP", "16"))
        wv1, wv3, wv2 = wviews_of(0)
        load_w(wv1, w1b[0], "w1_0")
        load_w(wv3, w3b[0], "w3")
        load_w(wv2, w2b[0], "w2")
        for ex_i in range(NEXP):
            w1t = w1b[ex_i % 2]
            w1k = f"w1_{ex_i % 2}"
            if ex_i + 1 < NEXP:
                nwv1, nwv3, nwv2 = wviews_of(ex_i + 1)
                load_w(nwv1, w1b[(ex_i + 1) % 2], f"w1_{(ex_i + 1) % 2}")
            for i in range(NT):
                b = nsel % 2
                nsel += 1
                kb.op("dve", lambda e, i=i, b=b, ex_i=ex_i: e.tensor_scalar(
                    out=selT[b][:], in0=c.iota[:, 0:CW], scalar1=slot[:, i, ex_i:ex_i + 1], scalar2=None,
                    op0=ALU.is_equal), reads=["c_iota", "slot"], writes=[f"selT{b}"])
                for kc in range(8):
                    bank = (kc * CW) // 512
                    off = (kc * CW) % 512
                    kb.op("pe", lambda e, i=i, b=b, kc=kc, bank=bank, off=off: e.matmul(
                        ps[bank][:, off:off + CW], lhsT=hbf[:, i, kc * 128:(kc + 1) * 128], rhs=selT[b][:],
                        start=(i == 0 and off == 0), stop=(i == NT - 1), skip_group_check=True), reads=[f"hbf{i}", f"selT{b}"], writes=[f"ps{bank}"])
            nb = (8 * CW + 511) // 512
            per = 512 // CW if CW < 512 else 1
            for bank in range(nb):
                k0 = bank * per
                k1 = min(8, k0 + per)
                kb.op("act" if bank % 2 else "dve", (lambda e, bank=bank, k0=k0, k1=k1: e.copy(
                    out=xeT[:, k0:k1, :], in_=ps[bank][:, 0:(k1 - k0) * CW].rearrange("p (k c) -> p k c", c=CW)))
                    if bank % 2 else (lambda e, bank=bank, k0=k0, k1=k1: e.tensor_copy(
                        out=xeT[:, k0:k1, :], in_=ps[bank][:, 0:(k1 - k0) * CW].rearrange("p (k c) -> p k c", c=CW))),
                    reads=[f"ps{bank}"], writes=["xeT"])
            if SUB < 2:
                continue
            for fc in range(8):
                pb = ps[4 + fc % 2]
                pk = f"ps{4 + fc % 2}"
                for kc in range(8):
                    kb.op("pe", lambda e, fc=fc, kc=kc, pb=pb, w1t=w1t: e.matmul(
                        pb[:, 0:CW], lhsT=w1t[:, kc, fc * 128:(fc + 1) * 128], rhs=xeT[:, kc, :],
                        start=(kc == 0), stop=(kc == 7)), reads=[w1k, "xeT"], writes=[pk])
                for kc in range(8):
                    kb.op("pe", lambda e, fc=fc, kc=kc, pb=pb: e.matmul(
                        pb[:, 256:256 + CW], lhsT=w3b[0][:, kc, fc * 128:(fc + 1) * 128], rhs=xeT[:, kc, :],
                        start=(kc == 0), stop=(kc == 7)), reads=["w3", "xeT"], writes=[pk])
                sb_ = sil[fc % 2]
                DBG = int(os.environ.get("MOE_DBG", "9"))
                if DBG < 1:
                    continue
                kb.op("act", lambda e, pb=pb, sb_=sb_: e.activation(out=sb_[:], in_=pb[:, 0:CW], func=AF.Silu),
                      reads=[pk], writes=[f"sil{fc % 2}"])
                if DBG < 2:
                    continue
                kb.op("dve", lambda e, pb=pb, sb_=sb_, fc=fc: e.tensor_tensor(
                    out=hidT[:, fc, :], in0=sb_[:], in1=pb[:, 256:256 + CW], op=ALU.mult),
                    reads=[f"sil{fc % 2}", pk], writes=["hidT"])
            if ex_i + 1 < NEXP:
                load_w(nwv3, w3b[0], "w3")
            if SUB < 3:
                continue
            for ct in range(CT):
                for half in range(2):
                    pb = ps[6 + half]
                    pk = f"ps{6 + half}"
                    for fc in range(8):
                        kb.op("pe", lambda e, ct=ct, half=half, fc=fc, pb=pb: e.matmul(
                            pb[0:cs, :], lhsT=hidT[:, fc, ct * 128:ct * 128 + cs],
                            rhs=w2b[0][:, fc, half * 512:(half + 1) * 512], start=(fc == 0), stop=(fc == 7)),
                            reads=["hidT", "w2"], writes=[pk])
                    ys = yest[ex_i % 2]
                    kb.op("act" if half else "dve", (lambda e, ct=ct, half=half, pb=pb, ys=ys: e.copy(
                        out=ys[0:cs, ct, half * 512:(half + 1) * 512], in_=pb[0:cs, :]))
                        if half else (lambda e, ct=ct, half=half, pb=pb, ys=ys: e.tensor_copy(
                            out=ys[0:cs, ct, half * 512:(half + 1) * 512], in_=pb[0:cs, :])),
                        reads=[pk], writes=[f"yest{ex_i % 2}"])
            if ex_i + 1 < NEXP:
                load_w(nwv2, w2b[0], "w2")
            kb.dma("sp", g["ye_scr"][ex_i, 0:cs, 0:CT, :], yest[ex_i % 2][0:cs, :, :], reads=[f"yest{ex_i % 2}"],
                   writes=[("yescr", ex_i)])

        if PH < 4:
            kb.barrier()
            return
        kb.barrier()
        make_bc(kb, nc, c, lambda kc: mod[:, 5 * 8 + kc, stream:stream + 1], gate2, ps[0], "gate2", [f"mod{layer}"])
        if final_norm:
            make_bc(kb, nc, c, lambda kc: c.fnT[:, kc:kc + 1], Gbc, ps[0], "Gbc", ["c_fnT"])
        for ex_i in range(16):
            kb.dma(["sp", "act"][ex_i % 2], yeall[0:cs, ex_i, :, :], g["ye_scr"][ex_i, 0:cs, 0:CT, :],
                   reads=[("yescr", ex_i)], writes=[f"ye{ex_i}"])
        nsg = 0
        EG = 4
        for i in range(NT):
            b = i % 2
            kb.dma("sp", xb[b][:], xin_v[i], writes=[f"xb{b}"])
            for g0 in range(0, 16, EG):
                sg = nsg % 2
                nsg += 1
                sga = selGa[sg]
                kb.op("dve", lambda e, i=i, g0=g0, sga=sga: e.tensor_tensor(
                    out=sga[:, :, :], in0=c.iota[:, 0:CW].unsqueeze(1).broadcast_to([128, EG, CW]),
                    in1=slot[:, i, g0:g0 + EG].unsqueeze(2).broadcast_to([128, EG, CW]), op=ALU.is_equal),
                    reads=["c_iota", "slot"], writes=[f"selGa{sg}"])
                kb.op("pool", lambda e, i=i, g0=g0, sga=sga: e.tensor_tensor(
                    out=sga[:, :, :], in0=sga[:, :, :],
                    in1=gate[:, i, g0:g0 + EG].unsqueeze(2).broadcast_to([128, EG, CW]), op=ALU.mult),
                    reads=[f"selGa{sg}", "gate"], writes=[f"selGa{sg}"])
                pst = ps[2 + sg][:].bitcast(BF16)
                for ee in range(EG):
                    for ct in range(CT):
                        kb.op("pe", lambda e, ct=ct, ee=ee, sga=sga, pst=pst: e.transpose(
                            out=pst[0:cs, (ee * CT + ct) * 128:(ee * CT + ct + 1) * 128],
                            in_=sga[:, ee, ct * 128:ct * 128 + cs], identity=c.identb[:]),
                            reads=[f"selGa{sg}", "c_identb"], writes=[f"ps{2 + sg}"])
                kb.op("act", lambda e, sg=sg, pst=pst: e.copy(
                    out=selGca[sg][0:cs, :, :], in_=pst[0:cs, 0:EG * CT * 128].rearrange("p (c t) -> p c t", t=128)),
                    reads=[f"ps{2 + sg}"], writes=[f"selGca{sg}"])
                for ee in range(EG):
                    ex_i = g0 + ee
                    for half in range(2):
                        for ct in range(CT):
                            kb.op("pe", lambda e, half=half, ct=ct, sg=sg, ex_i=ex_i, ee=ee: e.matmul(
                                ps[half][:, :], lhsT=selGca[sg][0:cs, ee * CT + ct, :],
                                rhs=yeall[0:cs, ex_i, ct, half * 512:(half + 1) * 512],
                                start=(ex_i == 0 and ct == 0), stop=(ex_i == 15 and ct == CT - 1)),
                                reads=[f"selGca{sg}", f"ye{ex_i}"], writes=[f"ps{half}"])
            for half in range(2):
                sl = slice(half * 512, (half + 1) * 512)
                kb.op("dve", lambda e, half=half, sl=sl: e.tensor_tensor(
                    out=tmpo[half][:], in0=ps[half][:], in1=gate2[:, sl], op=ALU.mult),
                    reads=[f"ps{half}", "gate2"], writes=[f"tmpo{half}"])
                kb.op("pool", lambda e, half=half, sl=sl, b=b: e.tensor_tensor(
                    out=xb[b][:, sl], in0=xb[b][:, sl], in1=tmpo[half][:], op=ALU.add),
                    reads=[f"tmpo{half}", f"xb{b}"], writes=[f"xb{b}"])
            if final_norm:
                norm_tile(kb, nc, xb[b][:], f"xb{b}", stt[b], Gbc, None, h32[b][:], f"h32{b}")
                kb.dma("sp", xout_v[i], h32[b][:], reads=[f"h32{b}"], writes=[("dram", x_out.tensor.name, i)])
            else:
                kb.dma("sp", xout_v[i], xb[b][:], reads=[f"xb{b}"], writes=[("dram", x_out.tensor.name, i)])
        kb.barrier()


def host_consts():
    k = {}
    k["k_ident"] = np.eye(128, dtype=np.float32)
    k["k_iota"] = np.tile(np.arange(256, dtype=np.float32)[None, :], (128, 1))
    C, S, perm = rope_tables()
    k["k_ropeC"], k["k_ropeS"], k["k_perm"] = C, S, perm
    kk = np.arange(128)[:, None]
    qq = np.arange(128)[None, :]
    k["k_mL"] = np.tile((kk >= qq).astype(np.float32), (1, 4))
    k["k_mU"] = np.tile((kk <= qq).astype(np.float32), (1, 4))
    ss = np.arange(CHUNK)[:, None]
    tt = np.arange(CHUNK)[None, :]
    mus = (ss < tt).astype(np.float32)
    mui = (ss <= tt).astype(np.float32)
    k["k_maskA"] = np.ascontiguousarray(np.concatenate([-mus, mus, mui, mui], axis=1))
    k["k_maskT"] = np.ascontiguousarray(-(tt < ss).astype(np.float32))
    rm = np.ones((128, TP), np.float32)
    rm[:, CHUNK_COLS] = 0.0
    k["k_rmask"] = rm
    k["k_J"] = np.ascontiguousarray(np.eye(128, dtype=np.float32)[::-1])
    sel = np.zeros((16, 16, 128), np.float32)
    for r in range(16):
        sel[r, r, :] = 1.0
    k["k_sel"] = sel
    bdm = np.zeros((128, 128), np.float32)
    bdm[0:64, 0:64] = 1.0
    bdm[64:128, 64:128] = 1.0
    k["k_bd"] = bdm
    return k


def fm(v):
    v = np.asarray(v, np.float32)
    return np.ascontiguousarray(v.reshape(-1, 128).T)


def host_inputs(inp, b):
    m = dict(host_consts())
    m["x"] = np.ascontiguousarray(inp["x"][b])
    m["ctx"] = np.ascontiguousarray(inp["ctx"][b])
    m["cT"] = fm(inp["c"][b])
    m["ccT"] = fm(inp["c_ctx"])
    m["ada_w"] = inp["ada_w"]
    m["ada_bT"] = np.ascontiguousarray(np.stack([fm(inp["ada_b"][l]) for l in range(2)], axis=1))
    m["normT"] = np.ascontiguousarray(np.stack(
        [np.stack([fm(inp["norm_mix"][l]), fm(inp["norm_ffn"][l])], axis=1) for l in range(2)], axis=1))
    m["fnT"] = fm(inp["final_norm"])
    for k in ("moe_router", "moe_w1", "moe_w3", "moe_w2", "o_w_out"):
        m[k] = inp[k]
    w = inp["o_w_in"][0]
    kd = w[:, 1024:1280].reshape(1024, 4, 1, 64)
    kd = np.concatenate([kd, kd], axis=2).reshape(1024, 512)
    m["o_w_in2"] = np.ascontiguousarray(np.concatenate([w[:, 0:1024], kd, w[:, 1280:1536]], axis=1))
    m["o_sink"] = np.ascontiguousarray(inp["o_sink"].reshape(1, 16))
    idx = []
    for p in range(4):
        for off in (0, 512, 1024):
            idx += list(range(off + p * 128, off + p * 128 + 128))
    for d in range(2):
        idx += list(range(1536 + d * 64, 1536 + d * 64 + 64)) + list(range(1664 + d * 64, 1664 + d * 64 + 64))
    idx += list(range(1792, 1920))
    idxa = list(idx)
    for h in range(4):
        for part in range(3):
            idx += list(range(1920 + part * 512 + h * 128, 1920 + part * 512 + h * 128 + 128))
        idx += list(range(3472 + h * 128, 3472 + h * 128 + 128))
    idx += list(range(3456, 3472))
    m["e_w_in2"] = np.ascontiguousarray(inp["e_w_in"][0][:, idx])
    mu2 = inp["a_mu"][0][idxa]
    p64 = np.zeros((64, 64), np.float32)
    p64[:, 0:30] = mu2.reshape(30, 64).T
    for d in range(2):
        for h in range(8):
            p64[:, 30 + d * 8 + h] = inp["a_w0"][0, d, h * 64:(h + 1) * 64]
            p64[:, 46 + d * 8 + h] = inp["a_a0"][0, d, h * 64:(h + 1) * 64]
    m["mx_par64"] = p64
    p128 = np.zeros((128, 112), np.float32)
    p128[:, 0] = mu2[1536:1664]; p128[:, 1] = mu2[1664:1792]; p128[:, 2] = mu2[1792:1920]
    for h in range(8):
        p128[0:64, 8 + h] = inp["a_k_k"][0, h * 64:(h + 1) * 64]
        p128[0:64, 16 + h] = inp["a_k_a"][0, h * 64:(h + 1) * 64]
        p128[0:64, 32 + h] = inp["a_r_k"][0, h]
    p128[0:8, 40] = inp["b_dt_bias"][0].reshape(8)
    p128[0:8, 41] = inp["b_a_log"][0].reshape(8)
    for h in range(4):
        for part in range(3):
            for j in range(5):
                p128[:, 48 + (h * 3 + part) * 5 + j] = inp["b_conv"][0, j, part * 512 + h * 128:part * 512 + (h + 1) * 128]
    m["mx_par128"] = p128
    pP = np.zeros((128, 64), np.float32)
    pP[:, 0:12] = mu2[0:1536].reshape(12, 128).T
    for p in range(4):
        sl = slice(p * 128, (p + 1) * 128)
        for d in range(2):
            pP[:, 12 + d * 4 + p] = inp["a_w0"][0, d, sl]
            pP[:, 20 + d * 4 + p] = inp["a_a0"][0, d, sl]
        pP[:, 28 + p] = inp["a_k_k"][0, sl]
        pP[:, 32 + p] = inp["a_k_a"][0, sl]
        pP[:, 40 + p] = inp["a_r_k"][0].reshape(512)[sl]
    m["mx_parP"] = pP
    m["mx_w2"] = np.ascontiguousarray(np.concatenate([inp["a_w2"][0].transpose(1, 0, 2), inp["a_a2"][0].transpose(1, 0, 2)], axis=0))
    m["mx_bc"] = np.ascontiguousarray(np.concatenate([inp["a_ln_w"][0], inp["a_ln_b"][0], np.tile(inp["b_norm"][0], 4)])[None, :])
    m["a_g2"] = inp["a_g2"]
    m["e_w_out"] = inp["e_w_out"]
    return m


IN_SHAPES = {
    "k_ident": [128, 128], "k_iota": [128, 256],
    "x": [2048, D], "ctx": [256, D], "cT": [128, 8], "ccT": [128, 8],
    "ada_w": [2, D, 6 * D], "ada_bT": [128, 2, 48], "normT": [128, 2, 2, 8], "fnT": [128, 8],
    "k_ropeC": [128, 2048], "k_ropeS": [128, 2048], "k_perm": [128, 128], "k_mL": [128, 512], "k_mU": [128, 512],
    "o_w_in2": [D, 1792], "o_w_out": [1, D, D], "o_sink": [1, 16],
    "k_maskA": [CHUNK, 4 * CHUNK], "k_maskT": [CHUNK, CHUNK], "k_rmask": [128, TP], "k_J": [128, 128], "k_sel": [16, 16, 128],
    "e_w_in2": [D, 3984], "mx_par64": [64, 64], "mx_parP": [128, 64], "k_bd": [128, 128], "mx_par128": [128, 112], "mx_w2": [128, 2, 512], "mx_bc": [1, 1536],
    "a_g2": [1, 128, 512], "e_w_out": [1, D, D],
    "moe_router": [2, D, 16], "moe_w1": [2, 16, D, D], "moe_w3": [2, 16, D, D], "moe_w2": [2, 16, D, D],
}


def build(stages=("all",), extra_in=(), outs=(("out", [2048, D]),)):
    from contextlib import ExitStack
    nc = bass.Bass("TRN2", target_bir_lowering=False)
    g = {}
    for name, shape in IN_SHAPES.items():
        g[name] = nc.dram_tensor(name, shape, F32, kind="ExternalInput").ap()
    for name, shape in extra_in:
        g[name] = nc.dram_tensor(name, shape, F32, kind="ExternalInput").ap()
    for name, shape in outs:
        g[name] = nc.dram_tensor(name, shape, F32, kind="ExternalOutput").ap()
    g["ye_scr"] = nc.dram_tensor("ye_scr", [16, 128, 2, D], BF16).ap()
    g["ys_scr"] = nc.dram_tensor("ys_scr", [2, 2304, 1536], F32).ap()
    g["z_scr"] = nc.dram_tensor("z_scr", [2304, 512], F32).ap()
    for nm, rows in (("xm_lat", 2048), ("xm_ctx", 256), ("x1_lat", 2048), ("x1_ctx", 256), ("x2_lat", 2048)):
        g[nm] = nc.dram_tensor(nm, [rows, D], F32).ap()
    kb = KB(nc)
    with ExitStack() as es:
        c = load_consts(kb, nc, es, g)
        c.fnT = es.enter_context(nc.sbuf_tensor("c_fnT", [128, 8], F32))
        kb.dma("sp", c.fnT[:], g["fnT"][:, :], writes=["c_fnT"])
        mods = prologue(kb, nc, es, g, c)
        for st in stages:
            if st == "moe0l_test":
                moe_stage(kb, nc, g, c, mods, 0, 0, g["t_in"], g["out"], 2048)
            elif st == "moe0c_test":
                moe_stage(kb, nc, g, c, mods, 0, 1, g["t_in"], g["out"], 256)
            elif st == "moe1l_test":
                moe_stage(kb, nc, g, c, mods, 1, 0, g["t_in"], g["out"], 2048, final_norm=True)
            elif st == "all":
                mixer_stage(kb, nc, g, c, mods, g["x"], g["ctx"], g["xm_lat"], g["xm_ctx"])
                moe_stage(kb, nc, g, c, mods, 0, 0, g["xm_lat"], g["x1_lat"], 2048)
                moe_stage(kb, nc, g, c, mods, 0, 1, g["xm_ctx"], g["x1_ctx"], 256)
                attn_stage(kb, nc, g, c, mods, g["x1_lat"], g["x1_ctx"], g["x2_lat"])
                moe_stage(kb, nc, g, c, mods, 1, 0, g["x2_lat"], g["out"], 2048, final_norm=True)
            elif st == "mixer_test":
                mixer_stage(kb, nc, g, c, mods, g["x"], g["ctx"], g["out"], g["out2"])
            elif st == "attn_test":
                attn_stage(kb, nc, g, c, mods, g["t_in"], g["t_in2"], g["out"])
            elif st == "mods_test":
                for l in range(2):
                    kb.dma("sp", g["out"][l * 128:(l + 1) * 128, 0:96], mods[l][:].rearrange("p a b -> p (a b)"),
                           reads=[f"mod{l}"], writes=[("o", l)])
        kb.finish([])
    return nc, kb


def rope_tables():
    quarter = 16
    inv = (10000.0 ** (-np.arange(quarter, dtype=np.float32) / quarter)).astype(np.float32)
    t = np.arange(2048)
    row = (t // 64).astype(np.float32)
    col = (t % 64).astype(np.float32)
    C = np.zeros((128, 2048), np.float32)
    S = np.zeros((128, 2048), np.float32)
    perm = np.zeros((128, 128), np.float32)
    for p in range(128):
        d = p % 64
        pos = row if d < 32 else col
        i = d % 16
        ang = (pos * inv[i]).astype(np.float32)
        C[p] = np.cos(ang)
        second = (d % 32) >= 16
        S[p] = np.sin(ang) if second else -np.sin(ang)
        partner = p - 16 if second else p + 16
        perm[partner, p] = 1.0
    return C, S, perm


def attn_stage(kb, nc, g, c, mods, x_lat_in, x_ctx_in, x_out):
    layer = 1
    mod = mods[layer]
    NT = 18
    from contextlib import ExitStack
    with ExitStack() as es:
        def sb(name, shape, dt):
            return es.enter_context(nc.sbuf_tensor(f"at_{name}", shape, dt))
        Gbc = sb("G", [128, D], F32)
        Sbc = sb("S", [128, D], F32)
        xb = [sb("x0", [128, D], F32), sb("x1", [128, D], F32)]
        hb = [sb("h0", [128, D], BF16), sb("h1", [128, D], BF16)]
        stt = [sb("st0", [128, 4], F32), sb("st1", [128, 4], F32)]
        hT = sb("hT", [128, 8, 2304], BF16)
        win = sb("win", [128, 8, 1792], BF16)
        wo = sb("wo", [128, 8, D], BF16)
        stg = [sb("stg0", [128, D], F32), sb("stg1", [128, D], F32)]
        Ct = sb("Ct", [128, 2048], F32)
        St = sb("St", [128, 2048], F32)
        perm = sb("perm", [128, 128], F32)
        qraw = [sb("qraw0", [128, 512], F32), sb("qraw1", [128, 512], F32)]
        rt1 = [sb("rt10", [128, 512], F32), sb("rt11", [128, 512], F32)]
        qT = sb("qT", [128, 8, 2048], BF16)
        kT = sb("kT", [128, 4, 2304], BF16)
        V = sb("V", [128, NT, 4, 65], BF16)
        mL = sb("mL", [128, 512], BF16)
        mU = sb("mU", [128, 512], BF16)
        esink = sb("esink", [128, 16], F32)
        PT = [sb(f"PT{i}", [128, 512], BF16) for i in range(2)]
        osb = stg[0]
        den = sb("den", [128, 4], F32)
        oT = sb("oT", [128, 8, 128], BF16)
        tmpo = qraw
        ps = [es.enter_context(nc.psum_tensor(f"at_ps{i}", [128, 512], F32)) for i in range(8)]

        kb.dma("sp", Ct[:], g["k_ropeC"][:, :], writes=["Ct"])
        kb.dma("sp", St[:], g["k_ropeS"][:, :], writes=["St"])
        kb.dma("sp", perm[:], g["k_perm"][:, :], writes=["perm"])
        kb.dma("sp", esink[:], g["o_sink"].partition_broadcast(128), writes=["esink"])
        kb.op("act", lambda e: e.activation(out=esink[:], in_=esink[:], func=AF.Exp), reads=["esink"], writes=["esink"])
        kb.dma("sp", stg[0][:, 0:512], g["k_mL"][:, :], writes=["stg0"])
        kb.op("dve", lambda e: e.tensor_copy(out=mL[:], in_=stg[0][:, 0:512]), reads=["stg0"], writes=["mL"])
        kb.dma("sp", stg[1][:, 0:512], g["k_mU"][:, :], writes=["stg1"])
        kb.op("dve", lambda e: e.tensor_copy(out=mU[:], in_=stg[1][:, 0:512]), reads=["stg1"], writes=["mU"])
        make_bc(kb, nc, c, lambda kc: mod[:, 1 * 8 + kc, 1:2], Gbc, ps[0], "Gbc", [f"mod{layer}"])
        make_bc(kb, nc, c, lambda kc: mod[:, 0 * 8 + kc, 1:2], Sbc, ps[0], "Sbc", [f"mod{layer}"])

        wcnt = [0]

        def load_rows(src_ap, dst_fn, ncols, key):
            for kc in range(8):
                j = wcnt[0] % 2
                wcnt[0] += 1
                kb.dma(["sp", "act"][kc % 2], stg[j][:, 0:ncols], src_ap[:, kc, :], writes=[f"stg{j}"])
                ce = ["dve", "pool"][wcnt[0] % 2]
                kb.op(ce, lambda e, j=j, kc=kc: e.tensor_copy(out=dst_fn(kc), in_=stg[j][:, 0:ncols]),
                      reads=[f"stg{j}"], writes=[key])
        wv = g["o_w_in2"].rearrange("(kc p) n -> p kc n", p=128)
        load_rows(wv[:, :, 0:1024], lambda kc: win[:, kc, 0:1024], 1024, "win")
        load_rows(wv[:, :, 1024:1792], lambda kc: win[:, kc, 1024:1792], 768, "win")
        load_rows(g["o_w_out"][0].rearrange("(kc p) n -> p kc n", p=128), lambda kc: wo[:, kc, :], 1024, "wo")

        for i in range(NT):
            b = i % 2
            if i == 2:
                kb.barrier()
                make_bc(kb, nc, c, lambda kc: mod[:, 1 * 8 + kc, 0:1], Gbc, ps[0], "Gbc", [f"mod{layer}"])
                make_bc(kb, nc, c, lambda kc: mod[:, 0 * 8 + kc, 0:1], Sbc, ps[0], "Sbc", [f"mod{layer}"])
            src = x_ctx_in[i * 128:(i + 1) * 128, :] if i < 2 else x_lat_in[(i - 2) * 128:(i - 1) * 128, :]
            kb.dma("sp", xb[b][:], src, writes=[f"xb{b}"])
            norm_tile(kb, nc, xb[b][:], f"xb{b}", stt[b], Gbc, Sbc, stg[b][:], f"stg{b}")
            kb.op("act", lambda e, b=b: e.copy(out=hb[b][:], in_=stg[b][:]), reads=[f"stg{b}"], writes=[f"hb{b}"])
            for half in range(2):
                pst = ps[half][:].bitcast(BF16)
                for q in range(4):
                    kc = half * 4 + q
                    kb.op("pe", lambda e, kc=kc, q=q, b=b, pst=pst: e.transpose(
                        out=pst[:, q * 128:(q + 1) * 128], in_=hb[b][:, kc * 128:(kc + 1) * 128],
                        identity=c.identb[:]), reads=[f"hb{b}", "c_identb"], writes=[f"ps{half}"])
                kb.op("dve" if half == 0 else "pool" if False else "act",
                      (lambda e, half=half, pst=pst, i=i: e.tensor_copy(
                          out=hT[:, half * 4:(half + 1) * 4, i * 128:(i + 1) * 128],
                          in_=pst[:, 0:512].rearrange("p (q t) -> p q t", q=4))) if half == 0 else
                      (lambda e, half=half, pst=pst, i=i: e.copy(
                          out=hT[:, half * 4:(half + 1) * 4, i * 128:(i + 1) * 128],
                          in_=pst[:, 0:512].rearrange("p (q t) -> p q t", q=4))),
                      reads=[f"ps{half}"], writes=["hT"])

        import os
        APH = int(os.environ.get("ATT_PH", "9"))
        if APH < 1:
            kb.barrier(); return
        nb = 0
        for nq in range(8):
            for tb in range(4):
                b = nb % 2
                nb += 1
                pq = ps[2 + b]
                t0 = 256 + tb * 512
                for kc in range(8):
                    kb.op("pe", lambda e, kc=kc, nq=nq, t0=t0, pq=pq: e.matmul(
                        pq[:], lhsT=win[:, kc, nq * 128:(nq + 1) * 128], rhs=hT[:, kc, t0:t0 + 512],
                        start=(kc == 0), stop=(kc == 7)), reads=["win", "hT"], writes=[f"ps{2 + b}"])
                kb.op("act", lambda e, b=b, pq=pq: e.copy(out=qraw[b][:], in_=pq[:]), reads=[f"ps{2 + b}"],
                      writes=[f"qraw{b}"])
                pw = ps[4 + b]
                kb.op("pe", lambda e, b=b, pw=pw: e.matmul(pw[:], lhsT=perm[:], rhs=qraw[b][:], start=True, stop=True),
                      reads=["perm", f"qraw{b}"], writes=[f"ps{4 + b}"])
                cs_ = slice(tb * 512, (tb + 1) * 512)
                kb.op("dve", lambda e, b=b, pw=pw, cs_=cs_: e.scalar_tensor_tensor(
                    out=rt1[b][:], in0=pw[:], scalar=0.125, in1=St[:, cs_], op0=ALU.mult, op1=ALU.mult),
                      reads=[f"ps{4 + b}", "St"], writes=[f"rt1{b}"])
                kb.op("pool", lambda e, b=b, cs_=cs_: e.tensor_tensor(out=qraw[b][:], in0=qraw[b][:], in1=Ct[:, cs_],
                                                                    op=ALU.mult),
                      reads=[f"qraw{b}", "Ct"], writes=[f"qraw{b}"])
                kb.op("dve", lambda e, b=b, nq=nq, cs_=cs_: e.scalar_tensor_tensor(
                    out=qT[:, nq, cs_], in0=qraw[b][:], scalar=0.125, in1=rt1[b][:], op0=ALU.mult, op1=ALU.add),
                    reads=[f"qraw{b}", f"rt1{b}"], writes=["qT"])
        if APH < 2:
            kb.barrier(); return
        for hk in range(4):
            for tb in range(5):
                b = nb % 2
                nb += 1
                pq = ps[2 + b]
                t0 = 0 if tb == 0 else 256 + (tb - 1) * 512
                tw = 256 if tb == 0 else 512
                for kc in range(8):
                    kb.op("pe", lambda e, kc=kc, hk=hk, t0=t0, tw=tw, pq=pq: e.matmul(
                        pq[:, 0:tw], lhsT=win[:, kc, 1024 + hk * 128:1024 + (hk + 1) * 128],
                        rhs=hT[:, kc, t0:t0 + tw], start=(kc == 0), stop=(kc == 7)),
                        reads=["win", "hT"], writes=[f"ps{2 + b}"])
                if tb == 0:
                    kb.op("act", lambda e, hk=hk, pq=pq: e.copy(out=kT[:, hk, 0:256], in_=pq[:, 0:256]),
                          reads=[f"ps{2 + b}"], writes=["kT"])
                    continue
                kb.op("act", lambda e, b=b, pq=pq: e.copy(out=qraw[b][:], in_=pq[:]), reads=[f"ps{2 + b}"],
                      writes=[f"qraw{b}"])
                pw = ps[4 + b]
                kb.op("pe", lambda e, b=b, pw=pw: e.matmul(pw[:], lhsT=perm[:], rhs=qraw[b][:], start=True, stop=True),
                      reads=["perm", f"qraw{b}"], writes=[f"ps{4 + b}"])
                cs_ = slice((tb - 1) * 512, tb * 512)
                kb.op("dve", lambda e, b=b, pw=pw, cs_=cs_: e.tensor_tensor(out=rt1[b][:], in0=pw[:], in1=St[:, cs_],
                                                                         op=ALU.mult),
                      reads=[f"ps{4 + b}", "St"], writes=[f"rt1{b}"])
                kb.op("pool", lambda e, b=b, cs_=cs_: e.tensor_tensor(out=qraw[b][:], in0=qraw[b][:], in1=Ct[:, cs_],
                                                                    op=ALU.mult),
                      reads=[f"qraw{b}", "Ct"], writes=[f"qraw{b}"])
                kb.op("dve", lambda e, b=b, hk=hk, t0=t0: e.tensor_tensor(
                    out=kT[:, hk, t0:t0 + 512], in0=qraw[b][:], in1=rt1[b][:], op=ALU.add),
                    reads=[f"qraw{b}", f"rt1{b}"], writes=["kT"])
        if APH < 3:
            kb.barrier(); return
        kb.op("pool", lambda e: e.memset(V[:], 1.0), writes=["V"])
        for i in range(NT):
            b = nb % 2
            nb += 1
            pq = ps[2 + b]
            for kc in range(8):
                kb.op("pe", lambda e, kc=kc, i=i, pq=pq: e.matmul(
                    pq[:, 0:256], lhsT=hT[:, kc, i * 128:(i + 1) * 128], rhs=win[:, kc, 1536:1792],
                    start=(kc == 0), stop=(kc == 7)), reads=["win", "hT"], writes=[f"ps{2 + b}"])
            kb.op("act", lambda e, i=i, pq=pq: e.copy(out=V[:, i, :, 0:64],
                                                     in_=pq[:, 0:256].rearrange("p (h d) -> p h d", h=4)),
                  reads=[f"ps{2 + b}"], writes=["V"])

        if APH < 4:
            kb.barrier(); return
        make_bc(kb, nc, c, lambda kc: mod[:, 2 * 8 + kc, 0:1], Gbc, ps[0], "Gbc", [f"mod{layer}"])
        nsb = 0
        for n in range(16):
            kb.dma("sp", xb[n % 2][:], x_lat_in[n * 128:(n + 1) * 128, :], writes=[f"xb{n % 2}"])
            for hk in range(4):
                tiles = []
                if n > 0:
                    tiles.append((2 + n - 1, mL, "mL"))
                tiles.append((2 + n, None, None))
                if n < 15:
                    tiles.append((2 + n + 1, mU, "mU"))
                tiles.append((0, None, None))
                tiles.append((1, None, None))
                po = ps[6 + (n * 4 + hk) % 2]
                pok = f"ps{6 + (n * 4 + hk) % 2}"
                for ti, (kt, msk, mk) in enumerate(tiles):
                    sbk = nsb % 2
                    nsb += 1
                    pSa, pSb = ps[1 + 2 * sbk], ps[2 + 2 * sbk]
                    ka, kbk = f"ps{1 + 2 * sbk}", f"ps{2 + 2 * sbk}"
                    for gq in range(4):
                        hq = hk * 4 + gq
                        bp = (hq % 2) * 64
                        pS = pSa if bp == 0 else pSb
                        kb.op("pe", lambda e, gq=gq, hq=hq, bp=bp, kt=kt, pS=pS, hk=hk, n=n: e.matmul(
                            pS[:, (gq // 2) * 128:(gq // 2 + 1) * 128], lhsT=kT[bp:bp + 64, hk, kt * 128:(kt + 1) * 128],
                            rhs=qT[bp:bp + 64, hq // 2, n * 128:(n + 1) * 128], start=True, stop=True),
                            reads=["kT", "qT"], writes=[ka if bp == 0 else kbk])
                    ptv = PT[sbk][:].rearrange("p (a b q) -> p a b q", a=2, b=2)
                    kb.op("act", lambda e, pSa=pSa, ptv=ptv: e.activation(
                        out=ptv[:, :, 0, :], in_=pSa[:, 0:256].rearrange("p (a q) -> p a q", a=2), func=AF.Exp),
                        reads=[ka], writes=[f"PT{sbk}"])
                    kb.op("act", lambda e, pSb=pSb, ptv=ptv: e.activation(
                        out=ptv[:, :, 1, :], in_=pSb[:, 0:256].rearrange("p (a q) -> p a q", a=2), func=AF.Exp),
                        reads=[kbk], writes=[f"PT{sbk}"])
                    ADBG = int(os.environ.get("ATT_DBG", "9"))
                    if ADBG < 2:
                        continue
                    if msk is not None:
                        kb.op("dve", lambda e, sbk=sbk, msk=msk: e.tensor_tensor(out=PT[sbk][:], in0=PT[sbk][:],
                                                                               in1=msk[:], op=ALU.mult),
                              reads=[f"PT{sbk}", mk], writes=[f"PT{sbk}"])
                    for gq in range(4):
                        kb.op("pe", lambda e, gq=gq, sbk=sbk, kt=kt, hk=hk, po=po, ti=ti: e.matmul(
                            po[:, gq * 65:(gq + 1) * 65], lhsT=PT[sbk][:, gq * 128:(gq + 1) * 128],
                            rhs=V[:, kt, hk, :], start=(ti == 0 and gq == 0), stop=(ti == len(tiles) - 1),
                            skip_group_check=True), reads=[f"PT{sbk}", "V"], writes=[pok])
                if ADBG < 3:
                    continue
                pov = po[:, 0:260].rearrange("p (g d) -> p g d", g=4)
                kb.op("dve", lambda e, pov=pov, hk=hk: e.tensor_tensor(
                    out=den[:], in0=pov[:, :, 64], in1=esink[:, hk * 4:(hk + 1) * 4], op=ALU.add),
                    reads=[pok, "esink"], writes=["den"])
                kb.op("dve", lambda e: e.reciprocal(out=den[:], in_=den[:]), reads=["den"], writes=["den"])
                kb.op("dve", lambda e, pov=pov, hk=hk: e.tensor_tensor(
                    out=osb[:, hk * 256:(hk + 1) * 256].rearrange("p (g d) -> p g d", g=4), in0=pov[:, :, 0:64],
                    in1=den[:].unsqueeze(2).broadcast_to([128, 4, 64]), op=ALU.mult),
                    reads=[pok, "den"], writes=["stg0"])
            if ADBG < 4:
                continue
            kb.op("act", lambda e: e.copy(out=hb[0][:], in_=osb[:]), reads=["stg0"], writes=["hb0"])
            pst = ps[0][:].bitcast(BF16)
            for kc in range(8):
                kb.op("pe", lambda e, kc=kc, pst=pst: e.transpose(
                    out=pst[:, kc * 128:(kc + 1) * 128], in_=hb[0][:, kc * 128:(kc + 1) * 128], identity=c.identb[:]),
                    reads=["hb0", "c_identb"], writes=["ps0"])
            kb.op("act", lambda e, pst=pst: e.copy(out=oT[:], in_=pst[:].rearrange("p (k t) -> p k t", k=8)),
                  reads=["ps0"], writes=["oT"])
            for half in range(2):
                pp = ps[5] if half == 0 else ps[0]
                for kc in range(8):
                    kb.op("pe", lambda e, kc=kc, half=half, pp=pp: e.matmul(
                        pp[:], lhsT=oT[:, kc, :], rhs=wo[:, kc, half * 512:(half + 1) * 512],
                        start=(kc == 0), stop=(kc == 7)), reads=["oT", "wo"], writes=["ps5" if half == 0 else "ps0"])
                sl = slice(half * 512, (half + 1) * 512)
                kb.op("dve", lambda e, half=half, pp=pp, sl=sl: e.tensor_tensor(
                    out=tmpo[half][:], in0=pp[:], in1=Gbc[:, sl], op=ALU.mult),
                    reads=["ps5" if half == 0 else "ps0", "Gbc"], writes=[f"qraw{half}"])
                kb.op("pool", lambda e, half=half, sl=sl, n=n: e.tensor_tensor(
                    out=xb[n % 2][:, sl], in0=xb[n % 2][:, sl], in1=tmpo[half][:], op=ALU.add),
                    reads=[f"qraw{half}", f"xb{n % 2}"], writes=[f"xb{n % 2}"])
            kb.dma("sp", x_out[n * 128:(n + 1) * 128, :], xb[n % 2][:], reads=[f"xb{n % 2}"],
                   writes=[("dram", x_out.tensor.name, n)])
        kb.barrier()


def dplr_scan(kb, nc, c, T, dk, rT, kkT, kT, bT, vT, Pinc, prodT, store_cb, hk_, Gb=None, rA=None, kkA=None, bp=0, rC=None, kkC=None):
    ps = T["ps"]
    rA = rT if rA is None else rA
    kkA = kkT if kkA is None else kkA
    rC = rT if rC is None else rC
    kkC = kkT if kkC is None else kkC
    tb = vT.dtype == BF16
    ident, maskA, maskT, ident64 = c.ident, T["maskA"], T["maskT"], c.ident
    ST = T["ST"]
    kb.op("dve", lambda e: e.memset(ST[bp:bp + dk, 0:dk], 0.0), writes=["ST"])
    kb.op("dve", lambda e: e.memset(T["STb"][bp:bp + dk, 0:dk], 0.0), writes=["STb"])
    CH = CHUNK
    NLV = {64: 5, 128: 6}[CH]
    NCH = len(CHUNK_COLS)
    GB = 2

    def inv_gen(g0):
        grp = list(range(g0, min(NCH, g0 + GB)))
        par = (g0 // GB) % 2
        for ci in grp:
            s = ci % GB + GB * par
            cs = slice(CHUNK_COLS[ci], CHUNK_COLS[ci] + CH)
            pa = ps[s % 2]
            pk = f"ps{s % 2}"
            pn = ps[2 + s % 2]
            pnk = f"ps{2 + s % 2}"
            for j, (l, r) in enumerate(((bT, kkA), (kT, kkA), (bT, rA), (kT, rA), (kkA, bT))):
                if j < 4:
                    kb.op("pe", lambda e, j=j, l=l, r=r, cs=cs, pa=pa: e.matmul(
                        pa[0:CH, j * CH:(j + 1) * CH], lhsT=l[bp:bp + dk, cs], rhs=r[bp:bp + dk, cs], start=True, stop=True),
                        reads=[hk_], writes=[pk])
                else:
                    kb.op("pe", lambda e, l=l, r=r, cs=cs, pn=pn: e.matmul(
                        pn[0:CH, 256:256 + CH], lhsT=l[bp:bp + dk, cs], rhs=r[bp:bp + dk, cs], start=True, stop=True),
                        reads=[hk_], writes=[pnk])
            mA, mT_, mAk, mTk = maskA[0:CH, :], maskT[0:CH, :], "maskA", "maskT"
            if Gb is not None:
                c0_ = CHUNK_COLS[ci]
                kb.op("pe", lambda e, cs=cs, pn=pn: e.transpose(out=pn[0:CH, 384:385], in_=Gb[0:1, cs],
                                                              identity=ident[0:1, 0:1]), reads=[hk_, "c_ident"], writes=[pnk])
                kb.op("act", lambda e, s=s, pn=pn: e.copy(out=T["Gc"][0:CH, s:s + 1], in_=pn[0:CH, 384:385]),
                      reads=[pnk], writes=[f"Gc{s}"])
                kb.op("dve", lambda e, s=s, cs=cs: e.tensor_scalar(out=T["Dt"][0:CH, s % GB, :], in0=Gb[0:CH, cs],
                                                                 scalar1=T["Gc"][0:CH, s:s + 1], scalar2=0.0,
                                                                 op0=ALU.subtract, op1=ALU.min),
                      reads=[hk_, f"Gc{s}"], writes=[f"Dt{s % GB}"])
                kb.op("act", lambda e, s=s: e.activation(out=T["Dt"][0:CH, s % GB, :], in_=T["Dt"][0:CH, s % GB, :], func=AF.Exp),
                      reads=[f"Dt{s % GB}"], writes=[f"Dt{s % GB}"])
                kb.op("dve", lambda e, s=s, cs=cs: e.tensor_scalar(out=T["Dts"][0:CH, s % GB, :], in0=Gb[0:CH, cs],
                                                                 scalar1=T["Gc"][0:CH, s:s + 1], scalar2=0.0,
                                                                 op0=ALU.subtract, op1=ALU.max),
                      reads=[hk_, f"Gc{s}"], writes=[f"Dts{s % GB}"])
                kb.op("act", lambda e, s=s: e.activation(out=T["Dts"][0:CH, s % GB, :], in_=T["Dts"][0:CH, s % GB, :], func=AF.Exp,
                                                         scale=-1.0), reads=[f"Dts{s % GB}"], writes=[f"Dts{s % GB}"])
                kb.op("dve", lambda e, s=s: e.tensor_tensor(
                    out=T["mD"][0:CH, s % GB, :].rearrange("p (a t) -> p a t", a=4),
                    in0=maskA[0:CH, :].rearrange("p (a t) -> p a t", a=4),
                    in1=T["Dt"][0:CH, s % GB, :].unsqueeze(1).broadcast_to([CH, 4, CH]), op=ALU.mult),
                    reads=["maskA", f"Dt{s % GB}"], writes=[f"mD{s % GB}"])
                kb.op("pool", lambda e, s=s: e.tensor_tensor(out=T["Dts"][0:CH, s % GB, :], in0=T["Dts"][0:CH, s % GB, :],
                                                            in1=maskT[0:CH, :], op=ALU.mult),
                      reads=["maskT", f"Dts{s % GB}"], writes=[f"Dts{s % GB}"])
                kb.op("act", lambda e, s=s, c0_=c0_: e.activation(out=T["dL"][0:CH, s:s + 1], in_=T["Gc"][0:CH, s:s + 1],
                                                                func=AF.Exp, scale=-1.0, bias=Gb[0:CH, c0_ + CH - 1:c0_ + CH]),
                      reads=[f"Gc{s}", hk_], writes=[f"dL{s}"])
                mA, mT_, mAk, mTk = T["mD"][0:CH, s % GB, :], T["Dts"][0:CH, s % GB, :], f"mD{s % GB}", f"Dts{s % GB}"
            kb.op("dve", lambda e, s=s, pa=pa, mA=mA: e.tensor_tensor(out=T["AMf"][0:CH, s, :], in0=pa[0:CH, 0:CH],
                                                                     in1=mA[:, 0:CH], op=ALU.mult),
                  reads=[pk, mAk], writes=[f"AM{s}"])
            kb.op("dve", lambda e, s=s, pa=pa, mA=mA: e.tensor_tensor(out=T["AMb"][0:CH, s, :], in0=pa[0:CH, CH:4 * CH],
                                                                     in1=mA[:, CH:4 * CH], op=ALU.mult),
                  reads=[pk, mAk], writes=[f"AMb{s}"])
            kb.op("dve", lambda e, s=s, pn=pn, mT_=mT_: e.tensor_tensor(out=T["MM"][0][0:CH, s, CH:2 * CH],
                                                                       in0=pn[0:CH, 256:256 + CH], in1=mT_, op=ALU.mult),
                  reads=[pnk, mTk], writes=[f"MM0_{s}"])
            kb.op("pool", lambda e, s=s: e.tensor_copy(out=T["MM"][0][0:CH, s, 0:CH], in_=T["AMf"][0:CH, s, :]),
                  reads=[f"AM{s}"], writes=[f"MM0_{s}"])
            kb.op("pool", lambda e, s=s: e.tensor_tensor(out=T["Q"][0][0:CH, s, :], in0=T["AMf"][0:CH, s, :],
                                                        in1=ident64[0:CH, 0:CH], op=ALU.add),
                  reads=[f"AM{s}", "c_ident"], writes=[f"Q0_{s}"])
            yield
        for lv in range(NLV):
            a, b = lv % 2, (lv + 1) % 2
            last = lv == NLV - 1
            for ci in grp:
                s = ci % GB + GB * par
                pm = ps[2 + s % 2]
                pmk = f"ps{2 + s % 2}"
                MMa = T["MM"][a]
                kb.op("pe", lambda e, s=s, pm=pm, MMa=MMa: e.matmul(
                    pm[0:CH, 0:CH], lhsT=MMa[0:CH, s, CH:2 * CH], rhs=MMa[0:CH, s, 0:CH], start=True, stop=True),
                    reads=[f"MM{a}_{s}"], writes=[pmk])
                kb.op("pe", lambda e, s=s, pm=pm, MMa=MMa: e.matmul(
                    pm[0:CH, CH:2 * CH], lhsT=MMa[0:CH, s, 0:CH], rhs=MMa[0:CH, s, CH:2 * CH], start=True, stop=True),
                    reads=[f"MM{a}_{s}"], writes=[pmk])
                kb.op("act", lambda e, s=s, pm=pm, b=b: e.copy(out=T["MM"][b][0:CH, s, :], in_=pm[0:CH, 0:2 * CH]),
                      reads=[pmk], writes=[f"MM{b}_{s}"])
                yield
            for ci in grp:
                s = ci % GB + GB * par
                pq = ps[4 + s % 2]
                pqk = f"ps{4 + s % 2}"
                kb.op("pe", lambda e, s=s, pq=pq, b=b, a=a: e.matmul(
                    pq[0:CH, 0:CH], lhsT=T["MM"][b][0:CH, s, CH:2 * CH], rhs=T["Q"][a][0:CH, s, :], start=True, stop=True),
                    reads=[f"MM{b}_{s}", f"Q{a}_{s}"], writes=[pqk])
                kb.op("dve", lambda e, s=s, pq=pq, a=a, b=b: e.tensor_tensor(
                    out=T["Q"][b][0:CH, s, :], in0=pq[0:CH, 0:CH], in1=T["Q"][a][0:CH, s, :], op=ALU.add),
                    reads=[pqk, f"Q{a}_{s}"], writes=[f"Q{b}_{s}"])
                yield

    def chain_gen(g0):
        grp = list(range(g0, min(NCH, g0 + GB)))
        par = (g0 // GB) % 2
        QF = T["Q"][NLV % 2]
        for ci in grp:
            s = ci % GB + GB * par
            c0 = CHUNK_COLS[ci]
            cs = slice(c0, c0 + CH)
            AMb = T["AMb"]
            STb = T["STb"]
            pt = ps[6][:].bitcast(BF16) if tb else ps[6]
            idt = c.identb if tb else ident
            for j, src in enumerate((vT, kT, bT)):
                kb.op("pe", lambda e, j=j, src=src, cs=cs, pt=pt: e.transpose(
                    out=pt[0:CH, j * dk:(j + 1) * dk], in_=src[bp:bp + dk, cs], identity=idt[bp:bp + dk, bp:bp + dk]),
                    reads=[hk_, "c_ident", "c_identb"], writes=["ps6"])
            TM = T["TM"][ci % 2]
            tmk = f"TM{ci % 2}"
            kb.op("act", lambda e, TM=TM, pt=pt: e.copy(out=TM[0:CH, 0:3 * dk], in_=pt[0:CH, 0:3 * dk]),
                  reads=["ps6"], writes=[tmk])
            yield
            Vtm, Ktm, Btm = TM[0:CH, 0:dk], TM[0:CH, dk:2 * dk], TM[0:CH, 2 * dk:3 * dk]
            if Gb is not None:
                kb.op("dve", lambda e, TM=TM, s=s: e.tensor_scalar(out=TM[0:CH, dk:3 * dk], in0=TM[0:CH, dk:3 * dk],
                                                                 scalar1=T["dL"][0:CH, s:s + 1], scalar2=None, op0=ALU.mult),
                      reads=[tmk, f"dL{s}"], writes=[tmk])
            pr = ps[7]
            kb.op("pe", lambda e, cs=cs, pr=pr: e.matmul(pr[0:CH, 0:dk], lhsT=kkC[bp:bp + dk, cs], rhs=STb[bp:bp + dk, 0:dk],
                                                       start=True, stop=False), reads=[hk_, "STb"], writes=["ps7"])
            kb.op("pe", lambda e, s=s, pr=pr, Vtm=Vtm: e.matmul(pr[0:CH, 0:dk], lhsT=AMb[0:CH, s, 0:CH], rhs=Vtm,
                                                              start=False, stop=True),
                  reads=[f"AMb{s}", tmk], writes=["ps7"])
            yield
            kb.op("dve", lambda e, pr=pr: e.tensor_scalar(out=T["nR"][0:CH, 0:dk], in0=pr[0:CH, 0:dk], scalar1=-1.0,
                                                         scalar2=None, op0=ALU.mult), reads=["ps7"], writes=["nR"])
            yield
            kb.op("pe", lambda e, s=s, pr=pr: e.matmul(pr[0:CH, 128:128 + dk], lhsT=QF[0:CH, s, :], rhs=T["nR"][0:CH, 0:dk],
                                                     start=True, stop=True), reads=[f"Q{NLV % 2}_{s}", "nR"], writes=["ps7"])
            yield
            kb.op("act", lambda e, pr=pr: e.copy(out=T["U"][0:CH, 0:dk], in_=pr[0:CH, 128:128 + dk]),
                  reads=["ps7"], writes=["U"])
            yield
            U = T["U"]
            kb.op("pe", lambda e, cs=cs, pr=pr: e.matmul(pr[0:CH, 256:256 + dk], lhsT=rC[bp:bp + dk, cs], rhs=STb[bp:bp + dk, 0:dk],
                                                       start=True, stop=False), reads=[hk_, "STb"], writes=["ps7"])
            kb.op("pe", lambda e, s=s, pr=pr: e.matmul(pr[0:CH, 256:256 + dk], lhsT=AMb[0:CH, s, CH:2 * CH],
                                                     rhs=U[0:CH, 0:dk], start=False, stop=False),
                  reads=[f"AMb{s}", "U"], writes=["ps7"])
            kb.op("pe", lambda e, s=s, pr=pr, Vtm=Vtm: e.matmul(pr[0:CH, 256:256 + dk], lhsT=AMb[0:CH, s, 2 * CH:3 * CH],
                                                              rhs=Vtm, start=False, stop=True),
                  reads=[f"AMb{s}", tmk], writes=["ps7"])
            kb.op("pe", lambda e, pr=pr, Btm=Btm: e.matmul(pr[bp:bp + dk, 384:384 + dk], lhsT=Btm, rhs=U[0:CH, 0:dk],
                                                         start=True, stop=False), reads=[tmk, "U"], writes=["ps7"])
            kb.op("pe", lambda e, pr=pr, Ktm=Ktm, Vtm=Vtm: e.matmul(pr[bp:bp + dk, 384:384 + dk], lhsT=Ktm, rhs=Vtm,
                                                                  start=False, stop=True),
                  reads=[tmk], writes=["ps7"])
            yield
            Ysb = T["Y"][ci % 2]
            yk = f"Y{ci % 2}"
            kb.op("act", lambda e, pr=pr, Ysb=Ysb: e.copy(out=Ysb[0:CH, 0:dk], in_=pr[0:CH, 256:256 + dk]),
                  reads=["ps7"], writes=[yk])
            if Gb is not None:
                kb.op("dve", lambda e, pr=pr, c0=c0: e.scalar_tensor_tensor(
                    out=ST[bp:bp + dk, 0:dk], in0=ST[bp:bp + dk, 0:dk], scalar=Pinc[bp:bp + dk, c0 + CH - 1:c0 + CH],
                    in1=pr[bp:bp + dk, 384:384 + dk], op0=ALU.mult, op1=ALU.add), reads=["ps7", "ST", hk_], writes=["ST"])
            else:
                kb.op("dve", lambda e, pr=pr: e.tensor_tensor(out=ST[bp:bp + dk, 0:dk], in0=pr[bp:bp + dk, 384:384 + dk],
                                                             in1=ST[bp:bp + dk, 0:dk], op=ALU.add),
                      reads=["ps7", "ST"], writes=["ST"])
                kb.op("dve", lambda e, c0=c0: e.tensor_scalar(out=ST[bp:bp + dk, 0:dk], in0=ST[bp:bp + dk, 0:dk],
                                                             scalar1=Pinc[bp:bp + dk, c0 + CH - 1:c0 + CH], scalar2=None,
                                                             op0=ALU.mult), reads=["ST", hk_], writes=["ST"])
            kb.op("act", lambda e: e.copy(out=T["STb"][bp:bp + dk, 0:dk], in_=ST[bp:bp + dk, 0:dk]),
                  reads=["ST"], writes=["STb"])
            if prodT is not None:
                pb = ps[6]
                kb.op("pe", lambda e, cs=cs, pb=pb: e.matmul(pb[0:CH, 448:449], lhsT=prodT[bp:bp + dk, cs],
                                                           rhs=c.onesb[bp:bp + dk, 0:1], start=True, stop=True),
                      reads=[hk_, "c_ones"], writes=["ps6"])
                kb.op("dve", lambda e, pb=pb, Ysb=Ysb, Vtm=Vtm: e.tensor_scalar(
                    out=Ysb[0:CH, dk:2 * dk], in0=Vtm, scalar1=pb[0:CH, 448:449], scalar2=None, op0=ALU.mult),
                    reads=["ps6", tmk], writes=[yk])
            store_cb(ci, Ysb, yk)
            yield

    from itertools import zip_longest
    for _ in inv_gen(0):
        pass
    for g0 in range(0, NCH, GB):
        gens = [chain_gen(g0)]
        if g0 + GB < NCH:
            gens.append(inv_gen(g0 + GB))
        for _ in zip_longest(*gens):
            pass


def mixer_stage(kb, nc, g, c, mods, x_lat_in, x_ctx_in, x_lat_out, x_ctx_out):
    from contextlib import ExitStack
    mod = mods[0]
    Ys = g["ys_scr"]
    Zs = g["z_scr"]
    with ExitStack() as es:
        def sb(name, shape, dt):
            return es.enter_context(nc.sbuf_tensor(f"mxs_{name}", shape, dt))
        hT = None
        es2 = ExitStack()

        def sb2(name, shape, dt):
            return es2.enter_context(nc.sbuf_tensor(f"mxs_{name}", shape, dt))
        hb = sb("hb", [128, D], BF16)
        stt = [sb("st0", [128, 4], F32), sb("st1", [128, 4], F32)]
        F11 = sb("F11", [128, TP], F32)
        Jb = sb("Jb", [128, 128], BF16)
        J32 = sb("J32", [128, 128], F32)
        par = sb("par", [64, 64], F32)
        parP = sb("parP", [128, 64], F32)
        bd = sb("bd", [128, 128], F32)
        par128 = sb("par128", [128, 112], F32)
        w2sb = sb("w2sb", [128, 2, 512], F32)
        sel = sb("sel", [16, 16, 128], F32)
        ST = sb("ST", [128, 128], F32)
        hT = es2.enter_context(nc.sbuf_tensor("mxs_hT", [128, 8, 2304], BF16))
        F = [sb2(f"F{i}", [128, TP], F32) for i in range(11)] + [F11]
        FK = [f"F{i}" for i in range(12)]
        Gbc = F[1][:, 0:D]
        Sbc = F[2][:, 0:D]
        h32 = F[3][:, 0:D]
        xb = [F[4][:, 0:D], F[5][:, 0:D]]
        h32s = [F[3][:, 0:D], F[6][:, 0:D]]
        hb2 = sb2("hb2", [128, D], BF16)
        hbs = [hb, hb2]
        rmask = sb2("rmask", [128, TP], BF16)
        rm32 = F[0]
        wsl = sb2("wsl", [128, 8, 128], BF16)
        T = {"ST": ST,
             "AMf": sb2("AMf", [CHUNK, 4, CHUNK], F32), "AMb": sb2("AMb", [CHUNK, 4, 3 * CHUNK], BF16),
             "STb": sb2("STb", [128, 128], BF16),
             "MM": [sb2("MMa", [CHUNK, 4, 2 * CHUNK], F32), sb2("MMb", [CHUNK, 4, 2 * CHUNK], F32)],
             "Q": [sb2("Qa", [CHUNK, 4, CHUNK], F32), sb2("Qb", [CHUNK, 4, CHUNK], F32)],
             "TM": [sb2("TMa", [CHUNK, 384], BF16), sb2("TMb", [CHUNK, 384], BF16)],
             "nR": sb2("nR", [CHUNK, 128], F32), "U": sb2("U", [CHUNK, 128], BF16), "Uf": sb2("Uf", [16, 4], F32),
             "Y": [sb2("Ya", [CHUNK, 256], F32), sb2("Yb", [CHUNK, 256], F32)],
             "maskA": sb2("maskA", [CHUNK, 4 * CHUNK], F32), "maskT": sb2("maskT", [CHUNK, CHUNK], F32),
             "Gc": sb2("Gc", [CHUNK, 4], F32), "dL": sb2("dL", [CHUNK, 4], F32), "Dt": sb2("Dt", [CHUNK, 2, CHUNK], F32),
             "Dts": sb2("Dts", [CHUNK, 2, CHUNK], F32), "mD": sb2("mD", [CHUNK, 2, 4 * CHUNK], F32)}
        ps = [es.enter_context(nc.psum_tensor(f"mx_ps{i}", [128, 512], F32)) for i in range(8)]
        T["ps"] = ps
        kb.dma("sp", rm32[:], g["k_rmask"][:, :], writes=["F0"])
        kb.op("dve", lambda e: e.tensor_copy(out=rmask[:], in_=rm32[:]), reads=["F0"], writes=["rmask"])
        kb.dma("sp", J32[:], g["k_J"][:, :], writes=["J32"])
        kb.op("dve", lambda e: e.tensor_copy(out=Jb[:], in_=J32[:]), reads=["J32"], writes=["Jb"])
        kb.dma("sp", par[:], g["mx_par64"][:, :], writes=["par"])
        kb.dma("sp", par128[:], g["mx_par128"][:, :], writes=["par128"])
        kb.dma("sp", w2sb[:], g["mx_w2"][:, :, :], writes=["w2sb"])
        kb.dma("sp", parP[:], g["mx_parP"][:, :], writes=["parP"])
        kb.dma("sp", bd[:], g["k_bd"][:, :], writes=["bd"])
        kb.op("dve", lambda e: e.tensor_scalar(out=parP[:, 36:40], in0=parP[:, 32:36], scalar1=-1.0, scalar2=1.0,
                                               op0=ALU.mult, op1=ALU.add), reads=["parP"], writes=["parP"])
        kb.op("dve", lambda e: e.tensor_scalar(out=par128[:, 24:32], in0=par128[:, 16:24], scalar1=-1.0, scalar2=1.0,
                                               op0=ALU.mult, op1=ALU.add), reads=["par128"], writes=["par128"])
        T["zt"] = [sb2("zta", [128, 128], F32), sb2("ztb", [128, 128], F32)]
        kb.dma("sp", sel[:], g["k_sel"][:, :, :], writes=["sel"])
        kb.dma("sp", T["maskA"][:], g["k_maskA"][:, :], writes=["maskA"])
        kb.dma("sp", T["maskT"][:], g["k_maskT"][:, :], writes=["maskT"])
        for f in range(12):
            kb.op("pool", lambda e, f=f: e.memset(F[f][:], 0.0), reads=["rmask"] if f == 0 else [], writes=[FK[f]])
        wv = g["e_w_in2"].rearrange("(kc p) n -> p kc n", p=128)
        P64 = lambda j: par[:, j:j + 1]

        def project(c0, M, dst, dk_, evac_eng="act"):
            kb.dma("pool", wsl[:, :, 0:M], wv[:, :, c0:c0 + M], writes=["wsl"])
            for bi, (t0, tw, col0) in enumerate(BLOCKS):
                pb = ps[bi % 2]
                for kc in range(8):
                    kb.op("pe", lambda e, kc=kc, t0=t0, tw=tw, pb=pb: e.matmul(
                        pb[0:M, 0:tw], lhsT=wsl[:, kc, 0:M], rhs=hT[:, kc, t0:t0 + tw], start=(kc == 0), stop=(kc == 7)),
                        reads=["wsl", "hT"], writes=[f"ps{bi % 2}"])
                kb.op("act", lambda e, tw=tw, col0=col0, pb=pb: e.copy(out=dst[0:M, col0:col0 + tw], in_=pb[0:M, 0:tw]),
                      reads=[f"ps{bi % 2}"], writes=[dk_])

        def tshift(src, sk, dst, dk_, tmp, tk, P, mucol):
            n = TP - 2
            kb.op("dve", lambda e: e.tensor_tensor(out=tmp[0:P, 1:1 + n], in0=src[0:P, 0:n], in1=src[0:P, 2:2 + n],
                                                   op=ALU.add), reads=[sk], writes=[tk])
            kb.op("dve", lambda e: e.scalar_tensor_tensor(out=tmp[0:P, 1:1 + n], in0=tmp[0:P, 1:1 + n], scalar=0.5,
                                                          in1=src[0:P, 1:1 + n], op0=ALU.mult, op1=ALU.subtract),
                  reads=[sk, tk], writes=[tk])
            kb.op("dve", lambda e: e.scalar_tensor_tensor(out=dst[0:P, 1:1 + n], in0=tmp[0:P, 1:1 + n], scalar=mucol,
                                                         in1=src[0:P, 1:1 + n], op0=ALU.mult, op1=ALU.add),
                  reads=[sk, tk, "par", "par128"], writes=[dk_])

        DC = [(CTX0, 256), (LAT0, 2048)]

        def ew(eng, fn, reads, writes):
            kb.op(eng, fn, reads=reads, writes=writes)

        for d in range(2):
            for stream in (1, 0):
                kb.barrier()
                make_bc(kb, nc, c, lambda kc: mod[:, 1 * 8 + kc, stream:stream + 1], Gbc, ps[0], "Gbc", ["mod0"])
                make_bc(kb, nc, c, lambda kc: mod[:, 0 * 8 + kc, stream:stream + 1], Sbc, ps[0], "Sbc", ["mod0"])
                nt = 2 if stream == 1 else 16
                src = x_ctx_in if stream == 1 else x_lat_in
                base = 0 if stream == 1 else 256
                def htile(i):
                    b = i % 2
                    hbb, h32b = hbs[b], h32s[b]
                    kb.dma("sp", xb[b][:], src[i * 128:(i + 1) * 128, :], writes=[f"xb{b}"])
                    yield
                    yield from norm_tile_g(kb, nc, xb[b][:], f"xb{b}", stt[b], Gbc, Sbc, h32b, f"h32{b}")
                    yield
                    kb.op("act", lambda e: e.copy(out=hbb[:], in_=h32b), reads=[f"h32{b}"], writes=[f"hb{b}"])
                    yield
                    pos = base + (i if d == 0 else nt - 1 - i) * 128
                    for half in range(2):
                        pbk = 2 + half + 2 * b
                        for q in range(4):
                            kc = half * 4 + q
                            kb.op("pe", lambda e, kc=kc, q=q, pbk=pbk: e.matmul(
                                ps[pbk][:, q * 128:(q + 1) * 128], lhsT=hbb[:, kc * 128:(kc + 1) * 128],
                                rhs=(c.identb[:] if d == 0 else Jb[:]), start=True, stop=True),
                                reads=[f"hb{b}", "c_identb", "Jb"], writes=[f"ps{pbk}"])
                        yield
                        kb.op("dve" if half == 0 else "act", (lambda e, half=half, pos=pos, pbk=pbk: e.tensor_copy(
                            out=hT[:, half * 4:(half + 1) * 4, pos:pos + 128],
                            in_=ps[pbk][:].rearrange("p (q t) -> p q t", q=4))) if half == 0 else
                            (lambda e, half=half, pos=pos, pbk=pbk: e.copy(
                                out=hT[:, half * 4:(half + 1) * 4, pos:pos + 128],
                                in_=ps[pbk][:].rearrange("p (q t) -> p q t", q=4))),
                            reads=[f"ps{pbk}"], writes=[("hT", pos, half)])
                        yield
                pending = [htile(i) for i in range(nt)]
                active = []
                while pending or active:
                    if pending and len(active) < 2:
                        active.append(pending.pop(0))
                    for g_ in list(active):
                        try:
                            next(g_)
                        except StopIteration:
                            active.remove(g_)
            kb.barrier()

            def store_cb_factory(col0, dk_):
                def cb(ci, Ysb, yk):
                    r0 = ci * CHUNK
                    kb.dma("sp", Ys[d, r0:r0 + CHUNK, col0:col0 + dk_], Ysb[0:CHUNK, 0:dk_], reads=[yk],
                           writes=[("ys", d, ci, col0)])
                    if col0 < 512:
                        kb.dma("sp", Ys[d, r0:r0 + CHUNK, 1024 + col0:1024 + col0 + dk_], Ysb[0:CHUNK, dk_:2 * dk_],
                               reads=[yk], writes=[("ysb", d, ci, col0)])
                return cb

            project(1536 + d * 128, 128, F[0], FK[0])
            tshift(F[0], FK[0], F[10], FK[10], F[1], FK[1], 128, par128[:, d:d + 1])
            ew("act", lambda e: e.activation(out=F[10][0:64, :], in_=F[10][0:64, :], func=AF.Tanh), [FK[10]], [FK[10]])
            if d == 0:
                project(1792, 128, F[0], FK[0])
                tshift(F[0], FK[0], F[11], FK[11], F[1], FK[1], 128, par128[:, 2:3])
                ew("act", lambda e: e.activation(out=F[11][:], in_=F[11][:], func=AF.Sigmoid), [FK[11]], [FK[11]])
            for p in range(4):
                hk_ = f"pair{d}_{p}"
                PP = lambda j: parP[:, j:j + 1]
                for part, dst in ((0, 1), (1, 2), (2, 3)):
                    project(p * 384 + part * 128, 128, F[0], FK[0])
                    tshift(F[0], FK[0], F[dst], FK[dst], F[7], FK[7], 128, PP(p * 3 + part))
                for bi, (t0, tw, col0) in enumerate(BLOCKS):
                    cs = slice(col0, col0 + tw)
                    kb.op("pe", lambda e, cs=cs, tw=tw: e.matmul(ps[2][:, 0:tw], lhsT=w2sb[0:64, d, p * 128:(p + 1) * 128],
                                                               rhs=F[10][0:64, cs], start=True, stop=True),
                          reads=["w2sb", FK[10]], writes=["ps2"])
                    kb.op("pe", lambda e, cs=cs, tw=tw: e.matmul(ps[3][:, 0:tw], lhsT=w2sb[64:128, d, p * 128:(p + 1) * 128],
                                                               rhs=F[10][64:128, cs], start=True, stop=True),
                          reads=["w2sb", FK[10]], writes=["ps3"])
                    kb.op("act", lambda e, cs=cs, tw=tw: e.activation(out=F[4][:, cs], in_=ps[2][:, 0:tw],
                                                                    func=AF.Sigmoid, bias=PP(12 + d * 4 + p)),
                          reads=["ps2", "parP"], writes=[FK[4]])
                    kb.op("act", lambda e, cs=cs, tw=tw: e.activation(out=F[5][:, cs], in_=ps[3][:, 0:tw],
                                                                    func=AF.Sigmoid, bias=PP(20 + d * 4 + p)),
                          reads=["ps3", "parP"], writes=[FK[5]])
                kkc, kac, kac1, rkc = PP(28 + p), PP(32 + p), PP(36 + p), PP(40 + p)
                ew("dve", lambda e: e.tensor_scalar(out=F[7][:, :], in0=F[2][:, :], scalar1=kkc, scalar2=None,
                                                    op0=ALU.mult), [FK[2], "parP"], [FK[7]])
                for (a0_, n_) in DC:
                    ew("pool", lambda e, a0_=a0_, n_=n_: e.tensor_tensor(
                        out=F[0][:, a0_:a0_ + n_], in0=F[7][:, a0_:a0_ + n_], in1=F[7][:, a0_:a0_ + n_],
                        op=ALU.mult), [FK[7]], [FK[0]])
                for bi, (t0, tw, col0) in enumerate(BLOCKS):
                    cs = slice(col0, col0 + tw)
                    kb.op("pe", lambda e, cs=cs, tw=tw: e.matmul(ps[2][:, 0:tw], lhsT=bd[:, :],
                                                               rhs=F[0][:, cs], start=True, stop=True),
                          reads=["bd", FK[0]], writes=["ps2"])
                    kb.op("act", lambda e, cs=cs, tw=tw: e.activation(out=F[9][:, cs], in_=ps[2][:, 0:tw],
                                                                    func=AF.Sqrt, bias=c.eps6[:, 0:1]),
                          reads=["ps2", "c_eps"], writes=[FK[9]])
                for (a0_, n_) in DC:
                    cs = slice(a0_, a0_ + n_)
                    ew("dve", lambda e, cs=cs: e.reciprocal(out=F[9][:, cs], in_=F[9][:, cs]), [FK[9]], [FK[9]])
                    ew("dve", lambda e, cs=cs: e.tensor_tensor(out=F[6][:, cs], in0=F[7][:, cs], in1=F[9][:, cs],
                                                               op=ALU.mult), [FK[7], FK[9]], [FK[6]])
                    ew("pool", lambda e, cs=cs: e.tensor_scalar(out=F[7][:, cs], in0=F[5][:, cs], scalar1=kac,
                                                                scalar2=kac1, op0=ALU.mult, op1=ALU.add),
                       [FK[5], "parP"], [FK[7]])
                    ew("pool", lambda e, cs=cs: e.tensor_tensor(out=F[7][:, cs], in0=F[7][:, cs], in1=F[2][:, cs],
                                                                op=ALU.mult), [FK[7], FK[2]], [FK[7]])
                    ew("dve", lambda e, cs=cs: e.tensor_tensor(out=F[9][:, cs], in0=F[6][:, cs], in1=F[5][:, cs],
                                                               op=ALU.mult), [FK[6], FK[5]], [FK[9]])
                    ew("dve", lambda e, cs=cs: e.scalar_tensor_tensor(out=F[0][:, cs], in0=F[1][:, cs], scalar=rkc,
                                                                      in1=F[7][:, cs], op0=ALU.mult, op1=ALU.mult),
                       [FK[1], FK[7], "parP"], [FK[0]])
                ew("dve", lambda e: e.tensor_tensor_scan(out=F[8][:, :], data0=rmask[:, :], data1=F[4][:, :],
                                                         initial=0.0, op0=ALU.mult, op1=ALU.add),
                   ["rmask", FK[4]], [FK[8]])
                B4 = F[4][:, :].bitcast(BF16)
                B5 = F[5][:, :].bitcast(BF16)
                B2 = F[2][:, :].bitcast(BF16)
                DCS = [slice(a0_, a0_ + n_) for (a0_, n_) in DC]
                DCH = [slice(TP + a0_, TP + a0_ + n_) for (a0_, n_) in DC]
                for cs in DCS:
                    ew("pool", lambda e, cs=cs: e.tensor_tensor(out=F[2][:, cs], in0=F[8][:, cs], in1=F[4][:, cs],
                                                                op=ALU.subtract), [FK[8], FK[4]], [FK[2]])
                    ew("act", lambda e, cs=cs: e.activation(out=F[2][:, cs], in_=F[2][:, cs], func=AF.Exp,
                                                            scale=-DECAY_K), [FK[2]], [FK[2]])
                for cs in DCS:
                    ew("dve", lambda e, cs=cs: e.tensor_tensor(out=B4[:, cs], in0=F[6][:, cs], in1=F[2][:, cs],
                                                               op=ALU.mult), [FK[6], FK[2]], [FK[4]])
                for cs in DCS:
                    ew("act", lambda e, cs=cs: e.activation(out=F[2][:, cs], in_=F[8][:, cs], func=AF.Exp,
                                                            scale=DECAY_K), [FK[8], FK[2], FK[4]], [FK[2]])
                for cs, ch in zip(DCS, DCH):
                    ew("dve", lambda e, cs=cs, ch=ch: e.tensor_tensor(out=B4[:, ch], in0=F[7][:, cs], in1=F[2][:, cs],
                                                                      op=ALU.mult), [FK[7], FK[2]], [FK[4]])
                    ew("pool", lambda e, cs=cs: e.tensor_tensor(out=B5[:, cs], in0=F[9][:, cs], in1=F[2][:, cs],
                                                                op=ALU.mult), [FK[9], FK[2]], [FK[5]])
                for cs in DCS:
                    ew("act", lambda e, cs=cs: e.activation(out=F[8][:, cs], in_=F[8][:, cs], func=AF.Exp,
                                                            scale=-DECAY_K), [FK[8]], [FK[8]])
                for cs, ch in zip(DCS, DCH):
                    ew("dve", lambda e, cs=cs: e.tensor_tensor(out=B2[:, cs], in0=F[1][:, cs], in1=F[8][:, cs],
                                                               op=ALU.mult), [FK[1], FK[8], FK[4], FK[5]], [FK[2]])
                    ew("pool", lambda e, cs=cs, ch=ch: e.tensor_copy(out=B5[:, ch], in_=F[3][:, cs]), [FK[3]], [FK[5]])
                    ew("act", lambda e, cs=cs, ch=ch: e.copy(out=B2[:, ch], in_=F[0][:, cs]), [FK[0]], [FK[2]])
                HI = lambda B: B[:, TP:2 * TP]
                kb.op("pool", lambda e: e.memset(T["nR"][:], 0.0),
                      reads=[FK[2], FK[4], FK[5], FK[8]], writes=[hk_, "nR"])
                for hh in range(2):
                    dplr_scan(kb, nc, c, T, 64, B2[:, 0:TP], B4[:, 0:TP], HI(B4), B5[:, 0:TP], HI(B5), F[8], HI(B2),
                              store_cb_factory((2 * p + hh) * 64, 64), hk_, bp=hh * 64)
                kb.op("pool", lambda e: e.memset(T["nR"][:], 0.0), reads=[hk_],
                      writes=[FK[2], FK[4], FK[5], FK[8], "nR"])
            mixer_gdn_pass(kb, nc, g, c, T, d, F, FK, rmask, par128, sel, project, store_cb_factory, ps, DC, wv, wsl,
                           hT, Zs)
        kb.barrier()
        es2.close()
        import os
        if os.environ.get("MIX_DUMP"):
            kb.dma("sp", x_lat_out[0:2048, :], Ys[0, 0:2048, 0:1024], writes=["o1"])
            kb.dma("sp", x_ctx_out[0:256, :], Ys[0, 0:256, 512:1536], writes=["o2"])
            kb.barrier()
            return
        mixer_output(kb, nc, g, c, mods, T, F, FK, x_lat_in, x_ctx_in, x_lat_out, x_ctx_out, Ys, Zs, J32, None, None, hb,
                     ps, par128)
        kb.barrier()


def mixer_gdn_pass(kb, nc, g, c, T, d, F, FK, rmask, par128, sel, project, store_cb_factory, ps, DC, wv, wsl, hT, Zs):
    ew = lambda eng, fn, r, w: kb.op(eng, fn, reads=r, writes=w)
    project(3968, 16, F[10], FK[10])
    R16 = lambda i: F[i][0:16, :]
    ew("act", lambda e: e.activation(out=T["Uf"][0:16, 0:1], in_=par128[0:16, 41:42], func=AF.Exp), ["par128"], ["U"])
    ew("dve", lambda e: e.tensor_scalar(out=T["Uf"][0:16, 0:1], in0=T["Uf"][0:16, 0:1], scalar1=-1.0, scalar2=None,
                                        op0=ALU.mult), ["U"], ["U"])
    ew("dve", lambda e: e.tensor_scalar(out=R16(1), in0=R16(10), scalar1=par128[0:16, 40:41], scalar2=None, op0=ALU.add),
       [FK[10], "par128"], [FK[1]])
    ew("act", lambda e: e.activation(out=R16(4), in_=R16(10), func=AF.Sigmoid), [FK[10]], [FK[4]])
    ew("act", lambda e: e.activation(out=R16(2), in_=R16(1), func=AF.Abs), [FK[1]], [FK[2]])
    ew("act", lambda e: e.activation(out=R16(2), in_=R16(2), func=AF.Exp, scale=-1.0), [FK[2]], [FK[2]])
    ew("dve", lambda e: e.tensor_scalar(out=R16(3), in0=R16(2), scalar1=2.0, scalar2=None, op0=ALU.add), [FK[2]], [FK[3]])
    ew("dve", lambda e: e.reciprocal(out=R16(3), in_=R16(3)), [FK[3]], [FK[3]])
    ew("dve", lambda e: e.tensor_tensor(out=R16(2), in0=R16(2), in1=R16(3), op=ALU.mult), [FK[2], FK[3]], [FK[2]])
    ew("dve", lambda e: e.tensor_tensor(out=R16(3), in0=R16(2), in1=R16(2), op=ALU.mult), [FK[2]], [FK[3]])
    ew("dve", lambda e: e.tensor_scalar(out=R16(6), in0=R16(3), scalar1=1.0 / 13, scalar2=1.0 / 11, op0=ALU.mult,
                                        op1=ALU.add), [FK[3]], [FK[6]])
    for cf in (1.0 / 9, 1.0 / 7, 1.0 / 5, 1.0 / 3, 1.0):
        ew("dve", lambda e: e.tensor_tensor(out=R16(6), in0=R16(6), in1=R16(3), op=ALU.mult), [FK[6], FK[3]], [FK[6]])
        ew("dve", lambda e, cf=cf: e.tensor_scalar(out=R16(6), in0=R16(6), scalar1=cf, scalar2=None, op0=ALU.add),
           [FK[6]], [FK[6]])
    ew("dve", lambda e: e.scalar_tensor_tensor(out=R16(6), in0=R16(6), scalar=2.0, in1=R16(2), op0=ALU.mult, op1=ALU.mult),
       [FK[6], FK[2]], [FK[6]])
    ew("dve", lambda e: e.tensor_scalar(out=R16(1), in0=R16(1), scalar1=0.0, scalar2=None, op0=ALU.max), [FK[1]], [FK[1]])
    ew("dve", lambda e: e.tensor_tensor(out=R16(6), in0=R16(6), in1=R16(1), op=ALU.add), [FK[6], FK[1]], [FK[6]])
    ew("dve", lambda e: e.tensor_scalar(out=R16(6), in0=R16(6), scalar1=T["Uf"][0:16, 0:1], scalar2=None, op0=ALU.mult),
       [FK[6], "U"], [FK[6]])
    ew("dve", lambda e: e.tensor_tensor_scan(out=R16(10), data0=rmask[0:16, :], data1=R16(6), initial=0.0,
                                             op0=ALU.mult, op1=ALU.add), ["rmask", FK[6]], [FK[10]])
    for h in range(4):
        hk_ = f"ghead{d}_{h}"
        c0 = 1920 + h * 512
        for part, dst in ((0, 1), (1, 2), (2, 3)):
            project(c0 + part * 128, 128, F[0], FK[0])
            n = TP - 4
            for j in range(5):
                jj = j if d == 0 else 4 - j
                wc = par128[:, 48 + (h * 3 + part) * 5 + jj:49 + (h * 3 + part) * 5 + jj]
                if j == 0:
                    ew("dve", lambda e, wc=wc, dst=dst: e.tensor_scalar(out=F[dst][:, 2:2 + n], in0=F[0][:, 0:n],
                                                                      scalar1=wc, scalar2=None, op0=ALU.mult),
                       [FK[0], "par128"], [FK[dst]])
                else:
                    ew("dve", lambda e, wc=wc, dst=dst, j=j: e.scalar_tensor_tensor(
                        out=F[dst][:, 2:2 + n], in0=F[0][:, j:j + n], scalar=wc, in1=F[dst][:, 2:2 + n],
                        op0=ALU.mult, op1=ALU.add), [FK[0], FK[dst], "par128"], [FK[dst]])
            ew("act", lambda e, dst=dst: e.activation(out=F[dst][:, :], in_=F[dst][:, :], func=AF.Silu), [FK[dst]], [FK[dst]])
        for src, scl in ((1, float(128 ** -0.5)), (2, 1.0)):
            for (a0_, n_) in DC:
                ew("pool", lambda e, src=src, a0_=a0_, n_=n_: e.tensor_tensor(
                    out=F[0][:, a0_:a0_ + n_], in0=F[src][:, a0_:a0_ + n_], in1=F[src][:, a0_:a0_ + n_], op=ALU.mult),
                    [FK[src]], [FK[0]])
            for bi, (t0, tw, col0) in enumerate(BLOCKS):
                cs = slice(col0, col0 + tw)
                kb.op("pe", lambda e, cs=cs, tw=tw: e.matmul(ps[2][:, 0:tw], lhsT=c.ones[:, :], rhs=F[0][:, cs],
                                                           start=True, stop=True), reads=["c_ones", FK[0]], writes=["ps2"])
                kb.op("act", lambda e, cs=cs, tw=tw: e.activation(out=F[9][:, cs], in_=ps[2][:, 0:tw], func=AF.Sqrt,
                                                                bias=c.eps6[:, 0:1]), reads=["ps2", "c_eps"], writes=[FK[9]])
            for (a0_, n_) in DC:
                cs = slice(a0_, a0_ + n_)
                ew("dve", lambda e, cs=cs: e.reciprocal(out=F[9][:, cs], in_=F[9][:, cs]), [FK[9]], [FK[9]])
                ew("dve", lambda e, cs=cs, src=src, scl=scl: e.scalar_tensor_tensor(
                    out=F[src][:, cs], in0=F[src][:, cs], scalar=scl, in1=F[9][:, cs], op0=ALU.mult, op1=ALU.mult),
                    [FK[src], FK[9]], [FK[src]])
        ra, rb = d * 4 + h, 8 + d * 4 + h
        for bi, (t0, tw, col0) in enumerate(BLOCKS):
            cs = slice(col0, col0 + tw)
            kb.op("pe", lambda e, cs=cs, tw=tw: e.matmul(ps[2][:, 0:tw], lhsT=sel[0:16, ra, :], rhs=F[10][0:16, cs],
                                                       start=True, stop=True), reads=["sel", FK[10]], writes=["ps2"])
            kb.op("pe", lambda e, cs=cs, tw=tw: e.matmul(ps[3][:, 0:tw], lhsT=sel[0:16, rb, :], rhs=F[4][0:16, cs],
                                                       start=True, stop=True), reads=["sel", FK[4]], writes=["ps3"])
            kb.op("act", lambda e, cs=cs, tw=tw: e.activation(out=F[6][:, cs], in_=ps[2][:, 0:tw], func=AF.Exp),
                  reads=["ps2"], writes=[FK[6]])
            kb.op("dve", lambda e, cs=cs, tw=tw: e.tensor_copy(out=F[0][:, cs], in_=ps[2][:, 0:tw]),
                  reads=["ps2"], writes=[FK[0]])
            kb.op("act", lambda e, cs=cs, tw=tw: e.copy(out=F[9][:, cs], in_=ps[3][:, 0:tw]),
                  reads=["ps3"], writes=[FK[9]])
        GB5 = F[5][:, :].bitcast(BF16)
        for (a0_, n_) in DC:
            cs = slice(a0_, a0_ + n_)
            ew("dve", lambda e, cs=cs: e.tensor_tensor(out=F[9][:, cs], in0=F[9][:, cs], in1=F[2][:, cs], op=ALU.mult),
               [FK[9], FK[2]], [FK[9]])
            ew("pool", lambda e, cs=cs: e.tensor_tensor(out=GB5[:, cs], in0=F[2][:, cs], in1=F[6][:, cs], op=ALU.mult),
               [FK[2], FK[6]], [FK[5]])
            ew("dve", lambda e, cs=cs: e.tensor_tensor(out=GB5[:, TP + cs.start:TP + cs.stop], in0=F[1][:, cs],
                                                       in1=F[6][:, cs], op=ALU.mult),
               [FK[1], FK[6]], [FK[5]])
        kb.op("pool", lambda e: e.memset(T["nR"][:], 0.0), reads=[FK[0], FK[1], FK[2], FK[3], FK[5], FK[6], FK[9]],
              writes=[hk_, "nR"])
        dplr_scan(kb, nc, c, T, 128, F[1], F[2], F[9], F[9], F[3], F[6], None, store_cb_factory(512 + h * 128, 128), hk_,
                  Gb=F[0], rA=F[1], kkA=F[2], rC=GB5[:, TP:2 * TP], kkC=GB5[:, 0:TP])
        kb.op("pool", lambda e: e.memset(T["nR"][:], 0.0), reads=[hk_],
              writes=[FK[0], FK[1], FK[2], FK[3], FK[5], FK[6], FK[9], "nR"])
    if d == 0:
        n = 0
        for h in range(4):
            kb.dma("pool", wsl[:, :, 0:128], wv[:, :, 1920 + h * 512 + 384:1920 + h * 512 + 512], writes=["wsl"])
            for i in range(18):
                pb = ps[n % 2]
                for kc in range(8):
                    kb.op("pe", lambda e, kc=kc, i=i, pb=pb: e.matmul(pb[:, 0:128], lhsT=hT[:, kc, i * 128:(i + 1) * 128],
                                                                   rhs=wsl[:, kc, 0:128], start=(kc == 0), stop=(kc == 7)),
                          reads=["wsl", "hT"], writes=[f"ps{n % 2}"])
                zt = T["zt"][n % 2]
                kb.op("act", lambda e, pb=pb, zt=zt: e.activation(out=zt[:], in_=pb[:, 0:128], func=AF.Silu),
                      reads=[f"ps{n % 2}"], writes=[f"zt{n % 2}"])
                kb.dma("sp", Zs[i * 128:(i + 1) * 128, h * 128:(h + 1) * 128], zt[:], reads=[f"zt{n % 2}"],
                       writes=[("zs", i, h)])
                n += 1


def mixer_output(kb, nc, g, c, mods, T, F, FK, x_lat_in, x_ctx_in, x_lat_out, x_ctx_out, Ys, Zs, J32, xb, Gbc, hb, ps,
                 par128):
    ew = lambda eng, fn, r, w: kb.op(eng, fn, reads=r, writes=w)
    kb.barrier()
    from contextlib import ExitStack
    with ExitStack() as es:
        def sb(name, shape, dt):
            return es.enter_context(nc.sbuf_tensor(f"mo_{name}", shape, dt))
        wo = sb("wo", [128, 8, D], BF16)
        g2 = sb("g2", [128, 512], F32)
        bcp = sb("bcp", [128, 3, 512], F32)
        yf = sb("yf", [128, 1536], F32)
        yb = sb("yb", [128, 1536], F32)
        zt = sb("zt", [128, 512], F32)
        o = sb("o", [128, D], F32)
        t1 = sb("t1", [128, 512], F32)
        s8 = sb("s8", [128, 4, 8], F32)
        oT = sb("oT", [128, 8, 128], BF16)
        Gbc = sb("Gbc", [128, D], F32)
        xb = [sb("x0", [128, D], F32), sb("x1", [128, D], F32)]
        for kc in range(8):
            kb.dma("pool", wo[:, kc, :], g["e_w_out"][0].rearrange("(kc p) n -> p kc n", p=128)[:, kc, :], writes=["wo"])
        kb.dma("sp", g2[:], g["a_g2"][0], writes=["g2"])
        kb.dma("sp", bcp[:].rearrange("p a n -> p (a n)"), g["mx_bc"].partition_broadcast(128), writes=["bcp"])
        for stream in (1, 0):
            kb.barrier()
            make_bc(kb, nc, c, lambda kc: mods[0][:, 2 * 8 + kc, stream:stream + 1], Gbc, ps[0], "Gbc", ["mod0"])
            nt = 2 if stream == 1 else 16
            src = x_ctx_in if stream == 1 else x_lat_in
            dst = x_ctx_out if stream == 1 else x_lat_out
            base = 0 if stream == 1 else 256
            colbase = CTX0 if stream == 1 else LAT0
            for i in range(nt):
                b = i % 2
                r0 = base + i * 128
                rb0 = base + (nt - 1 - i) * 128
                kb.dma("sp", xb[b][:], src[i * 128:(i + 1) * 128, :], writes=[f"xb{b}"])
                kb.dma("sp", yf[:], Ys[0, r0:r0 + 128, :], reads=[("ysall",)], writes=["yf"])
                kb.dma("act", yb[:], Ys[1, rb0:rb0 + 128, :], reads=[("ysall",)], writes=["yb"])
                kb.dma("sp", zt[:], Zs[r0:r0 + 128, :], reads=[("ysall",)], writes=["zt"])
                for q in range(3):
                    kb.op("pe", lambda e, q=q: e.matmul(ps[1 + q][:, :], lhsT=J32[:, :], rhs=yb[:, q * 512:(q + 1) * 512],
                                                      start=True, stop=True), reads=["J32", "yb"], writes=[f"ps{1 + q}"])
                    ew("dve", lambda e, q=q: e.tensor_tensor(out=yf[:, q * 512:(q + 1) * 512], in0=yf[:, q * 512:(q + 1) * 512],
                                                             in1=ps[1 + q][:, :], op=ALU.add), [f"ps{1 + q}", "yf"], ["yf"])
                y3 = yf[:, 0:512].rearrange("p (h j) -> p h j", h=8)
                ew("dve", lambda e: e.reduce_sum(out=s8[:, 0, :], in_=y3, axis=AX.X), ["yf"], ["s8"])
                ew("dve", lambda e: e.tensor_scalar(out=s8[:, 0, :], in0=s8[:, 0, :], scalar1=-1.0 / 64, scalar2=None,
                                                    op0=ALU.mult), ["s8"], ["s8"])
                ew("dve", lambda e: e.tensor_tensor(out=y3, in0=y3, in1=s8[:, 0, :].unsqueeze(2).broadcast_to([128, 8, 64]),
                                                    op=ALU.add), ["yf", "s8"], ["yf"])
                ew("pool", lambda e: e.tensor_tensor(out=t1[:], in0=yf[:, 0:512], in1=yf[:, 0:512], op=ALU.mult), ["yf"], ["t1"])
                ew("dve", lambda e: e.reduce_sum(out=s8[:, 1, :], in_=t1[:].rearrange("p (h j) -> p h j", h=8), axis=AX.X),
                   ["t1"], ["s8"])
                ew("dve", lambda e: e.tensor_scalar(out=s8[:, 1, :], in0=s8[:, 1, :], scalar1=1.0 / 64, scalar2=64e-5,
                                                    op0=ALU.mult, op1=ALU.add), ["s8"], ["s8"])
                ew("act", lambda e: e.activation(out=s8[:, 1, :], in_=s8[:, 1, :], func=AF.Sqrt), ["s8"], ["s8"])
                ew("dve", lambda e: e.reciprocal(out=s8[:, 1, :], in_=s8[:, 1, :]), ["s8"], ["s8"])
                ew("dve", lambda e: e.tensor_tensor(out=y3, in0=y3, in1=s8[:, 1, :].unsqueeze(2).broadcast_to([128, 8, 64]),
                                                    op=ALU.mult), ["yf", "s8"], ["yf"])
                ew("dve", lambda e: e.tensor_tensor(out=yf[:, 0:512], in0=yf[:, 0:512], in1=bcp[:, 0, :], op=ALU.mult),
                   ["yf", "bcp"], ["yf"])
                ew("pool", lambda e: e.tensor_tensor(out=yf[:, 0:512], in0=yf[:, 0:512], in1=bcp[:, 1, :], op=ALU.add),
                   ["yf", "bcp"], ["yf"])
                ew("pool", lambda e: e.tensor_tensor(out=yf[:, 0:512], in0=yf[:, 0:512], in1=yf[:, 1024:1536], op=ALU.add),
                   ["yf"], ["yf"])
                cg = colbase + i * 128
                kb.op("pe", lambda e, cg=cg: e.matmul(ps[4][:, :], lhsT=F[11][:, cg:cg + 128], rhs=g2[:, :], start=True, stop=True),
                      reads=[FK[11], "g2"], writes=["ps4"])
                ew("dve", lambda e: e.tensor_tensor(out=o[:, 0:512], in0=yf[:, 0:512], in1=ps[4][:, :], op=ALU.mult),
                   ["yf", "ps4"], ["o"])
                ew("pool", lambda e: e.tensor_tensor(out=t1[:], in0=yf[:, 512:1024], in1=yf[:, 512:1024], op=ALU.mult),
                   ["yf"], ["t1"])
                ew("dve", lambda e: e.reduce_sum(out=s8[:, 2, 0:4], in_=t1[:].rearrange("p (h j) -> p h j", h=4), axis=AX.X),
                   ["t1"], ["s8"])
                ew("dve", lambda e: e.tensor_scalar(out=s8[:, 2, 0:4], in0=s8[:, 2, 0:4], scalar1=1.0 / 128, scalar2=EPS,
                                                    op0=ALU.mult, op1=ALU.add), ["s8"], ["s8"])
                ew("act", lambda e: e.activation(out=s8[:, 2, 0:4], in_=s8[:, 2, 0:4], func=AF.Sqrt), ["s8"], ["s8"])
                ew("dve", lambda e: e.reciprocal(out=s8[:, 2, 0:4], in_=s8[:, 2, 0:4]), ["s8"], ["s8"])
                ew("dve", lambda e: e.tensor_tensor(
                    out=t1[:].rearrange("p (h j) -> p h j", h=4), in0=yf[:, 512:1024].rearrange("p (h j) -> p h j", h=4),
                    in1=s8[:, 2, 0:4].unsqueeze(2).broadcast_to([128, 4, 128]), op=ALU.mult), ["yf", "s8"], ["t1"])
                ew("pool", lambda e: e.tensor_tensor(out=t1[:], in0=t1[:], in1=bcp[:, 2, :], op=ALU.mult), ["t1", "bcp"], ["t1"])
                ew("dve", lambda e: e.tensor_tensor(out=o[:, 512:1024], in0=t1[:], in1=zt[:], op=ALU.mult), ["t1", "zt"], ["o"])
                ew("act", lambda e: e.copy(out=hb[:], in_=o[:]), ["o"], ["hb"])
                pst = ps[0][:].bitcast(BF16)
                for kc in range(8):
                    kb.op("pe", lambda e, kc=kc, pst=pst: e.transpose(out=pst[:, kc * 128:(kc + 1) * 128],
                                                                    in_=hb[:, kc * 128:(kc + 1) * 128], identity=c.identb[:]),
                          reads=["hb", "c_identb"], writes=["ps0"])
                ew("act", lambda e, pst=pst: e.copy(out=oT[:], in_=pst[:].rearrange("p (k t) -> p k t", k=8)), ["ps0"], ["oT"])
                for half in range(2):
                    pp = ps[5 + half]
                    for kc in range(8):
                        kb.op("pe", lambda e, kc=kc, half=half, pp=pp: e.matmul(
                            pp[:], lhsT=oT[:, kc, :], rhs=wo[:, kc, half * 512:(half + 1) * 512], start=(kc == 0), stop=(kc == 7)),
                            reads=["oT", "wo"], writes=[f"ps{5 + half}"])
                    sl = slice(half * 512, (half + 1) * 512)
                    ew("dve", lambda e, pp=pp, sl=sl: e.tensor_tensor(out=t1[:], in0=pp[:], in1=Gbc[:, sl], op=ALU.mult),
                       [f"ps{5 + half}", "Gbc"], ["t1"])
                    ew("pool", lambda e, sl=sl, b=b: e.tensor_tensor(out=xb[b][:, sl], in0=xb[b][:, sl], in1=t1[:], op=ALU.add),
                       ["t1", f"xb{b}"], [f"xb{b}"])
                kb.dma("sp", dst[i * 128:(i + 1) * 128, :], xb[b][:], reads=[f"xb{b}"], writes=[("dram", dst.tensor.name, i)])
        kb.barrier()


_CACHE = {}


def kernel(**inputs):
    inp = {k: np.asarray(v) for k, v in inputs.items()}
    if "nc" not in _CACHE:
        _CACHE["nc"] = build(stages=("all",))[0]
    nc = _CACHE["nc"]
    in_maps = [host_inputs(inp, b) for b in range(8)]
    res = run_bass_kernel_spmd(nc, in_maps, core_ids=list(range(8)))
    return np.stack([np.asarray(r["out"], dtype=np.float32) for r in res.results], axis=0)
```

```python
import numpy as np
import concourse.bass as bass
import concourse.mybir as mybir
from concourse.bass_utils import run_bass_kernel_spmd

F32 = mybir.dt.float32
BF16 = mybir.dt.bfloat16
I32 = mybir.dt.int32
U32 = mybir.dt.uint32
ALU = mybir.AluOpType
AF = mybir.ActivationFunctionType
AX = mybir.AxisListType

SEM_ROTATE = 20000
N_DMA_SEMS = 28
N_HW_SEMS = 18


class KB:
    def __init__(self, nc, same_engine_sync=True):
        self.nc = nc
        self.engs = {"pe": nc.tensor, "act": nc.scalar, "dve": nc.vector, "pool": nc.gpsimd, "sp": nc.sync}
        self.same_engine_sync = same_engine_sync
        self.esem = {}
        self.ecnt = {}
        self.sem_id = 0
        for e in ("pe", "act", "dve", "pool"):
            self._new_esem(e)
        self.dsems = [self._alloc_sem(f"dma{i}") for i in range(N_DMA_SEMS)]
        self.dcnt = [0] * N_DMA_SEMS
        self.dnext = 0
        self.dnext_sw = 0
        self.known = {e: {} for e in self.engs}
        self.state = {}
        self.n_ins = 0
        self._uid = 0
        self.out_tokens = []

    def _alloc_sem(self, name):
        self.sem_id += 1
        return self.nc.alloc_semaphore(f"{name}_{self.sem_id}")

    def _new_esem(self, e):
        self.esem[e] = self._alloc_sem(f"s_{e}")
        self.ecnt[e] = 0

    def uid(self, p="t"):
        self._uid += 1
        return f"{p}{self._uid}"

    def _deps(self, reads, writes):
        deps = []
        for r in reads:
            st = self.state.get(r)
            if st and st[0] is not None:
                deps.append(st[0])
        for w in writes:
            st = self.state.get(w)
            if st:
                if st[0] is not None:
                    deps.append(st[0])
                deps.extend(st[1].values())
        return deps

    def _wait(self, e, deps):
        eng = self.engs[e]
        kn = self.known[e]
        best = {}
        for (sem, val, src) in deps:
            if src == e and not (self.same_engine_sync and e != "pe"):
                continue
            key = id(sem)
            if kn.get(key, 0) >= val:
                continue
            if key not in best or best[key][1] < val:
                best[key] = (sem, val)
        for key, (sem, val) in best.items():
            eng.wait_ge(sem, val)
            kn[key] = val
            self.n_ins += 1

    def _commit(self, token, reads, writes):
        for w in writes:
            self.state[w] = [token, {}]
        for r in reads:
            st = self.state.get(r)
            if st is None:
                st = [None, {}]
                self.state[r] = st
            st[1][id(token[0])] = token

    def op(self, e, fn, reads=(), writes=()):
        reads = list(reads)
        writes = list(writes)
        writes += [r for r in reads if isinstance(r, str) and r.startswith("ps")]
        self._wait(e, self._deps(reads, writes))
        if self.ecnt[e] >= SEM_ROTATE:
            self._new_esem(e)
        ins = fn(self.engs[e])
        self.ecnt[e] += 1
        ins.then_inc(self.esem[e], 1)
        token = (self.esem[e], self.ecnt[e], e)
        self._commit(token, reads, writes)
        self.n_ins += 1
        return token

    def dma(self, q, out, in_, reads=(), writes=(), **kw):
        reads = list(reads)
        writes = list(writes)
        if q == "pool":
            i = N_HW_SEMS + self.dnext_sw
            self.dnext_sw = (self.dnext_sw + 1) % (N_DMA_SEMS - N_HW_SEMS)
        else:
            i = self.dnext
            self.dnext = (self.dnext + 1) % N_HW_SEMS
        deps = self._deps(reads, writes)
        if self.dcnt[i] > 0:
            deps.append((self.dsems[i], self.dcnt[i], "dma"))
        self._wait(q, deps)
        ins = self.engs[q].dma_start(out=out, in_=in_, **kw)
        self.dcnt[i] += 16
        ins.then_inc(self.dsems[i], 16)
        token = (self.dsems[i], self.dcnt[i], "dma")
        self._commit(token, reads, writes)
        self.n_ins += 1
        return token

    def finish(self, out_keys):
        deps = []
        for k in out_keys:
            st = self.state.get(k)
            if st and st[0] is not None:
                deps.append(st[0])
        self._wait("sp", deps)
        deps = [(self.dsems[i], self.dcnt[i], "dma") for i in range(N_DMA_SEMS) if self.dcnt[i] > 0]
        self._wait("sp", deps)

    def barrier(self):
        deps = [(self.esem[e], self.ecnt[e], "x") for e in self.esem if self.ecnt[e] > 0]
        deps += [(self.dsems[i], self.dcnt[i], "dma") for i in range(N_DMA_SEMS) if self.dcnt[i] > 0]
        for e in self.engs:
            self._wait(e, deps)
        self.state = {}


D = 1024
KC = 8
EPS = 1e-6
TP = 2310
CTX0, LAT0 = 2, 260
CHUNK = 128
CHUNK_COLS = [CTX0 + CHUNK * j for j in range(256 // CHUNK)] + [LAT0 + CHUNK * j for j in range(2048 // CHUNK)]
BLOCKS = [(0, 256, CTX0)] + [(256 + 512 * j, 512, LAT0 + 512 * j) for j in range(4)]
DECAY_K = float(np.exp(-0.5))


class Ctx:
    pass


def load_consts(kb, nc, es, g):
    c = Ctx()
    c.ident = es.enter_context(nc.sbuf_tensor("c_ident", [128, 128], F32))
    c.identb = es.enter_context(nc.sbuf_tensor("c_identb", [128, 128], BF16))
    c.ones = es.enter_context(nc.sbuf_tensor("c_ones", [128, 128], F32))
    c.iota = es.enter_context(nc.sbuf_tensor("c_iota", [128, 256], F32))
    kb.dma("sp", c.ident[:], g["k_ident"][:, :], writes=["c_ident"])
    kb.dma("sp", c.iota[:], g["k_iota"][:, :], writes=["c_iota"])
    kb.op("dve", lambda e: e.memset(c.ones[:], 1.0), writes=["c_ones"])
    c.eps6 = es.enter_context(nc.sbuf_tensor("c_eps6", [128, 1], F32))
    c.one1 = es.enter_context(nc.sbuf_tensor("c_one1", [128, 1], F32))
    kb.op("dve", lambda e: e.memset(c.eps6[:], 1e-6), writes=["c_eps"])
    kb.op("dve", lambda e: e.memset(c.one1[:], 1.0), writes=["c_eps"])
    c.onesb = es.enter_context(nc.sbuf_tensor("c_onesb", [128, 8], BF16))
    kb.op("dve", lambda e: e.memset(c.onesb[:], 1.0), writes=["c_ones"])
    kb.op("dve", lambda e: e.tensor_copy(out=c.identb[:], in_=c.ident[:]), reads=["c_ident"], writes=["c_identb"])
    return c


def prologue(kb, nc, es, g, c):
    mods = []
    for l in range(2):
        mods.append(es.enter_context(nc.sbuf_tensor(f"mod{l}", [128, 48, 2], F32)))
    with nc.sbuf_tensor("pl_sc", [128, 2, 8], F32) as sc, \
            nc.sbuf_tensor("pl_w0", [128, 8, 512], F32) as w0, \
            nc.sbuf_tensor("pl_w1", [128, 8, 512], F32) as w1, \
            nc.sbuf_tensor("pl_b", [128, 2, 48], F32) as adab, \
            nc.sbuf_tensor("pl_n", [128, 2, 2, 8], F32) as nrm, \
            nc.psum_tensor("pl_ps", [128, 512], F32) as ps:
        wb = [w0, w1]
        kb.dma("sp", sc[:, 0, :], g["cT"][:, :], writes=["sc"])
        kb.dma("sp", sc[:, 1, :], g["ccT"][:, :], writes=["sc"])
        kb.dma("sp", adab[:], g["ada_bT"][:, :, :], writes=["adab"])
        kb.dma("sp", nrm[:], g["normT"][:, :, :, :], writes=["nrm"])
        kb.op("act", lambda e: e.activation(out=sc[:], in_=sc[:], func=AF.Silu), reads=["sc"], writes=["sc"])
        blk = 0
        for l in range(2):
            wv = g["ada_w"][l].rearrange("(kc p) n -> p kc n", p=128)
            for nb in range(12):
                wt = wb[blk % 2]
                wk = f"plw{blk % 2}"
                kb.dma("sp" if blk % 2 == 0 else "act", wt[:], wv[:, :, nb * 512:(nb + 1) * 512], writes=[wk])
                for j in range(4):
                    for kc in range(8):
                        kb.op("pe", lambda e, kc=kc, j=j, wt=wt: e.matmul(
                            ps[:, (j * 2):(j * 2 + 2)], lhsT=wt[:, kc, j * 128:(j + 1) * 128], rhs=sc[:, :, kc],
                            start=(kc == 0), stop=(kc == 7)), reads=[wk, "sc"], writes=["psPL"])
                kb.op("dve", lambda e, l=l, nb=nb: e.tensor_tensor(
                    out=mods[l][:, nb * 4:(nb + 1) * 4, :],
                    in0=ps[:, 0:8].rearrange("p (j s) -> p j s", s=2),
                    in1=adab[:, l, nb * 4:(nb + 1) * 4].unsqueeze(2).broadcast_to([128, 4, 2]),
                    op=ALU.add), reads=["psPL", "adab"], writes=[f"mod{l}"])
                blk += 1
            for (m, which) in ((1, 0), (4, 1)):
                for s in range(2):
                    kb.op("dve", lambda e, l=l, m=m, which=which, s=s: e.scalar_tensor_tensor(
                        out=mods[l][:, m * 8:(m + 1) * 8, s], in0=mods[l][:, m * 8:(m + 1) * 8, s], scalar=1.0,
                        in1=nrm[:, l, which, :], op0=ALU.add, op1=ALU.mult),
                        reads=[f"mod{l}", "nrm"], writes=[f"mod{l}"])
    kb.barrier()
    return mods


def make_bc(kb, nc, c, col_ap_fn, out_tile, ps, key, src_keys):
    with nc.sbuf_tensor(kb.uid("bcd"), [128, 128], F32) as dg:
        dk = kb.uid("dg")
        for half in range(2):
            for q in range(4):
                kc = half * 4 + q
                kb.op("dve", lambda e, kc=kc: e.tensor_scalar(
                    out=dg[:], in0=c.ident[:], scalar1=col_ap_fn(kc), scalar2=None, op0=ALU.mult),
                    reads=["c_ident"] + src_keys, writes=[dk])
                kb.op("pe", lambda e, q=q: e.matmul(ps[:, q * 128:(q + 1) * 128], lhsT=c.ones[:], rhs=dg[:],
                                                   start=True, stop=True),
                      reads=[dk, "c_ones"], writes=["psBC" + key])
            kb.op("act", lambda e, half=half: e.copy(out=out_tile[:, half * 512:(half + 1) * 512], in_=ps[:]),
                  reads=["psBC" + key], writes=[key])
        kb.barrier()


def norm_tile_g(kb, nc, xt, xk, st, G, S, hout, hk, eps=EPS):
    sk = kb.uid("st")
    kb.op("act", lambda e: e.activation(out=hout, in_=xt, func=AF.Square, accum_out=st[:, 0:1]),
          reads=[xk], writes=[hk, sk])
    yield
    kb.op("dve", lambda e: e.tensor_scalar(out=st[:, 1:2], in0=st[:, 0:1], scalar1=1.0 / D, scalar2=eps,
                                           op0=ALU.mult, op1=ALU.add), reads=[sk], writes=[sk])
    yield
    kb.op("act", lambda e: e.activation(out=st[:, 2:3], in_=st[:, 1:2], func=AF.Sqrt), reads=[sk], writes=[sk])
    yield
    kb.op("dve", lambda e: e.reciprocal(out=st[:, 3:4], in_=st[:, 2:3]), reads=[sk], writes=[sk])
    yield
    if G is None:
        kb.op("dve", lambda e: e.tensor_scalar(out=hout, in0=xt, scalar1=st[:, 3:4], scalar2=None, op0=ALU.mult),
              reads=[xk, sk], writes=[hk])
        return
    kb.op("dve", lambda e: e.scalar_tensor_tensor(out=hout, in0=xt, scalar=st[:, 3:4], in1=G[:],
                                                  op0=ALU.mult, op1=ALU.mult),
          reads=[xk, sk, "Gbc"], writes=[hk])
    yield
    if S is not None:
        kb.op("pool", lambda e: e.tensor_tensor(out=hout, in0=hout, in1=S[:], op=ALU.add),
              reads=[hk, "Sbc"], writes=[hk])


def norm_tile(*a, **k):
    for _ in norm_tile_g(*a, **k):
        pass


def moe_stage(kb, nc, g, c, mods, layer, stream, x_in, x_out, T, final_norm=False, comp=None):
    NT = T // 128
    cap = 2 * T // 16
    CW = cap
    CT = (cap + 127) // 128
    cs = min(cap, 128)
    mod = mods[layer]
    xin_v = x_in.rearrange("(n p) d -> n p d", p=128)
    xout_v = x_out.rearrange("(n p) d -> n p d", p=128)
    sx = f"L{layer}s{stream}"
    from contextlib import ExitStack
    with ExitStack() as es:
        def sb(name, shape, dt):
            return es.enter_context(nc.sbuf_tensor(f"moe_{name}_{sx}", shape, dt))
        Gbc = sb("G", [128, D], F32)
        Sbc = sb("S", [128, D], F32)
        gate2 = Sbc
        hbf = sb("hbf", [128, NT, D], BF16)
        xb = [sb("x0", [128, D], F32), sb("x1", [128, D], F32)]
        h32 = [sb("h0", [128, D], F32), sb("h1", [128, D], F32)]
        hT = [sb("hT0", [128, 8, 128], F32), sb("hT1", [128, 8, 128], F32)]
        stt = [sb("st0", [128, 4], F32), sb("st1", [128, 4], F32)]
        rt = sb("rt", [128, 8, 16], F32)
        aff = sb("aff", [128, NT, 16], F32)
        sm = sb("sm", [128, 4], F32)
        ex = sb("ex", [128, 16], F32)
        affT = sb("affT", [16, T], F32)
        work = sb("work", [16, T], F32)
        mx8 = sb("mx8", [16, 8], F32)
        maskT = sb("maskT", [16, T], F32)
        onesT = work
        slotT = sb("slotT", [16, T], F32)
        gateT = affT
        slot = sb("slot", [128, NT, 16], F32)
        gate = sb("gate", [128, NT, 16], F32)
        selT = [sb("selT0", [128, CW], BF16), sb("selT1", [128, CW], BF16)]
        xeT = sb("xeT", [128, 8, CW], BF16)
        big = sb("big", [128, 4 * 8 * D], BF16)
        wviews = [big[:, j * 8 * D:(j + 1) * 8 * D].rearrange("p (k n) -> p k n", n=D) for j in range(4)]
        w1b = [wviews[0], wviews[1]]
        w3b = [wviews[2]]
        w2b = [wviews[3]]
        yest = [sb("yest0", [128, CT, D], BF16), sb("yest1", [128, CT, D], BF16)]
        sil = [sb("sil0", [128, CW], F32), sb("sil1", [128, CW], F32)]
        hidT = sb("hidT", [128, 8, CW], BF16)
        yeall = big[:, 0:16 * CT * D].rearrange("p (e c n) -> p e c n", e=16, c=CT)
        selGa = [sb("selGa0", [128, 4, CW], BF16), sb("selGa1", [128, 4, CW], BF16)]
        selGca = [sb("selGca0", [128, 4 * CT, 128], BF16), sb("selGca1", [128, 4 * CT, 128], BF16)]
        tmpo = [sb("tmpo0", [128, 512], F32), sb("tmpo1", [128, 512], F32)]
        ps = [es.enter_context(nc.psum_tensor(f"moe_ps{i}_{sx}", [128, 512], F32)) for i in range(8)]

        class SC:
            pass
        S0 = SC()
        S0.T, S0.NT, S0.cap, S0.CW, S0.CT, S0.cs, S0.stream = T, NT, cap, CW, CT, cs, stream
        S0.xin_v, S0.xout_v, S0.hbf, S0.aff, S0.slot, S0.gate, S0.ye_scr = xin_v, xout_v, hbf, aff, slot, gate, g["ye_scr"]
        streams = [S0]
        if comp is not None:
            S1 = SC()
            S1.T, S1.NT, S1.cap, S1.CW, S1.CT, S1.cs, S1.stream = 256, 2, 32, 32, 1, 32, 1
            S1.xin_v = comp[0].rearrange("(n p) d -> n p d", p=128)
            S1.xout_v = comp[1].rearrange("(n p) d -> n p d", p=128)
            S1.hbf = sb("hbfc", [128, 2, D], BF16)
            S1.aff = sb("affc", [128, 2, 16], F32)
            S1.slot = sb("slotc", [128, 2, 16], F32)
            S1.gate = sb("gatec", [128, 2, 16], F32)
            S1.ye_scr = g["ye_scr_c"]
            selTc = [sb("selTc0", [128, 32], BF16), sb("selTc1", [128, 32], BF16)]
            xeTc = sb("xeTc", [128, 8, 32], BF16)
            silc = sb("silc", [128, 8, 32], F32)
            hidTc = sb("hidTc", [128, 8, 32], BF16)
            yestc = [sb("yestc0", [32, 1, D], BF16)] * 2
            streams = [S1, S0]
        affT_full, work_full, maskT_full, slotT_full = affT, work, maskT, slotT
        kb.dma("sp", rt[:], g["moe_router"][layer].rearrange("(kc p) e -> p kc e", p=128), writes=["rt"])
        for S in streams:
            T, NT, cap, CW, CT, cs, stream = S.T, S.NT, S.cap, S.CW, S.CT, S.cs, S.stream
            xin_v, xout_v, hbf, aff, slot, gate = S.xin_v, S.xout_v, S.hbf, S.aff, S.slot, S.gate
            affT, work, maskT, slotT = affT_full[:, 0:T], work_full[:, 0:T], maskT_full[:, 0:T], slotT_full[:, 0:T]
            onesT, gateT = work, affT
            kb.barrier()
            make_bc(kb, nc, c, lambda kc: mod[:, 4 * 8 + kc, stream:stream + 1], Gbc, ps[0], "Gbc", [f"mod{layer}"])
            make_bc(kb, nc, c, lambda kc: mod[:, 3 * 8 + kc, stream:stream + 1], Sbc, ps[0], "Sbc", [f"mod{layer}"])

            def stageA(i):
                b = i % 2
                kb.dma("sp", xb[b][:], xin_v[i], writes=[f"xb{b}"])
                yield
                yield from norm_tile_g(kb, nc, xb[b][:], f"xb{b}", stt[b], Gbc, Sbc, h32[b][:], f"h32{b}")
                yield
                kb.op("act", lambda e, i=i, b=b: e.copy(out=hbf[:, i, :], in_=h32[b][:]), reads=[f"h32{b}"],
                      writes=[f"hbf{stream}_{i}"])
                for half in range(2):
                    for q in range(4):
                        kc = half * 4 + q
                        kb.op("pe", lambda e, kc=kc, q=q, b=b, half=half: e.transpose(
                            out=ps[half][:, q * 128:(q + 1) * 128], in_=h32[b][:, kc * 128:(kc + 1) * 128],
                            identity=c.ident[:]), reads=[f"h32{b}", "c_ident"], writes=[f"ps{half}"])
                    yield
                    kb.op("dve" if half == 0 else "act", (lambda e, half=half, b=b: e.tensor_copy(
                        out=hT[b][:, half * 4:(half + 1) * 4, :], in_=ps[half][:].rearrange("p (q t) -> p q t", q=4)))
                        if half == 0 else (lambda e, half=half, b=b: e.copy(
                            out=hT[b][:, half * 4:(half + 1) * 4, :], in_=ps[half][:].rearrange("p (q t) -> p q t", q=4))),
                        reads=[f"ps{half}"], writes=[f"hT{b}"])
                    yield

            def stageB(i):
                b = i % 2
                for kc in range(8):
                    kb.op("pe", lambda e, kc=kc, b=b: e.matmul(ps[2][:, 0:16], lhsT=hT[b][:, kc, :], rhs=rt[:, kc, :],
                                                             start=(kc == 0), stop=(kc == 7)),
                          reads=[f"hT{b}", "rt"], writes=["ps2"])
                yield
                kb.op("dve", lambda e: e.reduce_max(out=sm[:, 0:1], in_=ps[2][:, 0:16], axis=AX.X),
                      reads=["ps2"], writes=["sm"])
                kb.op("dve", lambda e: e.tensor_scalar(out=sm[:, 1:2], in0=sm[:, 0:1], scalar1=-1.0, scalar2=None,
                                                       op0=ALU.mult), reads=["sm"], writes=["sm"])
                yield
                kb.op("act", lambda e: e.activation(out=ex[:], in_=ps[2][:, 0:16], func=AF.Exp, bias=sm[:, 1:2],
                                                    accum_out=sm[:, 2:3]), reads=["ps2", "sm"], writes=["ex", "sm"])
                yield
                kb.op("dve", lambda e: e.reciprocal(out=sm[:, 3:4], in_=sm[:, 2:3]), reads=["sm"], writes=["sm"])
                kb.op("dve", lambda e, i=i: e.tensor_scalar(out=aff[:, i, :], in0=ex[:], scalar1=sm[:, 3:4], scalar2=None,
                                                            op0=ALU.mult), reads=["ex", "sm"], writes=["aff"])
                yield
                kb.op("pe", lambda e, i=i: e.transpose(out=ps[3][0:16, 0:128], in_=aff[:, i, :], identity=c.ident[:]),
                      reads=["aff", "c_ident"], writes=["ps3"])
                yield
                kb.op("act", lambda e, i=i: e.copy(out=affT[:, i * 128:(i + 1) * 128], in_=ps[3][0:16, 0:128]),
                      reads=["ps3"], writes=["affT"])
                yield

            from itertools import zip_longest
            prevB = iter(())
            for i in range(NT):
                for _ in zip_longest(stageA(i), prevB):
                    pass
                prevB = stageB(i)
            for _ in prevB:
                pass

            import os
            PH = int(os.environ.get("MOE_PH", "9"))
            if PH < 2:
                kb.dma("sp", x_out[0:128, 0:NT * 16], aff[:].rearrange("p n e -> p (n e)"), reads=["aff"], writes=["o"])
                kb.barrier()
                return
            kb.op("dve", lambda e: e.tensor_copy(out=work[:], in_=affT[:]), reads=["affT"], writes=["work"])
            nr = cap // 8
            for r in range(nr):
                kb.op("dve", lambda e: e.max(out=mx8[:], in_=work[:]), reads=["work"], writes=["mx8"])
                if r < nr - 1:
                    kb.op("dve", lambda e: e.match_replace(out=work[:], in_to_replace=mx8[:], in_values=work[:],
                                                           imm_value=-1.0), reads=["work", "mx8"], writes=["work"])
            kb.op("dve", lambda e: e.tensor_scalar(out=maskT[:], in0=affT[:], scalar1=mx8[:, 7:8], scalar2=None,
                                                   op0=ALU.is_ge), reads=["affT", "mx8"], writes=["maskT"])
            kb.op("pool", lambda e: e.memset(onesT[:], 1.0), reads=[], writes=["work"])
            kb.op("dve", lambda e: e.tensor_tensor_scan(out=slotT[:], data0=onesT[:], data1=maskT[:], initial=0.0,
                                                        op0=ALU.mult, op1=ALU.add),
                  reads=["work", "maskT"], writes=["slotT"])
            kb.op("dve", lambda e: e.tensor_tensor(out=slotT[:], in0=slotT[:], in1=maskT[:], op=ALU.mult),
                  reads=["slotT", "maskT"], writes=["slotT"])
            kb.op("dve", lambda e: e.tensor_scalar(out=slotT[:], in0=slotT[:], scalar1=-1.0, scalar2=None, op0=ALU.add),
                  reads=["slotT"], writes=["slotT"])
            kb.op("pool", lambda e: e.tensor_tensor(out=gateT[:], in0=affT[:], in1=maskT[:], op=ALU.mult),
                  reads=["affT", "maskT"], writes=["affT"])
            for i in range(NT):
                kb.op("pe", lambda e, i=i: e.transpose(out=ps[0][:, i * 16:(i + 1) * 16],
                                                       in_=slotT[:, i * 128:(i + 1) * 128], identity=c.ident[0:16, 0:16]),
                      reads=["slotT", "c_ident"], writes=["ps0"])
                kb.op("pe", lambda e, i=i: e.transpose(out=ps[1][:, i * 16:(i + 1) * 16],
                                                       in_=gateT[:, i * 128:(i + 1) * 128], identity=c.ident[0:16, 0:16]),
                      reads=["affT", "c_ident"], writes=["ps1"])
            kb.op("dve", lambda e: e.tensor_copy(out=slot[:], in_=ps[0][:, 0:NT * 16].rearrange("p (n e) -> p n e", e=16)),
                  reads=["ps0"], writes=["slot"])
            kb.op("act", lambda e: e.copy(out=gate[:], in_=ps[1][:, 0:NT * 16].rearrange("p (n e) -> p n e", e=16)),
                  reads=["ps1"], writes=["gate"])

            if PH < 3:
                kb.dma("sp", x_out[0:128, 0:NT * 16], slot[:].rearrange("p n e -> p (n e)"), reads=["slot"], writes=["o"])
                kb.dma("sp", x_out[128:256, 0:NT * 16], gate[:].rearrange("p n e -> p (n e)"), reads=["gate"], writes=["o2"])
                kb.barrier()
                return

        stg = [xb[0], xb[1], h32[0], h32[1]]
        stgk = ["xb0", "xb1", "h320", "h321"]
        wcnt = [0]

        def load_w(wv, wt, wk):
            for hh in range(2):
                kb.dma("pool", wt[:, hh * 4:(hh + 1) * 4, :], wv[:, hh * 4:(hh + 1) * 4, :], writes=[wk])

        def wviews_of(e_):
            return (g["moe_w1"][layer, e_].rearrange("(kc p) n -> p kc n", p=128),
                    g["moe_w3"][layer, e_].rearrange("(kc p) n -> p kc n", p=128),
                    g["moe_w2"][layer, e_].rearrange("(kc p) n -> p kc n", p=128))
        nsel = 0
        SUB = int(os.environ.get("MOE_SUB", "9"))
        NEXP = int(os.environ.get("MOE_NEXP", "16"))
        wv1, wv3, wv2 = wviews_of(0)
        load_w(wv1, w1b[0], "w1_0")
        load_w(wv3, w3b[0], "w3")
        load_w(wv2, w2b[0], "w2")
        for ex_i in range(NEXP):
            w1t = w1b[ex_i % 2]
            w1k = f"w1_{ex_i % 2}"
            if ex_i + 1 < NEXP:
                nwv1, nwv3, nwv2 = wviews_of(ex_i + 1)
                load_w(nwv1, w1b[(ex_i + 1) % 2], f"w1_{(ex_i + 1) % 2}")
            for i in range(NT):
                b = nsel % 2
                nsel += 1
                kb.op("dve", lambda e, i=i, b=b, ex_i=ex_i: e.tensor_scalar(
                    out=selT[b][:], in0=c.iota[:, 0:CW], scalar1=slot[:, i, ex_i:ex_i + 1], scalar2=None,
                    op0=ALU.is_equal), reads=["c_iota", "slot"], writes=[f"selT{b}"])
                for kc in range(8):
                    bank = (kc * CW) // 512
                    off = (kc * CW) % 512
                    kb.op("pe", lambda e, i=i, b=b, kc=kc, bank=bank, off=off: e.matmul(
                        ps[bank][:, off:off + CW], lhsT=hbf[:, i, kc * 128:(kc + 1) * 128], rhs=selT[b][:],
                        start=(i == 0 and off == 0), stop=(i == NT - 1), skip_group_check=True), reads=[f"hbf{stream}_{i}", f"selT{b}"], writes=[f"ps{bank}"])
            nb = (8 * CW + 511) // 512
            per = 512 // CW if CW < 512 else 1
            for bank in range(nb):
                k0 = bank * per
                k1 = min(8, k0 + per)
                kb.op("act" if bank % 2 else "dve", (lambda e, bank=bank, k0=k0, k1=k1: e.copy(
                    out=xeT[:, k0:k1, :], in_=ps[bank][:, 0:(k1 - k0) * CW].rearrange("p (k c) -> p k c", c=CW)))
                    if bank % 2 else (lambda e, bank=bank, k0=k0, k1=k1: e.tensor_copy(
                        out=xeT[:, k0:k1, :], in_=ps[bank][:, 0:(k1 - k0) * CW].rearrange("p (k c) -> p k c", c=CW))),
                    reads=[f"ps{bank}"], writes=["xeT"])
            if SUB < 2:
                continue
            for fc in range(8):
                pb = ps[4 + fc % 2]
                pk = f"ps{4 + fc % 2}"
                for kc in range(8):
                    kb.op("pe", lambda e, fc=fc, kc=kc, pb=pb, w1t=w1t: e.matmul(
                        pb[:, 0:CW], lhsT=w1t[:, kc, fc * 128:(fc + 1) * 128], rhs=xeT[:, kc, :],
                        start=(kc == 0), stop=(kc == 7)), reads=[w1k, "xeT"], writes=[pk])
                for kc in range(8):
                    kb.op("pe", lambda e, fc=fc, kc=kc, pb=pb: e.matmul(
                        pb[:, 256:256 + CW], lhsT=w3b[0][:, kc, fc * 128:(fc + 1) * 128], rhs=xeT[:, kc, :],
                        start=(kc == 0), stop=(kc == 7)), reads=["w3", "xeT"], writes=[pk])
                sb_ = sil[fc % 2]
                DBG = int(os.environ.get("MOE_DBG", "9"))
                if DBG < 1:
                    continue
                kb.op("act", lambda e, pb=pb, sb_=sb_: e.activation(out=sb_[:], in_=pb[:, 0:CW], func=AF.Silu),
                      reads=[pk], writes=[f"sil{fc % 2}"])
                if DBG < 2:
                    continue
                kb.op("dve", lambda e, pb=pb, sb_=sb_, fc=fc: e.tensor_tensor(
                    out=hidT[:, fc, :], in0=sb_[:], in1=pb[:, 256:256 + CW], op=ALU.mult),
                    reads=[f"sil{fc % 2}", pk], writes=["hidT"])
            if comp is not None:
                for i in range(2):
                    sc_ = selTc[i]
                    kb.op("dve", lambda e, i=i, sc_=sc_, ex_i=ex_i: e.tensor_scalar(
                        out=sc_[:], in0=c.iota[:, 0:32], scalar1=S1.slot[:, i, ex_i:ex_i + 1], scalar2=None,
                        op0=ALU.is_equal), reads=["c_iota", "slotc"], writes=[f"selTc{i}"])
                    for kc in range(8):
                        kb.op("pe", lambda e, i=i, kc=kc, sc_=sc_: e.matmul(
                            ps[6][:, kc * 32:(kc + 1) * 32], lhsT=S1.hbf[:, i, kc * 128:(kc + 1) * 128], rhs=sc_[:],
                            start=(i == 0 and kc == 0), stop=(i == 1), skip_group_check=True),
                            reads=[f"hbf1_{i}", f"selTc{i}"], writes=["ps6"])
                kb.op("act", lambda e: e.copy(out=xeTc[:], in_=ps[6][:, 0:256].rearrange("p (k c) -> p k c", c=32)),
                      reads=["ps6"], writes=["xeTc"])
                for fc in range(8):
                    for kc in range(8):
                        kb.op("pe", lambda e, fc=fc, kc=kc: e.matmul(
                            ps[7][:, fc * 64:fc * 64 + 32], lhsT=w1t[:, kc, fc * 128:(fc + 1) * 128], rhs=xeTc[:, kc, :],
                            start=(fc == 0 and kc == 0), stop=(kc == 7), skip_group_check=True),
                            reads=[w1k, "xeTc"], writes=["ps7"])
                    for kc in range(8):
                        kb.op("pe", lambda e, fc=fc, kc=kc: e.matmul(
                            ps[7][:, fc * 64 + 32:fc * 64 + 64], lhsT=w3b[0][:, kc, fc * 128:(fc + 1) * 128],
                            rhs=xeTc[:, kc, :], start=False, stop=(kc == 7), skip_group_check=True),
                            reads=["w3", "xeTc"], writes=["ps7"])
                p7v = ps[7][:, :].rearrange("p (f ab c) -> p f ab c", f=8, ab=2)
                kb.op("act", lambda e: e.activation(out=silc[:], in_=p7v[:, :, 0, :], func=AF.Silu),
                      reads=["ps7"], writes=["silc"])
                kb.op("dve", lambda e: e.tensor_tensor(out=hidTc[:], in0=silc[:], in1=p7v[:, :, 1, :], op=ALU.mult),
                      reads=["silc", "ps7"], writes=["hidTc"])
            if ex_i + 1 < NEXP:
                load_w(nwv3, w3b[0], "w3")
            if SUB < 3:
                continue
            for ct in range(CT):
                for half in range(2):
                    pb = ps[6 + half]
                    pk = f"ps{6 + half}"
                    for fc in range(8):
                        kb.op("pe", lambda e, ct=ct, half=half, fc=fc, pb=pb: e.matmul(
                            pb[0:cs, :], lhsT=hidT[:, fc, ct * 128:ct * 128 + cs],
                            rhs=w2b[0][:, fc, half * 512:(half + 1) * 512], start=(fc == 0), stop=(fc == 7)),
                            reads=["hidT", "w2"], writes=[pk])
                    ys = yest[ex_i % 2]
                    kb.op("act" if half else "dve", (lambda e, ct=ct, half=half, pb=pb, ys=ys: e.copy(
                        out=ys[0:cs, ct, half * 512:(half + 1) * 512], in_=pb[0:cs, :]))
                        if half else (lambda e, ct=ct, half=half, pb=pb, ys=ys: e.tensor_copy(
                            out=ys[0:cs, ct, half * 512:(half + 1) * 512], in_=pb[0:cs, :])),
                        reads=[pk], writes=[f"yest{ex_i % 2}"])
            if comp is not None:
                ysc = yestc[ex_i % 2]
                for half in range(2):
                    for fc in range(8):
                        kb.op("pe", lambda e, half=half, fc=fc: e.matmul(
                            ps[6][0:32, :], lhsT=hidTc[:, fc, :], rhs=w2b[0][:, fc, half * 512:(half + 1) * 512],
                            start=(fc == 0), stop=(fc == 7)), reads=["hidTc", "w2"], writes=["ps6"])
                    kb.op("act", lambda e, half=half, ysc=ysc: e.copy(out=ysc[0:32, 0, half * 512:(half + 1) * 512],
                                                                     in_=ps[6][0:32, :]),
                          reads=["ps6"], writes=["yestc"])
                kb.dma("act", S1.ye_scr[ex_i, 0:32, 0:1, :], ysc[0:32, :, :], reads=["yestc"],
                       writes=[("yescrc", ex_i)])
            if ex_i + 1 < NEXP:
                load_w(nwv2, w2b[0], "w2")
            kb.dma("sp", g["ye_scr"][ex_i, 0:cs, 0:CT, :], yest[ex_i % 2][0:cs, :, :], reads=[f"yest{ex_i % 2}"],
                   writes=[("yescr", ex_i)])

        if PH < 4:
            kb.barrier()
            return
        for S in streams[::-1]:
            T, NT, cap, CW, CT, cs, stream = S.T, S.NT, S.cap, S.CW, S.CT, S.cs, S.stream
            xin_v, xout_v, hbf, aff, slot, gate = S.xin_v, S.xout_v, S.hbf, S.aff, S.slot, S.gate
            yeall = big[:, 0:16 * CT * D].rearrange("p (e c n) -> p e c n", e=16, c=CT)
            ye_scr_S = S.ye_scr
            kb.barrier()
            make_bc(kb, nc, c, lambda kc: mod[:, 5 * 8 + kc, stream:stream + 1], gate2, ps[0], "gate2", [f"mod{layer}"])
            if final_norm:
                make_bc(kb, nc, c, lambda kc: c.fnT[:, kc:kc + 1], Gbc, ps[0], "Gbc", ["c_fnT"])
            for ex_i in range(16):
                kb.dma(["sp", "act"][ex_i % 2], yeall[0:cs, ex_i, :, :], ye_scr_S[ex_i, 0:cs, 0:CT, :],
                       reads=[("yescr", ex_i), ("yescrc", ex_i)], writes=[f"ye{ex_i}"])
            nsg = 0
            EG = 4
            for i in range(NT):
                b = i % 2
                kb.dma("sp", xb[b][:], xin_v[i], writes=[f"xb{b}"])
                for g0 in range(0, 16, EG):
                    sg = nsg % 2
                    nsg += 1
                    sga = selGa[sg][:, :, 0:CW]
                    kb.op("dve", lambda e, i=i, g0=g0, sga=sga: e.tensor_tensor(
                        out=sga[:, :, :], in0=c.iota[:, 0:CW].unsqueeze(1).broadcast_to([128, EG, CW]),
                        in1=slot[:, i, g0:g0 + EG].unsqueeze(2).broadcast_to([128, EG, CW]), op=ALU.is_equal),
                        reads=["c_iota", "slot"], writes=[f"selGa{sg}"])
                    kb.op("pool", lambda e, i=i, g0=g0, sga=sga: e.tensor_tensor(
                        out=sga[:, :, :], in0=sga[:, :, :],
                        in1=gate[:, i, g0:g0 + EG].unsqueeze(2).broadcast_to([128, EG, CW]), op=ALU.mult),
                        reads=[f"selGa{sg}", "gate"], writes=[f"selGa{sg}"])
                    pst = ps[2 + sg][:].bitcast(BF16)
                    for ee in range(EG):
                        for ct in range(CT):
                            kb.op("pe", lambda e, ct=ct, ee=ee, sga=sga, pst=pst: e.transpose(
                                out=pst[0:cs, (ee * CT + ct) * 128:(ee * CT + ct + 1) * 128],
                                in_=sga[:, ee, ct * 128:ct * 128 + cs], identity=c.identb[:]),
                                reads=[f"selGa{sg}", "c_identb"], writes=[f"ps{2 + sg}"])
                    kb.op("act", lambda e, sg=sg, pst=pst: e.copy(
                        out=selGca[sg][0:cs, 0:EG * CT, :], in_=pst[0:cs, 0:EG * CT * 128].rearrange("p (c t) -> p c t", t=128)),
                        reads=[f"ps{2 + sg}"], writes=[f"selGca{sg}"])
                    for ee in range(EG):
                        ex_i = g0 + ee
                        for half in range(2):
                            for ct in range(CT):
                                kb.op("pe", lambda e, half=half, ct=ct, sg=sg, ex_i=ex_i, ee=ee: e.matmul(
                                    ps[half][:, :], lhsT=selGca[sg][0:cs, ee * CT + ct, :],
                                    rhs=yeall[0:cs, ex_i, ct, half * 512:(half + 1) * 512],
                                    start=(ex_i == 0 and ct == 0), stop=(ex_i == 15 and ct == CT - 1)),
                                    reads=[f"selGca{sg}", f"ye{ex_i}"], writes=[f"ps{half}"])
                for half in range(2):
                    sl = slice(half * 512, (half + 1) * 512)
                    kb.op("dve", lambda e, half=half, sl=sl: e.tensor_tensor(
                        out=tmpo[half][:], in0=ps[half][:], in1=gate2[:, sl], op=ALU.mult),
                        reads=[f"ps{half}", "gate2"], writes=[f"tmpo{half}"])
                    kb.op("pool", lambda e, half=half, sl=sl, b=b: e.tensor_tensor(
                        out=xb[b][:, sl], in0=xb[b][:, sl], in1=tmpo[half][:], op=ALU.add),
                        reads=[f"tmpo{half}", f"xb{b}"], writes=[f"xb{b}"])
                if final_norm:
                    norm_tile(kb, nc, xb[b][:], f"xb{b}", stt[b], Gbc, None, h32[b][:], f"h32{b}")
                    kb.dma("sp", xout_v[i], h32[b][:], reads=[f"h32{b}"], writes=[("dram", x_out.tensor.name, i)])
                else:
                    kb.dma("sp", xout_v[i], xb[b][:], reads=[f"xb{b}"], writes=[("dram", x_out.tensor.name, i)])
            kb.barrier()


def host_consts():
    k = {}
    k["k_ident"] = np.eye(128, dtype=np.float32)
    k["k_iota"] = np.tile(np.arange(256, dtype=np.float32)[None, :], (128, 1))
    C, S, perm = rope_tables()
    k["k_ropeC"], k["k_ropeS"], k["k_perm"] = C, S, perm
    kk = np.arange(128)[:, None]
    qq = np.arange(128)[None, :]
    k["k_mL"] = np.tile((kk >= qq).astype(np.float32), (1, 4))
    k["k_mU"] = np.tile((kk <= qq).astype(np.float32), (1, 4))
    ss = np.arange(CHUNK)[:, None]
    tt = np.arange(CHUNK)[None, :]
    mus = (ss < tt).astype(np.float32)
    mui = (ss <= tt).astype(np.float32)
    k["k_maskA"] = np.ascontiguousarray(np.concatenate([-mus, mus, mui, mui], axis=1))
    k["k_maskT"] = np.ascontiguousarray(-(tt < ss).astype(np.float32))
    rm = np.ones((128, TP), np.float32)
    rm[:, CHUNK_COLS] = 0.0
    k["k_rmask"] = rm
    k["k_J"] = np.ascontiguousarray(np.eye(128, dtype=np.float32)[::-1])
    sel = np.zeros((16, 16, 128), np.float32)
    for r in range(16):
        sel[r, r, :] = 1.0
    k["k_sel"] = sel
    bdm = np.zeros((128, 128), np.float32)
    bdm[0:64, 0:64] = 1.0
    bdm[64:128, 64:128] = 1.0
    k["k_bd"] = bdm
    return k


def fm(v):
    v = np.asarray(v, np.float32)
    return np.ascontiguousarray(v.reshape(-1, 128).T)


def host_inputs(inp, b):
    m = dict(host_consts())
    m["x"] = np.ascontiguousarray(inp["x"][b])
    m["ctx"] = np.ascontiguousarray(inp["ctx"][b])
    m["cT"] = fm(inp["c"][b])
    m["ccT"] = fm(inp["c_ctx"])
    m["ada_w"] = inp["ada_w"]
    m["ada_bT"] = np.ascontiguousarray(np.stack([fm(inp["ada_b"][l]) for l in range(2)], axis=1))
    m["normT"] = np.ascontiguousarray(np.stack(
        [np.stack([fm(inp["norm_mix"][l]), fm(inp["norm_ffn"][l])], axis=1) for l in range(2)], axis=1))
    m["fnT"] = fm(inp["final_norm"])
    for k in ("moe_router", "moe_w1", "moe_w3", "moe_w2", "o_w_out"):
        m[k] = inp[k]
    w = inp["o_w_in"][0]
    kd = w[:, 1024:1280].reshape(1024, 4, 1, 64)
    kd = np.concatenate([kd, kd], axis=2).reshape(1024, 512)
    m["o_w_in2"] = np.ascontiguousarray(np.concatenate([w[:, 0:1024], kd, w[:, 1280:1536]], axis=1))
    m["o_sink"] = np.ascontiguousarray(inp["o_sink"].reshape(1, 16))
    idx = []
    for p in range(4):
        for off in (0, 512, 1024):
            idx += list(range(off + p * 128, off + p * 128 + 128))
    for d in range(2):
        idx += list(range(1536 + d * 64, 1536 + d * 64 + 64)) + list(range(1664 + d * 64, 1664 + d * 64 + 64))
    idx += list(range(1792, 1920))
    idxa = list(idx)
    for h in range(4):
        for part in range(3):
            idx += list(range(1920 + part * 512 + h * 128, 1920 + part * 512 + h * 128 + 128))
        idx += list(range(3472 + h * 128, 3472 + h * 128 + 128))
    idx += list(range(3456, 3472))
    m["e_w_in2"] = np.ascontiguousarray(inp["e_w_in"][0][:, idx])
    mu2 = inp["a_mu"][0][idxa]
    p64 = np.zeros((64, 64), np.float32)
    p64[:, 0:30] = mu2.reshape(30, 64).T
    for d in range(2):
        for h in range(8):
            p64[:, 30 + d * 8 + h] = inp["a_w0"][0, d, h * 64:(h + 1) * 64]
            p64[:, 46 + d * 8 + h] = inp["a_a0"][0, d, h * 64:(h + 1) * 64]
    m["mx_par64"] = p64
    p128 = np.zeros((128, 112), np.float32)
    p128[:, 0] = mu2[1536:1664]; p128[:, 1] = mu2[1664:1792]; p128[:, 2] = mu2[1792:1920]
    for h in range(8):
        p128[0:64, 8 + h] = inp["a_k_k"][0, h * 64:(h + 1) * 64]
        p128[0:64, 16 + h] = inp["a_k_a"][0, h * 64:(h + 1) * 64]
        p128[0:64, 32 + h] = inp["a_r_k"][0, h]
    p128[0:8, 40] = inp["b_dt_bias"][0].reshape(8)
    p128[0:8, 41] = inp["b_a_log"][0].reshape(8)
    for h in range(4):
        for part in range(3):
            for j in range(5):
                p128[:, 48 + (h * 3 + part) * 5 + j] = inp["b_conv"][0, j, part * 512 + h * 128:part * 512 + (h + 1) * 128]
    m["mx_par128"] = p128
    pP = np.zeros((128, 64), np.float32)
    pP[:, 0:12] = mu2[0:1536].reshape(12, 128).T
    for p in range(4):
        sl = slice(p * 128, (p + 1) * 128)
        for d in range(2):
            pP[:, 12 + d * 4 + p] = inp["a_w0"][0, d, sl]
            pP[:, 20 + d * 4 + p] = inp["a_a0"][0, d, sl]
        pP[:, 28 + p] = inp["a_k_k"][0, sl]
        pP[:, 32 + p] = inp["a_k_a"][0, sl]
        pP[:, 40 + p] = inp["a_r_k"][0].reshape(512)[sl]
    m["mx_parP"] = pP
    m["mx_w2"] = np.ascontiguousarray(np.concatenate([inp["a_w2"][0].transpose(1, 0, 2), inp["a_a2"][0].transpose(1, 0, 2)], axis=0))
    m["mx_bc"] = np.ascontiguousarray(np.concatenate([inp["a_ln_w"][0], inp["a_ln_b"][0], np.tile(inp["b_norm"][0], 4)])[None, :])
    m["a_g2"] = inp["a_g2"]
    m["e_w_out"] = inp["e_w_out"]
    return m


IN_SHAPES = {
    "k_ident": [128, 128], "k_iota": [128, 256],
    "x": [2048, D], "ctx": [256, D], "cT": [128, 8], "ccT": [128, 8],
    "ada_w": [2, D, 6 * D], "ada_bT": [128, 2, 48], "normT": [128, 2, 2, 8], "fnT": [128, 8],
    "k_ropeC": [128, 2048], "k_ropeS": [128, 2048], "k_perm": [128, 128], "k_mL": [128, 512], "k_mU": [128, 512],
    "o_w_in2": [D, 1792], "o_w_out": [1, D, D], "o_sink": [1, 16],
    "k_maskA": [CHUNK, 4 * CHUNK], "k_maskT": [CHUNK, CHUNK], "k_rmask": [128, TP], "k_J": [128, 128], "k_sel": [16, 16, 128],
    "e_w_in2": [D, 3984], "mx_par64": [64, 64], "mx_parP": [128, 64], "k_bd": [128, 128], "mx_par128": [128, 112], "mx_w2": [128, 2, 512], "mx_bc": [1, 1536],
    "a_g2": [1, 128, 512], "e_w_out": [1, D, D],
    "moe_router": [2, D, 16], "moe_w1": [2, 16, D, D], "moe_w3": [2, 16, D, D], "moe_w2": [2, 16, D, D],
}


def build(stages=("all",), extra_in=(), outs=(("out", [2048, D]),)):
    from contextlib import ExitStack
    nc = bass.Bass("TRN2", target_bir_lowering=False)
    g = {}
    for name, shape in IN_SHAPES.items():
        g[name] = nc.dram_tensor(name, shape, F32, kind="ExternalInput").ap()
    for name, shape in extra_in:
        g[name] = nc.dram_tensor(name, shape, F32, kind="ExternalInput").ap()
    for name, shape in outs:
        g[name] = nc.dram_tensor(name, shape, F32, kind="ExternalOutput").ap()
    g["ye_scr"] = nc.dram_tensor("ye_scr", [16, 128, 2, D], BF16).ap()
    g["ye_scr_c"] = nc.dram_tensor("ye_scr_c", [16, 32, 1, D], BF16).ap()
    g["ys_scr"] = nc.dram_tensor("ys_scr", [2, 2304, 1536], F32).ap()
    g["z_scr"] = nc.dram_tensor("z_scr", [2304, 512], F32).ap()
    for nm, rows in (("xm_lat", 2048), ("xm_ctx", 256), ("x1_lat", 2048), ("x1_ctx", 256), ("x2_lat", 2048)):
        g[nm] = nc.dram_tensor(nm, [rows, D], F32).ap()
    kb = KB(nc)
    with ExitStack() as es:
        c = load_consts(kb, nc, es, g)
        c.fnT = es.enter_context(nc.sbuf_tensor("c_fnT", [128, 8], F32))
        kb.dma("sp", c.fnT[:], g["fnT"][:, :], writes=["c_fnT"])
        mods = prologue(kb, nc, es, g, c)
        for st in stages:
            if st == "moe0l_test":
                moe_stage(kb, nc, g, c, mods, 0, 0, g["t_in"], g["out"], 2048)
            elif st == "moe0f_test":
                moe_stage(kb, nc, g, c, mods, 0, 0, g["t_in"], g["out"], 2048, comp=(g["t_in2"], g["out2"]))
            elif st == "moe0c_test":
                moe_stage(kb, nc, g, c, mods, 0, 1, g["t_in"], g["out"], 256)
            elif st == "moe1l_test":
                moe_stage(kb, nc, g, c, mods, 1, 0, g["t_in"], g["out"], 2048, final_norm=True)
            elif st == "all":
                mixer_stage(kb, nc, g, c, mods, g["x"], g["ctx"], g["xm_lat"], g["xm_ctx"])
                moe_stage(kb, nc, g, c, mods, 0, 0, g["xm_lat"], g["x1_lat"], 2048, comp=(g["xm_ctx"], g["x1_ctx"]))
                attn_stage(kb, nc, g, c, mods, g["x1_lat"], g["x1_ctx"], g["x2_lat"])
                moe_stage(kb, nc, g, c, mods, 1, 0, g["x2_lat"], g["out"], 2048, final_norm=True)
            elif st == "mixer_test":
                mixer_stage(kb, nc, g, c, mods, g["x"], g["ctx"], g["out"], g["out2"])
            elif st == "attn_test":
                attn_stage(kb, nc, g, c, mods, g["t_in"], g["t_in2"], g["out"])
            elif st == "mods_test":
                for l in range(2):
                    kb.dma("sp", g["out"][l * 128:(l + 1) * 128, 0:96], mods[l][:].rearrange("p a b -> p (a b)"),
                           reads=[f"mod{l}"], writes=[("o", l)])
        kb.finish([])
    return nc, kb


def rope_tables():
    quarter = 16
    inv = (10000.0 ** (-np.arange(quarter, dtype=np.float32) / quarter)).astype(np.float32)
    t = np.arange(2048)
    row = (t // 64).astype(np.float32)
    col = (t % 64).astype(np.float32)
    C = np.zeros((128, 2048), np.float32)
    S = np.zeros((128, 2048), np.float32)
    perm = np.zeros((128, 128), np.float32)
    for p in range(128):
        d = p % 64
        pos = row if d < 32 else col
        i = d % 16
        ang = (pos * inv[i]).astype(np.float32)
        C[p] = np.cos(ang)
        second = (d % 32) >= 16
        S[p] = np.sin(ang) if second else -np.sin(ang)
        partner = p - 16 if second else p + 16
        perm[partner, p] = 1.0
    return C, S, perm


def attn_stage(kb, nc, g, c, mods, x_lat_in, x_ctx_in, x_out):
    layer = 1
    mod = mods[layer]
    NT = 18
    from contextlib import ExitStack
    with ExitStack() as es:
        def sb(name, shape, dt):
            return es.enter_context(nc.sbuf_tensor(f"at_{name}", shape, dt))
        Gbc = sb("G", [128, D], F32)
        Sbc = sb("S", [128, D], F32)
        xb = [sb("x0", [128, D], F32), sb("x1", [128, D], F32)]
        hb = [sb("h0", [128, D], BF16), sb("h1", [128, D], BF16)]
        stt = [sb("st0", [128, 4], F32), sb("st1", [128, 4], F32)]
        hT = sb("hT", [128, 8, 2304], BF16)
        win = sb("win", [128, 8, 1792], BF16)
        wo = sb("wo", [128, 8, D], BF16)
        stg = [sb("stg0", [128, D], F32), sb("stg1", [128, D], F32)]
        Ct = sb("Ct", [128, 2048], F32)
        St = sb("St", [128, 2048], F32)
        perm = sb("perm", [128, 128], F32)
        qraw = [sb("qraw0", [128, 512], F32), sb("qraw1", [128, 512], F32)]
        rt1 = [sb("rt10", [128, 512], F32), sb("rt11", [128, 512], F32)]
        qT = sb("qT", [128, 8, 2048], BF16)
        kT = sb("kT", [128, 4, 2304], BF16)
        V = sb("V", [128, NT, 4, 65], BF16)
        mL = sb("mL", [128, 512], BF16)
        mU = sb("mU", [128, 512], BF16)
        esink = sb("esink", [128, 16], F32)
        PT = [sb(f"PT{i}", [128, 512], BF16) for i in range(2)]
        osb = stg[0]
        den = sb("den", [128, 4], F32)
        oT = sb("oT", [128, 8, 128], BF16)
        tmpo = qraw
        ps = [es.enter_context(nc.psum_tensor(f"at_ps{i}", [128, 512], F32)) for i in range(8)]

        kb.dma("sp", Ct[:], g["k_ropeC"][:, :], writes=["Ct"])
        kb.dma("sp", St[:], g["k_ropeS"][:, :], writes=["St"])
        kb.dma("sp", perm[:], g["k_perm"][:, :], writes=["perm"])
        kb.dma("sp", esink[:], g["o_sink"].partition_broadcast(128), writes=["esink"])
        kb.op("act", lambda e: e.activation(out=esink[:], in_=esink[:], func=AF.Exp), reads=["esink"], writes=["esink"])
        kb.dma("sp", stg[0][:, 0:512], g["k_mL"][:, :], writes=["stg0"])
        kb.op("dve", lambda e: e.tensor_copy(out=mL[:], in_=stg[0][:, 0:512]), reads=["stg0"], writes=["mL"])
        kb.dma("sp", stg[1][:, 0:512], g["k_mU"][:, :], writes=["stg1"])
        kb.op("dve", lambda e: e.tensor_copy(out=mU[:], in_=stg[1][:, 0:512]), reads=["stg1"], writes=["mU"])
        make_bc(kb, nc, c, lambda kc: mod[:, 1 * 8 + kc, 1:2], Gbc, ps[0], "Gbc", [f"mod{layer}"])
        make_bc(kb, nc, c, lambda kc: mod[:, 0 * 8 + kc, 1:2], Sbc, ps[0], "Sbc", [f"mod{layer}"])

        wcnt = [0]

        def load_rows(src_ap, dst_fn, ncols, key):
            for kc in range(8):
                j = wcnt[0] % 2
                wcnt[0] += 1
                kb.dma(["sp", "act"][kc % 2], stg[j][:, 0:ncols], src_ap[:, kc, :], writes=[f"stg{j}"])
                ce = ["dve", "pool"][wcnt[0] % 2]
                kb.op(ce, lambda e, j=j, kc=kc: e.tensor_copy(out=dst_fn(kc), in_=stg[j][:, 0:ncols]),
                      reads=[f"stg{j}"], writes=[key])
        wv = g["o_w_in2"].rearrange("(kc p) n -> p kc n", p=128)
        load_rows(wv[:, :, 0:1024], lambda kc: win[:, kc, 0:1024], 1024, "win")
        load_rows(wv[:, :, 1024:1792], lambda kc: win[:, kc, 1024:1792], 768, "win")
        load_rows(g["o_w_out"][0].rearrange("(kc p) n -> p kc n", p=128), lambda kc: wo[:, kc, :], 1024, "wo")

        for i in range(NT):
            b = i % 2
            if i == 2:
                kb.barrier()
                make_bc(kb, nc, c, lambda kc: mod[:, 1 * 8 + kc, 0:1], Gbc, ps[0], "Gbc", [f"mod{layer}"])
                make_bc(kb, nc, c, lambda kc: mod[:, 0 * 8 + kc, 0:1], Sbc, ps[0], "Sbc", [f"mod{layer}"])
            src = x_ctx_in[i * 128:(i + 1) * 128, :] if i < 2 else x_lat_in[(i - 2) * 128:(i - 1) * 128, :]
            kb.dma("sp", xb[b][:], src, writes=[f"xb{b}"])
            norm_tile(kb, nc, xb[b][:], f"xb{b}", stt[b], Gbc, Sbc, stg[b][:], f"stg{b}")
            kb.op("act", lambda e, b=b: e.copy(out=hb[b][:], in_=stg[b][:]), reads=[f"stg{b}"], writes=[f"hb{b}"])
            for half in range(2):
                pst = ps[half][:].bitcast(BF16)
                for q in range(4):
                    kc = half * 4 + q
                    kb.op("pe", lambda e, kc=kc, q=q, b=b, pst=pst: e.transpose(
                        out=pst[:, q * 128:(q + 1) * 128], in_=hb[b][:, kc * 128:(kc + 1) * 128],
                        identity=c.identb[:]), reads=[f"hb{b}", "c_identb"], writes=[f"ps{half}"])
                kb.op("dve" if half == 0 else "pool" if False else "act",
                      (lambda e, half=half, pst=pst, i=i: e.tensor_copy(
                          out=hT[:, half * 4:(half + 1) * 4, i * 128:(i + 1) * 128],
                          in_=pst[:, 0:512].rearrange("p (q t) -> p q t", q=4))) if half == 0 else
                      (lambda e, half=half, pst=pst, i=i: e.copy(
                          out=hT[:, half * 4:(half + 1) * 4, i * 128:(i + 1) * 128],
                          in_=pst[:, 0:512].rearrange("p (q t) -> p q t", q=4))),
                      reads=[f"ps{half}"], writes=["hT"])

        import os
        APH = int(os.environ.get("ATT_PH", "9"))
        if APH < 1:
            kb.barrier(); return
        nb = 0
        for nq in range(8):
            for tb in range(4):
                b = nb % 2
                nb += 1
                pq = ps[2 + b]
                t0 = 256 + tb * 512
                for kc in range(8):
                    kb.op("pe", lambda e, kc=kc, nq=nq, t0=t0, pq=pq: e.matmul(
                        pq[:], lhsT=win[:, kc, nq * 128:(nq + 1) * 128], rhs=hT[:, kc, t0:t0 + 512],
                        start=(kc == 0), stop=(kc == 7)), reads=["win", "hT"], writes=[f"ps{2 + b}"])
                kb.op("act", lambda e, b=b, pq=pq: e.copy(out=qraw[b][:], in_=pq[:]), reads=[f"ps{2 + b}"],
                      writes=[f"qraw{b}"])
                pw = ps[4 + b]
                kb.op("pe", lambda e, b=b, pw=pw: e.matmul(pw[:], lhsT=perm[:], rhs=qraw[b][:], start=True, stop=True),
                      reads=["perm", f"qraw{b}"], writes=[f"ps{4 + b}"])
                cs_ = slice(tb * 512, (tb + 1) * 512)
                kb.op("dve", lambda e, b=b, pw=pw, cs_=cs_: e.scalar_tensor_tensor(
                    out=rt1[b][:], in0=pw[:], scalar=0.125, in1=St[:, cs_], op0=ALU.mult, op1=ALU.mult),
                      reads=[f"ps{4 + b}", "St"], writes=[f"rt1{b}"])
                kb.op("pool", lambda e, b=b, cs_=cs_: e.tensor_tensor(out=qraw[b][:], in0=qraw[b][:], in1=Ct[:, cs_],
                                                                    op=ALU.mult),
                      reads=[f"qraw{b}", "Ct"], writes=[f"qraw{b}"])
                kb.op("dve", lambda e, b=b, nq=nq, cs_=cs_: e.scalar_tensor_tensor(
                    out=qT[:, nq, cs_], in0=qraw[b][:], scalar=0.125, in1=rt1[b][:], op0=ALU.mult, op1=ALU.add),
                    reads=[f"qraw{b}", f"rt1{b}"], writes=["qT"])
        if APH < 2:
            kb.barrier(); return
        for hk in range(4):
            for tb in range(5):
                b = nb % 2
                nb += 1
                pq = ps[2 + b]
                t0 = 0 if tb == 0 else 256 + (tb - 1) * 512
                tw = 256 if tb == 0 else 512
                for kc in range(8):
                    kb.op("pe", lambda e, kc=kc, hk=hk, t0=t0, tw=tw, pq=pq: e.matmul(
                        pq[:, 0:tw], lhsT=win[:, kc, 1024 + hk * 128:1024 + (hk + 1) * 128],
                        rhs=hT[:, kc, t0:t0 + tw], start=(kc == 0), stop=(kc == 7)),
                        reads=["win", "hT"], writes=[f"ps{2 + b}"])
                if tb == 0:
                    kb.op("act", lambda e, hk=hk, pq=pq: e.copy(out=kT[:, hk, 0:256], in_=pq[:, 0:256]),
                          reads=[f"ps{2 + b}"], writes=["kT"])
                    continue
                kb.op("act", lambda e, b=b, pq=pq: e.copy(out=qraw[b][:], in_=pq[:]), reads=[f"ps{2 + b}"],
                      writes=[f"qraw{b}"])
                pw = ps[4 + b]
                kb.op("pe", lambda e, b=b, pw=pw: e.matmul(pw[:], lhsT=perm[:], rhs=qraw[b][:], start=True, stop=True),
                      reads=["perm", f"qraw{b}"], writes=[f"ps{4 + b}"])
                cs_ = slice((tb - 1) * 512, tb * 512)
                kb.op("dve", lambda e, b=b, pw=pw, cs_=cs_: e.tensor_tensor(out=rt1[b][:], in0=pw[:], in1=St[:, cs_],
                                                                         op=ALU.mult),
                      reads=[f"ps{4 + b}", "St"], writes=[f"rt1{b}"])
                kb.op("pool", lambda e, b=b, cs_=cs_: e.tensor_tensor(out=qraw[b][:], in0=qraw[b][:], in1=Ct[:, cs_],
                                                                    op=ALU.mult),
                      reads=[f"qraw{b}", "Ct"], writes=[f"qraw{b}"])
                kb.op("dve", lambda e, b=b, hk=hk, t0=t0: e.tensor_tensor(
                    out=kT[:, hk, t0:t0 + 512], in0=qraw[b][:], in1=rt1[b][:], op=ALU.add),
                    reads=[f"qraw{b}", f"rt1{b}"], writes=["kT"])
        if APH < 3:
            kb.barrier(); return
        kb.op("pool", lambda e: e.memset(V[:], 1.0), writes=["V"])
        for i in range(NT):
            b = nb % 2
            nb += 1
            pq = ps[2 + b]
            for kc in range(8):
                kb.op("pe", lambda e, kc=kc, i=i, pq=pq: e.matmul(
                    pq[:, 0:256], lhsT=hT[:, kc, i * 128:(i + 1) * 128], rhs=win[:, kc, 1536:1792],
                    start=(kc == 0), stop=(kc == 7)), reads=["win", "hT"], writes=[f"ps{2 + b}"])
            kb.op("act", lambda e, i=i, pq=pq: e.copy(out=V[:, i, :, 0:64],
                                                     in_=pq[:, 0:256].rearrange("p (h d) -> p h d", h=4)),
                  reads=[f"ps{2 + b}"], writes=["V"])

        if APH < 4:
            kb.barrier(); return
        make_bc(kb, nc, c, lambda kc: mod[:, 2 * 8 + kc, 0:1], Gbc, ps[0], "Gbc", [f"mod{layer}"])
        nsb = 0
        for n in range(16):
            kb.dma("sp", xb[n % 2][:], x_lat_in[n * 128:(n + 1) * 128, :], writes=[f"xb{n % 2}"])
            for hk in range(4):
                tiles = []
                if n > 0:
                    tiles.append((2 + n - 1, mL, "mL"))
                tiles.append((2 + n, None, None))
                if n < 15:
                    tiles.append((2 + n + 1, mU, "mU"))
                tiles.append((0, None, None))
                tiles.append((1, None, None))
                po = ps[6 + (n * 4 + hk) % 2]
                pok = f"ps{6 + (n * 4 + hk) % 2}"
                for ti, (kt, msk, mk) in enumerate(tiles):
                    sbk = nsb % 2
                    nsb += 1
                    pSa, pSb = ps[1 + 2 * sbk], ps[2 + 2 * sbk]
                    ka, kbk = f"ps{1 + 2 * sbk}", f"ps{2 + 2 * sbk}"
                    for gq in range(4):
                        hq = hk * 4 + gq
                        bp = (hq % 2) * 64
                        pS = pSa if bp == 0 else pSb
                        kb.op("pe", lambda e, gq=gq, hq=hq, bp=bp, kt=kt, pS=pS, hk=hk, n=n: e.matmul(
                            pS[:, (gq // 2) * 128:(gq // 2 + 1) * 128], lhsT=kT[bp:bp + 64, hk, kt * 128:(kt + 1) * 128],
                            rhs=qT[bp:bp + 64, hq // 2, n * 128:(n + 1) * 128], start=True, stop=True),
                            reads=["kT", "qT"], writes=[ka if bp == 0 else kbk])
                    ptv = PT[sbk][:].rearrange("p (a b q) -> p a b q", a=2, b=2)
                    kb.op("act", lambda e, pSa=pSa, ptv=ptv: e.activation(
                        out=ptv[:, :, 0, :], in_=pSa[:, 0:256].rearrange("p (a q) -> p a q", a=2), func=AF.Exp),
                        reads=[ka], writes=[f"PT{sbk}"])
                    kb.op("act", lambda e, pSb=pSb, ptv=ptv: e.activation(
                        out=ptv[:, :, 1, :], in_=pSb[:, 0:256].rearrange("p (a q) -> p a q", a=2), func=AF.Exp),
                        reads=[kbk], writes=[f"PT{sbk}"])
                    ADBG = int(os.environ.get("ATT_DBG", "9"))
                    if ADBG < 2:
                        continue
                    if msk is not None:
                        kb.op("dve", lambda e, sbk=sbk, msk=msk: e.tensor_tensor(out=PT[sbk][:], in0=PT[sbk][:],
                                                                               in1=msk[:], op=ALU.mult),
                              reads=[f"PT{sbk}", mk], writes=[f"PT{sbk}"])
                    for gq in range(4):
                        kb.op("pe", lambda e, gq=gq, sbk=sbk, kt=kt, hk=hk, po=po, ti=ti: e.matmul(
                            po[:, gq * 65:(gq + 1) * 65], lhsT=PT[sbk][:, gq * 128:(gq + 1) * 128],
                            rhs=V[:, kt, hk, :], start=(ti == 0 and gq == 0), stop=(ti == len(tiles) - 1),
                            skip_group_check=True), reads=[f"PT{sbk}", "V"], writes=[pok])
                if ADBG < 3:
                    continue
                pov = po[:, 0:260].rearrange("p (g d) -> p g d", g=4)
                kb.op("dve", lambda e, pov=pov, hk=hk: e.tensor_tensor(
                    out=den[:], in0=pov[:, :, 64], in1=esink[:, hk * 4:(hk + 1) * 4], op=ALU.add),
                    reads=[pok, "esink"], writes=["den"])
                kb.op("dve", lambda e: e.reciprocal(out=den[:], in_=den[:]), reads=["den"], writes=["den"])
                kb.op("dve", lambda e, pov=pov, hk=hk: e.tensor_tensor(
                    out=osb[:, hk * 256:(hk + 1) * 256].rearrange("p (g d) -> p g d", g=4), in0=pov[:, :, 0:64],
                    in1=den[:].unsqueeze(2).broadcast_to([128, 4, 64]), op=ALU.mult),
                    reads=[pok, "den"], writes=["stg0"])
            if ADBG < 4:
                continue
            kb.op("act", lambda e: e.copy(out=hb[0][:], in_=osb[:]), reads=["stg0"], writes=["hb0"])
            pst = ps[0][:].bitcast(BF16)
            for kc in range(8):
                kb.op("pe", lambda e, kc=kc, pst=pst: e.transpose(
                    out=pst[:, kc * 128:(kc + 1) * 128], in_=hb[0][:, kc * 128:(kc + 1) * 128], identity=c.identb[:]),
                    reads=["hb0", "c_identb"], writes=["ps0"])
            kb.op("act", lambda e, pst=pst: e.copy(out=oT[:], in_=pst[:].rearrange("p (k t) -> p k t", k=8)),
                  reads=["ps0"], writes=["oT"])
            for half in range(2):
                pp = ps[5] if half == 0 else ps[0]
                for kc in range(8):
                    kb.op("pe", lambda e, kc=kc, half=half, pp=pp: e.matmul(
                        pp[:], lhsT=oT[:, kc, :], rhs=wo[:, kc, half * 512:(half + 1) * 512],
                        start=(kc == 0), stop=(kc == 7)), reads=["oT", "wo"], writes=["ps5" if half == 0 else "ps0"])
                sl = slice(half * 512, (half + 1) * 512)
                kb.op("dve", lambda e, half=half, pp=pp, sl=sl: e.tensor_tensor(
                    out=tmpo[half][:], in0=pp[:], in1=Gbc[:, sl], op=ALU.mult),
                    reads=["ps5" if half == 0 else "ps0", "Gbc"], writes=[f"qraw{half}"])
                kb.op("pool", lambda e, half=half, sl=sl, n=n: e.tensor_tensor(
                    out=xb[n % 2][:, sl], in0=xb[n % 2][:, sl], in1=tmpo[half][:], op=ALU.add),
                    reads=[f"qraw{half}", f"xb{n % 2}"], writes=[f"xb{n % 2}"])
            kb.dma("sp", x_out[n * 128:(n + 1) * 128, :], xb[n % 2][:], reads=[f"xb{n % 2}"],
                   writes=[("dram", x_out.tensor.name, n)])
        kb.barrier()


def dplr_scan(kb, nc, c, T, dk, rT, kkT, kT, bT, vT, Pinc, prodT, store_cb, hk_, Gb=None, rA=None, kkA=None, bp=0, rC=None, kkC=None):
    ps = T["ps"]
    rA = rT if rA is None else rA
    kkA = kkT if kkA is None else kkA
    rC = rT if rC is None else rC
    kkC = kkT if kkC is None else kkC
    tb = vT.dtype == BF16
    ident, maskA, maskT, ident64 = c.ident, T["maskA"], T["maskT"], c.ident
    ST = T["ST"]
    kb.op("dve", lambda e: e.memset(ST[bp:bp + dk, 0:dk], 0.0), writes=["ST"])
    kb.op("dve", lambda e: e.memset(T["STb"][bp:bp + dk, 0:dk], 0.0), writes=["STb"])
    CH = CHUNK
    NLV = {64: 5, 128: 6}[CH]
    NCH = len(CHUNK_COLS)
    GB = 2

    def inv_gen(g0):
        grp = list(range(g0, min(NCH, g0 + GB)))
        par = (g0 // GB) % 2
        for ci in grp:
            s = ci % GB + GB * par
            cs = slice(CHUNK_COLS[ci], CHUNK_COLS[ci] + CH)
            pa = ps[s % 2]
            pk = f"ps{s % 2}"
            pn = ps[2 + s % 2]
            pnk = f"ps{2 + s % 2}"
            for j, (l, r) in enumerate(((bT, kkA), (kT, kkA), (bT, rA), (kT, rA), (kkA, bT))):
                if j < 4:
                    kb.op("pe", lambda e, j=j, l=l, r=r, cs=cs, pa=pa: e.matmul(
                        pa[0:CH, j * CH:(j + 1) * CH], lhsT=l[bp:bp + dk, cs], rhs=r[bp:bp + dk, cs], start=True, stop=True),
                        reads=[hk_], writes=[pk])
                else:
                    kb.op("pe", lambda e, l=l, r=r, cs=cs, pn=pn: e.matmul(
                        pn[0:CH, 256:256 + CH], lhsT=l[bp:bp + dk, cs], rhs=r[bp:bp + dk, cs], start=True, stop=True),
                        reads=[hk_], writes=[pnk])
            mA, mT_, mAk, mTk = maskA[0:CH, :], maskT[0:CH, :], "maskA", "maskT"
            if Gb is not None:
                c0_ = CHUNK_COLS[ci]
                kb.op("pe", lambda e, cs=cs, pn=pn: e.transpose(out=pn[0:CH, 384:385], in_=Gb[0:1, cs],
                                                              identity=ident[0:1, 0:1]), reads=[hk_, "c_ident"], writes=[pnk])
                kb.op("act", lambda e, s=s, pn=pn: e.copy(out=T["Gc"][0:CH, s:s + 1], in_=pn[0:CH, 384:385]),
                      reads=[pnk], writes=[f"Gc{s}"])
                kb.op("dve", lambda e, s=s, cs=cs: e.tensor_scalar(out=T["Dt"][0:CH, s % GB, :], in0=Gb[0:CH, cs],
                                                                 scalar1=T["Gc"][0:CH, s:s + 1], scalar2=0.0,
                                                                 op0=ALU.subtract, op1=ALU.min),
                      reads=[hk_, f"Gc{s}"], writes=[f"Dt{s % GB}"])
                kb.op("act", lambda e, s=s: e.activation(out=T["Dt"][0:CH, s % GB, :], in_=T["Dt"][0:CH, s % GB, :], func=AF.Exp),
                      reads=[f"Dt{s % GB}"], writes=[f"Dt{s % GB}"])
                kb.op("dve", lambda e, s=s, cs=cs: e.tensor_scalar(out=T["Dts"][0:CH, s % GB, :], in0=Gb[0:CH, cs],
                                                                 scalar1=T["Gc"][0:CH, s:s + 1], scalar2=0.0,
                                                                 op0=ALU.subtract, op1=ALU.max),
                      reads=[hk_, f"Gc{s}"], writes=[f"Dts{s % GB}"])
                kb.op("act", lambda e, s=s: e.activation(out=T["Dts"][0:CH, s % GB, :], in_=T["Dts"][0:CH, s % GB, :], func=AF.Exp,
                                                         scale=-1.0), reads=[f"Dts{s % GB}"], writes=[f"Dts{s % GB}"])
                kb.op("dve", lambda e, s=s: e.tensor_tensor(
                    out=T["mD"][0:CH, s % GB, :].rearrange("p (a t) -> p a t", a=4),
                    in0=maskA[0:CH, :].rearrange("p (a t) -> p a t", a=4),
                    in1=T["Dt"][0:CH, s % GB, :].unsqueeze(1).broadcast_to([CH, 4, CH]), op=ALU.mult),
                    reads=["maskA", f"Dt{s % GB}"], writes=[f"mD{s % GB}"])
                kb.op("pool", lambda e, s=s: e.tensor_tensor(out=T["Dts"][0:CH, s % GB, :], in0=T["Dts"][0:CH, s % GB, :],
                                                            in1=maskT[0:CH, :], op=ALU.mult),
                      reads=["maskT", f"Dts{s % GB}"], writes=[f"Dts{s % GB}"])
                kb.op("act", lambda e, s=s, c0_=c0_: e.activation(out=T["dL"][0:CH, s:s + 1], in_=T["Gc"][0:CH, s:s + 1],
                                                                func=AF.Exp, scale=-1.0, bias=Gb[0:CH, c0_ + CH - 1:c0_ + CH]),
                      reads=[f"Gc{s}", hk_], writes=[f"dL{s}"])
                mA, mT_, mAk, mTk = T["mD"][0:CH, s % GB, :], T["Dts"][0:CH, s % GB, :], f"mD{s % GB}", f"Dts{s % GB}"
            kb.op("dve", lambda e, s=s, pa=pa, mA=mA: e.tensor_tensor(out=T["AMf"][0:CH, s, :], in0=pa[0:CH, 0:CH],
                                                                     in1=mA[:, 0:CH], op=ALU.mult),
                  reads=[pk, mAk], writes=[f"AM{s}"])
            kb.op("dve", lambda e, s=s, pa=pa, mA=mA: e.tensor_tensor(out=T["AMb"][0:CH, s, :], in0=pa[0:CH, CH:4 * CH],
                                                                     in1=mA[:, CH:4 * CH], op=ALU.mult),
                  reads=[pk, mAk], writes=[f"AMb{s}"])
            kb.op("dve", lambda e, s=s, pn=pn, mT_=mT_: e.tensor_tensor(out=T["MM"][0][0:CH, s, CH:2 * CH],
                                                                       in0=pn[0:CH, 256:256 + CH], in1=mT_, op=ALU.mult),
                  reads=[pnk, mTk], writes=[f"MM0_{s}"])
            kb.op("pool", lambda e, s=s: e.tensor_copy(out=T["MM"][0][0:CH, s, 0:CH], in_=T["AMf"][0:CH, s, :]),
                  reads=[f"AM{s}"], writes=[f"MM0_{s}"])
            kb.op("pool", lambda e, s=s: e.tensor_tensor(out=T["Q"][0][0:CH, s, :], in0=T["AMf"][0:CH, s, :],
                                                        in1=ident64[0:CH, 0:CH], op=ALU.add),
                  reads=[f"AM{s}", "c_ident"], writes=[f"Q0_{s}"])
            yield
        for lv in range(NLV):
            a, b = lv % 2, (lv + 1) % 2
            last = lv == NLV - 1
            for ci in grp:
                s = ci % GB + GB * par
                pm = ps[2 + s % 2]
                pmk = f"ps{2 + s % 2}"
                MMa = T["MM"][a]
                kb.op("pe", lambda e, s=s, pm=pm, MMa=MMa: e.matmul(
                    pm[0:CH, 0:CH], lhsT=MMa[0:CH, s, CH:2 * CH], rhs=MMa[0:CH, s, 0:CH], start=True, stop=True),
                    reads=[f"MM{a}_{s}"], writes=[pmk])
                kb.op("pe", lambda e, s=s, pm=pm, MMa=MMa: e.matmul(
                    pm[0:CH, CH:2 * CH], lhsT=MMa[0:CH, s, 0:CH], rhs=MMa[0:CH, s, CH:2 * CH], start=True, stop=True),
                    reads=[f"MM{a}_{s}"], writes=[pmk])
                kb.op("act", lambda e, s=s, pm=pm, b=b: e.copy(out=T["MM"][b][0:CH, s, :], in_=pm[0:CH, 0:2 * CH]),
                      reads=[pmk], writes=[f"MM{b}_{s}"])
                yield
            for ci in grp:
                s = ci % GB + GB * par
                pq = ps[4 + s % 2]
                pqk = f"ps{4 + s % 2}"
                kb.op("pe", lambda e, s=s, pq=pq, b=b, a=a: e.matmul(
                    pq[0:CH, 0:CH], lhsT=T["MM"][b][0:CH, s, CH:2 * CH], rhs=T["Q"][a][0:CH, s, :], start=True, stop=True),
                    reads=[f"MM{b}_{s}", f"Q{a}_{s}"], writes=[pqk])
                kb.op("dve", lambda e, s=s, pq=pq, a=a, b=b: e.tensor_tensor(
                    out=T["Q"][b][0:CH, s, :], in0=pq[0:CH, 0:CH], in1=T["Q"][a][0:CH, s, :], op=ALU.add),
                    reads=[pqk, f"Q{a}_{s}"], writes=[f"Q{b}_{s}"])
                yield

    def chain_gen(g0):
        grp = list(range(g0, min(NCH, g0 + GB)))
        par = (g0 // GB) % 2
        QF = T["Q"][NLV % 2]
        for ci in grp:
            s = ci % GB + GB * par
            c0 = CHUNK_COLS[ci]
            cs = slice(c0, c0 + CH)
            AMb = T["AMb"]
            STb = T["STb"]
            pt = ps[6][:].bitcast(BF16) if tb else ps[6]
            idt = c.identb if tb else ident
            for j, src in enumerate((vT, kT, bT)):
                kb.op("pe", lambda e, j=j, src=src, cs=cs, pt=pt: e.transpose(
                    out=pt[0:CH, j * dk:(j + 1) * dk], in_=src[bp:bp + dk, cs], identity=idt[bp:bp + dk, bp:bp + dk]),
                    reads=[hk_, "c_ident", "c_identb"], writes=["ps6"])
            TM = T["TM"][ci % 2]
            tmk = f"TM{ci % 2}"
            kb.op("act", lambda e, TM=TM, pt=pt: e.copy(out=TM[0:CH, 0:3 * dk], in_=pt[0:CH, 0:3 * dk]),
                  reads=["ps6"], writes=[tmk])
            yield
            Vtm, Ktm, Btm = TM[0:CH, 0:dk], TM[0:CH, dk:2 * dk], TM[0:CH, 2 * dk:3 * dk]
            if Gb is not None:
                kb.op("dve", lambda e, TM=TM, s=s: e.tensor_scalar(out=TM[0:CH, dk:3 * dk], in0=TM[0:CH, dk:3 * dk],
                                                                 scalar1=T["dL"][0:CH, s:s + 1], scalar2=None, op0=ALU.mult),
                      reads=[tmk, f"dL{s}"], writes=[tmk])
            pr = ps[7]
            kb.op("pe", lambda e, cs=cs, pr=pr: e.matmul(pr[0:CH, 0:dk], lhsT=kkC[bp:bp + dk, cs], rhs=STb[bp:bp + dk, 0:dk],
                                                       start=True, stop=False), reads=[hk_, "STb"], writes=["ps7"])
            kb.op("pe", lambda e, s=s, pr=pr, Vtm=Vtm: e.matmul(pr[0:CH, 0:dk], lhsT=AMb[0:CH, s, 0:CH], rhs=Vtm,
                                                              start=False, stop=True),
                  reads=[f"AMb{s}", tmk], writes=["ps7"])
            yield
            kb.op("dve", lambda e, pr=pr: e.tensor_scalar(out=T["nR"][0:CH, 0:dk], in0=pr[0:CH, 0:dk], scalar1=-1.0,
                                                         scalar2=None, op0=ALU.mult), reads=["ps7"], writes=["nR"])
            yield
            kb.op("pe", lambda e, s=s, pr=pr: e.matmul(pr[0:CH, 128:128 + dk], lhsT=QF[0:CH, s, :], rhs=T["nR"][0:CH, 0:dk],
                                                     start=True, stop=True), reads=[f"Q{NLV % 2}_{s}", "nR"], writes=["ps7"])
            yield
            kb.op("act", lambda e, pr=pr: e.copy(out=T["U"][0:CH, 0:dk], in_=pr[0:CH, 128:128 + dk]),
                  reads=["ps7"], writes=["U"])
            yield
            U = T["U"]
            kb.op("pe", lambda e, cs=cs, pr=pr: e.matmul(pr[0:CH, 256:256 + dk], lhsT=rC[bp:bp + dk, cs], rhs=STb[bp:bp + dk, 0:dk],
                                                       start=True, stop=False), reads=[hk_, "STb"], writes=["ps7"])
            kb.op("pe", lambda e, s=s, pr=pr: e.matmul(pr[0:CH, 256:256 + dk], lhsT=AMb[0:CH, s, CH:2 * CH],
                                                     rhs=U[0:CH, 0:dk], start=False, stop=False),
                  reads=[f"AMb{s}", "U"], writes=["ps7"])
            kb.op("pe", lambda e, s=s, pr=pr, Vtm=Vtm: e.matmul(pr[0:CH, 256:256 + dk], lhsT=AMb[0:CH, s, 2 * CH:3 * CH],
                                                              rhs=Vtm, start=False, stop=True),
                  reads=[f"AMb{s}", tmk], writes=["ps7"])
            kb.op("pe", lambda e, pr=pr, Btm=Btm: e.matmul(pr[bp:bp + dk, 384:384 + dk], lhsT=Btm, rhs=U[0:CH, 0:dk],
                                                         start=True, stop=False), reads=[tmk, "U"], writes=["ps7"])
            kb.op("pe", lambda e, pr=pr, Ktm=Ktm, Vtm=Vtm: e.matmul(pr[bp:bp + dk, 384:384 + dk], lhsT=Ktm, rhs=Vtm,
                                                                  start=False, stop=True),
                  reads=[tmk], writes=["ps7"])
            yield
            Ysb = T["Y"][ci % 2]
            yk = f"Y{ci % 2}"
            kb.op("act", lambda e, pr=pr, Ysb=Ysb: e.copy(out=Ysb[0:CH, 0:dk], in_=pr[0:CH, 256:256 + dk]),
                  reads=["ps7"], writes=[yk])
            if Gb is not None:
                kb.op("dve", lambda e, pr=pr, c0=c0: e.scalar_tensor_tensor(
                    out=ST[bp:bp + dk, 0:dk], in0=ST[bp:bp + dk, 0:dk], scalar=Pinc[bp:bp + dk, c0 + CH - 1:c0 + CH],
                    in1=pr[bp:bp + dk, 384:384 + dk], op0=ALU.mult, op1=ALU.add), reads=["ps7", "ST", hk_], writes=["ST"])
            else:
                kb.op("dve", lambda e, pr=pr: e.tensor_tensor(out=ST[bp:bp + dk, 0:dk], in0=pr[bp:bp + dk, 384:384 + dk],
                                                             in1=ST[bp:bp + dk, 0:dk], op=ALU.add),
                      reads=["ps7", "ST"], writes=["ST"])
                kb.op("dve", lambda e, c0=c0: e.tensor_scalar(out=ST[bp:bp + dk, 0:dk], in0=ST[bp:bp + dk, 0:dk],
                                                             scalar1=Pinc[bp:bp + dk, c0 + CH - 1:c0 + CH], scalar2=None,
                                                             op0=ALU.mult), reads=["ST", hk_], writes=["ST"])
            kb.op("act", lambda e: e.copy(out=T["STb"][bp:bp + dk, 0:dk], in_=ST[bp:bp + dk, 0:dk]),
                  reads=["ST"], writes=["STb"])
            if prodT is not None:
                pb = ps[6]
                kb.op("pe", lambda e, cs=cs, pb=pb: e.matmul(pb[0:CH, 448:449], lhsT=prodT[bp:bp + dk, cs],
                                                           rhs=c.onesb[bp:bp + dk, 0:1], start=True, stop=True),
                      reads=[hk_, "c_ones"], writes=["ps6"])
                kb.op("dve", lambda e, pb=pb, Ysb=Ysb, Vtm=Vtm: e.tensor_scalar(
                    out=Ysb[0:CH, dk:2 * dk], in0=Vtm, scalar1=pb[0:CH, 448:449], scalar2=None, op0=ALU.mult),
                    reads=["ps6", tmk], writes=[yk])
            store_cb(ci, Ysb, yk)
            yield

    from itertools import zip_longest
    for _ in inv_gen(0):
        pass
    for g0 in range(0, NCH, GB):
        gens = [chain_gen(g0)]
        if g0 + GB < NCH:
            gens.append(inv_gen(g0 + GB))
        for _ in zip_longest(*gens):
            pass


def mixer_stage(kb, nc, g, c, mods, x_lat_in, x_ctx_in, x_lat_out, x_ctx_out):
    from contextlib import ExitStack
    mod = mods[0]
    Ys = g["ys_scr"]
    Zs = g["z_scr"]
    with ExitStack() as es:
        def sb(name, shape, dt):
            return es.enter_context(nc.sbuf_tensor(f"mxs_{name}", shape, dt))
        hT = None
        es2 = ExitStack()

        def sb2(name, shape, dt):
            return es2.enter_context(nc.sbuf_tensor(f"mxs_{name}", shape, dt))
        hb = sb("hb", [128, D], BF16)
        stt = [sb("st0", [128, 4], F32), sb("st1", [128, 4], F32)]
        F11 = sb("F11", [128, TP], F32)
        Jb = sb("Jb", [128, 128], BF16)
        J32 = sb("J32", [128, 128], F32)
        par = sb("par", [64, 64], F32)
        parP = sb("parP", [128, 64], F32)
        bd = sb("bd", [128, 128], F32)
        par128 = sb("par128", [128, 112], F32)
        w2sb = sb("w2sb", [128, 2, 512], F32)
        sel = sb("sel", [16, 16, 128], F32)
        ST = sb("ST", [128, 128], F32)
        hT = es2.enter_context(nc.sbuf_tensor("mxs_hT", [128, 8, 2304], BF16))
        F = [sb2(f"F{i}", [128, TP], F32) for i in range(11)] + [F11]
        FK = [f"F{i}" for i in range(12)]
        Gbc = F[1][:, 0:D]
        Sbc = F[2][:, 0:D]
        h32 = F[3][:, 0:D]
        xb = [F[4][:, 0:D], F[5][:, 0:D]]
        h32s = [F[3][:, 0:D], F[6][:, 0:D]]
        hb2 = sb2("hb2", [128, D], BF16)
        hbs = [hb, hb2]
        rmask = sb2("rmask", [128, TP], BF16)
        rm32 = F[0]
        wsl = sb2("wsl", [128, 8, 128], BF16)
        T = {"ST": ST,
             "AMf": sb2("AMf", [CHUNK, 4, CHUNK], F32), "AMb": sb2("AMb", [CHUNK, 4, 3 * CHUNK], BF16),
             "STb": sb2("STb", [128, 128], BF16),
             "MM": [sb2("MMa", [CHUNK, 4, 2 * CHUNK], F32), sb2("MMb", [CHUNK, 4, 2 * CHUNK], F32)],
             "Q": [sb2("Qa", [CHUNK, 4, CHUNK], F32), sb2("Qb", [CHUNK, 4, CHUNK], F32)],
             "TM": [sb2("TMa", [CHUNK, 384], BF16), sb2("TMb", [CHUNK, 384], BF16)],
             "nR": sb2("nR", [CHUNK, 128], F32), "U": sb2("U", [CHUNK, 128], BF16), "Uf": sb2("Uf", [16, 4], F32),
             "Y": [sb2("Ya", [CHUNK, 256], F32), sb2("Yb", [CHUNK, 256], F32)],
             "maskA": sb2("maskA", [CHUNK, 4 * CHUNK], F32), "maskT": sb2("maskT", [CHUNK, CHUNK], F32),
             "Gc": sb2("Gc", [CHUNK, 4], F32), "dL": sb2("dL", [CHUNK, 4], F32), "Dt": sb2("Dt", [CHUNK, 2, CHUNK], F32),
             "Dts": sb2("Dts", [CHUNK, 2, CHUNK], F32), "mD": sb2("mD", [CHUNK, 2, 4 * CHUNK], F32)}
        ps = [es.enter_context(nc.psum_tensor(f"mx_ps{i}", [128, 512], F32)) for i in range(8)]
        T["ps"] = ps
        kb.dma("sp", rm32[:], g["k_rmask"][:, :], writes=["F0"])
        kb.op("dve", lambda e: e.tensor_copy(out=rmask[:], in_=rm32[:]), reads=["F0"], writes=["rmask"])
        kb.dma("sp", J32[:], g["k_J"][:, :], writes=["J32"])
        kb.op("dve", lambda e: e.tensor_copy(out=Jb[:], in_=J32[:]), reads=["J32"], writes=["Jb"])
        kb.dma("sp", par[:], g["mx_par64"][:, :], writes=["par"])
        kb.dma("sp", par128[:], g["mx_par128"][:, :], writes=["par128"])
        kb.dma("sp", w2sb[:], g["mx_w2"][:, :, :], writes=["w2sb"])
        kb.dma("sp", parP[:], g["mx_parP"][:, :], writes=["parP"])
        kb.dma("sp", bd[:], g["k_bd"][:, :], writes=["bd"])
        kb.op("dve", lambda e: e.tensor_scalar(out=parP[:, 36:40], in0=parP[:, 32:36], scalar1=-1.0, scalar2=1.0,
                                               op0=ALU.mult, op1=ALU.add), reads=["parP"], writes=["parP"])
        kb.op("dve", lambda e: e.tensor_scalar(out=par128[:, 24:32], in0=par128[:, 16:24], scalar1=-1.0, scalar2=1.0,
                                               op0=ALU.mult, op1=ALU.add), reads=["par128"], writes=["par128"])
        T["zt"] = [sb2("zta", [128, 128], F32), sb2("ztb", [128, 128], F32)]
        kb.dma("sp", sel[:], g["k_sel"][:, :, :], writes=["sel"])
        kb.dma("sp", T["maskA"][:], g["k_maskA"][:, :], writes=["maskA"])
        kb.dma("sp", T["maskT"][:], g["k_maskT"][:, :], writes=["maskT"])
        for f in range(12):
            kb.op("pool", lambda e, f=f: e.memset(F[f][:], 0.0), reads=["rmask"] if f == 0 else [], writes=[FK[f]])
        wv = g["e_w_in2"].rearrange("(kc p) n -> p kc n", p=128)
        P64 = lambda j: par[:, j:j + 1]

        def project(c0, M, dst, dk_, evac_eng="act"):
            kb.dma("pool", wsl[:, :, 0:M], wv[:, :, c0:c0 + M], writes=["wsl"])
            for bi, (t0, tw, col0) in enumerate(BLOCKS):
                pb = ps[bi % 2]
                for kc in range(8):
                    kb.op("pe", lambda e, kc=kc, t0=t0, tw=tw, pb=pb: e.matmul(
                        pb[0:M, 0:tw], lhsT=wsl[:, kc, 0:M], rhs=hT[:, kc, t0:t0 + tw], start=(kc == 0), stop=(kc == 7)),
                        reads=["wsl", "hT"], writes=[f"ps{bi % 2}"])
                kb.op("act", lambda e, tw=tw, col0=col0, pb=pb: e.copy(out=dst[0:M, col0:col0 + tw], in_=pb[0:M, 0:tw]),
                      reads=[f"ps{bi % 2}"], writes=[dk_])

        def tshift(src, sk, dst, dk_, tmp, tk, P, mucol):
            n = TP - 2
            kb.op("dve", lambda e: e.tensor_tensor(out=tmp[0:P, 1:1 + n], in0=src[0:P, 0:n], in1=src[0:P, 2:2 + n],
                                                   op=ALU.add), reads=[sk], writes=[tk])
            kb.op("dve", lambda e: e.scalar_tensor_tensor(out=tmp[0:P, 1:1 + n], in0=tmp[0:P, 1:1 + n], scalar=0.5,
                                                          in1=src[0:P, 1:1 + n], op0=ALU.mult, op1=ALU.subtract),
                  reads=[sk, tk], writes=[tk])
            kb.op("dve", lambda e: e.scalar_tensor_tensor(out=dst[0:P, 1:1 + n], in0=tmp[0:P, 1:1 + n], scalar=mucol,
                                                         in1=src[0:P, 1:1 + n], op0=ALU.mult, op1=ALU.add),
                  reads=[sk, tk, "par", "par128"], writes=[dk_])

        DC = [(CTX0, 256), (LAT0, 2048)]

        def ew(eng, fn, reads, writes):
            kb.op(eng, fn, reads=reads, writes=writes)

        for d in range(2):
            for stream in (1, 0):
                kb.barrier()
                make_bc(kb, nc, c, lambda kc: mod[:, 1 * 8 + kc, stream:stream + 1], Gbc, ps[0], "Gbc", ["mod0"])
                make_bc(kb, nc, c, lambda kc: mod[:, 0 * 8 + kc, stream:stream + 1], Sbc, ps[0], "Sbc", ["mod0"])
                nt = 2 if stream == 1 else 16
                src = x_ctx_in if stream == 1 else x_lat_in
                base = 0 if stream == 1 else 256
                def htile(i):
                    b = i % 2
                    hbb, h32b = hbs[b], h32s[b]
                    kb.dma("sp", xb[b][:], src[i * 128:(i + 1) * 128, :], writes=[f"xb{b}"])
                    yield
                    yield from norm_tile_g(kb, nc, xb[b][:], f"xb{b}", stt[b], Gbc, Sbc, h32b, f"h32{b}")
                    yield
                    kb.op("act", lambda e: e.copy(out=hbb[:], in_=h32b), reads=[f"h32{b}"], writes=[f"hb{b}"])
                    yield
                    pos = base + (i if d == 0 else nt - 1 - i) * 128
                    for half in range(2):
                        pbk = 2 + half + 2 * b
                        for q in range(4):
                            kc = half * 4 + q
                            kb.op("pe", lambda e, kc=kc, q=q, pbk=pbk: e.matmul(
                                ps[pbk][:, q * 128:(q + 1) * 128], lhsT=hbb[:, kc * 128:(kc + 1) * 128],
                                rhs=(c.identb[:] if d == 0 else Jb[:]), start=True, stop=True),
                                reads=[f"hb{b}", "c_identb", "Jb"], writes=[f"ps{pbk}"])
                        yield
                        kb.op("dve" if half == 0 else "act", (lambda e, half=half, pos=pos, pbk=pbk: e.tensor_copy(
                            out=hT[:, half * 4:(half + 1) * 4, pos:pos + 128],
                            in_=ps[pbk][:].rearrange("p (q t) -> p q t", q=4))) if half == 0 else
                            (lambda e, half=half, pos=pos, pbk=pbk: e.copy(
                                out=hT[:, half * 4:(half + 1) * 4, pos:pos + 128],
                                in_=ps[pbk][:].rearrange("p (q t) -> p q t", q=4))),
                            reads=[f"ps{pbk}"], writes=[("hT", pos, half)])
                        yield
                pending = [htile(i) for i in range(nt)]
                active = []
                while pending or active:
                    if pending and len(active) < 2:
                        active.append(pending.pop(0))
                    for g_ in list(active):
                        try:
                            next(g_)
                        except StopIteration:
                            active.remove(g_)
            kb.barrier()

            def store_cb_factory(col0, dk_):
                def cb(ci, Ysb, yk):
                    r0 = ci * CHUNK
                    kb.dma("sp", Ys[d, r0:r0 + CHUNK, col0:col0 + dk_], Ysb[0:CHUNK, 0:dk_], reads=[yk],
                           writes=[("ys", d, ci, col0)])
                    if col0 < 512:
                        kb.dma("sp", Ys[d, r0:r0 + CHUNK, 1024 + col0:1024 + col0 + dk_], Ysb[0:CHUNK, dk_:2 * dk_],
                               reads=[yk], writes=[("ysb", d, ci, col0)])
                return cb

            project(1536 + d * 128, 128, F[0], FK[0])
            tshift(F[0], FK[0], F[10], FK[10], F[1], FK[1], 128, par128[:, d:d + 1])
            ew("act", lambda e: e.activation(out=F[10][0:64, :], in_=F[10][0:64, :], func=AF.Tanh), [FK[10]], [FK[10]])
            if d == 0:
                project(1792, 128, F[0], FK[0])
                tshift(F[0], FK[0], F[11], FK[11], F[1], FK[1], 128, par128[:, 2:3])
                ew("act", lambda e: e.activation(out=F[11][:], in_=F[11][:], func=AF.Sigmoid), [FK[11]], [FK[11]])
            for p in range(4):
                hk_ = f"pair{d}_{p}"
                PP = lambda j: parP[:, j:j + 1]
                for part, dst in ((0, 1), (1, 2), (2, 3)):
                    project(p * 384 + part * 128, 128, F[0], FK[0])
                    tshift(F[0], FK[0], F[dst], FK[dst], F[7], FK[7], 128, PP(p * 3 + part))
                for bi, (t0, tw, col0) in enumerate(BLOCKS):
                    cs = slice(col0, col0 + tw)
                    kb.op("pe", lambda e, cs=cs, tw=tw: e.matmul(ps[2][:, 0:tw], lhsT=w2sb[0:64, d, p * 128:(p + 1) * 128],
                                                               rhs=F[10][0:64, cs], start=True, stop=True),
                          reads=["w2sb", FK[10]], writes=["ps2"])
                    kb.op("pe", lambda e, cs=cs, tw=tw: e.matmul(ps[3][:, 0:tw], lhsT=w2sb[64:128, d, p * 128:(p + 1) * 128],
                                                               rhs=F[10][64:128, cs], start=True, stop=True),
                          reads=["w2sb", FK[10]], writes=["ps3"])
                    kb.op("act", lambda e, cs=cs, tw=tw: e.activation(out=F[4][:, cs], in_=ps[2][:, 0:tw],
                                                                    func=AF.Sigmoid, bias=PP(12 + d * 4 + p)),
                          reads=["ps2", "parP"], writes=[FK[4]])
                    kb.op("act", lambda e, cs=cs, tw=tw: e.activation(out=F[5][:, cs], in_=ps[3][:, 0:tw],
                                                                    func=AF.Sigmoid, bias=PP(20 + d * 4 + p)),
                          reads=["ps3", "parP"], writes=[FK[5]])
                kkc, kac, kac1, rkc = PP(28 + p), PP(32 + p), PP(36 + p), PP(40 + p)
                ew("dve", lambda e: e.tensor_scalar(out=F[7][:, :], in0=F[2][:, :], scalar1=kkc, scalar2=None,
                                                    op0=ALU.mult), [FK[2], "parP"], [FK[7]])
                for (a0_, n_) in DC:
                    ew("pool", lambda e, a0_=a0_, n_=n_: e.tensor_tensor(
                        out=F[0][:, a0_:a0_ + n_], in0=F[7][:, a0_:a0_ + n_], in1=F[7][:, a0_:a0_ + n_],
                        op=ALU.mult), [FK[7]], [FK[0]])
                for bi, (t0, tw, col0) in enumerate(BLOCKS):
                    cs = slice(col0, col0 + tw)
                    kb.op("pe", lambda e, cs=cs, tw=tw: e.matmul(ps[2][:, 0:tw], lhsT=bd[:, :],
                                                               rhs=F[0][:, cs], start=True, stop=True),
                          reads=["bd", FK[0]], writes=["ps2"])
                    kb.op("act", lambda e, cs=cs, tw=tw: e.activation(out=F[9][:, cs], in_=ps[2][:, 0:tw],
                                                                    func=AF.Sqrt, bias=c.eps6[:, 0:1]),
                          reads=["ps2", "c_eps"], writes=[FK[9]])
                for (a0_, n_) in DC:
                    cs = slice(a0_, a0_ + n_)
                    ew("dve", lambda e, cs=cs: e.reciprocal(out=F[9][:, cs], in_=F[9][:, cs]), [FK[9]], [FK[9]])
                    ew("dve", lambda e, cs=cs: e.tensor_tensor(out=F[6][:, cs], in0=F[7][:, cs], in1=F[9][:, cs],
                                                               op=ALU.mult), [FK[7], FK[9]], [FK[6]])
                    ew("pool", lambda e, cs=cs: e.tensor_scalar(out=F[7][:, cs], in0=F[5][:, cs], scalar1=kac,
                                                                scalar2=kac1, op0=ALU.mult, op1=ALU.add),
                       [FK[5], "parP"], [FK[7]])
                    ew("pool", lambda e, cs=cs: e.tensor_tensor(out=F[7][:, cs], in0=F[7][:, cs], in1=F[2][:, cs],
                                                                op=ALU.mult), [FK[7], FK[2]], [FK[7]])
                    ew("dve", lambda e, cs=cs: e.tensor_tensor(out=F[9][:, cs], in0=F[6][:, cs], in1=F[5][:, cs],
                                                               op=ALU.mult), [FK[6], FK[5]], [FK[9]])
                    ew("dve", lambda e, cs=cs: e.scalar_tensor_tensor(out=F[0][:, cs], in0=F[1][:, cs], scalar=rkc,
                                                                      in1=F[7][:, cs], op0=ALU.mult, op1=ALU.mult),
                       [FK[1], FK[7], "parP"], [FK[0]])
                ew("dve", lambda e: e.tensor_tensor_scan(out=F[8][:, :], data0=rmask[:, :], data1=F[4][:, :],
                                                         initial=0.0, op0=ALU.mult, op1=ALU.add),
                   ["rmask", FK[4]], [FK[8]])
                B4 = F[4][:, :].bitcast(BF16)
                B5 = F[5][:, :].bitcast(BF16)
                B2 = F[2][:, :].bitcast(BF16)
                DCS = [slice(a0_, a0_ + n_) for (a0_, n_) in DC]
                DCH = [slice(TP + a0_, TP + a0_ + n_) for (a0_, n_) in DC]
                for cs in DCS:
                    ew("pool", lambda e, cs=cs: e.tensor_tensor(out=F[2][:, cs], in0=F[8][:, cs], in1=F[4][:, cs],
                                                                op=ALU.subtract), [FK[8], FK[4]], [FK[2]])
                    ew("act", lambda e, cs=cs: e.activation(out=F[2][:, cs], in_=F[2][:, cs], func=AF.Exp,
                                                            scale=-DECAY_K), [FK[2]], [FK[2]])
                for cs in DCS:
                    ew("dve", lambda e, cs=cs: e.tensor_tensor(out=B4[:, cs], in0=F[6][:, cs], in1=F[2][:, cs],
                                                               op=ALU.mult), [FK[6], FK[2]], [FK[4]])
                for cs in DCS:
                    ew("act", lambda e, cs=cs: e.activation(out=F[2][:, cs], in_=F[8][:, cs], func=AF.Exp,
                                                            scale=DECAY_K), [FK[8], FK[2], FK[4]], [FK[2]])
                for cs, ch in zip(DCS, DCH):
                    ew("dve", lambda e, cs=cs, ch=ch: e.tensor_tensor(out=B4[:, ch], in0=F[7][:, cs], in1=F[2][:, cs],
                                                                      op=ALU.mult), [FK[7], FK[2]], [FK[4]])
                    ew("pool", lambda e, cs=cs: e.tensor_tensor(out=B5[:, cs], in0=F[9][:, cs], in1=F[2][:, cs],
                                                                op=ALU.mult), [FK[9], FK[2]], [FK[5]])
                for cs in DCS:
                    ew("act", lambda e, cs=cs: e.activation(out=F[8][:, cs], in_=F[8][:, cs], func=AF.Exp,
                                                            scale=-DECAY_K), [FK[8]], [FK[8]])
                for cs, ch in zip(DCS, DCH):
                    ew("dve", lambda e, cs=cs: e.tensor_tensor(out=B2[:, cs], in0=F[1][:, cs], in1=F[8][:, cs],
                                                               op=ALU.mult), [FK[1], FK[8], FK[4], FK[5]], [FK[2]])
                    ew("pool", lambda e, cs=cs, ch=ch: e.tensor_copy(out=B5[:, ch], in_=F[3][:, cs]), [FK[3]], [FK[5]])
                    ew("act", lambda e, cs=cs, ch=ch: e.copy(out=B2[:, ch], in_=F[0][:, cs]), [FK[0]], [FK[2]])
                HI = lambda B: B[:, TP:2 * TP]
                kb.op("pool", lambda e: e.memset(T["nR"][:], 0.0),
                      reads=[FK[2], FK[4], FK[5], FK[8]], writes=[hk_, "nR"])
                for hh in range(2):
                    dplr_scan(kb, nc, c, T, 64, B2[:, 0:TP], B4[:, 0:TP], HI(B4), B5[:, 0:TP], HI(B5), F[8], HI(B2),
                              store_cb_factory((2 * p + hh) * 64, 64), hk_, bp=hh * 64)
                kb.op("pool", lambda e: e.memset(T["nR"][:], 0.0), reads=[hk_],
                      writes=[FK[2], FK[4], FK[5], FK[8], "nR"])
            mixer_gdn_pass(kb, nc, g, c, T, d, F, FK, rmask, par128, sel, project, store_cb_factory, ps, DC, wv, wsl,
                           hT, Zs)
        kb.barrier()
        es2.close()
        import os
        if os.environ.get("MIX_DUMP"):
            kb.dma("sp", x_lat_out[0:2048, :], Ys[0, 0:2048, 0:1024], writes=["o1"])
            kb.dma("sp", x_ctx_out[0:256, :], Ys[0, 0:256, 512:1536], writes=["o2"])
            kb.barrier()
            return
        mixer_output(kb, nc, g, c, mods, T, F, FK, x_lat_in, x_ctx_in, x_lat_out, x_ctx_out, Ys, Zs, J32, None, None, hb,
                     ps, par128)
        kb.barrier()


def mixer_gdn_pass(kb, nc, g, c, T, d, F, FK, rmask, par128, sel, project, store_cb_factory, ps, DC, wv, wsl, hT, Zs):
    ew = lambda eng, fn, r, w: kb.op(eng, fn, reads=r, writes=w)
    project(3968, 16, F[10], FK[10])
    R16 = lambda i: F[i][0:16, :]
    ew("act", lambda e: e.activation(out=T["Uf"][0:16, 0:1], in_=par128[0:16, 41:42], func=AF.Exp), ["par128"], ["U"])
    ew("dve", lambda e: e.tensor_scalar(out=T["Uf"][0:16, 0:1], in0=T["Uf"][0:16, 0:1], scalar1=-1.0, scalar2=None,
                                        op0=ALU.mult), ["U"], ["U"])
    ew("dve", lambda e: e.tensor_scalar(out=R16(1), in0=R16(10), scalar1=par128[0:16, 40:41], scalar2=None, op0=ALU.add),
       [FK[10], "par128"], [FK[1]])
    ew("act", lambda e: e.activation(out=R16(4), in_=R16(10), func=AF.Sigmoid), [FK[10]], [FK[4]])
    ew("act", lambda e: e.activation(out=R16(2), in_=R16(1), func=AF.Abs), [FK[1]], [FK[2]])
    ew("act", lambda e: e.activation(out=R16(2), in_=R16(2), func=AF.Exp, scale=-1.0), [FK[2]], [FK[2]])
    ew("dve", lambda e: e.tensor_scalar(out=R16(3), in0=R16(2), scalar1=2.0, scalar2=None, op0=ALU.add), [FK[2]], [FK[3]])
    ew("dve", lambda e: e.reciprocal(out=R16(3), in_=R16(3)), [FK[3]], [FK[3]])
    ew("dve", lambda e: e.tensor_tensor(out=R16(2), in0=R16(2), in1=R16(3), op=ALU.mult), [FK[2], FK[3]], [FK[2]])
    ew("dve", lambda e: e.tensor_tensor(out=R16(3), in0=R16(2), in1=R16(2), op=ALU.mult), [FK[2]], [FK[3]])
    ew("dve", lambda e: e.tensor_scalar(out=R16(6), in0=R16(3), scalar1=1.0 / 13, scalar2=1.0 / 11, op0=ALU.mult,
                                        op1=ALU.add), [FK[3]], [FK[6]])
    for cf in (1.0 / 9, 1.0 / 7, 1.0 / 5, 1.0 / 3, 1.0):
        ew("dve", lambda e: e.tensor_tensor(out=R16(6), in0=R16(6), in1=R16(3), op=ALU.mult), [FK[6], FK[3]], [FK[6]])
        ew("dve", lambda e, cf=cf: e.tensor_scalar(out=R16(6), in0=R16(6), scalar1=cf, scalar2=None, op0=ALU.add),
           [FK[6]], [FK[6]])
    ew("dve", lambda e: e.scalar_tensor_tensor(out=R16(6), in0=R16(6), scalar=2.0, in1=R16(2), op0=ALU.mult, op1=ALU.mult),
       [FK[6], FK[2]], [FK[6]])
    ew("dve", lambda e: e.tensor_scalar(out=R16(1), in0=R16(1), scalar1=0.0, scalar2=None, op0=ALU.max), [FK[1]], [FK[1]])
    ew("dve", lambda e: e.tensor_tensor(out=R16(6), in0=R16(6), in1=R16(1), op=ALU.add), [FK[6], FK[1]], [FK[6]])
    ew("dve", lambda e: e.tensor_scalar(out=R16(6), in0=R16(6), scalar1=T["Uf"][0:16, 0:1], scalar2=None, op0=ALU.mult),
       [FK[6], "U"], [FK[6]])
    ew("dve", lambda e: e.tensor_tensor_scan(out=R16(10), data0=rmask[0:16, :], data1=R16(6), initial=0.0,
                                             op0=ALU.mult, op1=ALU.add), ["rmask", FK[6]], [FK[10]])
    for h in range(4):
        hk_ = f"ghead{d}_{h}"
        c0 = 1920 + h * 512
        for part, dst in ((0, 1), (1, 2), (2, 3)):
            project(c0 + part * 128, 128, F[0], FK[0])
            n = TP - 4
            for j in range(5):
                jj = j if d == 0 else 4 - j
                wc = par128[:, 48 + (h * 3 + part) * 5 + jj:49 + (h * 3 + part) * 5 + jj]
                if j == 0:
                    ew("dve", lambda e, wc=wc, dst=dst: e.tensor_scalar(out=F[dst][:, 2:2 + n], in0=F[0][:, 0:n],
                                                                      scalar1=wc, scalar2=None, op0=ALU.mult),
                       [FK[0], "par128"], [FK[dst]])
                else:
                    ew("dve", lambda e, wc=wc, dst=dst, j=j: e.scalar_tensor_tensor(
                        out=F[dst][:, 2:2 + n], in0=F[0][:, j:j + n], scalar=wc, in1=F[dst][:, 2:2 + n],
                        op0=ALU.mult, op1=ALU.add), [FK[0], FK[dst], "par128"], [FK[dst]])
            ew("act", lambda e, dst=dst: e.activation(out=F[dst][:, :], in_=F[dst][:, :], func=AF.Silu), [FK[dst]], [FK[dst]])
        for src, scl in ((1, float(128 ** -0.5)), (2, 1.0)):
            for (a0_, n_) in DC:
                ew("pool", lambda e, src=src, a0_=a0_, n_=n_: e.tensor_tensor(
                    out=F[0][:, a0_:a0_ + n_], in0=F[src][:, a0_:a0_ + n_], in1=F[src][:, a0_:a0_ + n_], op=ALU.mult),
                    [FK[src]], [FK[0]])
            for bi, (t0, tw, col0) in enumerate(BLOCKS):
                cs = slice(col0, col0 + tw)
                kb.op("pe", lambda e, cs=cs, tw=tw: e.matmul(ps[2][:, 0:tw], lhsT=c.ones[:, :], rhs=F[0][:, cs],
                                                           start=True, stop=True), reads=["c_ones", FK[0]], writes=["ps2"])
                kb.op("act", lambda e, cs=cs, tw=tw: e.activation(out=F[9][:, cs], in_=ps[2][:, 0:tw], func=AF.Sqrt,
                                                                bias=c.eps6[:, 0:1]), reads=["ps2", "c_eps"], writes=[FK[9]])
            for (a0_, n_) in DC:
                cs = slice(a0_, a0_ + n_)
                ew("dve", lambda e, cs=cs: e.reciprocal(out=F[9][:, cs], in_=F[9][:, cs]), [FK[9]], [FK[9]])
                ew("dve", lambda e, cs=cs, src=src, scl=scl: e.scalar_tensor_tensor(
                    out=F[src][:, cs], in0=F[src][:, cs], scalar=scl, in1=F[9][:, cs], op0=ALU.mult, op1=ALU.mult),
                    [FK[src], FK[9]], [FK[src]])
        ra, rb = d * 4 + h, 8 + d * 4 + h
        for bi, (t0, tw, col0) in enumerate(BLOCKS):
            cs = slice(col0, col0 + tw)
            kb.op("pe", lambda e, cs=cs, tw=tw: e.matmul(ps[2][:, 0:tw], lhsT=sel[0:16, ra, :], rhs=F[10][0:16, cs],
                                                       start=True, stop=True), reads=["sel", FK[10]], writes=["ps2"])
            kb.op("pe", lambda e, cs=cs, tw=tw: e.matmul(ps[3][:, 0:tw], lhsT=sel[0:16, rb, :], rhs=F[4][0:16, cs],
                                                       start=True, stop=True), reads=["sel", FK[4]], writes=["ps3"])
            kb.op("act", lambda e, cs=cs, tw=tw: e.activation(out=F[6][:, cs], in_=ps[2][:, 0:tw], func=AF.Exp),
                  reads=["ps2"], writes=[FK[6]])
            kb.op("dve", lambda e, cs=cs, tw=tw: e.tensor_copy(out=F[0][:, cs], in_=ps[2][:, 0:tw]),
                  reads=["ps2"], writes=[FK[0]])
            kb.op("act", lambda e, cs=cs, tw=tw: e.copy(out=F[9][:, cs], in_=ps[3][:, 0:tw]),
                  reads=["ps3"], writes=[FK[9]])
        GB5 = F[5][:, :].bitcast(BF16)
        for (a0_, n_) in DC:
            cs = slice(a0_, a0_ + n_)
            ew("dve", lambda e, cs=cs: e.tensor_tensor(out=F[9][:, cs], in0=F[9][:, cs], in1=F[2][:, cs], op=ALU.mult),
               [FK[9], FK[2]], [FK[9]])
            ew("pool", lambda e, cs=cs: e.tensor_tensor(out=GB5[:, cs], in0=F[2][:, cs], in1=F[6][:, cs], op=ALU.mult),
               [FK[2], FK[6]], [FK[5]])
            ew("dve", lambda e, cs=cs: e.tensor_tensor(out=GB5[:, TP + cs.start:TP + cs.stop], in0=F[1][:, cs],
                                                       in1=F[6][:, cs], op=ALU.mult),
               [FK[1], FK[6]], [FK[5]])
        kb.op("pool", lambda e: e.memset(T["nR"][:], 0.0), reads=[FK[0], FK[1], FK[2], FK[3], FK[5], FK[6], FK[9]],
              writes=[hk_, "nR"])
        dplr_scan(kb, nc, c, T, 128, F[1], F[2], F[9], F[9], F[3], F[6], None, store_cb_factory(512 + h * 128, 128), hk_,
                  Gb=F[0], rA=F[1], kkA=F[2], rC=GB5[:, TP:2 * TP], kkC=GB5[:, 0:TP])
        kb.op("pool", lambda e: e.memset(T["nR"][:], 0.0), reads=[hk_],
              writes=[FK[0], FK[1], FK[2], FK[3], FK[5], FK[6], FK[9], "nR"])
    if d == 0:
        n = 0
        for h in range(4):
            kb.dma("pool", wsl[:, :, 0:128], wv[:, :, 1920 + h * 512 + 384:1920 + h * 512 + 512], writes=["wsl"])
            for i in range(18):
                pb = ps[n % 2]
                for kc in range(8):
                    kb.op("pe", lambda e, kc=kc, i=i, pb=pb: e.matmul(pb[:, 0:128], lhsT=hT[:, kc, i * 128:(i + 1) * 128],
                                                                   rhs=wsl[:, kc, 0:128], start=(kc == 0), stop=(kc == 7)),
                          reads=["wsl", "hT"], writes=[f"ps{n % 2}"])
                zt = T["zt"][n % 2]
                kb.op("act", lambda e, pb=pb, zt=zt: e.activation(out=zt[:], in_=pb[:, 0:128], func=AF.Silu),
                      reads=[f"ps{n % 2}"], writes=[f"zt{n % 2}"])
                kb.dma("sp", Zs[i * 128:(i + 1) * 128, h * 128:(h + 1) * 128], zt[:], reads=[f"zt{n % 2}"],
                       writes=[("zs", i, h)])
                n += 1


def mixer_output(kb, nc, g, c, mods, T, F, FK, x_lat_in, x_ctx_in, x_lat_out, x_ctx_out, Ys, Zs, J32, xb, Gbc, hb, ps,
                 par128):
    ew = lambda eng, fn, r, w: kb.op(eng, fn, reads=r, writes=w)
    kb.barrier()
    from contextlib import ExitStack
    with ExitStack() as es:
        def sb(name, shape, dt):
            return es.enter_context(nc.sbuf_tensor(f"mo_{name}", shape, dt))
        wo = sb("wo", [128, 8, D], BF16)
        g2 = sb("g2", [128, 512], F32)
        bcp = sb("bcp", [128, 3, 512], F32)
        yf = sb("yf", [128, 1536], F32)
        yb = sb("yb", [128, 1536], F32)
        zt = sb("zt", [128, 512], F32)
        o = sb("o", [128, D], F32)
        t1 = sb("t1", [128, 512], F32)
        s8 = sb("s8", [128, 4, 8], F32)
        oT = sb("oT", [128, 8, 128], BF16)
        Gbc = sb("Gbc", [128, D], F32)
        xb = [sb("x0", [128, D], F32), sb("x1", [128, D], F32)]
        for kc in range(8):
            kb.dma("pool", wo[:, kc, :], g["e_w_out"][0].rearrange("(kc p) n -> p kc n", p=128)[:, kc, :], writes=["wo"])
        kb.dma("sp", g2[:], g["a_g2"][0], writes=["g2"])
        kb.dma("sp", bcp[:].rearrange("p a n -> p (a n)"), g["mx_bc"].partition_broadcast(128), writes=["bcp"])
        for stream in (1, 0):
            kb.barrier()
            make_bc(kb, nc, c, lambda kc: mods[0][:, 2 * 8 + kc, stream:stream + 1], Gbc, ps[0], "Gbc", ["mod0"])
            nt = 2 if stream == 1 else 16
            src = x_ctx_in if stream == 1 else x_lat_in
            dst = x_ctx_out if stream == 1 else x_lat_out
            base = 0 if stream == 1 else 256
            colbase = CTX0 if stream == 1 else LAT0
            for i in range(nt):
                b = i % 2
                r0 = base + i * 128
                rb0 = base + (nt - 1 - i) * 128
                kb.dma("sp", xb[b][:], src[i * 128:(i + 1) * 128, :], writes=[f"xb{b}"])
                kb.dma("sp", yf[:], Ys[0, r0:r0 + 128, :], reads=[("ysall",)], writes=["yf"])
                kb.dma("act", yb[:], Ys[1, rb0:rb0 + 128, :], reads=[("ysall",)], writes=["yb"])
                kb.dma("sp", zt[:], Zs[r0:r0 + 128, :], reads=[("ysall",)], writes=["zt"])
                for q in range(3):
                    kb.op("pe", lambda e, q=q: e.matmul(ps[1 + q][:, :], lhsT=J32[:, :], rhs=yb[:, q * 512:(q + 1) * 512],
                                                      start=True, stop=True), reads=["J32", "yb"], writes=[f"ps{1 + q}"])
                    ew("dve", lambda e, q=q: e.tensor_tensor(out=yf[:, q * 512:(q + 1) * 512], in0=yf[:, q * 512:(q + 1) * 512],
                                                             in1=ps[1 + q][:, :], op=ALU.add), [f"ps{1 + q}", "yf"], ["yf"])
                y3 = yf[:, 0:512].rearrange("p (h j) -> p h j", h=8)
                ew("dve", lambda e: e.reduce_sum(out=s8[:, 0, :], in_=y3, axis=AX.X), ["yf"], ["s8"])
                ew("dve", lambda e: e.tensor_scalar(out=s8[:, 0, :], in0=s8[:, 0, :], scalar1=-1.0 / 64, scalar2=None,
                                                    op0=ALU.mult), ["s8"], ["s8"])
                ew("dve", lambda e: e.tensor_tensor(out=y3, in0=y3, in1=s8[:, 0, :].unsqueeze(2).broadcast_to([128, 8, 64]),
                                                    op=ALU.add), ["yf", "s8"], ["yf"])
                ew("pool", lambda e: e.tensor_tensor(out=t1[:], in0=yf[:, 0:512], in1=yf[:, 0:512], op=ALU.mult), ["yf"], ["t1"])
                ew("dve", lambda e: e.reduce_sum(out=s8[:, 1, :], in_=t1[:].rearrange("p (h j) -> p h j", h=8), axis=AX.X),
                   ["t1"], ["s8"])
                ew("dve", lambda e: e.tensor_scalar(out=s8[:, 1, :], in0=s8[:, 1, :], scalar1=1.0 / 64, scalar2=64e-5,
                                                    op0=ALU.mult, op1=ALU.add), ["s8"], ["s8"])
                ew("act", lambda e: e.activation(out=s8[:, 1, :], in_=s8[:, 1, :], func=AF.Sqrt), ["s8"], ["s8"])
                ew("dve", lambda e: e.reciprocal(out=s8[:, 1, :], in_=s8[:, 1, :]), ["s8"], ["s8"])
                ew("dve", lambda e: e.tensor_tensor(out=y3, in0=y3, in1=s8[:, 1, :].unsqueeze(2).broadcast_to([128, 8, 64]),
                                                    op=ALU.mult), ["yf", "s8"], ["yf"])
                ew("dve", lambda e: e.tensor_tensor(out=yf[:, 0:512], in0=yf[:, 0:512], in1=bcp[:, 0, :], op=ALU.mult),
                   ["yf", "bcp"], ["yf"])
                ew("pool", lambda e: e.tensor_tensor(out=yf[:, 0:512], in0=yf[:, 0:512], in1=bcp[:, 1, :], op=ALU.add),
                   ["yf", "bcp"], ["yf"])
                ew("pool", lambda e: e.tensor_tensor(out=yf[:, 0:512], in0=yf[:, 0:512], in1=yf[:, 1024:1536], op=ALU.add),
                   ["yf"], ["yf"])
                cg = colbase + i * 128
                kb.op("pe", lambda e, cg=cg: e.matmul(ps[4][:, :], lhsT=F[11][:, cg:cg + 128], rhs=g2[:, :], start=True, stop=True),
                      reads=[FK[11], "g2"], writes=["ps4"])
                ew("dve", lambda e: e.tensor_tensor(out=o[:, 0:512], in0=yf[:, 0:512], in1=ps[4][:, :], op=ALU.mult),
                   ["yf", "ps4"], ["o"])
                ew("pool", lambda e: e.tensor_tensor(out=t1[:], in0=yf[:, 512:1024], in1=yf[:, 512:1024], op=ALU.mult),
                   ["yf"], ["t1"])
                ew("dve", lambda e: e.reduce_sum(out=s8[:, 2, 0:4], in_=t1[:].rearrange("p (h j) -> p h j", h=4), axis=AX.X),
                   ["t1"], ["s8"])
                ew("dve", lambda e: e.tensor_scalar(out=s8[:, 2, 0:4], in0=s8[:, 2, 0:4], scalar1=1.0 / 128, scalar2=EPS,
                                                    op0=ALU.mult, op1=ALU.add), ["s8"], ["s8"])
                ew("act", lambda e: e.activation(out=s8[:, 2, 0:4], in_=s8[:, 2, 0:4], func=AF.Sqrt), ["s8"], ["s8"])
                ew("dve", lambda e: e.reciprocal(out=s8[:, 2, 0:4], in_=s8[:, 2, 0:4]), ["s8"], ["s8"])
                ew("dve", lambda e: e.tensor_tensor(
                    out=t1[:].rearrange("p (h j) -> p h j", h=4), in0=yf[:, 512:1024].rearrange("p (h j) -> p h j", h=4),
                    in1=s8[:, 2, 0:4].unsqueeze(2).broadcast_to([128, 4, 128]), op=ALU.mult), ["yf", "s8"], ["t1"])
                ew("pool", lambda e: e.tensor_tensor(out=t1[:], in0=t1[:], in1=bcp[:, 2, :], op=ALU.mult), ["t1", "bcp"], ["t1"])
                ew("dve", lambda e: e.tensor_tensor(out=o[:, 512:1024], in0=t1[:], in1=zt[:], op=ALU.mult), ["t1", "zt"], ["o"])
                ew("act", lambda e: e.copy(out=hb[:], in_=o[:]), ["o"], ["hb"])
                pst = ps[0][:].bitcast(BF16)
                for kc in range(8):
                    kb.op("pe", lambda e, kc=kc, pst=pst: e.transpose(out=pst[:, kc * 128:(kc + 1) * 128],
                                                                    in_=hb[:, kc * 128:(kc + 1) * 128], identity=c.identb[:]),
                          reads=["hb", "c_identb"], writes=["ps0"])
                ew("act", lambda e, pst=pst: e.copy(out=oT[:], in_=pst[:].rearrange("p (k t) -> p k t", k=8)), ["ps0"], ["oT"])
                for half in range(2):
                    pp = ps[5 + half]
                    for kc in range(8):
                        kb.op("pe", lambda e, kc=kc, half=half, pp=pp: e.matmul(
                            pp[:], lhsT=oT[:, kc, :], rhs=wo[:, kc, half * 512:(half + 1) * 512], start=(kc == 0), stop=(kc == 7)),
                            reads=["oT", "wo"], writes=[f"ps{5 + half}"])
                    sl = slice(half * 512, (half + 1) * 512)
                    ew("dve", lambda e, pp=pp, sl=sl: e.tensor_tensor(out=t1[:], in0=pp[:], in1=Gbc[:, sl], op=ALU.mult),
                       [f"ps{5 + half}", "Gbc"], ["t1"])
                    ew("pool", lambda e, sl=sl, b=b: e.tensor_tensor(out=xb[b][:, sl], in0=xb[b][:, sl], in1=t1[:], op=ALU.add),
                       ["t1", f"xb{b}"], [f"xb{b}"])
                kb.dma("sp", dst[i * 128:(i + 1) * 128, :], xb[b][:], reads=[f"xb{b}"], writes=[("dram", dst.tensor.name, i)])
        kb.barrier()


_CACHE = {}


def kernel(**inputs):
    inp = {k: np.asarray(v) for k, v in inputs.items()}
    if "nc" not in _CACHE:
        _CACHE["nc"] = build(stages=("all",))[0]
    nc = _CACHE["nc"]
    in_maps = [host_inputs(inp, b) for b in range(8)]
    res = run_bass_kernel_spmd(nc, in_maps, core_ids=list(range(8)))
    return np.stack([np.asarray(r["out"], dtype=np.float32) for r in res.results], axis=0)
```

```python
import numpy as np
import concourse.bass as bass
import concourse.mybir as mybir
from concourse.bass_utils import run_bass_kernel_spmd

F32 = mybir.dt.float32
BF16 = mybir.dt.bfloat16
I32 = mybir.dt.int32
U32 = mybir.dt.uint32
ALU = mybir.AluOpType
AF = mybir.ActivationFunctionType
AX = mybir.AxisListType

SEM_ROTATE = 20000
N_DMA_SEMS = 28
N_HW_SEMS = 18


class KB:
    def __init__(self, nc, same_engine_sync=True):
        self.nc = nc
        self.engs = {"pe": nc.tensor, "act": nc.scalar, "dve": nc.vector, "pool": nc.gpsimd, "sp": nc.sync}
        self.same_engine_sync = same_engine_sync
        self.esem = {}
        self.ecnt = {}
        self.sem_id = 0
        for e in ("pe", "act", "dve", "pool"):
            self._new_esem(e)
        self.dsems = [self._alloc_sem(f"dma{i}") for i in range(N_DMA_SEMS)]
        self.dcnt = [0] * N_DMA_SEMS
        self.dnext = 0
        self.dnext_sw = 0
        self.known = {e: {} for e in self.engs}
        self.state = {}
        self.n_ins = 0
        self._uid = 0
        self.out_tokens = []

    def _alloc_sem(self, name):
        self.sem_id += 1
        return self.nc.alloc_semaphore(f"{name}_{self.sem_id}")

    def _new_esem(self, e):
        self.esem[e] = self._alloc_sem(f"s_{e}")
        self.ecnt[e] = 0

    def uid(self, p="t"):
        self._uid += 1
        return f"{p}{self._uid}"

    def _deps(self, reads, writes):
        deps = []
        for r in reads:
            st = self.state.get(r)
            if st and st[0] is not None:
                deps.append(st[0])
        for w in writes:
            st = self.state.get(w)
            if st:
                if st[0] is not None:
                    deps.append(st[0])
                deps.extend(st[1].values())
        return deps

    def _wait(self, e, deps):
        eng = self.engs[e]
        kn = self.known[e]
        best = {}
        for (sem, val, src) in deps:
            if src == e and not (self.same_engine_sync and e != "pe"):
                continue
            key = id(sem)
            if kn.get(key, 0) >= val:
                continue
            if key not in best or best[key][1] < val:
                best[key] = (sem, val)
        for key, (sem, val) in best.items():
            eng.wait_ge(sem, val)
            kn[key] = val
            self.n_ins += 1

    def _commit(self, token, reads, writes):
        for w in writes:
            self.state[w] = [token, {}]
        for r in reads:
            st = self.state.get(r)
            if st is None:
                st = [None, {}]
                self.state[r] = st
            st[1][id(token[0])] = token

    def op(self, e, fn, reads=(), writes=()):
        reads = list(reads)
        writes = list(writes)
        writes += [r for r in reads if isinstance(r, str) and r.startswith("ps")]
        self._wait(e, self._deps(reads, writes))
        if self.ecnt[e] >= SEM_ROTATE:
            self._new_esem(e)
        ins = fn(self.engs[e])
        self.ecnt[e] += 1
        ins.then_inc(self.esem[e], 1)
        token = (self.esem[e], self.ecnt[e], e)
        self._commit(token, reads, writes)
        self.n_ins += 1
        return token

    def dma(self, q, out, in_, reads=(), writes=(), **kw):
        reads = list(reads)
        writes = list(writes)
        if q == "pool":
            i = N_HW_SEMS + self.dnext_sw
            self.dnext_sw = (self.dnext_sw + 1) % (N_DMA_SEMS - N_HW_SEMS)
        else:
            i = self.dnext
            self.dnext = (self.dnext + 1) % N_HW_SEMS
        deps = self._deps(reads, writes)
        if self.dcnt[i] > 0:
            deps.append((self.dsems[i], self.dcnt[i], "dma"))
        self._wait(q, deps)
        ins = self.engs[q].dma_start(out=out, in_=in_, **kw)
        self.dcnt[i] += 16
        ins.then_inc(self.dsems[i], 16)
        token = (self.dsems[i], self.dcnt[i], "dma")
        self._commit(token, reads, writes)
        self.n_ins += 1
        return token

    def finish(self, out_keys):
        deps = []
        for k in out_keys:
            st = self.state.get(k)
            if st and st[0] is not None:
                deps.append(st[0])
        self._wait("sp", deps)
        deps = [(self.dsems[i], self.dcnt[i], "dma") for i in range(N_DMA_SEMS) if self.dcnt[i] > 0]
        self._wait("sp", deps)

    def barrier(self):
        deps = [(self.esem[e], self.ecnt[e], "x") for e in self.esem if self.ecnt[e] > 0]
        deps += [(self.dsems[i], self.dcnt[i], "dma") for i in range(N_DMA_SEMS) if self.dcnt[i] > 0]
        for e in self.engs:
            self._wait(e, deps)
        self.state = {}


D = 1024
KC = 8
EPS = 1e-6
TP = 2310
CTX0, LAT0 = 2, 260
CHUNK = 128
CHUNK_COLS = [CTX0 + CHUNK * j for j in range(256 // CHUNK)] + [LAT0 + CHUNK * j for j in range(2048 // CHUNK)]
BLOCKS = [(0, 256, CTX0)] + [(256 + 512 * j, 512, LAT0 + 512 * j) for j in range(4)]
DECAY_K = float(np.exp(-0.5))


class Ctx:
    pass


def load_consts(kb, nc, es, g):
    c = Ctx()
    c.ident = es.enter_context(nc.sbuf_tensor("c_ident", [128, 128], F32))
    c.identb = es.enter_context(nc.sbuf_tensor("c_identb", [128, 128], BF16))
    c.ones = es.enter_context(nc.sbuf_tensor("c_ones", [128, 128], F32))
    c.iota = es.enter_context(nc.sbuf_tensor("c_iota", [128, 256], F32))
    kb.dma("sp", c.ident[:], g["k_ident"][:, :], writes=["c_ident"])
    kb.dma("sp", c.iota[:], g["k_iota"][:, :], writes=["c_iota"])
    kb.op("dve", lambda e: e.memset(c.ones[:], 1.0), writes=["c_ones"])
    c.eps6 = es.enter_context(nc.sbuf_tensor("c_eps6", [128, 1], F32))
    c.one1 = es.enter_context(nc.sbuf_tensor("c_one1", [128, 1], F32))
    kb.op("dve", lambda e: e.memset(c.eps6[:], 1e-6), writes=["c_eps"])
    kb.op("dve", lambda e: e.memset(c.one1[:], 1.0), writes=["c_eps"])
    c.onesb = es.enter_context(nc.sbuf_tensor("c_onesb", [128, 8], BF16))
    kb.op("dve", lambda e: e.memset(c.onesb[:], 1.0), writes=["c_ones"])
    kb.op("dve", lambda e: e.tensor_copy(out=c.identb[:], in_=c.ident[:]), reads=["c_ident"], writes=["c_identb"])
    return c


def prologue(kb, nc, es, g, c):
    mods = []
    for l in range(2):
        mods.append(es.enter_context(nc.sbuf_tensor(f"mod{l}", [128, 48, 2], F32)))
    with nc.sbuf_tensor("pl_sc", [128, 2, 8], F32) as sc, \
            nc.sbuf_tensor("pl_w0", [128, 8, 512], F32) as w0, \
            nc.sbuf_tensor("pl_w1", [128, 8, 512], F32) as w1, \
            nc.sbuf_tensor("pl_b", [128, 2, 48], F32) as adab, \
            nc.sbuf_tensor("pl_n", [128, 2, 2, 8], F32) as nrm, \
            nc.psum_tensor("pl_ps", [128, 512], F32) as ps:
        wb = [w0, w1]
        kb.dma("sp", sc[:, 0, :], g["cT"][:, :], writes=["sc"])
        kb.dma("sp", sc[:, 1, :], g["ccT"][:, :], writes=["sc"])
        kb.dma("sp", adab[:], g["ada_bT"][:, :, :], writes=["adab"])
        kb.dma("sp", nrm[:], g["normT"][:, :, :, :], writes=["nrm"])
        kb.op("act", lambda e: e.activation(out=sc[:], in_=sc[:], func=AF.Silu), reads=["sc"], writes=["sc"])
        blk = 0
        for l in range(2):
            wv = g["ada_w"][l].rearrange("(kc p) n -> p kc n", p=128)
            for nb in range(12):
                wt = wb[blk % 2]
                wk = f"plw{blk % 2}"
                kb.dma("sp" if blk % 2 == 0 else "act", wt[:], wv[:, :, nb * 512:(nb + 1) * 512], writes=[wk])
                for j in range(4):
                    for kc in range(8):
                        kb.op("pe", lambda e, kc=kc, j=j, wt=wt: e.matmul(
                            ps[:, (j * 2):(j * 2 + 2)], lhsT=wt[:, kc, j * 128:(j + 1) * 128], rhs=sc[:, :, kc],
                            start=(kc == 0), stop=(kc == 7)), reads=[wk, "sc"], writes=["psPL"])
                kb.op("dve", lambda e, l=l, nb=nb: e.tensor_tensor(
                    out=mods[l][:, nb * 4:(nb + 1) * 4, :],
                    in0=ps[:, 0:8].rearrange("p (j s) -> p j s", s=2),
                    in1=adab[:, l, nb * 4:(nb + 1) * 4].unsqueeze(2).broadcast_to([128, 4, 2]),
                    op=ALU.add), reads=["psPL", "adab"], writes=[f"mod{l}"])
                blk += 1
            for (m, which) in ((1, 0), (4, 1)):
                for s in range(2):
                    kb.op("dve", lambda e, l=l, m=m, which=which, s=s: e.scalar_tensor_tensor(
                        out=mods[l][:, m * 8:(m + 1) * 8, s], in0=mods[l][:, m * 8:(m + 1) * 8, s], scalar=1.0,
                        in1=nrm[:, l, which, :], op0=ALU.add, op1=ALU.mult),
                        reads=[f"mod{l}", "nrm"], writes=[f"mod{l}"])
    kb.barrier()
    return mods


def make_bc(kb, nc, c, col_ap_fn, out_tile, ps, key, src_keys):
    with nc.sbuf_tensor(kb.uid("bcd"), [128, 128], F32) as dg:
        dk = kb.uid("dg")
        for half in range(2):
            for q in range(4):
                kc = half * 4 + q
                kb.op("dve", lambda e, kc=kc: e.tensor_scalar(
                    out=dg[:], in0=c.ident[:], scalar1=col_ap_fn(kc), scalar2=None, op0=ALU.mult),
                    reads=["c_ident"] + src_keys, writes=[dk])
                kb.op("pe", lambda e, q=q: e.matmul(ps[:, q * 128:(q + 1) * 128], lhsT=c.ones[:], rhs=dg[:],
                                                   start=True, stop=True),
                      reads=[dk, "c_ones"], writes=["psBC" + key])
            kb.op("act", lambda e, half=half: e.copy(out=out_tile[:, half * 512:(half + 1) * 512], in_=ps[:]),
                  reads=["psBC" + key], writes=[key])
        kb.barrier()


def norm_tile_g(kb, nc, xt, xk, st, G, S, hout, hk, eps=EPS):
    sk = kb.uid("st")
    kb.op("act", lambda e: e.activation(out=hout, in_=xt, func=AF.Square, accum_out=st[:, 0:1]),
          reads=[xk], writes=[hk, sk])
    yield
    kb.op("dve", lambda e: e.tensor_scalar(out=st[:, 1:2], in0=st[:, 0:1], scalar1=1.0 / D, scalar2=eps,
                                           op0=ALU.mult, op1=ALU.add), reads=[sk], writes=[sk])
    yield
    kb.op("act", lambda e: e.activation(out=st[:, 2:3], in_=st[:, 1:2], func=AF.Sqrt), reads=[sk], writes=[sk])
    yield
    kb.op("dve", lambda e: e.reciprocal(out=st[:, 3:4], in_=st[:, 2:3]), reads=[sk], writes=[sk])
    yield
    if G is None:
        kb.op("dve", lambda e: e.tensor_scalar(out=hout, in0=xt, scalar1=st[:, 3:4], scalar2=None, op0=ALU.mult),
              reads=[xk, sk], writes=[hk])
        return
    kb.op("dve", lambda e: e.scalar_tensor_tensor(out=hout, in0=xt, scalar=st[:, 3:4], in1=G[:],
                                                  op0=ALU.mult, op1=ALU.mult),
          reads=[xk, sk, "Gbc"], writes=[hk])
    yield
    if S is not None:
        kb.op("pool", lambda e: e.tensor_tensor(out=hout, in0=hout, in1=S[:], op=ALU.add),
              reads=[hk, "Sbc"], writes=[hk])


def norm_tile(*a, **k):
    for _ in norm_tile_g(*a, **k):
        pass


def moe_stage(kb, nc, g, c, mods, layer, stream, x_in, x_out, T, final_norm=False, comp=None):
    NT = T // 128
    cap = 2 * T // 16
    CW = cap
    CT = (cap + 127) // 128
    cs = min(cap, 128)
    mod = mods[layer]
    xin_v = x_in.rearrange("(n p) d -> n p d", p=128)
    xout_v = x_out.rearrange("(n p) d -> n p d", p=128)
    sx = f"L{layer}s{stream}"
    from contextlib import ExitStack
    with ExitStack() as es:
        def sb(name, shape, dt):
            return es.enter_context(nc.sbuf_tensor(f"moe_{name}_{sx}", shape, dt))
        Gbc = sb("G", [128, D], F32)
        Sbc = sb("S", [128, D], F32)
        gate2 = Sbc
        hbf = sb("hbf", [128, NT, D], BF16)
        xb = [sb("x0", [128, D], F32), sb("x1", [128, D], F32)]
        h32 = [sb("h0", [128, D], F32), sb("h1", [128, D], F32)]
        hT = [sb("hT0", [128, 8, 128], F32), sb("hT1", [128, 8, 128], F32)]
        stt = [sb("st0", [128, 4], F32), sb("st1", [128, 4], F32)]
        rt = sb("rt", [128, 8, 16], F32)
        aff = sb("aff", [128, NT, 16], F32)
        sm = sb("sm", [128, 4], F32)
        ex = sb("ex", [128, 16], F32)
        affT = sb("affT", [16, T], F32)
        work = sb("work", [16, T], F32)
        mx8 = sb("mx8", [16, 8], F32)
        maskT = sb("maskT", [16, T], F32)
        onesT = work
        slotT = sb("slotT", [16, T], F32)
        gateT = affT
        slot = sb("slot", [128, NT, 16], F32)
        gate = sb("gate", [128, NT, 16], F32)
        selT = [sb("selT0", [128, CW], BF16), sb("selT1", [128, CW], BF16)]
        xeT = sb("xeT", [128, 8, CW], BF16)
        big = sb("big", [128, 4 * 8 * D], BF16)
        wviews = [big[:, j * 8 * D:(j + 1) * 8 * D].rearrange("p (k n) -> p k n", n=D) for j in range(4)]
        w1b = [wviews[0], wviews[1]]
        w3b = [wviews[2]]
        w2b = [wviews[3]]
        yest = [sb("yest0", [128, CT, D], BF16), sb("yest1", [128, CT, D], BF16)]
        sil = [sb("sil0", [128, CW], F32), sb("sil1", [128, CW], F32)]
        hidT = sb("hidT", [128, 8, CW], BF16)
        yeall = big[:, 0:16 * CT * D].rearrange("p (e c n) -> p e c n", e=16, c=CT)
        selGa = [sb("selGa0", [128, 4, CW], BF16), sb("selGa1", [128, 4, CW], BF16)]
        selGca = [sb("selGca0", [128, 4 * CT, 128], BF16), sb("selGca1", [128, 4 * CT, 128], BF16)]
        tmpo = [sb("tmpo0", [128, 512], F32), sb("tmpo1", [128, 512], F32)]
        ps = [es.enter_context(nc.psum_tensor(f"moe_ps{i}_{sx}", [128, 512], F32)) for i in range(8)]

        class SC:
            pass
        S0 = SC()
        S0.T, S0.NT, S0.cap, S0.CW, S0.CT, S0.cs, S0.stream = T, NT, cap, CW, CT, cs, stream
        S0.xin_v, S0.xout_v, S0.hbf, S0.aff, S0.slot, S0.gate, S0.ye_scr = xin_v, xout_v, hbf, aff, slot, gate, g["ye_scr"]
        streams = [S0]
        if comp is not None:
            S1 = SC()
            S1.T, S1.NT, S1.cap, S1.CW, S1.CT, S1.cs, S1.stream = 256, 2, 32, 32, 1, 32, 1
            S1.xin_v = comp[0].rearrange("(n p) d -> n p d", p=128)
            S1.xout_v = comp[1].rearrange("(n p) d -> n p d", p=128)
            S1.hbf = sb("hbfc", [128, 2, D], BF16)
            S1.aff = sb("affc", [128, 2, 16], F32)
            S1.slot = sb("slotc", [128, 2, 16], F32)
            S1.gate = sb("gatec", [128, 2, 16], F32)
            S1.ye_scr = g["ye_scr_c"]
            selTc = [sb("selTc0", [128, 32], BF16), sb("selTc1", [128, 32], BF16)]
            xeTc = sb("xeTc", [128, 8, 32], BF16)
            silc = sb("silc", [128, 8, 32], F32)
            hidTc = sb("hidTc", [128, 8, 32], BF16)
            yestc = [sb("yestc0", [32, 1, D], BF16)] * 2
            streams = [S1, S0]
        affT_full, work_full, maskT_full, slotT_full = affT, work, maskT, slotT
        kb.dma("sp", rt[:], g["moe_router"][layer].rearrange("(kc p) e -> p kc e", p=128), writes=["rt"])
        for S in streams:
            T, NT, cap, CW, CT, cs, stream = S.T, S.NT, S.cap, S.CW, S.CT, S.cs, S.stream
            xin_v, xout_v, hbf, aff, slot, gate = S.xin_v, S.xout_v, S.hbf, S.aff, S.slot, S.gate
            affT, work, maskT, slotT = affT_full[:, 0:T], work_full[:, 0:T], maskT_full[:, 0:T], slotT_full[:, 0:T]
            onesT, gateT = work, affT
            kb.barrier()
            make_bc(kb, nc, c, lambda kc: mod[:, 4 * 8 + kc, stream:stream + 1], Gbc, ps[0], "Gbc", [f"mod{layer}"])
            make_bc(kb, nc, c, lambda kc: mod[:, 3 * 8 + kc, stream:stream + 1], Sbc, ps[0], "Sbc", [f"mod{layer}"])

            def stageA(i):
                b = i % 2
                kb.dma("sp", xb[b][:], xin_v[i], writes=[f"xb{b}"])
                yield
                yield from norm_tile_g(kb, nc, xb[b][:], f"xb{b}", stt[b], Gbc, Sbc, h32[b][:], f"h32{b}")
                yield
                kb.op("act", lambda e, i=i, b=b: e.copy(out=hbf[:, i, :], in_=h32[b][:]), reads=[f"h32{b}"],
                      writes=[f"hbf{stream}_{i}"])
                for half in range(2):
                    for q in range(4):
                        kc = half * 4 + q
                        kb.op("pe", lambda e, kc=kc, q=q, b=b, half=half: e.transpose(
                            out=ps[half][:, q * 128:(q + 1) * 128], in_=h32[b][:, kc * 128:(kc + 1) * 128],
                            identity=c.ident[:]), reads=[f"h32{b}", "c_ident"], writes=[f"ps{half}"])
                    yield
                    kb.op("dve" if half == 0 else "act", (lambda e, half=half, b=b: e.tensor_copy(
                        out=hT[b][:, half * 4:(half + 1) * 4, :], in_=ps[half][:].rearrange("p (q t) -> p q t", q=4)))
                        if half == 0 else (lambda e, half=half, b=b: e.copy(
                            out=hT[b][:, half * 4:(half + 1) * 4, :], in_=ps[half][:].rearrange("p (q t) -> p q t", q=4))),
                        reads=[f"ps{half}"], writes=[f"hT{b}"])
                    yield

            def stageB(i):
                b = i % 2
                for kc in range(8):
                    kb.op("pe", lambda e, kc=kc, b=b: e.matmul(ps[2][:, 0:16], lhsT=hT[b][:, kc, :], rhs=rt[:, kc, :],
                                                             start=(kc == 0), stop=(kc == 7)),
                          reads=[f"hT{b}", "rt"], writes=["ps2"])
                yield
                kb.op("dve", lambda e: e.reduce_max(out=sm[:, 0:1], in_=ps[2][:, 0:16], axis=AX.X),
                      reads=["ps2"], writes=["sm"])
                kb.op("dve", lambda e: e.tensor_scalar(out=sm[:, 1:2], in0=sm[:, 0:1], scalar1=-1.0, scalar2=None,
                                                       op0=ALU.mult), reads=["sm"], writes=["sm"])
                yield
                kb.op("act", lambda e: e.activation(out=ex[:], in_=ps[2][:, 0:16], func=AF.Exp, bias=sm[:, 1:2],
                                                    accum_out=sm[:, 2:3]), reads=["ps2", "sm"], writes=["ex", "sm"])
                yield
                kb.op("dve", lambda e: e.reciprocal(out=sm[:, 3:4], in_=sm[:, 2:3]), reads=["sm"], writes=["sm"])
                kb.op("dve", lambda e, i=i: e.tensor_scalar(out=aff[:, i, :], in0=ex[:], scalar1=sm[:, 3:4], scalar2=None,
                                                            op0=ALU.mult), reads=["ex", "sm"], writes=["aff"])
                yield
                kb.op("pe", lambda e, i=i: e.transpose(out=ps[3][0:16, 0:128], in_=aff[:, i, :], identity=c.ident[:]),
                      reads=["aff", "c_ident"], writes=["ps3"])
                yield
                kb.op("act", lambda e, i=i: e.copy(out=affT[:, i * 128:(i + 1) * 128], in_=ps[3][0:16, 0:128]),
                      reads=["ps3"], writes=["affT"])
                yield

            from itertools import zip_longest
            prevB = iter(())
            for i in range(NT):
                for _ in zip_longest(stageA(i), prevB):
                    pass
                prevB = stageB(i)
            for _ in prevB:
                pass

            import os
            PH = int(os.environ.get("MOE_PH", "9"))
            if PH < 2:
                kb.dma("sp", x_out[0:128, 0:NT * 16], aff[:].rearrange("p n e -> p (n e)"), reads=["aff"], writes=["o"])
                kb.barrier()
                return
            kb.op("dve", lambda e: e.tensor_copy(out=work[:], in_=affT[:]), reads=["affT"], writes=["work"])
            nr = cap // 8
            for r in range(nr):
                kb.op("dve", lambda e: e.max(out=mx8[:], in_=work[:]), reads=["work"], writes=["mx8"])
                if r < nr - 1:
                    kb.op("dve", lambda e: e.match_replace(out=work[:], in_to_replace=mx8[:], in_values=work[:],
                                                           imm_value=-1.0), reads=["work", "mx8"], writes=["work"])
            kb.op("dve", lambda e: e.tensor_scalar(out=maskT[:], in0=affT[:], scalar1=mx8[:, 7:8], scalar2=None,
                                                   op0=ALU.is_ge), reads=["affT", "mx8"], writes=["maskT"])
            kb.op("pool", lambda e: e.memset(onesT[:], 1.0), reads=[], writes=["work"])
            kb.op("dve", lambda e: e.tensor_tensor_scan(out=slotT[:], data0=onesT[:], data1=maskT[:], initial=0.0,
                                                        op0=ALU.mult, op1=ALU.add),
                  reads=["work", "maskT"], writes=["slotT"])
            kb.op("dve", lambda e: e.tensor_tensor(out=slotT[:], in0=slotT[:], in1=maskT[:], op=ALU.mult),
                  reads=["slotT", "maskT"], writes=["slotT"])
            kb.op("dve", lambda e: e.tensor_scalar(out=slotT[:], in0=slotT[:], scalar1=-1.0, scalar2=None, op0=ALU.add),
                  reads=["slotT"], writes=["slotT"])
            kb.op("pool", lambda e: e.tensor_tensor(out=gateT[:], in0=affT[:], in1=maskT[:], op=ALU.mult),
                  reads=["affT", "maskT"], writes=["affT"])
            for i in range(NT):
                kb.op("pe", lambda e, i=i: e.transpose(out=ps[0][:, i * 16:(i + 1) * 16],
                                                       in_=slotT[:, i * 128:(i + 1) * 128], identity=c.ident[0:16, 0:16]),
                      reads=["slotT", "c_ident"], writes=["ps0"])
                kb.op("pe", lambda e, i=i: e.transpose(out=ps[1][:, i * 16:(i + 1) * 16],
                                                       in_=gateT[:, i * 128:(i + 1) * 128], identity=c.ident[0:16, 0:16]),
                      reads=["affT", "c_ident"], writes=["ps1"])
            kb.op("dve", lambda e: e.tensor_copy(out=slot[:], in_=ps[0][:, 0:NT * 16].rearrange("p (n e) -> p n e", e=16)),
                  reads=["ps0"], writes=["slot"])
            kb.op("act", lambda e: e.copy(out=gate[:], in_=ps[1][:, 0:NT * 16].rearrange("p (n e) -> p n e", e=16)),
                  reads=["ps1"], writes=["gate"])

            if PH < 3:
                kb.dma("sp", x_out[0:128, 0:NT * 16], slot[:].rearrange("p n e -> p (n e)"), reads=["slot"], writes=["o"])
                kb.dma("sp", x_out[128:256, 0:NT * 16], gate[:].rearrange("p n e -> p (n e)"), reads=["gate"], writes=["o2"])
                kb.barrier()
                return

        stg = [xb[0], xb[1], h32[0], h32[1]]
        stgk = ["xb0", "xb1", "h320", "h321"]
        wcnt = [0]

        def load_w(wv, wt, wk):
            for hh in range(2):
                kb.dma("pool", wt[:, hh * 4:(hh + 1) * 4, :], wv[:, hh * 4:(hh + 1) * 4, :], writes=[wk])

        def wviews_of(e_):
            return (g["moe_w1"][layer, e_].rearrange("(kc p) n -> p kc n", p=128),
                    g["moe_w3"][layer, e_].rearrange("(kc p) n -> p kc n", p=128),
                    g["moe_w2"][layer, e_].rearrange("(kc p) n -> p kc n", p=128))
        nsel = 0
        SUB = int(os.environ.get("MOE_SUB", "9"))
        NEXP = int(os.environ.get("MOE_NEXP", "16"))
        wv1, wv3, wv2 = wviews_of(0)
        load_w(wv1, w1b[0], "w1_0")
        load_w(wv3, w3b[0], "w3")
        load_w(wv2, w2b[0], "w2")
        for ex_i in range(NEXP):
            w1t = w1b[ex_i % 2]
            w1k = f"w1_{ex_i % 2}"
            if ex_i + 1 < NEXP:
                nwv1, nwv3, nwv2 = wviews_of(ex_i + 1)
                load_w(nwv1, w1b[(ex_i + 1) % 2], f"w1_{(ex_i + 1) % 2}")
            for i in range(NT):
                b = nsel % 2
                nsel += 1
                kb.op("dve", lambda e, i=i, b=b, ex_i=ex_i: e.tensor_scalar(
                    out=selT[b][:], in0=c.iota[:, 0:CW], scalar1=slot[:, i, ex_i:ex_i + 1], scalar2=None,
                    op0=ALU.is_equal), reads=["c_iota", "slot"], writes=[f"selT{b}"])
                for kc in range(8):
                    bank = (kc * CW) // 512
                    off = (kc * CW) % 512
                    kb.op("pe", lambda e, i=i, b=b, kc=kc, bank=bank, off=off: e.matmul(
                        ps[bank][:, off:off + CW], lhsT=hbf[:, i, kc * 128:(kc + 1) * 128], rhs=selT[b][:],
                        start=(i == 0 and off == 0), stop=(i == NT - 1), skip_group_check=True), reads=[f"hbf{stream}_{i}", f"selT{b}"], writes=[f"ps{bank}"])
            nb = (8 * CW + 511) // 512
            per = 512 // CW if CW < 512 else 1
            for bank in range(nb):
                k0 = bank * per
                k1 = min(8, k0 + per)
                kb.op("act" if bank % 2 else "dve", (lambda e, bank=bank, k0=k0, k1=k1: e.copy(
                    out=xeT[:, k0:k1, :], in_=ps[bank][:, 0:(k1 - k0) * CW].rearrange("p (k c) -> p k c", c=CW)))
                    if bank % 2 else (lambda e, bank=bank, k0=k0, k1=k1: e.tensor_copy(
                        out=xeT[:, k0:k1, :], in_=ps[bank][:, 0:(k1 - k0) * CW].rearrange("p (k c) -> p k c", c=CW))),
                    reads=[f"ps{bank}"], writes=["xeT"])
            if SUB < 2:
                continue
            for fc in range(8):
                pb = ps[4 + fc % 2]
                pk = f"ps{4 + fc % 2}"
                for kc in range(8):
                    kb.op("pe", lambda e, fc=fc, kc=kc, pb=pb, w1t=w1t: e.matmul(
                        pb[:, 0:CW], lhsT=w1t[:, kc, fc * 128:(fc + 1) * 128], rhs=xeT[:, kc, :],
                        start=(kc == 0), stop=(kc == 7)), reads=[w1k, "xeT"], writes=[pk])
                for kc in range(8):
                    kb.op("pe", lambda e, fc=fc, kc=kc, pb=pb: e.matmul(
                        pb[:, 256:256 + CW], lhsT=w3b[0][:, kc, fc * 128:(fc + 1) * 128], rhs=xeT[:, kc, :],
                        start=(kc == 0), stop=(kc == 7)), reads=["w3", "xeT"], writes=[pk])
                sb_ = sil[fc % 2]
                DBG = int(os.environ.get("MOE_DBG", "9"))
                if DBG < 1:
                    continue
                kb.op("act", lambda e, pb=pb, sb_=sb_: e.activation(out=sb_[:], in_=pb[:, 0:CW], func=AF.Silu),
                      reads=[pk], writes=[f"sil{fc % 2}"])
                if DBG < 2:
                    continue
                kb.op("dve", lambda e, pb=pb, sb_=sb_, fc=fc: e.tensor_tensor(
                    out=hidT[:, fc, :], in0=sb_[:], in1=pb[:, 256:256 + CW], op=ALU.mult),
                    reads=[f"sil{fc % 2}", pk], writes=["hidT"])
            if comp is not None:
                for i in range(2):
                    sc_ = selTc[i]
                    kb.op("dve", lambda e, i=i, sc_=sc_, ex_i=ex_i: e.tensor_scalar(
                        out=sc_[:], in0=c.iota[:, 0:32], scalar1=S1.slot[:, i, ex_i:ex_i + 1], scalar2=None,
                        op0=ALU.is_equal), reads=["c_iota", "slotc"], writes=[f"selTc{i}"])
                    for kc in range(8):
                        kb.op("pe", lambda e, i=i, kc=kc, sc_=sc_: e.matmul(
                            ps[6][:, kc * 32:(kc + 1) * 32], lhsT=S1.hbf[:, i, kc * 128:(kc + 1) * 128], rhs=sc_[:],
                            start=(i == 0 and kc == 0), stop=(i == 1), skip_group_check=True),
                            reads=[f"hbf1_{i}", f"selTc{i}"], writes=["ps6"])
                kb.op("act", lambda e: e.copy(out=xeTc[:], in_=ps[6][:, 0:256].rearrange("p (k c) -> p k c", c=32)),
                      reads=["ps6"], writes=["xeTc"])
                for fc in range(8):
                    for kc in range(8):
                        kb.op("pe", lambda e, fc=fc, kc=kc: e.matmul(
                            ps[7][:, fc * 64:fc * 64 + 32], lhsT=w1t[:, kc, fc * 128:(fc + 1) * 128], rhs=xeTc[:, kc, :],
                            start=(fc == 0 and kc == 0), stop=(kc == 7), skip_group_check=True),
                            reads=[w1k, "xeTc"], writes=["ps7"])
                    for kc in range(8):
                        kb.op("pe", lambda e, fc=fc, kc=kc: e.matmul(
                            ps[7][:, fc * 64 + 32:fc * 64 + 64], lhsT=w3b[0][:, kc, fc * 128:(fc + 1) * 128],
                            rhs=xeTc[:, kc, :], start=False, stop=(kc == 7), skip_group_check=True),
                            reads=["w3", "xeTc"], writes=["ps7"])
                p7v = ps[7][:, :].rearrange("p (f ab c) -> p f ab c", f=8, ab=2)
                kb.op("act", lambda e: e.activation(out=silc[:], in_=p7v[:, :, 0, :], func=AF.Silu),
                      reads=["ps7"], writes=["silc"])
                kb.op("dve", lambda e: e.tensor_tensor(out=hidTc[:], in0=silc[:], in1=p7v[:, :, 1, :], op=ALU.mult),
                      reads=["silc", "ps7"], writes=["hidTc"])
            if ex_i + 1 < NEXP:
                load_w(nwv3, w3b[0], "w3")
            if SUB < 3:
                continue
            for ct in range(CT):
                for half in range(2):
                    pb = ps[6 + half]
                    pk = f"ps{6 + half}"
                    for fc in range(8):
                        kb.op("pe", lambda e, ct=ct, half=half, fc=fc, pb=pb: e.matmul(
                            pb[0:cs, :], lhsT=hidT[:, fc, ct * 128:ct * 128 + cs],
                            rhs=w2b[0][:, fc, half * 512:(half + 1) * 512], start=(fc == 0), stop=(fc == 7)),
                            reads=["hidT", "w2"], writes=[pk])
                    ys = yest[ex_i % 2]
                    kb.op("act" if half else "dve", (lambda e, ct=ct, half=half, pb=pb, ys=ys: e.copy(
                        out=ys[0:cs, ct, half * 512:(half + 1) * 512], in_=pb[0:cs, :]))
                        if half else (lambda e, ct=ct, half=half, pb=pb, ys=ys: e.tensor_copy(
                            out=ys[0:cs, ct, half * 512:(half + 1) * 512], in_=pb[0:cs, :])),
                        reads=[pk], writes=[f"yest{ex_i % 2}"])
            if comp is not None:
                ysc = yestc[ex_i % 2]
                for half in range(2):
                    for fc in range(8):
                        kb.op("pe", lambda e, half=half, fc=fc: e.matmul(
                            ps[6][0:32, :], lhsT=hidTc[:, fc, :], rhs=w2b[0][:, fc, half * 512:(half + 1) * 512],
                            start=(fc == 0), stop=(fc == 7)), reads=["hidTc", "w2"], writes=["ps6"])
                    kb.op("act", lambda e, half=half, ysc=ysc: e.copy(out=ysc[0:32, 0, half * 512:(half + 1) * 512],
                                                                     in_=ps[6][0:32, :]),
                          reads=["ps6"], writes=["yestc"])
                kb.dma("act", S1.ye_scr[ex_i, 0:32, 0:1, :], ysc[0:32, :, :], reads=["yestc"],
                       writes=[("yescrc", ex_i)])
            if ex_i + 1 < NEXP:
                load_w(nwv2, w2b[0], "w2")
            kb.dma("sp", g["ye_scr"][ex_i, 0:cs, 0:CT, :], yest[ex_i % 2][0:cs, :, :], reads=[f"yest{ex_i % 2}"],
                   writes=[("yescr", ex_i)])

        if PH < 4:
            kb.barrier()
            return
        for S in streams[::-1]:
            T, NT, cap, CW, CT, cs, stream = S.T, S.NT, S.cap, S.CW, S.CT, S.cs, S.stream
            xin_v, xout_v, hbf, aff, slot, gate = S.xin_v, S.xout_v, S.hbf, S.aff, S.slot, S.gate
            yeall = big[:, 0:16 * CT * D].rearrange("p (e c n) -> p e c n", e=16, c=CT)
            ye_scr_S = S.ye_scr
            kb.barrier()
            make_bc(kb, nc, c, lambda kc: mod[:, 5 * 8 + kc, stream:stream + 1], gate2, ps[0], "gate2", [f"mod{layer}"])
            if final_norm:
                make_bc(kb, nc, c, lambda kc: c.fnT[:, kc:kc + 1], Gbc, ps[0], "Gbc", ["c_fnT"])
            for ex_i in range(16):
                kb.dma(["sp", "act"][ex_i % 2], yeall[0:cs, ex_i, :, :], ye_scr_S[ex_i, 0:cs, 0:CT, :],
                       reads=[("yescr", ex_i), ("yescrc", ex_i)], writes=[f"ye{ex_i}"])
            nsg = 0
            EG = 4
            for i in range(NT):
                b = i % 2
                kb.dma("sp", xb[b][:], xin_v[i], writes=[f"xb{b}"])
                for g0 in range(0, 16, EG):
                    sg = nsg % 2
                    nsg += 1
                    sga = selGa[sg][:, :, 0:CW]
                    kb.op("dve", lambda e, i=i, g0=g0, sga=sga: e.tensor_tensor(
                        out=sga[:, :, :], in0=c.iota[:, 0:CW].unsqueeze(1).broadcast_to([128, EG, CW]),
                        in1=slot[:, i, g0:g0 + EG].unsqueeze(2).broadcast_to([128, EG, CW]), op=ALU.is_equal),
                        reads=["c_iota", "slot"], writes=[f"selGa{sg}"])
                    kb.op("pool", lambda e, i=i, g0=g0, sga=sga: e.tensor_tensor(
                        out=sga[:, :, :], in0=sga[:, :, :],
                        in1=gate[:, i, g0:g0 + EG].unsqueeze(2).broadcast_to([128, EG, CW]), op=ALU.mult),
                        reads=[f"selGa{sg}", "gate"], writes=[f"selGa{sg}"])
                    pst = ps[2 + sg][:].bitcast(BF16)
                    for ee in range(EG):
                        for ct in range(CT):
                            kb.op("pe", lambda e, ct=ct, ee=ee, sga=sga, pst=pst: e.transpose(
                                out=pst[0:cs, (ee * CT + ct) * 128:(ee * CT + ct + 1) * 128],
                                in_=sga[:, ee, ct * 128:ct * 128 + cs], identity=c.identb[:]),
                                reads=[f"selGa{sg}", "c_identb"], writes=[f"ps{2 + sg}"])
                    kb.op("act", lambda e, sg=sg, pst=pst: e.copy(
                        out=selGca[sg][0:cs, 0:EG * CT, :], in_=pst[0:cs, 0:EG * CT * 128].rearrange("p (c t) -> p c t", t=128)),
                        reads=[f"ps{2 + sg}"], writes=[f"selGca{sg}"])
                    for ee in range(EG):
                        ex_i = g0 + ee
                        for half in range(2):
                            for ct in range(CT):
                                kb.op("pe", lambda e, half=half, ct=ct, sg=sg, ex_i=ex_i, ee=ee: e.matmul(
                                    ps[half][:, :], lhsT=selGca[sg][0:cs, ee * CT + ct, :],
                                    rhs=yeall[0:cs, ex_i, ct, half * 512:(half + 1) * 512],
                                    start=(ex_i == 0 and ct == 0), stop=(ex_i == 15 and ct == CT - 1)),
                                    reads=[f"selGca{sg}", f"ye{ex_i}"], writes=[f"ps{half}"])
                for half in range(2):
                    sl = slice(half * 512, (half + 1) * 512)
                    kb.op("dve", lambda e, half=half, sl=sl: e.tensor_tensor(
                        out=tmpo[half][:], in0=ps[half][:], in1=gate2[:, sl], op=ALU.mult),
                        reads=[f"ps{half}", "gate2"], writes=[f"tmpo{half}"])
                    kb.op("pool", lambda e, half=half, sl=sl, b=b: e.tensor_tensor(
                        out=xb[b][:, sl], in0=xb[b][:, sl], in1=tmpo[half][:], op=ALU.add),
                        reads=[f"tmpo{half}", f"xb{b}"], writes=[f"xb{b}"])
                if final_norm:
                    norm_tile(kb, nc, xb[b][:], f"xb{b}", stt[b], Gbc, None, h32[b][:], f"h32{b}")
                    kb.dma("sp", xout_v[i], h32[b][:], reads=[f"h32{b}"], writes=[("dram", x_out.tensor.name, i)])
                else:
                    kb.dma("sp", xout_v[i], xb[b][:], reads=[f"xb{b}"], writes=[("dram", x_out.tensor.name, i)])
            kb.barrier()


def host_consts():
    k = {}
    k["k_ident"] = np.eye(128, dtype=np.float32)
    k["k_iota"] = np.tile(np.arange(256, dtype=np.float32)[None, :], (128, 1))
    C, S, perm = rope_tables()
    k["k_ropeC"], k["k_ropeS"], k["k_perm"] = C, S, perm
    kk = np.arange(128)[:, None]
    qq = np.arange(128)[None, :]
    k["k_mL"] = np.tile((kk >= qq).astype(np.float32), (1, 4))
    k["k_mU"] = np.tile((kk <= qq).astype(np.float32), (1, 4))
    ss = np.arange(CHUNK)[:, None]
    tt = np.arange(CHUNK)[None, :]
    mus = (ss < tt).astype(np.float32)
    mui = (ss <= tt).astype(np.float32)
    k["k_maskA"] = np.ascontiguousarray(np.concatenate([-mus, mus, mui, mui], axis=1))
    k["k_maskT"] = np.ascontiguousarray(-(tt < ss).astype(np.float32))
    rm = np.ones((128, TP), np.float32)
    rm[:, CHUNK_COLS] = 0.0
    k["k_rmask"] = rm
    k["k_J"] = np.ascontiguousarray(np.eye(128, dtype=np.float32)[::-1])
    sel = np.zeros((16, 16, 128), np.float32)
    for r in range(16):
        sel[r, r, :] = 1.0
    k["k_sel"] = sel
    bdm = np.zeros((128, 128), np.float32)
    bdm[0:64, 0:64] = 1.0
    bdm[64:128, 64:128] = 1.0
    k["k_bd"] = bdm
    return k


def fm(v):
    v = np.asarray(v, np.float32)
    return np.ascontiguousarray(v.reshape(-1, 128).T)


def host_inputs(inp, b):
    m = dict(host_consts())
    m["x"] = np.ascontiguousarray(inp["x"][b])
    m["ctx"] = np.ascontiguousarray(inp["ctx"][b])
    m["cT"] = fm(inp["c"][b])
    m["ccT"] = fm(inp["c_ctx"])
    m["ada_w"] = inp["ada_w"]
    m["ada_bT"] = np.ascontiguousarray(np.stack([fm(inp["ada_b"][l]) for l in range(2)], axis=1))
    m["normT"] = np.ascontiguousarray(np.stack(
        [np.stack([fm(inp["norm_mix"][l]), fm(inp["norm_ffn"][l])], axis=1) for l in range(2)], axis=1))
    m["fnT"] = fm(inp["final_norm"])
    for k in ("moe_router", "moe_w1", "moe_w3", "moe_w2", "o_w_out"):
        m[k] = inp[k]
    w = inp["o_w_in"][0]
    kd = w[:, 1024:1280].reshape(1024, 4, 1, 64)
    kd = np.concatenate([kd, kd], axis=2).reshape(1024, 512)
    m["o_w_in2"] = np.ascontiguousarray(np.concatenate([w[:, 0:1024], kd, w[:, 1280:1536]], axis=1))
    m["o_sink"] = np.ascontiguousarray(inp["o_sink"].reshape(1, 16))
    idx = []
    for p in range(4):
        for off in (0, 512, 1024):
            idx += list(range(off + p * 128, off + p * 128 + 128))
    for d in range(2):
        idx += list(range(1536 + d * 64, 1536 + d * 64 + 64)) + list(range(1664 + d * 64, 1664 + d * 64 + 64))
    idx += list(range(1792, 1920))
    idxa = list(idx)
    for h in range(4):
        for part in range(3):
            idx += list(range(1920 + part * 512 + h * 128, 1920 + part * 512 + h * 128 + 128))
        idx += list(range(3472 + h * 128, 3472 + h * 128 + 128))
    idx += list(range(3456, 3472))
    m["e_w_in2"] = np.ascontiguousarray(inp["e_w_in"][0][:, idx])
    mu2 = inp["a_mu"][0][idxa]
    p64 = np.zeros((64, 64), np.float32)
    p64[:, 0:30] = mu2.reshape(30, 64).T
    for d in range(2):
        for h in range(8):
            p64[:, 30 + d * 8 + h] = inp["a_w0"][0, d, h * 64:(h + 1) * 64]
            p64[:, 46 + d * 8 + h] = inp["a_a0"][0, d, h * 64:(h + 1) * 64]
    m["mx_par64"] = p64
    p128 = np.zeros((128, 112), np.float32)
    p128[:, 0] = mu2[1536:1664]; p128[:, 1] = mu2[1664:1792]; p128[:, 2] = mu2[1792:1920]
    for h in range(8):
        p128[0:64, 8 + h] = inp["a_k_k"][0, h * 64:(h + 1) * 64]
        p128[0:64, 16 + h] = inp["a_k_a"][0, h * 64:(h + 1) * 64]
        p128[0:64, 32 + h] = inp["a_r_k"][0, h]
    p128[0:8, 40] = inp["b_dt_bias"][0].reshape(8)
    p128[0:8, 41] = inp["b_a_log"][0].reshape(8)
    for h in range(4):
        for part in range(3):
            for j in range(5):
                p128[:, 48 + (h * 3 + part) * 5 + j] = inp["b_conv"][0, j, part * 512 + h * 128:part * 512 + (h + 1) * 128]
    m["mx_par128"] = p128
    pP = np.zeros((128, 64), np.float32)
    pP[:, 0:12] = mu2[0:1536].reshape(12, 128).T
    for p in range(4):
        sl = slice(p * 128, (p + 1) * 128)
        for d in range(2):
            pP[:, 12 + d * 4 + p] = inp["a_w0"][0, d, sl]
            pP[:, 20 + d * 4 + p] = inp["a_a0"][0, d, sl]
        pP[:, 28 + p] = inp["a_k_k"][0, sl]
        pP[:, 32 + p] = inp["a_k_a"][0, sl]
        pP[:, 40 + p] = inp["a_r_k"][0].reshape(512)[sl]
    m["mx_parP"] = pP
    m["mx_w2"] = np.ascontiguousarray(np.concatenate([inp["a_w2"][0].transpose(1, 0, 2), inp["a_a2"][0].transpose(1, 0, 2)], axis=0))
    m["mx_bc"] = np.ascontiguousarray(np.concatenate([inp["a_ln_w"][0], inp["a_ln_b"][0], np.tile(inp["b_norm"][0], 4)])[None, :])
    m["a_g2"] = inp["a_g2"]
    m["e_w_out"] = inp["e_w_out"]
    return m


IN_SHAPES = {
    "k_ident": [128, 128], "k_iota": [128, 256],
    "x": [2048, D], "ctx": [256, D], "cT": [128, 8], "ccT": [128, 8],
    "ada_w": [2, D, 6 * D], "ada_bT": [128, 2, 48], "normT": [128, 2, 2, 8], "fnT": [128, 8],
    "k_ropeC": [128, 2048], "k_ropeS": [128, 2048], "k_perm": [128, 128], "k_mL": [128, 512], "k_mU": [128, 512],
    "o_w_in2": [D, 1792], "o_w_out": [1, D, D], "o_sink": [1, 16],
    "k_maskA": [CHUNK, 4 * CHUNK], "k_maskT": [CHUNK, CHUNK], "k_rmask": [128, TP], "k_J": [128, 128], "k_sel": [16, 16, 128],
    "e_w_in2": [D, 3984], "mx_par64": [64, 64], "mx_parP": [128, 64], "k_bd": [128, 128], "mx_par128": [128, 112], "mx_w2": [128, 2, 512], "mx_bc": [1, 1536],
    "a_g2": [1, 128, 512], "e_w_out": [1, D, D],
    "moe_router": [2, D, 16], "moe_w1": [2, 16, D, D], "moe_w3": [2, 16, D, D], "moe_w2": [2, 16, D, D],
}


def build(stages=("all",), extra_in=(), outs=(("out", [2048, D]),)):
    from contextlib import ExitStack
    nc = bass.Bass("TRN2", target_bir_lowering=False)
    g = {}
    for name, shape in IN_SHAPES.items():
        g[name] = nc.dram_tensor(name, shape, F32, kind="ExternalInput").ap()
    for name, shape in extra_in:
        g[name] = nc.dram_tensor(name, shape, F32, kind="ExternalInput").ap()
    for name, shape in outs:
        g[name] = nc.dram_tensor(name, shape, F32, kind="ExternalOutput").ap()
    g["ye_scr"] = nc.dram_tensor("ye_scr", [16, 128, 2, D], BF16).ap()
    g["ye_scr_c"] = nc.dram_tensor("ye_scr_c", [16, 32, 1, D], BF16).ap()
    g["ys_scr"] = nc.dram_tensor("ys_scr", [2, 2304, 1536], F32).ap()
    g["z_scr"] = nc.dram_tensor("z_scr", [2304, 512], F32).ap()
    for nm, rows in (("xm_lat", 2048), ("xm_ctx", 256), ("x1_lat", 2048), ("x1_ctx", 256), ("x2_lat", 2048)):
        g[nm] = nc.dram_tensor(nm, [rows, D], F32).ap()
    kb = KB(nc)
    with ExitStack() as es:
        c = load_consts(kb, nc, es, g)
        c.fnT = es.enter_context(nc.sbuf_tensor("c_fnT", [128, 8], F32))
        kb.dma("sp", c.fnT[:], g["fnT"][:, :], writes=["c_fnT"])
        mods = prologue(kb, nc, es, g, c)
        for st in stages:
            if st == "moe0l_test":
                moe_stage(kb, nc, g, c, mods, 0, 0, g["t_in"], g["out"], 2048)
            elif st == "moe0f_test":
                moe_stage(kb, nc, g, c, mods, 0, 0, g["t_in"], g["out"], 2048, comp=(g["t_in2"], g["out2"]))
            elif st == "moe0c_test":
                moe_stage(kb, nc, g, c, mods, 0, 1, g["t_in"], g["out"], 256)
            elif st == "moe1l_test":
                moe_stage(kb, nc, g, c, mods, 1, 0, g["t_in"], g["out"], 2048, final_norm=True)
            elif st == "all":
                mixer_stage(kb, nc, g, c, mods, g["x"], g["ctx"], g["xm_lat"], g["xm_ctx"])
                moe_stage(kb, nc, g, c, mods, 0, 0, g["xm_lat"], g["x1_lat"], 2048, comp=(g["xm_ctx"], g["x1_ctx"]))
                attn_stage(kb, nc, g, c, mods, g["x1_lat"], g["x1_ctx"], g["x2_lat"])
                moe_stage(kb, nc, g, c, mods, 1, 0, g["x2_lat"], g["out"], 2048, final_norm=True)
            elif st == "mixer_test":
                mixer_stage(kb, nc, g, c, mods, g["x"], g["ctx"], g["out"], g["out2"])
            elif st == "attn_test":
                attn_stage(kb, nc, g, c, mods, g["t_in"], g["t_in2"], g["out"])
            elif st == "mods_test":
                for l in range(2):
                    kb.dma("sp", g["out"][l * 128:(l + 1) * 128, 0:96], mods[l][:].rearrange("p a b -> p (a b)"),
                           reads=[f"mod{l}"], writes=[("o", l)])
        kb.finish([])
    return nc, kb


def rope_tables():
    quarter = 16
    inv = (10000.0 ** (-np.arange(quarter, dtype=np.float32) / quarter)).astype(np.float32)
    t = np.arange(2048)
    row = (t // 64).astype(np.float32)
    col = (t % 64).astype(np.float32)
    C = np.zeros((128, 2048), np.float32)
    S = np.zeros((128, 2048), np.float32)
    perm = np.zeros((128, 128), np.float32)
    for p in range(128):
        d = p % 64
        pos = row if d < 32 else col
        i = d % 16
        ang = (pos * inv[i]).astype(np.float32)
        C[p] = np.cos(ang)
        second = (d % 32) >= 16
        S[p] = np.sin(ang) if second else -np.sin(ang)
        partner = p - 16 if second else p + 16
        perm[partner, p] = 1.0
    return C, S, perm


def attn_stage(kb, nc, g, c, mods, x_lat_in, x_ctx_in, x_out):
    layer = 1
    mod = mods[layer]
    NT = 18
    from contextlib import ExitStack
    with ExitStack() as es:
        def sb(name, shape, dt):
            return es.enter_context(nc.sbuf_tensor(f"at_{name}", shape, dt))
        Gbc = sb("G", [128, D], F32)
        Sbc = sb("S", [128, D], F32)
        xb = [sb("x0", [128, D], F32), sb("x1", [128, D], F32)]
        hb = [sb("h0", [128, D], BF16), sb("h1", [128, D], BF16)]
        stt = [sb("st0", [128, 4], F32), sb("st1", [128, 4], F32)]
        hT = sb("hT", [128, 8, 2304], BF16)
        win = sb("win", [128, 8, 1792], BF16)
        wo = sb("wo", [128, 8, D], BF16)
        stg = [sb("stg0", [128, D], F32), sb("stg1", [128, D], F32)]
        Ct = sb("Ct", [128, 2048], F32)
        St = sb("St", [128, 2048], F32)
        perm = sb("perm", [128, 128], F32)
        qraw = [sb("qraw0", [128, 512], F32), sb("qraw1", [128, 512], F32)]
        rt1 = [sb("rt10", [128, 512], F32), sb("rt11", [128, 512], F32)]
        qT = sb("qT", [128, 8, 2048], BF16)
        kT = sb("kT", [128, 4, 2304], BF16)
        V = sb("V", [128, NT, 4, 65], BF16)
        mL = sb("mL", [128, 512], BF16)
        mU = sb("mU", [128, 512], BF16)
        esink = sb("esink", [128, 16], F32)
        PT = [sb(f"PT{i}", [128, 512], BF16) for i in range(2)]
        osb = stg[0]
        den = sb("den", [128, 4], F32)
        oT = sb("oT", [128, 8, 128], BF16)
        tmpo = qraw
        ps = [es.enter_context(nc.psum_tensor(f"at_ps{i}", [128, 512], F32)) for i in range(8)]

        kb.dma("sp", Ct[:], g["k_ropeC"][:, :], writes=["Ct"])
        kb.dma("sp", St[:], g["k_ropeS"][:, :], writes=["St"])
        kb.dma("sp", perm[:], g["k_perm"][:, :], writes=["perm"])
        kb.dma("sp", esink[:], g["o_sink"].partition_broadcast(128), writes=["esink"])
        kb.op("act", lambda e: e.activation(out=esink[:], in_=esink[:], func=AF.Exp), reads=["esink"], writes=["esink"])
        kb.dma("sp", stg[0][:, 0:512], g["k_mL"][:, :], writes=["stg0"])
        kb.op("dve", lambda e: e.tensor_copy(out=mL[:], in_=stg[0][:, 0:512]), reads=["stg0"], writes=["mL"])
        kb.dma("sp", stg[1][:, 0:512], g["k_mU"][:, :], writes=["stg1"])
        kb.op("dve", lambda e: e.tensor_copy(out=mU[:], in_=stg[1][:, 0:512]), reads=["stg1"], writes=["mU"])
        make_bc(kb, nc, c, lambda kc: mod[:, 1 * 8 + kc, 1:2], Gbc, ps[0], "Gbc", [f"mod{layer}"])
        make_bc(kb, nc, c, lambda kc: mod[:, 0 * 8 + kc, 1:2], Sbc, ps[0], "Sbc", [f"mod{layer}"])

        wcnt = [0]

        wv = g["o_w_in2"].rearrange("(kc p) n -> p kc n", p=128)
        for kc in range(0, 8, 2):
            kb.dma("pool", win[:, kc:kc + 2, :], wv[:, kc:kc + 2, :], writes=["win"])
        wov = g["o_w_out"][0].rearrange("(kc p) n -> p kc n", p=128)
        for kc in range(0, 8, 4):
            kb.dma("pool", wo[:, kc:kc + 4, :], wov[:, kc:kc + 4, :], writes=["wo"])

        for i in range(NT):
            b = i % 2
            if i == 2:
                kb.barrier()
                make_bc(kb, nc, c, lambda kc: mod[:, 1 * 8 + kc, 0:1], Gbc, ps[0], "Gbc", [f"mod{layer}"])
                make_bc(kb, nc, c, lambda kc: mod[:, 0 * 8 + kc, 0:1], Sbc, ps[0], "Sbc", [f"mod{layer}"])
            src = x_ctx_in[i * 128:(i + 1) * 128, :] if i < 2 else x_lat_in[(i - 2) * 128:(i - 1) * 128, :]
            kb.dma("sp", xb[b][:], src, writes=[f"xb{b}"])
            norm_tile(kb, nc, xb[b][:], f"xb{b}", stt[b], Gbc, Sbc, stg[b][:], f"stg{b}")
            kb.op("act", lambda e, b=b: e.copy(out=hb[b][:], in_=stg[b][:]), reads=[f"stg{b}"], writes=[f"hb{b}"])
            for half in range(2):
                pst = ps[half][:].bitcast(BF16)
                for q in range(4):
                    kc = half * 4 + q
                    kb.op("pe", lambda e, kc=kc, q=q, b=b, pst=pst: e.transpose(
                        out=pst[:, q * 128:(q + 1) * 128], in_=hb[b][:, kc * 128:(kc + 1) * 128],
                        identity=c.identb[:]), reads=[f"hb{b}", "c_identb"], writes=[f"ps{half}"])
                kb.op("dve" if half == 0 else "pool" if False else "act",
                      (lambda e, half=half, pst=pst, i=i: e.tensor_copy(
                          out=hT[:, half * 4:(half + 1) * 4, i * 128:(i + 1) * 128],
                          in_=pst[:, 0:512].rearrange("p (q t) -> p q t", q=4))) if half == 0 else
                      (lambda e, half=half, pst=pst, i=i: e.copy(
                          out=hT[:, half * 4:(half + 1) * 4, i * 128:(i + 1) * 128],
                          in_=pst[:, 0:512].rearrange("p (q t) -> p q t", q=4))),
                      reads=[f"ps{half}"], writes=["hT"])

        import os
        APH = int(os.environ.get("ATT_PH", "9"))
        if APH < 1:
            kb.barrier(); return
        nb = 0
        for nq in range(8):
            for tb in range(4):
                b = nb % 2
                nb += 1
                pq = ps[2 + b]
                t0 = 256 + tb * 512
                for kc in range(8):
                    kb.op("pe", lambda e, kc=kc, nq=nq, t0=t0, pq=pq: e.matmul(
                        pq[:], lhsT=win[:, kc, nq * 128:(nq + 1) * 128], rhs=hT[:, kc, t0:t0 + 512],
                        start=(kc == 0), stop=(kc == 7)), reads=["win", "hT"], writes=[f"ps{2 + b}"])
                kb.op("act", lambda e, b=b, pq=pq: e.copy(out=qraw[b][:], in_=pq[:]), reads=[f"ps{2 + b}"],
                      writes=[f"qraw{b}"])
                pw = ps[4 + b]
                kb.op("pe", lambda e, b=b, pw=pw: e.matmul(pw[:], lhsT=perm[:], rhs=qraw[b][:], start=True, stop=True),
                      reads=["perm", f"qraw{b}"], writes=[f"ps{4 + b}"])
                cs_ = slice(tb * 512, (tb + 1) * 512)
                kb.op("dve", lambda e, b=b, pw=pw, cs_=cs_: e.scalar_tensor_tensor(
                    out=rt1[b][:], in0=pw[:], scalar=0.125, in1=St[:, cs_], op0=ALU.mult, op1=ALU.mult),
                      reads=[f"ps{4 + b}", "St"], writes=[f"rt1{b}"])
                kb.op("pool", lambda e, b=b, cs_=cs_: e.tensor_tensor(out=qraw[b][:], in0=qraw[b][:], in1=Ct[:, cs_],
                                                                    op=ALU.mult),
                      reads=[f"qraw{b}", "Ct"], writes=[f"qraw{b}"])
                kb.op("dve", lambda e, b=b, nq=nq, cs_=cs_: e.scalar_tensor_tensor(
                    out=qT[:, nq, cs_], in0=qraw[b][:], scalar=0.125, in1=rt1[b][:], op0=ALU.mult, op1=ALU.add),
                    reads=[f"qraw{b}", f"rt1{b}"], writes=["qT"])
        if APH < 2:
            kb.barrier(); return
        for hk in range(4):
            for tb in range(5):
                b = nb % 2
                nb += 1
                pq = ps[2 + b]
                t0 = 0 if tb == 0 else 256 + (tb - 1) * 512
                tw = 256 if tb == 0 else 512
                for kc in range(8):
                    kb.op("pe", lambda e, kc=kc, hk=hk, t0=t0, tw=tw, pq=pq: e.matmul(
                        pq[:, 0:tw], lhsT=win[:, kc, 1024 + hk * 128:1024 + (hk + 1) * 128],
                        rhs=hT[:, kc, t0:t0 + tw], start=(kc == 0), stop=(kc == 7)),
                        reads=["win", "hT"], writes=[f"ps{2 + b}"])
                if tb == 0:
                    kb.op("act", lambda e, hk=hk, pq=pq: e.copy(out=kT[:, hk, 0:256], in_=pq[:, 0:256]),
                          reads=[f"ps{2 + b}"], writes=["kT"])
                    continue
                kb.op("act", lambda e, b=b, pq=pq: e.copy(out=qraw[b][:], in_=pq[:]), reads=[f"ps{2 + b}"],
                      writes=[f"qraw{b}"])
                pw = ps[4 + b]
                kb.op("pe", lambda e, b=b, pw=pw: e.matmul(pw[:], lhsT=perm[:], rhs=qraw[b][:], start=True, stop=True),
                      reads=["perm", f"qraw{b}"], writes=[f"ps{4 + b}"])
                cs_ = slice((tb - 1) * 512, tb * 512)
                kb.op("dve", lambda e, b=b, pw=pw, cs_=cs_: e.tensor_tensor(out=rt1[b][:], in0=pw[:], in1=St[:, cs_],
                                                                         op=ALU.mult),
                      reads=[f"ps{4 + b}", "St"], writes=[f"rt1{b}"])
                kb.op("pool", lambda e, b=b, cs_=cs_: e.tensor_tensor(out=qraw[b][:], in0=qraw[b][:], in1=Ct[:, cs_],
                                                                    op=ALU.mult),
                      reads=[f"qraw{b}", "Ct"], writes=[f"qraw{b}"])
                kb.op("dve", lambda e, b=b, hk=hk, t0=t0: e.tensor_tensor(
                    out=kT[:, hk, t0:t0 + 512], in0=qraw[b][:], in1=rt1[b][:], op=ALU.add),
                    reads=[f"qraw{b}", f"rt1{b}"], writes=["kT"])
        if APH < 3:
            kb.barrier(); return
        kb.op("pool", lambda e: e.memset(V[:], 1.0), writes=["V"])
        for i in range(NT):
            b = nb % 2
            nb += 1
            pq = ps[2 + b]
            for kc in range(8):
                kb.op("pe", lambda e, kc=kc, i=i, pq=pq: e.matmul(
                    pq[:, 0:256], lhsT=hT[:, kc, i * 128:(i + 1) * 128], rhs=win[:, kc, 1536:1792],
                    start=(kc == 0), stop=(kc == 7)), reads=["win", "hT"], writes=[f"ps{2 + b}"])
            kb.op("act", lambda e, i=i, pq=pq: e.copy(out=V[:, i, :, 0:64],
                                                     in_=pq[:, 0:256].rearrange("p (h d) -> p h d", h=4)),
                  reads=[f"ps{2 + b}"], writes=["V"])

        if APH < 4:
            kb.barrier(); return
        make_bc(kb, nc, c, lambda kc: mod[:, 2 * 8 + kc, 0:1], Gbc, ps[0], "Gbc", [f"mod{layer}"])
        nsb = 0
        for n in range(16):
            kb.dma("sp", xb[n % 2][:], x_lat_in[n * 128:(n + 1) * 128, :], writes=[f"xb{n % 2}"])
            for hk in range(4):
                tiles = []
                if n > 0:
                    tiles.append((2 + n - 1, mL, "mL"))
                tiles.append((2 + n, None, None))
                if n < 15:
                    tiles.append((2 + n + 1, mU, "mU"))
                tiles.append((0, None, None))
                tiles.append((1, None, None))
                po = ps[6 + (n * 4 + hk) % 2]
                pok = f"ps{6 + (n * 4 + hk) % 2}"
                for ti, (kt, msk, mk) in enumerate(tiles):
                    sbk = nsb % 2
                    nsb += 1
                    pSa, pSb = ps[1 + 2 * sbk], ps[2 + 2 * sbk]
                    ka, kbk = f"ps{1 + 2 * sbk}", f"ps{2 + 2 * sbk}"
                    for gq in range(4):
                        hq = hk * 4 + gq
                        bp = (hq % 2) * 64
                        pS = pSa if bp == 0 else pSb
                        kb.op("pe", lambda e, gq=gq, hq=hq, bp=bp, kt=kt, pS=pS, hk=hk, n=n: e.matmul(
                            pS[:, (gq // 2) * 128:(gq // 2 + 1) * 128], lhsT=kT[bp:bp + 64, hk, kt * 128:(kt + 1) * 128],
                            rhs=qT[bp:bp + 64, hq // 2, n * 128:(n + 1) * 128], start=True, stop=True),
                            reads=["kT", "qT"], writes=[ka if bp == 0 else kbk])
                    ptv = PT[sbk][:].rearrange("p (a b q) -> p a b q", a=2, b=2)
                    kb.op("act", lambda e, pSa=pSa, ptv=ptv: e.activation(
                        out=ptv[:, :, 0, :], in_=pSa[:, 0:256].rearrange("p (a q) -> p a q", a=2), func=AF.Exp),
                        reads=[ka], writes=[f"PT{sbk}"])
                    kb.op("act", lambda e, pSb=pSb, ptv=ptv: e.activation(
                        out=ptv[:, :, 1, :], in_=pSb[:, 0:256].rearrange("p (a q) -> p a q", a=2), func=AF.Exp),
                        reads=[kbk], writes=[f"PT{sbk}"])
                    ADBG = int(os.environ.get("ATT_DBG", "9"))
                    if ADBG < 2:
                        continue
                    if msk is not None:
                        kb.op("dve", lambda e, sbk=sbk, msk=msk: e.tensor_tensor(out=PT[sbk][:], in0=PT[sbk][:],
                                                                               in1=msk[:], op=ALU.mult),
                              reads=[f"PT{sbk}", mk], writes=[f"PT{sbk}"])
                    for gq in range(4):
                        kb.op("pe", lambda e, gq=gq, sbk=sbk, kt=kt, hk=hk, po=po, ti=ti: e.matmul(
                            po[:, gq * 65:(gq + 1) * 65], lhsT=PT[sbk][:, gq * 128:(gq + 1) * 128],
                            rhs=V[:, kt, hk, :], start=(ti == 0 and gq == 0), stop=(ti == len(tiles) - 1),
                            skip_group_check=True), reads=[f"PT{sbk}", "V"], writes=[pok])
                if ADBG < 3:
                    continue
                pov = po[:, 0:260].rearrange("p (g d) -> p g d", g=4)
                kb.op("dve", lambda e, pov=pov, hk=hk: e.tensor_tensor(
                    out=den[:], in0=pov[:, :, 64], in1=esink[:, hk * 4:(hk + 1) * 4], op=ALU.add),
                    reads=[pok, "esink"], writes=["den"])
                kb.op("dve", lambda e: e.reciprocal(out=den[:], in_=den[:]), reads=["den"], writes=["den"])
                kb.op("dve", lambda e, pov=pov, hk=hk: e.tensor_tensor(
                    out=osb[:, hk * 256:(hk + 1) * 256].rearrange("p (g d) -> p g d", g=4), in0=pov[:, :, 0:64],
                    in1=den[:].unsqueeze(2).broadcast_to([128, 4, 64]), op=ALU.mult),
                    reads=[pok, "den"], writes=["stg0"])
            if ADBG < 4:
                continue
            kb.op("act", lambda e: e.copy(out=hb[0][:], in_=osb[:]), reads=["stg0"], writes=["hb0"])
            pst = ps[0][:].bitcast(BF16)
            for kc in range(8):
                kb.op("pe", lambda e, kc=kc, pst=pst: e.transpose(
                    out=pst[:, kc * 128:(kc + 1) * 128], in_=hb[0][:, kc * 128:(kc + 1) * 128], identity=c.identb[:]),
                    reads=["hb0", "c_identb"], writes=["ps0"])
            kb.op("act", lambda e, pst=pst: e.copy(out=oT[:], in_=pst[:].rearrange("p (k t) -> p k t", k=8)),
                  reads=["ps0"], writes=["oT"])
            for half in range(2):
                pp = ps[5] if half == 0 else ps[0]
                for kc in range(8):
                    kb.op("pe", lambda e, kc=kc, half=half, pp=pp: e.matmul(
                        pp[:], lhsT=oT[:, kc, :], rhs=wo[:, kc, half * 512:(half + 1) * 512],
                        start=(kc == 0), stop=(kc == 7)), reads=["oT", "wo"], writes=["ps5" if half == 0 else "ps0"])
                sl = slice(half * 512, (half + 1) * 512)
                kb.op("dve", lambda e, half=half, pp=pp, sl=sl: e.tensor_tensor(
                    out=tmpo[half][:], in0=pp[:], in1=Gbc[:, sl], op=ALU.mult),
                    reads=["ps5" if half == 0 else "ps0", "Gbc"], writes=[f"qraw{half}"])
                kb.op("pool", lambda e, half=half, sl=sl, n=n: e.tensor_tensor(
                    out=xb[n % 2][:, sl], in0=xb[n % 2][:, sl], in1=tmpo[half][:], op=ALU.add),
                    reads=[f"qraw{half}", f"xb{n % 2}"], writes=[f"xb{n % 2}"])
            kb.dma("sp", x_out[n * 128:(n + 1) * 128, :], xb[n % 2][:], reads=[f"xb{n % 2}"],
                   writes=[("dram", x_out.tensor.name, n)])
        kb.barrier()


def dplr_scan(kb, nc, c, T, dk, rT, kkT, kT, bT, vT, Pinc, prodT, store_cb, hk_, Gb=None, rA=None, kkA=None, bp=0, rC=None, kkC=None):
    ps = T["ps"]
    rA = rT if rA is None else rA
    kkA = kkT if kkA is None else kkA
    rC = rT if rC is None else rC
    kkC = kkT if kkC is None else kkC
    tb = vT.dtype == BF16
    ident, maskA, maskT, ident64 = c.ident, T["maskA"], T["maskT"], c.ident
    ST = T["ST"]
    kb.op("dve", lambda e: e.memset(ST[bp:bp + dk, 0:dk], 0.0), writes=["ST"])
    kb.op("dve", lambda e: e.memset(T["STb"][bp:bp + dk, 0:dk], 0.0), writes=["STb"])
    CH = CHUNK
    NLV = {64: 5, 128: 6}[CH]
    NCH = len(CHUNK_COLS)
    GB = 2

    def inv_gen(g0):
        grp = list(range(g0, min(NCH, g0 + GB)))
        par = (g0 // GB) % 2
        for ci in grp:
            s = ci % GB + GB * par
            cs = slice(CHUNK_COLS[ci], CHUNK_COLS[ci] + CH)
            pa = ps[s % 2]
            pk = f"ps{s % 2}"
            pn = ps[2 + s % 2]
            pnk = f"ps{2 + s % 2}"
            for j, (l, r) in enumerate(((bT, kkA), (kT, kkA), (bT, rA), (kT, rA), (kkA, bT))):
                if j < 4:
                    kb.op("pe", lambda e, j=j, l=l, r=r, cs=cs, pa=pa: e.matmul(
                        pa[0:CH, j * CH:(j + 1) * CH], lhsT=l[bp:bp + dk, cs], rhs=r[bp:bp + dk, cs], start=True, stop=True),
                        reads=[hk_], writes=[pk])
                else:
                    kb.op("pe", lambda e, l=l, r=r, cs=cs, pn=pn: e.matmul(
                        pn[0:CH, 256:256 + CH], lhsT=l[bp:bp + dk, cs], rhs=r[bp:bp + dk, cs], start=True, stop=True),
                        reads=[hk_], writes=[pnk])
            mA, mT_, mAk, mTk = maskA[0:CH, :], maskT[0:CH, :], "maskA", "maskT"
            if Gb is not None:
                c0_ = CHUNK_COLS[ci]
                kb.op("pe", lambda e, cs=cs, pn=pn: e.transpose(out=pn[0:CH, 384:385], in_=Gb[0:1, cs],
                                                              identity=ident[0:1, 0:1]), reads=[hk_, "c_ident"], writes=[pnk])
                kb.op("act", lambda e, s=s, pn=pn: e.copy(out=T["Gc"][0:CH, s:s + 1], in_=pn[0:CH, 384:385]),
                      reads=[pnk], writes=[f"Gc{s}"])
                kb.op("dve", lambda e, s=s, cs=cs: e.tensor_scalar(out=T["Dt"][0:CH, s % GB, :], in0=Gb[0:CH, cs],
                                                                 scalar1=T["Gc"][0:CH, s:s + 1], scalar2=0.0,
                                                                 op0=ALU.subtract, op1=ALU.min),
                      reads=[hk_, f"Gc{s}"], writes=[f"Dt{s % GB}"])
                kb.op("act", lambda e, s=s: e.activation(out=T["Dt"][0:CH, s % GB, :], in_=T["Dt"][0:CH, s % GB, :], func=AF.Exp),
                      reads=[f"Dt{s % GB}"], writes=[f"Dt{s % GB}"])
                kb.op("dve", lambda e, s=s, cs=cs: e.tensor_scalar(out=T["Dts"][0:CH, s % GB, :], in0=Gb[0:CH, cs],
                                                                 scalar1=T["Gc"][0:CH, s:s + 1], scalar2=0.0,
                                                                 op0=ALU.subtract, op1=ALU.max),
                      reads=[hk_, f"Gc{s}"], writes=[f"Dts{s % GB}"])
                kb.op("act", lambda e, s=s: e.activation(out=T["Dts"][0:CH, s % GB, :], in_=T["Dts"][0:CH, s % GB, :], func=AF.Exp,
                                                         scale=-1.0), reads=[f"Dts{s % GB}"], writes=[f"Dts{s % GB}"])
                kb.op("dve", lambda e, s=s: e.tensor_tensor(
                    out=T["mD"][0:CH, s % GB, :].rearrange("p (a t) -> p a t", a=4),
                    in0=maskA[0:CH, :].rearrange("p (a t) -> p a t", a=4),
                    in1=T["Dt"][0:CH, s % GB, :].unsqueeze(1).broadcast_to([CH, 4, CH]), op=ALU.mult),
                    reads=["maskA", f"Dt{s % GB}"], writes=[f"mD{s % GB}"])
                kb.op("pool", lambda e, s=s: e.tensor_tensor(out=T["Dts"][0:CH, s % GB, :], in0=T["Dts"][0:CH, s % GB, :],
                                                            in1=maskT[0:CH, :], op=ALU.mult),
                      reads=["maskT", f"Dts{s % GB}"], writes=[f"Dts{s % GB}"])
                kb.op("act", lambda e, s=s, c0_=c0_: e.activation(out=T["dL"][0:CH, s:s + 1], in_=T["Gc"][0:CH, s:s + 1],
                                                                func=AF.Exp, scale=-1.0, bias=Gb[0:CH, c0_ + CH - 1:c0_ + CH]),
                      reads=[f"Gc{s}", hk_], writes=[f"dL{s}"])
                mA, mT_, mAk, mTk = T["mD"][0:CH, s % GB, :], T["Dts"][0:CH, s % GB, :], f"mD{s % GB}", f"Dts{s % GB}"
            kb.op("dve", lambda e, s=s, pa=pa, mA=mA: e.tensor_tensor(out=T["AMf"][0:CH, s, :], in0=pa[0:CH, 0:CH],
                                                                     in1=mA[:, 0:CH], op=ALU.mult),
                  reads=[pk, mAk], writes=[f"AM{s}"])
            kb.op("dve", lambda e, s=s, pa=pa, mA=mA: e.tensor_tensor(out=T["AMb"][0:CH, s, :], in0=pa[0:CH, CH:4 * CH],
                                                                     in1=mA[:, CH:4 * CH], op=ALU.mult),
                  reads=[pk, mAk], writes=[f"AMb{s}"])
            kb.op("dve", lambda e, s=s, pn=pn, mT_=mT_: e.tensor_tensor(out=T["MM"][0][0:CH, s, CH:2 * CH],
                                                                       in0=pn[0:CH, 256:256 + CH], in1=mT_, op=ALU.mult),
                  reads=[pnk, mTk], writes=[f"MM0_{s}"])
            kb.op("pool", lambda e, s=s: e.tensor_copy(out=T["MM"][0][0:CH, s, 0:CH], in_=T["AMf"][0:CH, s, :]),
                  reads=[f"AM{s}"], writes=[f"MM0_{s}"])
            kb.op("pool", lambda e, s=s: e.tensor_tensor(out=T["Q"][0][0:CH, s, :], in0=T["AMf"][0:CH, s, :],
                                                        in1=ident64[0:CH, 0:CH], op=ALU.add),
                  reads=[f"AM{s}", "c_ident"], writes=[f"Q0_{s}"])
            yield
        for lv in range(NLV):
            a, b = lv % 2, (lv + 1) % 2
            last = lv == NLV - 1
            for ci in grp:
                s = ci % GB + GB * par
                pm = ps[2 + s % 2]
                pmk = f"ps{2 + s % 2}"
                MMa = T["MM"][a]
                if not last:
                    kb.op("pe", lambda e, s=s, pm=pm, MMa=MMa: e.matmul(
                        pm[0:CH, 0:CH], lhsT=MMa[0:CH, s, CH:2 * CH], rhs=MMa[0:CH, s, 0:CH], start=True, stop=True),
                        reads=[f"MM{a}_{s}"], writes=[pmk])
                kb.op("pe", lambda e, s=s, pm=pm, MMa=MMa: e.matmul(
                    pm[0:CH, CH:2 * CH], lhsT=MMa[0:CH, s, 0:CH], rhs=MMa[0:CH, s, CH:2 * CH], start=True, stop=True),
                    reads=[f"MM{a}_{s}"], writes=[pmk])
                lo = CH if last else 0
                kb.op("act", lambda e, s=s, pm=pm, b=b, lo=lo: e.copy(out=T["MM"][b][0:CH, s, lo:2 * CH],
                                                                     in_=pm[0:CH, lo:2 * CH]),
                      reads=[pmk], writes=[f"MM{b}_{s}"])
                yield
            for ci in grp:
                s = ci % GB + GB * par
                pq = ps[4 + s % 2]
                pqk = f"ps{4 + s % 2}"
                kb.op("pe", lambda e, s=s, pq=pq, b=b, a=a: e.matmul(
                    pq[0:CH, 0:CH], lhsT=T["MM"][b][0:CH, s, CH:2 * CH], rhs=T["Q"][a][0:CH, s, :], start=True, stop=True),
                    reads=[f"MM{b}_{s}", f"Q{a}_{s}"], writes=[pqk])
                kb.op("dve", lambda e, s=s, pq=pq, a=a, b=b: e.tensor_tensor(
                    out=T["Q"][b][0:CH, s, :], in0=pq[0:CH, 0:CH], in1=T["Q"][a][0:CH, s, :], op=ALU.add),
                    reads=[pqk, f"Q{a}_{s}"], writes=[f"Q{b}_{s}"])
                yield

    def chain_gen(g0):
        grp = list(range(g0, min(NCH, g0 + GB)))
        par = (g0 // GB) % 2
        QF = T["Q"][NLV % 2]
        for ci in grp:
            s = ci % GB + GB * par
            c0 = CHUNK_COLS[ci]
            cs = slice(c0, c0 + CH)
            AMb = T["AMb"]
            STb = T["STb"]
            pt = ps[6][:].bitcast(BF16) if tb else ps[6]
            idt = c.identb if tb else ident
            for j, src in enumerate((vT, kT, bT)):
                kb.op("pe", lambda e, j=j, src=src, cs=cs, pt=pt: e.transpose(
                    out=pt[0:CH, j * dk:(j + 1) * dk], in_=src[bp:bp + dk, cs], identity=idt[bp:bp + dk, bp:bp + dk]),
                    reads=[hk_, "c_ident", "c_identb"], writes=["ps6"])
            TM = T["TM"][ci % 2]
            tmk = f"TM{ci % 2}"
            kb.op("act", lambda e, TM=TM, pt=pt: e.copy(out=TM[0:CH, 0:3 * dk], in_=pt[0:CH, 0:3 * dk]),
                  reads=["ps6"], writes=[tmk])
            yield
            Vtm, Ktm, Btm = TM[0:CH, 0:dk], TM[0:CH, dk:2 * dk], TM[0:CH, 2 * dk:3 * dk]
            if Gb is not None:
                kb.op("dve", lambda e, TM=TM, s=s: e.tensor_scalar(out=TM[0:CH, dk:3 * dk], in0=TM[0:CH, dk:3 * dk],
                                                                 scalar1=T["dL"][0:CH, s:s + 1], scalar2=None, op0=ALU.mult),
                      reads=[tmk, f"dL{s}"], writes=[tmk])
            pr = ps[7]
            kb.op("pe", lambda e, cs=cs, pr=pr: e.matmul(pr[0:CH, 0:dk], lhsT=kkC[bp:bp + dk, cs], rhs=STb[bp:bp + dk, 0:dk],
                                                       start=True, stop=False), reads=[hk_, "STb"], writes=["ps7"])
            kb.op("pe", lambda e, s=s, pr=pr, Vtm=Vtm: e.matmul(pr[0:CH, 0:dk], lhsT=AMb[0:CH, s, 0:CH], rhs=Vtm,
                                                              start=False, stop=True),
                  reads=[f"AMb{s}", tmk], writes=["ps7"])
            yield
            kb.op("dve", lambda e, pr=pr: e.tensor_scalar(out=T["nR"][0:CH, 0:dk], in0=pr[0:CH, 0:dk], scalar1=-1.0,
                                                         scalar2=None, op0=ALU.mult), reads=["ps7"], writes=["nR"])
            yield
            kb.op("pe", lambda e, s=s, pr=pr: e.matmul(pr[0:CH, 128:128 + dk], lhsT=QF[0:CH, s, :], rhs=T["nR"][0:CH, 0:dk],
                                                     start=True, stop=True), reads=[f"Q{NLV % 2}_{s}", "nR"], writes=["ps7"])
            yield
            kb.op("act", lambda e, pr=pr: e.copy(out=T["U"][0:CH, 0:dk], in_=pr[0:CH, 128:128 + dk]),
                  reads=["ps7"], writes=["U"])
            yield
            U = T["U"]
            kb.op("pe", lambda e, cs=cs, pr=pr: e.matmul(pr[0:CH, 256:256 + dk], lhsT=rC[bp:bp + dk, cs], rhs=STb[bp:bp + dk, 0:dk],
                                                       start=True, stop=False), reads=[hk_, "STb"], writes=["ps7"])
            kb.op("pe", lambda e, s=s, pr=pr: e.matmul(pr[0:CH, 256:256 + dk], lhsT=AMb[0:CH, s, CH:2 * CH],
                                                     rhs=U[0:CH, 0:dk], start=False, stop=False),
                  reads=[f"AMb{s}", "U"], writes=["ps7"])
            kb.op("pe", lambda e, s=s, pr=pr, Vtm=Vtm: e.matmul(pr[0:CH, 256:256 + dk], lhsT=AMb[0:CH, s, 2 * CH:3 * CH],
                                                              rhs=Vtm, start=False, stop=True),
                  reads=[f"AMb{s}", tmk], writes=["ps7"])
            kb.op("pe", lambda e, pr=pr, Btm=Btm: e.matmul(pr[bp:bp + dk, 384:384 + dk], lhsT=Btm, rhs=U[0:CH, 0:dk],
                                                         start=True, stop=False), reads=[tmk, "U"], writes=["ps7"])
            kb.op("pe", lambda e, pr=pr, Ktm=Ktm, Vtm=Vtm: e.matmul(pr[bp:bp + dk, 384:384 + dk], lhsT=Ktm, rhs=Vtm,
                                                                  start=False, stop=True),
                  reads=[tmk], writes=["ps7"])
            yield
            Ysb = T["Y"][ci % 2]
            yk = f"Y{ci % 2}"
            kb.op("act", lambda e, pr=pr, Ysb=Ysb: e.copy(out=Ysb[0:CH, 0:dk], in_=pr[0:CH, 256:256 + dk]),
                  reads=["ps7"], writes=[yk])
            if Gb is not None:
                kb.op("dve", lambda e, pr=pr, c0=c0: e.scalar_tensor_tensor(
                    out=ST[bp:bp + dk, 0:dk], in0=ST[bp:bp + dk, 0:dk], scalar=Pinc[bp:bp + dk, c0 + CH - 1:c0 + CH],
                    in1=pr[bp:bp + dk, 384:384 + dk], op0=ALU.mult, op1=ALU.add), reads=["ps7", "ST", hk_], writes=["ST"])
            else:
                kb.op("dve", lambda e, pr=pr: e.tensor_tensor(out=ST[bp:bp + dk, 0:dk], in0=pr[bp:bp + dk, 384:384 + dk],
                                                             in1=ST[bp:bp + dk, 0:dk], op=ALU.add),
                      reads=["ps7", "ST"], writes=["ST"])
                kb.op("dve", lambda e, c0=c0: e.tensor_scalar(out=ST[bp:bp + dk, 0:dk], in0=ST[bp:bp + dk, 0:dk],
                                                             scalar1=Pinc[bp:bp + dk, c0 + CH - 1:c0 + CH], scalar2=None,
                                                             op0=ALU.mult), reads=["ST", hk_], writes=["ST"])
            kb.op("act", lambda e: e.copy(out=T["STb"][bp:bp + dk, 0:dk], in_=ST[bp:bp + dk, 0:dk]),
                  reads=["ST"], writes=["STb"])
            if prodT is not None:
                pb = ps[6]
                kb.op("pe", lambda e, cs=cs, pb=pb: e.matmul(pb[0:CH, 448:449], lhsT=prodT[bp:bp + dk, cs],
                                                           rhs=c.onesb[bp:bp + dk, 0:1], start=True, stop=True),
                      reads=[hk_, "c_ones"], writes=["ps6"])
                kb.op("dve", lambda e, pb=pb, Ysb=Ysb, Vtm=Vtm: e.tensor_scalar(
                    out=Ysb[0:CH, dk:2 * dk], in0=Vtm, scalar1=pb[0:CH, 448:449], scalar2=None, op0=ALU.mult),
                    reads=["ps6", tmk], writes=[yk])
            store_cb(ci, Ysb, yk)
            yield

    from itertools import zip_longest
    for _ in inv_gen(0):
        pass
    for g0 in range(0, NCH, GB):
        gens = [chain_gen(g0)]
        if g0 + GB < NCH:
            gens.append(inv_gen(g0 + GB))
        for _ in zip_longest(*gens):
            pass


def mixer_stage(kb, nc, g, c, mods, x_lat_in, x_ctx_in, x_lat_out, x_ctx_out):
    from contextlib import ExitStack
    mod = mods[0]
    Ys = g["ys_scr"]
    Zs = g["z_scr"]
    with ExitStack() as es:
        def sb(name, shape, dt):
            return es.enter_context(nc.sbuf_tensor(f"mxs_{name}", shape, dt))
        hT = None
        es2 = ExitStack()

        def sb2(name, shape, dt):
            return es2.enter_context(nc.sbuf_tensor(f"mxs_{name}", shape, dt))
        hb = sb("hb", [128, D], BF16)
        stt = [sb("st0", [128, 4], F32), sb("st1", [128, 4], F32)]
        F11 = sb("F11", [128, TP], F32)
        Jb = sb("Jb", [128, 128], BF16)
        J32 = sb("J32", [128, 128], F32)
        par = sb("par", [64, 64], F32)
        parP = sb("parP", [128, 64], F32)
        bd = sb("bd", [128, 128], F32)
        par128 = sb("par128", [128, 112], F32)
        w2sb = sb("w2sb", [128, 2, 512], F32)
        sel = sb("sel", [16, 16, 128], F32)
        ST = sb("ST", [128, 128], F32)
        hT = es2.enter_context(nc.sbuf_tensor("mxs_hT", [128, 8, 2304], BF16))
        F = [sb2(f"F{i}", [128, TP], F32) for i in range(11)] + [F11]
        FK = [f"F{i}" for i in range(12)]
        Gbc = F[1][:, 0:D]
        Sbc = F[2][:, 0:D]
        h32 = F[3][:, 0:D]
        xb = [F[4][:, 0:D], F[5][:, 0:D]]
        h32s = [F[3][:, 0:D], F[6][:, 0:D]]
        hb2 = sb2("hb2", [128, D], BF16)
        hbs = [hb, hb2]
        rmask = sb2("rmask", [128, TP], BF16)
        rm32 = F[0]
        wsl = sb2("wsl", [128, 8, 128], BF16)
        T = {"ST": ST,
             "AMf": sb2("AMf", [CHUNK, 4, CHUNK], F32), "AMb": sb2("AMb", [CHUNK, 4, 3 * CHUNK], BF16),
             "STb": sb2("STb", [128, 128], BF16),
             "MM": [sb2("MMa", [CHUNK, 4, 2 * CHUNK], F32), sb2("MMb", [CHUNK, 4, 2 * CHUNK], F32)],
             "Q": [sb2("Qa", [CHUNK, 4, CHUNK], F32), sb2("Qb", [CHUNK, 4, CHUNK], F32)],
             "TM": [sb2("TMa", [CHUNK, 384], BF16), sb2("TMb", [CHUNK, 384], BF16)],
             "nR": sb2("nR", [CHUNK, 128], F32), "U": sb2("U", [CHUNK, 128], BF16), "Uf": sb2("Uf", [16, 4], F32),
             "Y": [sb2("Ya", [CHUNK, 256], F32), sb2("Yb", [CHUNK, 256], F32)],
             "maskA": sb2("maskA", [CHUNK, 4 * CHUNK], F32), "maskT": sb2("maskT", [CHUNK, CHUNK], F32),
             "Gc": sb2("Gc", [CHUNK, 4], F32), "dL": sb2("dL", [CHUNK, 4], F32), "Dt": sb2("Dt", [CHUNK, 2, CHUNK], F32),
             "Dts": sb2("Dts", [CHUNK, 2, CHUNK], F32), "mD": sb2("mD", [CHUNK, 2, 4 * CHUNK], F32)}
        ps = [es.enter_context(nc.psum_tensor(f"mx_ps{i}", [128, 512], F32)) for i in range(8)]
        T["ps"] = ps
        kb.dma("sp", rm32[:], g["k_rmask"][:, :], writes=["F0"])
        kb.op("dve", lambda e: e.tensor_copy(out=rmask[:], in_=rm32[:]), reads=["F0"], writes=["rmask"])
        kb.dma("sp", J32[:], g["k_J"][:, :], writes=["J32"])
        kb.op("dve", lambda e: e.tensor_copy(out=Jb[:], in_=J32[:]), reads=["J32"], writes=["Jb"])
        kb.dma("sp", par[:], g["mx_par64"][:, :], writes=["par"])
        kb.dma("sp", par128[:], g["mx_par128"][:, :], writes=["par128"])
        kb.dma("sp", w2sb[:], g["mx_w2"][:, :, :], writes=["w2sb"])
        kb.dma("sp", parP[:], g["mx_parP"][:, :], writes=["parP"])
        kb.dma("sp", bd[:], g["k_bd"][:, :], writes=["bd"])
        kb.op("dve", lambda e: e.tensor_scalar(out=parP[:, 36:40], in0=parP[:, 32:36], scalar1=-1.0, scalar2=1.0,
                                               op0=ALU.mult, op1=ALU.add), reads=["parP"], writes=["parP"])
        kb.op("dve", lambda e: e.tensor_scalar(out=par128[:, 24:32], in0=par128[:, 16:24], scalar1=-1.0, scalar2=1.0,
                                               op0=ALU.mult, op1=ALU.add), reads=["par128"], writes=["par128"])
        T["zt"] = [sb2("zta", [128, 128], F32), sb2("ztb", [128, 128], F32)]
        kb.dma("sp", sel[:], g["k_sel"][:, :, :], writes=["sel"])
        kb.dma("sp", T["maskA"][:], g["k_maskA"][:, :], writes=["maskA"])
        kb.dma("sp", T["maskT"][:], g["k_maskT"][:, :], writes=["maskT"])
        for f in range(12):
            kb.op("pool", lambda e, f=f: e.memset(F[f][:], 0.0), reads=["rmask"] if f == 0 else [], writes=[FK[f]])
        wv = g["e_w_in2"].rearrange("(kc p) n -> p kc n", p=128)
        P64 = lambda j: par[:, j:j + 1]

        def project(c0, M, dst, dk_, evac_eng="act"):
            kb.dma("pool", wsl[:, :, 0:M], wv[:, :, c0:c0 + M], writes=["wsl"])
            for bi, (t0, tw, col0) in enumerate(BLOCKS):
                pb = ps[bi % 2]
                for kc in range(8):
                    kb.op("pe", lambda e, kc=kc, t0=t0, tw=tw, pb=pb: e.matmul(
                        pb[0:M, 0:tw], lhsT=wsl[:, kc, 0:M], rhs=hT[:, kc, t0:t0 + tw], start=(kc == 0), stop=(kc == 7)),
                        reads=["wsl", "hT"], writes=[f"ps{bi % 2}"])
                kb.op("act", lambda e, tw=tw, col0=col0, pb=pb: e.copy(out=dst[0:M, col0:col0 + tw], in_=pb[0:M, 0:tw]),
                      reads=[f"ps{bi % 2}"], writes=[dk_])

        def tshift(src, sk, dst, dk_, tmp, tk, P, mucol):
            n = TP - 2
            kb.op("dve", lambda e: e.tensor_tensor(out=tmp[0:P, 1:1 + n], in0=src[0:P, 0:n], in1=src[0:P, 2:2 + n],
                                                   op=ALU.add), reads=[sk], writes=[tk])
            kb.op("dve", lambda e: e.scalar_tensor_tensor(out=tmp[0:P, 1:1 + n], in0=tmp[0:P, 1:1 + n], scalar=0.5,
                                                          in1=src[0:P, 1:1 + n], op0=ALU.mult, op1=ALU.subtract),
                  reads=[sk, tk], writes=[tk])
            kb.op("dve", lambda e: e.scalar_tensor_tensor(out=dst[0:P, 1:1 + n], in0=tmp[0:P, 1:1 + n], scalar=mucol,
                                                         in1=src[0:P, 1:1 + n], op0=ALU.mult, op1=ALU.add),
                  reads=[sk, tk, "par", "par128"], writes=[dk_])

        DC = [(CTX0, 256), (LAT0, 2048)]

        def ew(eng, fn, reads, writes):
            kb.op(eng, fn, reads=reads, writes=writes)

        for d in range(2):
            for stream in (1, 0):
                kb.barrier()
                make_bc(kb, nc, c, lambda kc: mod[:, 1 * 8 + kc, stream:stream + 1], Gbc, ps[0], "Gbc", ["mod0"])
                make_bc(kb, nc, c, lambda kc: mod[:, 0 * 8 + kc, stream:stream + 1], Sbc, ps[0], "Sbc", ["mod0"])
                nt = 2 if stream == 1 else 16
                src = x_ctx_in if stream == 1 else x_lat_in
                base = 0 if stream == 1 else 256
                def htile(i):
                    b = i % 2
                    hbb, h32b = hbs[b], h32s[b]
                    kb.dma("sp", xb[b][:], src[i * 128:(i + 1) * 128, :], writes=[f"xb{b}"])
                    yield
                    yield from norm_tile_g(kb, nc, xb[b][:], f"xb{b}", stt[b], Gbc, Sbc, h32b, f"h32{b}")
                    yield
                    kb.op("act", lambda e: e.copy(out=hbb[:], in_=h32b), reads=[f"h32{b}"], writes=[f"hb{b}"])
                    yield
                    pos = base + (i if d == 0 else nt - 1 - i) * 128
                    for half in range(2):
                        pbk = 2 + half + 2 * b
                        for q in range(4):
                            kc = half * 4 + q
                            kb.op("pe", lambda e, kc=kc, q=q, pbk=pbk: e.matmul(
                                ps[pbk][:, q * 128:(q + 1) * 128], lhsT=hbb[:, kc * 128:(kc + 1) * 128],
                                rhs=(c.identb[:] if d == 0 else Jb[:]), start=True, stop=True),
                                reads=[f"hb{b}", "c_identb", "Jb"], writes=[f"ps{pbk}"])
                        yield
                        kb.op("dve" if half == 0 else "act", (lambda e, half=half, pos=pos, pbk=pbk: e.tensor_copy(
                            out=hT[:, half * 4:(half + 1) * 4, pos:pos + 128],
                            in_=ps[pbk][:].rearrange("p (q t) -> p q t", q=4))) if half == 0 else
                            (lambda e, half=half, pos=pos, pbk=pbk: e.copy(
                                out=hT[:, half * 4:(half + 1) * 4, pos:pos + 128],
                                in_=ps[pbk][:].rearrange("p (q t) -> p q t", q=4))),
                            reads=[f"ps{pbk}"], writes=[("hT", pos, half)])
                        yield
                pending = [htile(i) for i in range(nt)]
                active = []
                while pending or active:
                    if pending and len(active) < 2:
                        active.append(pending.pop(0))
                    for g_ in list(active):
                        try:
                            next(g_)
                        except StopIteration:
                            active.remove(g_)
            kb.barrier()

            def store_cb_factory(col0, dk_):
                def cb(ci, Ysb, yk):
                    r0 = ci * CHUNK
                    kb.dma("sp", Ys[d, r0:r0 + CHUNK, col0:col0 + dk_], Ysb[0:CHUNK, 0:dk_], reads=[yk],
                           writes=[("ys", d, ci, col0)])
                    if col0 < 512:
                        kb.dma("sp", Ys[d, r0:r0 + CHUNK, 1024 + col0:1024 + col0 + dk_], Ysb[0:CHUNK, dk_:2 * dk_],
                               reads=[yk], writes=[("ysb", d, ci, col0)])
                return cb

            project(1536 + d * 128, 128, F[0], FK[0])
            tshift(F[0], FK[0], F[10], FK[10], F[1], FK[1], 128, par128[:, d:d + 1])
            ew("act", lambda e: e.activation(out=F[10][0:64, :], in_=F[10][0:64, :], func=AF.Tanh), [FK[10]], [FK[10]])
            if d == 0:
                project(1792, 128, F[0], FK[0])
                tshift(F[0], FK[0], F[11], FK[11], F[1], FK[1], 128, par128[:, 2:3])
                ew("act", lambda e: e.activation(out=F[11][:], in_=F[11][:], func=AF.Sigmoid), [FK[11]], [FK[11]])
            for p in range(4):
                hk_ = f"pair{d}_{p}"
                PP = lambda j: parP[:, j:j + 1]
                for part, dst in ((0, 1), (1, 2), (2, 3)):
                    project(p * 384 + part * 128, 128, F[0], FK[0])
                    tshift(F[0], FK[0], F[dst], FK[dst], F[7], FK[7], 128, PP(p * 3 + part))
                for bi, (t0, tw, col0) in enumerate(BLOCKS):
                    cs = slice(col0, col0 + tw)
                    kb.op("pe", lambda e, cs=cs, tw=tw: e.matmul(ps[2][:, 0:tw], lhsT=w2sb[0:64, d, p * 128:(p + 1) * 128],
                                                               rhs=F[10][0:64, cs], start=True, stop=True),
                          reads=["w2sb", FK[10]], writes=["ps2"])
                    kb.op("pe", lambda e, cs=cs, tw=tw: e.matmul(ps[3][:, 0:tw], lhsT=w2sb[64:128, d, p * 128:(p + 1) * 128],
                                                               rhs=F[10][64:128, cs], start=True, stop=True),
                          reads=["w2sb", FK[10]], writes=["ps3"])
                    kb.op("act", lambda e, cs=cs, tw=tw: e.activation(out=F[4][:, cs], in_=ps[2][:, 0:tw],
                                                                    func=AF.Sigmoid, bias=PP(12 + d * 4 + p)),
                          reads=["ps2", "parP"], writes=[FK[4]])
                    kb.op("act", lambda e, cs=cs, tw=tw: e.activation(out=F[5][:, cs], in_=ps[3][:, 0:tw],
                                                                    func=AF.Sigmoid, bias=PP(20 + d * 4 + p)),
                          reads=["ps3", "parP"], writes=[FK[5]])
                kkc, kac, kac1, rkc = PP(28 + p), PP(32 + p), PP(36 + p), PP(40 + p)
                ew("dve", lambda e: e.tensor_scalar(out=F[7][:, :], in0=F[2][:, :], scalar1=kkc, scalar2=None,
                                                    op0=ALU.mult), [FK[2], "parP"], [FK[7]])
                for (a0_, n_) in DC:
                    ew("pool", lambda e, a0_=a0_, n_=n_: e.tensor_tensor(
                        out=F[0][:, a0_:a0_ + n_], in0=F[7][:, a0_:a0_ + n_], in1=F[7][:, a0_:a0_ + n_],
                        op=ALU.mult), [FK[7]], [FK[0]])
                for bi, (t0, tw, col0) in enumerate(BLOCKS):
                    cs = slice(col0, col0 + tw)
                    kb.op("pe", lambda e, cs=cs, tw=tw: e.matmul(ps[2][:, 0:tw], lhsT=bd[:, :],
                                                               rhs=F[0][:, cs], start=True, stop=True),
                          reads=["bd", FK[0]], writes=["ps2"])
                    kb.op("act", lambda e, cs=cs, tw=tw: e.activation(out=F[9][:, cs], in_=ps[2][:, 0:tw],
                                                                    func=AF.Sqrt, bias=c.eps6[:, 0:1]),
                          reads=["ps2", "c_eps"], writes=[FK[9]])
                for (a0_, n_) in DC:
                    cs = slice(a0_, a0_ + n_)
                    ew("dve", lambda e, cs=cs: e.reciprocal(out=F[9][:, cs], in_=F[9][:, cs]), [FK[9]], [FK[9]])
                    ew("dve", lambda e, cs=cs: e.tensor_tensor(out=F[6][:, cs], in0=F[7][:, cs], in1=F[9][:, cs],
                                                               op=ALU.mult), [FK[7], FK[9]], [FK[6]])
                    ew("pool", lambda e, cs=cs: e.tensor_scalar(out=F[7][:, cs], in0=F[5][:, cs], scalar1=kac,
                                                                scalar2=kac1, op0=ALU.mult, op1=ALU.add),
                       [FK[5], "parP"], [FK[7]])
                    ew("pool", lambda e, cs=cs: e.tensor_tensor(out=F[7][:, cs], in0=F[7][:, cs], in1=F[2][:, cs],
                                                                op=ALU.mult), [FK[7], FK[2]], [FK[7]])
                    ew("dve", lambda e, cs=cs: e.tensor_tensor(out=F[9][:, cs], in0=F[6][:, cs], in1=F[5][:, cs],
                                                               op=ALU.mult), [FK[6], FK[5]], [FK[9]])
                    ew("dve", lambda e, cs=cs: e.scalar_tensor_tensor(out=F[0][:, cs], in0=F[1][:, cs], scalar=rkc,
                                                                      in1=F[7][:, cs], op0=ALU.mult, op1=ALU.mult),
                       [FK[1], FK[7], "parP"], [FK[0]])
                ew("dve", lambda e: e.tensor_tensor_scan(out=F[8][:, :], data0=rmask[:, :], data1=F[4][:, :],
                                                         initial=0.0, op0=ALU.mult, op1=ALU.add),
                   ["rmask", FK[4]], [FK[8]])
                B4 = F[4][:, :].bitcast(BF16)
                B5 = F[5][:, :].bitcast(BF16)
                B2 = F[2][:, :].bitcast(BF16)
                DCS = [slice(a0_, a0_ + n_) for (a0_, n_) in DC]
                DCH = [slice(TP + a0_, TP + a0_ + n_) for (a0_, n_) in DC]
                for cs in DCS:
                    ew("pool", lambda e, cs=cs: e.tensor_tensor(out=F[2][:, cs], in0=F[8][:, cs], in1=F[4][:, cs],
                                                                op=ALU.subtract), [FK[8], FK[4]], [FK[2]])
                    ew("act", lambda e, cs=cs: e.activation(out=F[2][:, cs], in_=F[2][:, cs], func=AF.Exp,
                                                            scale=-DECAY_K), [FK[2]], [FK[2]])
                for cs in DCS:
                    ew("dve", lambda e, cs=cs: e.tensor_tensor(out=B4[:, cs], in0=F[6][:, cs], in1=F[2][:, cs],
                                                               op=ALU.mult), [FK[6], FK[2]], [FK[4]])
                for cs in DCS:
                    ew("act", lambda e, cs=cs: e.activation(out=F[2][:, cs], in_=F[8][:, cs], func=AF.Exp,
                                                            scale=DECAY_K), [FK[8], FK[2], FK[4]], [FK[2]])
                for cs, ch in zip(DCS, DCH):
                    ew("dve", lambda e, cs=cs, ch=ch: e.tensor_tensor(out=B4[:, ch], in0=F[7][:, cs], in1=F[2][:, cs],
                                                                      op=ALU.mult), [FK[7], FK[2]], [FK[4]])
                    ew("pool", lambda e, cs=cs: e.tensor_tensor(out=B5[:, cs], in0=F[9][:, cs], in1=F[2][:, cs],
                                                                op=ALU.mult), [FK[9], FK[2]], [FK[5]])
                for cs in DCS:
                    ew("act", lambda e, cs=cs: e.activation(out=F[8][:, cs], in_=F[8][:, cs], func=AF.Exp,
                                                            scale=-DECAY_K), [FK[8]], [FK[8]])
                for cs, ch in zip(DCS, DCH):
                    ew("dve", lambda e, cs=cs: e.tensor_tensor(out=B2[:, cs], in0=F[1][:, cs], in1=F[8][:, cs],
                                                               op=ALU.mult), [FK[1], FK[8], FK[4], FK[5]], [FK[2]])
                    ew("pool", lambda e, cs=cs, ch=ch: e.tensor_copy(out=B5[:, ch], in_=F[3][:, cs]), [FK[3]], [FK[5]])
                    ew("act", lambda e, cs=cs, ch=ch: e.copy(out=B2[:, ch], in_=F[0][:, cs]), [FK[0]], [FK[2]])
                HI = lambda B: B[:, TP:2 * TP]
                kb.op("pool", lambda e: e.memset(T["nR"][:], 0.0),
                      reads=[FK[2], FK[4], FK[5], FK[8]], writes=[hk_, "nR"])
                for hh in range(2):
                    dplr_scan(kb, nc, c, T, 64, B2[:, 0:TP], B4[:, 0:TP], HI(B4), B5[:, 0:TP], HI(B5), F[8], HI(B2),
                              store_cb_factory((2 * p + hh) * 64, 64), hk_, bp=hh * 64)
                kb.op("pool", lambda e: e.memset(T["nR"][:], 0.0), reads=[hk_],
                      writes=[FK[2], FK[4], FK[5], FK[8], "nR"])
            mixer_gdn_pass(kb, nc, g, c, T, d, F, FK, rmask, par128, sel, project, store_cb_factory, ps, DC, wv, wsl,
                           hT, Zs)
        kb.barrier()
        es2.close()
        import os
        if os.environ.get("MIX_DUMP"):
            kb.dma("sp", x_lat_out[0:2048, :], Ys[0, 0:2048, 0:1024], writes=["o1"])
            kb.dma("sp", x_ctx_out[0:256, :], Ys[0, 0:256, 512:1536], writes=["o2"])
            kb.barrier()
            return
        mixer_output(kb, nc, g, c, mods, T, F, FK, x_lat_in, x_ctx_in, x_lat_out, x_ctx_out, Ys, Zs, J32, None, None, hb,
                     ps, par128)
        kb.barrier()


def mixer_gdn_pass(kb, nc, g, c, T, d, F, FK, rmask, par128, sel, project, store_cb_factory, ps, DC, wv, wsl, hT, Zs):
    ew = lambda eng, fn, r, w: kb.op(eng, fn, reads=r, writes=w)
    project(3968, 16, F[10], FK[10])
    R16 = lambda i: F[i][0:16, :]
    ew("act", lambda e: e.activation(out=T["Uf"][0:16, 0:1], in_=par128[0:16, 41:42], func=AF.Exp), ["par128"], ["U"])
    ew("dve", lambda e: e.tensor_scalar(out=T["Uf"][0:16, 0:1], in0=T["Uf"][0:16, 0:1], scalar1=-1.0, scalar2=None,
                                        op0=ALU.mult), ["U"], ["U"])
    ew("dve", lambda e: e.tensor_scalar(out=R16(1), in0=R16(10), scalar1=par128[0:16, 40:41], scalar2=None, op0=ALU.add),
       [FK[10], "par128"], [FK[1]])
    ew("act", lambda e: e.activation(out=R16(4), in_=R16(10), func=AF.Sigmoid), [FK[10]], [FK[4]])
    ew("act", lambda e: e.activation(out=R16(2), in_=R16(1), func=AF.Abs), [FK[1]], [FK[2]])
    ew("act", lambda e: e.activation(out=R16(2), in_=R16(2), func=AF.Exp, scale=-1.0), [FK[2]], [FK[2]])
    ew("dve", lambda e: e.tensor_scalar(out=R16(3), in0=R16(2), scalar1=2.0, scalar2=None, op0=ALU.add), [FK[2]], [FK[3]])
    ew("dve", lambda e: e.reciprocal(out=R16(3), in_=R16(3)), [FK[3]], [FK[3]])
    ew("dve", lambda e: e.tensor_tensor(out=R16(2), in0=R16(2), in1=R16(3), op=ALU.mult), [FK[2], FK[3]], [FK[2]])
    ew("dve", lambda e: e.tensor_tensor(out=R16(3), in0=R16(2), in1=R16(2), op=ALU.mult), [FK[2]], [FK[3]])
    ew("dve", lambda e: e.tensor_scalar(out=R16(6), in0=R16(3), scalar1=1.0 / 13, scalar2=1.0 / 11, op0=ALU.mult,
                                        op1=ALU.add), [FK[3]], [FK[6]])
    for cf in (1.0 / 9, 1.0 / 7, 1.0 / 5, 1.0 / 3, 1.0):
        ew("dve", lambda e: e.tensor_tensor(out=R16(6), in0=R16(6), in1=R16(3), op=ALU.mult), [FK[6], FK[3]], [FK[6]])
        ew("dve", lambda e, cf=cf: e.tensor_scalar(out=R16(6), in0=R16(6), scalar1=cf, scalar2=None, op0=ALU.add),
           [FK[6]], [FK[6]])
    ew("dve", lambda e: e.scalar_tensor_tensor(out=R16(6), in0=R16(6), scalar=2.0, in1=R16(2), op0=ALU.mult, op1=ALU.mult),
       [FK[6], FK[2]], [FK[6]])
    ew("dve", lambda e: e.tensor_scalar(out=R16(1), in0=R16(1), scalar1=0.0, scalar2=None, op0=ALU.max), [FK[1]], [FK[1]])
    ew("dve", lambda e: e.tensor_tensor(out=R16(6), in0=R16(6), in1=R16(1), op=ALU.add), [FK[6], FK[1]], [FK[6]])
    ew("dve", lambda e: e.tensor_scalar(out=R16(6), in0=R16(6), scalar1=T["Uf"][0:16, 0:1], scalar2=None, op0=ALU.mult),
       [FK[6], "U"], [FK[6]])
    ew("dve", lambda e: e.tensor_tensor_scan(out=R16(10), data0=rmask[0:16, :], data1=R16(6), initial=0.0,
                                             op0=ALU.mult, op1=ALU.add), ["rmask", FK[6]], [FK[10]])
    for h in range(4):
        hk_ = f"ghead{d}_{h}"
        c0 = 1920 + h * 512
        for part, dst in ((0, 1), (1, 2), (2, 3)):
            project(c0 + part * 128, 128, F[0], FK[0])
            n = TP - 4
            for j in range(5):
                jj = j if d == 0 else 4 - j
                wc = par128[:, 48 + (h * 3 + part) * 5 + jj:49 + (h * 3 + part) * 5 + jj]
                if j == 0:
                    ew("dve", lambda e, wc=wc, dst=dst: e.tensor_scalar(out=F[dst][:, 2:2 + n], in0=F[0][:, 0:n],
                                                                      scalar1=wc, scalar2=None, op0=ALU.mult),
                       [FK[0], "par128"], [FK[dst]])
                else:
                    ew("dve", lambda e, wc=wc, dst=dst, j=j: e.scalar_tensor_tensor(
                        out=F[dst][:, 2:2 + n], in0=F[0][:, j:j + n], scalar=wc, in1=F[dst][:, 2:2 + n],
                        op0=ALU.mult, op1=ALU.add), [FK[0], FK[dst], "par128"], [FK[dst]])
            ew("act", lambda e, dst=dst: e.activation(out=F[dst][:, :], in_=F[dst][:, :], func=AF.Silu), [FK[dst]], [FK[dst]])
        for src, scl in ((1, float(128 ** -0.5)), (2, 1.0)):
            for (a0_, n_) in DC:
                ew("pool", lambda e, src=src, a0_=a0_, n_=n_: e.tensor_tensor(
                    out=F[0][:, a0_:a0_ + n_], in0=F[src][:, a0_:a0_ + n_], in1=F[src][:, a0_:a0_ + n_], op=ALU.mult),
                    [FK[src]], [FK[0]])
            for bi, (t0, tw, col0) in enumerate(BLOCKS):
                cs = slice(col0, col0 + tw)
                kb.op("pe", lambda e, cs=cs, tw=tw: e.matmul(ps[2][:, 0:tw], lhsT=c.ones[:, :], rhs=F[0][:, cs],
                                                           start=True, stop=True), reads=["c_ones", FK[0]], writes=["ps2"])
                kb.op("act", lambda e, cs=cs, tw=tw: e.activation(out=F[9][:, cs], in_=ps[2][:, 0:tw], func=AF.Sqrt,
                                                                bias=c.eps6[:, 0:1]), reads=["ps2", "c_eps"], writes=[FK[9]])
            for (a0_, n_) in DC:
                cs = slice(a0_, a0_ + n_)
                ew("dve", lambda e, cs=cs: e.reciprocal(out=F[9][:, cs], in_=F[9][:, cs]), [FK[9]], [FK[9]])
                ew("dve", lambda e, cs=cs, src=src, scl=scl: e.scalar_tensor_tensor(
                    out=F[src][:, cs], in0=F[src][:, cs], scalar=scl, in1=F[9][:, cs], op0=ALU.mult, op1=ALU.mult),
                    [FK[src], FK[9]], [FK[src]])
        ra, rb = d * 4 + h, 8 + d * 4 + h
        for bi, (t0, tw, col0) in enumerate(BLOCKS):
            cs = slice(col0, col0 + tw)
            kb.op("pe", lambda e, cs=cs, tw=tw: e.matmul(ps[2][:, 0:tw], lhsT=sel[0:16, ra, :], rhs=F[10][0:16, cs],
                                                       start=True, stop=True), reads=["sel", FK[10]], writes=["ps2"])
            kb.op("pe", lambda e, cs=cs, tw=tw: e.matmul(ps[3][:, 0:tw], lhsT=sel[0:16, rb, :], rhs=F[4][0:16, cs],
                                                       start=True, stop=True), reads=["sel", FK[4]], writes=["ps3"])
            kb.op("act", lambda e, cs=cs, tw=tw: e.activation(out=F[6][:, cs], in_=ps[2][:, 0:tw], func=AF.Exp),
                  reads=["ps2"], writes=[FK[6]])
            kb.op("dve", lambda e, cs=cs, tw=tw: e.tensor_copy(out=F[0][:, cs], in_=ps[2][:, 0:tw]),
                  reads=["ps2"], writes=[FK[0]])
            kb.op("act", lambda e, cs=cs, tw=tw: e.copy(out=F[9][:, cs], in_=ps[3][:, 0:tw]),
                  reads=["ps3"], writes=[FK[9]])
        GB5 = F[5][:, :].bitcast(BF16)
        for (a0_, n_) in DC:
            cs = slice(a0_, a0_ + n_)
            ew("dve", lambda e, cs=cs: e.tensor_tensor(out=F[9][:, cs], in0=F[9][:, cs], in1=F[2][:, cs], op=ALU.mult),
               [FK[9], FK[2]], [FK[9]])
            ew("pool", lambda e, cs=cs: e.tensor_tensor(out=GB5[:, cs], in0=F[2][:, cs], in1=F[6][:, cs], op=ALU.mult),
               [FK[2], FK[6]], [FK[5]])
            ew("dve", lambda e, cs=cs: e.tensor_tensor(out=GB5[:, TP + cs.start:TP + cs.stop], in0=F[1][:, cs],
                                                       in1=F[6][:, cs], op=ALU.mult),
               [FK[1], FK[6]], [FK[5]])
        kb.op("pool", lambda e: e.memset(T["nR"][:], 0.0), reads=[FK[0], FK[1], FK[2], FK[3], FK[5], FK[6], FK[9]],
              writes=[hk_, "nR"])
        dplr_scan(kb, nc, c, T, 128, F[1], F[2], F[9], F[9], F[3], F[6], None, store_cb_factory(512 + h * 128, 128), hk_,
                  Gb=F[0], rA=F[1], kkA=F[2], rC=GB5[:, TP:2 * TP], kkC=GB5[:, 0:TP])
        kb.op("pool", lambda e: e.memset(T["nR"][:], 0.0), reads=[hk_],
              writes=[FK[0], FK[1], FK[2], FK[3], FK[5], FK[6], FK[9], "nR"])
    if d == 0:
        n = 0
        for h in range(4):
            kb.dma("pool", wsl[:, :, 0:128], wv[:, :, 1920 + h * 512 + 384:1920 + h * 512 + 512], writes=["wsl"])
            for i in range(18):
                pb = ps[n % 2]
                for kc in range(8):
                    kb.op("pe", lambda e, kc=kc, i=i, pb=pb: e.matmul(pb[:, 0:128], lhsT=hT[:, kc, i * 128:(i + 1) * 128],
                                                                   rhs=wsl[:, kc, 0:128], start=(kc == 0), stop=(kc == 7)),
                          reads=["wsl", "hT"], writes=[f"ps{n % 2}"])
                zt = T["zt"][n % 2]
                kb.op("act", lambda e, pb=pb, zt=zt: e.activation(out=zt[:], in_=pb[:, 0:128], func=AF.Silu),
                      reads=[f"ps{n % 2}"], writes=[f"zt{n % 2}"])
                kb.dma("sp", Zs[i * 128:(i + 1) * 128, h * 128:(h + 1) * 128], zt[:], reads=[f"zt{n % 2}"],
                       writes=[("zs", i, h)])
                n += 1


def mixer_output(kb, nc, g, c, mods, T, F, FK, x_lat_in, x_ctx_in, x_lat_out, x_ctx_out, Ys, Zs, J32, xb, Gbc, hb, ps,
                 par128):
    ew = lambda eng, fn, r, w: kb.op(eng, fn, reads=r, writes=w)
    kb.barrier()
    from contextlib import ExitStack
    with ExitStack() as es:
        def sb(name, shape, dt):
            return es.enter_context(nc.sbuf_tensor(f"mo_{name}", shape, dt))
        wo = sb("wo", [128, 8, D], BF16)
        g2 = sb("g2", [128, 512], F32)
        bcp = sb("bcp", [128, 3, 512], F32)
        yf = sb("yf", [128, 1536], F32)
        yb = sb("yb", [128, 1536], F32)
        zt = sb("zt", [128, 512], F32)
        o = sb("o", [128, D], F32)
        t1 = sb("t1", [128, 512], F32)
        s8 = sb("s8", [128, 4, 8], F32)
        oT = sb("oT", [128, 8, 128], BF16)
        Gbc = sb("Gbc", [128, D], F32)
        xb = [sb("x0", [128, D], F32), sb("x1", [128, D], F32)]
        for kc in range(8):
            kb.dma("pool", wo[:, kc, :], g["e_w_out"][0].rearrange("(kc p) n -> p kc n", p=128)[:, kc, :], writes=["wo"])
        kb.dma("sp", g2[:], g["a_g2"][0], writes=["g2"])
        kb.dma("sp", bcp[:].rearrange("p a n -> p (a n)"), g["mx_bc"].partition_broadcast(128), writes=["bcp"])
        for stream in (1, 0):
            kb.barrier()
            make_bc(kb, nc, c, lambda kc: mods[0][:, 2 * 8 + kc, stream:stream + 1], Gbc, ps[0], "Gbc", ["mod0"])
            nt = 2 if stream == 1 else 16
            src = x_ctx_in if stream == 1 else x_lat_in
            dst = x_ctx_out if stream == 1 else x_lat_out
            base = 0 if stream == 1 else 256
            colbase = CTX0 if stream == 1 else LAT0
            for i in range(nt):
                b = i % 2
                r0 = base + i * 128
                rb0 = base + (nt - 1 - i) * 128
                kb.dma("sp", xb[b][:], src[i * 128:(i + 1) * 128, :], writes=[f"xb{b}"])
                kb.dma("sp", yf[:], Ys[0, r0:r0 + 128, :], reads=[("ysall",)], writes=["yf"])
                kb.dma("act", yb[:], Ys[1, rb0:rb0 + 128, :], reads=[("ysall",)], writes=["yb"])
                kb.dma("sp", zt[:], Zs[r0:r0 + 128, :], reads=[("ysall",)], writes=["zt"])
                for q in range(3):
                    kb.op("pe", lambda e, q=q: e.matmul(ps[1 + q][:, :], lhsT=J32[:, :], rhs=yb[:, q * 512:(q + 1) * 512],
                                                      start=True, stop=True), reads=["J32", "yb"], writes=[f"ps{1 + q}"])
                    ew("dve", lambda e, q=q: e.tensor_tensor(out=yf[:, q * 512:(q + 1) * 512], in0=yf[:, q * 512:(q + 1) * 512],
                                                             in1=ps[1 + q][:, :], op=ALU.add), [f"ps{1 + q}", "yf"], ["yf"])
                y3 = yf[:, 0:512].rearrange("p (h j) -> p h j", h=8)
                ew("dve", lambda e: e.reduce_sum(out=s8[:, 0, :], in_=y3, axis=AX.X), ["yf"], ["s8"])
                ew("dve", lambda e: e.tensor_scalar(out=s8[:, 0, :], in0=s8[:, 0, :], scalar1=-1.0 / 64, scalar2=None,
                                                    op0=ALU.mult), ["s8"], ["s8"])
                ew("dve", lambda e: e.tensor_tensor(out=y3, in0=y3, in1=s8[:, 0, :].unsqueeze(2).broadcast_to([128, 8, 64]),
                                                    op=ALU.add), ["yf", "s8"], ["yf"])
                ew("pool", lambda e: e.tensor_tensor(out=t1[:], in0=yf[:, 0:512], in1=yf[:, 0:512], op=ALU.mult), ["yf"], ["t1"])
                ew("dve", lambda e: e.reduce_sum(out=s8[:, 1, :], in_=t1[:].rearrange("p (h j) -> p h j", h=8), axis=AX.X),
                   ["t1"], ["s8"])
                ew("dve", lambda e: e.tensor_scalar(out=s8[:, 1, :], in0=s8[:, 1, :], scalar1=1.0 / 64, scalar2=64e-5,
                                                    op0=ALU.mult, op1=ALU.add), ["s8"], ["s8"])
                ew("act", lambda e: e.activation(out=s8[:, 1, :], in_=s8[:, 1, :], func=AF.Sqrt), ["s8"], ["s8"])
                ew("dve", lambda e: e.reciprocal(out=s8[:, 1, :], in_=s8[:, 1, :]), ["s8"], ["s8"])
                ew("dve", lambda e: e.tensor_tensor(out=y3, in0=y3, in1=s8[:, 1, :].unsqueeze(2).broadcast_to([128, 8, 64]),
                                                    op=ALU.mult), ["yf", "s8"], ["yf"])
                ew("dve", lambda e: e.tensor_tensor(out=yf[:, 0:512], in0=yf[:, 0:512], in1=bcp[:, 0, :], op=ALU.mult),
                   ["yf", "bcp"], ["yf"])
                ew("pool", lambda e: e.tensor_tensor(out=yf[:, 0:512], in0=yf[:, 0:512], in1=bcp[:, 1, :], op=ALU.add),
                   ["yf", "bcp"], ["yf"])
                ew("pool", lambda e: e.tensor_tensor(out=yf[:, 0:512], in0=yf[:, 0:512], in1=yf[:, 1024:1536], op=ALU.add),
                   ["yf"], ["yf"])
                cg = colbase + i * 128
                kb.op("pe", lambda e, cg=cg: e.matmul(ps[4][:, :], lhsT=F[11][:, cg:cg + 128], rhs=g2[:, :], start=True, stop=True),
                      reads=[FK[11], "g2"], writes=["ps4"])
                ew("dve", lambda e: e.tensor_tensor(out=o[:, 0:512], in0=yf[:, 0:512], in1=ps[4][:, :], op=ALU.mult),
                   ["yf", "ps4"], ["o"])
                ew("pool", lambda e: e.tensor_tensor(out=t1[:], in0=yf[:, 512:1024], in1=yf[:, 512:1024], op=ALU.mult),
                   ["yf"], ["t1"])
                ew("dve", lambda e: e.reduce_sum(out=s8[:, 2, 0:4], in_=t1[:].rearrange("p (h j) -> p h j", h=4), axis=AX.X),
                   ["t1"], ["s8"])
                ew("dve", lambda e: e.tensor_scalar(out=s8[:, 2, 0:4], in0=s8[:, 2, 0:4], scalar1=1.0 / 128, scalar2=EPS,
                                                    op0=ALU.mult, op1=ALU.add), ["s8"], ["s8"])
                ew("act", lambda e: e.activation(out=s8[:, 2, 0:4], in_=s8[:, 2, 0:4], func=AF.Sqrt), ["s8"], ["s8"])
                ew("dve", lambda e: e.reciprocal(out=s8[:, 2, 0:4], in_=s8[:, 2, 0:4]), ["s8"], ["s8"])
                ew("dve", lambda e: e.tensor_tensor(
                    out=t1[:].rearrange("p (h j) -> p h j", h=4), in0=yf[:, 512:1024].rearrange("p (h j) -> p h j", h=4),
                    in1=s8[:, 2, 0:4].unsqueeze(2).broadcast_to([128, 4, 128]), op=ALU.mult), ["yf", "s8"], ["t1"])
                ew("pool", lambda e: e.tensor_tensor(out=t1[:], in0=t1[:], in1=bcp[:, 2, :], op=ALU.mult), ["t1", "bcp"], ["t1"])
                ew("dve", lambda e: e.tensor_tensor(out=o[:, 512:1024], in0=t1[:], in1=zt[:], op=ALU.mult), ["t1", "zt"], ["o"])
                ew("act", lambda e: e.copy(out=hb[:], in_=o[:]), ["o"], ["hb"])
                pst = ps[0][:].bitcast(BF16)
                for kc in range(8):
                    kb.op("pe", lambda e, kc=kc, pst=pst: e.transpose(out=pst[:, kc * 128:(kc + 1) * 128],
                                                                    in_=hb[:, kc * 128:(kc + 1) * 128], identity=c.identb[:]),
                          reads=["hb", "c_identb"], writes=["ps0"])
                ew("act", lambda e, pst=pst: e.copy(out=oT[:], in_=pst[:].rearrange("p (k t) -> p k t", k=8)), ["ps0"], ["oT"])
                for half in range(2):
                    pp = ps[5 + half]
                    for kc in range(8):
                        kb.op("pe", lambda e, kc=kc, half=half, pp=pp: e.matmul(
                            pp[:], lhsT=oT[:, kc, :], rhs=wo[:, kc, half * 512:(half + 1) * 512], start=(kc == 0), stop=(kc == 7)),
                            reads=["oT", "wo"], writes=[f"ps{5 + half}"])
                    sl = slice(half * 512, (half + 1) * 512)
                    ew("dve", lambda e, pp=pp, sl=sl: e.tensor_tensor(out=t1[:], in0=pp[:], in1=Gbc[:, sl], op=ALU.mult),
                       [f"ps{5 + half}", "Gbc"], ["t1"])
                    ew("pool", lambda e, sl=sl, b=b: e.tensor_tensor(out=xb[b][:, sl], in0=xb[b][:, sl], in1=t1[:], op=ALU.add),
                       ["t1", f"xb{b}"], [f"xb{b}"])
                kb.dma("sp", dst[i * 128:(i + 1) * 128, :], xb[b][:], reads=[f"xb{b}"], writes=[("dram", dst.tensor.name, i)])
        kb.barrier()


_CACHE = {}


def kernel(**inputs):
    inp = {k: np.asarray(v) for k, v in inputs.items()}
    if "nc" not in _CACHE:
        _CACHE["nc"] = build(stages=("all",))[0]
    nc = _CACHE["nc"]
    in_maps = [host_inputs(inp, b) for b in range(8)]
    res = run_bass_kernel_spmd(nc, in_maps, core_ids=list(range(8)))
    return np.stack([np.asarray(r["out"], dtype=np.float32) for r in res.results], axis=0)
```

```python
import numpy as np
import concourse.bass as bass
import concourse.mybir as mybir
from concourse.bass_utils import run_bass_kernel_spmd

F32 = mybir.dt.float32
BF16 = mybir.dt.bfloat16
I32 = mybir.dt.int32
U32 = mybir.dt.uint32
ALU = mybir.AluOpType
AF = mybir.ActivationFunctionType
AX = mybir.AxisListType

SEM_ROTATE = 20000
N_DMA_SEMS = 28
N_HW_SEMS = 18


class KB:
    def __init__(self, nc, same_engine_sync=True):
        self.nc = nc
        self.engs = {"pe": nc.tensor, "act": nc.scalar, "dve": nc.vector, "pool": nc.gpsimd, "sp": nc.sync}
        self.same_engine_sync = same_engine_sync
        self.esem = {}
        self.ecnt = {}
        self.sem_id = 0
        for e in ("pe", "act", "dve", "pool"):
            self._new_esem(e)
        self.dsems = [self._alloc_sem(f"dma{i}") for i in range(N_DMA_SEMS)]
        self.dcnt = [0] * N_DMA_SEMS
        self.dnext = 0
        self.dnext_sw = 0
        self.known = {e: {} for e in self.engs}
        self.state = {}
        self.n_ins = 0
        self._uid = 0
        self.out_tokens = []

    def _alloc_sem(self, name):
        self.sem_id += 1
        return self.nc.alloc_semaphore(f"{name}_{self.sem_id}")

    def _new_esem(self, e):
        self.esem[e] = self._alloc_sem(f"s_{e}")
        self.ecnt[e] = 0

    def uid(self, p="t"):
        self._uid += 1
        return f"{p}{self._uid}"

    def _deps(self, reads, writes):
        deps = []
        for r in reads:
            st = self.state.get(r)
            if st and st[0] is not None:
                deps.append(st[0])
        for w in writes:
            st = self.state.get(w)
            if st:
                if st[0] is not None:
                    deps.append(st[0])
                deps.extend(st[1].values())
        return deps

    def _wait(self, e, deps):
        eng = self.engs[e]
        kn = self.known[e]
        best = {}
        for (sem, val, src) in deps:
            if src == e and not (self.same_engine_sync and e != "pe"):
                continue
            key = id(sem)
            if kn.get(key, 0) >= val:
                continue
            if key not in best or best[key][1] < val:
                best[key] = (sem, val)
        for key, (sem, val) in best.items():
            eng.wait_ge(sem, val)
            kn[key] = val
            self.n_ins += 1

    def _commit(self, token, reads, writes):
        for w in writes:
            self.state[w] = [token, {}]
        for r in reads:
            st = self.state.get(r)
            if st is None:
                st = [None, {}]
                self.state[r] = st
            st[1][id(token[0])] = token

    def op(self, e, fn, reads=(), writes=()):
        reads = list(reads)
        writes = list(writes)
        writes += [r for r in reads if isinstance(r, str) and r.startswith("ps")]
        self._wait(e, self._deps(reads, writes))
        if self.ecnt[e] >= SEM_ROTATE:
            self._new_esem(e)
        ins = fn(self.engs[e])
        self.ecnt[e] += 1
        ins.then_inc(self.esem[e], 1)
        token = (self.esem[e], self.ecnt[e], e)
        self._commit(token, reads, writes)
        self.n_ins += 1
        return token

    def dma(self, q, out, in_, reads=(), writes=(), **kw):
        reads = list(reads)
        writes = list(writes)
        if q == "pool":
            i = N_HW_SEMS + self.dnext_sw
            self.dnext_sw = (self.dnext_sw + 1) % (N_DMA_SEMS - N_HW_SEMS)
        else:
            i = self.dnext
            self.dnext = (self.dnext + 1) % N_HW_SEMS
        deps = self._deps(reads, writes)
        if self.dcnt[i] > 0:
            deps.append((self.dsems[i], self.dcnt[i], "dma"))
        self._wait(q, deps)
        ins = self.engs[q].dma_start(out=out, in_=in_, **kw)
        self.dcnt[i] += 16
        ins.then_inc(self.dsems[i], 16)
        token = (self.dsems[i], self.dcnt[i], "dma")
        self._commit(token, reads, writes)
        self.n_ins += 1
        return token

    def finish(self, out_keys):
        deps = []
        for k in out_keys:
            st = self.state.get(k)
            if st and st[0] is not None:
                deps.append(st[0])
        self._wait("sp", deps)
        deps = [(self.dsems[i], self.dcnt[i], "dma") for i in range(N_DMA_SEMS) if self.dcnt[i] > 0]
        self._wait("sp", deps)

    def barrier(self):
        deps = [(self.esem[e], self.ecnt[e], "x") for e in self.esem if self.ecnt[e] > 0]
        deps += [(self.dsems[i], self.dcnt[i], "dma") for i in range(N_DMA_SEMS) if self.dcnt[i] > 0]
        for e in self.engs:
            self._wait(e, deps)
        self.state = {}


D = 1024
KC = 8
EPS = 1e-6
TP = 2310
CTX0, LAT0 = 2, 260
CHUNK = 128
CHUNK_COLS = [CTX0 + CHUNK * j for j in range(256 // CHUNK)] + [LAT0 + CHUNK * j for j in range(2048 // CHUNK)]
BLOCKS = [(0, 256, CTX0)] + [(256 + 512 * j, 512, LAT0 + 512 * j) for j in range(4)]
DECAY_K = float(np.exp(-0.5))


class Ctx:
    pass


def load_consts(kb, nc, es, g):
    c = Ctx()
    c.ident = es.enter_context(nc.sbuf_tensor("c_ident", [128, 128], F32))
    c.identb = es.enter_context(nc.sbuf_tensor("c_identb", [128, 128], BF16))
    c.ones = es.enter_context(nc.sbuf_tensor("c_ones", [128, 128], F32))
    c.iota = es.enter_context(nc.sbuf_tensor("c_iota", [128, 256], F32))
    kb.dma("sp", c.ident[:], g["k_ident"][:, :], writes=["c_ident"])
    kb.dma("sp", c.iota[:], g["k_iota"][:, :], writes=["c_iota"])
    kb.op("dve", lambda e: e.memset(c.ones[:], 1.0), writes=["c_ones"])
    c.eps6 = es.enter_context(nc.sbuf_tensor("c_eps6", [128, 1], F32))
    c.one1 = es.enter_context(nc.sbuf_tensor("c_one1", [128, 1], F32))
    kb.op("dve", lambda e: e.memset(c.eps6[:], 1e-6), writes=["c_eps"])
    kb.op("dve", lambda e: e.memset(c.one1[:], 1.0), writes=["c_eps"])
    c.onesb = es.enter_context(nc.sbuf_tensor("c_onesb", [128, 8], BF16))
    kb.op("dve", lambda e: e.memset(c.onesb[:], 1.0), writes=["c_ones"])
    kb.op("dve", lambda e: e.tensor_copy(out=c.identb[:], in_=c.ident[:]), reads=["c_ident"], writes=["c_identb"])
    return c


def prologue(kb, nc, es, g, c):
    mods = []
    for l in range(2):
        mods.append(es.enter_context(nc.sbuf_tensor(f"mod{l}", [128, 48, 2], F32)))
    with nc.sbuf_tensor("pl_sc", [128, 2, 8], F32) as sc, \
            nc.sbuf_tensor("pl_w0", [128, 8, 512], F32) as w0, \
            nc.sbuf_tensor("pl_w1", [128, 8, 512], F32) as w1, \
            nc.sbuf_tensor("pl_b", [128, 2, 48], F32) as adab, \
            nc.sbuf_tensor("pl_n", [128, 2, 2, 8], F32) as nrm, \
            nc.psum_tensor("pl_ps", [128, 512], F32) as ps:
        wb = [w0, w1]
        kb.dma("sp", sc[:, 0, :], g["cT"][:, :], writes=["sc"])
        kb.dma("sp", sc[:, 1, :], g["ccT"][:, :], writes=["sc"])
        kb.dma("sp", adab[:], g["ada_bT"][:, :, :], writes=["adab"])
        kb.dma("sp", nrm[:], g["normT"][:, :, :, :], writes=["nrm"])
        kb.op("act", lambda e: e.activation(out=sc[:], in_=sc[:], func=AF.Silu), reads=["sc"], writes=["sc"])
        blk = 0
        for l in range(2):
            wv = g["ada_w"][l].rearrange("(kc p) n -> p kc n", p=128)
            for nb in range(12):
                wt = wb[blk % 2]
                wk = f"plw{blk % 2}"
                kb.dma("sp" if blk % 2 == 0 else "act", wt[:], wv[:, :, nb * 512:(nb + 1) * 512], writes=[wk])
                for j in range(4):
                    for kc in range(8):
                        kb.op("pe", lambda e, kc=kc, j=j, wt=wt: e.matmul(
                            ps[:, (j * 2):(j * 2 + 2)], lhsT=wt[:, kc, j * 128:(j + 1) * 128], rhs=sc[:, :, kc],
                            start=(kc == 0), stop=(kc == 7)), reads=[wk, "sc"], writes=["psPL"])
                kb.op("dve", lambda e, l=l, nb=nb: e.tensor_tensor(
                    out=mods[l][:, nb * 4:(nb + 1) * 4, :],
                    in0=ps[:, 0:8].rearrange("p (j s) -> p j s", s=2),
                    in1=adab[:, l, nb * 4:(nb + 1) * 4].unsqueeze(2).broadcast_to([128, 4, 2]),
                    op=ALU.add), reads=["psPL", "adab"], writes=[f"mod{l}"])
                blk += 1
            for (m, which) in ((1, 0), (4, 1)):
                for s in range(2):
                    kb.op("dve", lambda e, l=l, m=m, which=which, s=s: e.scalar_tensor_tensor(
                        out=mods[l][:, m * 8:(m + 1) * 8, s], in0=mods[l][:, m * 8:(m + 1) * 8, s], scalar=1.0,
                        in1=nrm[:, l, which, :], op0=ALU.add, op1=ALU.mult),
                        reads=[f"mod{l}", "nrm"], writes=[f"mod{l}"])
    kb.barrier()
    return mods


def make_bc(kb, nc, c, col_ap_fn, out_tile, ps, key, src_keys):
    with nc.sbuf_tensor(kb.uid("bcd"), [128, 128], F32) as dg:
        dk = kb.uid("dg")
        for half in range(2):
            for q in range(4):
                kc = half * 4 + q
                kb.op("dve", lambda e, kc=kc: e.tensor_scalar(
                    out=dg[:], in0=c.ident[:], scalar1=col_ap_fn(kc), scalar2=None, op0=ALU.mult),
                    reads=["c_ident"] + src_keys, writes=[dk])
                kb.op("pe", lambda e, q=q: e.matmul(ps[:, q * 128:(q + 1) * 128], lhsT=c.ones[:], rhs=dg[:],
                                                   start=True, stop=True),
                      reads=[dk, "c_ones"], writes=["psBC" + key])
            kb.op("act", lambda e, half=half: e.copy(out=out_tile[:, half * 512:(half + 1) * 512], in_=ps[:]),
                  reads=["psBC" + key], writes=[key])
        kb.barrier()


def norm_tile_g(kb, nc, xt, xk, st, G, S, hout, hk, eps=EPS):
    sk = kb.uid("st")
    kb.op("act", lambda e: e.activation(out=hout, in_=xt, func=AF.Square, accum_out=st[:, 0:1]),
          reads=[xk], writes=[hk, sk])
    yield
    kb.op("dve", lambda e: e.tensor_scalar(out=st[:, 1:2], in0=st[:, 0:1], scalar1=1.0 / D, scalar2=eps,
                                           op0=ALU.mult, op1=ALU.add), reads=[sk], writes=[sk])
    yield
    kb.op("act", lambda e: e.activation(out=st[:, 2:3], in_=st[:, 1:2], func=AF.Sqrt), reads=[sk], writes=[sk])
    yield
    kb.op("dve", lambda e: e.reciprocal(out=st[:, 3:4], in_=st[:, 2:3]), reads=[sk], writes=[sk])
    yield
    if G is None:
        kb.op("dve", lambda e: e.tensor_scalar(out=hout, in0=xt, scalar1=st[:, 3:4], scalar2=None, op0=ALU.mult),
              reads=[xk, sk], writes=[hk])
        return
    kb.op("dve", lambda e: e.scalar_tensor_tensor(out=hout, in0=xt, scalar=st[:, 3:4], in1=G[:],
                                                  op0=ALU.mult, op1=ALU.mult),
          reads=[xk, sk, "Gbc"], writes=[hk])
    yield
    if S is not None:
        kb.op("pool", lambda e: e.tensor_tensor(out=hout, in0=hout, in1=S[:], op=ALU.add),
              reads=[hk, "Sbc"], writes=[hk])


def norm_tile(*a, **k):
    for _ in norm_tile_g(*a, **k):
        pass


def moe_stage(kb, nc, g, c, mods, layer, stream, x_in, x_out, T, final_norm=False, comp=None):
    NT = T // 128
    cap = 2 * T // 16
    CW = cap
    CT = (cap + 127) // 128
    cs = min(cap, 128)
    mod = mods[layer]
    xin_v = x_in.rearrange("(n p) d -> n p d", p=128)
    xout_v = x_out.rearrange("(n p) d -> n p d", p=128)
    sx = f"L{layer}s{stream}"
    from contextlib import ExitStack
    with ExitStack() as es:
        def sb(name, shape, dt):
            return es.enter_context(nc.sbuf_tensor(f"moe_{name}_{sx}", shape, dt))
        Gbc = sb("G", [128, D], F32)
        Sbc = sb("S", [128, D], F32)
        gate2 = Sbc
        hbf = sb("hbf", [128, NT, D], BF16)
        xb = [sb("x0", [128, D], F32), sb("x1", [128, D], F32)]
        h32 = [sb("h0", [128, D], F32), sb("h1", [128, D], F32)]
        hT = [sb("hT0", [128, 8, 128], F32), sb("hT1", [128, 8, 128], F32)]
        stt = [sb("st0", [128, 4], F32), sb("st1", [128, 4], F32)]
        rt = sb("rt", [128, 8, 16], F32)
        aff = sb("aff", [128, NT, 16], F32)
        sm = sb("sm", [128, 4], F32)
        ex = sb("ex", [128, 16], F32)
        affT = sb("affT", [16, T], F32)
        work = sb("work", [16, T], F32)
        mx8 = sb("mx8", [16, 8], F32)
        maskT = sb("maskT", [16, T], F32)
        onesT = work
        slotT = sb("slotT", [16, T], F32)
        gateT = affT
        slot = sb("slot", [128, NT, 16], F32)
        gate = sb("gate", [128, NT, 16], F32)
        selT = [sb("selT0", [128, CW], BF16), sb("selT1", [128, CW], BF16)]
        xeT = sb("xeT", [128, 8, CW], BF16)
        big = sb("big", [128, 4 * 8 * D], BF16)
        wviews = [big[:, j * 8 * D:(j + 1) * 8 * D].rearrange("p (k n) -> p k n", n=D) for j in range(4)]
        w1b = [wviews[0], wviews[1]]
        w3b = [wviews[2]]
        w2b = [wviews[3]]
        yest = [sb("yest0", [128, CT, D], BF16), sb("yest1", [128, CT, D], BF16)]
        sil = [sb("sil0", [128, CW], F32), sb("sil1", [128, CW], F32)]
        hidT = sb("hidT", [128, 8, CW], BF16)
        yeall = big[:, 0:16 * CT * D].rearrange("p (e c n) -> p e c n", e=16, c=CT)
        selGa = [sb("selGa0", [128, 4, CW], BF16), sb("selGa1", [128, 4, CW], BF16)]
        selGca = [sb("selGca0", [128, 4 * CT, 128], BF16), sb("selGca1", [128, 4 * CT, 128], BF16)]
        tmpo = [sb("tmpo0", [128, 512], F32), sb("tmpo1", [128, 512], F32)]
        ps = [es.enter_context(nc.psum_tensor(f"moe_ps{i}_{sx}", [128, 512], F32)) for i in range(8)]

        class SC:
            pass
        S0 = SC()
        S0.T, S0.NT, S0.cap, S0.CW, S0.CT, S0.cs, S0.stream = T, NT, cap, CW, CT, cs, stream
        S0.xin_v, S0.xout_v, S0.hbf, S0.aff, S0.slot, S0.gate, S0.ye_scr = xin_v, xout_v, hbf, aff, slot, gate, g["ye_scr"]
        streams = [S0]
        if comp is not None:
            S1 = SC()
            S1.T, S1.NT, S1.cap, S1.CW, S1.CT, S1.cs, S1.stream = 256, 2, 32, 32, 1, 32, 1
            S1.xin_v = comp[0].rearrange("(n p) d -> n p d", p=128)
            S1.xout_v = comp[1].rearrange("(n p) d -> n p d", p=128)
            S1.hbf = sb("hbfc", [128, 2, D], BF16)
            S1.aff = sb("affc", [128, 2, 16], F32)
            S1.slot = sb("slotc", [128, 2, 16], F32)
            S1.gate = sb("gatec", [128, 2, 16], F32)
            S1.ye_scr = g["ye_scr_c"]
            selTc = [sb("selTc0", [128, 32], BF16), sb("selTc1", [128, 32], BF16)]
            xeTc = sb("xeTc", [128, 8, 32], BF16)
            silc = sb("silc", [128, 8, 32], F32)
            hidTc = sb("hidTc", [128, 8, 32], BF16)
            yestc = [sb("yestc0", [32, 1, D], BF16)] * 2
            streams = [S1, S0]
        affT_full, work_full, maskT_full, slotT_full = affT, work, maskT, slotT
        kb.dma("sp", rt[:], g["moe_router"][layer].rearrange("(kc p) e -> p kc e", p=128), writes=["rt"])
        for S in streams:
            T, NT, cap, CW, CT, cs, stream = S.T, S.NT, S.cap, S.CW, S.CT, S.cs, S.stream
            xin_v, xout_v, hbf, aff, slot, gate = S.xin_v, S.xout_v, S.hbf, S.aff, S.slot, S.gate
            affT, work, maskT, slotT = affT_full[:, 0:T], work_full[:, 0:T], maskT_full[:, 0:T], slotT_full[:, 0:T]
            onesT, gateT = work, affT
            kb.barrier()
            make_bc(kb, nc, c, lambda kc: mod[:, 4 * 8 + kc, stream:stream + 1], Gbc, ps[0], "Gbc", [f"mod{layer}"])
            make_bc(kb, nc, c, lambda kc: mod[:, 3 * 8 + kc, stream:stream + 1], Sbc, ps[0], "Sbc", [f"mod{layer}"])

            def stageA(i):
                b = i % 2
                kb.dma("sp", xb[b][:], xin_v[i], writes=[f"xb{b}"])
                yield
                yield from norm_tile_g(kb, nc, xb[b][:], f"xb{b}", stt[b], Gbc, Sbc, h32[b][:], f"h32{b}")
                yield
                kb.op("act", lambda e, i=i, b=b: e.copy(out=hbf[:, i, :], in_=h32[b][:]), reads=[f"h32{b}"],
                      writes=[f"hbf{stream}_{i}"])
                for half in range(2):
                    for q in range(4):
                        kc = half * 4 + q
                        kb.op("pe", lambda e, kc=kc, q=q, b=b, half=half: e.transpose(
                            out=ps[half][:, q * 128:(q + 1) * 128], in_=h32[b][:, kc * 128:(kc + 1) * 128],
                            identity=c.ident[:]), reads=[f"h32{b}", "c_ident"], writes=[f"ps{half}"])
                    yield
                    kb.op("dve" if half == 0 else "act", (lambda e, half=half, b=b: e.tensor_copy(
                        out=hT[b][:, half * 4:(half + 1) * 4, :], in_=ps[half][:].rearrange("p (q t) -> p q t", q=4)))
                        if half == 0 else (lambda e, half=half, b=b: e.copy(
                            out=hT[b][:, half * 4:(half + 1) * 4, :], in_=ps[half][:].rearrange("p (q t) -> p q t", q=4))),
                        reads=[f"ps{half}"], writes=[f"hT{b}"])
                    yield

            def stageB(i):
                b = i % 2
                for kc in range(8):
                    kb.op("pe", lambda e, kc=kc, b=b: e.matmul(ps[2][:, 0:16], lhsT=hT[b][:, kc, :], rhs=rt[:, kc, :],
                                                             start=(kc == 0), stop=(kc == 7)),
                          reads=[f"hT{b}", "rt"], writes=["ps2"])
                yield
                kb.op("dve", lambda e: e.reduce_max(out=sm[:, 0:1], in_=ps[2][:, 0:16], axis=AX.X),
                      reads=["ps2"], writes=["sm"])
                kb.op("dve", lambda e: e.tensor_scalar(out=sm[:, 1:2], in0=sm[:, 0:1], scalar1=-1.0, scalar2=None,
                                                       op0=ALU.mult), reads=["sm"], writes=["sm"])
                yield
                kb.op("act", lambda e: e.activation(out=ex[:], in_=ps[2][:, 0:16], func=AF.Exp, bias=sm[:, 1:2],
                                                    accum_out=sm[:, 2:3]), reads=["ps2", "sm"], writes=["ex", "sm"])
                yield
                kb.op("dve", lambda e: e.reciprocal(out=sm[:, 3:4], in_=sm[:, 2:3]), reads=["sm"], writes=["sm"])
                kb.op("dve", lambda e, i=i: e.tensor_scalar(out=aff[:, i, :], in0=ex[:], scalar1=sm[:, 3:4], scalar2=None,
                                                            op0=ALU.mult), reads=["ex", "sm"], writes=["aff"])
                yield
                kb.op("pe", lambda e, i=i: e.transpose(out=ps[3][0:16, 0:128], in_=aff[:, i, :], identity=c.ident[:]),
                      reads=["aff", "c_ident"], writes=["ps3"])
                yield
                kb.op("act", lambda e, i=i: e.copy(out=affT[:, i * 128:(i + 1) * 128], in_=ps[3][0:16, 0:128]),
                      reads=["ps3"], writes=["affT"])
                yield

            from itertools import zip_longest
            prevB = iter(())
            for i in range(NT):
                for _ in zip_longest(stageA(i), prevB):
                    pass
                prevB = stageB(i)
            for _ in prevB:
                pass

            import os
            PH = int(os.environ.get("MOE_PH", "9"))
            if PH < 2:
                kb.dma("sp", x_out[0:128, 0:NT * 16], aff[:].rearrange("p n e -> p (n e)"), reads=["aff"], writes=["o"])
                kb.barrier()
                return
            kb.op("dve", lambda e: e.tensor_copy(out=work[:], in_=affT[:]), reads=["affT"], writes=["work"])
            nr = cap // 8
            for r in range(nr):
                kb.op("dve", lambda e: e.max(out=mx8[:], in_=work[:]), reads=["work"], writes=["mx8"])
                if r < nr - 1:
                    kb.op("dve", lambda e: e.match_replace(out=work[:], in_to_replace=mx8[:], in_values=work[:],
                                                           imm_value=-1.0), reads=["work", "mx8"], writes=["work"])
            kb.op("dve", lambda e: e.tensor_scalar(out=maskT[:], in0=affT[:], scalar1=mx8[:, 7:8], scalar2=None,
                                                   op0=ALU.is_ge), reads=["affT", "mx8"], writes=["maskT"])
            kb.op("pool", lambda e: e.memset(onesT[:], 1.0), reads=[], writes=["work"])
            kb.op("dve", lambda e: e.tensor_tensor_scan(out=slotT[:], data0=onesT[:], data1=maskT[:], initial=0.0,
                                                        op0=ALU.mult, op1=ALU.add),
                  reads=["work", "maskT"], writes=["slotT"])
            kb.op("dve", lambda e: e.tensor_tensor(out=slotT[:], in0=slotT[:], in1=maskT[:], op=ALU.mult),
                  reads=["slotT", "maskT"], writes=["slotT"])
            kb.op("dve", lambda e: e.tensor_scalar(out=slotT[:], in0=slotT[:], scalar1=-1.0, scalar2=None, op0=ALU.add),
                  reads=["slotT"], writes=["slotT"])
            kb.op("pool", lambda e: e.tensor_tensor(out=gateT[:], in0=affT[:], in1=maskT[:], op=ALU.mult),
                  reads=["affT", "maskT"], writes=["affT"])
            for i in range(NT):
                kb.op("pe", lambda e, i=i: e.transpose(out=ps[0][:, i * 16:(i + 1) * 16],
                                                       in_=slotT[:, i * 128:(i + 1) * 128], identity=c.ident[0:16, 0:16]),
                      reads=["slotT", "c_ident"], writes=["ps0"])
                kb.op("pe", lambda e, i=i: e.transpose(out=ps[1][:, i * 16:(i + 1) * 16],
                                                       in_=gateT[:, i * 128:(i + 1) * 128], identity=c.ident[0:16, 0:16]),
                      reads=["affT", "c_ident"], writes=["ps1"])
            kb.op("dve", lambda e: e.tensor_copy(out=slot[:], in_=ps[0][:, 0:NT * 16].rearrange("p (n e) -> p n e", e=16)),
                  reads=["ps0"], writes=["slot"])
            kb.op("act", lambda e: e.copy(out=gate[:], in_=ps[1][:, 0:NT * 16].rearrange("p (n e) -> p n e", e=16)),
                  reads=["ps1"], writes=["gate"])

            if PH < 3:
                kb.dma("sp", x_out[0:128, 0:NT * 16], slot[:].rearrange("p n e -> p (n e)"), reads=["slot"], writes=["o"])
                kb.dma("sp", x_out[128:256, 0:NT * 16], gate[:].rearrange("p n e -> p (n e)"), reads=["gate"], writes=["o2"])
                kb.barrier()
                return

        stg = [xb[0], xb[1], h32[0], h32[1]]
        stgk = ["xb0", "xb1", "h320", "h321"]
        wcnt = [0]

        def load_w(wv, wt, wk):
            for hh in range(2):
                kb.dma("pool", wt[:, hh * 4:(hh + 1) * 4, :], wv[:, hh * 4:(hh + 1) * 4, :], writes=[wk])

        def wviews_of(e_):
            return (g["moe_w1"][layer, e_].rearrange("(kc p) n -> p kc n", p=128),
                    g["moe_w3"][layer, e_].rearrange("(kc p) n -> p kc n", p=128),
                    g["moe_w2"][layer, e_].rearrange("(kc p) n -> p kc n", p=128))
        nsel = 0
        SUB = int(os.environ.get("MOE_SUB", "9"))
        NEXP = int(os.environ.get("MOE_NEXP", "16"))
        wv1, wv3, wv2 = wviews_of(0)
        load_w(wv1, w1b[0], "w1_0")
        load_w(wv3, w3b[0], "w3")
        load_w(wv2, w2b[0], "w2")
        for ex_i in range(NEXP):
            w1t = w1b[ex_i % 2]
            w1k = f"w1_{ex_i % 2}"
            if ex_i + 1 < NEXP:
                nwv1, nwv3, nwv2 = wviews_of(ex_i + 1)
                load_w(nwv1, w1b[(ex_i + 1) % 2], f"w1_{(ex_i + 1) % 2}")
            for i in range(NT):
                b = nsel % 2
                nsel += 1
                kb.op("dve", lambda e, i=i, b=b, ex_i=ex_i: e.tensor_scalar(
                    out=selT[b][:], in0=c.iota[:, 0:CW], scalar1=slot[:, i, ex_i:ex_i + 1], scalar2=None,
                    op0=ALU.is_equal), reads=["c_iota", "slot"], writes=[f"selT{b}"])
                for kc in range(8):
                    bank = (kc * CW) // 512
                    off = (kc * CW) % 512
                    kb.op("pe", lambda e, i=i, b=b, kc=kc, bank=bank, off=off: e.matmul(
                        ps[bank][:, off:off + CW], lhsT=hbf[:, i, kc * 128:(kc + 1) * 128], rhs=selT[b][:],
                        start=(i == 0 and off == 0), stop=(i == NT - 1), skip_group_check=True), reads=[f"hbf{stream}_{i}", f"selT{b}"], writes=[f"ps{bank}"])
            nb = (8 * CW + 511) // 512
            per = 512 // CW if CW < 512 else 1
            for bank in range(nb):
                k0 = bank * per
                k1 = min(8, k0 + per)
                kb.op("act" if bank % 2 else "dve", (lambda e, bank=bank, k0=k0, k1=k1: e.copy(
                    out=xeT[:, k0:k1, :], in_=ps[bank][:, 0:(k1 - k0) * CW].rearrange("p (k c) -> p k c", c=CW)))
                    if bank % 2 else (lambda e, bank=bank, k0=k0, k1=k1: e.tensor_copy(
                        out=xeT[:, k0:k1, :], in_=ps[bank][:, 0:(k1 - k0) * CW].rearrange("p (k c) -> p k c", c=CW))),
                    reads=[f"ps{bank}"], writes=["xeT"])
            if SUB < 2:
                continue
            for fc in range(8):
                pb = ps[4 + fc % 2]
                pk = f"ps{4 + fc % 2}"
                for kc in range(8):
                    kb.op("pe", lambda e, fc=fc, kc=kc, pb=pb, w1t=w1t: e.matmul(
                        pb[:, 0:CW], lhsT=w1t[:, kc, fc * 128:(fc + 1) * 128], rhs=xeT[:, kc, :],
                        start=(kc == 0), stop=(kc == 7)), reads=[w1k, "xeT"], writes=[pk])
                for kc in range(8):
                    kb.op("pe", lambda e, fc=fc, kc=kc, pb=pb: e.matmul(
                        pb[:, 256:256 + CW], lhsT=w3b[0][:, kc, fc * 128:(fc + 1) * 128], rhs=xeT[:, kc, :],
                        start=(kc == 0), stop=(kc == 7)), reads=["w3", "xeT"], writes=[pk])
                sb_ = sil[fc % 2]
                DBG = int(os.environ.get("MOE_DBG", "9"))
                if DBG < 1:
                    continue
                kb.op("act", lambda e, pb=pb, sb_=sb_: e.activation(out=sb_[:], in_=pb[:, 0:CW], func=AF.Silu),
                      reads=[pk], writes=[f"sil{fc % 2}"])
                if DBG < 2:
                    continue
                kb.op("dve", lambda e, pb=pb, sb_=sb_, fc=fc: e.tensor_tensor(
                    out=hidT[:, fc, :], in0=sb_[:], in1=pb[:, 256:256 + CW], op=ALU.mult),
                    reads=[f"sil{fc % 2}", pk], writes=["hidT"])
            if comp is not None:
                for i in range(2):
                    sc_ = selTc[i]
                    kb.op("dve", lambda e, i=i, sc_=sc_, ex_i=ex_i: e.tensor_scalar(
                        out=sc_[:], in0=c.iota[:, 0:32], scalar1=S1.slot[:, i, ex_i:ex_i + 1], scalar2=None,
                        op0=ALU.is_equal), reads=["c_iota", "slotc"], writes=[f"selTc{i}"])
                    for kc in range(8):
                        kb.op("pe", lambda e, i=i, kc=kc, sc_=sc_: e.matmul(
                            ps[6][:, kc * 32:(kc + 1) * 32], lhsT=S1.hbf[:, i, kc * 128:(kc + 1) * 128], rhs=sc_[:],
                            start=(i == 0 and kc == 0), stop=(i == 1), skip_group_check=True),
                            reads=[f"hbf1_{i}", f"selTc{i}"], writes=["ps6"])
                kb.op("act", lambda e: e.copy(out=xeTc[:], in_=ps[6][:, 0:256].rearrange("p (k c) -> p k c", c=32)),
                      reads=["ps6"], writes=["xeTc"])
                for fc in range(8):
                    for kc in range(8):
                        kb.op("pe", lambda e, fc=fc, kc=kc: e.matmul(
                            ps[7][:, fc * 64:fc * 64 + 32], lhsT=w1t[:, kc, fc * 128:(fc + 1) * 128], rhs=xeTc[:, kc, :],
                            start=(fc == 0 and kc == 0), stop=(kc == 7), skip_group_check=True),
                            reads=[w1k, "xeTc"], writes=["ps7"])
                    for kc in range(8):
                        kb.op("pe", lambda e, fc=fc, kc=kc: e.matmul(
                            ps[7][:, fc * 64 + 32:fc * 64 + 64], lhsT=w3b[0][:, kc, fc * 128:(fc + 1) * 128],
                            rhs=xeTc[:, kc, :], start=False, stop=(kc == 7), skip_group_check=True),
                            reads=["w3", "xeTc"], writes=["ps7"])
                p7v = ps[7][:, :].rearrange("p (f ab c) -> p f ab c", f=8, ab=2)
                kb.op("act", lambda e: e.activation(out=silc[:], in_=p7v[:, :, 0, :], func=AF.Silu),
                      reads=["ps7"], writes=["silc"])
                kb.op("dve", lambda e: e.tensor_tensor(out=hidTc[:], in0=silc[:], in1=p7v[:, :, 1, :], op=ALU.mult),
                      reads=["silc", "ps7"], writes=["hidTc"])
            if ex_i + 1 < NEXP:
                load_w(nwv3, w3b[0], "w3")
            if SUB < 3:
                continue
            for ct in range(CT):
                for half in range(2):
                    pb = ps[6 + half]
                    pk = f"ps{6 + half}"
                    for fc in range(8):
                        kb.op("pe", lambda e, ct=ct, half=half, fc=fc, pb=pb: e.matmul(
                            pb[0:cs, :], lhsT=hidT[:, fc, ct * 128:ct * 128 + cs],
                            rhs=w2b[0][:, fc, half * 512:(half + 1) * 512], start=(fc == 0), stop=(fc == 7)),
                            reads=["hidT", "w2"], writes=[pk])
                    ys = yest[ex_i % 2]
                    kb.op("act" if half else "dve", (lambda e, ct=ct, half=half, pb=pb, ys=ys: e.copy(
                        out=ys[0:cs, ct, half * 512:(half + 1) * 512], in_=pb[0:cs, :]))
                        if half else (lambda e, ct=ct, half=half, pb=pb, ys=ys: e.tensor_copy(
                            out=ys[0:cs, ct, half * 512:(half + 1) * 512], in_=pb[0:cs, :])),
                        reads=[pk], writes=[f"yest{ex_i % 2}"])
            if comp is not None:
                ysc = yestc[ex_i % 2]
                for half in range(2):
                    for fc in range(8):
                        kb.op("pe", lambda e, half=half, fc=fc: e.matmul(
                            ps[6][0:32, :], lhsT=hidTc[:, fc, :], rhs=w2b[0][:, fc, half * 512:(half + 1) * 512],
                            start=(fc == 0), stop=(fc == 7)), reads=["hidTc", "w2"], writes=["ps6"])
                    kb.op("act", lambda e, half=half, ysc=ysc: e.copy(out=ysc[0:32, 0, half * 512:(half + 1) * 512],
                                                                     in_=ps[6][0:32, :]),
                          reads=["ps6"], writes=["yestc"])
                kb.dma("act", S1.ye_scr[ex_i, 0:32, 0:1, :], ysc[0:32, :, :], reads=["yestc"],
                       writes=[("yescrc", ex_i)])
            if ex_i + 1 < NEXP:
                load_w(nwv2, w2b[0], "w2")
            kb.dma("sp", g["ye_scr"][ex_i, 0:cs, 0:CT, :], yest[ex_i % 2][0:cs, :, :], reads=[f"yest{ex_i % 2}"],
                   writes=[("yescr", ex_i)])

        if PH < 4:
            kb.barrier()
            return
        for S in streams[::-1]:
            T, NT, cap, CW, CT, cs, stream = S.T, S.NT, S.cap, S.CW, S.CT, S.cs, S.stream
            xin_v, xout_v, hbf, aff, slot, gate = S.xin_v, S.xout_v, S.hbf, S.aff, S.slot, S.gate
            yeall = big[:, 0:16 * CT * D].rearrange("p (e c n) -> p e c n", e=16, c=CT)
            ye_scr_S = S.ye_scr
            kb.barrier()
            make_bc(kb, nc, c, lambda kc: mod[:, 5 * 8 + kc, stream:stream + 1], gate2, ps[0], "gate2", [f"mod{layer}"])
            if final_norm:
                make_bc(kb, nc, c, lambda kc: c.fnT[:, kc:kc + 1], Gbc, ps[0], "Gbc", ["c_fnT"])
            for ex_i in range(16):
                kb.dma(["sp", "act"][ex_i % 2], yeall[0:cs, ex_i, :, :], ye_scr_S[ex_i, 0:cs, 0:CT, :],
                       reads=[("yescr", ex_i), ("yescrc", ex_i)], writes=[f"ye{ex_i}"])
            nsg = 0
            EG = 4
            for i in range(NT):
                b = i % 2
                kb.dma("sp", xb[b][:], xin_v[i], writes=[f"xb{b}"])
                for g0 in range(0, 16, EG):
                    sg = nsg % 2
                    nsg += 1
                    sga = selGa[sg][:, :, 0:CW]
                    kb.op("dve", lambda e, i=i, g0=g0, sga=sga: e.tensor_tensor(
                        out=sga[:, :, :], in0=c.iota[:, 0:CW].unsqueeze(1).broadcast_to([128, EG, CW]),
                        in1=slot[:, i, g0:g0 + EG].unsqueeze(2).broadcast_to([128, EG, CW]), op=ALU.is_equal),
                        reads=["c_iota", "slot"], writes=[f"selGa{sg}"])
                    kb.op("pool", lambda e, i=i, g0=g0, sga=sga: e.tensor_tensor(
                        out=sga[:, :, :], in0=sga[:, :, :],
                        in1=gate[:, i, g0:g0 + EG].unsqueeze(2).broadcast_to([128, EG, CW]), op=ALU.mult),
                        reads=[f"selGa{sg}", "gate"], writes=[f"selGa{sg}"])
                    pst = ps[2 + sg][:].bitcast(BF16)
                    for ee in range(EG):
                        for ct in range(CT):
                            kb.op("pe", lambda e, ct=ct, ee=ee, sga=sga, pst=pst: e.transpose(
                                out=pst[0:cs, (ee * CT + ct) * 128:(ee * CT + ct + 1) * 128],
                                in_=sga[:, ee, ct * 128:ct * 128 + cs], identity=c.identb[:]),
                                reads=[f"selGa{sg}", "c_identb"], writes=[f"ps{2 + sg}"])
                    kb.op("act", lambda e, sg=sg, pst=pst: e.copy(
                        out=selGca[sg][0:cs, 0:EG * CT, :], in_=pst[0:cs, 0:EG * CT * 128].rearrange("p (c t) -> p c t", t=128)),
                        reads=[f"ps{2 + sg}"], writes=[f"selGca{sg}"])
                    for ee in range(EG):
                        ex_i = g0 + ee
                        for half in range(2):
                            for ct in range(CT):
                                kb.op("pe", lambda e, half=half, ct=ct, sg=sg, ex_i=ex_i, ee=ee: e.matmul(
                                    ps[half][:, :], lhsT=selGca[sg][0:cs, ee * CT + ct, :],
                                    rhs=yeall[0:cs, ex_i, ct, half * 512:(half + 1) * 512],
                                    start=(ex_i == 0 and ct == 0), stop=(ex_i == 15 and ct == CT - 1)),
                                    reads=[f"selGca{sg}", f"ye{ex_i}"], writes=[f"ps{half}"])
                for half in range(2):
                    sl = slice(half * 512, (half + 1) * 512)
                    kb.op("dve", lambda e, half=half, sl=sl: e.tensor_tensor(
                        out=tmpo[half][:], in0=ps[half][:], in1=gate2[:, sl], op=ALU.mult),
                        reads=[f"ps{half}", "gate2"], writes=[f"tmpo{half}"])
                    kb.op("pool", lambda e, half=half, sl=sl, b=b: e.tensor_tensor(
                        out=xb[b][:, sl], in0=xb[b][:, sl], in1=tmpo[half][:], op=ALU.add),
                        reads=[f"tmpo{half}", f"xb{b}"], writes=[f"xb{b}"])
                if final_norm:
                    norm_tile(kb, nc, xb[b][:], f"xb{b}", stt[b], Gbc, None, h32[b][:], f"h32{b}")
                    kb.dma("sp", xout_v[i], h32[b][:], reads=[f"h32{b}"], writes=[("dram", x_out.tensor.name, i)])
                else:
                    kb.dma("sp", xout_v[i], xb[b][:], reads=[f"xb{b}"], writes=[("dram", x_out.tensor.name, i)])
            kb.barrier()


def host_consts():
    k = {}
    k["k_ident"] = np.eye(128, dtype=np.float32)
    k["k_iota"] = np.tile(np.arange(256, dtype=np.float32)[None, :], (128, 1))
    C, S, perm = rope_tables()
    k["k_ropeC"], k["k_ropeS"], k["k_perm"] = C, S, perm
    kk = np.arange(128)[:, None]
    qq = np.arange(128)[None, :]
    k["k_mL"] = np.tile((kk >= qq).astype(np.float32), (1, 4))
    k["k_mU"] = np.tile((kk <= qq).astype(np.float32), (1, 4))
    ss = np.arange(CHUNK)[:, None]
    tt = np.arange(CHUNK)[None, :]
    mus = (ss < tt).astype(np.float32)
    mui = (ss <= tt).astype(np.float32)
    k["k_maskA"] = np.ascontiguousarray(np.concatenate([-mus, mus, mui, mui], axis=1))
    k["k_maskT"] = np.ascontiguousarray(-(tt < ss).astype(np.float32))
    rm = np.ones((128, TP), np.float32)
    rm[:, CHUNK_COLS] = 0.0
    k["k_rmask"] = rm
    k["k_J"] = np.ascontiguousarray(np.eye(128, dtype=np.float32)[::-1])
    sel = np.zeros((16, 16, 128), np.float32)
    for r in range(16):
        sel[r, r, :] = 1.0
    k["k_sel"] = sel
    bdm = np.zeros((128, 128), np.float32)
    bdm[0:64, 0:64] = 1.0
    bdm[64:128, 64:128] = 1.0
    k["k_bd"] = bdm
    return k


def fm(v):
    v = np.asarray(v, np.float32)
    return np.ascontiguousarray(v.reshape(-1, 128).T)


def host_inputs(inp, b):
    m = dict(host_consts())
    m["x"] = np.ascontiguousarray(inp["x"][b])
    m["ctx"] = np.ascontiguousarray(inp["ctx"][b])
    m["cT"] = fm(inp["c"][b])
    m["ccT"] = fm(inp["c_ctx"])
    m["ada_w"] = inp["ada_w"]
    m["ada_bT"] = np.ascontiguousarray(np.stack([fm(inp["ada_b"][l]) for l in range(2)], axis=1))
    m["normT"] = np.ascontiguousarray(np.stack(
        [np.stack([fm(inp["norm_mix"][l]), fm(inp["norm_ffn"][l])], axis=1) for l in range(2)], axis=1))
    m["fnT"] = fm(inp["final_norm"])
    for k in ("moe_router", "moe_w1", "moe_w3", "moe_w2", "o_w_out"):
        m[k] = inp[k]
    w = inp["o_w_in"][0]
    kd = w[:, 1024:1280].reshape(1024, 4, 1, 64)
    kd = np.concatenate([kd, kd], axis=2).reshape(1024, 512)
    m["o_w_in2"] = np.ascontiguousarray(np.concatenate([w[:, 0:1024], kd, w[:, 1280:1536]], axis=1))
    m["o_sink"] = np.ascontiguousarray(inp["o_sink"].reshape(1, 16))
    idx = []
    for p in range(4):
        for off in (0, 512, 1024):
            idx += list(range(off + p * 128, off + p * 128 + 128))
    for d in range(2):
        idx += list(range(1536 + d * 64, 1536 + d * 64 + 64)) + list(range(1664 + d * 64, 1664 + d * 64 + 64))
    idx += list(range(1792, 1920))
    idxa = list(idx)
    for h in range(4):
        for part in range(3):
            idx += list(range(1920 + part * 512 + h * 128, 1920 + part * 512 + h * 128 + 128))
        idx += list(range(3472 + h * 128, 3472 + h * 128 + 128))
    idx += list(range(3456, 3472))
    m["e_w_in2"] = np.ascontiguousarray(inp["e_w_in"][0][:, idx])
    mu2 = inp["a_mu"][0][idxa]
    p64 = np.zeros((64, 64), np.float32)
    p64[:, 0:30] = mu2.reshape(30, 64).T
    for d in range(2):
        for h in range(8):
            p64[:, 30 + d * 8 + h] = inp["a_w0"][0, d, h * 64:(h + 1) * 64]
            p64[:, 46 + d * 8 + h] = inp["a_a0"][0, d, h * 64:(h + 1) * 64]
    m["mx_par64"] = p64
    p128 = np.zeros((128, 112), np.float32)
    p128[:, 0] = mu2[1536:1664]; p128[:, 1] = mu2[1664:1792]; p128[:, 2] = mu2[1792:1920]
    for h in range(8):
        p128[0:64, 8 + h] = inp["a_k_k"][0, h * 64:(h + 1) * 64]
        p128[0:64, 16 + h] = inp["a_k_a"][0, h * 64:(h + 1) * 64]
        p128[0:64, 32 + h] = inp["a_r_k"][0, h]
    p128[0:8, 40] = inp["b_dt_bias"][0].reshape(8)
    p128[0:8, 41] = inp["b_a_log"][0].reshape(8)
    for h in range(4):
        for part in range(3):
            for j in range(5):
                p128[:, 48 + (h * 3 + part) * 5 + j] = inp["b_conv"][0, j, part * 512 + h * 128:part * 512 + (h + 1) * 128]
    m["mx_par128"] = p128
    pP = np.zeros((128, 64), np.float32)
    pP[:, 0:12] = mu2[0:1536].reshape(12, 128).T
    for p in range(4):
        sl = slice(p * 128, (p + 1) * 128)
        for d in range(2):
            pP[:, 12 + d * 4 + p] = inp["a_w0"][0, d, sl]
            pP[:, 20 + d * 4 + p] = inp["a_a0"][0, d, sl]
        pP[:, 28 + p] = inp["a_k_k"][0, sl]
        pP[:, 32 + p] = inp["a_k_a"][0, sl]
        pP[:, 40 + p] = inp["a_r_k"][0].reshape(512)[sl]
    m["mx_parP"] = pP
    m["mx_w2"] = np.ascontiguousarray(np.concatenate([inp["a_w2"][0].transpose(1, 0, 2), inp["a_a2"][0].transpose(1, 0, 2)], axis=0))
    m["mx_bc"] = np.ascontiguousarray(np.concatenate([inp["a_ln_w"][0], inp["a_ln_b"][0], np.tile(inp["b_norm"][0], 4)])[None, :])
    m["a_g2"] = inp["a_g2"]
    m["e_w_out"] = inp["e_w_out"]
    return m


IN_SHAPES = {
    "k_ident": [128, 128], "k_iota": [128, 256],
    "x": [2048, D], "ctx": [256, D], "cT": [128, 8], "ccT": [128, 8],
    "ada_w": [2, D, 6 * D], "ada_bT": [128, 2, 48], "normT": [128, 2, 2, 8], "fnT": [128, 8],
    "k_ropeC": [128, 2048], "k_ropeS": [128, 2048], "k_perm": [128, 128], "k_mL": [128, 512], "k_mU": [128, 512],
    "o_w_in2": [D, 1792], "o_w_out": [1, D, D], "o_sink": [1, 16],
    "k_maskA": [CHUNK, 4 * CHUNK], "k_maskT": [CHUNK, CHUNK], "k_rmask": [128, TP], "k_J": [128, 128], "k_sel": [16, 16, 128],
    "e_w_in2": [D, 3984], "mx_par64": [64, 64], "mx_parP": [128, 64], "k_bd": [128, 128], "mx_par128": [128, 112], "mx_w2": [128, 2, 512], "mx_bc": [1, 1536],
    "a_g2": [1, 128, 512], "e_w_out": [1, D, D],
    "moe_router": [2, D, 16], "moe_w1": [2, 16, D, D], "moe_w3": [2, 16, D, D], "moe_w2": [2, 16, D, D],
}


def build(stages=("all",), extra_in=(), outs=(("out", [2048, D]),)):
    from contextlib import ExitStack
    nc = bass.Bass("TRN2", target_bir_lowering=False)
    g = {}
    for name, shape in IN_SHAPES.items():
        g[name] = nc.dram_tensor(name, shape, F32, kind="ExternalInput").ap()
    for name, shape in extra_in:
        g[name] = nc.dram_tensor(name, shape, F32, kind="ExternalInput").ap()
    for name, shape in outs:
        g[name] = nc.dram_tensor(name, shape, F32, kind="ExternalOutput").ap()
    g["ye_scr"] = nc.dram_tensor("ye_scr", [16, 128, 2, D], BF16).ap()
    g["ye_scr_c"] = nc.dram_tensor("ye_scr_c", [16, 32, 1, D], BF16).ap()
    g["ys_scr"] = nc.dram_tensor("ys_scr", [2, 2304, 1536], F32).ap()
    g["z_scr"] = nc.dram_tensor("z_scr", [2304, 512], F32).ap()
    for nm, rows in (("xm_lat", 2048), ("xm_ctx", 256), ("x1_lat", 2048), ("x1_ctx", 256), ("x2_lat", 2048)):
        g[nm] = nc.dram_tensor(nm, [rows, D], F32).ap()
    kb = KB(nc)
    with ExitStack() as es:
        c = load_consts(kb, nc, es, g)
        c.fnT = es.enter_context(nc.sbuf_tensor("c_fnT", [128, 8], F32))
        kb.dma("sp", c.fnT[:], g["fnT"][:, :], writes=["c_fnT"])
        mods = prologue(kb, nc, es, g, c)
        for st in stages:
            if st == "moe0l_test":
                moe_stage(kb, nc, g, c, mods, 0, 0, g["t_in"], g["out"], 2048)
            elif st == "moe0f_test":
                moe_stage(kb, nc, g, c, mods, 0, 0, g["t_in"], g["out"], 2048, comp=(g["t_in2"], g["out2"]))
            elif st == "moe0c_test":
                moe_stage(kb, nc, g, c, mods, 0, 1, g["t_in"], g["out"], 256)
            elif st == "moe1l_test":
                moe_stage(kb, nc, g, c, mods, 1, 0, g["t_in"], g["out"], 2048, final_norm=True)
            elif st == "all":
                mixer_stage(kb, nc, g, c, mods, g["x"], g["ctx"], g["xm_lat"], g["xm_ctx"])
                moe_stage(kb, nc, g, c, mods, 0, 0, g["xm_lat"], g["x1_lat"], 2048, comp=(g["xm_ctx"], g["x1_ctx"]))
                attn_stage(kb, nc, g, c, mods, g["x1_lat"], g["x1_ctx"], g["x2_lat"])
                moe_stage(kb, nc, g, c, mods, 1, 0, g["x2_lat"], g["out"], 2048, final_norm=True)
            elif st == "mixer_test":
                mixer_stage(kb, nc, g, c, mods, g["x"], g["ctx"], g["out"], g["out2"])
            elif st == "attn_test":
                attn_stage(kb, nc, g, c, mods, g["t_in"], g["t_in2"], g["out"])
            elif st == "mods_test":
                for l in range(2):
                    kb.dma("sp", g["out"][l * 128:(l + 1) * 128, 0:96], mods[l][:].rearrange("p a b -> p (a b)"),
                           reads=[f"mod{l}"], writes=[("o", l)])
        kb.finish([])
    return nc, kb


def rope_tables():
    quarter = 16
    inv = (10000.0 ** (-np.arange(quarter, dtype=np.float32) / quarter)).astype(np.float32)
    t = np.arange(2048)
    row = (t // 64).astype(np.float32)
    col = (t % 64).astype(np.float32)
    C = np.zeros((128, 2048), np.float32)
    S = np.zeros((128, 2048), np.float32)
    perm = np.zeros((128, 128), np.float32)
    for p in range(128):
        d = p % 64
        pos = row if d < 32 else col
        i = d % 16
        ang = (pos * inv[i]).astype(np.float32)
        C[p] = np.cos(ang)
        second = (d % 32) >= 16
        S[p] = np.sin(ang) if second else -np.sin(ang)
        partner = p - 16 if second else p + 16
        perm[partner, p] = 1.0
    return C, S, perm


def attn_stage(kb, nc, g, c, mods, x_lat_in, x_ctx_in, x_out):
    layer = 1
    mod = mods[layer]
    NT = 18
    from contextlib import ExitStack
    with ExitStack() as es:
        def sb(name, shape, dt):
            return es.enter_context(nc.sbuf_tensor(f"at_{name}", shape, dt))
        Gbc = sb("G", [128, D], F32)
        Sbc = sb("S", [128, D], F32)
        xb = [sb("x0", [128, D], F32), sb("x1", [128, D], F32)]
        hb = [sb("h0", [128, D], BF16), sb("h1", [128, D], BF16)]
        stt = [sb("st0", [128, 4], F32), sb("st1", [128, 4], F32)]
        hT = sb("hT", [128, 8, 2304], BF16)
        win = sb("win", [128, 8, 1792], BF16)
        wo = sb("wo", [128, 8, D], BF16)
        stg = [sb("stg0", [128, D], F32), sb("stg1", [128, D], F32)]
        Ct = sb("Ct", [128, 2048], F32)
        St = sb("St", [128, 2048], F32)
        perm = sb("perm", [128, 128], F32)
        qraw = [sb("qraw0", [128, 512], F32), sb("qraw1", [128, 512], F32)]
        rt1 = [sb("rt10", [128, 512], F32), sb("rt11", [128, 512], F32)]
        qT = sb("qT", [128, 8, 2048], BF16)
        kT = sb("kT", [128, 4, 2304], BF16)
        V = sb("V", [128, NT, 4, 65], BF16)
        mL = sb("mL", [128, 512], BF16)
        mU = sb("mU", [128, 512], BF16)
        esink = sb("esink", [128, 16], F32)
        PT = [sb(f"PT{i}", [128, 512], BF16) for i in range(2)]
        osb = stg[0]
        den = sb("den", [128, 4], F32)
        oT = sb("oT", [128, 8, 128], BF16)
        tmpo = qraw
        ps = [es.enter_context(nc.psum_tensor(f"at_ps{i}", [128, 512], F32)) for i in range(8)]

        kb.dma("sp", Ct[:], g["k_ropeC"][:, :], writes=["Ct"])
        kb.dma("sp", St[:], g["k_ropeS"][:, :], writes=["St"])
        kb.dma("sp", perm[:], g["k_perm"][:, :], writes=["perm"])
        kb.dma("sp", esink[:], g["o_sink"].partition_broadcast(128), writes=["esink"])
        kb.op("act", lambda e: e.activation(out=esink[:], in_=esink[:], func=AF.Exp), reads=["esink"], writes=["esink"])
        kb.dma("sp", stg[0][:, 0:512], g["k_mL"][:, :], writes=["stg0"])
        kb.op("dve", lambda e: e.tensor_copy(out=mL[:], in_=stg[0][:, 0:512]), reads=["stg0"], writes=["mL"])
        kb.dma("sp", stg[1][:, 0:512], g["k_mU"][:, :], writes=["stg1"])
        kb.op("dve", lambda e: e.tensor_copy(out=mU[:], in_=stg[1][:, 0:512]), reads=["stg1"], writes=["mU"])
        make_bc(kb, nc, c, lambda kc: mod[:, 1 * 8 + kc, 1:2], Gbc, ps[0], "Gbc", [f"mod{layer}"])
        make_bc(kb, nc, c, lambda kc: mod[:, 0 * 8 + kc, 1:2], Sbc, ps[0], "Sbc", [f"mod{layer}"])

        wcnt = [0]

        wv = g["o_w_in2"].rearrange("(kc p) n -> p kc n", p=128)
        for kc in range(0, 8, 2):
            kb.dma("pool", win[:, kc:kc + 2, :], wv[:, kc:kc + 2, :], writes=["win"])
        wov = g["o_w_out"][0].rearrange("(kc p) n -> p kc n", p=128)
        for kc in range(0, 8, 4):
            kb.dma("pool", wo[:, kc:kc + 4, :], wov[:, kc:kc + 4, :], writes=["wo"])

        for i in range(NT):
            b = i % 2
            if i == 2:
                kb.barrier()
                make_bc(kb, nc, c, lambda kc: mod[:, 1 * 8 + kc, 0:1], Gbc, ps[0], "Gbc", [f"mod{layer}"])
                make_bc(kb, nc, c, lambda kc: mod[:, 0 * 8 + kc, 0:1], Sbc, ps[0], "Sbc", [f"mod{layer}"])
            src = x_ctx_in[i * 128:(i + 1) * 128, :] if i < 2 else x_lat_in[(i - 2) * 128:(i - 1) * 128, :]
            kb.dma("sp", xb[b][:], src, writes=[f"xb{b}"])
            norm_tile(kb, nc, xb[b][:], f"xb{b}", stt[b], Gbc, Sbc, stg[b][:], f"stg{b}")
            kb.op("act", lambda e, b=b: e.copy(out=hb[b][:], in_=stg[b][:]), reads=[f"stg{b}"], writes=[f"hb{b}"])
            for half in range(2):
                pst = ps[half][:].bitcast(BF16)
                for q in range(4):
                    kc = half * 4 + q
                    kb.op("pe", lambda e, kc=kc, q=q, b=b, pst=pst: e.transpose(
                        out=pst[:, q * 128:(q + 1) * 128], in_=hb[b][:, kc * 128:(kc + 1) * 128],
                        identity=c.identb[:]), reads=[f"hb{b}", "c_identb"], writes=[f"ps{half}"])
                kb.op("dve" if half == 0 else "pool" if False else "act",
                      (lambda e, half=half, pst=pst, i=i: e.tensor_copy(
                          out=hT[:, half * 4:(half + 1) * 4, i * 128:(i + 1) * 128],
                          in_=pst[:, 0:512].rearrange("p (q t) -> p q t", q=4))) if half == 0 else
                      (lambda e, half=half, pst=pst, i=i: e.copy(
                          out=hT[:, half * 4:(half + 1) * 4, i * 128:(i + 1) * 128],
                          in_=pst[:, 0:512].rearrange("p (q t) -> p q t", q=4))),
                      reads=[f"ps{half}"], writes=["hT"])

        import os
        APH = int(os.environ.get("ATT_PH", "9"))
        if APH < 1:
            kb.barrier(); return
        nb = 0
        for nq in range(8):
            for tb in range(4):
                b = nb % 2
                nb += 1
                pq = ps[2 + b]
                t0 = 256 + tb * 512
                for kc in range(8):
                    kb.op("pe", lambda e, kc=kc, nq=nq, t0=t0, pq=pq: e.matmul(
                        pq[:], lhsT=win[:, kc, nq * 128:(nq + 1) * 128], rhs=hT[:, kc, t0:t0 + 512],
                        start=(kc == 0), stop=(kc == 7)), reads=["win", "hT"], writes=[f"ps{2 + b}"])
                kb.op("act", lambda e, b=b, pq=pq: e.copy(out=qraw[b][:], in_=pq[:]), reads=[f"ps{2 + b}"],
                      writes=[f"qraw{b}"])
                pw = ps[4 + b]
                kb.op("pe", lambda e, b=b, pw=pw: e.matmul(pw[:], lhsT=perm[:], rhs=qraw[b][:], start=True, stop=True),
                      reads=["perm", f"qraw{b}"], writes=[f"ps{4 + b}"])
                cs_ = slice(tb * 512, (tb + 1) * 512)
                kb.op("dve", lambda e, b=b, pw=pw, cs_=cs_: e.scalar_tensor_tensor(
                    out=rt1[b][:], in0=pw[:], scalar=0.125, in1=St[:, cs_], op0=ALU.mult, op1=ALU.mult),
                      reads=[f"ps{4 + b}", "St"], writes=[f"rt1{b}"])
                kb.op("pool", lambda e, b=b, cs_=cs_: e.tensor_tensor(out=qraw[b][:], in0=qraw[b][:], in1=Ct[:, cs_],
                                                                    op=ALU.mult),
                      reads=[f"qraw{b}", "Ct"], writes=[f"qraw{b}"])
                kb.op("dve", lambda e, b=b, nq=nq, cs_=cs_: e.scalar_tensor_tensor(
                    out=qT[:, nq, cs_], in0=qraw[b][:], scalar=0.125, in1=rt1[b][:], op0=ALU.mult, op1=ALU.add),
                    reads=[f"qraw{b}", f"rt1{b}"], writes=["qT"])
        if APH < 2:
            kb.barrier(); return
        for hk in range(4):
            for tb in range(5):
                b = nb % 2
                nb += 1
                pq = ps[2 + b]
                t0 = 0 if tb == 0 else 256 + (tb - 1) * 512
                tw = 256 if tb == 0 else 512
                for kc in range(8):
                    kb.op("pe", lambda e, kc=kc, hk=hk, t0=t0, tw=tw, pq=pq: e.matmul(
                        pq[:, 0:tw], lhsT=win[:, kc, 1024 + hk * 128:1024 + (hk + 1) * 128],
                        rhs=hT[:, kc, t0:t0 + tw], start=(kc == 0), stop=(kc == 7)),
                        reads=["win", "hT"], writes=[f"ps{2 + b}"])
                if tb == 0:
                    kb.op("act", lambda e, hk=hk, pq=pq: e.copy(out=kT[:, hk, 0:256], in_=pq[:, 0:256]),
                          reads=[f"ps{2 + b}"], writes=["kT"])
                    continue
                kb.op("act", lambda e, b=b, pq=pq: e.copy(out=qraw[b][:], in_=pq[:]), reads=[f"ps{2 + b}"],
                      writes=[f"qraw{b}"])
                pw = ps[4 + b]
                kb.op("pe", lambda e, b=b, pw=pw: e.matmul(pw[:], lhsT=perm[:], rhs=qraw[b][:], start=True, stop=True),
                      reads=["perm", f"qraw{b}"], writes=[f"ps{4 + b}"])
                cs_ = slice((tb - 1) * 512, tb * 512)
                kb.op("dve", lambda e, b=b, pw=pw, cs_=cs_: e.tensor_tensor(out=rt1[b][:], in0=pw[:], in1=St[:, cs_],
                                                                         op=ALU.mult),
                      reads=[f"ps{4 + b}", "St"], writes=[f"rt1{b}"])
                kb.op("pool", lambda e, b=b, cs_=cs_: e.tensor_tensor(out=qraw[b][:], in0=qraw[b][:], in1=Ct[:, cs_],
                                                                    op=ALU.mult),
                      reads=[f"qraw{b}", "Ct"], writes=[f"qraw{b}"])
                kb.op("dve", lambda e, b=b, hk=hk, t0=t0: e.tensor_tensor(
                    out=kT[:, hk, t0:t0 + 512], in0=qraw[b][:], in1=rt1[b][:], op=ALU.add),
                    reads=[f"qraw{b}", f"rt1{b}"], writes=["kT"])
        if APH < 3:
            kb.barrier(); return
        kb.op("pool", lambda e: e.memset(V[:], 1.0), writes=["V"])
        for i in range(NT):
            b = nb % 2
            nb += 1
            pq = ps[2 + b]
            for kc in range(8):
                kb.op("pe", lambda e, kc=kc, i=i, pq=pq: e.matmul(
                    pq[:, 0:256], lhsT=hT[:, kc, i * 128:(i + 1) * 128], rhs=win[:, kc, 1536:1792],
                    start=(kc == 0), stop=(kc == 7)), reads=["win", "hT"], writes=[f"ps{2 + b}"])
            kb.op("act", lambda e, i=i, pq=pq: e.copy(out=V[:, i, :, 0:64],
                                                     in_=pq[:, 0:256].rearrange("p (h d) -> p h d", h=4)),
                  reads=[f"ps{2 + b}"], writes=["V"])

        if APH < 4:
            kb.barrier(); return
        make_bc(kb, nc, c, lambda kc: mod[:, 2 * 8 + kc, 0:1], Gbc, ps[0], "Gbc", [f"mod{layer}"])
        nsb = 0
        for n in range(16):
            kb.dma("sp", xb[n % 2][:], x_lat_in[n * 128:(n + 1) * 128, :], writes=[f"xb{n % 2}"])
            for hk in range(4):
                tiles = []
                if n > 0:
                    tiles.append((2 + n - 1, mL, "mL"))
                tiles.append((2 + n, None, None))
                if n < 15:
                    tiles.append((2 + n + 1, mU, "mU"))
                tiles.append((0, None, None))
                tiles.append((1, None, None))
                po = ps[6 + (n * 4 + hk) % 2]
                pok = f"ps{6 + (n * 4 + hk) % 2}"
                for ti, (kt, msk, mk) in enumerate(tiles):
                    sbk = nsb % 2
                    nsb += 1
                    pSa, pSb = ps[1 + 2 * sbk], ps[2 + 2 * sbk]
                    ka, kbk = f"ps{1 + 2 * sbk}", f"ps{2 + 2 * sbk}"
                    for gq in range(4):
                        hq = hk * 4 + gq
                        bp = (hq % 2) * 64
                        pS = pSa if bp == 0 else pSb
                        kb.op("pe", lambda e, gq=gq, hq=hq, bp=bp, kt=kt, pS=pS, hk=hk, n=n: e.matmul(
                            pS[:, (gq // 2) * 128:(gq // 2 + 1) * 128], lhsT=kT[bp:bp + 64, hk, kt * 128:(kt + 1) * 128],
                            rhs=qT[bp:bp + 64, hq // 2, n * 128:(n + 1) * 128], start=True, stop=True),
                            reads=["kT", "qT"], writes=[ka if bp == 0 else kbk])
                    ptv = PT[sbk][:].rearrange("p (a b q) -> p a b q", a=2, b=2)
                    kb.op("act", lambda e, pSa=pSa, ptv=ptv: e.activation(
                        out=ptv[:, :, 0, :], in_=pSa[:, 0:256].rearrange("p (a q) -> p a q", a=2), func=AF.Exp),
                        reads=[ka], writes=[f"PT{sbk}"])
                    kb.op("act", lambda e, pSb=pSb, ptv=ptv: e.activation(
                        out=ptv[:, :, 1, :], in_=pSb[:, 0:256].rearrange("p (a q) -> p a q", a=2), func=AF.Exp),
                        reads=[kbk], writes=[f"PT{sbk}"])
                    ADBG = int(os.environ.get("ATT_DBG", "9"))
                    if ADBG < 2:
                        continue
                    if msk is not None:
                        kb.op("dve", lambda e, sbk=sbk, msk=msk: e.tensor_tensor(out=PT[sbk][:], in0=PT[sbk][:],
                                                                               in1=msk[:], op=ALU.mult),
                              reads=[f"PT{sbk}", mk], writes=[f"PT{sbk}"])
                    for gq in range(4):
                        kb.op("pe", lambda e, gq=gq, sbk=sbk, kt=kt, hk=hk, po=po, ti=ti: e.matmul(
                            po[:, gq * 65:(gq + 1) * 65], lhsT=PT[sbk][:, gq * 128:(gq + 1) * 128],
                            rhs=V[:, kt, hk, :], start=(ti == 0 and gq == 0), stop=(ti == len(tiles) - 1),
                            skip_group_check=True), reads=[f"PT{sbk}", "V"], writes=[pok])
                if ADBG < 3:
                    continue
                pov = po[:, 0:260].rearrange("p (g d) -> p g d", g=4)
                kb.op("dve", lambda e, pov=pov, hk=hk: e.tensor_tensor(
                    out=den[:], in0=pov[:, :, 64], in1=esink[:, hk * 4:(hk + 1) * 4], op=ALU.add),
                    reads=[pok, "esink"], writes=["den"])
                kb.op("dve", lambda e: e.reciprocal(out=den[:], in_=den[:]), reads=["den"], writes=["den"])
                kb.op("dve", lambda e, pov=pov, hk=hk: e.tensor_tensor(
                    out=osb[:, hk * 256:(hk + 1) * 256].rearrange("p (g d) -> p g d", g=4), in0=pov[:, :, 0:64],
                    in1=den[:].unsqueeze(2).broadcast_to([128, 4, 64]), op=ALU.mult),
                    reads=[pok, "den"], writes=["stg0"])
            if ADBG < 4:
                continue
            kb.op("act", lambda e: e.copy(out=hb[0][:], in_=osb[:]), reads=["stg0"], writes=["hb0"])
            pst = ps[0][:].bitcast(BF16)
            for kc in range(8):
                kb.op("pe", lambda e, kc=kc, pst=pst: e.transpose(
                    out=pst[:, kc * 128:(kc + 1) * 128], in_=hb[0][:, kc * 128:(kc + 1) * 128], identity=c.identb[:]),
                    reads=["hb0", "c_identb"], writes=["ps0"])
            kb.op("act", lambda e, pst=pst: e.copy(out=oT[:], in_=pst[:].rearrange("p (k t) -> p k t", k=8)),
                  reads=["ps0"], writes=["oT"])
            for half in range(2):
                pp = ps[5] if half == 0 else ps[0]
                for kc in range(8):
                    kb.op("pe", lambda e, kc=kc, half=half, pp=pp: e.matmul(
                        pp[:], lhsT=oT[:, kc, :], rhs=wo[:, kc, half * 512:(half + 1) * 512],
                        start=(kc == 0), stop=(kc == 7)), reads=["oT", "wo"], writes=["ps5" if half == 0 else "ps0"])
                sl = slice(half * 512, (half + 1) * 512)
                kb.op("dve", lambda e, half=half, pp=pp, sl=sl: e.tensor_tensor(
                    out=tmpo[half][:], in0=pp[:], in1=Gbc[:, sl], op=ALU.mult),
                    reads=["ps5" if half == 0 else "ps0", "Gbc"], writes=[f"qraw{half}"])
                kb.op("pool", lambda e, half=half, sl=sl, n=n: e.tensor_tensor(
                    out=xb[n % 2][:, sl], in0=xb[n % 2][:, sl], in1=tmpo[half][:], op=ALU.add),
                    reads=[f"qraw{half}", f"xb{n % 2}"], writes=[f"xb{n % 2}"])
            kb.dma("sp", x_out[n * 128:(n + 1) * 128, :], xb[n % 2][:], reads=[f"xb{n % 2}"],
                   writes=[("dram", x_out.tensor.name, n)])
        kb.barrier()


def dplr_scan(kb, nc, c, T, dk, rT, kkT, kT, bT, vT, Pinc, prodT, store_cb, hk_, Gb=None, rA=None, kkA=None, bp=0, rC=None, kkC=None):
    ps = T["ps"]
    rA = rT if rA is None else rA
    kkA = kkT if kkA is None else kkA
    rC = rT if rC is None else rC
    kkC = kkT if kkC is None else kkC
    tb = vT.dtype == BF16
    ident, maskA, maskT, ident64 = c.ident, T["maskA"], T["maskT"], c.ident
    ST = T["ST"]
    kb.op("dve", lambda e: e.memset(ST[bp:bp + dk, 0:dk], 0.0), writes=["ST"])
    kb.op("dve", lambda e: e.memset(T["STb"][bp:bp + dk, 0:dk], 0.0), writes=["STb"])
    CH = CHUNK
    NLV = {64: 5, 128: 6}[CH]
    NCH = len(CHUNK_COLS)
    GB = 2

    def inv_gen(g0):
        grp = list(range(g0, min(NCH, g0 + GB)))
        par = (g0 // GB) % 2
        for ci in grp:
            s = ci % GB + GB * par
            cs = slice(CHUNK_COLS[ci], CHUNK_COLS[ci] + CH)
            pa = ps[s % 2]
            pk = f"ps{s % 2}"
            pn = ps[2 + s % 2]
            pnk = f"ps{2 + s % 2}"
            for j, (l, r) in enumerate(((bT, kkA), (kT, kkA), (bT, rA), (kT, rA), (kkA, bT))):
                if j < 4:
                    kb.op("pe", lambda e, j=j, l=l, r=r, cs=cs, pa=pa: e.matmul(
                        pa[0:CH, j * CH:(j + 1) * CH], lhsT=l[bp:bp + dk, cs], rhs=r[bp:bp + dk, cs], start=True, stop=True),
                        reads=[hk_], writes=[pk])
                else:
                    kb.op("pe", lambda e, l=l, r=r, cs=cs, pn=pn: e.matmul(
                        pn[0:CH, 256:256 + CH], lhsT=l[bp:bp + dk, cs], rhs=r[bp:bp + dk, cs], start=True, stop=True),
                        reads=[hk_], writes=[pnk])
            mA, mT_, mAk, mTk = maskA[0:CH, :], maskT[0:CH, :], "maskA", "maskT"
            if Gb is not None:
                c0_ = CHUNK_COLS[ci]
                kb.op("pe", lambda e, cs=cs, pn=pn: e.transpose(out=pn[0:CH, 384:385], in_=Gb[0:1, cs],
                                                              identity=ident[0:1, 0:1]), reads=[hk_, "c_ident"], writes=[pnk])
                kb.op("act", lambda e, s=s, pn=pn: e.copy(out=T["Gc"][0:CH, s:s + 1], in_=pn[0:CH, 384:385]),
                      reads=[pnk], writes=[f"Gc{s}"])
                kb.op("dve", lambda e, s=s, cs=cs: e.tensor_scalar(out=T["Dt"][0:CH, s % GB, :], in0=Gb[0:CH, cs],
                                                                 scalar1=T["Gc"][0:CH, s:s + 1], scalar2=0.0,
                                                                 op0=ALU.subtract, op1=ALU.min),
                      reads=[hk_, f"Gc{s}"], writes=[f"Dt{s % GB}"])
                kb.op("act", lambda e, s=s: e.activation(out=T["Dt"][0:CH, s % GB, :], in_=T["Dt"][0:CH, s % GB, :], func=AF.Exp),
                      reads=[f"Dt{s % GB}"], writes=[f"Dt{s % GB}"])
                kb.op("dve", lambda e, s=s, cs=cs: e.tensor_scalar(out=T["Dts"][0:CH, s % GB, :], in0=Gb[0:CH, cs],
                                                                 scalar1=T["Gc"][0:CH, s:s + 1], scalar2=0.0,
                                                                 op0=ALU.subtract, op1=ALU.max),
                      reads=[hk_, f"Gc{s}"], writes=[f"Dts{s % GB}"])
                kb.op("act", lambda e, s=s: e.activation(out=T["Dts"][0:CH, s % GB, :], in_=T["Dts"][0:CH, s % GB, :], func=AF.Exp,
                                                         scale=-1.0), reads=[f"Dts{s % GB}"], writes=[f"Dts{s % GB}"])
                kb.op("dve", lambda e, s=s: e.tensor_tensor(
                    out=T["mD"][0:CH, s % GB, :].rearrange("p (a t) -> p a t", a=4),
                    in0=maskA[0:CH, :].rearrange("p (a t) -> p a t", a=4),
                    in1=T["Dt"][0:CH, s % GB, :].unsqueeze(1).broadcast_to([CH, 4, CH]), op=ALU.mult),
                    reads=["maskA", f"Dt{s % GB}"], writes=[f"mD{s % GB}"])
                kb.op("pool", lambda e, s=s: e.tensor_tensor(out=T["Dts"][0:CH, s % GB, :], in0=T["Dts"][0:CH, s % GB, :],
                                                            in1=maskT[0:CH, :], op=ALU.mult),
                      reads=["maskT", f"Dts{s % GB}"], writes=[f"Dts{s % GB}"])
                kb.op("act", lambda e, s=s, c0_=c0_: e.activation(out=T["dL"][0:CH, s:s + 1], in_=T["Gc"][0:CH, s:s + 1],
                                                                func=AF.Exp, scale=-1.0, bias=Gb[0:CH, c0_ + CH - 1:c0_ + CH]),
                      reads=[f"Gc{s}", hk_], writes=[f"dL{s}"])
                mA, mT_, mAk, mTk = T["mD"][0:CH, s % GB, :], T["Dts"][0:CH, s % GB, :], f"mD{s % GB}", f"Dts{s % GB}"
            kb.op("dve", lambda e, s=s, pa=pa, mA=mA: e.tensor_tensor(out=T["AMf"][0:CH, s, :], in0=pa[0:CH, 0:CH],
                                                                     in1=mA[:, 0:CH], op=ALU.mult),
                  reads=[pk, mAk], writes=[f"AM{s}"])
            kb.op("dve", lambda e, s=s, pa=pa, mA=mA: e.tensor_tensor(out=T["AMb"][0:CH, s, :], in0=pa[0:CH, CH:4 * CH],
                                                                     in1=mA[:, CH:4 * CH], op=ALU.mult),
                  reads=[pk, mAk], writes=[f"AMb{s}"])
            kb.op("dve", lambda e, s=s, pn=pn, mT_=mT_: e.tensor_tensor(out=T["MM"][0][0:CH, s, CH:2 * CH],
                                                                       in0=pn[0:CH, 256:256 + CH], in1=mT_, op=ALU.mult),
                  reads=[pnk, mTk], writes=[f"MM0_{s}"])
            kb.op("pool", lambda e, s=s: e.tensor_copy(out=T["MM"][0][0:CH, s, 0:CH], in_=T["AMf"][0:CH, s, :]),
                  reads=[f"AM{s}"], writes=[f"MM0_{s}"])
            kb.op("pool", lambda e, s=s: e.tensor_tensor(out=T["Q"][0][0:CH, s, :], in0=T["AMf"][0:CH, s, :],
                                                        in1=ident64[0:CH, 0:CH], op=ALU.add),
                  reads=[f"AM{s}", "c_ident"], writes=[f"Q0_{s}"])
            yield
        for lv in range(NLV):
            a, b = lv % 2, (lv + 1) % 2
            last = lv == NLV - 1
            for ci in grp:
                s = ci % GB + GB * par
                pm = ps[2 + s % 2]
                pmk = f"ps{2 + s % 2}"
                MMa = T["MM"][a]
                if not last:
                    kb.op("pe", lambda e, s=s, pm=pm, MMa=MMa: e.matmul(
                        pm[0:CH, 0:CH], lhsT=MMa[0:CH, s, CH:2 * CH], rhs=MMa[0:CH, s, 0:CH], start=True, stop=True),
                        reads=[f"MM{a}_{s}"], writes=[pmk])
                kb.op("pe", lambda e, s=s, pm=pm, MMa=MMa: e.matmul(
                    pm[0:CH, CH:2 * CH], lhsT=MMa[0:CH, s, 0:CH], rhs=MMa[0:CH, s, CH:2 * CH], start=True, stop=True),
                    reads=[f"MM{a}_{s}"], writes=[pmk])
                lo = CH if last else 0
                kb.op("act", lambda e, s=s, pm=pm, b=b, lo=lo: e.copy(out=T["MM"][b][0:CH, s, lo:2 * CH],
                                                                     in_=pm[0:CH, lo:2 * CH]),
                      reads=[pmk], writes=[f"MM{b}_{s}"])
                yield
            for ci in grp:
                s = ci % GB + GB * par
                pq = ps[4 + s % 2]
                pqk = f"ps{4 + s % 2}"
                kb.op("pe", lambda e, s=s, pq=pq, b=b, a=a: e.matmul(
                    pq[0:CH, 0:CH], lhsT=T["MM"][b][0:CH, s, CH:2 * CH], rhs=T["Q"][a][0:CH, s, :], start=True, stop=True),
                    reads=[f"MM{b}_{s}", f"Q{a}_{s}"], writes=[pqk])
                kb.op("dve", lambda e, s=s, pq=pq, a=a, b=b: e.tensor_tensor(
                    out=T["Q"][b][0:CH, s, :], in0=pq[0:CH, 0:CH], in1=T["Q"][a][0:CH, s, :], op=ALU.add),
                    reads=[pqk, f"Q{a}_{s}"], writes=[f"Q{b}_{s}"])
                yield

    def chain_gen(g0):
        grp = list(range(g0, min(NCH, g0 + GB)))
        par = (g0 // GB) % 2
        QF = T["Q"][NLV % 2]
        for ci in grp:
            s = ci % GB + GB * par
            c0 = CHUNK_COLS[ci]
            cs = slice(c0, c0 + CH)
            AMb = T["AMb"]
            STb = T["STb"]
            pt = ps[6][:].bitcast(BF16) if tb else ps[6]
            idt = c.identb if tb else ident
            for j, src in enumerate((vT, kT, bT)):
                kb.op("pe", lambda e, j=j, src=src, cs=cs, pt=pt: e.transpose(
                    out=pt[0:CH, j * dk:(j + 1) * dk], in_=src[bp:bp + dk, cs], identity=idt[bp:bp + dk, bp:bp + dk]),
                    reads=[hk_, "c_ident", "c_identb"], writes=["ps6"])
            TM = T["TM"][ci % 2]
            tmk = f"TM{ci % 2}"
            kb.op("act", lambda e, TM=TM, pt=pt: e.copy(out=TM[0:CH, 0:3 * dk], in_=pt[0:CH, 0:3 * dk]),
                  reads=["ps6"], writes=[tmk])
            yield
            Vtm, Ktm, Btm = TM[0:CH, 0:dk], TM[0:CH, dk:2 * dk], TM[0:CH, 2 * dk:3 * dk]
            if Gb is not None:
                kb.op("dve", lambda e, TM=TM, s=s: e.tensor_scalar(out=TM[0:CH, dk:3 * dk], in0=TM[0:CH, dk:3 * dk],
                                                                 scalar1=T["dL"][0:CH, s:s + 1], scalar2=None, op0=ALU.mult),
                      reads=[tmk, f"dL{s}"], writes=[tmk])
            pr = ps[7]
            kb.op("pe", lambda e, cs=cs, pr=pr: e.matmul(pr[0:CH, 0:dk], lhsT=kkC[bp:bp + dk, cs], rhs=STb[bp:bp + dk, 0:dk],
                                                       start=True, stop=False), reads=[hk_, "STb"], writes=["ps7"])
            kb.op("pe", lambda e, s=s, pr=pr, Vtm=Vtm: e.matmul(pr[0:CH, 0:dk], lhsT=AMb[0:CH, s, 0:CH], rhs=Vtm,
                                                              start=False, stop=True),
                  reads=[f"AMb{s}", tmk], writes=["ps7"])
            yield
            kb.op("dve", lambda e, pr=pr: e.tensor_scalar(out=T["nR"][0:CH, 0:dk], in0=pr[0:CH, 0:dk], scalar1=-1.0,
                                                         scalar2=None, op0=ALU.mult), reads=["ps7"], writes=["nR"])
            yield
            kb.op("pe", lambda e, s=s, pr=pr: e.matmul(pr[0:CH, 128:128 + dk], lhsT=QF[0:CH, s, :], rhs=T["nR"][0:CH, 0:dk],
                                                     start=True, stop=True), reads=[f"Q{NLV % 2}_{s}", "nR"], writes=["ps7"])
            yield
            kb.op("act", lambda e, pr=pr: e.copy(out=T["U"][0:CH, 0:dk], in_=pr[0:CH, 128:128 + dk]),
                  reads=["ps7"], writes=["U"])
            yield
            U = T["U"]
            kb.op("pe", lambda e, cs=cs, pr=pr: e.matmul(pr[0:CH, 256:256 + dk], lhsT=rC[bp:bp + dk, cs], rhs=STb[bp:bp + dk, 0:dk],
                                                       start=True, stop=False), reads=[hk_, "STb"], writes=["ps7"])
            kb.op("pe", lambda e, s=s, pr=pr: e.matmul(pr[0:CH, 256:256 + dk], lhsT=AMb[0:CH, s, CH:2 * CH],
                                                     rhs=U[0:CH, 0:dk], start=False, stop=False),
                  reads=[f"AMb{s}", "U"], writes=["ps7"])
            kb.op("pe", lambda e, s=s, pr=pr, Vtm=Vtm: e.matmul(pr[0:CH, 256:256 + dk], lhsT=AMb[0:CH, s, 2 * CH:3 * CH],
                                                              rhs=Vtm, start=False, stop=True),
                  reads=[f"AMb{s}", tmk], writes=["ps7"])
            kb.op("pe", lambda e, pr=pr, Btm=Btm: e.matmul(pr[bp:bp + dk, 384:384 + dk], lhsT=Btm, rhs=U[0:CH, 0:dk],
                                                         start=True, stop=False), reads=[tmk, "U"], writes=["ps7"])
            kb.op("pe", lambda e, pr=pr, Ktm=Ktm, Vtm=Vtm: e.matmul(pr[bp:bp + dk, 384:384 + dk], lhsT=Ktm, rhs=Vtm,
                                                                  start=False, stop=True),
                  reads=[tmk], writes=["ps7"])
            yield
            Ysb = T["Y"][ci % 2]
            yk = f"Y{ci % 2}"
            kb.op("act", lambda e, pr=pr, Ysb=Ysb: e.copy(out=Ysb[0:CH, 0:dk], in_=pr[0:CH, 256:256 + dk]),
                  reads=["ps7"], writes=[yk])
            if Gb is not None:
                kb.op("dve", lambda e, pr=pr, c0=c0: e.scalar_tensor_tensor(
                    out=ST[bp:bp + dk, 0:dk], in0=ST[bp:bp + dk, 0:dk], scalar=Pinc[bp:bp + dk, c0 + CH - 1:c0 + CH],
                    in1=pr[bp:bp + dk, 384:384 + dk], op0=ALU.mult, op1=ALU.add), reads=["ps7", "ST", hk_], writes=["ST"])
            else:
                kb.op("dve", lambda e, pr=pr: e.tensor_tensor(out=ST[bp:bp + dk, 0:dk], in0=pr[bp:bp + dk, 384:384 + dk],
                                                             in1=ST[bp:bp + dk, 0:dk], op=ALU.add),
                      reads=["ps7", "ST"], writes=["ST"])
                kb.op("dve", lambda e, c0=c0: e.tensor_scalar(out=ST[bp:bp + dk, 0:dk], in0=ST[bp:bp + dk, 0:dk],
                                                             scalar1=Pinc[bp:bp + dk, c0 + CH - 1:c0 + CH], scalar2=None,
                                                             op0=ALU.mult), reads=["ST", hk_], writes=["ST"])
            kb.op("act", lambda e: e.copy(out=T["STb"][bp:bp + dk, 0:dk], in_=ST[bp:bp + dk, 0:dk]),
                  reads=["ST"], writes=["STb"])
            if prodT is not None:
                pb = ps[6]
                kb.op("pe", lambda e, cs=cs, pb=pb: e.matmul(pb[0:CH, 448:449], lhsT=prodT[bp:bp + dk, cs],
                                                           rhs=c.onesb[bp:bp + dk, 0:1], start=True, stop=True),
                      reads=[hk_, "c_ones"], writes=["ps6"])
                kb.op("dve", lambda e, pb=pb, Ysb=Ysb, Vtm=Vtm: e.tensor_scalar(
                    out=Ysb[0:CH, dk:2 * dk], in0=Vtm, scalar1=pb[0:CH, 448:449], scalar2=None, op0=ALU.mult),
                    reads=["ps6", tmk], writes=[yk])
            store_cb(ci, Ysb, yk)
            yield

    from itertools import zip_longest
    for _ in inv_gen(0):
        pass
    for g0 in range(0, NCH, GB):
        gens = [chain_gen(g0)]
        if g0 + GB < NCH:
            gens.append(inv_gen(g0 + GB))
        for _ in zip_longest(*gens):
            pass


def mixer_stage(kb, nc, g, c, mods, x_lat_in, x_ctx_in, x_lat_out, x_ctx_out):
    from contextlib import ExitStack
    mod = mods[0]
    Ys = g["ys_scr"]
    Zs = g["z_scr"]
    with ExitStack() as es:
        def sb(name, shape, dt):
            return es.enter_context(nc.sbuf_tensor(f"mxs_{name}", shape, dt))
        hT = None
        es2 = ExitStack()

        def sb2(name, shape, dt):
            return es2.enter_context(nc.sbuf_tensor(f"mxs_{name}", shape, dt))
        hb = sb("hb", [128, D], BF16)
        stt = [sb("st0", [128, 4], F32), sb("st1", [128, 4], F32)]
        F11 = sb("F11", [128, TP], F32)
        Jb = sb("Jb", [128, 128], BF16)
        J32 = sb("J32", [128, 128], F32)
        par = sb("par", [64, 64], F32)
        parP = sb("parP", [128, 64], F32)
        bd = sb("bd", [128, 128], F32)
        par128 = sb("par128", [128, 112], F32)
        w2sb = sb("w2sb", [128, 2, 512], F32)
        sel = sb("sel", [16, 16, 128], F32)
        ST = sb("ST", [128, 128], F32)
        hT = es2.enter_context(nc.sbuf_tensor("mxs_hT", [128, 8, 2304], BF16))
        F = [sb2(f"F{i}", [128, TP], F32) for i in range(11)] + [F11]
        FK = [f"F{i}" for i in range(12)]
        Gbc = F[1][:, 0:D]
        Sbc = F[2][:, 0:D]
        h32 = F[3][:, 0:D]
        xb = [F[4][:, 0:D], F[5][:, 0:D]]
        h32s = [F[3][:, 0:D], F[6][:, 0:D]]
        hb2 = sb2("hb2", [128, D], BF16)
        hbs = [hb, hb2]
        rmask = sb2("rmask", [128, TP], BF16)
        rm32 = F[0]
        wsl = sb2("wsl", [128, 8, 128], BF16)
        T = {"ST": ST,
             "AMf": sb2("AMf", [CHUNK, 4, CHUNK], F32), "AMb": sb2("AMb", [CHUNK, 4, 3 * CHUNK], BF16),
             "STb": sb2("STb", [128, 128], BF16),
             "MM": [sb2("MMa", [CHUNK, 4, 2 * CHUNK], F32), sb2("MMb", [CHUNK, 4, 2 * CHUNK], F32)],
             "Q": [sb2("Qa", [CHUNK, 4, CHUNK], F32), sb2("Qb", [CHUNK, 4, CHUNK], F32)],
             "TM": [sb2("TMa", [CHUNK, 384], BF16), sb2("TMb", [CHUNK, 384], BF16)],
             "nR": sb2("nR", [CHUNK, 128], F32), "U": sb2("U", [CHUNK, 128], BF16), "Uf": sb2("Uf", [16, 4], F32),
             "Y": [sb2("Ya", [CHUNK, 256], F32), sb2("Yb", [CHUNK, 256], F32)],
             "maskA": sb2("maskA", [CHUNK, 4 * CHUNK], F32), "maskT": sb2("maskT", [CHUNK, CHUNK], F32),
             "Gc": sb2("Gc", [CHUNK, 4], F32), "dL": sb2("dL", [CHUNK, 4], F32), "Dt": sb2("Dt", [CHUNK, 2, CHUNK], F32),
             "Dts": sb2("Dts", [CHUNK, 2, CHUNK], F32), "mD": sb2("mD", [CHUNK, 2, 4 * CHUNK], F32)}
        ps = [es.enter_context(nc.psum_tensor(f"mx_ps{i}", [128, 512], F32)) for i in range(8)]
        T["ps"] = ps
        kb.dma("sp", rm32[:], g["k_rmask"][:, :], writes=["F0"])
        kb.op("dve", lambda e: e.tensor_copy(out=rmask[:], in_=rm32[:]), reads=["F0"], writes=["rmask"])
        kb.dma("sp", J32[:], g["k_J"][:, :], writes=["J32"])
        kb.op("dve", lambda e: e.tensor_copy(out=Jb[:], in_=J32[:]), reads=["J32"], writes=["Jb"])
        kb.dma("sp", par[:], g["mx_par64"][:, :], writes=["par"])
        kb.dma("sp", par128[:], g["mx_par128"][:, :], writes=["par128"])
        kb.dma("sp", w2sb[:], g["mx_w2"][:, :, :], writes=["w2sb"])
        kb.dma("sp", parP[:], g["mx_parP"][:, :], writes=["parP"])
        kb.dma("sp", bd[:], g["k_bd"][:, :], writes=["bd"])
        kb.op("dve", lambda e: e.tensor_scalar(out=parP[:, 36:40], in0=parP[:, 32:36], scalar1=-1.0, scalar2=1.0,
                                               op0=ALU.mult, op1=ALU.add), reads=["parP"], writes=["parP"])
        kb.op("dve", lambda e: e.tensor_scalar(out=par128[:, 24:32], in0=par128[:, 16:24], scalar1=-1.0, scalar2=1.0,
                                               op0=ALU.mult, op1=ALU.add), reads=["par128"], writes=["par128"])
        T["zt"] = [sb2("zta", [128, 128], F32), sb2("ztb", [128, 128], F32)]
        kb.dma("sp", sel[:], g["k_sel"][:, :, :], writes=["sel"])
        kb.dma("sp", T["maskA"][:], g["k_maskA"][:, :], writes=["maskA"])
        kb.dma("sp", T["maskT"][:], g["k_maskT"][:, :], writes=["maskT"])
        for f in range(12):
            kb.op("pool", lambda e, f=f: e.memset(F[f][:], 0.0), reads=["rmask"] if f == 0 else [], writes=[FK[f]])
        wv = g["e_w_in2"].rearrange("(kc p) n -> p kc n", p=128)
        P64 = lambda j: par[:, j:j + 1]

        def project(c0, M, dst, dk_, evac_eng="act"):
            kb.dma("pool", wsl[:, :, 0:M], wv[:, :, c0:c0 + M], writes=["wsl"])
            for bi, (t0, tw, col0) in enumerate(BLOCKS):
                pb = ps[bi % 2]
                for kc in range(8):
                    kb.op("pe", lambda e, kc=kc, t0=t0, tw=tw, pb=pb: e.matmul(
                        pb[0:M, 0:tw], lhsT=wsl[:, kc, 0:M], rhs=hT[:, kc, t0:t0 + tw], start=(kc == 0), stop=(kc == 7)),
                        reads=["wsl", "hT"], writes=[f"ps{bi % 2}"])
                kb.op("act", lambda e, tw=tw, col0=col0, pb=pb: e.copy(out=dst[0:M, col0:col0 + tw], in_=pb[0:M, 0:tw]),
                      reads=[f"ps{bi % 2}"], writes=[dk_])

        def tshift(src, sk, dst, dk_, tmp, tk, P, mucol):
            n = TP - 2
            kb.op("dve", lambda e: e.tensor_tensor(out=tmp[0:P, 1:1 + n], in0=src[0:P, 0:n], in1=src[0:P, 2:2 + n],
                                                   op=ALU.add), reads=[sk], writes=[tk])
            kb.op("dve", lambda e: e.scalar_tensor_tensor(out=tmp[0:P, 1:1 + n], in0=tmp[0:P, 1:1 + n], scalar=0.5,
                                                          in1=src[0:P, 1:1 + n], op0=ALU.mult, op1=ALU.subtract),
                  reads=[sk, tk], writes=[tk])
            kb.op("dve", lambda e: e.scalar_tensor_tensor(out=dst[0:P, 1:1 + n], in0=tmp[0:P, 1:1 + n], scalar=mucol,
                                                         in1=src[0:P, 1:1 + n], op0=ALU.mult, op1=ALU.add),
                  reads=[sk, tk, "par", "par128"], writes=[dk_])

        DC = [(CTX0, 256), (LAT0, 2048)]

        def ew(eng, fn, reads, writes):
            kb.op(eng, fn, reads=reads, writes=writes)

        for d in range(2):
            for stream in (1, 0):
                kb.barrier()
                make_bc(kb, nc, c, lambda kc: mod[:, 1 * 8 + kc, stream:stream + 1], Gbc, ps[0], "Gbc", ["mod0"])
                make_bc(kb, nc, c, lambda kc: mod[:, 0 * 8 + kc, stream:stream + 1], Sbc, ps[0], "Sbc", ["mod0"])
                nt = 2 if stream == 1 else 16
                src = x_ctx_in if stream == 1 else x_lat_in
                base = 0 if stream == 1 else 256
                def htile(i):
                    b = i % 2
                    hbb, h32b = hbs[b], h32s[b]
                    kb.dma("sp", xb[b][:], src[i * 128:(i + 1) * 128, :], writes=[f"xb{b}"])
                    yield
                    yield from norm_tile_g(kb, nc, xb[b][:], f"xb{b}", stt[b], Gbc, Sbc, h32b, f"h32{b}")
                    yield
                    kb.op("act", lambda e: e.copy(out=hbb[:], in_=h32b), reads=[f"h32{b}"], writes=[f"hb{b}"])
                    yield
                    pos = base + (i if d == 0 else nt - 1 - i) * 128
                    for half in range(2):
                        pbk = 2 + half + 2 * b
                        for q in range(4):
                            kc = half * 4 + q
                            kb.op("pe", lambda e, kc=kc, q=q, pbk=pbk: e.matmul(
                                ps[pbk][:, q * 128:(q + 1) * 128], lhsT=hbb[:, kc * 128:(kc + 1) * 128],
                                rhs=(c.identb[:] if d == 0 else Jb[:]), start=True, stop=True),
                                reads=[f"hb{b}", "c_identb", "Jb"], writes=[f"ps{pbk}"])
                        yield
                        kb.op("dve" if half == 0 else "act", (lambda e, half=half, pos=pos, pbk=pbk: e.tensor_copy(
                            out=hT[:, half * 4:(half + 1) * 4, pos:pos + 128],
                            in_=ps[pbk][:].rearrange("p (q t) -> p q t", q=4))) if half == 0 else
                            (lambda e, half=half, pos=pos, pbk=pbk: e.copy(
                                out=hT[:, half * 4:(half + 1) * 4, pos:pos + 128],
                                in_=ps[pbk][:].rearrange("p (q t) -> p q t", q=4))),
                            reads=[f"ps{pbk}"], writes=[("hT", pos, half)])
                        yield
                pending = [htile(i) for i in range(nt)]
                active = []
                while pending or active:
                    if pending and len(active) < 2:
                        active.append(pending.pop(0))
                    for g_ in list(active):
                        try:
                            next(g_)
                        except StopIteration:
                            active.remove(g_)
            kb.barrier()

            def store_cb_factory(col0, dk_):
                def cb(ci, Ysb, yk):
                    r0 = ci * CHUNK
                    kb.dma("sp", Ys[d, r0:r0 + CHUNK, col0:col0 + dk_], Ysb[0:CHUNK, 0:dk_], reads=[yk],
                           writes=[("ys", d, ci, col0)])
                    if col0 < 512:
                        kb.dma("sp", Ys[d, r0:r0 + CHUNK, 1024 + col0:1024 + col0 + dk_], Ysb[0:CHUNK, dk_:2 * dk_],
                               reads=[yk], writes=[("ysb", d, ci, col0)])
                return cb

            project(1536 + d * 128, 128, F[0], FK[0])
            tshift(F[0], FK[0], F[10], FK[10], F[1], FK[1], 128, par128[:, d:d + 1])
            ew("act", lambda e: e.activation(out=F[10][0:64, :], in_=F[10][0:64, :], func=AF.Tanh), [FK[10]], [FK[10]])
            if d == 0:
                project(1792, 128, F[0], FK[0])
                tshift(F[0], FK[0], F[11], FK[11], F[1], FK[1], 128, par128[:, 2:3])
                ew("act", lambda e: e.activation(out=F[11][:], in_=F[11][:], func=AF.Sigmoid), [FK[11]], [FK[11]])
            for p in range(4):
                hk_ = f"pair{d}_{p}"
                PP = lambda j: parP[:, j:j + 1]
                for part, dst in ((0, 1), (1, 2), (2, 3)):
                    project(p * 384 + part * 128, 128, F[0], FK[0])
                    tshift(F[0], FK[0], F[dst], FK[dst], F[7], FK[7], 128, PP(p * 3 + part))
                for bi, (t0, tw, col0) in enumerate(BLOCKS):
                    cs = slice(col0, col0 + tw)
                    kb.op("pe", lambda e, cs=cs, tw=tw: e.matmul(ps[2][:, 0:tw], lhsT=w2sb[0:64, d, p * 128:(p + 1) * 128],
                                                               rhs=F[10][0:64, cs], start=True, stop=True),
                          reads=["w2sb", FK[10]], writes=["ps2"])
                    kb.op("pe", lambda e, cs=cs, tw=tw: e.matmul(ps[3][:, 0:tw], lhsT=w2sb[64:128, d, p * 128:(p + 1) * 128],
                                                               rhs=F[10][64:128, cs], start=True, stop=True),
                          reads=["w2sb", FK[10]], writes=["ps3"])
                    kb.op("act", lambda e, cs=cs, tw=tw: e.activation(out=F[4][:, cs], in_=ps[2][:, 0:tw],
                                                                    func=AF.Sigmoid, bias=PP(12 + d * 4 + p)),
                          reads=["ps2", "parP"], writes=[FK[4]])
                    kb.op("act", lambda e, cs=cs, tw=tw: e.activation(out=F[5][:, cs], in_=ps[3][:, 0:tw],
                                                                    func=AF.Sigmoid, bias=PP(20 + d * 4 + p)),
                          reads=["ps3", "parP"], writes=[FK[5]])
                kkc, kac, kac1, rkc = PP(28 + p), PP(32 + p), PP(36 + p), PP(40 + p)
                ew("dve", lambda e: e.tensor_scalar(out=F[7][:, :], in0=F[2][:, :], scalar1=kkc, scalar2=None,
                                                    op0=ALU.mult), [FK[2], "parP"], [FK[7]])
                for (a0_, n_) in DC:
                    ew("pool", lambda e, a0_=a0_, n_=n_: e.tensor_tensor(
                        out=F[0][:, a0_:a0_ + n_], in0=F[7][:, a0_:a0_ + n_], in1=F[7][:, a0_:a0_ + n_],
                        op=ALU.mult), [FK[7]], [FK[0]])
                for bi, (t0, tw, col0) in enumerate(BLOCKS):
                    cs = slice(col0, col0 + tw)
                    kb.op("pe", lambda e, cs=cs, tw=tw: e.matmul(ps[2][:, 0:tw], lhsT=bd[:, :],
                                                               rhs=F[0][:, cs], start=True, stop=True),
                          reads=["bd", FK[0]], writes=["ps2"])
                    kb.op("act", lambda e, cs=cs, tw=tw: e.activation(out=F[9][:, cs], in_=ps[2][:, 0:tw],
                                                                    func=AF.Sqrt, bias=c.eps6[:, 0:1]),
                          reads=["ps2", "c_eps"], writes=[FK[9]])
                for (a0_, n_) in DC:
                    cs = slice(a0_, a0_ + n_)
                    ew("dve", lambda e, cs=cs: e.reciprocal(out=F[9][:, cs], in_=F[9][:, cs]), [FK[9]], [FK[9]])
                    ew("dve", lambda e, cs=cs: e.tensor_tensor(out=F[6][:, cs], in0=F[7][:, cs], in1=F[9][:, cs],
                                                               op=ALU.mult), [FK[7], FK[9]], [FK[6]])
                    ew("pool", lambda e, cs=cs: e.tensor_scalar(out=F[7][:, cs], in0=F[5][:, cs], scalar1=kac,
                                                                scalar2=kac1, op0=ALU.mult, op1=ALU.add),
                       [FK[5], "parP"], [FK[7]])
                    ew("pool", lambda e, cs=cs: e.tensor_tensor(out=F[7][:, cs], in0=F[7][:, cs], in1=F[2][:, cs],
                                                                op=ALU.mult), [FK[7], FK[2]], [FK[7]])
                    ew("dve", lambda e, cs=cs: e.tensor_tensor(out=F[9][:, cs], in0=F[6][:, cs], in1=F[5][:, cs],
                                                               op=ALU.mult), [FK[6], FK[5]], [FK[9]])
                    ew("dve", lambda e, cs=cs: e.scalar_tensor_tensor(out=F[0][:, cs], in0=F[1][:, cs], scalar=rkc,
                                                                      in1=F[7][:, cs], op0=ALU.mult, op1=ALU.mult),
                       [FK[1], FK[7], "parP"], [FK[0]])
                ew("dve", lambda e: e.tensor_tensor_scan(out=F[8][:, :], data0=rmask[:, :], data1=F[4][:, :],
                                                         initial=0.0, op0=ALU.mult, op1=ALU.add),
                   ["rmask", FK[4]], [FK[8]])
                B4 = F[4][:, :].bitcast(BF16)
                B5 = F[5][:, :].bitcast(BF16)
                B2 = F[2][:, :].bitcast(BF16)
                DCS = [slice(a0_, a0_ + n_) for (a0_, n_) in DC]
                DCH = [slice(TP + a0_, TP + a0_ + n_) for (a0_, n_) in DC]
                for cs in DCS:
                    ew("pool", lambda e, cs=cs: e.tensor_tensor(out=F[2][:, cs], in0=F[8][:, cs], in1=F[4][:, cs],
                                                                op=ALU.subtract), [FK[8], FK[4]], [FK[2]])
                    ew("act", lambda e, cs=cs: e.activation(out=F[2][:, cs], in_=F[2][:, cs], func=AF.Exp,
                                                            scale=-DECAY_K), [FK[2]], [FK[2]])
                for cs in DCS:
                    ew("dve", lambda e, cs=cs: e.tensor_tensor(out=B4[:, cs], in0=F[6][:, cs], in1=F[2][:, cs],
                                                               op=ALU.mult), [FK[6], FK[2]], [FK[4]])
                for cs in DCS:
                    ew("act", lambda e, cs=cs: e.activation(out=F[2][:, cs], in_=F[8][:, cs], func=AF.Exp,
                                                            scale=DECAY_K), [FK[8], FK[2], FK[4]], [FK[2]])
                for cs, ch in zip(DCS, DCH):
                    ew("dve", lambda e, cs=cs, ch=ch: e.tensor_tensor(out=B4[:, ch], in0=F[7][:, cs], in1=F[2][:, cs],
                                                                      op=ALU.mult), [FK[7], FK[2]], [FK[4]])
                    ew("pool", lambda e, cs=cs: e.tensor_tensor(out=B5[:, cs], in0=F[9][:, cs], in1=F[2][:, cs],
                                                                op=ALU.mult), [FK[9], FK[2]], [FK[5]])
                for cs in DCS:
                    ew("act", lambda e, cs=cs: e.activation(out=F[8][:, cs], in_=F[8][:, cs], func=AF.Exp,
                                                            scale=-DECAY_K), [FK[8]], [FK[8]])
                for cs, ch in zip(DCS, DCH):
                    ew("dve", lambda e, cs=cs: e.tensor_tensor(out=B2[:, cs], in0=F[1][:, cs], in1=F[8][:, cs],
                                                               op=ALU.mult), [FK[1], FK[8], FK[4], FK[5]], [FK[2]])
                    ew("pool", lambda e, cs=cs, ch=ch: e.tensor_copy(out=B5[:, ch], in_=F[3][:, cs]), [FK[3]], [FK[5]])
                    ew("act", lambda e, cs=cs, ch=ch: e.copy(out=B2[:, ch], in_=F[0][:, cs]), [FK[0]], [FK[2]])
                HI = lambda B: B[:, TP:2 * TP]
                kb.op("pool", lambda e: e.memset(T["nR"][:], 0.0),
                      reads=[FK[2], FK[4], FK[5], FK[8]], writes=[hk_, "nR"])
                for hh in range(2):
                    dplr_scan(kb, nc, c, T, 64, B2[:, 0:TP], B4[:, 0:TP], HI(B4), B5[:, 0:TP], HI(B5), F[8], HI(B2),
                              store_cb_factory((2 * p + hh) * 64, 64), hk_, bp=hh * 64)
                kb.op("pool", lambda e: e.memset(T["nR"][:], 0.0), reads=[hk_],
                      writes=[FK[2], FK[4], FK[5], FK[8], "nR"])
            mixer_gdn_pass(kb, nc, g, c, T, d, F, FK, rmask, par128, sel, project, store_cb_factory, ps, DC, wv, wsl,
                           hT, Zs)
        kb.barrier()
        es2.close()
        import os
        if os.environ.get("MIX_DUMP"):
            kb.dma("sp", x_lat_out[0:2048, :], Ys[0, 0:2048, 0:1024], writes=["o1"])
            kb.dma("sp", x_ctx_out[0:256, :], Ys[0, 0:256, 512:1536], writes=["o2"])
            kb.barrier()
            return
        mixer_output(kb, nc, g, c, mods, T, F, FK, x_lat_in, x_ctx_in, x_lat_out, x_ctx_out, Ys, Zs, J32, None, None, hb,
                     ps, par128)
        kb.barrier()


def mixer_gdn_pass(kb, nc, g, c, T, d, F, FK, rmask, par128, sel, project, store_cb_factory, ps, DC, wv, wsl, hT, Zs):
    ew = lambda eng, fn, r, w: kb.op(eng, fn, reads=r, writes=w)
    project(3968, 16, F[10], FK[10])
    R16 = lambda i: F[i][0:16, :]
    ew("act", lambda e: e.activation(out=T["Uf"][0:16, 0:1], in_=par128[0:16, 41:42], func=AF.Exp), ["par128"], ["U"])
    ew("dve", lambda e: e.tensor_scalar(out=T["Uf"][0:16, 0:1], in0=T["Uf"][0:16, 0:1], scalar1=-1.0, scalar2=None,
                                        op0=ALU.mult), ["U"], ["U"])
    ew("dve", lambda e: e.tensor_scalar(out=R16(1), in0=R16(10), scalar1=par128[0:16, 40:41], scalar2=None, op0=ALU.add),
       [FK[10], "par128"], [FK[1]])
    ew("act", lambda e: e.activation(out=R16(4), in_=R16(10), func=AF.Sigmoid), [FK[10]], [FK[4]])
    ew("act", lambda e: e.activation(out=R16(2), in_=R16(1), func=AF.Abs), [FK[1]], [FK[2]])
    ew("act", lambda e: e.activation(out=R16(2), in_=R16(2), func=AF.Exp, scale=-1.0), [FK[2]], [FK[2]])
    ew("dve", lambda e: e.tensor_scalar(out=R16(3), in0=R16(2), scalar1=2.0, scalar2=None, op0=ALU.add), [FK[2]], [FK[3]])
    ew("dve", lambda e: e.reciprocal(out=R16(3), in_=R16(3)), [FK[3]], [FK[3]])
    ew("dve", lambda e: e.tensor_tensor(out=R16(2), in0=R16(2), in1=R16(3), op=ALU.mult), [FK[2], FK[3]], [FK[2]])
    ew("dve", lambda e: e.tensor_tensor(out=R16(3), in0=R16(2), in1=R16(2), op=ALU.mult), [FK[2]], [FK[3]])
    ew("dve", lambda e: e.tensor_scalar(out=R16(6), in0=R16(3), scalar1=1.0 / 13, scalar2=1.0 / 11, op0=ALU.mult,
                                        op1=ALU.add), [FK[3]], [FK[6]])
    for cf in (1.0 / 9, 1.0 / 7, 1.0 / 5, 1.0 / 3, 1.0):
        ew("dve", lambda e: e.tensor_tensor(out=R16(6), in0=R16(6), in1=R16(3), op=ALU.mult), [FK[6], FK[3]], [FK[6]])
        ew("dve", lambda e, cf=cf: e.tensor_scalar(out=R16(6), in0=R16(6), scalar1=cf, scalar2=None, op0=ALU.add),
           [FK[6]], [FK[6]])
    ew("dve", lambda e: e.scalar_tensor_tensor(out=R16(6), in0=R16(6), scalar=2.0, in1=R16(2), op0=ALU.mult, op1=ALU.mult),
       [FK[6], FK[2]], [FK[6]])
    ew("dve", lambda e: e.tensor_scalar(out=R16(1), in0=R16(1), scalar1=0.0, scalar2=None, op0=ALU.max), [FK[1]], [FK[1]])
    ew("dve", lambda e: e.tensor_tensor(out=R16(6), in0=R16(6), in1=R16(1), op=ALU.add), [FK[6], FK[1]], [FK[6]])
    ew("dve", lambda e: e.tensor_scalar(out=R16(6), in0=R16(6), scalar1=T["Uf"][0:16, 0:1], scalar2=None, op0=ALU.mult),
       [FK[6], "U"], [FK[6]])
    ew("dve", lambda e: e.tensor_tensor_scan(out=R16(10), data0=rmask[0:16, :], data1=R16(6), initial=0.0,
                                             op0=ALU.mult, op1=ALU.add), ["rmask", FK[6]], [FK[10]])
    for h in range(4):
        hk_ = f"ghead{d}_{h}"
        c0 = 1920 + h * 512
        for part, dst in ((0, 1), (1, 2), (2, 3)):
            project(c0 + part * 128, 128, F[0], FK[0])
            n = TP - 4
            for j in range(5):
                jj = j if d == 0 else 4 - j
                wc = par128[:, 48 + (h * 3 + part) * 5 + jj:49 + (h * 3 + part) * 5 + jj]
                if j == 0:
                    ew("dve", lambda e, wc=wc, dst=dst: e.tensor_scalar(out=F[dst][:, 2:2 + n], in0=F[0][:, 0:n],
                                                                      scalar1=wc, scalar2=None, op0=ALU.mult),
                       [FK[0], "par128"], [FK[dst]])
                else:
                    ew("dve", lambda e, wc=wc, dst=dst, j=j: e.scalar_tensor_tensor(
                        out=F[dst][:, 2:2 + n], in0=F[0][:, j:j + n], scalar=wc, in1=F[dst][:, 2:2 + n],
                        op0=ALU.mult, op1=ALU.add), [FK[0], FK[dst], "par128"], [FK[dst]])
            ew("act", lambda e, dst=dst: e.activation(out=F[dst][:, :], in_=F[dst][:, :], func=AF.Silu), [FK[dst]], [FK[dst]])
        for src, scl in ((1, float(128 ** -0.5)), (2, 1.0)):
            for (a0_, n_) in DC:
                ew("pool", lambda e, src=src, a0_=a0_, n_=n_: e.tensor_tensor(
                    out=F[0][:, a0_:a0_ + n_], in0=F[src][:, a0_:a0_ + n_], in1=F[src][:, a0_:a0_ + n_], op=ALU.mult),
                    [FK[src]], [FK[0]])
            for bi, (t0, tw, col0) in enumerate(BLOCKS):
                cs = slice(col0, col0 + tw)
                kb.op("pe", lambda e, cs=cs, tw=tw: e.matmul(ps[2][:, 0:tw], lhsT=c.ones[:, :], rhs=F[0][:, cs],
                                                           start=True, stop=True), reads=["c_ones", FK[0]], writes=["ps2"])
                kb.op("act", lambda e, cs=cs, tw=tw: e.activation(out=F[9][:, cs], in_=ps[2][:, 0:tw], func=AF.Sqrt,
                                                                bias=c.eps6[:, 0:1]), reads=["ps2", "c_eps"], writes=[FK[9]])
            for (a0_, n_) in DC:
                cs = slice(a0_, a0_ + n_)
                ew("dve", lambda e, cs=cs: e.reciprocal(out=F[9][:, cs], in_=F[9][:, cs]), [FK[9]], [FK[9]])
                ew("dve", lambda e, cs=cs, src=src, scl=scl: e.scalar_tensor_tensor(
                    out=F[src][:, cs], in0=F[src][:, cs], scalar=scl, in1=F[9][:, cs], op0=ALU.mult, op1=ALU.mult),
                    [FK[src], FK[9]], [FK[src]])
        ra, rb = d * 4 + h, 8 + d * 4 + h
        for bi, (t0, tw, col0) in enumerate(BLOCKS):
            cs = slice(col0, col0 + tw)
            kb.op("pe", lambda e, cs=cs, tw=tw: e.matmul(ps[2][:, 0:tw], lhsT=sel[0:16, ra, :], rhs=F[10][0:16, cs],
                                                       start=True, stop=True), reads=["sel", FK[10]], writes=["ps2"])
            kb.op("pe", lambda e, cs=cs, tw=tw: e.matmul(ps[3][:, 0:tw], lhsT=sel[0:16, rb, :], rhs=F[4][0:16, cs],
                                                       start=True, stop=True), reads=["sel", FK[4]], writes=["ps3"])
            kb.op("act", lambda e, cs=cs, tw=tw: e.activation(out=F[6][:, cs], in_=ps[2][:, 0:tw], func=AF.Exp),
                  reads=["ps2"], writes=[FK[6]])
            kb.op("dve", lambda e, cs=cs, tw=tw: e.tensor_copy(out=F[0][:, cs], in_=ps[2][:, 0:tw]),
                  reads=["ps2"], writes=[FK[0]])
            kb.op("act", lambda e, cs=cs, tw=tw: e.copy(out=F[9][:, cs], in_=ps[3][:, 0:tw]),
                  reads=["ps3"], writes=[FK[9]])
        GB5 = F[5][:, :].bitcast(BF16)
        for (a0_, n_) in DC:
            cs = slice(a0_, a0_ + n_)
            ew("dve", lambda e, cs=cs: e.tensor_tensor(out=F[9][:, cs], in0=F[9][:, cs], in1=F[2][:, cs], op=ALU.mult),
               [FK[9], FK[2]], [FK[9]])
            ew("pool", lambda e, cs=cs: e.tensor_tensor(out=GB5[:, cs], in0=F[2][:, cs], in1=F[6][:, cs], op=ALU.mult),
               [FK[2], FK[6]], [FK[5]])
            ew("dve", lambda e, cs=cs: e.tensor_tensor(out=GB5[:, TP + cs.start:TP + cs.stop], in0=F[1][:, cs],
                                                       in1=F[6][:, cs], op=ALU.mult),
               [FK[1], FK[6]], [FK[5]])
        kb.op("pool", lambda e: e.memset(T["nR"][:], 0.0), reads=[FK[0], FK[1], FK[2], FK[3], FK[5], FK[6], FK[9]],
              writes=[hk_, "nR"])
        dplr_scan(kb, nc, c, T, 128, F[1], F[2], F[9], F[9], F[3], F[6], None, store_cb_factory(512 + h * 128, 128), hk_,
                  Gb=F[0], rA=F[1], kkA=F[2], rC=GB5[:, TP:2 * TP], kkC=GB5[:, 0:TP])
        kb.op("pool", lambda e: e.memset(T["nR"][:], 0.0), reads=[hk_],
              writes=[FK[0], FK[1], FK[2], FK[3], FK[5], FK[6], FK[9], "nR"])
    if d == 0:
        n = 0
        for h in range(4):
            kb.dma("pool", wsl[:, :, 0:128], wv[:, :, 1920 + h * 512 + 384:1920 + h * 512 + 512], writes=["wsl"])
            for i in range(18):
                pb = ps[n % 2]
                for kc in range(8):
                    kb.op("pe", lambda e, kc=kc, i=i, pb=pb: e.matmul(pb[:, 0:128], lhsT=hT[:, kc, i * 128:(i + 1) * 128],
                                                                   rhs=wsl[:, kc, 0:128], start=(kc == 0), stop=(kc == 7)),
                          reads=["wsl", "hT"], writes=[f"ps{n % 2}"])
                zt = T["zt"][n % 2]
                kb.op("act", lambda e, pb=pb, zt=zt: e.activation(out=zt[:], in_=pb[:, 0:128], func=AF.Silu),
                      reads=[f"ps{n % 2}"], writes=[f"zt{n % 2}"])
                kb.dma("sp", Zs[i * 128:(i + 1) * 128, h * 128:(h + 1) * 128], zt[:], reads=[f"zt{n % 2}"],
                       writes=[("zs", i, h)])
                n += 1


def mixer_output(kb, nc, g, c, mods, T, F, FK, x_lat_in, x_ctx_in, x_lat_out, x_ctx_out, Ys, Zs, J32, xb, Gbc, hb, ps,
                 par128):
    ew = lambda eng, fn, r, w: kb.op(eng, fn, reads=r, writes=w)
    kb.barrier()
    from contextlib import ExitStack
    with ExitStack() as es:
        def sb(name, shape, dt):
            return es.enter_context(nc.sbuf_tensor(f"mo_{name}", shape, dt))
        wo = sb("wo", [128, 8, D], BF16)
        g2 = sb("g2", [128, 512], F32)
        bcp = sb("bcp", [128, 3, 512], F32)
        yfs = [sb(f"yf{j}", [128, 1536], F32) for j in range(2)]
        ybs = [sb(f"yb{j}", [128, 1536], F32) for j in range(2)]
        zts = [sb(f"zt{j}", [128, 512], F32) for j in range(2)]
        os_ = [sb(f"o{j}", [128, D], F32) for j in range(2)]
        t1s = [sb(f"t1{j}", [128, 512], F32) for j in range(2)]
        s8s = [sb(f"s8{j}", [128, 4, 8], F32) for j in range(2)]
        oTs = [sb(f"oT{j}", [128, 8, 128], BF16) for j in range(2)]
        hbs = [sb(f"hb{j}", [128, D], BF16) for j in range(2)]
        kb_real = kb

        class Defer:
            SHARED = ("ps", "J32", "bcp", "g2", "sgd", "Gbc", "c_", "wo", "xb", "F1")

            def __init__(self, b):
                self.q, self.b = [], b

            def kx(self, k):
                return k if (not isinstance(k, str) or k.startswith(self.SHARED)) else f"{k}#{self.b}"

            def op(self, e, fn, reads=(), writes=()):
                ent = ("op", (e, fn), dict(reads=[self.kx(k) for k in reads], writes=[self.kx(k) for k in writes]))
                pk = [k for k in writes if isinstance(k, str) and k.startswith("ps")]
                if e == "pe" and pk and self.q and isinstance(self.q[-1], list) and self.q[-1][0] == pk[0]:
                    self.q[-1].append(ent)
                elif e == "pe" and pk:
                    self.q.append([pk[0], ent])
                else:
                    self.q.append(ent)

            def dma(self, q, out, in_, reads=(), writes=(), **kw):
                self.q.append(("dma", (q, out, in_), dict(reads=[self.kx(k) for k in reads],
                                                           writes=[self.kx(k) for k in writes], **kw)))
        Gbc = sb("Gbc", [128, D], F32)
        xb = [sb("x0", [128, D], F32), sb("x1", [128, D], F32)]
        for kc in range(8):
            kb.dma("pool", wo[:, kc, :], g["e_w_out"][0].rearrange("(kc p) n -> p kc n", p=128)[:, kc, :], writes=["wo"])
        kb.dma("sp", g2[:], g["a_g2"][0], writes=["g2"])
        kb.dma("sp", bcp[:].rearrange("p a n -> p (a n)"), g["mx_bc"].partition_broadcast(128), writes=["bcp"])
        for stream in (1, 0):
            kb.barrier()
            make_bc(kb, nc, c, lambda kc: mods[0][:, 2 * 8 + kc, stream:stream + 1], Gbc, ps[0], "Gbc", ["mod0"])
            nt = 2 if stream == 1 else 16
            src = x_ctx_in if stream == 1 else x_lat_in
            dst = x_ctx_out if stream == 1 else x_lat_out
            base = 0 if stream == 1 else 256
            colbase = CTX0 if stream == 1 else LAT0
            def tile_body(kb, i):
                ew = lambda eng, fn, r, w: kb.op(eng, fn, reads=r, writes=w)
                b = i % 2
                yf, yb, zt, o, t1, s8, oT, hb = yfs[b], ybs[b], zts[b], os_[b], t1s[b], s8s[b], oTs[b], hbs[b]
                pb = 4 * b
                r0 = base + i * 128
                rb0 = base + (nt - 1 - i) * 128
                kb.dma("sp", xb[b][:], src[i * 128:(i + 1) * 128, :], writes=[f"xb{b}"])
                kb.dma("sp", yf[:], Ys[0, r0:r0 + 128, :], reads=[("ysall",)], writes=["yf"])
                kb.dma("act", yb[:], Ys[1, rb0:rb0 + 128, :], reads=[("ysall",)], writes=["yb"])
                kb.dma("sp", zt[:], Zs[r0:r0 + 128, :], reads=[("ysall",)], writes=["zt"])
                for q in range(3):
                    kb.op("pe", lambda e, q=q: e.matmul(ps[pb + 1][:, :], lhsT=J32[:, :], rhs=yb[:, q * 512:(q + 1) * 512],
                                                      start=True, stop=True), reads=["J32", "yb"], writes=[f"ps{pb + 1}"])
                    ew("dve", lambda e, q=q: e.tensor_tensor(out=yf[:, q * 512:(q + 1) * 512], in0=yf[:, q * 512:(q + 1) * 512],
                                                             in1=ps[pb + 1][:, :], op=ALU.add), [f"ps{pb + 1}", "yf"], ["yf"])
                y3 = yf[:, 0:512].rearrange("p (h j) -> p h j", h=8)
                ew("dve", lambda e: e.reduce_sum(out=s8[:, 0, :], in_=y3, axis=AX.X), ["yf"], ["s8"])
                ew("dve", lambda e: e.tensor_scalar(out=s8[:, 0, :], in0=s8[:, 0, :], scalar1=-1.0 / 64, scalar2=None,
                                                    op0=ALU.mult), ["s8"], ["s8"])
                ew("dve", lambda e: e.tensor_tensor(out=y3, in0=y3, in1=s8[:, 0, :].unsqueeze(2).broadcast_to([128, 8, 64]),
                                                    op=ALU.add), ["yf", "s8"], ["yf"])
                ew("pool", lambda e: e.tensor_tensor(out=t1[:], in0=yf[:, 0:512], in1=yf[:, 0:512], op=ALU.mult), ["yf"], ["t1"])
                ew("dve", lambda e: e.reduce_sum(out=s8[:, 1, :], in_=t1[:].rearrange("p (h j) -> p h j", h=8), axis=AX.X),
                   ["t1"], ["s8"])
                ew("dve", lambda e: e.tensor_scalar(out=s8[:, 1, :], in0=s8[:, 1, :], scalar1=1.0 / 64, scalar2=64e-5,
                                                    op0=ALU.mult, op1=ALU.add), ["s8"], ["s8"])
                ew("act", lambda e: e.activation(out=s8[:, 1, :], in_=s8[:, 1, :], func=AF.Sqrt), ["s8"], ["s8"])
                ew("dve", lambda e: e.reciprocal(out=s8[:, 1, :], in_=s8[:, 1, :]), ["s8"], ["s8"])
                ew("dve", lambda e: e.tensor_tensor(out=y3, in0=y3, in1=s8[:, 1, :].unsqueeze(2).broadcast_to([128, 8, 64]),
                                                    op=ALU.mult), ["yf", "s8"], ["yf"])
                ew("dve", lambda e: e.tensor_tensor(out=yf[:, 0:512], in0=yf[:, 0:512], in1=bcp[:, 0, :], op=ALU.mult),
                   ["yf", "bcp"], ["yf"])
                ew("pool", lambda e: e.tensor_tensor(out=yf[:, 0:512], in0=yf[:, 0:512], in1=bcp[:, 1, :], op=ALU.add),
                   ["yf", "bcp"], ["yf"])
                ew("pool", lambda e: e.tensor_tensor(out=yf[:, 0:512], in0=yf[:, 0:512], in1=yf[:, 1024:1536], op=ALU.add),
                   ["yf"], ["yf"])
                cg = colbase + i * 128
                kb.op("pe", lambda e, cg=cg: e.matmul(ps[pb][:, :], lhsT=F[11][:, cg:cg + 128], rhs=g2[:, :], start=True, stop=True),
                      reads=[FK[11], "g2"], writes=[f"ps{pb}"])
                ew("dve", lambda e: e.tensor_tensor(out=o[:, 0:512], in0=yf[:, 0:512], in1=ps[pb][:, :], op=ALU.mult),
                   ["yf", f"ps{pb}"], ["o"])
                ew("pool", lambda e: e.tensor_tensor(out=t1[:], in0=yf[:, 512:1024], in1=yf[:, 512:1024], op=ALU.mult),
                   ["yf"], ["t1"])
                ew("dve", lambda e: e.reduce_sum(out=s8[:, 2, 0:4], in_=t1[:].rearrange("p (h j) -> p h j", h=4), axis=AX.X),
                   ["t1"], ["s8"])
                ew("dve", lambda e: e.tensor_scalar(out=s8[:, 2, 0:4], in0=s8[:, 2, 0:4], scalar1=1.0 / 128, scalar2=EPS,
                                                    op0=ALU.mult, op1=ALU.add), ["s8"], ["s8"])
                ew("act", lambda e: e.activation(out=s8[:, 2, 0:4], in_=s8[:, 2, 0:4], func=AF.Sqrt), ["s8"], ["s8"])
                ew("dve", lambda e: e.reciprocal(out=s8[:, 2, 0:4], in_=s8[:, 2, 0:4]), ["s8"], ["s8"])
                ew("dve", lambda e: e.tensor_tensor(
                    out=t1[:].rearrange("p (h j) -> p h j", h=4), in0=yf[:, 512:1024].rearrange("p (h j) -> p h j", h=4),
                    in1=s8[:, 2, 0:4].unsqueeze(2).broadcast_to([128, 4, 128]), op=ALU.mult), ["yf", "s8"], ["t1"])
                ew("pool", lambda e: e.tensor_tensor(out=t1[:], in0=t1[:], in1=bcp[:, 2, :], op=ALU.mult), ["t1", "bcp"], ["t1"])
                ew("dve", lambda e: e.tensor_tensor(out=o[:, 512:1024], in0=t1[:], in1=zt[:], op=ALU.mult), ["t1", "zt"], ["o"])
                ew("act", lambda e: e.copy(out=hb[:], in_=o[:]), ["o"], ["hb"])
                pst = ps[pb][:].bitcast(BF16)
                for kc in range(8):
                    kb.op("pe", lambda e, kc=kc, pst=pst: e.transpose(out=pst[:, kc * 128:(kc + 1) * 128],
                                                                    in_=hb[:, kc * 128:(kc + 1) * 128], identity=c.identb[:]),
                          reads=["hb", "c_identb"], writes=[f"ps{pb}"])
                ew("act", lambda e, pst=pst: e.copy(out=oT[:], in_=pst[:].rearrange("p (k t) -> p k t", k=8)), [f"ps{pb}"], ["oT"])
                for half in range(2):
                    pp = ps[pb + 2 + half]
                    for kc in range(8):
                        kb.op("pe", lambda e, kc=kc, half=half, pp=pp: e.matmul(
                            pp[:], lhsT=oT[:, kc, :], rhs=wo[:, kc, half * 512:(half + 1) * 512], start=(kc == 0), stop=(kc == 7)),
                            reads=["oT", "wo"], writes=[f"ps{pb + 2 + half}"])
                    sl = slice(half * 512, (half + 1) * 512)
                    ew("dve", lambda e, pp=pp, sl=sl: e.tensor_tensor(out=t1[:], in0=pp[:], in1=Gbc[:, sl], op=ALU.mult),
                       [f"ps{pb + 2 + half}", "Gbc"], ["t1"])
                    ew("pool", lambda e, sl=sl, b=b: e.tensor_tensor(out=xb[b][:, sl], in0=xb[b][:, sl], in1=t1[:], op=ALU.add),
                       ["t1", f"xb{b}"], [f"xb{b}"])
                kb.dma("sp", dst[i * 128:(i + 1) * 128, :], xb[b][:], reads=[f"xb{b}"], writes=[("dram", dst.tensor.name, i)])

            for i0 in range(0, nt, 2):
                qs = []
                for i in range(i0, min(nt, i0 + 2)):
                    dfr = Defer(i % 2)
                    tile_body(dfr, i)
                    qs.append(dfr.q)
                for k in range(max(len(q_) for q_ in qs)):
                    for q_ in qs:
                        if k < len(q_):
                            ents = q_[k][1:] if isinstance(q_[k], list) else [q_[k]]
                            for kind, a_, kw_ in ents:
                                (kb_real.op if kind == "op" else kb_real.dma)(*a_, **kw_)
        kb.barrier()


_CACHE = {}


def kernel(**inputs):
    inp = {k: np.asarray(v) for k, v in inputs.items()}
    if "nc" not in _CACHE:
        _CACHE["nc"] = build(stages=("all",))[0]
    nc = _CACHE["nc"]
    in_maps = [host_inputs(inp, b) for b in range(8)]
    res = run_bass_kernel_spmd(nc, in_maps, core_ids=list(range(8)))
    return np.stack([np.asarray(r["out"], dtype=np.float32) for r in res.results], axis=0)
```

```python
import numpy as np
import concourse.bass as bass
import concourse.mybir as mybir
from concourse.bass_utils import run_bass_kernel_spmd

F32 = mybir.dt.float32
BF16 = mybir.dt.bfloat16
I32 = mybir.dt.int32
U32 = mybir.dt.uint32
ALU = mybir.AluOpType
AF = mybir.ActivationFunctionType
AX = mybir.AxisListType

SEM_ROTATE = 20000
N_DMA_SEMS = 28
N_HW_SEMS = 18


class KB:
    def __init__(self, nc, same_engine_sync=True):
        self.nc = nc
        self.engs = {"pe": nc.tensor, "act": nc.scalar, "dve": nc.vector, "pool": nc.gpsimd, "sp": nc.sync}
        self.same_engine_sync = same_engine_sync
        self.esem = {}
        self.ecnt = {}
        self.sem_id = 0
        for e in ("pe", "act", "dve", "pool"):
            self._new_esem(e)
        self.dsems = [self._alloc_sem(f"dma{i}") for i in range(N_DMA_SEMS)]
        self.dcnt = [0] * N_DMA_SEMS
        self.dnext = 0
        self.dnext_sw = 0
        self.known = {e: {} for e in self.engs}
        self.state = {}
        self.n_ins = 0
        self._uid = 0
        self.out_tokens = []

    def _alloc_sem(self, name):
        self.sem_id += 1
        return self.nc.alloc_semaphore(f"{name}_{self.sem_id}")

    def _new_esem(self, e):
        self.esem[e] = self._alloc_sem(f"s_{e}")
        self.ecnt[e] = 0

    def uid(self, p="t"):
        self._uid += 1
        return f"{p}{self._uid}"

    def _deps(self, reads, writes):
        deps = []
        for r in reads:
            st = self.state.get(r)
            if st and st[0] is not None:
                deps.append(st[0])
        for w in writes:
            st = self.state.get(w)
            if st:
                if st[0] is not None:
                    deps.append(st[0])
                deps.extend(st[1].values())
        return deps

    def _wait(self, e, deps):
        eng = self.engs[e]
        kn = self.known[e]
        best = {}
        for (sem, val, src) in deps:
            if src == e and not (self.same_engine_sync and e != "pe"):
                continue
            key = id(sem)
            if kn.get(key, 0) >= val:
                continue
            if key not in best or best[key][1] < val:
                best[key] = (sem, val)
        for key, (sem, val) in best.items():
            eng.wait_ge(sem, val)
            kn[key] = val
            self.n_ins += 1

    def _commit(self, token, reads, writes):
        for w in writes:
            self.state[w] = [token, {}]
        for r in reads:
            st = self.state.get(r)
            if st is None:
                st = [None, {}]
                self.state[r] = st
            st[1][id(token[0])] = token

    def op(self, e, fn, reads=(), writes=()):
        reads = list(reads)
        writes = list(writes)
        writes += [r for r in reads if isinstance(r, str) and r.startswith("ps")]
        self._wait(e, self._deps(reads, writes))
        if self.ecnt[e] >= SEM_ROTATE:
            self._new_esem(e)
        ins = fn(self.engs[e])
        self.ecnt[e] += 1
        ins.then_inc(self.esem[e], 1)
        token = (self.esem[e], self.ecnt[e], e)
        self._commit(token, reads, writes)
        self.n_ins += 1
        return token

    def dma(self, q, out, in_, reads=(), writes=(), **kw):
        reads = list(reads)
        writes = list(writes)
        if q == "pool":
            i = N_HW_SEMS + self.dnext_sw
            self.dnext_sw = (self.dnext_sw + 1) % (N_DMA_SEMS - N_HW_SEMS)
        else:
            i = self.dnext
            self.dnext = (self.dnext + 1) % N_HW_SEMS
        deps = self._deps(reads, writes)
        if self.dcnt[i] > 0:
            deps.append((self.dsems[i], self.dcnt[i], "dma"))
        self._wait(q, deps)
        ins = self.engs[q].dma_start(out=out, in_=in_, **kw)
        self.dcnt[i] += 16
        ins.then_inc(self.dsems[i], 16)
        token = (self.dsems[i], self.dcnt[i], "dma")
        self._commit(token, reads, writes)
        self.n_ins += 1
        return token

    def finish(self, out_keys):
        deps = []
        for k in out_keys:
            st = self.state.get(k)
            if st and st[0] is not None:
                deps.append(st[0])
        self._wait("sp", deps)
        deps = [(self.dsems[i], self.dcnt[i], "dma") for i in range(N_DMA_SEMS) if self.dcnt[i] > 0]
        self._wait("sp", deps)

    def barrier(self):
        deps = [(self.esem[e], self.ecnt[e], "x") for e in self.esem if self.ecnt[e] > 0]
        deps += [(self.dsems[i], self.dcnt[i], "dma") for i in range(N_DMA_SEMS) if self.dcnt[i] > 0]
        for e in self.engs:
            self._wait(e, deps)
        self.state = {}


D = 1024
KC = 8
EPS = 1e-6
TP = 2310
CTX0, LAT0 = 2, 260
CHUNK = 128
CHUNK_COLS = [CTX0 + CHUNK * j for j in range(256 // CHUNK)] + [LAT0 + CHUNK * j for j in range(2048 // CHUNK)]
BLOCKS = [(0, 256, CTX0)] + [(256 + 512 * j, 512, LAT0 + 512 * j) for j in range(4)]
DECAY_K = float(np.exp(-0.5))


class Ctx:
    pass


def load_consts(kb, nc, es, g):
    c = Ctx()
    c.ident = es.enter_context(nc.sbuf_tensor("c_ident", [128, 128], F32))
    c.identb = es.enter_context(nc.sbuf_tensor("c_identb", [128, 128], BF16))
    c.ones = es.enter_context(nc.sbuf_tensor("c_ones", [128, 128], F32))
    c.iota = es.enter_context(nc.sbuf_tensor("c_iota", [128, 256], F32))
    kb.dma("sp", c.ident[:], g["k_ident"][:, :], writes=["c_ident"])
    kb.dma("sp", c.iota[:], g["k_iota"][:, :], writes=["c_iota"])
    kb.op("dve", lambda e: e.memset(c.ones[:], 1.0), writes=["c_ones"])
    c.eps6 = es.enter_context(nc.sbuf_tensor("c_eps6", [128, 1], F32))
    c.one1 = es.enter_context(nc.sbuf_tensor("c_one1", [128, 1], F32))
    kb.op("dve", lambda e: e.memset(c.eps6[:], 1e-6), writes=["c_eps"])
    kb.op("dve", lambda e: e.memset(c.one1[:], 1.0), writes=["c_eps"])
    c.onesb = es.enter_context(nc.sbuf_tensor("c_onesb", [128, 8], BF16))
    kb.op("dve", lambda e: e.memset(c.onesb[:], 1.0), writes=["c_ones"])
    kb.op("dve", lambda e: e.tensor_copy(out=c.identb[:], in_=c.ident[:]), reads=["c_ident"], writes=["c_identb"])
    return c


def prologue(kb, nc, es, g, c):
    mods = []
    for l in range(2):
        mods.append(es.enter_context(nc.sbuf_tensor(f"mod{l}", [128, 48, 2], F32)))
    with nc.sbuf_tensor("pl_sc", [128, 2, 8], F32) as sc, \
            nc.sbuf_tensor("pl_w0", [128, 8, 512], F32) as w0, \
            nc.sbuf_tensor("pl_w1", [128, 8, 512], F32) as w1, \
            nc.sbuf_tensor("pl_b", [128, 2, 48], F32) as adab, \
            nc.sbuf_tensor("pl_n", [128, 2, 2, 8], F32) as nrm, \
            nc.psum_tensor("pl_ps", [128, 512], F32) as ps:
        wb = [w0, w1]
        kb.dma("sp", sc[:, 0, :], g["cT"][:, :], writes=["sc"])
        kb.dma("sp", sc[:, 1, :], g["ccT"][:, :], writes=["sc"])
        kb.dma("sp", adab[:], g["ada_bT"][:, :, :], writes=["adab"])
        kb.dma("sp", nrm[:], g["normT"][:, :, :, :], writes=["nrm"])
        kb.op("act", lambda e: e.activation(out=sc[:], in_=sc[:], func=AF.Silu), reads=["sc"], writes=["sc"])
        blk = 0
        for l in range(2):
            wv = g["ada_w"][l].rearrange("(kc p) n -> p kc n", p=128)
            for nb in range(12):
                wt = wb[blk % 2]
                wk = f"plw{blk % 2}"
                kb.dma("sp" if blk % 2 == 0 else "act", wt[:], wv[:, :, nb * 512:(nb + 1) * 512], writes=[wk])
                for j in range(4):
                    for kc in range(8):
                        kb.op("pe", lambda e, kc=kc, j=j, wt=wt: e.matmul(
                            ps[:, (j * 2):(j * 2 + 2)], lhsT=wt[:, kc, j * 128:(j + 1) * 128], rhs=sc[:, :, kc],
                            start=(kc == 0), stop=(kc == 7)), reads=[wk, "sc"], writes=["psPL"])
                kb.op("dve", lambda e, l=l, nb=nb: e.tensor_tensor(
                    out=mods[l][:, nb * 4:(nb + 1) * 4, :],
                    in0=ps[:, 0:8].rearrange("p (j s) -> p j s", s=2),
                    in1=adab[:, l, nb * 4:(nb + 1) * 4].unsqueeze(2).broadcast_to([128, 4, 2]),
                    op=ALU.add), reads=["psPL", "adab"], writes=[f"mod{l}"])
                blk += 1
            for (m, which) in ((1, 0), (4, 1)):
                for s in range(2):
                    kb.op("dve", lambda e, l=l, m=m, which=which, s=s: e.scalar_tensor_tensor(
                        out=mods[l][:, m * 8:(m + 1) * 8, s], in0=mods[l][:, m * 8:(m + 1) * 8, s], scalar=1.0,
                        in1=nrm[:, l, which, :], op0=ALU.add, op1=ALU.mult),
                        reads=[f"mod{l}", "nrm"], writes=[f"mod{l}"])
    kb.barrier()
    return mods


def make_bc(kb, nc, c, col_ap_fn, out_tile, ps, key, src_keys):
    with nc.sbuf_tensor(kb.uid("bcd"), [128, 128], F32) as dg:
        dk = kb.uid("dg")
        for half in range(2):
            for q in range(4):
                kc = half * 4 + q
                kb.op("dve", lambda e, kc=kc: e.tensor_scalar(
                    out=dg[:], in0=c.ident[:], scalar1=col_ap_fn(kc), scalar2=None, op0=ALU.mult),
                    reads=["c_ident"] + src_keys, writes=[dk])
                kb.op("pe", lambda e, q=q: e.matmul(ps[:, q * 128:(q + 1) * 128], lhsT=c.ones[:], rhs=dg[:],
                                                   start=True, stop=True),
                      reads=[dk, "c_ones"], writes=["psBC" + key])
            kb.op("act", lambda e, half=half: e.copy(out=out_tile[:, half * 512:(half + 1) * 512], in_=ps[:]),
                  reads=["psBC" + key], writes=[key])
        kb.barrier()


def norm_tile_g(kb, nc, xt, xk, st, G, S, hout, hk, eps=EPS):
    sk = kb.uid("st")
    kb.op("act", lambda e: e.activation(out=hout, in_=xt, func=AF.Square, accum_out=st[:, 0:1]),
          reads=[xk], writes=[hk, sk])
    yield
    kb.op("dve", lambda e: e.tensor_scalar(out=st[:, 1:2], in0=st[:, 0:1], scalar1=1.0 / D, scalar2=eps,
                                           op0=ALU.mult, op1=ALU.add), reads=[sk], writes=[sk])
    yield
    kb.op("act", lambda e: e.activation(out=st[:, 2:3], in_=st[:, 1:2], func=AF.Sqrt), reads=[sk], writes=[sk])
    yield
    kb.op("dve", lambda e: e.reciprocal(out=st[:, 3:4], in_=st[:, 2:3]), reads=[sk], writes=[sk])
    yield
    if G is None:
        kb.op("dve", lambda e: e.tensor_scalar(out=hout, in0=xt, scalar1=st[:, 3:4], scalar2=None, op0=ALU.mult),
              reads=[xk, sk], writes=[hk])
        return
    kb.op("dve", lambda e: e.scalar_tensor_tensor(out=hout, in0=xt, scalar=st[:, 3:4], in1=G[:],
                                                  op0=ALU.mult, op1=ALU.mult),
          reads=[xk, sk, "Gbc"], writes=[hk])
    yield
    if S is not None:
        kb.op("pool", lambda e: e.tensor_tensor(out=hout, in0=hout, in1=S[:], op=ALU.add),
              reads=[hk, "Sbc"], writes=[hk])


def norm_tile(*a, **k):
    for _ in norm_tile_g(*a, **k):
        pass


def moe_stage(kb, nc, g, c, mods, layer, stream, x_in, x_out, T, final_norm=False, comp=None):
    NT = T // 128
    cap = 2 * T // 16
    CW = cap
    CT = (cap + 127) // 128
    cs = min(cap, 128)
    mod = mods[layer]
    xin_v = x_in.rearrange("(n p) d -> n p d", p=128)
    xout_v = x_out.rearrange("(n p) d -> n p d", p=128)
    sx = f"L{layer}s{stream}"
    from contextlib import ExitStack
    with ExitStack() as es:
        def sb(name, shape, dt):
            return es.enter_context(nc.sbuf_tensor(f"moe_{name}_{sx}", shape, dt))
        Gbc = sb("G", [128, D], F32)
        Sbc = sb("S", [128, D], F32)
        gate2 = Sbc
        hbf = sb("hbf", [128, NT, D], BF16)
        xb = [sb("x0", [128, D], F32), sb("x1", [128, D], F32)]
        h32 = [sb("h0", [128, D], F32), sb("h1", [128, D], F32)]
        hT = [sb("hT0", [128, 8, 128], F32), sb("hT1", [128, 8, 128], F32)]
        stt = [sb("st0", [128, 4], F32), sb("st1", [128, 4], F32)]
        rt = sb("rt", [128, 8, 16], F32)
        aff = sb("aff", [128, NT, 16], F32)
        sm = sb("sm", [128, 4], F32)
        ex = sb("ex", [128, 16], F32)
        affT = sb("affT", [16, T], F32)
        work = sb("work", [16, T], F32)
        mx8 = sb("mx8", [16, 8], F32)
        maskT = sb("maskT", [16, T], F32)
        onesT = work
        slotT = sb("slotT", [16, T], F32)
        gateT = affT
        slot = sb("slot", [128, NT, 16], F32)
        gate = sb("gate", [128, NT, 16], F32)
        selT = [sb("selT0", [128, CW], BF16), sb("selT1", [128, CW], BF16)]
        xeT = sb("xeT", [128, 8, CW], BF16)
        big = sb("big", [128, 4 * 8 * D], BF16)
        wviews = [big[:, j * 8 * D:(j + 1) * 8 * D].rearrange("p (k n) -> p k n", n=D) for j in range(4)]
        w1b = [wviews[0], wviews[1]]
        w3b = [wviews[2]]
        w2b = [wviews[3]]
        yest = [sb("yest0", [128, CT, D], BF16), sb("yest1", [128, CT, D], BF16)]
        sil = [sb("sil0", [128, CW], F32), sb("sil1", [128, CW], F32)]
        hidT = sb("hidT", [128, 8, CW], BF16)
        yeall = big[:, 0:16 * CT * D].rearrange("p (e c n) -> p e c n", e=16, c=CT)
        selGa = [sb("selGa0", [128, 4, CW], BF16), sb("selGa1", [128, 4, CW], BF16)]
        selGca = [sb("selGca0", [128, 4 * CT, 128], BF16), sb("selGca1", [128, 4 * CT, 128], BF16)]
        tmpo = [sb("tmpo0", [128, 512], F32), sb("tmpo1", [128, 512], F32)]
        ps = [es.enter_context(nc.psum_tensor(f"moe_ps{i}_{sx}", [128, 512], F32)) for i in range(8)]

        class SC:
            pass
        S0 = SC()
        S0.T, S0.NT, S0.cap, S0.CW, S0.CT, S0.cs, S0.stream = T, NT, cap, CW, CT, cs, stream
        S0.xin_v, S0.xout_v, S0.hbf, S0.aff, S0.slot, S0.gate, S0.ye_scr = xin_v, xout_v, hbf, aff, slot, gate, g["ye_scr"]
        streams = [S0]
        if comp is not None:
            S1 = SC()
            S1.T, S1.NT, S1.cap, S1.CW, S1.CT, S1.cs, S1.stream = 256, 2, 32, 32, 1, 32, 1
            S1.xin_v = comp[0].rearrange("(n p) d -> n p d", p=128)
            S1.xout_v = comp[1].rearrange("(n p) d -> n p d", p=128)
            S1.hbf = sb("hbfc", [128, 2, D], BF16)
            S1.aff = sb("affc", [128, 2, 16], F32)
            S1.slot = sb("slotc", [128, 2, 16], F32)
            S1.gate = sb("gatec", [128, 2, 16], F32)
            S1.ye_scr = g["ye_scr_c"]
            selTc = [sb("selTc0", [128, 32], BF16), sb("selTc1", [128, 32], BF16)]
            xeTc = sb("xeTc", [128, 8, 32], BF16)
            silc = sb("silc", [128, 8, 32], F32)
            hidTc = sb("hidTc", [128, 8, 32], BF16)
            yestc = [sb("yestc0", [32, 1, D], BF16)] * 2
            streams = [S1, S0]
        affT_full, work_full, maskT_full, slotT_full = affT, work, maskT, slotT
        kb.dma("sp", rt[:], g["moe_router"][layer].rearrange("(kc p) e -> p kc e", p=128), writes=["rt"])
        for S in streams:
            T, NT, cap, CW, CT, cs, stream = S.T, S.NT, S.cap, S.CW, S.CT, S.cs, S.stream
            xin_v, xout_v, hbf, aff, slot, gate = S.xin_v, S.xout_v, S.hbf, S.aff, S.slot, S.gate
            affT, work, maskT, slotT = affT_full[:, 0:T], work_full[:, 0:T], maskT_full[:, 0:T], slotT_full[:, 0:T]
            onesT, gateT = work, affT
            kb.barrier()
            make_bc(kb, nc, c, lambda kc: mod[:, 4 * 8 + kc, stream:stream + 1], Gbc, ps[0], "Gbc", [f"mod{layer}"])
            make_bc(kb, nc, c, lambda kc: mod[:, 3 * 8 + kc, stream:stream + 1], Sbc, ps[0], "Sbc", [f"mod{layer}"])

            def stageA(i):
                b = i % 2
                kb.dma("sp", xb[b][:], xin_v[i], writes=[f"xb{b}"])
                yield
                yield from norm_tile_g(kb, nc, xb[b][:], f"xb{b}", stt[b], Gbc, Sbc, h32[b][:], f"h32{b}")
                yield
                kb.op("act", lambda e, i=i, b=b: e.copy(out=hbf[:, i, :], in_=h32[b][:]), reads=[f"h32{b}"],
                      writes=[f"hbf{stream}_{i}"])
                for half in range(2):
                    for q in range(4):
                        kc = half * 4 + q
                        kb.op("pe", lambda e, kc=kc, q=q, b=b, half=half: e.transpose(
                            out=ps[half][:, q * 128:(q + 1) * 128], in_=h32[b][:, kc * 128:(kc + 1) * 128],
                            identity=c.ident[:]), reads=[f"h32{b}", "c_ident"], writes=[f"ps{half}"])
                    yield
                    kb.op("dve" if half == 0 else "act", (lambda e, half=half, b=b: e.tensor_copy(
                        out=hT[b][:, half * 4:(half + 1) * 4, :], in_=ps[half][:].rearrange("p (q t) -> p q t", q=4)))
                        if half == 0 else (lambda e, half=half, b=b: e.copy(
                            out=hT[b][:, half * 4:(half + 1) * 4, :], in_=ps[half][:].rearrange("p (q t) -> p q t", q=4))),
                        reads=[f"ps{half}"], writes=[f"hT{b}"])
                    yield

            def stageB(i):
                b = i % 2
                for kc in range(8):
                    kb.op("pe", lambda e, kc=kc, b=b: e.matmul(ps[2][:, 0:16], lhsT=hT[b][:, kc, :], rhs=rt[:, kc, :],
                                                             start=(kc == 0), stop=(kc == 7)),
                          reads=[f"hT{b}", "rt"], writes=["ps2"])
                yield
                kb.op("dve", lambda e: e.reduce_max(out=sm[:, 0:1], in_=ps[2][:, 0:16], axis=AX.X),
                      reads=["ps2"], writes=["sm"])
                kb.op("dve", lambda e: e.tensor_scalar(out=sm[:, 1:2], in0=sm[:, 0:1], scalar1=-1.0, scalar2=None,
                                                       op0=ALU.mult), reads=["sm"], writes=["sm"])
                yield
                kb.op("act", lambda e: e.activation(out=ex[:], in_=ps[2][:, 0:16], func=AF.Exp, bias=sm[:, 1:2],
                                                    accum_out=sm[:, 2:3]), reads=["ps2", "sm"], writes=["ex", "sm"])
                yield
                kb.op("dve", lambda e: e.reciprocal(out=sm[:, 3:4], in_=sm[:, 2:3]), reads=["sm"], writes=["sm"])
                kb.op("dve", lambda e, i=i: e.tensor_scalar(out=aff[:, i, :], in0=ex[:], scalar1=sm[:, 3:4], scalar2=None,
                                                            op0=ALU.mult), reads=["ex", "sm"], writes=["aff"])
                yield
                kb.op("pe", lambda e, i=i: e.transpose(out=ps[3][0:16, 0:128], in_=aff[:, i, :], identity=c.ident[:]),
                      reads=["aff", "c_ident"], writes=["ps3"])
                yield
                kb.op("act", lambda e, i=i: e.copy(out=affT[:, i * 128:(i + 1) * 128], in_=ps[3][0:16, 0:128]),
                      reads=["ps3"], writes=["affT"])
                yield

            from itertools import zip_longest
            prevB = iter(())
            for i in range(NT):
                for _ in zip_longest(stageA(i), prevB):
                    pass
                prevB = stageB(i)
            for _ in prevB:
                pass

            import os
            PH = int(os.environ.get("MOE_PH", "9"))
            if PH < 2:
                kb.dma("sp", x_out[0:128, 0:NT * 16], aff[:].rearrange("p n e -> p (n e)"), reads=["aff"], writes=["o"])
                kb.barrier()
                return
            kb.op("dve", lambda e: e.tensor_copy(out=work[:], in_=affT[:]), reads=["affT"], writes=["work"])
            nr = cap // 8
            for r in range(nr):
                kb.op("dve", lambda e: e.max(out=mx8[:], in_=work[:]), reads=["work"], writes=["mx8"])
                if r < nr - 1:
                    kb.op("dve", lambda e: e.match_replace(out=work[:], in_to_replace=mx8[:], in_values=work[:],
                                                           imm_value=-1.0), reads=["work", "mx8"], writes=["work"])
            kb.op("dve", lambda e: e.tensor_scalar(out=maskT[:], in0=affT[:], scalar1=mx8[:, 7:8], scalar2=None,
                                                   op0=ALU.is_ge), reads=["affT", "mx8"], writes=["maskT"])
            kb.op("pool", lambda e: e.memset(onesT[:], 1.0), reads=[], writes=["work"])
            kb.op("dve", lambda e: e.tensor_tensor_scan(out=slotT[:], data0=onesT[:], data1=maskT[:], initial=0.0,
                                                        op0=ALU.mult, op1=ALU.add),
                  reads=["work", "maskT"], writes=["slotT"])
            kb.op("dve", lambda e: e.tensor_tensor(out=slotT[:], in0=slotT[:], in1=maskT[:], op=ALU.mult),
                  reads=["slotT", "maskT"], writes=["slotT"])
            kb.op("dve", lambda e: e.tensor_scalar(out=slotT[:], in0=slotT[:], scalar1=-1.0, scalar2=None, op0=ALU.add),
                  reads=["slotT"], writes=["slotT"])
            kb.op("pool", lambda e: e.tensor_tensor(out=gateT[:], in0=affT[:], in1=maskT[:], op=ALU.mult),
                  reads=["affT", "maskT"], writes=["affT"])
            for i in range(NT):
                kb.op("pe", lambda e, i=i: e.transpose(out=ps[0][:, i * 16:(i + 1) * 16],
                                                       in_=slotT[:, i * 128:(i + 1) * 128], identity=c.ident[0:16, 0:16]),
                      reads=["slotT", "c_ident"], writes=["ps0"])
                kb.op("pe", lambda e, i=i: e.transpose(out=ps[1][:, i * 16:(i + 1) * 16],
                                                       in_=gateT[:, i * 128:(i + 1) * 128], identity=c.ident[0:16, 0:16]),
                      reads=["affT", "c_ident"], writes=["ps1"])
            kb.op("dve", lambda e: e.tensor_copy(out=slot[:], in_=ps[0][:, 0:NT * 16].rearrange("p (n e) -> p n e", e=16)),
                  reads=["ps0"], writes=["slot"])
            kb.op("act", lambda e: e.copy(out=gate[:], in_=ps[1][:, 0:NT * 16].rearrange("p (n e) -> p n e", e=16)),
                  reads=["ps1"], writes=["gate"])

            if PH < 3:
                kb.dma("sp", x_out[0:128, 0:NT * 16], slot[:].rearrange("p n e -> p (n e)"), reads=["slot"], writes=["o"])
                kb.dma("sp", x_out[128:256, 0:NT * 16], gate[:].rearrange("p n e -> p (n e)"), reads=["gate"], writes=["o2"])
                kb.barrier()
                return

        stg = [xb[0], xb[1], h32[0], h32[1]]
        stgk = ["xb0", "xb1", "h320", "h321"]
        wcnt = [0]

        def load_w(wv, wt, wk):
            for hh in range(2):
                kb.dma("pool", wt[:, hh * 4:(hh + 1) * 4, :], wv[:, hh * 4:(hh + 1) * 4, :], writes=[wk])

        def wviews_of(e_):
            return (g["moe_w1"][layer, e_].rearrange("(kc p) n -> p kc n", p=128),
                    g["moe_w3"][layer, e_].rearrange("(kc p) n -> p kc n", p=128),
                    g["moe_w2"][layer, e_].rearrange("(kc p) n -> p kc n", p=128))
        nsel = 0
        SUB = int(os.environ.get("MOE_SUB", "9"))
        NEXP = int(os.environ.get("MOE_NEXP", "16"))
        wv1, wv3, wv2 = wviews_of(0)
        load_w(wv1, w1b[0], "w1_0")
        load_w(wv3, w3b[0], "w3")
        load_w(wv2, w2b[0], "w2")
        for ex_i in range(NEXP):
            w1t = w1b[ex_i % 2]
            w1k = f"w1_{ex_i % 2}"
            if ex_i + 1 < NEXP:
                nwv1, nwv3, nwv2 = wviews_of(ex_i + 1)
                load_w(nwv1, w1b[(ex_i + 1) % 2], f"w1_{(ex_i + 1) % 2}")
            for i in range(NT):
                b = nsel % 2
                nsel += 1
                kb.op("dve", lambda e, i=i, b=b, ex_i=ex_i: e.tensor_scalar(
                    out=selT[b][:], in0=c.iota[:, 0:CW], scalar1=slot[:, i, ex_i:ex_i + 1], scalar2=None,
                    op0=ALU.is_equal), reads=["c_iota", "slot"], writes=[f"selT{b}"])
                for kc in range(8):
                    bank = (kc * CW) // 512
                    off = (kc * CW) % 512
                    kb.op("pe", lambda e, i=i, b=b, kc=kc, bank=bank, off=off: e.matmul(
                        ps[bank][:, off:off + CW], lhsT=hbf[:, i, kc * 128:(kc + 1) * 128], rhs=selT[b][:],
                        start=(i == 0 and off == 0), stop=(i == NT - 1), skip_group_check=True), reads=[f"hbf{stream}_{i}", f"selT{b}"], writes=[f"ps{bank}"])
            nb = (8 * CW + 511) // 512
            per = 512 // CW if CW < 512 else 1
            for bank in range(nb):
                k0 = bank * per
                k1 = min(8, k0 + per)
                kb.op("act" if bank % 2 else "dve", (lambda e, bank=bank, k0=k0, k1=k1: e.copy(
                    out=xeT[:, k0:k1, :], in_=ps[bank][:, 0:(k1 - k0) * CW].rearrange("p (k c) -> p k c", c=CW)))
                    if bank % 2 else (lambda e, bank=bank, k0=k0, k1=k1: e.tensor_copy(
                        out=xeT[:, k0:k1, :], in_=ps[bank][:, 0:(k1 - k0) * CW].rearrange("p (k c) -> p k c", c=CW))),
                    reads=[f"ps{bank}"], writes=["xeT"])
            if SUB < 2:
                continue
            for fc in range(8):
                pb = ps[4 + fc % 2]
                pk = f"ps{4 + fc % 2}"
                for kc in range(8):
                    kb.op("pe", lambda e, fc=fc, kc=kc, pb=pb, w1t=w1t: e.matmul(
                        pb[:, 0:CW], lhsT=w1t[:, kc, fc * 128:(fc + 1) * 128], rhs=xeT[:, kc, :],
                        start=(kc == 0), stop=(kc == 7)), reads=[w1k, "xeT"], writes=[pk])
                for kc in range(8):
                    kb.op("pe", lambda e, fc=fc, kc=kc, pb=pb: e.matmul(
                        pb[:, 256:256 + CW], lhsT=w3b[0][:, kc, fc * 128:(fc + 1) * 128], rhs=xeT[:, kc, :],
                        start=(kc == 0), stop=(kc == 7)), reads=["w3", "xeT"], writes=[pk])
                sb_ = sil[fc % 2]
                DBG = int(os.environ.get("MOE_DBG", "9"))
                if DBG < 1:
                    continue
                kb.op("act", lambda e, pb=pb, sb_=sb_: e.activation(out=sb_[:], in_=pb[:, 0:CW], func=AF.Silu),
                      reads=[pk], writes=[f"sil{fc % 2}"])
                if DBG < 2:
                    continue
                kb.op("dve", lambda e, pb=pb, sb_=sb_, fc=fc: e.tensor_tensor(
                    out=hidT[:, fc, :], in0=sb_[:], in1=pb[:, 256:256 + CW], op=ALU.mult),
                    reads=[f"sil{fc % 2}", pk], writes=["hidT"])
            if comp is not None:
                for i in range(2):
                    sc_ = selTc[i]
                    kb.op("dve", lambda e, i=i, sc_=sc_, ex_i=ex_i: e.tensor_scalar(
                        out=sc_[:], in0=c.iota[:, 0:32], scalar1=S1.slot[:, i, ex_i:ex_i + 1], scalar2=None,
                        op0=ALU.is_equal), reads=["c_iota", "slotc"], writes=[f"selTc{i}"])
                    for kc in range(8):
                        kb.op("pe", lambda e, i=i, kc=kc, sc_=sc_: e.matmul(
                            ps[6][:, kc * 32:(kc + 1) * 32], lhsT=S1.hbf[:, i, kc * 128:(kc + 1) * 128], rhs=sc_[:],
                            start=(i == 0 and kc == 0), stop=(i == 1), skip_group_check=True),
                            reads=[f"hbf1_{i}", f"selTc{i}"], writes=["ps6"])
                kb.op("act", lambda e: e.copy(out=xeTc[:], in_=ps[6][:, 0:256].rearrange("p (k c) -> p k c", c=32)),
                      reads=["ps6"], writes=["xeTc"])
                for fc in range(8):
                    for kc in range(8):
                        kb.op("pe", lambda e, fc=fc, kc=kc: e.matmul(
                            ps[7][:, fc * 64:fc * 64 + 32], lhsT=w1t[:, kc, fc * 128:(fc + 1) * 128], rhs=xeTc[:, kc, :],
                            start=(fc == 0 and kc == 0), stop=(kc == 7), skip_group_check=True),
                            reads=[w1k, "xeTc"], writes=["ps7"])
                    for kc in range(8):
                        kb.op("pe", lambda e, fc=fc, kc=kc: e.matmul(
                            ps[7][:, fc * 64 + 32:fc * 64 + 64], lhsT=w3b[0][:, kc, fc * 128:(fc + 1) * 128],
                            rhs=xeTc[:, kc, :], start=False, stop=(kc == 7), skip_group_check=True),
                            reads=["w3", "xeTc"], writes=["ps7"])
                p7v = ps[7][:, :].rearrange("p (f ab c) -> p f ab c", f=8, ab=2)
                kb.op("act", lambda e: e.activation(out=silc[:], in_=p7v[:, :, 0, :], func=AF.Silu),
                      reads=["ps7"], writes=["silc"])
                kb.op("dve", lambda e: e.tensor_tensor(out=hidTc[:], in0=silc[:], in1=p7v[:, :, 1, :], op=ALU.mult),
                      reads=["silc", "ps7"], writes=["hidTc"])
            if ex_i + 1 < NEXP:
                load_w(nwv3, w3b[0], "w3")
            if SUB < 3:
                continue
            for ct in range(CT):
                for half in range(2):
                    pb = ps[6 + half]
                    pk = f"ps{6 + half}"
                    for fc in range(8):
                        kb.op("pe", lambda e, ct=ct, half=half, fc=fc, pb=pb: e.matmul(
                            pb[0:cs, :], lhsT=hidT[:, fc, ct * 128:ct * 128 + cs],
                            rhs=w2b[0][:, fc, half * 512:(half + 1) * 512], start=(fc == 0), stop=(fc == 7)),
                            reads=["hidT", "w2"], writes=[pk])
                    ys = yest[ex_i % 2]
                    kb.op("act" if half else "dve", (lambda e, ct=ct, half=half, pb=pb, ys=ys: e.copy(
                        out=ys[0:cs, ct, half * 512:(half + 1) * 512], in_=pb[0:cs, :]))
                        if half else (lambda e, ct=ct, half=half, pb=pb, ys=ys: e.tensor_copy(
                            out=ys[0:cs, ct, half * 512:(half + 1) * 512], in_=pb[0:cs, :])),
                        reads=[pk], writes=[f"yest{ex_i % 2}"])
            if comp is not None:
                ysc = yestc[ex_i % 2]
                for half in range(2):
                    for fc in range(8):
                        kb.op("pe", lambda e, half=half, fc=fc: e.matmul(
                            ps[6][0:32, :], lhsT=hidTc[:, fc, :], rhs=w2b[0][:, fc, half * 512:(half + 1) * 512],
                            start=(fc == 0), stop=(fc == 7)), reads=["hidTc", "w2"], writes=["ps6"])
                    kb.op("act", lambda e, half=half, ysc=ysc: e.copy(out=ysc[0:32, 0, half * 512:(half + 1) * 512],
                                                                     in_=ps[6][0:32, :]),
                          reads=["ps6"], writes=["yestc"])
                kb.dma("act", S1.ye_scr[ex_i, 0:32, 0:1, :], ysc[0:32, :, :], reads=["yestc"],
                       writes=[("yescrc", ex_i)])
            if ex_i + 1 < NEXP:
                load_w(nwv2, w2b[0], "w2")
            kb.dma("sp", g["ye_scr"][ex_i, 0:cs, 0:CT, :], yest[ex_i % 2][0:cs, :, :], reads=[f"yest{ex_i % 2}"],
                   writes=[("yescr", ex_i)])

        if PH < 4:
            kb.barrier()
            return
        for S in streams[::-1]:
            T, NT, cap, CW, CT, cs, stream = S.T, S.NT, S.cap, S.CW, S.CT, S.cs, S.stream
            xin_v, xout_v, hbf, aff, slot, gate = S.xin_v, S.xout_v, S.hbf, S.aff, S.slot, S.gate
            yeall = big[:, 0:16 * CT * D].rearrange("p (e c n) -> p e c n", e=16, c=CT)
            ye_scr_S = S.ye_scr
            kb.barrier()
            make_bc(kb, nc, c, lambda kc: mod[:, 5 * 8 + kc, stream:stream + 1], gate2, ps[0], "gate2", [f"mod{layer}"])
            if final_norm:
                make_bc(kb, nc, c, lambda kc: c.fnT[:, kc:kc + 1], Gbc, ps[0], "Gbc", ["c_fnT"])
            for ex_i in range(16):
                kb.dma(["sp", "act"][ex_i % 2], yeall[0:cs, ex_i, :, :], ye_scr_S[ex_i, 0:cs, 0:CT, :],
                       reads=[("yescr", ex_i), ("yescrc", ex_i)], writes=[f"ye{ex_i}"])
            nsg = 0
            EG = 4
            for i in range(NT):
                b = i % 2
                kb.dma("sp", xb[b][:], xin_v[i], writes=[f"xb{b}"])
                for g0 in range(0, 16, EG):
                    sg = nsg % 2
                    nsg += 1
                    sga = selGa[sg][:, :, 0:CW]
                    kb.op("dve", lambda e, i=i, g0=g0, sga=sga: e.tensor_tensor(
                        out=sga[:, :, :], in0=c.iota[:, 0:CW].unsqueeze(1).broadcast_to([128, EG, CW]),
                        in1=slot[:, i, g0:g0 + EG].unsqueeze(2).broadcast_to([128, EG, CW]), op=ALU.is_equal),
                        reads=["c_iota", "slot"], writes=[f"selGa{sg}"])
                    kb.op("pool", lambda e, i=i, g0=g0, sga=sga: e.tensor_tensor(
                        out=sga[:, :, :], in0=sga[:, :, :],
                        in1=gate[:, i, g0:g0 + EG].unsqueeze(2).broadcast_to([128, EG, CW]), op=ALU.mult),
                        reads=[f"selGa{sg}", "gate"], writes=[f"selGa{sg}"])
                    pst = ps[2 + sg][:].bitcast(BF16)
                    for ee in range(EG):
                        for ct in range(CT):
                            kb.op("pe", lambda e, ct=ct, ee=ee, sga=sga, pst=pst: e.transpose(
                                out=pst[0:cs, (ee * CT + ct) * 128:(ee * CT + ct + 1) * 128],
                                in_=sga[:, ee, ct * 128:ct * 128 + cs], identity=c.identb[:]),
                                reads=[f"selGa{sg}", "c_identb"], writes=[f"ps{2 + sg}"])
                    kb.op("act", lambda e, sg=sg, pst=pst: e.copy(
                        out=selGca[sg][0:cs, 0:EG * CT, :], in_=pst[0:cs, 0:EG * CT * 128].rearrange("p (c t) -> p c t", t=128)),
                        reads=[f"ps{2 + sg}"], writes=[f"selGca{sg}"])
                    for ee in range(EG):
                        ex_i = g0 + ee
                        for half in range(2):
                            for ct in range(CT):
                                kb.op("pe", lambda e, half=half, ct=ct, sg=sg, ex_i=ex_i, ee=ee: e.matmul(
                                    ps[half][:, :], lhsT=selGca[sg][0:cs, ee * CT + ct, :],
                                    rhs=yeall[0:cs, ex_i, ct, half * 512:(half + 1) * 512],
                                    start=(ex_i == 0 and ct == 0), stop=(ex_i == 15 and ct == CT - 1)),
                                    reads=[f"selGca{sg}", f"ye{ex_i}"], writes=[f"ps{half}"])
                for half in range(2):
                    sl = slice(half * 512, (half + 1) * 512)
                    kb.op("dve", lambda e, half=half, sl=sl: e.tensor_tensor(
                        out=tmpo[half][:], in0=ps[half][:], in1=gate2[:, sl], op=ALU.mult),
                        reads=[f"ps{half}", "gate2"], writes=[f"tmpo{half}"])
                    kb.op("pool", lambda e, half=half, sl=sl, b=b: e.tensor_tensor(
                        out=xb[b][:, sl], in0=xb[b][:, sl], in1=tmpo[half][:], op=ALU.add),
                        reads=[f"tmpo{half}", f"xb{b}"], writes=[f"xb{b}"])
                if final_norm:
                    norm_tile(kb, nc, xb[b][:], f"xb{b}", stt[b], Gbc, None, h32[b][:], f"h32{b}")
                    kb.dma("sp", xout_v[i], h32[b][:], reads=[f"h32{b}"], writes=[("dram", x_out.tensor.name, i)])
                else:
                    kb.dma("sp", xout_v[i], xb[b][:], reads=[f"xb{b}"], writes=[("dram", x_out.tensor.name, i)])
            kb.barrier()


def host_consts():
    k = {}
    k["k_ident"] = np.eye(128, dtype=np.float32)
    k["k_iota"] = np.tile(np.arange(256, dtype=np.float32)[None, :], (128, 1))
    C, S, perm = rope_tables()
    k["k_ropeC"], k["k_ropeS"], k["k_perm"] = C, S, perm
    kk = np.arange(128)[:, None]
    qq = np.arange(128)[None, :]
    k["k_mL"] = np.tile((kk >= qq).astype(np.float32), (1, 4))
    k["k_mU"] = np.tile((kk <= qq).astype(np.float32), (1, 4))
    ss = np.arange(CHUNK)[:, None]
    tt = np.arange(CHUNK)[None, :]
    mus = (ss < tt).astype(np.float32)
    mui = (ss <= tt).astype(np.float32)
    k["k_maskA"] = np.ascontiguousarray(np.concatenate([-mus, mus, mui, mui], axis=1))
    k["k_maskT"] = np.ascontiguousarray(-(tt < ss).astype(np.float32))
    rm = np.ones((128, TP), np.float32)
    rm[:, CHUNK_COLS] = 0.0
    k["k_rmask"] = rm
    k["k_J"] = np.ascontiguousarray(np.eye(128, dtype=np.float32)[::-1])
    sel = np.zeros((16, 16, 128), np.float32)
    for r in range(16):
        sel[r, r, :] = 1.0
    k["k_sel"] = sel
    bdm = np.zeros((128, 128), np.float32)
    bdm[0:64, 0:64] = 1.0
    bdm[64:128, 64:128] = 1.0
    k["k_bd"] = bdm
    return k


def fm(v):
    v = np.asarray(v, np.float32)
    return np.ascontiguousarray(v.reshape(-1, 128).T)


def host_inputs(inp, b):
    m = dict(host_consts())
    m["x"] = np.ascontiguousarray(inp["x"][b])
    m["ctx"] = np.ascontiguousarray(inp["ctx"][b])
    m["cT"] = fm(inp["c"][b])
    m["ccT"] = fm(inp["c_ctx"])
    m["ada_w"] = inp["ada_w"]
    m["ada_bT"] = np.ascontiguousarray(np.stack([fm(inp["ada_b"][l]) for l in range(2)], axis=1))
    m["normT"] = np.ascontiguousarray(np.stack(
        [np.stack([fm(inp["norm_mix"][l]), fm(inp["norm_ffn"][l])], axis=1) for l in range(2)], axis=1))
    m["fnT"] = fm(inp["final_norm"])
    for k in ("moe_router", "moe_w1", "moe_w3", "moe_w2", "o_w_out"):
        m[k] = inp[k]
    w = inp["o_w_in"][0]
    kd = w[:, 1024:1280].reshape(1024, 4, 1, 64)
    kd = np.concatenate([kd, kd], axis=2).reshape(1024, 512)
    m["o_w_in2"] = np.ascontiguousarray(np.concatenate([w[:, 0:1024], kd, w[:, 1280:1536]], axis=1))
    m["o_sink"] = np.ascontiguousarray(inp["o_sink"].reshape(1, 16))
    idx = []
    for p in range(4):
        for off in (0, 512, 1024):
            idx += list(range(off + p * 128, off + p * 128 + 128))
    for d in range(2):
        idx += list(range(1536 + d * 64, 1536 + d * 64 + 64)) + list(range(1664 + d * 64, 1664 + d * 64 + 64))
    idx += list(range(1792, 1920))
    idxa = list(idx)
    for h in range(4):
        for part in range(3):
            idx += list(range(1920 + part * 512 + h * 128, 1920 + part * 512 + h * 128 + 128))
        idx += list(range(3472 + h * 128, 3472 + h * 128 + 128))
    idx += list(range(3456, 3472))
    m["e_w_in2"] = np.ascontiguousarray(inp["e_w_in"][0][:, idx])
    mu2 = inp["a_mu"][0][idxa]
    p64 = np.zeros((64, 64), np.float32)
    p64[:, 0:30] = mu2.reshape(30, 64).T
    for d in range(2):
        for h in range(8):
            p64[:, 30 + d * 8 + h] = inp["a_w0"][0, d, h * 64:(h + 1) * 64]
            p64[:, 46 + d * 8 + h] = inp["a_a0"][0, d, h * 64:(h + 1) * 64]
    m["mx_par64"] = p64
    p128 = np.zeros((128, 112), np.float32)
    p128[:, 0] = mu2[1536:1664]; p128[:, 1] = mu2[1664:1792]; p128[:, 2] = mu2[1792:1920]
    for h in range(8):
        p128[0:64, 8 + h] = inp["a_k_k"][0, h * 64:(h + 1) * 64]
        p128[0:64, 16 + h] = inp["a_k_a"][0, h * 64:(h + 1) * 64]
        p128[0:64, 32 + h] = inp["a_r_k"][0, h]
    p128[0:8, 40] = inp["b_dt_bias"][0].reshape(8)
    p128[0:8, 41] = inp["b_a_log"][0].reshape(8)
    for h in range(4):
        for part in range(3):
            for j in range(5):
                p128[:, 48 + (h * 3 + part) * 5 + j] = inp["b_conv"][0, j, part * 512 + h * 128:part * 512 + (h + 1) * 128]
    m["mx_par128"] = p128
    pP = np.zeros((128, 64), np.float32)
    pP[:, 0:12] = mu2[0:1536].reshape(12, 128).T
    for p in range(4):
        sl = slice(p * 128, (p + 1) * 128)
        for d in range(2):
            pP[:, 12 + d * 4 + p] = inp["a_w0"][0, d, sl]
            pP[:, 20 + d * 4 + p] = inp["a_a0"][0, d, sl]
        pP[:, 28 + p] = inp["a_k_k"][0, sl]
        pP[:, 32 + p] = inp["a_k_a"][0, sl]
        pP[:, 40 + p] = inp["a_r_k"][0].reshape(512)[sl]
    m["mx_parP"] = pP
    m["mx_w2"] = np.ascontiguousarray(np.concatenate([inp["a_w2"][0].transpose(1, 0, 2), inp["a_a2"][0].transpose(1, 0, 2)], axis=0))
    m["mx_bc"] = np.ascontiguousarray(np.concatenate([inp["a_ln_w"][0], inp["a_ln_b"][0], np.tile(inp["b_norm"][0], 4)])[None, :])
    m["a_g2"] = inp["a_g2"]
    m["e_w_out"] = inp["e_w_out"]
    return m


IN_SHAPES = {
    "k_ident": [128, 128], "k_iota": [128, 256],
    "x": [2048, D], "ctx": [256, D], "cT": [128, 8], "ccT": [128, 8],
    "ada_w": [2, D, 6 * D], "ada_bT": [128, 2, 48], "normT": [128, 2, 2, 8], "fnT": [128, 8],
    "k_ropeC": [128, 2048], "k_ropeS": [128, 2048], "k_perm": [128, 128], "k_mL": [128, 512], "k_mU": [128, 512],
    "o_w_in2": [D, 1792], "o_w_out": [1, D, D], "o_sink": [1, 16],
    "k_maskA": [CHUNK, 4 * CHUNK], "k_maskT": [CHUNK, CHUNK], "k_rmask": [128, TP], "k_J": [128, 128], "k_sel": [16, 16, 128],
    "e_w_in2": [D, 3984], "mx_par64": [64, 64], "mx_parP": [128, 64], "k_bd": [128, 128], "mx_par128": [128, 112], "mx_w2": [128, 2, 512], "mx_bc": [1, 1536],
    "a_g2": [1, 128, 512], "e_w_out": [1, D, D],
    "moe_router": [2, D, 16], "moe_w1": [2, 16, D, D], "moe_w3": [2, 16, D, D], "moe_w2": [2, 16, D, D],
}


def build(stages=("all",), extra_in=(), outs=(("out", [2048, D]),)):
    from contextlib import ExitStack
    nc = bass.Bass("TRN2", target_bir_lowering=False)
    g = {}
    for name, shape in IN_SHAPES.items():
        g[name] = nc.dram_tensor(name, shape, F32, kind="ExternalInput").ap()
    for name, shape in extra_in:
        g[name] = nc.dram_tensor(name, shape, F32, kind="ExternalInput").ap()
    for name, shape in outs:
        g[name] = nc.dram_tensor(name, shape, F32, kind="ExternalOutput").ap()
    g["ye_scr"] = nc.dram_tensor("ye_scr", [16, 128, 2, D], BF16).ap()
    g["ye_scr_c"] = nc.dram_tensor("ye_scr_c", [16, 32, 1, D], BF16).ap()
    g["ys_scr"] = nc.dram_tensor("ys_scr", [2, 2304, 1536], F32).ap()
    g["z_scr"] = nc.dram_tensor("z_scr", [2304, 512], F32).ap()
    for nm, rows in (("xm_lat", 2048), ("xm_ctx", 256), ("x1_lat", 2048), ("x1_ctx", 256), ("x2_lat", 2048)):
        g[nm] = nc.dram_tensor(nm, [rows, D], F32).ap()
    kb = KB(nc)
    with ExitStack() as es:
        c = load_consts(kb, nc, es, g)
        c.fnT = es.enter_context(nc.sbuf_tensor("c_fnT", [128, 8], F32))
        kb.dma("sp", c.fnT[:], g["fnT"][:, :], writes=["c_fnT"])
        mods = prologue(kb, nc, es, g, c)
        for st in stages:
            if st == "moe0l_test":
                moe_stage(kb, nc, g, c, mods, 0, 0, g["t_in"], g["out"], 2048)
            elif st == "moe0f_test":
                moe_stage(kb, nc, g, c, mods, 0, 0, g["t_in"], g["out"], 2048, comp=(g["t_in2"], g["out2"]))
            elif st == "moe0c_test":
                moe_stage(kb, nc, g, c, mods, 0, 1, g["t_in"], g["out"], 256)
            elif st == "moe1l_test":
                moe_stage(kb, nc, g, c, mods, 1, 0, g["t_in"], g["out"], 2048, final_norm=True)
            elif st == "all":
                mixer_stage(kb, nc, g, c, mods, g["x"], g["ctx"], g["xm_lat"], g["xm_ctx"])
                moe_stage(kb, nc, g, c, mods, 0, 0, g["xm_lat"], g["x1_lat"], 2048, comp=(g["xm_ctx"], g["x1_ctx"]))
                attn_stage(kb, nc, g, c, mods, g["x1_lat"], g["x1_ctx"], g["x2_lat"])
                moe_stage(kb, nc, g, c, mods, 1, 0, g["x2_lat"], g["out"], 2048, final_norm=True)
            elif st == "mixer_test":
                mixer_stage(kb, nc, g, c, mods, g["x"], g["ctx"], g["out"], g["out2"])
            elif st == "attn_test":
                attn_stage(kb, nc, g, c, mods, g["t_in"], g["t_in2"], g["out"])
            elif st == "mods_test":
                for l in range(2):
                    kb.dma("sp", g["out"][l * 128:(l + 1) * 128, 0:96], mods[l][:].rearrange("p a b -> p (a b)"),
                           reads=[f"mod{l}"], writes=[("o", l)])
        kb.finish([])
    return nc, kb


def rope_tables():
    quarter = 16
    inv = (10000.0 ** (-np.arange(quarter, dtype=np.float32) / quarter)).astype(np.float32)
    t = np.arange(2048)
    row = (t // 64).astype(np.float32)
    col = (t % 64).astype(np.float32)
    C = np.zeros((128, 2048), np.float32)
    S = np.zeros((128, 2048), np.float32)
    perm = np.zeros((128, 128), np.float32)
    for p in range(128):
        d = p % 64
        pos = row if d < 32 else col
        i = d % 16
        ang = (pos * inv[i]).astype(np.float32)
        C[p] = np.cos(ang)
        second = (d % 32) >= 16
        S[p] = np.sin(ang) if second else -np.sin(ang)
        partner = p - 16 if second else p + 16
        perm[partner, p] = 1.0
    return C, S, perm


def attn_stage(kb, nc, g, c, mods, x_lat_in, x_ctx_in, x_out):
    layer = 1
    mod = mods[layer]
    NT = 18
    from contextlib import ExitStack
    with ExitStack() as es:
        def sb(name, shape, dt):
            return es.enter_context(nc.sbuf_tensor(f"at_{name}", shape, dt))
        Gbc = sb("G", [128, D], F32)
        Sbc = sb("S", [128, D], F32)
        xb = [sb("x0", [128, D], F32), sb("x1", [128, D], F32)]
        hb = [sb("h0", [128, D], BF16), sb("h1", [128, D], BF16)]
        stt = [sb("st0", [128, 4], F32), sb("st1", [128, 4], F32)]
        hT = sb("hT", [128, 8, 2304], BF16)
        win = sb("win", [128, 8, 1792], BF16)
        wo = sb("wo", [128, 8, D], BF16)
        stg = [sb("stg0", [128, D], F32), sb("stg1", [128, D], F32)]
        Ct = sb("Ct", [128, 2048], F32)
        St = sb("St", [128, 2048], F32)
        perm = sb("perm", [128, 128], F32)
        qraw = [sb("qraw0", [128, 512], F32), sb("qraw1", [128, 512], F32)]
        rt1 = [sb("rt10", [128, 512], F32), sb("rt11", [128, 512], F32)]
        qT = sb("qT", [128, 8, 2048], BF16)
        kT = sb("kT", [128, 4, 2304], BF16)
        V = sb("V", [128, NT, 4, 65], BF16)
        mL = sb("mL", [128, 512], BF16)
        mU = sb("mU", [128, 512], BF16)
        esink = sb("esink", [128, 16], F32)
        PT = [sb(f"PT{i}", [128, 512], BF16) for i in range(2)]
        osb = stg[0]
        den = sb("den", [128, 4], F32)
        oT = sb("oT", [128, 8, 128], BF16)
        tmpo = qraw
        ps = [es.enter_context(nc.psum_tensor(f"at_ps{i}", [128, 512], F32)) for i in range(8)]

        kb.dma("sp", Ct[:], g["k_ropeC"][:, :], writes=["Ct"])
        kb.dma("sp", St[:], g["k_ropeS"][:, :], writes=["St"])
        kb.dma("sp", perm[:], g["k_perm"][:, :], writes=["perm"])
        kb.dma("sp", esink[:], g["o_sink"].partition_broadcast(128), writes=["esink"])
        kb.op("act", lambda e: e.activation(out=esink[:], in_=esink[:], func=AF.Exp), reads=["esink"], writes=["esink"])
        kb.dma("sp", stg[0][:, 0:512], g["k_mL"][:, :], writes=["stg0"])
        kb.op("dve", lambda e: e.tensor_copy(out=mL[:], in_=stg[0][:, 0:512]), reads=["stg0"], writes=["mL"])
        kb.dma("sp", stg[1][:, 0:512], g["k_mU"][:, :], writes=["stg1"])
        kb.op("dve", lambda e: e.tensor_copy(out=mU[:], in_=stg[1][:, 0:512]), reads=["stg1"], writes=["mU"])
        make_bc(kb, nc, c, lambda kc: mod[:, 1 * 8 + kc, 1:2], Gbc, ps[0], "Gbc", [f"mod{layer}"])
        make_bc(kb, nc, c, lambda kc: mod[:, 0 * 8 + kc, 1:2], Sbc, ps[0], "Sbc", [f"mod{layer}"])

        wcnt = [0]

        wv = g["o_w_in2"].rearrange("(kc p) n -> p kc n", p=128)
        for kc in range(0, 8, 2):
            kb.dma("pool", win[:, kc:kc + 2, :], wv[:, kc:kc + 2, :], writes=["win"])
        wov = g["o_w_out"][0].rearrange("(kc p) n -> p kc n", p=128)
        for kc in range(0, 8, 4):
            kb.dma("pool", wo[:, kc:kc + 4, :], wov[:, kc:kc + 4, :], writes=["wo"])

        for i in range(NT):
            b = i % 2
            if i == 2:
                kb.barrier()
                make_bc(kb, nc, c, lambda kc: mod[:, 1 * 8 + kc, 0:1], Gbc, ps[0], "Gbc", [f"mod{layer}"])
                make_bc(kb, nc, c, lambda kc: mod[:, 0 * 8 + kc, 0:1], Sbc, ps[0], "Sbc", [f"mod{layer}"])
            src = x_ctx_in[i * 128:(i + 1) * 128, :] if i < 2 else x_lat_in[(i - 2) * 128:(i - 1) * 128, :]
            kb.dma("sp", xb[b][:], src, writes=[f"xb{b}"])
            norm_tile(kb, nc, xb[b][:], f"xb{b}", stt[b], Gbc, Sbc, stg[b][:], f"stg{b}")
            kb.op("act", lambda e, b=b: e.copy(out=hb[b][:], in_=stg[b][:]), reads=[f"stg{b}"], writes=[f"hb{b}"])
            for half in range(2):
                pst = ps[half][:].bitcast(BF16)
                for q in range(4):
                    kc = half * 4 + q
                    kb.op("pe", lambda e, kc=kc, q=q, b=b, pst=pst: e.transpose(
                        out=pst[:, q * 128:(q + 1) * 128], in_=hb[b][:, kc * 128:(kc + 1) * 128],
                        identity=c.identb[:]), reads=[f"hb{b}", "c_identb"], writes=[f"ps{half}"])
                kb.op("dve" if half == 0 else "pool" if False else "act",
                      (lambda e, half=half, pst=pst, i=i: e.tensor_copy(
                          out=hT[:, half * 4:(half + 1) * 4, i * 128:(i + 1) * 128],
                          in_=pst[:, 0:512].rearrange("p (q t) -> p q t", q=4))) if half == 0 else
                      (lambda e, half=half, pst=pst, i=i: e.copy(
                          out=hT[:, half * 4:(half + 1) * 4, i * 128:(i + 1) * 128],
                          in_=pst[:, 0:512].rearrange("p (q t) -> p q t", q=4))),
                      reads=[f"ps{half}"], writes=["hT"])

        import os
        APH = int(os.environ.get("ATT_PH", "9"))
        if APH < 1:
            kb.barrier(); return
        nb = 0
        for nq in range(8):
            for tb in range(4):
                b = nb % 2
                nb += 1
                pq = ps[2 + b]
                t0 = 256 + tb * 512
                for kc in range(8):
                    kb.op("pe", lambda e, kc=kc, nq=nq, t0=t0, pq=pq: e.matmul(
                        pq[:], lhsT=win[:, kc, nq * 128:(nq + 1) * 128], rhs=hT[:, kc, t0:t0 + 512],
                        start=(kc == 0), stop=(kc == 7)), reads=["win", "hT"], writes=[f"ps{2 + b}"])
                kb.op("act", lambda e, b=b, pq=pq: e.copy(out=qraw[b][:], in_=pq[:]), reads=[f"ps{2 + b}"],
                      writes=[f"qraw{b}"])
                pw = ps[4 + b]
                kb.op("pe", lambda e, b=b, pw=pw: e.matmul(pw[:], lhsT=perm[:], rhs=qraw[b][:], start=True, stop=True),
                      reads=["perm", f"qraw{b}"], writes=[f"ps{4 + b}"])
                cs_ = slice(tb * 512, (tb + 1) * 512)
                kb.op("dve", lambda e, b=b, pw=pw, cs_=cs_: e.scalar_tensor_tensor(
                    out=rt1[b][:], in0=pw[:], scalar=0.125, in1=St[:, cs_], op0=ALU.mult, op1=ALU.mult),
                      reads=[f"ps{4 + b}", "St"], writes=[f"rt1{b}"])
                kb.op("pool", lambda e, b=b, cs_=cs_: e.tensor_tensor(out=qraw[b][:], in0=qraw[b][:], in1=Ct[:, cs_],
                                                                    op=ALU.mult),
                      reads=[f"qraw{b}", "Ct"], writes=[f"qraw{b}"])
                kb.op("dve", lambda e, b=b, nq=nq, cs_=cs_: e.scalar_tensor_tensor(
                    out=qT[:, nq, cs_], in0=qraw[b][:], scalar=0.125, in1=rt1[b][:], op0=ALU.mult, op1=ALU.add),
                    reads=[f"qraw{b}", f"rt1{b}"], writes=["qT"])
        if APH < 2:
            kb.barrier(); return
        for hk in range(4):
            for tb in range(5):
                b = nb % 2
                nb += 1
                pq = ps[2 + b]
                t0 = 0 if tb == 0 else 256 + (tb - 1) * 512
                tw = 256 if tb == 0 else 512
                for kc in range(8):
                    kb.op("pe", lambda e, kc=kc, hk=hk, t0=t0, tw=tw, pq=pq: e.matmul(
                        pq[:, 0:tw], lhsT=win[:, kc, 1024 + hk * 128:1024 + (hk + 1) * 128],
                        rhs=hT[:, kc, t0:t0 + tw], start=(kc == 0), stop=(kc == 7)),
                        reads=["win", "hT"], writes=[f"ps{2 + b}"])
                if tb == 0:
                    kb.op("act", lambda e, hk=hk, pq=pq: e.copy(out=kT[:, hk, 0:256], in_=pq[:, 0:256]),
                          reads=[f"ps{2 + b}"], writes=["kT"])
                    continue
                kb.op("act", lambda e, b=b, pq=pq: e.copy(out=qraw[b][:], in_=pq[:]), reads=[f"ps{2 + b}"],
                      writes=[f"qraw{b}"])
                pw = ps[4 + b]
                kb.op("pe", lambda e, b=b, pw=pw: e.matmul(pw[:], lhsT=perm[:], rhs=qraw[b][:], start=True, stop=True),
                      reads=["perm", f"qraw{b}"], writes=[f"ps{4 + b}"])
                cs_ = slice((tb - 1) * 512, tb * 512)
                kb.op("dve", lambda e, b=b, pw=pw, cs_=cs_: e.tensor_tensor(out=rt1[b][:], in0=pw[:], in1=St[:, cs_],
                                                                         op=ALU.mult),
                      reads=[f"ps{4 + b}", "St"], writes=[f"rt1{b}"])
                kb.op("pool", lambda e, b=b, cs_=cs_: e.tensor_tensor(out=qraw[b][:], in0=qraw[b][:], in1=Ct[:, cs_],
                                                                    op=ALU.mult),
                      reads=[f"qraw{b}", "Ct"], writes=[f"qraw{b}"])
                kb.op("dve", lambda e, b=b, hk=hk, t0=t0: e.tensor_tensor(
                    out=kT[:, hk, t0:t0 + 512], in0=qraw[b][:], in1=rt1[b][:], op=ALU.add),
                    reads=[f"qraw{b}", f"rt1{b}"], writes=["kT"])
        if APH < 3:
            kb.barrier(); return
        kb.op("pool", lambda e: e.memset(V[:], 1.0), writes=["V"])
        for i in range(NT):
            b = nb % 2
            nb += 1
            pq = ps[2 + b]
            for kc in range(8):
                kb.op("pe", lambda e, kc=kc, i=i, pq=pq: e.matmul(
                    pq[:, 0:256], lhsT=hT[:, kc, i * 128:(i + 1) * 128], rhs=win[:, kc, 1536:1792],
                    start=(kc == 0), stop=(kc == 7)), reads=["win", "hT"], writes=[f"ps{2 + b}"])
            kb.op("act", lambda e, i=i, pq=pq: e.copy(out=V[:, i, :, 0:64],
                                                     in_=pq[:, 0:256].rearrange("p (h d) -> p h d", h=4)),
                  reads=[f"ps{2 + b}"], writes=["V"])

        if APH < 4:
            kb.barrier(); return
        make_bc(kb, nc, c, lambda kc: mod[:, 2 * 8 + kc, 0:1], Gbc, ps[0], "Gbc", [f"mod{layer}"])
        nsb = 0
        for n in range(16):
            kb.dma("sp", xb[n % 2][:], x_lat_in[n * 128:(n + 1) * 128, :], writes=[f"xb{n % 2}"])
            for hk in range(4):
                tiles = []
                if n > 0:
                    tiles.append((2 + n - 1, mL, "mL"))
                tiles.append((2 + n, None, None))
                if n < 15:
                    tiles.append((2 + n + 1, mU, "mU"))
                tiles.append((0, None, None))
                tiles.append((1, None, None))
                po = ps[6 + (n * 4 + hk) % 2]
                pok = f"ps{6 + (n * 4 + hk) % 2}"
                for ti, (kt, msk, mk) in enumerate(tiles):
                    sbk = nsb % 2
                    nsb += 1
                    pSa, pSb = ps[1 + 2 * sbk], ps[2 + 2 * sbk]
                    ka, kbk = f"ps{1 + 2 * sbk}", f"ps{2 + 2 * sbk}"
                    for gq in range(4):
                        hq = hk * 4 + gq
                        bp = (hq % 2) * 64
                        pS = pSa if bp == 0 else pSb
                        kb.op("pe", lambda e, gq=gq, hq=hq, bp=bp, kt=kt, pS=pS, hk=hk, n=n: e.matmul(
                            pS[:, (gq // 2) * 128:(gq // 2 + 1) * 128], lhsT=kT[bp:bp + 64, hk, kt * 128:(kt + 1) * 128],
                            rhs=qT[bp:bp + 64, hq // 2, n * 128:(n + 1) * 128], start=True, stop=True),
                            reads=["kT", "qT"], writes=[ka if bp == 0 else kbk])
                    ptv = PT[sbk][:].rearrange("p (a b q) -> p a b q", a=2, b=2)
                    kb.op("act", lambda e, pSa=pSa, ptv=ptv: e.activation(
                        out=ptv[:, :, 0, :], in_=pSa[:, 0:256].rearrange("p (a q) -> p a q", a=2), func=AF.Exp),
                        reads=[ka], writes=[f"PT{sbk}"])
                    kb.op("act", lambda e, pSb=pSb, ptv=ptv: e.activation(
                        out=ptv[:, :, 1, :], in_=pSb[:, 0:256].rearrange("p (a q) -> p a q", a=2), func=AF.Exp),
                        reads=[kbk], writes=[f"PT{sbk}"])
                    ADBG = int(os.environ.get("ATT_DBG", "9"))
                    if ADBG < 2:
                        continue
                    if msk is not None:
                        kb.op("dve", lambda e, sbk=sbk, msk=msk: e.tensor_tensor(out=PT[sbk][:], in0=PT[sbk][:],
                                                                               in1=msk[:], op=ALU.mult),
                              reads=[f"PT{sbk}", mk], writes=[f"PT{sbk}"])
                    for gq in range(4):
                        kb.op("pe", lambda e, gq=gq, sbk=sbk, kt=kt, hk=hk, po=po, ti=ti: e.matmul(
                            po[:, gq * 65:(gq + 1) * 65], lhsT=PT[sbk][:, gq * 128:(gq + 1) * 128],
                            rhs=V[:, kt, hk, :], start=(ti == 0 and gq == 0), stop=(ti == len(tiles) - 1),
                            skip_group_check=True), reads=[f"PT{sbk}", "V"], writes=[pok])
                if ADBG < 3:
                    continue
                pov = po[:, 0:260].rearrange("p (g d) -> p g d", g=4)
                kb.op("dve", lambda e, pov=pov, hk=hk: e.tensor_tensor(
                    out=den[:], in0=pov[:, :, 64], in1=esink[:, hk * 4:(hk + 1) * 4], op=ALU.add),
                    reads=[pok, "esink"], writes=["den"])
                kb.op("dve", lambda e: e.reciprocal(out=den[:], in_=den[:]), reads=["den"], writes=["den"])
                kb.op("dve", lambda e, pov=pov, hk=hk: e.tensor_tensor(
                    out=osb[:, hk * 256:(hk + 1) * 256].rearrange("p (g d) -> p g d", g=4), in0=pov[:, :, 0:64],
                    in1=den[:].unsqueeze(2).broadcast_to([128, 4, 64]), op=ALU.mult),
                    reads=[pok, "den"], writes=["stg0"])
            if ADBG < 4:
                continue
            kb.op("act", lambda e: e.copy(out=hb[0][:], in_=osb[:]), reads=["stg0"], writes=["hb0"])
            pst = ps[0][:].bitcast(BF16)
            for kc in range(8):
                kb.op("pe", lambda e, kc=kc, pst=pst: e.transpose(
                    out=pst[:, kc * 128:(kc + 1) * 128], in_=hb[0][:, kc * 128:(kc + 1) * 128], identity=c.identb[:]),
                    reads=["hb0", "c_identb"], writes=["ps0"])
            kb.op("act", lambda e, pst=pst: e.copy(out=oT[:], in_=pst[:].rearrange("p (k t) -> p k t", k=8)),
                  reads=["ps0"], writes=["oT"])
            for half in range(2):
                pp = ps[5] if half == 0 else ps[0]
                for kc in range(8):
                    kb.op("pe", lambda e, kc=kc, half=half, pp=pp: e.matmul(
                        pp[:], lhsT=oT[:, kc, :], rhs=wo[:, kc, half * 512:(half + 1) * 512],
                        start=(kc == 0), stop=(kc == 7)), reads=["oT", "wo"], writes=["ps5" if half == 0 else "ps0"])
                sl = slice(half * 512, (half + 1) * 512)
                kb.op("dve", lambda e, half=half, pp=pp, sl=sl: e.tensor_tensor(
                    out=tmpo[half][:], in0=pp[:], in1=Gbc[:, sl], op=ALU.mult),
                    reads=["ps5" if half == 0 else "ps0", "Gbc"], writes=[f"qraw{half}"])
                kb.op("pool", lambda e, half=half, sl=sl, n=n: e.tensor_tensor(
                    out=xb[n % 2][:, sl], in0=xb[n % 2][:, sl], in1=tmpo[half][:], op=ALU.add),
                    reads=[f"qraw{half}", f"xb{n % 2}"], writes=[f"xb{n % 2}"])
            kb.dma("sp", x_out[n * 128:(n + 1) * 128, :], xb[n % 2][:], reads=[f"xb{n % 2}"],
                   writes=[("dram", x_out.tensor.name, n)])
        kb.barrier()


def dplr_scan(kb, nc, c, T, dk, rT, kkT, kT, bT, vT, Pinc, prodT, store_cb, hk_, Gb=None, rA=None, kkA=None, bp=0, rC=None, kkC=None):
    ps = T["ps"]
    rA = rT if rA is None else rA
    kkA = kkT if kkA is None else kkA
    rC = rT if rC is None else rC
    kkC = kkT if kkC is None else kkC
    tb = vT.dtype == BF16
    ident, maskA, maskT, ident64 = c.ident, T["maskA"], T["maskT"], c.ident
    ST = T["ST"]
    kb.op("dve", lambda e: e.memset(ST[bp:bp + dk, 0:dk], 0.0), writes=["ST"])
    kb.op("dve", lambda e: e.memset(T["STb"][bp:bp + dk, 0:dk], 0.0), writes=["STb"])
    CH = CHUNK
    NLV = {64: 5, 128: 6}[CH]
    NCH = len(CHUNK_COLS)
    GB = 2

    def inv_gen(g0):
        grp = list(range(g0, min(NCH, g0 + GB)))
        par = (g0 // GB) % 2
        for ci in grp:
            s = ci % GB + GB * par
            cs = slice(CHUNK_COLS[ci], CHUNK_COLS[ci] + CH)
            pa = ps[s % 2]
            pk = f"ps{s % 2}"
            pn = ps[2 + s % 2]
            pnk = f"ps{2 + s % 2}"
            for j, (l, r) in enumerate(((bT, kkA), (kT, kkA), (bT, rA), (kT, rA), (kkA, bT))):
                if j < 4:
                    kb.op("pe", lambda e, j=j, l=l, r=r, cs=cs, pa=pa: e.matmul(
                        pa[0:CH, j * CH:(j + 1) * CH], lhsT=l[bp:bp + dk, cs], rhs=r[bp:bp + dk, cs], start=True, stop=True),
                        reads=[hk_], writes=[pk])
                else:
                    kb.op("pe", lambda e, l=l, r=r, cs=cs, pn=pn: e.matmul(
                        pn[0:CH, 256:256 + CH], lhsT=l[bp:bp + dk, cs], rhs=r[bp:bp + dk, cs], start=True, stop=True),
                        reads=[hk_], writes=[pnk])
            mA, mT_, mAk, mTk = maskA[0:CH, :], maskT[0:CH, :], "maskA", "maskT"
            if Gb is not None:
                c0_ = CHUNK_COLS[ci]
                kb.op("pe", lambda e, cs=cs, pn=pn: e.transpose(out=pn[0:CH, 384:385], in_=Gb[0:1, cs],
                                                              identity=ident[0:1, 0:1]), reads=[hk_, "c_ident"], writes=[pnk])
                kb.op("act", lambda e, s=s, pn=pn: e.copy(out=T["Gc"][0:CH, s:s + 1], in_=pn[0:CH, 384:385]),
                      reads=[pnk], writes=[f"Gc{s}"])
                kb.op("dve", lambda e, s=s, cs=cs: e.tensor_scalar(out=T["Dt"][0:CH, s % GB, :], in0=Gb[0:CH, cs],
                                                                 scalar1=T["Gc"][0:CH, s:s + 1], scalar2=0.0,
                                                                 op0=ALU.subtract, op1=ALU.min),
                      reads=[hk_, f"Gc{s}"], writes=[f"Dt{s % GB}"])
                kb.op("act", lambda e, s=s: e.activation(out=T["Dt"][0:CH, s % GB, :], in_=T["Dt"][0:CH, s % GB, :], func=AF.Exp),
                      reads=[f"Dt{s % GB}"], writes=[f"Dt{s % GB}"])
                kb.op("dve", lambda e, s=s, cs=cs: e.tensor_scalar(out=T["Dts"][0:CH, s % GB, :], in0=Gb[0:CH, cs],
                                                                 scalar1=T["Gc"][0:CH, s:s + 1], scalar2=0.0,
                                                                 op0=ALU.subtract, op1=ALU.max),
                      reads=[hk_, f"Gc{s}"], writes=[f"Dts{s % GB}"])
                kb.op("act", lambda e, s=s: e.activation(out=T["Dts"][0:CH, s % GB, :], in_=T["Dts"][0:CH, s % GB, :], func=AF.Exp,
                                                         scale=-1.0), reads=[f"Dts{s % GB}"], writes=[f"Dts{s % GB}"])
                kb.op("dve", lambda e, s=s: e.tensor_tensor(
                    out=T["mD"][0:CH, s % GB, :].rearrange("p (a t) -> p a t", a=4),
                    in0=maskA[0:CH, :].rearrange("p (a t) -> p a t", a=4),
                    in1=T["Dt"][0:CH, s % GB, :].unsqueeze(1).broadcast_to([CH, 4, CH]), op=ALU.mult),
                    reads=["maskA", f"Dt{s % GB}"], writes=[f"mD{s % GB}"])
                kb.op("pool", lambda e, s=s: e.tensor_tensor(out=T["Dts"][0:CH, s % GB, :], in0=T["Dts"][0:CH, s % GB, :],
                                                            in1=maskT[0:CH, :], op=ALU.mult),
                      reads=["maskT", f"Dts{s % GB}"], writes=[f"Dts{s % GB}"])
                kb.op("act", lambda e, s=s, c0_=c0_: e.activation(out=T["dL"][0:CH, s:s + 1], in_=T["Gc"][0:CH, s:s + 1],
                                                                func=AF.Exp, scale=-1.0, bias=Gb[0:CH, c0_ + CH - 1:c0_ + CH]),
                      reads=[f"Gc{s}", hk_], writes=[f"dL{s}"])
                mA, mT_, mAk, mTk = T["mD"][0:CH, s % GB, :], T["Dts"][0:CH, s % GB, :], f"mD{s % GB}", f"Dts{s % GB}"
            kb.op("dve", lambda e, s=s, pa=pa, mA=mA: e.tensor_tensor(out=T["AMf"][0:CH, s, :], in0=pa[0:CH, 0:CH],
                                                                     in1=mA[:, 0:CH], op=ALU.mult),
                  reads=[pk, mAk], writes=[f"AM{s}"])
            kb.op("dve", lambda e, s=s, pa=pa, mA=mA: e.tensor_tensor(out=T["AMb"][0:CH, s, :], in0=pa[0:CH, CH:4 * CH],
                                                                     in1=mA[:, CH:4 * CH], op=ALU.mult),
                  reads=[pk, mAk], writes=[f"AMb{s}"])
            kb.op("dve", lambda e, s=s, pn=pn, mT_=mT_: e.tensor_tensor(out=T["MM"][0][0:CH, s, CH:2 * CH],
                                                                       in0=pn[0:CH, 256:256 + CH], in1=mT_, op=ALU.mult),
                  reads=[pnk, mTk], writes=[f"MM0_{s}"])
            kb.op("pool", lambda e, s=s: e.tensor_copy(out=T["MM"][0][0:CH, s, 0:CH], in_=T["AMf"][0:CH, s, :]),
                  reads=[f"AM{s}"], writes=[f"MM0_{s}"])
            kb.op("pool", lambda e, s=s: e.tensor_tensor(out=T["Q"][0][0:CH, s, :], in0=T["AMf"][0:CH, s, :],
                                                        in1=ident64[0:CH, 0:CH], op=ALU.add),
                  reads=[f"AM{s}", "c_ident"], writes=[f"Q0_{s}"])
            yield
        for lv in range(NLV):
            a, b = lv % 2, (lv + 1) % 2
            last = lv == NLV - 1
            for ci in grp:
                s = ci % GB + GB * par
                pm = ps[2 + s % 2]
                pmk = f"ps{2 + s % 2}"
                MMa = T["MM"][a]
                if not last:
                    kb.op("pe", lambda e, s=s, pm=pm, MMa=MMa: e.matmul(
                        pm[0:CH, 0:CH], lhsT=MMa[0:CH, s, CH:2 * CH], rhs=MMa[0:CH, s, 0:CH], start=True, stop=True),
                        reads=[f"MM{a}_{s}"], writes=[pmk])
                kb.op("pe", lambda e, s=s, pm=pm, MMa=MMa: e.matmul(
                    pm[0:CH, CH:2 * CH], lhsT=MMa[0:CH, s, 0:CH], rhs=MMa[0:CH, s, CH:2 * CH], start=True, stop=True),
                    reads=[f"MM{a}_{s}"], writes=[pmk])
                lo = CH if last else 0
                kb.op("act", lambda e, s=s, pm=pm, b=b, lo=lo: e.copy(out=T["MM"][b][0:CH, s, lo:2 * CH],
                                                                     in_=pm[0:CH, lo:2 * CH]),
                      reads=[pmk], writes=[f"MM{b}_{s}"])
                yield
            for ci in grp:
                s = ci % GB + GB * par
                pq = ps[4 + s % 2]
                pqk = f"ps{4 + s % 2}"
                kb.op("pe", lambda e, s=s, pq=pq, b=b, a=a: e.matmul(
                    pq[0:CH, 0:CH], lhsT=T["MM"][b][0:CH, s, CH:2 * CH], rhs=T["Q"][a][0:CH, s, :], start=True, stop=True),
                    reads=[f"MM{b}_{s}", f"Q{a}_{s}"], writes=[pqk])
                kb.op("dve", lambda e, s=s, pq=pq, a=a, b=b: e.tensor_tensor(
                    out=T["Q"][b][0:CH, s, :], in0=pq[0:CH, 0:CH], in1=T["Q"][a][0:CH, s, :], op=ALU.add),
                    reads=[pqk, f"Q{a}_{s}"], writes=[f"Q{b}_{s}"])
                yield

    def chain_gen(g0):
        grp = list(range(g0, min(NCH, g0 + GB)))
        par = (g0 // GB) % 2
        QF = T["Q"][NLV % 2]
        for ci in grp:
            s = ci % GB + GB * par
            c0 = CHUNK_COLS[ci]
            cs = slice(c0, c0 + CH)
            AMb = T["AMb"]
            STb = T["STb"]
            pt = ps[6][:].bitcast(BF16) if tb else ps[6]
            idt = c.identb if tb else ident
            for j, src in enumerate((vT, kT, bT)):
                kb.op("pe", lambda e, j=j, src=src, cs=cs, pt=pt: e.transpose(
                    out=pt[0:CH, j * dk:(j + 1) * dk], in_=src[bp:bp + dk, cs], identity=idt[bp:bp + dk, bp:bp + dk]),
                    reads=[hk_, "c_ident", "c_identb"], writes=["ps6"])
            TM = T["TM"][ci % 2]
            tmk = f"TM{ci % 2}"
            kb.op("act", lambda e, TM=TM, pt=pt: e.copy(out=TM[0:CH, 0:3 * dk], in_=pt[0:CH, 0:3 * dk]),
                  reads=["ps6"], writes=[tmk])
            yield
            Vtm, Ktm, Btm = TM[0:CH, 0:dk], TM[0:CH, dk:2 * dk], TM[0:CH, 2 * dk:3 * dk]
            if Gb is not None:
                kb.op("dve", lambda e, TM=TM, s=s: e.tensor_scalar(out=TM[0:CH, dk:3 * dk], in0=TM[0:CH, dk:3 * dk],
                                                                 scalar1=T["dL"][0:CH, s:s + 1], scalar2=None, op0=ALU.mult),
                      reads=[tmk, f"dL{s}"], writes=[tmk])
            pr = ps[7]
            kb.op("pe", lambda e, cs=cs, pr=pr: e.matmul(pr[0:CH, 0:dk], lhsT=kkC[bp:bp + dk, cs], rhs=STb[bp:bp + dk, 0:dk],
                                                       start=True, stop=False), reads=[hk_, "STb"], writes=["ps7"])
            kb.op("pe", lambda e, s=s, pr=pr, Vtm=Vtm: e.matmul(pr[0:CH, 0:dk], lhsT=AMb[0:CH, s, 0:CH], rhs=Vtm,
                                                              start=False, stop=True),
                  reads=[f"AMb{s}", tmk], writes=["ps7"])
            yield
            kb.op("dve", lambda e, pr=pr: e.tensor_scalar(out=T["nR"][0:CH, 0:dk], in0=pr[0:CH, 0:dk], scalar1=-1.0,
                                                         scalar2=None, op0=ALU.mult), reads=["ps7"], writes=["nR"])
            yield
            kb.op("pe", lambda e, s=s, pr=pr: e.matmul(pr[0:CH, 128:128 + dk], lhsT=QF[0:CH, s, :], rhs=T["nR"][0:CH, 0:dk],
                                                     start=True, stop=True), reads=[f"Q{NLV % 2}_{s}", "nR"], writes=["ps7"])
            yield
            kb.op("act", lambda e, pr=pr: e.copy(out=T["U"][0:CH, 0:dk], in_=pr[0:CH, 128:128 + dk]),
                  reads=["ps7"], writes=["U"])
            yield
            U = T["U"]
            kb.op("pe", lambda e, cs=cs, pr=pr: e.matmul(pr[0:CH, 256:256 + dk], lhsT=rC[bp:bp + dk, cs], rhs=STb[bp:bp + dk, 0:dk],
                                                       start=True, stop=False), reads=[hk_, "STb"], writes=["ps7"])
            kb.op("pe", lambda e, s=s, pr=pr: e.matmul(pr[0:CH, 256:256 + dk], lhsT=AMb[0:CH, s, CH:2 * CH],
                                                     rhs=U[0:CH, 0:dk], start=False, stop=False),
                  reads=[f"AMb{s}", "U"], writes=["ps7"])
            kb.op("pe", lambda e, s=s, pr=pr, Vtm=Vtm: e.matmul(pr[0:CH, 256:256 + dk], lhsT=AMb[0:CH, s, 2 * CH:3 * CH],
                                                              rhs=Vtm, start=False, stop=True),
                  reads=[f"AMb{s}", tmk], writes=["ps7"])
            kb.op("pe", lambda e, pr=pr, Btm=Btm: e.matmul(pr[bp:bp + dk, 384:384 + dk], lhsT=Btm, rhs=U[0:CH, 0:dk],
                                                         start=True, stop=False), reads=[tmk, "U"], writes=["ps7"])
            kb.op("pe", lambda e, pr=pr, Ktm=Ktm, Vtm=Vtm: e.matmul(pr[bp:bp + dk, 384:384 + dk], lhsT=Ktm, rhs=Vtm,
                                                                  start=False, stop=True),
                  reads=[tmk], writes=["ps7"])
            yield
            Ysb = T["Y"][ci % 2]
            yk = f"Y{ci % 2}"
            kb.op("act", lambda e, pr=pr, Ysb=Ysb: e.copy(out=Ysb[0:CH, 0:dk], in_=pr[0:CH, 256:256 + dk]),
                  reads=["ps7"], writes=[yk])
            if Gb is not None:
                kb.op("dve", lambda e, pr=pr, c0=c0: e.scalar_tensor_tensor(
                    out=ST[bp:bp + dk, 0:dk], in0=ST[bp:bp + dk, 0:dk], scalar=Pinc[bp:bp + dk, c0 + CH - 1:c0 + CH],
                    in1=pr[bp:bp + dk, 384:384 + dk], op0=ALU.mult, op1=ALU.add), reads=["ps7", "ST", hk_], writes=["ST"])
            else:
                kb.op("dve", lambda e, pr=pr: e.tensor_tensor(out=ST[bp:bp + dk, 0:dk], in0=pr[bp:bp + dk, 384:384 + dk],
                                                             in1=ST[bp:bp + dk, 0:dk], op=ALU.add),
                      reads=["ps7", "ST"], writes=["ST"])
                kb.op("dve", lambda e, c0=c0: e.tensor_scalar(out=ST[bp:bp + dk, 0:dk], in0=ST[bp:bp + dk, 0:dk],
                                                             scalar1=Pinc[bp:bp + dk, c0 + CH - 1:c0 + CH], scalar2=None,
                                                             op0=ALU.mult), reads=["ST", hk_], writes=["ST"])
            kb.op("act", lambda e: e.copy(out=T["STb"][bp:bp + dk, 0:dk], in_=ST[bp:bp + dk, 0:dk]),
                  reads=["ST"], writes=["STb"])
            if prodT is not None:
                pb = ps[6]
                kb.op("pe", lambda e, cs=cs, pb=pb: e.matmul(pb[0:CH, 448:449], lhsT=prodT[bp:bp + dk, cs],
                                                           rhs=c.onesb[bp:bp + dk, 0:1], start=True, stop=True),
                      reads=[hk_, "c_ones"], writes=["ps6"])
                kb.op("dve", lambda e, pb=pb, Ysb=Ysb, Vtm=Vtm: e.tensor_scalar(
                    out=Ysb[0:CH, dk:2 * dk], in0=Vtm, scalar1=pb[0:CH, 448:449], scalar2=None, op0=ALU.mult),
                    reads=["ps6", tmk], writes=[yk])
            store_cb(ci, Ysb, yk)
            yield

    from itertools import zip_longest
    for _ in inv_gen(0):
        pass
    for g0 in range(0, NCH, GB):
        gens = [chain_gen(g0)]
        if g0 + GB < NCH:
            gens.append(inv_gen(g0 + GB))
        live = list(gens)
        while live:
            for gi, g_ in enumerate(list(live)):
                for _ in range(1 if g_ is gens[0] else 2):
                    try:
                        next(g_)
                    except StopIteration:
                        if g_ in live:
                            live.remove(g_)
                        break


def mixer_stage(kb, nc, g, c, mods, x_lat_in, x_ctx_in, x_lat_out, x_ctx_out):
    from contextlib import ExitStack
    mod = mods[0]
    Ys = g["ys_scr"]
    Zs = g["z_scr"]
    with ExitStack() as es:
        def sb(name, shape, dt):
            return es.enter_context(nc.sbuf_tensor(f"mxs_{name}", shape, dt))
        hT = None
        es2 = ExitStack()

        def sb2(name, shape, dt):
            return es2.enter_context(nc.sbuf_tensor(f"mxs_{name}", shape, dt))
        hb = sb("hb", [128, D], BF16)
        stt = [sb("st0", [128, 4], F32), sb("st1", [128, 4], F32)]
        F11 = sb("F11", [128, TP], F32)
        Jb = sb("Jb", [128, 128], BF16)
        J32 = sb("J32", [128, 128], F32)
        par = sb("par", [64, 64], F32)
        parP = sb("parP", [128, 64], F32)
        bd = sb("bd", [128, 128], F32)
        par128 = sb("par128", [128, 112], F32)
        w2sb = sb("w2sb", [128, 2, 512], F32)
        sel = sb("sel", [16, 16, 128], F32)
        ST = sb("ST", [128, 128], F32)
        hT = es2.enter_context(nc.sbuf_tensor("mxs_hT", [128, 8, 2304], BF16))
        F = [sb2(f"F{i}", [128, TP], F32) for i in range(11)] + [F11]
        FK = [f"F{i}" for i in range(12)]
        Gbc = F[1][:, 0:D]
        Sbc = F[2][:, 0:D]
        h32 = F[3][:, 0:D]
        xb = [F[4][:, 0:D], F[5][:, 0:D]]
        h32s = [F[3][:, 0:D], F[6][:, 0:D]]
        hb2 = sb2("hb2", [128, D], BF16)
        hbs = [hb, hb2]
        rmask = sb2("rmask", [128, TP], BF16)
        rm32 = F[0]
        wsl = sb2("wsl", [128, 8, 128], BF16)
        T = {"ST": ST,
             "AMf": sb2("AMf", [CHUNK, 4, CHUNK], F32), "AMb": sb2("AMb", [CHUNK, 4, 3 * CHUNK], BF16),
             "STb": sb2("STb", [128, 128], BF16),
             "MM": [sb2("MMa", [CHUNK, 4, 2 * CHUNK], F32), sb2("MMb", [CHUNK, 4, 2 * CHUNK], F32)],
             "Q": [sb2("Qa", [CHUNK, 4, CHUNK], F32), sb2("Qb", [CHUNK, 4, CHUNK], F32)],
             "TM": [sb2("TMa", [CHUNK, 384], BF16), sb2("TMb", [CHUNK, 384], BF16)],
             "nR": sb2("nR", [CHUNK, 128], F32), "U": sb2("U", [CHUNK, 128], BF16), "Uf": sb2("Uf", [16, 4], F32),
             "Y": [sb2("Ya", [CHUNK, 256], F32), sb2("Yb", [CHUNK, 256], F32)],
             "maskA": sb2("maskA", [CHUNK, 4 * CHUNK], F32), "maskT": sb2("maskT", [CHUNK, CHUNK], F32),
             "Gc": sb2("Gc", [CHUNK, 4], F32), "dL": sb2("dL", [CHUNK, 4], F32), "Dt": sb2("Dt", [CHUNK, 2, CHUNK], F32),
             "Dts": sb2("Dts", [CHUNK, 2, CHUNK], F32), "mD": sb2("mD", [CHUNK, 2, 4 * CHUNK], F32)}
        ps = [es.enter_context(nc.psum_tensor(f"mx_ps{i}", [128, 512], F32)) for i in range(8)]
        T["ps"] = ps
        kb.dma("sp", rm32[:], g["k_rmask"][:, :], writes=["F0"])
        kb.op("dve", lambda e: e.tensor_copy(out=rmask[:], in_=rm32[:]), reads=["F0"], writes=["rmask"])
        kb.dma("sp", J32[:], g["k_J"][:, :], writes=["J32"])
        kb.op("dve", lambda e: e.tensor_copy(out=Jb[:], in_=J32[:]), reads=["J32"], writes=["Jb"])
        kb.dma("sp", par[:], g["mx_par64"][:, :], writes=["par"])
        kb.dma("sp", par128[:], g["mx_par128"][:, :], writes=["par128"])
        kb.dma("sp", w2sb[:], g["mx_w2"][:, :, :], writes=["w2sb"])
        kb.dma("sp", parP[:], g["mx_parP"][:, :], writes=["parP"])
        kb.dma("sp", bd[:], g["k_bd"][:, :], writes=["bd"])
        kb.op("dve", lambda e: e.tensor_scalar(out=parP[:, 36:40], in0=parP[:, 32:36], scalar1=-1.0, scalar2=1.0,
                                               op0=ALU.mult, op1=ALU.add), reads=["parP"], writes=["parP"])
        kb.op("dve", lambda e: e.tensor_scalar(out=par128[:, 24:32], in0=par128[:, 16:24], scalar1=-1.0, scalar2=1.0,
                                               op0=ALU.mult, op1=ALU.add), reads=["par128"], writes=["par128"])
        T["zt"] = [sb2("zta", [128, 128], F32), sb2("ztb", [128, 128], F32)]
        kb.dma("sp", sel[:], g["k_sel"][:, :, :], writes=["sel"])
        kb.dma("sp", T["maskA"][:], g["k_maskA"][:, :], writes=["maskA"])
        kb.dma("sp", T["maskT"][:], g["k_maskT"][:, :], writes=["maskT"])
        for f in range(12):
            kb.op("pool", lambda e, f=f: e.memset(F[f][:], 0.0), reads=["rmask"] if f == 0 else [], writes=[FK[f]])
        wv = g["e_w_in2"].rearrange("(kc p) n -> p kc n", p=128)
        P64 = lambda j: par[:, j:j + 1]

        def project(c0, M, dst, dk_, evac_eng="act"):
            kb.dma("pool", wsl[:, :, 0:M], wv[:, :, c0:c0 + M], writes=["wsl"])
            for bi, (t0, tw, col0) in enumerate(BLOCKS):
                pb = ps[bi % 2]
                for kc in range(8):
                    kb.op("pe", lambda e, kc=kc, t0=t0, tw=tw, pb=pb: e.matmul(
                        pb[0:M, 0:tw], lhsT=wsl[:, kc, 0:M], rhs=hT[:, kc, t0:t0 + tw], start=(kc == 0), stop=(kc == 7)),
                        reads=["wsl", "hT"], writes=[f"ps{bi % 2}"])
                kb.op("act", lambda e, tw=tw, col0=col0, pb=pb: e.copy(out=dst[0:M, col0:col0 + tw], in_=pb[0:M, 0:tw]),
                      reads=[f"ps{bi % 2}"], writes=[dk_])

        def tshift(src, sk, dst, dk_, tmp, tk, P, mucol):
            n = TP - 2
            kb.op("dve", lambda e: e.tensor_tensor(out=tmp[0:P, 1:1 + n], in0=src[0:P, 0:n], in1=src[0:P, 2:2 + n],
                                                   op=ALU.add), reads=[sk], writes=[tk])
            kb.op("dve", lambda e: e.scalar_tensor_tensor(out=tmp[0:P, 1:1 + n], in0=tmp[0:P, 1:1 + n], scalar=0.5,
                                                          in1=src[0:P, 1:1 + n], op0=ALU.mult, op1=ALU.subtract),
                  reads=[sk, tk], writes=[tk])
            kb.op("dve", lambda e: e.scalar_tensor_tensor(out=dst[0:P, 1:1 + n], in0=tmp[0:P, 1:1 + n], scalar=mucol,
                                                         in1=src[0:P, 1:1 + n], op0=ALU.mult, op1=ALU.add),
                  reads=[sk, tk, "par", "par128"], writes=[dk_])

        DC = [(CTX0, 256), (LAT0, 2048)]

        def ew(eng, fn, reads, writes):
            kb.op(eng, fn, reads=reads, writes=writes)

        for d in range(2):
            for stream in (1, 0):
                kb.barrier()
                make_bc(kb, nc, c, lambda kc: mod[:, 1 * 8 + kc, stream:stream + 1], Gbc, ps[0], "Gbc", ["mod0"])
                make_bc(kb, nc, c, lambda kc: mod[:, 0 * 8 + kc, stream:stream + 1], Sbc, ps[0], "Sbc", ["mod0"])
                nt = 2 if stream == 1 else 16
                src = x_ctx_in if stream == 1 else x_lat_in
                base = 0 if stream == 1 else 256
                def htile(i):
                    b = i % 2
                    hbb, h32b = hbs[b], h32s[b]
                    kb.dma("sp", xb[b][:], src[i * 128:(i + 1) * 128, :], writes=[f"xb{b}"])
                    yield
                    yield from norm_tile_g(kb, nc, xb[b][:], f"xb{b}", stt[b], Gbc, Sbc, h32b, f"h32{b}")
                    yield
                    kb.op("act", lambda e: e.copy(out=hbb[:], in_=h32b), reads=[f"h32{b}"], writes=[f"hb{b}"])
                    yield
                    pos = base + (i if d == 0 else nt - 1 - i) * 128
                    for half in range(2):
                        pbk = 2 + half + 2 * b
                        for q in range(4):
                            kc = half * 4 + q
                            kb.op("pe", lambda e, kc=kc, q=q, pbk=pbk: e.matmul(
                                ps[pbk][:, q * 128:(q + 1) * 128], lhsT=hbb[:, kc * 128:(kc + 1) * 128],
                                rhs=(c.identb[:] if d == 0 else Jb[:]), start=True, stop=True),
                                reads=[f"hb{b}", "c_identb", "Jb"], writes=[f"ps{pbk}"])
                        yield
                        kb.op("dve" if half == 0 else "act", (lambda e, half=half, pos=pos, pbk=pbk: e.tensor_copy(
                            out=hT[:, half * 4:(half + 1) * 4, pos:pos + 128],
                            in_=ps[pbk][:].rearrange("p (q t) -> p q t", q=4))) if half == 0 else
                            (lambda e, half=half, pos=pos, pbk=pbk: e.copy(
                                out=hT[:, half * 4:(half + 1) * 4, pos:pos + 128],
                                in_=ps[pbk][:].rearrange("p (q t) -> p q t", q=4))),
                            reads=[f"ps{pbk}"], writes=[("hT", pos, half)])
                        yield
                pending = [htile(i) for i in range(nt)]
                active = []
                while pending or active:
                    if pending and len(active) < 2:
                        active.append(pending.pop(0))
                    for g_ in list(active):
                        try:
                            next(g_)
                        except StopIteration:
                            active.remove(g_)
            kb.barrier()

            def store_cb_factory(col0, dk_):
                def cb(ci, Ysb, yk):
                    r0 = ci * CHUNK
                    kb.dma("sp", Ys[d, r0:r0 + CHUNK, col0:col0 + dk_], Ysb[0:CHUNK, 0:dk_], reads=[yk],
                           writes=[("ys", d, ci, col0)])
                    if col0 < 512:
                        kb.dma("sp", Ys[d, r0:r0 + CHUNK, 1024 + col0:1024 + col0 + dk_], Ysb[0:CHUNK, dk_:2 * dk_],
                               reads=[yk], writes=[("ysb", d, ci, col0)])
                return cb

            project(1536 + d * 128, 128, F[0], FK[0])
            tshift(F[0], FK[0], F[10], FK[10], F[1], FK[1], 128, par128[:, d:d + 1])
            ew("act", lambda e: e.activation(out=F[10][0:64, :], in_=F[10][0:64, :], func=AF.Tanh), [FK[10]], [FK[10]])
            if d == 0:
                project(1792, 128, F[0], FK[0])
                tshift(F[0], FK[0], F[11], FK[11], F[1], FK[1], 128, par128[:, 2:3])
                ew("act", lambda e: e.activation(out=F[11][:], in_=F[11][:], func=AF.Sigmoid), [FK[11]], [FK[11]])
            for p in range(4):
                hk_ = f"pair{d}_{p}"
                PP = lambda j: parP[:, j:j + 1]
                for part, dst in ((0, 1), (1, 2), (2, 3)):
                    project(p * 384 + part * 128, 128, F[0], FK[0])
                    tshift(F[0], FK[0], F[dst], FK[dst], F[7], FK[7], 128, PP(p * 3 + part))
                for bi, (t0, tw, col0) in enumerate(BLOCKS):
                    cs = slice(col0, col0 + tw)
                    kb.op("pe", lambda e, cs=cs, tw=tw: e.matmul(ps[2][:, 0:tw], lhsT=w2sb[0:64, d, p * 128:(p + 1) * 128],
                                                               rhs=F[10][0:64, cs], start=True, stop=True),
                          reads=["w2sb", FK[10]], writes=["ps2"])
                    kb.op("pe", lambda e, cs=cs, tw=tw: e.matmul(ps[3][:, 0:tw], lhsT=w2sb[64:128, d, p * 128:(p + 1) * 128],
                                                               rhs=F[10][64:128, cs], start=True, stop=True),
                          reads=["w2sb", FK[10]], writes=["ps3"])
                    kb.op("act", lambda e, cs=cs, tw=tw: e.activation(out=F[4][:, cs], in_=ps[2][:, 0:tw],
                                                                    func=AF.Sigmoid, bias=PP(12 + d * 4 + p)),
                          reads=["ps2", "parP"], writes=[FK[4]])
                    kb.op("act", lambda e, cs=cs, tw=tw: e.activation(out=F[5][:, cs], in_=ps[3][:, 0:tw],
                                                                    func=AF.Sigmoid, bias=PP(20 + d * 4 + p)),
                          reads=["ps3", "parP"], writes=[FK[5]])
                kkc, kac, kac1, rkc = PP(28 + p), PP(32 + p), PP(36 + p), PP(40 + p)
                ew("dve", lambda e: e.tensor_scalar(out=F[7][:, :], in0=F[2][:, :], scalar1=kkc, scalar2=None,
                                                    op0=ALU.mult), [FK[2], "parP"], [FK[7]])
                for (a0_, n_) in DC:
                    ew("pool", lambda e, a0_=a0_, n_=n_: e.tensor_tensor(
                        out=F[0][:, a0_:a0_ + n_], in0=F[7][:, a0_:a0_ + n_], in1=F[7][:, a0_:a0_ + n_],
                        op=ALU.mult), [FK[7]], [FK[0]])
                for bi, (t0, tw, col0) in enumerate(BLOCKS):
                    cs = slice(col0, col0 + tw)
                    kb.op("pe", lambda e, cs=cs, tw=tw: e.matmul(ps[2][:, 0:tw], lhsT=bd[:, :],
                                                               rhs=F[0][:, cs], start=True, stop=True),
                          reads=["bd", FK[0]], writes=["ps2"])
                    kb.op("act", lambda e, cs=cs, tw=tw: e.activation(out=F[9][:, cs], in_=ps[2][:, 0:tw],
                                                                    func=AF.Sqrt, bias=c.eps6[:, 0:1]),
                          reads=["ps2", "c_eps"], writes=[FK[9]])
                for (a0_, n_) in DC:
                    cs = slice(a0_, a0_ + n_)
                    ew("dve", lambda e, cs=cs: e.reciprocal(out=F[9][:, cs], in_=F[9][:, cs]), [FK[9]], [FK[9]])
                    ew("dve", lambda e, cs=cs: e.tensor_tensor(out=F[6][:, cs], in0=F[7][:, cs], in1=F[9][:, cs],
                                                               op=ALU.mult), [FK[7], FK[9]], [FK[6]])
                    ew("pool", lambda e, cs=cs: e.tensor_scalar(out=F[7][:, cs], in0=F[5][:, cs], scalar1=kac,
                                                                scalar2=kac1, op0=ALU.mult, op1=ALU.add),
                       [FK[5], "parP"], [FK[7]])
                    ew("pool", lambda e, cs=cs: e.tensor_tensor(out=F[7][:, cs], in0=F[7][:, cs], in1=F[2][:, cs],
                                                                op=ALU.mult), [FK[7], FK[2]], [FK[7]])
                    ew("dve", lambda e, cs=cs: e.tensor_tensor(out=F[9][:, cs], in0=F[6][:, cs], in1=F[5][:, cs],
                                                               op=ALU.mult), [FK[6], FK[5]], [FK[9]])
                    ew("dve", lambda e, cs=cs: e.scalar_tensor_tensor(out=F[0][:, cs], in0=F[1][:, cs], scalar=rkc,
                                                                      in1=F[7][:, cs], op0=ALU.mult, op1=ALU.mult),
                       [FK[1], FK[7], "parP"], [FK[0]])
                ew("dve", lambda e: e.tensor_tensor_scan(out=F[8][:, :], data0=rmask[:, :], data1=F[4][:, :],
                                                         initial=0.0, op0=ALU.mult, op1=ALU.add),
                   ["rmask", FK[4]], [FK[8]])
                B4 = F[4][:, :].bitcast(BF16)
                B5 = F[5][:, :].bitcast(BF16)
                B2 = F[2][:, :].bitcast(BF16)
                DCS = [slice(a0_, a0_ + n_) for (a0_, n_) in DC]
                DCH = [slice(TP + a0_, TP + a0_ + n_) for (a0_, n_) in DC]
                for cs in DCS:
                    ew("pool", lambda e, cs=cs: e.tensor_tensor(out=F[2][:, cs], in0=F[8][:, cs], in1=F[4][:, cs],
                                                                op=ALU.subtract), [FK[8], FK[4]], [FK[2]])
                    ew("act", lambda e, cs=cs: e.activation(out=F[2][:, cs], in_=F[2][:, cs], func=AF.Exp,
                                                            scale=-DECAY_K), [FK[2]], [FK[2]])
                for cs in DCS:
                    ew("dve", lambda e, cs=cs: e.tensor_tensor(out=B4[:, cs], in0=F[6][:, cs], in1=F[2][:, cs],
                                                               op=ALU.mult), [FK[6], FK[2]], [FK[4]])
                for cs in DCS:
                    ew("act", lambda e, cs=cs: e.activation(out=F[2][:, cs], in_=F[8][:, cs], func=AF.Exp,
                                                            scale=DECAY_K), [FK[8], FK[2], FK[4]], [FK[2]])
                for cs, ch in zip(DCS, DCH):
                    ew("dve", lambda e, cs=cs, ch=ch: e.tensor_tensor(out=B4[:, ch], in0=F[7][:, cs], in1=F[2][:, cs],
                                                                      op=ALU.mult), [FK[7], FK[2]], [FK[4]])
                    ew("pool", lambda e, cs=cs: e.tensor_tensor(out=B5[:, cs], in0=F[9][:, cs], in1=F[2][:, cs],
                                                                op=ALU.mult), [FK[9], FK[2]], [FK[5]])
                for cs in DCS:
                    ew("act", lambda e, cs=cs: e.activation(out=F[8][:, cs], in_=F[8][:, cs], func=AF.Exp,
                                                            scale=-DECAY_K), [FK[8]], [FK[8]])
                for cs, ch in zip(DCS, DCH):
                    ew("dve", lambda e, cs=cs: e.tensor_tensor(out=B2[:, cs], in0=F[1][:, cs], in1=F[8][:, cs],
                                                               op=ALU.mult), [FK[1], FK[8], FK[4], FK[5]], [FK[2]])
                    ew("pool", lambda e, cs=cs, ch=ch: e.tensor_copy(out=B5[:, ch], in_=F[3][:, cs]), [FK[3]], [FK[5]])
                    ew("act", lambda e, cs=cs, ch=ch: e.copy(out=B2[:, ch], in_=F[0][:, cs]), [FK[0]], [FK[2]])
                HI = lambda B: B[:, TP:2 * TP]
                kb.op("pool", lambda e: e.memset(T["nR"][:], 0.0),
                      reads=[FK[2], FK[4], FK[5], FK[8]], writes=[hk_, "nR"])
                for hh in range(2):
                    dplr_scan(kb, nc, c, T, 64, B2[:, 0:TP], B4[:, 0:TP], HI(B4), B5[:, 0:TP], HI(B5), F[8], HI(B2),
                              store_cb_factory((2 * p + hh) * 64, 64), hk_, bp=hh * 64)
                kb.op("pool", lambda e: e.memset(T["nR"][:], 0.0), reads=[hk_],
                      writes=[FK[2], FK[4], FK[5], FK[8], "nR"])
            mixer_gdn_pass(kb, nc, g, c, T, d, F, FK, rmask, par128, sel, project, store_cb_factory, ps, DC, wv, wsl,
                           hT, Zs)
        kb.barrier()
        es2.close()
        import os
        if os.environ.get("MIX_DUMP"):
            kb.dma("sp", x_lat_out[0:2048, :], Ys[0, 0:2048, 0:1024], writes=["o1"])
            kb.dma("sp", x_ctx_out[0:256, :], Ys[0, 0:256, 512:1536], writes=["o2"])
            kb.barrier()
            return
        mixer_output(kb, nc, g, c, mods, T, F, FK, x_lat_in, x_ctx_in, x_lat_out, x_ctx_out, Ys, Zs, J32, None, None, hb,
                     ps, par128)
        kb.barrier()


def mixer_gdn_pass(kb, nc, g, c, T, d, F, FK, rmask, par128, sel, project, store_cb_factory, ps, DC, wv, wsl, hT, Zs):
    ew = lambda eng, fn, r, w: kb.op(eng, fn, reads=r, writes=w)
    project(3968, 16, F[10], FK[10])
    R16 = lambda i: F[i][0:16, :]
    ew("act", lambda e: e.activation(out=T["Uf"][0:16, 0:1], in_=par128[0:16, 41:42], func=AF.Exp), ["par128"], ["U"])
    ew("dve", lambda e: e.tensor_scalar(out=T["Uf"][0:16, 0:1], in0=T["Uf"][0:16, 0:1], scalar1=-1.0, scalar2=None,
                                        op0=ALU.mult), ["U"], ["U"])
    ew("dve", lambda e: e.tensor_scalar(out=R16(1), in0=R16(10), scalar1=par128[0:16, 40:41], scalar2=None, op0=ALU.add),
       [FK[10], "par128"], [FK[1]])
    ew("act", lambda e: e.activation(out=R16(4), in_=R16(10), func=AF.Sigmoid), [FK[10]], [FK[4]])
    ew("act", lambda e: e.activation(out=R16(2), in_=R16(1), func=AF.Abs), [FK[1]], [FK[2]])
    ew("act", lambda e: e.activation(out=R16(2), in_=R16(2), func=AF.Exp, scale=-1.0), [FK[2]], [FK[2]])
    ew("dve", lambda e: e.tensor_scalar(out=R16(3), in0=R16(2), scalar1=2.0, scalar2=None, op0=ALU.add), [FK[2]], [FK[3]])
    ew("dve", lambda e: e.reciprocal(out=R16(3), in_=R16(3)), [FK[3]], [FK[3]])
    ew("dve", lambda e: e.tensor_tensor(out=R16(2), in0=R16(2), in1=R16(3), op=ALU.mult), [FK[2], FK[3]], [FK[2]])
    ew("dve", lambda e: e.tensor_tensor(out=R16(3), in0=R16(2), in1=R16(2), op=ALU.mult), [FK[2]], [FK[3]])
    ew("dve", lambda e: e.tensor_scalar(out=R16(6), in0=R16(3), scalar1=1.0 / 13, scalar2=1.0 / 11, op0=ALU.mult,
                                        op1=ALU.add), [FK[3]], [FK[6]])
    for cf in (1.0 / 9, 1.0 / 7, 1.0 / 5, 1.0 / 3, 1.0):
        ew("dve", lambda e: e.tensor_tensor(out=R16(6), in0=R16(6), in1=R16(3), op=ALU.mult), [FK[6], FK[3]], [FK[6]])
        ew("dve", lambda e, cf=cf: e.tensor_scalar(out=R16(6), in0=R16(6), scalar1=cf, scalar2=None, op0=ALU.add),
           [FK[6]], [FK[6]])
    ew("dve", lambda e: e.scalar_tensor_tensor(out=R16(6), in0=R16(6), scalar=2.0, in1=R16(2), op0=ALU.mult, op1=ALU.mult),
       [FK[6], FK[2]], [FK[6]])
    ew("dve", lambda e: e.tensor_scalar(out=R16(1), in0=R16(1), scalar1=0.0, scalar2=None, op0=ALU.max), [FK[1]], [FK[1]])
    ew("dve", lambda e: e.tensor_tensor(out=R16(6), in0=R16(6), in1=R16(1), op=ALU.add), [FK[6], FK[1]], [FK[6]])
    ew("dve", lambda e: e.tensor_scalar(out=R16(6), in0=R16(6), scalar1=T["Uf"][0:16, 0:1], scalar2=None, op0=ALU.mult),
       [FK[6], "U"], [FK[6]])
    ew("dve", lambda e: e.tensor_tensor_scan(out=R16(10), data0=rmask[0:16, :], data1=R16(6), initial=0.0,
                                             op0=ALU.mult, op1=ALU.add), ["rmask", FK[6]], [FK[10]])
    for h in range(4):
        hk_ = f"ghead{d}_{h}"
        c0 = 1920 + h * 512
        for part, dst in ((0, 1), (1, 2), (2, 3)):
            project(c0 + part * 128, 128, F[0], FK[0])
            n = TP - 4
            for j in range(5):
                jj = j if d == 0 else 4 - j
                wc = par128[:, 48 + (h * 3 + part) * 5 + jj:49 + (h * 3 + part) * 5 + jj]
                if j == 0:
                    ew("dve", lambda e, wc=wc, dst=dst: e.tensor_scalar(out=F[dst][:, 2:2 + n], in0=F[0][:, 0:n],
                                                                      scalar1=wc, scalar2=None, op0=ALU.mult),
                       [FK[0], "par128"], [FK[dst]])
                else:
                    ew("dve", lambda e, wc=wc, dst=dst, j=j: e.scalar_tensor_tensor(
                        out=F[dst][:, 2:2 + n], in0=F[0][:, j:j + n], scalar=wc, in1=F[dst][:, 2:2 + n],
                        op0=ALU.mult, op1=ALU.add), [FK[0], FK[dst], "par128"], [FK[dst]])
            ew("act", lambda e, dst=dst: e.activation(out=F[dst][:, :], in_=F[dst][:, :], func=AF.Silu), [FK[dst]], [FK[dst]])
        for src, scl in ((1, float(128 ** -0.5)), (2, 1.0)):
            for (a0_, n_) in DC:
                ew("pool", lambda e, src=src, a0_=a0_, n_=n_: e.tensor_tensor(
                    out=F[0][:, a0_:a0_ + n_], in0=F[src][:, a0_:a0_ + n_], in1=F[src][:, a0_:a0_ + n_], op=ALU.mult),
                    [FK[src]], [FK[0]])
            for bi, (t0, tw, col0) in enumerate(BLOCKS):
                cs = slice(col0, col0 + tw)
                kb.op("pe", lambda e, cs=cs, tw=tw: e.matmul(ps[2][:, 0:tw], lhsT=c.ones[:, :], rhs=F[0][:, cs],
                                                           start=True, stop=True), reads=["c_ones", FK[0]], writes=["ps2"])
                kb.op("act", lambda e, cs=cs, tw=tw: e.activation(out=F[9][:, cs], in_=ps[2][:, 0:tw], func=AF.Sqrt,
                                                                bias=c.eps6[:, 0:1]), reads=["ps2", "c_eps"], writes=[FK[9]])
            for (a0_, n_) in DC:
                cs = slice(a0_, a0_ + n_)
                ew("dve", lambda e, cs=cs: e.reciprocal(out=F[9][:, cs], in_=F[9][:, cs]), [FK[9]], [FK[9]])
                ew("dve", lambda e, cs=cs, src=src, scl=scl: e.scalar_tensor_tensor(
                    out=F[src][:, cs], in0=F[src][:, cs], scalar=scl, in1=F[9][:, cs], op0=ALU.mult, op1=ALU.mult),
                    [FK[src], FK[9]], [FK[src]])
        ra, rb = d * 4 + h, 8 + d * 4 + h
        for bi, (t0, tw, col0) in enumerate(BLOCKS):
            cs = slice(col0, col0 + tw)
            kb.op("pe", lambda e, cs=cs, tw=tw: e.matmul(ps[2][:, 0:tw], lhsT=sel[0:16, ra, :], rhs=F[10][0:16, cs],
                                                       start=True, stop=True), reads=["sel", FK[10]], writes=["ps2"])
            kb.op("pe", lambda e, cs=cs, tw=tw: e.matmul(ps[3][:, 0:tw], lhsT=sel[0:16, rb, :], rhs=F[4][0:16, cs],
                                                       start=True, stop=True), reads=["sel", FK[4]], writes=["ps3"])
            kb.op("act", lambda e, cs=cs, tw=tw: e.activation(out=F[6][:, cs], in_=ps[2][:, 0:tw], func=AF.Exp),
                  reads=["ps2"], writes=[FK[6]])
            kb.op("dve", lambda e, cs=cs, tw=tw: e.tensor_copy(out=F[0][:, cs], in_=ps[2][:, 0:tw]),
                  reads=["ps2"], writes=[FK[0]])
            kb.op("act", lambda e, cs=cs, tw=tw: e.copy(out=F[9][:, cs], in_=ps[3][:, 0:tw]),
                  reads=["ps3"], writes=[FK[9]])
        GB5 = F[5][:, :].bitcast(BF16)
        for (a0_, n_) in DC:
            cs = slice(a0_, a0_ + n_)
            ew("dve", lambda e, cs=cs: e.tensor_tensor(out=F[9][:, cs], in0=F[9][:, cs], in1=F[2][:, cs], op=ALU.mult),
               [FK[9], FK[2]], [FK[9]])
            ew("pool", lambda e, cs=cs: e.tensor_tensor(out=GB5[:, cs], in0=F[2][:, cs], in1=F[6][:, cs], op=ALU.mult),
               [FK[2], FK[6]], [FK[5]])
            ew("dve", lambda e, cs=cs: e.tensor_tensor(out=GB5[:, TP + cs.start:TP + cs.stop], in0=F[1][:, cs],
                                                       in1=F[6][:, cs], op=ALU.mult),
               [FK[1], FK[6]], [FK[5]])
        kb.op("pool", lambda e: e.memset(T["nR"][:], 0.0), reads=[FK[0], FK[1], FK[2], FK[3], FK[5], FK[6], FK[9]],
              writes=[hk_, "nR"])
        dplr_scan(kb, nc, c, T, 128, F[1], F[2], F[9], F[9], F[3], F[6], None, store_cb_factory(512 + h * 128, 128), hk_,
                  Gb=F[0], rA=F[1], kkA=F[2], rC=GB5[:, TP:2 * TP], kkC=GB5[:, 0:TP])
        kb.op("pool", lambda e: e.memset(T["nR"][:], 0.0), reads=[hk_],
              writes=[FK[0], FK[1], FK[2], FK[3], FK[5], FK[6], FK[9], "nR"])
    if d == 0:
        n = 0
        for h in range(4):
            kb.dma("pool", wsl[:, :, 0:128], wv[:, :, 1920 + h * 512 + 384:1920 + h * 512 + 512], writes=["wsl"])
            for i in range(18):
                pb = ps[n % 2]
                for kc in range(8):
                    kb.op("pe", lambda e, kc=kc, i=i, pb=pb: e.matmul(pb[:, 0:128], lhsT=hT[:, kc, i * 128:(i + 1) * 128],
                                                                   rhs=wsl[:, kc, 0:128], start=(kc == 0), stop=(kc == 7)),
                          reads=["wsl", "hT"], writes=[f"ps{n % 2}"])
                zt = T["zt"][n % 2]
                kb.op("act", lambda e, pb=pb, zt=zt: e.activation(out=zt[:], in_=pb[:, 0:128], func=AF.Silu),
                      reads=[f"ps{n % 2}"], writes=[f"zt{n % 2}"])
                kb.dma("sp", Zs[i * 128:(i + 1) * 128, h * 128:(h + 1) * 128], zt[:], reads=[f"zt{n % 2}"],
                       writes=[("zs", i, h)])
                n += 1


def mixer_output(kb, nc, g, c, mods, T, F, FK, x_lat_in, x_ctx_in, x_lat_out, x_ctx_out, Ys, Zs, J32, xb, Gbc, hb, ps,
                 par128):
    ew = lambda eng, fn, r, w: kb.op(eng, fn, reads=r, writes=w)
    kb.barrier()
    from contextlib import ExitStack
    with ExitStack() as es:
        def sb(name, shape, dt):
            return es.enter_context(nc.sbuf_tensor(f"mo_{name}", shape, dt))
        wo = sb("wo", [128, 8, D], BF16)
        g2 = sb("g2", [128, 512], F32)
        bcp = sb("bcp", [128, 3, 512], F32)
        yfs = [sb(f"yf{j}", [128, 1536], F32) for j in range(2)]
        ybs = [sb(f"yb{j}", [128, 1536], F32) for j in range(2)]
        zts = [sb(f"zt{j}", [128, 512], F32) for j in range(2)]
        os_ = [sb(f"o{j}", [128, D], F32) for j in range(2)]
        t1s = [sb(f"t1{j}", [128, 512], F32) for j in range(2)]
        s8s = [sb(f"s8{j}", [128, 4, 8], F32) for j in range(2)]
        oTs = [sb(f"oT{j}", [128, 8, 128], BF16) for j in range(2)]
        hbs = [sb(f"hb{j}", [128, D], BF16) for j in range(2)]
        kb_real = kb

        class Defer:
            SHARED = ("ps", "J32", "bcp", "g2", "sgd", "Gbc", "c_", "wo", "xb", "F1")

            def __init__(self, b):
                self.q, self.b = [], b

            def kx(self, k):
                return k if (not isinstance(k, str) or k.startswith(self.SHARED)) else f"{k}#{self.b}"

            def op(self, e, fn, reads=(), writes=()):
                ent = ("op", (e, fn), dict(reads=[self.kx(k) for k in reads], writes=[self.kx(k) for k in writes]))
                pk = [k for k in writes if isinstance(k, str) and k.startswith("ps")]
                if e == "pe" and pk and self.q and isinstance(self.q[-1], list) and self.q[-1][0] == pk[0]:
                    self.q[-1].append(ent)
                elif e == "pe" and pk:
                    self.q.append([pk[0], ent])
                else:
                    self.q.append(ent)

            def dma(self, q, out, in_, reads=(), writes=(), **kw):
                self.q.append(("dma", (q, out, in_), dict(reads=[self.kx(k) for k in reads],
                                                           writes=[self.kx(k) for k in writes], **kw)))
        Gbc = sb("Gbc", [128, D], F32)
        xb = [sb("x0", [128, D], F32), sb("x1", [128, D], F32)]
        for kc in range(8):
            kb.dma("pool", wo[:, kc, :], g["e_w_out"][0].rearrange("(kc p) n -> p kc n", p=128)[:, kc, :], writes=["wo"])
        kb.dma("sp", g2[:], g["a_g2"][0], writes=["g2"])
        kb.dma("sp", bcp[:].rearrange("p a n -> p (a n)"), g["mx_bc"].partition_broadcast(128), writes=["bcp"])
        for stream in (1, 0):
            kb.barrier()
            make_bc(kb, nc, c, lambda kc: mods[0][:, 2 * 8 + kc, stream:stream + 1], Gbc, ps[0], "Gbc", ["mod0"])
            nt = 2 if stream == 1 else 16
            src = x_ctx_in if stream == 1 else x_lat_in
            dst = x_ctx_out if stream == 1 else x_lat_out
            base = 0 if stream == 1 else 256
            colbase = CTX0 if stream == 1 else LAT0
            def tile_body(kb, i):
                ew = lambda eng, fn, r, w: kb.op(eng, fn, reads=r, writes=w)
                b = i % 2
                yf, yb, zt, o, t1, s8, oT, hb = yfs[b], ybs[b], zts[b], os_[b], t1s[b], s8s[b], oTs[b], hbs[b]
                pb = 4 * b
                r0 = base + i * 128
                rb0 = base + (nt - 1 - i) * 128
                kb.dma("sp", xb[b][:], src[i * 128:(i + 1) * 128, :], writes=[f"xb{b}"])
                kb.dma("sp", yf[:], Ys[0, r0:r0 + 128, :], reads=[("ysall",)], writes=["yf"])
                kb.dma("act", yb[:], Ys[1, rb0:rb0 + 128, :], reads=[("ysall",)], writes=["yb"])
                kb.dma("sp", zt[:], Zs[r0:r0 + 128, :], reads=[("ysall",)], writes=["zt"])
                for q in range(3):
                    kb.op("pe", lambda e, q=q: e.matmul(ps[pb + 1][:, :], lhsT=J32[:, :], rhs=yb[:, q * 512:(q + 1) * 512],
                                                      start=True, stop=True), reads=["J32", "yb"], writes=[f"ps{pb + 1}"])
                    ew("dve", lambda e, q=q: e.tensor_tensor(out=yf[:, q * 512:(q + 1) * 512], in0=yf[:, q * 512:(q + 1) * 512],
                                                             in1=ps[pb + 1][:, :], op=ALU.add), [f"ps{pb + 1}", "yf"], ["yf"])
                y3 = yf[:, 0:512].rearrange("p (h j) -> p h j", h=8)
                ew("dve", lambda e: e.reduce_sum(out=s8[:, 0, :], in_=y3, axis=AX.X), ["yf"], ["s8"])
                ew("dve", lambda e: e.tensor_scalar(out=s8[:, 0, :], in0=s8[:, 0, :], scalar1=-1.0 / 64, scalar2=None,
                                                    op0=ALU.mult), ["s8"], ["s8"])
                ew("dve", lambda e: e.tensor_tensor(out=y3, in0=y3, in1=s8[:, 0, :].unsqueeze(2).broadcast_to([128, 8, 64]),
                                                    op=ALU.add), ["yf", "s8"], ["yf"])
                ew("pool", lambda e: e.tensor_tensor(out=t1[:], in0=yf[:, 0:512], in1=yf[:, 0:512], op=ALU.mult), ["yf"], ["t1"])
                ew("dve", lambda e: e.reduce_sum(out=s8[:, 1, :], in_=t1[:].rearrange("p (h j) -> p h j", h=8), axis=AX.X),
                   ["t1"], ["s8"])
                ew("dve", lambda e: e.tensor_scalar(out=s8[:, 1, :], in0=s8[:, 1, :], scalar1=1.0 / 64, scalar2=64e-5,
                                                    op0=ALU.mult, op1=ALU.add), ["s8"], ["s8"])
                ew("act", lambda e: e.activation(out=s8[:, 1, :], in_=s8[:, 1, :], func=AF.Sqrt), ["s8"], ["s8"])
                ew("dve", lambda e: e.reciprocal(out=s8[:, 1, :], in_=s8[:, 1, :]), ["s8"], ["s8"])
                ew("dve", lambda e: e.tensor_tensor(out=y3, in0=y3, in1=s8[:, 1, :].unsqueeze(2).broadcast_to([128, 8, 64]),
                                                    op=ALU.mult), ["yf", "s8"], ["yf"])
                ew("dve", lambda e: e.tensor_tensor(out=yf[:, 0:512], in0=yf[:, 0:512], in1=bcp[:, 0, :], op=ALU.mult),
                   ["yf", "bcp"], ["yf"])
                ew("pool", lambda e: e.tensor_tensor(out=yf[:, 0:512], in0=yf[:, 0:512], in1=bcp[:, 1, :], op=ALU.add),
                   ["yf", "bcp"], ["yf"])
                ew("pool", lambda e: e.tensor_tensor(out=yf[:, 0:512], in0=yf[:, 0:512], in1=yf[:, 1024:1536], op=ALU.add),
                   ["yf"], ["yf"])
                cg = colbase + i * 128
                kb.op("pe", lambda e, cg=cg: e.matmul(ps[pb][:, :], lhsT=F[11][:, cg:cg + 128], rhs=g2[:, :], start=True, stop=True),
                      reads=[FK[11], "g2"], writes=[f"ps{pb}"])
                ew("dve", lambda e: e.tensor_tensor(out=o[:, 0:512], in0=yf[:, 0:512], in1=ps[pb][:, :], op=ALU.mult),
                   ["yf", f"ps{pb}"], ["o"])
                ew("pool", lambda e: e.tensor_tensor(out=t1[:], in0=yf[:, 512:1024], in1=yf[:, 512:1024], op=ALU.mult),
                   ["yf"], ["t1"])
                ew("dve", lambda e: e.reduce_sum(out=s8[:, 2, 0:4], in_=t1[:].rearrange("p (h j) -> p h j", h=4), axis=AX.X),
                   ["t1"], ["s8"])
                ew("dve", lambda e: e.tensor_scalar(out=s8[:, 2, 0:4], in0=s8[:, 2, 0:4], scalar1=1.0 / 128, scalar2=EPS,
                                                    op0=ALU.mult, op1=ALU.add), ["s8"], ["s8"])
                ew("act", lambda e: e.activation(out=s8[:, 2, 0:4], in_=s8[:, 2, 0:4], func=AF.Sqrt), ["s8"], ["s8"])
                ew("dve", lambda e: e.reciprocal(out=s8[:, 2, 0:4], in_=s8[:, 2, 0:4]), ["s8"], ["s8"])
                ew("dve", lambda e: e.tensor_tensor(
                    out=t1[:].rearrange("p (h j) -> p h j", h=4), in0=yf[:, 512:1024].rearrange("p (h j) -> p h j", h=4),
                    in1=s8[:, 2, 0:4].unsqueeze(2).broadcast_to([128, 4, 128]), op=ALU.mult), ["yf", "s8"], ["t1"])
                ew("pool", lambda e: e.tensor_tensor(out=t1[:], in0=t1[:], in1=bcp[:, 2, :], op=ALU.mult), ["t1", "bcp"], ["t1"])
                ew("dve", lambda e: e.tensor_tensor(out=o[:, 512:1024], in0=t1[:], in1=zt[:], op=ALU.mult), ["t1", "zt"], ["o"])
                ew("act", lambda e: e.copy(out=hb[:], in_=o[:]), ["o"], ["hb"])
                pst = ps[pb][:].bitcast(BF16)
                for kc in range(8):
                    kb.op("pe", lambda e, kc=kc, pst=pst: e.transpose(out=pst[:, kc * 128:(kc + 1) * 128],
                                                                    in_=hb[:, kc * 128:(kc + 1) * 128], identity=c.identb[:]),
                          reads=["hb", "c_identb"], writes=[f"ps{pb}"])
                ew("act", lambda e, pst=pst: e.copy(out=oT[:], in_=pst[:].rearrange("p (k t) -> p k t", k=8)), [f"ps{pb}"], ["oT"])
                for half in range(2):
                    pp = ps[pb + 2 + half]
                    for kc in range(8):
                        kb.op("pe", lambda e, kc=kc, half=half, pp=pp: e.matmul(
                            pp[:], lhsT=oT[:, kc, :], rhs=wo[:, kc, half * 512:(half + 1) * 512], start=(kc == 0), stop=(kc == 7)),
                            reads=["oT", "wo"], writes=[f"ps{pb + 2 + half}"])
                    sl = slice(half * 512, (half + 1) * 512)
                    ew("dve", lambda e, pp=pp, sl=sl: e.tensor_tensor(out=t1[:], in0=pp[:], in1=Gbc[:, sl], op=ALU.mult),
                       [f"ps{pb + 2 + half}", "Gbc"], ["t1"])
                    ew("pool", lambda e, sl=sl, b=b: e.tensor_tensor(out=xb[b][:, sl], in0=xb[b][:, sl], in1=t1[:], op=ALU.add),
                       ["t1", f"xb{b}"], [f"xb{b}"])
                kb.dma("sp", dst[i * 128:(i + 1) * 128, :], xb[b][:], reads=[f"xb{b}"], writes=[("dram", dst.tensor.name, i)])

            for i0 in range(0, nt, 2):
                qs = []
                for i in range(i0, min(nt, i0 + 2)):
                    dfr = Defer(i % 2)
                    tile_body(dfr, i)
                    qs.append(dfr.q)
                for k in range(max(len(q_) for q_ in qs)):
                    for q_ in qs:
                        if k < len(q_):
                            ents = q_[k][1:] if isinstance(q_[k], list) else [q_[k]]
                            for kind, a_, kw_ in ents:
                                (kb_real.op if kind == "op" else kb_real.dma)(*a_, **kw_)
        kb.barrier()


_CACHE = {}


def kernel(**inputs):
    inp = {k: np.asarray(v) for k, v in inputs.items()}
    if "nc" not in _CACHE:
        _CACHE["nc"] = build(stages=("all",))[0]
    nc = _CACHE["nc"]
    in_maps = [host_inputs(inp, b) for b in range(8)]
    res = run_bass_kernel_spmd(nc, in_maps, core_ids=list(range(8)))
    return np.stack([np.asarray(r["out"], dtype=np.float32) for r in res.results], axis=0)
```
